# Optimizing a Trainium2 kernel written in Bass

```python
import math
import jax, jax.numpy as jnp
from jax import lax
import numpy as np

D_MODEL = 1024
BATCH = 8
SEQ = 2048
DEPTH = 1

D_MIX = D_MODEL
S5_WIDTH = D_MIX // 2
S5_GROUP = 16
S5_GROUPS = S5_WIDTH // S5_GROUP
S5_STATE = 64
GDN_HEADS = 4
GDN_HEAD_DIM = 128
GDN_WIDTH = GDN_HEADS * GDN_HEAD_DIM
GDN_CONV = 5
GDN_CHUNK = 64
MEM_LEN = 256
XA_HEADS = 4
XA_HEAD_DIM = D_MODEL // XA_HEADS
MOE_GROUPS = 4
MOE_PER_GROUP = 8
MOE_EXPERTS = MOE_GROUPS * MOE_PER_GROUP
MOE_TOPK = 2
D_EXPERT = D_MODEL // 4
RMS_EPS = 1e-6
L2_EPS = 1e-6
IN_SPLITS = (S5_WIDTH, GDN_WIDTH, GDN_WIDTH, GDN_WIDTH, GDN_WIDTH, GDN_HEADS, GDN_HEADS, GDN_HEADS, GDN_HEADS)
D_IN = S5_WIDTH + 4 * GDN_WIDTH + 4 * GDN_HEADS
F32 = jnp.float32

kernel_name = 'hybrid_s5_gdn_memxattn_hiermoe_encoder'


def _rmsnorm(x, gain):
    xf = x.astype(F32)
    y = xf * lax.rsqrt(jnp.mean(xf * xf, axis=-1, keepdims=True) + RMS_EPS)
    return (y * gain.astype(F32)).astype(x.dtype)


def _l2norm(x):
    return x * lax.rsqrt(jnp.sum(x * x, axis=-1, keepdims=True) + L2_EPS)


def _split_cols(h):
    parts, off = [], 0
    for w in IN_SPLITS:
        parts.append(h[..., off:off + w])
        off += w
    return parts


def _s5_scan(u, lam_re, lam_im, log_step, b_re, b_im, c_re, c_im, reverse):
    lam = lax.complex(lam_re.astype(F32), lam_im.astype(F32))
    step = jnp.exp(log_step.astype(F32))[:, None]
    lam_bar = jnp.exp(lam * step)
    b = lax.complex(b_re.astype(F32), b_im.astype(F32))
    b_bar = ((lam_bar - 1.0) / lam)[:, :, None] * b
    bu = jnp.einsum('gpc,bsgc->bsgp', b_bar, u.astype(jnp.complex64))
    a = jnp.broadcast_to(lam_bar[None, None], (1, u.shape[1]) + lam_bar.shape)

    def combine(e1, e2):
        a1, b1 = e1
        a2, b2 = e2
        return a1 * a2, a2 * b1 + b2

    _, h = lax.associative_scan(combine, (a, bu), reverse=reverse, axis=1)
    return (jnp.einsum('bsgp,gcp->bsgc', h.real, c_re.astype(F32))
            - jnp.einsum('bsgp,gcp->bsgc', h.imag, c_im.astype(F32)))


def _s5_mixer(u, lam_re_f, lam_im_f, log_step_f, b_re_f, b_im_f, c_re_f, c_im_f,
              lam_re_b, lam_im_b, log_step_b, b_re_b, b_im_b, c_re_b, c_im_b,
              d, w_glu, b_glu, norm_w):
    bsz, s, _ = u.shape
    uf = u.astype(F32)
    ug = uf.reshape(bsz, s, S5_GROUPS, S5_GROUP)
    y = (_s5_scan(ug, lam_re_f, lam_im_f, log_step_f, b_re_f, b_im_f, c_re_f, c_im_f, False)
         + _s5_scan(ug, lam_re_b, lam_im_b, log_step_b, b_re_b, b_im_b, c_re_b, c_im_b, True))
    y = y.reshape(bsz, s, S5_WIDTH) + d.astype(F32) * uf
    g = jax.nn.gelu(y, approximate=False)
    g = g * jax.nn.sigmoid(g @ w_glu.astype(F32) + b_glu.astype(F32))
    return _rmsnorm(g, norm_w).astype(u.dtype)


def _centred_depthwise_conv(x, w):
    c = x.shape[-1]
    pad = (GDN_CONV - 1) // 2
    return lax.conv_general_dilated(x, w[:, None, :].astype(x.dtype), window_strides=(1,),
                                    padding=[(pad, pad)], dimension_numbers=('NWC', 'WIO', 'NWC'),
                                    feature_group_count=c)


def _gated_delta_chunked(q, k, v, g, beta):
    bsz, s, nh, dk = q.shape
    dv = v.shape[-1]
    n = s // GDN_CHUNK
    q = q * (dk ** -0.5)

    def chunks(t):
        return jnp.transpose(t.reshape(bsz, n, GDN_CHUNK, nh, t.shape[-1]), (0, 3, 1, 2, 4))

    qc, kc, vc = chunks(q), chunks(k), chunks(v)
    bc = chunks(beta[..., None])
    gc = jnp.cumsum(chunks(g[..., None])[..., 0], axis=-1)
    idx = jnp.arange(GDN_CHUNK)
    lower = idx[:, None] >= idx[None, :]
    strict = idx[:, None] > idx[None, :]
    diff = gc[..., :, None] - gc[..., None, :]
    decay = jnp.where(lower, jnp.exp(jnp.where(lower, diff, 0.0)), 0.0)
    k_beta = kc * bc
    v_beta = vc * bc
    l_mat = jnp.where(strict, jnp.einsum('bhncd,bhnsd->bhncs', k_beta, kc) * decay, 0.0)
    u_c = lax.linalg.triangular_solve(l_mat, v_beta, left_side=True, lower=True, unit_diagonal=True)
    w_c = lax.linalg.triangular_solve(l_mat, k_beta * jnp.exp(gc)[..., None], left_side=True,
                                      lower=True, unit_diagonal=True)
    qk = jnp.where(lower, jnp.einsum('bhncd,bhnsd->bhncs', qc, kc) * decay, 0.0)

    def step(state, inp):
        q_i, k_i, u_i, w_i, g_i, qk_i = inp
        v_new = u_i - jnp.einsum('bhcd,bhdv->bhcv', w_i, state)
        o_i = (jnp.einsum('bhcd,bhdv->bhcv', q_i * jnp.exp(g_i)[..., None], state)
               + jnp.einsum('bhcs,bhsv->bhcv', qk_i, v_new))
        g_last = g_i[..., -1]
        state = (state * jnp.exp(g_last)[..., None, None]
                 + jnp.einsum('bhcd,bhcv->bhdv', k_i * jnp.exp(g_last[..., None] - g_i)[..., None], v_new))
        return state, o_i

    xs = tuple(jnp.moveaxis(t, 2, 0) for t in (qc, kc, u_c, w_c, gc, qk))
    state0 = jnp.zeros((bsz, nh, dk, dv), F32)
    _, o = lax.scan(step, state0, xs)
    return jnp.transpose(o, (1, 0, 3, 2, 4)).reshape(bsz, s, nh, dv)


def _gdn_mixer(q, k, v, z, beta_f, beta_b, a_f, a_b, conv_w, a_log_f, dt_bias_f, a_log_b, dt_bias_b, norm_w):
    bsz, s, _ = q.shape
    qkv = jax.nn.silu(_centred_depthwise_conv(jnp.concatenate([q, k, v], axis=-1), conv_w))
    qkv = qkv.astype(F32).reshape(bsz, s, 3, GDN_HEADS, GDN_HEAD_DIM)
    qh = _l2norm(qkv[:, :, 0])
    kh = _l2norm(qkv[:, :, 1])
    vh = qkv[:, :, 2]

    def log_decay(a, a_log, dt_bias):
        return -jnp.exp(a_log.astype(F32)) * jax.nn.softplus(a.astype(F32) + dt_bias.astype(F32))

    g_f = log_decay(a_f, a_log_f, dt_bias_f)
    g_b = log_decay(a_b, a_log_b, dt_bias_b)
    bt_f = jax.nn.sigmoid(beta_f.astype(F32))
    bt_b = jax.nn.sigmoid(beta_b.astype(F32))
    flip = lambda t: jnp.flip(t, axis=1)
    o = (_gated_delta_chunked(qh, kh, vh, g_f, bt_f)
         + flip(_gated_delta_chunked(flip(qh), flip(kh), flip(vh), flip(g_b), flip(bt_b))))
    o = o * lax.rsqrt(jnp.mean(o * o, axis=-1, keepdims=True) + RMS_EPS) * norm_w.astype(F32)
    o = o * jax.nn.silu(z.astype(F32).reshape(bsz, s, GDN_HEADS, GDN_HEAD_DIM))
    return o.reshape(bsz, s, GDN_WIDTH).astype(q.dtype)


def _mem_xattn(xn, memn, wq, wk, wv, wo):
    bsz, s, _ = xn.shape
    m = memn.shape[1]
    q = (xn @ wq).reshape(bsz, s, XA_HEADS, XA_HEAD_DIM)
    k = (memn @ wk).reshape(bsz, m, XA_HEADS, XA_HEAD_DIM)
    v = (memn @ wv).reshape(bsz, m, XA_HEADS, XA_HEAD_DIM)
    sc = jnp.einsum('bshd,bmhd->bhsm', q, k).astype(F32) * (XA_HEAD_DIM ** -0.5)
    p = jax.nn.softmax(sc, axis=-1).astype(v.dtype)
    o = jnp.einsum('bhsm,bmhd->bshd', p, v).reshape(bsz, s, XA_HEADS * XA_HEAD_DIM)
    return o @ wo


def _hier_moe(xn, wg, bg, we, be, w_gate, w_up, w_down):
    bsz, s, d = xn.shape
    t = xn.reshape(bsz * s, d)
    g_logits = (t @ wg).astype(F32) + bg.astype(F32)
    p_top, g_idx = lax.top_k(jax.nn.softmax(g_logits, axis=-1), 1)
    e_logits = ((t @ we).astype(F32) + be.astype(F32)).reshape(-1, MOE_GROUPS, MOE_PER_GROUP)
    e_sel = jnp.take_along_axis(e_logits, g_idx[:, :, None], axis=1)[:, 0]
    e_val, e_idx = lax.top_k(e_sel, MOE_TOPK)
    e_w = jax.nn.softmax(e_val, axis=-1) * p_top
    within = jnp.einsum('tk,tke->te', e_w, jax.nn.one_hot(e_idx, MOE_PER_GROUP, dtype=F32))
    combine = (jax.nn.one_hot(g_idx[:, 0], MOE_GROUPS, dtype=F32)[:, :, None]
               * within[:, None, :]).astype(xn.dtype)
    out = jnp.zeros_like(t)
    for grp in range(MOE_GROUPS):
        sl = slice(grp * MOE_PER_GROUP, (grp + 1) * MOE_PER_GROUP)
        h = (jax.nn.silu(jnp.einsum('td,edf->tef', t, w_gate[sl]))
             * jnp.einsum('td,edf->tef', t, w_up[sl]))
        h = h * combine[:, grp][..., None]
        out = out + jnp.einsum('tef,efd->td', h, w_down[sl])
    return out.reshape(bsz, s, d)


def setup_inputs(seed: int = 0) -> dict:
    key = jax.random.key(seed)
    k = jax.random.split(key, 48)
    L = DEPTH

    def nrm(i, shape, scale):
        return jax.random.normal(k[i], shape, F32) * scale

    def gain(i, shape):
        return 1.0 + nrm(i, shape, 0.02)

    def uni(i, shape, lo, hi):
        return jax.random.uniform(k[i], shape, F32, minval=lo, maxval=hi)

    n_idx = jnp.arange(S5_STATE, dtype=F32)
    dt_f = jnp.exp(uni(25, (L, GDN_HEADS), math.log(1e-3), math.log(1e-1)))
    dt_b = jnp.exp(uni(27, (L, GDN_HEADS), math.log(1e-3), math.log(1e-1)))
    return {
        'x': nrm(0, (BATCH, SEQ, D_MODEL), 1.0),
        'mem': nrm(1, (BATCH, MEM_LEN, D_MODEL), 1.0),
        'norm_mix': gain(2, (L, D_MODEL)),
        'w_in': nrm(3, (L, D_MODEL, D_IN), D_MODEL ** -0.5),
        'w_out': nrm(4, (L, D_MIX, D_MODEL), D_MIX ** -0.5),
        's5_lam_re_f': -0.5 + nrm(5, (L, S5_GROUPS, S5_STATE), 0.01),
        's5_lam_im_f': math.pi * n_idx + nrm(6, (L, S5_GROUPS, S5_STATE), 0.01),
        's5_log_step_f': uni(7, (L, S5_GROUPS), math.log(1e-3), math.log(1e-1)),
        's5_b_re_f': nrm(8, (L, S5_GROUPS, S5_STATE, S5_GROUP), (2 * S5_GROUP) ** -0.5),
        's5_b_im_f': nrm(9, (L, S5_GROUPS, S5_STATE, S5_GROUP), (2 * S5_GROUP) ** -0.5),
        's5_c_re_f': nrm(10, (L, S5_GROUPS, S5_GROUP, S5_STATE), S5_STATE ** -0.5),
        's5_c_im_f': nrm(11, (L, S5_GROUPS, S5_GROUP, S5_STATE), S5_STATE ** -0.5),
        's5_lam_re_b': -0.5 + nrm(12, (L, S5_GROUPS, S5_STATE), 0.01),
        's5_lam_im_b': math.pi * n_idx + nrm(13, (L, S5_GROUPS, S5_STATE), 0.01),
        's5_log_step_b': uni(14, (L, S5_GROUPS), math.log(1e-3), math.log(1e-1)),
        's5_b_re_b': nrm(15, (L, S5_GROUPS, S5_STATE, S5_GROUP), (2 * S5_GROUP) ** -0.5),
        's5_b_im_b': nrm(16, (L, S5_GROUPS, S5_STATE, S5_GROUP), (2 * S5_GROUP) ** -0.5),
        's5_c_re_b': nrm(17, (L, S5_GROUPS, S5_GROUP, S5_STATE), S5_STATE ** -0.5),
        's5_c_im_b': nrm(18, (L, S5_GROUPS, S5_GROUP, S5_STATE), S5_STATE ** -0.5),
        's5_d': nrm(19, (L, S5_WIDTH), 1.0),
        's5_w_glu': nrm(20, (L, S5_WIDTH, S5_WIDTH), S5_WIDTH ** -0.5),
        's5_b_glu': nrm(21, (L, S5_WIDTH), 0.02),
        's5_norm': gain(22, (L, S5_WIDTH)),
        'gdn_conv': nrm(23, (L, GDN_CONV, 3 * GDN_WIDTH), GDN_CONV ** -0.5),
        'gdn_a_log_f': jnp.log(uni(24, (L, GDN_HEADS), 1.0, 16.0)),
        'gdn_dt_bias_f': dt_f + jnp.log(-jnp.expm1(-dt_f)),
        'gdn_a_log_b': jnp.log(uni(26, (L, GDN_HEADS), 1.0, 16.0)),
        'gdn_dt_bias_b': dt_b + jnp.log(-jnp.expm1(-dt_b)),
        'gdn_norm': gain(28, (L, GDN_HEAD_DIM)),
        'norm_xattn': gain(29, (L, D_MODEL)),
        'norm_mem': gain(30, (L, D_MODEL)),
        'xa_wq': nrm(31, (L, D_MODEL, XA_HEADS * XA_HEAD_DIM), D_MODEL ** -0.5),
        'xa_wk': nrm(32, (L, D_MODEL, XA_HEADS * XA_HEAD_DIM), D_MODEL ** -0.5),
        'xa_wv': nrm(33, (L, D_MODEL, XA_HEADS * XA_HEAD_DIM), D_MODEL ** -0.5),
        'xa_wo': nrm(34, (L, XA_HEADS * XA_HEAD_DIM, D_MODEL), D_MODEL ** -0.5),
        'norm_moe': gain(35, (L, D_MODEL)),
        'router_group_w': nrm(36, (L, D_MODEL, MOE_GROUPS), D_MODEL ** -0.5),
        'router_group_b': nrm(37, (L, MOE_GROUPS), 0.01),
        'router_expert_w': nrm(38, (L, D_MODEL, MOE_EXPERTS), D_MODEL ** -0.5),
        'router_expert_b': nrm(39, (L, MOE_EXPERTS), 0.01),
        'moe_w_gate': nrm(40, (L, MOE_EXPERTS, D_MODEL, D_EXPERT), D_MODEL ** -0.5),
        'moe_w_up': nrm(41, (L, MOE_EXPERTS, D_MODEL, D_EXPERT), D_MODEL ** -0.5),
        'moe_w_down': nrm(42, (L, MOE_EXPERTS, D_EXPERT, D_MODEL), D_EXPERT ** -0.5),
        'norm_final': gain(43, (D_MODEL,)),
    }


def reference(x, mem, norm_mix, w_in, w_out,
              s5_lam_re_f, s5_lam_im_f, s5_log_step_f, s5_b_re_f, s5_b_im_f, s5_c_re_f, s5_c_im_f,
              s5_lam_re_b, s5_lam_im_b, s5_log_step_b, s5_b_re_b, s5_b_im_b, s5_c_re_b, s5_c_im_b,
              s5_d, s5_w_glu, s5_b_glu, s5_norm,
              gdn_conv, gdn_a_log_f, gdn_dt_bias_f, gdn_a_log_b, gdn_dt_bias_b, gdn_norm,
              norm_xattn, norm_mem, xa_wq, xa_wk, xa_wv, xa_wo,
              norm_moe, router_group_w, router_group_b, router_expert_w, router_expert_b,
              moe_w_gate, moe_w_up, moe_w_down, norm_final):
    for l in range(DEPTH):
        h = _rmsnorm(x, norm_mix[l])
        u, q, k, v, z, beta_f, beta_b, a_f, a_b = _split_cols(h @ w_in[l])
        y_s5 = _s5_mixer(u, s5_lam_re_f[l], s5_lam_im_f[l], s5_log_step_f[l], s5_b_re_f[l], s5_b_im_f[l],
                         s5_c_re_f[l], s5_c_im_f[l], s5_lam_re_b[l], s5_lam_im_b[l], s5_log_step_b[l],
                         s5_b_re_b[l], s5_b_im_b[l], s5_c_re_b[l], s5_c_im_b[l],
                         s5_d[l], s5_w_glu[l], s5_b_glu[l], s5_norm[l])
        y_gdn = _gdn_mixer(q, k, v, z, beta_f, beta_b, a_f, a_b, gdn_conv[l], gdn_a_log_f[l], gdn_dt_bias_f[l],
                           gdn_a_log_b[l], gdn_dt_bias_b[l], gdn_norm[l])
        x = x + jnp.concatenate([y_s5, y_gdn], axis=-1) @ w_out[l]
        x = x + _mem_xattn(_rmsnorm(x, norm_xattn[l]), _rmsnorm(mem, norm_mem[l]),
                           xa_wq[l], xa_wk[l], xa_wv[l], xa_wo[l])
        x = x + _hier_moe(_rmsnorm(x, norm_moe[l]), router_group_w[l], router_group_b[l],
                          router_expert_w[l], router_expert_b[l], moe_w_gate[l], moe_w_up[l], moe_w_down[l])
    return _rmsnorm(x, norm_final)
```

```python
import contextlib
import math
import numpy as np
import ml_dtypes
import concourse.bass as bass
import concourse.mybir as mybir
from concourse.bass_utils import run_bass_kernel_spmd

F32 = mybir.dt.float32
BF16 = mybir.dt.bfloat16
F32R = mybir.dt.float32r
I32 = mybir.dt.int32
AF = mybir.ActivationFunctionType
ALU = mybir.AluOpType
AX = mybir.AxisListType

ENGS = ("sync", "scalar", "gpsimd", "vector", "tensor")
S = 2048
DM = 1024
NT = 16
EPS = 1e-6


class Trk:
    __slots__ = ("name", "w", "r", "excl")

    def __init__(self, name="", excl=False):
        self.name = name
        self.w = None
        self.r = {}
        self.excl = excl


class DmaSlot:
    def __init__(self, ctx, name):
        self.key = "d_" + name + str(ctx.nsem)
        ctx.sems[self.key] = ctx.new_sem(self.key)
        self.total = 0


class Ctx:
    def __init__(self, nc, stack):
        self.nc = nc
        self.stack = stack
        self.q = {e: [] for e in ENGS}
        self.sems = {}
        self.nsem = 0
        self.cnt = {e: 0 for e in ENGS}
        self.known = {e: {} for e in ENGS}
        for e in ENGS:
            self.sems[e] = self.new_sem("s_" + e)
        self.slots = []
        self.pool = []
        self.pool_i = 0
        self.n_ops = 0

    def new_sem(self, name):
        self.nsem += 1
        return self.stack.enter_context(self.nc.semaphore(name))

    def slot(self, name):
        s = DmaSlot(self, name)
        self.slots.append(s)
        return s

    def fresh(self):
        if self.pool_i >= len(self.pool):
            assert len(self.pool) < 48, "slot pool exhausted"
            self.pool.append(self.slot("p%d" % len(self.pool)))
        sl = self.pool[self.pool_i]
        self.pool_i += 1
        return sl

    def sb(self, name, shape, dt):
        return self.stack.enter_context(self.nc.sbuf_tensor("sb_" + name, list(shape), dt))

    def ps(self, name, shape, dt=F32):
        return self.stack.enter_context(self.nc.psum_tensor(name, list(shape), dt))

    def _waits_for(self, eng, r, w, extra=()):
        need = {}

        def req(dep):
            if dep is None:
                return
            k, c = dep
            if k == eng and eng in ("tensor", "sync"):
                return
            if c > need.get(k, 0):
                need[k] = c
        for t in r:
            req(t.w)
        for t in w:
            req(t.w)
            for k, c in t.r.items():
                req((k, c))
        for d in extra:
            req(d)
        out = []
        kn = self.known[eng]
        for k, c in need.items():
            if kn.get(k, 0) < c:
                kn[k] = c
                out.append((self.sems[k], c))
        return out

    def op(self, eng, fn, r=(), w=(), inc=True, extra=()):
        w = list(w) + [t for t in r if t.excl]
        r = [t for t in r if not t.excl]
        waits = self._waits_for(eng, r, w, extra)
        c = self.cnt[eng] + 1
        if inc:
            self.cnt[eng] = c
        sem = self.sems[eng]

        def emit(h, fn=fn, waits=waits, inc=inc, sem=sem):
            for s, v in waits:
                h.wait_ge(s, v)
            ins = fn(h)
            if inc:
                ins.then_inc(sem, 1)
        self.q[eng].append(emit)
        for t in r:
            t.r[eng] = c
        for t in w:
            t.w = (eng, c)
            t.r = {}
        self.n_ops += 1

    def dma(self, eng, out, in_, slot, r=(), w=(), extra=(), **kw):
        waits = self._waits_for(eng, r, w, extra)
        slot.total += 16
        sem = self.sems[slot.key]

        def emit(h, waits=waits, sem=sem, out=out, in_=in_, kw=kw):
            for s, v in waits:
                h.wait_ge(s, v)
            h.dma_start(out=out, in_=in_, **kw).then_inc(sem, 16)
        self.q[eng].append(emit)
        dep = (slot.key, slot.total)
        for t in r:
            t.r[slot.key] = slot.total
        for t in w:
            t.w = dep
            t.r = {}
        self.n_ops += 1
        return dep

    def wait_deps(self, eng, deps):
        waits = self._waits_for(eng, (), (), deps)

        def emit(h, waits=waits):
            for s, v in waits:
                h.wait_ge(s, v)
        self.q[eng].append(emit)

    def barrier(self):
        deps = [(e, self.cnt[e]) for e in ENGS if e != "sync" and self.cnt[e] > 0]
        deps += [(s.key, s.total) for s in self.slots if s.total > 0]
        for e in ENGS:
            self.wait_deps(e, deps)
        self.pool_i = 0

    def emit_all(self, block):
        q = self.q

        @block.sync
        def _(h):
            for f in q["sync"]:
                f(h)

        @block.scalar
        def _(h):
            for f in q["scalar"]:
                f(h)

        @block.gpsimd
        def _(h):
            for f in q["gpsimd"]:
                f(h)

        @block.vector
        def _(h):
            for f in q["vector"]:
                f(h)

        @block.tensor
        def _(h):
            for f in q["tensor"]:
                f(h)


class Arena:
    def __init__(self, cx, words, base=None):
        self.t = cx.sb("arena", [128, words], F32) if base is None else base
        self.cx = cx
        self.words = words
        self.top = 0

    def mark(self):
        return self.top

    def release(self, m):
        if m != self.top:
            self.cx.barrier()
        self.top = m

    def alloc(self, shape, dt):
        n = int(np.prod(shape))
        w = n if dt in (F32, F32R, I32) else (n + 1) // 2
        w = (w + 1) // 2 * 2
        o = self.top
        self.top += w
        assert self.top <= self.words, ("arena overflow", self.top, self.words)
        v = self.t[:, o:o + w]
        if dt != F32:
            v = v.bitcast(dt)
        v = v[:, 0:n]
        if len(shape) > 1:
            names = " ".join("d%d" % i for i in range(len(shape)))
            v = v.rearrange("p (%s) -> p %s" % (names, names), **{"d%d" % i: shape[i] for i in range(len(shape))})
        return v


def pap(ap, part0, nparts, off, dims):
    base = ap.ap[0][0]
    return bass.AP(ap.tensor, ap.offset + part0 * base + off, [[base, nparts]] + [list(d) for d in dims])


def host_consts():
    c = {}
    c["ident"] = np.eye(128, dtype=np.float32)
    c["identb"] = np.eye(128, dtype=np.float32).astype(ml_dtypes.bfloat16)
    c["ones"] = np.ones((128, 128), np.float32)
    selT = np.zeros((128, 2, 8, 128), np.float32)
    selB = np.zeros((128, 2, 8, 128), np.float32)
    for q in range(4):
        for r in range(32):
            loc, cc = r // 16, r % 16
            for s in range(8):
                selT[q * 32 + r, loc, s, s * 16 + cc] = 1.0
                selB[q * 32 + r, loc, s, s * 16 + cc] = 1.0
    c["selT"] = selT.astype(ml_dtypes.bfloat16)
    c["selB"] = selB.astype(ml_dtypes.bfloat16)
    sidx = np.arange(128) // 16
    c["s5mf"] = (sidx[None, :] >= sidx[:, None]).astype(np.float32)
    c["s5mb"] = (sidx[None, :] <= sidx[:, None]).astype(np.float32)
    c["kvec"] = np.tile((np.arange(16, dtype=np.float32) - 7.0)[None, :], (128, 1))
    k = np.arange(128)[:, None]
    cc = np.arange(128)[None, :]
    same = (k // 64) == (cc // 64)
    gm = np.zeros((128, 8, 128), np.float32)
    gm[:, 0] = same & (k <= cc)
    gm[:, 1] = same & (k >= cc)
    gm[:, 2] = np.where(same & (cc >= k), 0.0, -30000.0)
    gm[:, 3] = np.where(same & (cc <= k), 0.0, -30000.0)
    gm[:, 4] = same & (cc > k)
    gm[:, 5] = same & (cc < k)
    gm[:, 6] = same
    c["gmask"] = gm
    return c


def host_s5(inp):
    o = {}

    pairs = {"lam_re": ("s5_lam_re_f", "s5_lam_re_b"), "lam_im": ("s5_lam_im_f", "s5_lam_im_b"),
             "log_step": ("s5_log_step_f", "s5_log_step_b"), "b_re": ("s5_b_re_f", "s5_b_re_b"),
             "b_im": ("s5_b_im_f", "s5_b_im_b"), "c_re": ("s5_c_re_f", "s5_c_re_b"), "c_im": ("s5_c_im_f", "s5_c_im_b")}

    def st(nm):
        f_, b_ = pairs[nm]
        return np.stack([inp[f_][0], inp[b_][0]], 0)
    lam = np.stack([st("lam_re"), st("lam_im")], 0)
    lam = lam.reshape(2, 2, 2, 16, 64).transpose(2, 4, 0, 1, 3)
    o["s5_lam"] = np.ascontiguousarray(lam.reshape(128, 2, 32))
    ls = st("log_step").reshape(2, 2, 16)
    ls = np.broadcast_to(ls.transpose(1, 0, 2)[:, None], (2, 64, 2, 16))
    o["s5_step"] = np.ascontiguousarray(ls.reshape(128, 32))
    b = np.stack([st("b_re"), st("b_im")], 0)
    b = b.reshape(2, 2, 2, 16, 64, 16).transpose(2, 4, 0, 1, 3, 5)
    o["s5_b"] = np.ascontiguousarray(b.reshape(128, 2, 512))
    cm = np.stack([st("c_re"), st("c_im")], 0)
    cm = cm.reshape(2, 2, 2, 16, 16, 64).transpose(2, 5, 0, 1, 3, 4)
    o["s5_c"] = np.ascontiguousarray(cm.reshape(128, 2, 512))
    d = inp["s5_d"][0].reshape(32, 16)
    o["s5_dvec"] = np.ascontiguousarray(np.broadcast_to(d.T[None], (8, 16, 32)).reshape(128, 32))
    o["s5_bglu"] = np.ascontiguousarray(inp["s5_b_glu"][0].reshape(4, 128).T)
    o["s5_normw"] = np.ascontiguousarray(inp["s5_norm"][0].reshape(4, 128).T)
    return o


def np2dt(a):
    if a.dtype == np.float32:
        return F32
    if a.dtype == ml_dtypes.bfloat16:
        return BF16
    raise ValueError(a.dtype)


class K:
    pass


def dram_bcast(ap, nparts, n, off=0):
    return bass.AP(ap.tensor, ap.offset + off, [[0, nparts], [1, n]])


def norm_transpose(k, name, src_fn, ntiles, gain_dram, outT, out_dt, outT_trk):
    cx, ar = k.cx, k.ar
    m = ar.mark()
    gB = ar.alloc([1024], F32)
    tg = Trk()
    cx.dma("sync", gB, dram_bcast(gain_dram, 128, 1024), cx.fresh(), w=[tg])
    junk = ar.alloc([1024], BF16)
    tj = Trk()
    xn = [ar.alloc([1024], out_dt) for _ in range(2)]
    txn = [Trk(), Trk()]
    ss = ar.alloc([NT * 2, 1], F32)
    tss = [Trk() for _ in range(ntiles)]
    pdt = BF16 if out_dt == BF16 else F32
    ident = k.identb if out_dt == BF16 else k.ident
    for i in range(ntiles):
        src, ts = src_fn(i)
        ssi = ss[:, 2 * i:2 * i + 1]
        rsi = ss[:, 2 * i + 1:2 * i + 2]
        cx.op("scalar", lambda h, src=src, ssi=ssi: h.activation(junk, src, AF.Square, accum_out=ssi), r=[ts], w=[tj, tss[i]])
        cx.op("vector", lambda h, ssi=ssi, rsi=rsi: h.tensor_scalar(rsi, ssi, 1.0 / 1024, EPS, op0=ALU.mult, op1=ALU.add), r=[tss[i]], w=[tss[i]])
        cx.op("scalar", lambda h, rsi=rsi: h.activation(rsi, rsi, AF.Sqrt), r=[tss[i]], w=[tss[i]])
        cx.op("vector", lambda h, rsi=rsi: h.reciprocal(rsi, rsi), r=[tss[i]], w=[tss[i]])
        b = i % 2
        cx.op("vector", lambda h, src=src, rsi=rsi, b=b: h.scalar_tensor_tensor(out=xn[b], in0=src, scalar=rsi, in1=gB, op0=ALU.mult, op1=ALU.mult),
              r=[ts, tss[i], tg], w=[txn[b]])
        if out_dt == BF16:
            pb = k.psum[i % 2]
            tp = k.tpsum[i % 2]
            pv = pb.bitcast(BF16)
            for c in range(8):
                cx.op("tensor", lambda h, b=b, c=c, pv=pv: h.transpose(pv[:, c * 128:(c + 1) * 128], xn[b][:, c * 128:(c + 1) * 128], ident),
                      r=[txn[b]], w=[tp], inc=(c == 7))
            dst = outT[:, :, i * 128:(i + 1) * 128]
            eng = "scalar" if i % 2 == 0 else "vector"
            if eng == "scalar":
                cx.op(eng, lambda h, dst=dst, pv=pv: h.copy(dst, pv.rearrange("p (c t) -> p c t", c=8)), r=[tp], w=[outT_trk])
            else:
                cx.op(eng, lambda h, dst=dst, pv=pv: h.tensor_copy(dst, pv.rearrange("p (c t) -> p c t", c=8)), r=[tp], w=[outT_trk])
        else:
            for half in range(2):
                pb = k.psum[(2 * i + half) % 4]
                tp = k.tpsum[(2 * i + half) % 4]
                for c4 in range(4):
                    c = half * 4 + c4
                    cx.op("tensor", lambda h, b=b, c=c, c4=c4, pb=pb: h.transpose(pb[:, c4 * 128:(c4 + 1) * 128], xn[b][:, c * 128:(c + 1) * 128].bitcast(F32), ident),
                          r=[txn[b]], w=[tp], inc=(c4 == 3))
                dst = outT[:, half * 4:(half + 1) * 4, i * 128:(i + 1) * 128]
                if half == 0:
                    cx.op("scalar", lambda h, dst=dst, pb=pb: h.copy(dst, pb.rearrange("p (c t) -> p c t", c=4)), r=[tp], w=[outT_trk])
                else:
                    cx.op("vector", lambda h, dst=dst, pb=pb: h.tensor_copy(dst, pb.rearrange("p (c t) -> p c t", c=4)), r=[tp], w=[outT_trk])
    ar.release(m)


def s5_prep(k):
    cx, ar, D = k.cx, k.ar, k.D
    V = "vector"
    m0 = ar.mark()
    lam = ar.alloc([2, 32], F32)
    step = ar.alloc([32], F32)
    bb = ar.alloc([2, 512], F32)
    cc = ar.alloc([2, 512], F32)
    kvec = ar.alloc([16], F32)
    tl = Trk()
    sl_ = cx.fresh()
    for dst, nm in ((lam, "s5_lam"), (step, "s5_step"), (bb, "s5_b"), (cc, "s5_c"), (kvec, "kvec")):
        cx.dma("sync", dst, D[nm], sl_, w=[tl])
    T = Trk()

    def vop(fn, extra_r=()):
        cx.op(V, fn, r=[T, tl] + list(extra_r), w=[T])

    def aop(fn):
        cx.op("scalar", fn, r=[T, tl], w=[T])
    lre, lim = lam[:, 0, :], lam[:, 1, :]
    dl = ar.alloc([32], F32)
    re1 = ar.alloc([32], F32)
    im1 = ar.alloc([32], F32)
    aop(lambda h: h.activation(dl, step, AF.Exp))
    vop(lambda h: h.tensor_tensor(out=re1, in0=dl, in1=lre, op=ALU.mult))
    vop(lambda h: h.tensor_tensor(out=im1, in0=dl, in1=lim, op=ALU.mult))
    PWI = ar.alloc([16, 32], F32)
    PWR = ar.alloc([16, 32], F32)
    m_pw = ar.mark()
    KR = ar.alloc([16, 32], F32)
    KI = ar.alloc([16, 32], F32)
    kv_b = kvec.unsqueeze(2).to_broadcast([128, 16, 32])
    vop(lambda h: h.tensor_tensor(out=KR, in0=kv_b, in1=re1.unsqueeze(1).to_broadcast([128, 16, 32]), op=ALU.mult))
    vop(lambda h: h.tensor_tensor(out=KI, in0=kv_b, in1=im1.unsqueeze(1).to_broadcast([128, 16, 32]), op=ALU.mult))
    MAG = ar.alloc([16, 32], F32)
    aop(lambda h: h.activation(MAG, KR, AF.Exp))
    YI = ar.alloc([16, 32], I32)
    YF = ar.alloc([16, 32], F32)
    vop(lambda h: h.tensor_scalar(KI, KI, 1.0 / (2 * math.pi), None, op0=ALU.mult))
    vop(lambda h: h.tensor_copy(YI, KI))
    vop(lambda h: h.tensor_copy(YF, YI))
    vop(lambda h: h.tensor_tensor(out=KI, in0=KI, in1=YF, op=ALU.subtract))
    SH_ = ar.alloc([16, 32], F32)
    SQ_ = ar.alloc([16, 32], F32)
    aop(lambda h: h.activation(SH_, KI, AF.Sin, scale=math.pi))
    aop(lambda h: h.activation(SQ_, KI, AF.Sin, scale=math.pi / 2))
    CH_ = ar.alloc([16, 32], F32)
    vop(lambda h: h.tensor_tensor(out=CH_, in0=SQ_, in1=SQ_, op=ALU.mult))
    vop(lambda h: h.tensor_scalar(CH_, CH_, -2.0, 1.0, op0=ALU.mult, op1=ALU.add))
    vop(lambda h: h.tensor_tensor(out=PWI, in0=SH_, in1=CH_, op=ALU.mult))
    vop(lambda h: h.scalar_tensor_tensor(out=PWI, in0=PWI, scalar=2.0, in1=MAG, op0=ALU.mult, op1=ALU.mult))
    vop(lambda h: h.tensor_tensor(out=PWR, in0=SH_, in1=SH_, op=ALU.mult))
    vop(lambda h: h.tensor_scalar(PWR, PWR, -2.0, 1.0, op0=ALU.mult, op1=ALU.add))
    vop(lambda h: h.tensor_tensor(out=PWR, in0=PWR, in1=MAG, op=ALU.mult))
    ar.release(m_pw)
    lrm1 = ar.alloc([32], F32)
    li = PWI[:, 8, :]
    t1 = ar.alloc([32], F32)
    t2 = ar.alloc([32], F32)
    den = ar.alloc([32], F32)
    c0r = ar.alloc([32], F32)
    c0i = ar.alloc([32], F32)
    vop(lambda h: h.tensor_scalar(lrm1, PWR[:, 8, :], -1.0, None, op0=ALU.add))
    vop(lambda h: h.tensor_tensor(out=t1, in0=lre, in1=lre, op=ALU.mult))
    vop(lambda h: h.tensor_tensor(out=t2, in0=lim, in1=lim, op=ALU.mult))
    vop(lambda h: h.tensor_tensor(out=den, in0=t1, in1=t2, op=ALU.add))
    vop(lambda h: h.reciprocal(den, den))
    vop(lambda h: h.tensor_tensor(out=t1, in0=lrm1, in1=lre, op=ALU.mult))
    vop(lambda h: h.tensor_tensor(out=t2, in0=li, in1=lim, op=ALU.mult))
    vop(lambda h: h.tensor_tensor(out=t1, in0=t1, in1=t2, op=ALU.add))
    vop(lambda h: h.tensor_tensor(out=c0r, in0=t1, in1=den, op=ALU.mult))
    vop(lambda h: h.tensor_tensor(out=t1, in0=li, in1=lre, op=ALU.mult))
    vop(lambda h: h.tensor_tensor(out=t2, in0=lrm1, in1=lim, op=ALU.mult))
    vop(lambda h: h.tensor_tensor(out=t1, in0=t1, in1=t2, op=ALU.subtract))
    vop(lambda h: h.tensor_tensor(out=c0i, in0=t1, in1=den, op=ALU.mult))
    BBR = ar.alloc([32, 16], F32)
    BBI = ar.alloc([32, 16], F32)
    TA = ar.alloc([32, 16], F32)
    br = bb[:, 0, :].rearrange("p (a c) -> p a c", c=16)
    bi = bb[:, 1, :].rearrange("p (a c) -> p a c", c=16)
    c0r_b = c0r.unsqueeze(2).to_broadcast([128, 32, 16])
    c0i_b = c0i.unsqueeze(2).to_broadcast([128, 32, 16])
    vop(lambda h: h.tensor_tensor(out=BBR, in0=br, in1=c0r_b, op=ALU.mult))
    vop(lambda h: h.tensor_tensor(out=TA, in0=bi, in1=c0i_b, op=ALU.mult))
    vop(lambda h: h.tensor_tensor(out=BBR, in0=BBR, in1=TA, op=ALU.subtract))
    vop(lambda h: h.tensor_tensor(out=BBI, in0=bi, in1=c0r_b, op=ALU.mult))
    vop(lambda h: h.tensor_tensor(out=TA, in0=br, in1=c0i_b, op=ALU.mult))
    vop(lambda h: h.tensor_tensor(out=BBI, in0=BBI, in1=TA, op=ALU.add))
    ASd = ar.alloc([16, 2, 8, 16], F32)
    CS2d = ar.alloc([16, 2, 8, 16], F32)
    T1 = ar.alloc([8, 16, 16], F32)
    T2 = ar.alloc([8, 16, 16], F32)
    cr = cc[:, 0, :].rearrange("p (d a c) -> p d a c", d=2, c=16)
    ci = cc[:, 1, :].rearrange("p (d a c) -> p d a c", d=2, c=16)
    BBR4 = BBR.rearrange("p (d a) c -> p d a c", d=2)
    BBI4 = BBI.rearrange("p (d a) c -> p d a c", d=2)

    def pw(arr, d, k0, kstep):
        return pap(arr, 0, 128, k0 * 32 + d * 16, [[kstep * 32, 8], [1, 16], [0, 16]])

    def dst(arr, dofs, ri):
        return pap(arr, 0, 128, dofs * 4096 + ri * 128, [[16, 8], [256, 16], [1, 16]])

    def vec(v4, d):
        a_ = v4[:, d]
        return bass.AP(a_.tensor, a_.offset, [list(a_.ap[0]), [0, 8], list(a_.ap[1]), list(a_.ap[2])])

    T1f = T1.rearrange("p a b c -> p (a b c)")

    def cmul(out_arr, dofs, d, k0, kstep, vr, vi, neg_im):
        pr, pi_ = pw(PWR, d, k0, kstep), pw(PWI, d, k0, kstep)
        vop(lambda h: h.tensor_tensor(out=T1, in0=pr, in1=vec(vr, d), op=ALU.mult))
        vop(lambda h: h.tensor_tensor(out=T2, in0=pi_, in1=vec(vi, d), op=ALU.mult))
        vop(lambda h: h.tensor_tensor(out=dst(out_arr, dofs, 0), in0=T1, in1=T2, op=ALU.subtract))
        vop(lambda h: h.tensor_tensor(out=T1, in0=pr, in1=vec(vi, d), op=ALU.mult))
        vop(lambda h: h.tensor_tensor(out=T2, in0=pi_, in1=vec(vr, d), op=ALU.mult))
        if neg_im:
            vop(lambda h: h.tensor_scalar(T1f, T1f, -1.0, None, op0=ALU.mult))
            vop(lambda h: h.tensor_tensor(out=dst(out_arr, dofs, 1), in0=T1, in1=T2, op=ALU.subtract))
        else:
            vop(lambda h: h.tensor_tensor(out=dst(out_arr, dofs, 1), in0=T1, in1=T2, op=ALU.add))
    vop(lambda h: h.tensor_copy(k.s5A1[:, 0:32], PWR[:, 15, :]))
    vop(lambda h: h.tensor_copy(k.s5A1[:, 32:64], PWR[:, 15, :]))
    vop(lambda h: h.tensor_scalar(k.s5A2[:, 0:32], PWI[:, 15, :], -1.0, None, op0=ALU.mult))
    vop(lambda h: h.tensor_copy(k.s5A2[:, 32:64], PWI[:, 15, :]))
    cmul(k.s5CS, 0, 0, 8, 1, cr, ci, True)
    cmul(k.s5CS, 1, 1, 15, -1, cr, ci, True)
    k.t_s5w = T
    mf = ar.alloc([2, 128], F32)
    dv = ar.alloc([32], F32)
    tm = Trk()
    sl_ = cx.fresh()
    cx.dma("sync", mf[:, 0, :], D["s5mf"], sl_, w=[tm])
    cx.dma("sync", mf[:, 1, :], D["s5mb"], sl_, w=[tm])
    cx.dma("sync", dv, D["s5_dvec"], sl_, w=[tm])
    tt1 = [ar.alloc([128], F32) for _ in range(2)]
    ttt = [Trk(), Trk()]
    ASb = ASd.rearrange("p a r s c -> p (a r) (s c)")
    ASm = ASd.rearrange("p a r s c -> p a r (s c)")
    CSm = CS2d.rearrange("p a r s c -> p a r (s c)")
    for d in range(2):
        if d == 0:
            cmul(ASd, 0, 0, 14, -1, BBR4, BBI4, False)
            cmul(CS2d, 0, 0, 0, 1, cr, ci, True)
        else:
            cmul(ASd, 0, 1, 7, 1, BBR4, BBI4, False)
            cmul(CS2d, 0, 1, 7, -1, cr, ci, True)
        for grp in range(8):
            pb = k.psum[grp % 4]
            tp = k.tpsum[grp % 4]
            for j in range(4):
                blk = grp * 4 + j
                cx.op("tensor", lambda h, pb=pb, j=j, blk=blk: h.transpose(pb[:, j * 128:(j + 1) * 128], ASb[:, blk, :], k.ident),
                      r=[T], w=[tp], inc=(j == 3))
            dstv = k.s5AT[:, d * 32 + grp * 4:d * 32 + (grp + 1) * 4, :]
            if grp % 2 == 0:
                cx.op("scalar", lambda h, dstv=dstv, pb=pb: h.copy(dstv, pb.rearrange("p (j x) -> p j x", j=4)), r=[tp], w=[k.t_s5at])
            else:
                cx.op("vector", lambda h, dstv=dstv, pb=pb: h.tensor_copy(dstv, pb.rearrange("p (j x) -> p j x", j=4)), r=[tp], w=[k.t_s5at])
        for g in range(32):
            gh, gl = g // 16, g % 16
            pb = k.psum[4 + g % 4]
            tp = k.tpsum[4 + g % 4]
            for ri in range(2):
                cx.op("tensor", lambda h, pb=pb, ri=ri, gh=gh, gl=gl: h.matmul(
                    pb[:, 0:128], ASm[gh * 64:(gh + 1) * 64, gl, ri, :], CSm[gh * 64:(gh + 1) * 64, gl, ri, :],
                    start=(ri == 0), stop=(ri == 1)), r=[T], w=[tp], inc=(ri == 1))
            b_ = g % 2
            cx.op(V, lambda h, pb=pb, b_=b_, d=d: h.tensor_tensor(out=tt1[b_], in0=pb[:, 0:128], in1=mf[:, d, :], op=ALU.mult), r=[tp, tm], w=[ttt[b_]])
            if d == 0:
                cx.op(V, lambda h, b_=b_, g=g: h.scalar_tensor_tensor(out=k.s5TT[:, g, :], in0=k.ident, scalar=dv[:, g:g + 1], in1=tt1[b_], op0=ALU.mult, op1=ALU.add),
                      r=[ttt[b_], tm], w=[k.t_s5tt])
            else:
                cx.op(V, lambda h, b_=b_, g=g: h.tensor_tensor(out=k.s5TT[:, g, :], in0=k.s5TT[:, g, :], in1=tt1[b_], op=ALU.add),
                      r=[ttt[b_]], w=[k.t_s5tt])
    cx.barrier()
    ar.release(m0)


def load_w_cols(k, wdram, col0, ncols, dst, trk, slot, eng="gpsimd"):
    src = bass.AP(wdram.tensor, wdram.offset + col0, [[wdram.ap[0][0] * 1, 128], [wdram.ap[0][0] * 128, 8], [1, ncols]])
    return k.cx.dma(eng, dst, src, slot, w=[trk])


def proj_fm(k, wt, wtrk, consume):
    cx = k.cx
    for n in range(4):
        pb = k.psum[n % 2 + 2]
        tp = k.tpsum[n % 2 + 2]
        for c in range(8):
            cx.op("tensor", lambda h, pb=pb, c=c, n=n: h.matmul(pb[:, :], wt[:, c, :], k.hT[:, c, n * 512:(n + 1) * 512], start=(c == 0), stop=(c == 7)),
                  r=[wtrk, k.t_hT], w=[tp], inc=(c == 7))
        consume(n, pb, tp)


def s5_build_U(k):
    cx, ar = k.cx, k.ar
    m0 = ar.mark()
    wt = [ar.alloc([8, 128], BF16) for _ in range(2)]
    twt = [Trk(), Trk()]
    swt = [cx.fresh(), cx.fresh()]
    uT = [ar.alloc([2048], BF16) for _ in range(2)]
    tuT = [Trk(), Trk()]
    for ct in range(4):
        b = ct % 2
        load_w_cols(k, k.D["w_in"], ct * 128, 128, wt[b], twt[b], swt[b])

        def consume(n, pb, tp, b=b):
            if n % 2 == 0:
                cx.op("scalar", lambda h: h.copy(uT[b][:, n * 512:(n + 1) * 512], pb[:, :]), r=[tp], w=[tuT[b]])
            else:
                cx.op("vector", lambda h: h.tensor_copy(uT[b][:, n * 512:(n + 1) * 512], pb[:, :]), r=[tp], w=[tuT[b]])
        proj_fm(k, wt[b], twt[b], consume)
        for gi in range(8):
            g = ct * 8 + gi
            q0 = 32 * (gi // 2)
            pb = k.psum[4 + gi % 4]
            tp = k.tpsum[4 + gi % 4]
            for s in range(8):
                rhs = pap(uT[b], q0, 32, s, [[8, 256]])
                cx.op("tensor", lambda h, pb=pb, s=s, rhs=rhs, q0=q0, gi=gi: h.matmul(pb[:, 0:256], k.selT[q0:q0 + 32, gi % 2, s, :], rhs, start=(s == 0), stop=(s == 7), tile_position=(q0, 0)),
                      r=[tuT[b]], w=[tp], inc=(s == 7))
            if gi % 2 == 0:
                cx.op("scalar", lambda h, pb=pb, g=g: h.copy(k.s5U[:, g, :], pb[:, 0:256]), r=[tp], w=[k.t_s5U])
            else:
                cx.op("vector", lambda h, pb=pb, g=g: h.tensor_copy(k.s5U[:, g, :], pb[:, 0:256]), r=[tp], w=[k.t_s5U])
    cx.barrier()
    ar.release(m0)


def s5_main(k, yT, t_yT):
    cx, ar, D = k.cx, k.ar, k.D
    V = "vector"
    m0 = ar.mark()
    SH = ar.alloc([2, 257, 2, 16], BF16)
    tSH = Trk()
    tSHh = Trk()
    X = [ar.alloc([64], F32) for _ in range(3)]
    tX = [Trk() for _ in range(3)]
    t1 = ar.alloc([64], F32)
    t2 = ar.alloc([64], F32)
    tt = Trk()
    cx.op("gpsimd", lambda h: h.memset(SH[:, 0, 0, :, :], 0.0), w=[tSH])
    cx.op("gpsimd", lambda h: h.memset(SH[:, 1, 256, :, :], 0.0), w=[tSH])
    cx.op("gpsimd", lambda h: h.memset(X[0], 0.0), w=[tX[0]])
    n = 0
    for gl in range(16):
        for d in range(2):
            for ri in range(2):
                blk = d * 32 + gl * 2 + ri
                pb = k.psum[n % 4]
                tp = k.tpsum[n % 4]
                cx.op("tensor", lambda h, pb=pb, blk=blk, gl=gl: h.matmul(pb[0:64, 0:256], k.s5AT[:, blk, 0:64], k.s5U[:, gl, :], start=True, stop=True),
                      r=[k.t_s5at, k.t_s5U], w=[tp], inc=False)
                cx.op("tensor", lambda h, pb=pb, blk=blk, gl=gl: h.matmul(pb[64:128, 0:256], k.s5AT[:, blk, 64:128], k.s5U[:, 16 + gl, :], start=True, stop=True),
                      r=[k.t_s5at, k.t_s5U], w=[tp])
                slot0 = 1 if d == 0 else 0
                dstv = pap(SH, 0, 128, d * 257 * 32 + slot0 * 32 + ri * 16 + gl, [[32, 256]])
                if n % 2 == 0:
                    cx.op("scalar", lambda h, dstv=dstv, pb=pb: h.copy(dstv, pb[:, 0:256]), r=[tp], w=[tSH])
                else:
                    cx.op(V, lambda h, dstv=dstv, pb=pb: h.tensor_copy(dstv, pb[:, 0:256]), r=[tp], w=[tSH])
                n += 1
    import os
    S5STOP = os.environ.get('S5_STOP', '')
    if S5STOP == 'a':
        cx.barrier(); ar.release(m0); return
    for i in range(256):
        xp, xn = X[i % 3], X[(i + 1) % 3]
        txp, txn = tX[i % 3], tX[(i + 1) % 3]
        xsw = pap(xp, 0, 128, 32, [[-32, 2], [1, 32]])
        bf = (i + 1) * 32
        bb_ = 257 * 32 + (255 - i) * 32
        sview = pap(SH, 0, 128, bf, [[16, 2], [bb_ - bf, 2], [1, 16]])
        xp3 = xp.rearrange("p (r x) -> p r x", r=2)
        cx.op(V, lambda h, xp=xp: h.tensor_tensor(out=t1, in0=k.s5A1, in1=xp, op=ALU.mult), r=[txp, k.t_s5w], w=[tt])
        cx.op(V, lambda h, xsw=xsw: h.tensor_tensor(out=t2.rearrange("p (r x) -> p r x", r=2), in0=k.s5A2.rearrange("p (r x) -> p r x", r=2), in1=xsw, op=ALU.mult), r=[txp, k.t_s5w], w=[tt])
        cx.op(V, lambda h: h.tensor_tensor(out=t1, in0=t1, in1=t2, op=ALU.add), r=[tt], w=[tt])
        cx.op(V, lambda h, xn=xn, sview=sview: h.tensor_tensor(out=xn.rearrange("p (r d x) -> p r d x", r=2, d=2), in0=t1.rearrange("p (r d x) -> p r d x", r=2, d=2), in1=sview, op=ALU.add),
              r=[tt, tSH], w=[txn])
        cx.op("scalar", lambda h, xn=xn, sview=sview: h.copy(sview, xn.rearrange("p (r d x) -> p r d x", r=2, d=2)), r=[txn], w=[tSHh])
    if S5STOP == 'rec':
        cx.barrier(); ar.release(m0); return
    gT = ar.alloc([4, 2048], F32)
    gTb = ar.alloc([4, 2048], BF16)
    tgT = [Trk() for _ in range(4)]
    tgTb = [Trk() for _ in range(4)]
    ybuf = [k.arA.alloc([8, 256], BF16) for _ in range(2)]
    tyb = [Trk(), Trk()]
    for ct in range(4):
        b = ct % 2
        for gi in range(8):
            g = ct * 8 + gi
            gh, gl = g // 16, g % 16
            pb = k.psum[gi % 2]
            tp = k.tpsum[gi % 2]
            cx.op("tensor", lambda h, pb=pb, g=g: h.matmul(pb[:, 0:256], k.s5TT[:, g, :], k.s5U[:, g, :], start=True, stop=False),
                  r=[k.t_s5tt, k.t_s5U], w=[tp], inc=False)
            for d in range(2):
                for ri in range(2):
                    slot0 = 0 if d == 0 else 1
                    rhs = pap(SH, gh * 64, 64, d * 257 * 32 + slot0 * 32 + ri * 16 + gl, [[32, 256]])
                    last = (d == 1 and ri == 1)
                    cx.op("tensor", lambda h, pb=pb, rhs=rhs, d=d, ri=ri, gh=gh, gl=gl, last=last: h.matmul(
                        pb[:, 0:256], k.s5CS[gh * 64:(gh + 1) * 64, d, gl, ri, :], rhs, start=False, stop=last),
                        r=[tSH, tSHh, k.t_s5w], w=[tp], inc=last)
            if gi % 2 == 0:
                cx.op("scalar", lambda h, pb=pb, b=b, gi=gi: h.copy(ybuf[b][:, gi, :], pb[:, 0:256]), r=[tp], w=[tyb[b]])
            else:
                cx.op(V, lambda h, pb=pb, b=b, gi=gi: h.tensor_copy(ybuf[b][:, gi, :], pb[:, 0:256]), r=[tp], w=[tyb[b]])
        for t in range(8):
            q0 = 32 * (t // 2)
            pb = k.psum[2 + t % 4]
            tp = k.tpsum[2 + t % 4]
            for gi in range(8):
                cx.op("tensor", lambda h, pb=pb, t=t, gi=gi, q0=q0, b=b: h.matmul(pb[:, 0:256], k.selT[q0:q0 + 32, t % 2, gi, :], ybuf[b][q0:q0 + 32, gi, :], start=(gi == 0), stop=(gi == 7), tile_position=(q0, 0)),
                      r=[tyb[b]], w=[tp], inc=(gi == 7))
            dstv = pap(gT, 0, 128, ct * 2048 + t, [[8, 256]])
            cx.op("scalar", lambda h, pb=pb, dstv=dstv: h.activation(dstv, pb[:, 0:256], AF.Gelu), r=[tp], w=[tgT[ct]])
        cx.op("vector", lambda h, ct=ct: h.tensor_copy(gTb[:, ct, :], gT[:, ct, :]), r=[tgT[ct]], w=[tgTb[ct]])
    k.dbg_add("s5_g", gT, tgT)
    if S5STOP == 'c':
        cx.barrier(); ar.release(m0); return
    wg = ar.alloc([4, 512], BF16)
    twg = Trk()
    wsrc = D["s5_w_glu"]
    cx.dma("gpsimd", wg, bass.AP(wsrc.tensor, wsrc.offset, [[512, 128], [512 * 128, 4], [1, 512]]), cx.fresh(), w=[twg])
    bgl = ar.alloc([4], F32)
    nw = ar.alloc([4], F32)
    tb = Trk()
    sl_ = cx.fresh()
    cx.dma("sync", bgl, D["s5_bglu"], sl_, w=[tb])
    cx.dma("sync", nw, D["s5_normw"], sl_, w=[tb])
    sig = ar.alloc([4, 512], BF16)
    tsig = Trk()
    sq = ar.alloc([4, 512], BF16)
    tsq = Trk()
    rs = ar.alloc([512], F32)
    trs = Trk()
    for nck in range(4):
        ts = slice(nck * 512, (nck + 1) * 512)
        for co in range(4):
            pb = k.psum[co % 2]
            tp = k.tpsum[co % 2]
            for ci in range(4):
                cx.op("tensor", lambda h, pb=pb, co=co, ci=ci, ts=ts: h.matmul(pb[:, :], wg[:, ci, co * 128:(co + 1) * 128], gTb[:, ci, ts], start=(ci == 0), stop=(ci == 3)),
                      r=[twg] + tgTb, w=[tp], inc=(ci == 3))
            cx.op("scalar", lambda h, pb=pb, co=co: h.activation(sig[:, co, :], pb[:, :], AF.Sigmoid, bias=bgl[:, co:co + 1]), r=[tp, tb], w=[tsig])
        for co in range(4):
            cx.op(V, lambda h, co=co, ts=ts: h.tensor_tensor(out=gT[:, co, ts], in0=gT[:, co, ts], in1=sig[:, co, :], op=ALU.mult), r=[tsig, tgT[co]], w=[tgT[co]])
            cx.op("scalar", lambda h, co=co, ts=ts: h.activation(sq[:, co, :], gT[:, co, ts], AF.Square), r=[tgT[co]], w=[tsq])
        pb = k.psum[2 + nck % 2]
        tp = k.tpsum[2 + nck % 2]
        for co in range(4):
            cx.op("tensor", lambda h, pb=pb, co=co: h.matmul(pb[:, :], k.onesb, sq[:, co, :], start=(co == 0), stop=(co == 3)), r=[tsq], w=[tp], inc=(co == 3))
        cx.op("scalar", lambda h, pb=pb: h.activation(rs, pb[:, :], AF.Sqrt, scale=1.0 / 512, bias=k.epsc), r=[tp], w=[trs])
        cx.op(V, lambda h: h.reciprocal(rs, rs), r=[trs], w=[trs])
        for co in range(4):
            cx.op(V, lambda h, co=co, ts=ts: h.scalar_tensor_tensor(out=yT[:, co, ts], in0=gT[:, co, ts], scalar=nw[:, co:co + 1], in1=rs, op0=ALU.mult, op1=ALU.mult),
                  r=[tgT[co], trs, tb, tsq], w=[t_yT])
    k.dbg_add("s5_gl", gT, tgT)
    cx.barrier()
    ar.release(m0)


def build(in_shapes, stage="full", dbg_names=(), n_heads=4, n_experts=32):
    nc = bass.Bass("TRN2", target_bir_lowering=False)
    k = K()
    k.n_heads = n_heads
    k.n_experts = n_experts
    k.nc = nc
    D = {}
    for nm, (shape, dt) in in_shapes.items():
        D[nm] = nc.dram_tensor(nm, list(shape), dt, kind="ExternalInput").ap()
    k.D = D
    out = nc.dram_tensor("out", [S, DM], F32, kind="ExternalOutput").ap()
    k.dbg = {}
    k.dbg_req = set(dbg_names)

    with contextlib.ExitStack() as st:
        cx = Ctx(nc, st)
        k.cx = cx

        def finish():
            deps = [(s_.key, s_.total) for s_ in cx.slots if s_.total > 0]
            cx.wait_deps("sync", deps + [(e, cx.cnt[e]) for e in ENGS if e != "sync" and cx.cnt[e] > 0])
            with nc.Block() as block:
                cx.emit_all(block)
            k.n_ops = cx.n_ops
            return nc, k
        k.slot_c = cx.slot("c")
        k.slot_w = cx.slot("w")
        k.slot_x = [cx.slot("x0"), cx.slot("x1")]
        k.slot_o = cx.slot("o")
        k.psum = [cx.ps("ps%d" % i, [128, 512], F32) for i in range(8)]
        k.psum = [p[:, :] for p in k.psum]
        k.tpsum = [Trk("ps%d" % i, excl=True) for i in range(8)]
        k.ident = cx.sb("ident", [128, 128], F32)[:, :]
        k.identb = cx.sb("identb", [128, 128], BF16)[:, :]
        k.ones = cx.sb("ones", [128, 128], F32)[:, :]
        k.onesb = cx.sb("onesb", [128, 128], BF16)[:, :]
        k.epsc = cx.sb("epsc", [128, 1], F32)[:, :]
        k.selT = cx.sb("selT", [128, 2, 8, 128], BF16)[:, :, :, :]
        tc = Trk()
        cx.dma("sync", k.ident, D["ident"], k.slot_c, w=[tc])
        cx.dma("sync", k.identb, D["identb"], k.slot_c, w=[tc])
        cx.dma("sync", k.ones, D["ones"], k.slot_c, w=[tc])
        cx.dma("gpsimd", k.onesb, D["ones"], k.slot_c, w=[tc])
        cx.op("vector", lambda h: h.memset(k.epsc, EPS), w=[tc])
        cx.dma("sync", k.selT, D["selT"], k.slot_c, w=[tc])
        k.s5A1 = cx.sb("s5A1", [128, 64], F32)[:, :]
        k.s5A2 = cx.sb("s5A2", [128, 64], F32)[:, :]
        ar = Arena(cx, 51456)
        k.ar = ar
        cx.barrier()

        def dbg_add(name, ap, trks):
            if name in k.dbg_req:
                shape = list(ap.shape)
                dt_ = F32
                o = nc.dram_tensor("dbg_" + name, shape, dt_, kind="ExternalOutput").ap()
                cx.dma("gpsimd" if ap.dtype != F32 else "sync", o, ap, cx.fresh(), r=list(trks))
        k.dbg_add = dbg_add

        regA = ar.alloc([NT * 1024], F32)
        arA = Arena(cx, NT * 1024, base=regA)
        k.arA = arA
        yT = ar.alloc([8, 2048], BF16)
        t_yT = Trk()
        k.s5U = arA.alloc([32, 256], BF16)
        k.t_s5U = Trk()
        m_h = arA.mark()
        k.hT = arA.alloc([8, 2048], BF16)
        k.t_hT = Trk()

        m1 = ar.mark()
        xt = [ar.alloc([1024], F32) for _ in range(2)]
        txt = [Trk(), Trk()]

        def src_x(i):
            b = i % 2
            cx.dma("sync", xt[b], D["x"][i * 128:(i + 1) * 128, :], k.slot_x[b], w=[txt[b]])
            return xt[b], txt[b]
        norm_transpose(k, "mix", src_x, NT, D["norm_mix"], k.hT, BF16, k.t_hT)
        cx.barrier()
        ar.release(m1)

        if stage == 'p1':
            return finish()
        s5_build_U(k)
        if stage == 'U':
            return finish()
        mg = ar.mark()
        gdn_setup(k)
        for hd in range(k.n_heads):
            gdn_head(k, hd, yT, t_yT)
        ar.release(mg)
        k.dbg_add("ygdnT", yT[:, 4:8, :], [t_yT])
        if stage == 'gdn':
            return finish()
        cx.barrier()
        arA.release(m_h)
        k.s5AT = arA.alloc([64, 128], BF16)
        k.t_s5at = Trk()
        k.s5CS = arA.alloc([2, 16, 2, 128], BF16)
        k.s5TT = arA.alloc([32, 128], BF16)
        k.t_s5tt = Trk()
        s5_prep(k)
        if stage == 's5prep':
            return finish()
        s5_main(k, yT[:, 0:4, :], t_yT)
        k.dbg_add("ys5T", yT[:, 0:4, :], [t_yT])
        if stage == "s5":
            return finish()
        if True:
            cx.barrier()
            k.xacc = regA.rearrange('p (a b) -> p a b', a=NT)
            k.txacc = [Trk() for _ in range(NT)]
            out_proj(k, yT, t_yT)
            k.dbg_add("x1", k.xacc, k.txacc)
            if stage == 'oproj':
                return finish()
            xattn(k)
            if stage == 'xattn':
                return finish()
            k.dbg_add("x2", k.xacc, k.txacc)
            moe(k)
            k.dbg_add("x3", k.xacc, k.txacc)
            final_norm(k, out)

        return finish()


def host_inputs(inp, b):
    m = {}
    m["x"] = np.ascontiguousarray(inp["x"][b])
    m["mem"] = np.ascontiguousarray(inp["mem"][b])
    m["norm_mix"] = inp["norm_mix"][0]
    m["w_in"] = inp["w_in"][0]
    m["w_out"] = inp["w_out"][0]
    m["s5_w_glu"] = inp["s5_w_glu"][0]
    m.update(host_s5(inp))
    cv = inp["gdn_conv"][0]
    m["gdn_convw"] = np.ascontiguousarray(cv.reshape(5, 3, 4, 128).transpose(3, 2, 1, 0))
    for nm in ("gdn_a_log_f", "gdn_dt_bias_f", "gdn_a_log_b", "gdn_dt_bias_b"):
        m[nm] = inp[nm][0]
    m["gdn_norm"] = inp["gdn_norm"][0]
    for nm in ("norm_xattn", "norm_mem", "xa_wq", "xa_wk", "xa_wv", "xa_wo", "norm_moe", "router_group_w", "router_group_b",
               "router_expert_w", "router_expert_b", "moe_w_gate", "moe_w_up", "moe_w_down"):
        m[nm] = inp[nm][0]
    m["norm_final"] = inp["norm_final"]
    m.update(host_consts())
    return m


def gdn_setup(k):
    cx, ar, D = k.cx, k.ar, k.D
    V = "vector"
    G = K()
    k.G = G
    G.mask = ar.alloc([8, 128], F32)
    G.tmask = Trk()
    cx.dma("sync", G.mask, D["gmask"], cx.fresh(), w=[G.tmask])
    wsm = ar.alloc([8, 16], BF16)
    tw = Trk()
    load_w_cols(k, D["w_in"], 2560, 16, wsm, tw, cx.fresh())
    BA = ar.alloc([16, 16], F32)
    tBA = Trk()
    for i in range(NT):
        pb = k.psum[i % 4]
        tp = k.tpsum[i % 4]
        for c in range(8):
            cx.op("tensor", lambda h, pb=pb, c=c, i=i: h.matmul(pb[:, 0:16], k.hT[:, c, i * 128:(i + 1) * 128], wsm[:, c, :], start=(c == 0), stop=(c == 7)),
                  r=[tw, k.t_hT], w=[tp], inc=(c == 7))
        cx.op("scalar", lambda h, pb=pb, i=i: h.copy(BA[:, i, :], pb[:, 0:16]), r=[tp], w=[tBA])
    pr = ar.alloc([4, 4], F32)
    tpr = Trk()
    sl_ = cx.fresh()
    for j, nm in enumerate(("gdn_a_log_f", "gdn_dt_bias_f", "gdn_a_log_b", "gdn_dt_bias_b")):
        cx.dma("sync", pr[:, j, :], dram_bcast(D[nm], 128, 4), sl_, w=[tpr])
    G.nw = ar.alloc([128], F32)
    cx.dma("sync", G.nw, dram_bcast(D["gdn_norm"], 128, 128), sl_, w=[tpr])
    G.tpr = tpr
    T = Trk()
    G.T = T
    G.beta, G.nb, G.gc, G.eg, G.neg, G.ed = [], [], [], [], [], []
    def per_dir(d):
        beta = ar.alloc([16, 4], F32)
        nb = ar.alloc([16, 4], F32)
        g = ar.alloc([16, 4], F32)
        gc = ar.alloc([16, 4], F32)
        gt = ar.alloc([16, 4], F32)
        eg = ar.alloc([16, 4], F32)
        neg = ar.alloc([16, 4], F32)
        ed = ar.alloc([16, 4], F32)
        ea = ar.alloc([4], F32)
        braw = BA[:, :, d * 4:(d + 1) * 4]
        araw = BA[:, :, 8 + d * 4:8 + (d + 1) * 4]
        cx.op("scalar", lambda h: h.activation(beta, braw, AF.Sigmoid), r=[tBA, T], w=[T])
        cx.op(V, lambda h: h.tensor_scalar(nb, beta, -1.0, None, op0=ALU.mult), r=[T], w=[T])
        cx.op("scalar", lambda h: h.activation(ea, pr[:, 2 * d, :], AF.Exp), r=[tpr, T], w=[T])
        cx.op(V, lambda h: h.tensor_tensor(out=g, in0=araw, in1=pr[:, 2 * d + 1, :].unsqueeze(1).to_broadcast([128, 16, 4]), op=ALU.add), r=[tBA, tpr, T], w=[T])
        cx.op("scalar", lambda h: h.activation(g, g, AF.Exp), r=[T], w=[T])
        cx.op("scalar", lambda h: h.activation(g, g, AF.Ln, bias=1.0), r=[T], w=[T])
        cx.op(V, lambda h: h.scalar_tensor_tensor(out=g, in0=g, scalar=-1.0, in1=ea.unsqueeze(1).to_broadcast([128, 16, 4]), op0=ALU.mult, op1=ALU.mult), r=[T], w=[T])
        g2 = g.rearrange("p a b -> p (a b)")
        pb = k.psum[4 + d]
        tp = k.tpsum[4 + d]
        cx.op("tensor", lambda h, pb=pb, d=d: h.matmul(pb[:, 0:64], G.mask[:, d, :], g2, start=True, stop=True), r=[T, G.tmask], w=[tp])
        cx.op("tensor", lambda h, pb=pb: h.matmul(pb[:, 64:128], G.mask[:, 6, :], g2, start=True, stop=True), r=[T, G.tmask], w=[tp])
        cx.op(V, lambda h, pb=pb: h.tensor_copy(gc.rearrange("p a b -> p (a b)"), pb[:, 0:64]), r=[tp], w=[T])
        cx.op(V, lambda h, pb=pb: h.tensor_tensor(out=gt.rearrange("p a b -> p (a b)"), in0=pb[:, 64:128], in1=gc.rearrange("p a b -> p (a b)"), op=ALU.subtract), r=[tp, T], w=[T])
        cx.op("scalar", lambda h: h.activation(eg, gc, AF.Exp), r=[T], w=[T])
        cx.op("scalar", lambda h: h.activation(ed, gt, AF.Exp), r=[T], w=[T])
        cx.op(V, lambda h: h.tensor_scalar(neg, eg, -1.0, None, op0=ALU.mult), r=[T], w=[T])
        G.g = getattr(G, "g", []) + [g]
        G.beta.append(beta); G.nb.append(nb); G.gc.append(gc); G.eg.append(eg); G.neg.append(neg); G.ed.append(ed)
    per_dir(0)
    per_dir(1)
    G.osum = ar.alloc([16, 128], F32)
    G.tosum = [Trk() for _ in range(NT)]


def gdn_head(k, hd, yT, t_yT):
    cx, ar, D, G = k.cx, k.ar, k.D, k.G
    V = "vector"
    m0 = ar.mark()
    qnT = ar.alloc([2048], BF16)
    knT = ar.alloc([2048], BF16)
    Ktok = ar.alloc([16, 128], BF16)
    Vtok = ar.alloc([16, 128], BF16)
    tq, tk_, tKt, tVt = Trk(), Trk(), Trk(), Trk()
    wz = ar.alloc([8, 128], BF16)
    twz = Trk()
    load_w_cols(k, D["w_in"], 512 + 1536 + hd * 128, 128, wz, twz, cx.fresh())
    mA = ar.mark()
    w3 = [ar.alloc([8, 128], BF16) for _ in range(3)]
    tw3 = [Trk() for _ in range(3)]
    for j in range(3):
        load_w_cols(k, D["w_in"], 512 + j * 512 + hd * 128, 128, w3[j], tw3[j], cx.fresh())
    cw = ar.alloc([3, 5], F32)
    tcw = Trk()
    cx.dma("sync", cw, D["gdn_convw"][:, hd, :, :], cx.fresh(), w=[tcw])
    diag = ar.alloc([15, 128], BF16)
    tdg = Trk()
    for j in range(3):
        for t in range(5):
            cx.op("vector", lambda h, j=j, t=t: h.tensor_scalar(diag[:, j * 5 + t, :], k.identb, cw[:, j, t:t + 1], None, op0=ALU.mult), r=[tcw], w=[tdg])
    import os
    ALV = int(os.environ.get("GDN_ALV", "9"))
    if ALV == 0:
        cx.barrier(); ar.release(m0); return
    raw = [ar.alloc([2052], BF16) for _ in range(2)]
    traw = [Trk(), Trk()]
    for b in range(2):
        cx.op("gpsimd", lambda h, b=b: h.memset(raw[b][:, 0:2], 0.0), w=[traw[b]])
        cx.op("gpsimd", lambda h, b=b: h.memset(raw[b][:, 2050:2052], 0.0), w=[traw[b]])
    act = ar.alloc([2048], F32)
    tact = Trk()
    vT = ar.alloc([2048], BF16)
    tvT = Trk()
    sqb = ar.alloc([2048], BF16)
    tsqb = Trk()
    rn = [ar.alloc([512], F32) for _ in range(2)]
    trn = [Trk(), Trk()]
    if ALV == 1:
        cx.barrier(); ar.release(m0); return
    for j in range(3):
        b = j % 2

        def consume(n, pb, tp, b=b):
            cx.op(V if n % 2 else "scalar", (lambda h: h.tensor_copy(raw[b][:, 2 + n * 512:2 + (n + 1) * 512], pb[:, :])) if n % 2 else
                  (lambda h: h.copy(raw[b][:, 2 + n * 512:2 + (n + 1) * 512], pb[:, :])), r=[tp], w=[traw[b]])
        proj_fm(k, w3[j], tw3[j], consume)
        for n in range(4):
            pb = k.psum[4 + n % 2]
            tp = k.tpsum[4 + n % 2]
            for t in range(5):
                cx.op("tensor", lambda h, pb=pb, t=t, n=n, j=j, b=b: h.matmul(pb[:, :], diag[:, j * 5 + t, :], raw[b][:, n * 512 + t:n * 512 + t + 512], start=(t == 0), stop=(t == 4)),
                      r=[tdg, traw[b]], w=[tp], inc=(t == 4))
            ts = slice(n * 512, (n + 1) * 512)
            if j == 2:
                cx.op("scalar", lambda h, pb=pb, ts=ts: h.activation(vT[:, ts], pb[:, :], AF.Silu), r=[tp], w=[tvT])
            elif ALV == 2:
                cx.op("scalar", lambda h, pb=pb, ts=ts: h.activation(act[:, ts], pb[:, :], AF.Silu), r=[tp], w=[tact])
            else:
                cx.op("scalar", lambda h, pb=pb, ts=ts: h.activation(act[:, ts], pb[:, :], AF.Silu), r=[tp], w=[tact])
                cx.op("scalar", lambda h, ts=ts: h.activation(sqb[:, ts], act[:, ts], AF.Square), r=[tact], w=[tsqb])
                pb2 = k.psum[6 + n % 2]
                tp2 = k.tpsum[6 + n % 2]
                cx.op("tensor", lambda h, pb2=pb2, ts=ts: h.matmul(pb2[:, :], k.onesb, sqb[:, ts], start=True, stop=True), r=[tsqb], w=[tp2])
                rb = n % 2
                cx.op("scalar", lambda h, pb2=pb2, rb=rb: h.activation(rn[rb], pb2[:, :], AF.Sqrt, bias=k.epsc), r=[tp2], w=[trn[rb]])
                cx.op(V, lambda h, rb=rb: h.reciprocal(rn[rb], rn[rb]), r=[trn[rb]], w=[trn[rb]])
                dstT, tdst, scl = (qnT, tq, 128.0 ** -0.5) if j == 0 else (knT, tk_, 1.0)
                cx.op(V, lambda h, ts=ts, rb=rb, dstT=dstT, scl=scl: h.scalar_tensor_tensor(out=dstT[:, ts], in0=act[:, ts], scalar=scl, in1=rn[rb], op0=ALU.mult, op1=ALU.mult),
                      r=[tact, trn[rb]], w=[tdst])
    if ALV <= 3:
        cx.barrier(); ar.release(m0); return
    TV = int(os.environ.get("GDN_TV", "0"))
    for i in range(NT):
        pb = k.psum[i % 2].bitcast(BF16)
        tp = k.tpsum[i % 2]
        if TV == 0:
            cx.op("tensor", lambda h, pb=pb, i=i: h.transpose(pb[:, 0:128], knT[:, i * 128:(i + 1) * 128], k.identb), r=[tk_], w=[tp])
            cx.op("tensor", lambda h, pb=pb, i=i: h.transpose(pb[:, 128:256], vT[:, i * 128:(i + 1) * 128], k.identb), r=[tvT], w=[tp])
            cx.op("scalar", lambda h, pb=pb, i=i: h.copy(Ktok[:, i, :], pb[:, 0:128]), r=[tp], w=[tKt])
            cx.op(V, lambda h, pb=pb, i=i: h.tensor_copy(Vtok[:, i, :], pb[:, 128:256]), r=[tp], w=[tVt])
        elif TV == 1:
            cx.op("tensor", lambda h, pb=pb, i=i: h.transpose(pb[:, 0:128], knT[:, i * 128:(i + 1) * 128], k.identb), r=[tk_], w=[tp])
            cx.op("scalar", lambda h, pb=pb, i=i: h.copy(Ktok[:, i, :], pb[:, 0:128]), r=[tp], w=[tKt])
        elif TV == 2:
            cx.op("tensor", lambda h, pb=pb, i=i: h.transpose(pb[:, 0:128], vT[:, i * 128:(i + 1) * 128], k.identb), r=[tvT], w=[tp])
            cx.op(V, lambda h, pb=pb, i=i: h.tensor_copy(Vtok[:, i, :], pb[:, 0:128]), r=[tp], w=[tVt])
    if hd == 0:
        k.dbg_add("gdn_qn", qnT, [tq])
        k.dbg_add("gdn_kn", knT, [tk_])
        k.dbg_add("gdn_vtok", Vtok, [tVt])
    cx.barrier()
    ar.release(mA)
    import os
    STOP = os.environ.get("GDN_STOP", "")
    if STOP == "A":
        ar.release(m0)
        return
    qgT = [ar.alloc([2048], BF16) for _ in range(2)]
    Kd = [ar.alloc([16, 128], BF16) for _ in range(2)]
    Pm = [ar.alloc([16, 128], BF16) for _ in range(2)]
    QKm = [ar.alloc([16, 128], BF16) for _ in range(2)]
    etot = [ar.alloc([32], F32) for _ in range(2)]
    tqg = [[Trk() for _ in range(NT)] for _ in range(2)]
    tKd = [[Trk() for _ in range(NT)] for _ in range(2)]
    tPm = [[Trk() for _ in range(NT)] for _ in range(2)]
    tQK = [[Trk() for _ in range(NT)] for _ in range(2)]
    tet = [[Trk() for _ in range(NT)] for _ in range(2)]
    NI = 8
    mN = ar.mark()
    Xb = [[ar.alloc([128], F32) for _ in range(2)] for _ in range(NI)]
    XTb = [[ar.alloc([128], F32) for _ in range(2)] for _ in range(NI)]
    Pb = [[ar.alloc([128], F32) for _ in range(2)] for _ in range(NI)]
    tX = [Trk() for _ in range(NI)]
    EGB = [ar.alloc([128], F32) for _ in range(NI)]
    ET = [ar.alloc([128], F32) for _ in range(NI)]
    ETs = ET
    tE = [Trk() for _ in range(NI)]
    insts = [(i, d) for i in range(NT) for d in range(2)]
    for g0 in range(0, len(insts), NI):
        grp = insts[g0:g0 + NI]
        for s_, (i, d) in enumerate(grp):
            tsl = slice(i * 128, (i + 1) * 128)
            col = pap(G.g[d], 0, 128, i * 4 + hd, [[0, 128]])
            gcc = G.gc[d][:, i, hd:hd + 1]
            pb = k.psum[s_]
            tp = k.tpsum[s_]
            cx.op("tensor", lambda h, pb=pb, col=col, d=d: h.matmul(pb[:, 0:128], col, G.mask[:, d, :], start=True, stop=True), r=[G.T, G.tmask], w=[tp], inc=False)
            cx.op("tensor", lambda h, pb=pb, tsl=tsl: h.matmul(pb[:, 128:256], knT[:, tsl], knT[:, tsl], start=True, stop=True), r=[tk_], w=[tp], inc=False)
            cx.op("tensor", lambda h, pb=pb, tsl=tsl: h.matmul(pb[:, 256:384], knT[:, tsl], qnT[:, tsl], start=True, stop=True), r=[tk_, tq], w=[tp])
            cx.op("scalar", lambda h, pb=pb, s_=s_: h.activation(EGB[s_], pb[:, 0:128], AF.Exp), r=[tp], w=[tE[s_]])
            cx.op(V, lambda h, pb=pb, s_=s_, gcc=gcc, d=d: h.scalar_tensor_tensor(out=ET[s_], in0=pb[:, 0:128], scalar=gcc, in1=G.mask[:, 2 + d, :], op0=ALU.subtract, op1=ALU.min),
                  r=[tp, G.T, G.tmask], w=[tE[s_]])
            cx.op("scalar", lambda h, s_=s_: h.activation(ET[s_], ET[s_], AF.Exp), r=[tE[s_]], w=[tE[s_]])
            cx.op(V, lambda h, s_=s_, tsl=tsl, d=d: h.tensor_tensor(out=qgT[d][:, tsl], in0=qnT[:, tsl], in1=EGB[s_], op=ALU.mult), r=[tq, tE[s_]], w=[tqg[d][i]])
            c0, c1 = (63, 127) if d == 0 else (0, 64)
            cx.op("scalar", lambda h, s_=s_, d=d, i=i, c0=c0: h.copy(etot[d][:, 2 * i:2 * i + 1], EGB[s_][:, c0:c0 + 1]), r=[tE[s_]], w=[tet[d][i]])
            cx.op("scalar", lambda h, s_=s_, d=d, i=i, c1=c1: h.copy(etot[d][:, 2 * i + 1:2 * i + 2], EGB[s_][:, c1:c1 + 1]), r=[tE[s_]], w=[tet[d][i]])
            cx.op(V, lambda h, pb=pb, s_=s_, d=d, i=i: h.tensor_tensor(out=QKm[d][:, i, :], in0=pb[:, 256:384], in1=ET[s_], op=ALU.mult), r=[tp, tE[s_]], w=[tQK[d][i]])
            cx.op(V, lambda h, s_=s_, d=d: h.tensor_tensor(out=ETs[s_], in0=ET[s_], in1=G.mask[:, 4 + d, :], op=ALU.mult), r=[tE[s_], G.tmask], w=[tE[s_]])
            nbc = G.nb[d][:, i, hd:hd + 1]
            cx.op(V, lambda h, pb=pb, s_=s_, nbc=nbc: h.scalar_tensor_tensor(out=Xb[s_][0], in0=pb[:, 128:256], scalar=nbc, in1=ETs[s_], op0=ALU.mult, op1=ALU.mult),
                  r=[tp, tE[s_], G.T], w=[tX[s_]])
            cx.op(V, lambda h, s_=s_: h.tensor_tensor(out=Pb[s_][0], in0=Xb[s_][0], in1=k.ident, op=ALU.add), r=[tX[s_]], w=[tX[s_]])
            edc = G.ed[d][:, i, hd:hd + 1]
            cx.op("scalar", lambda h, d=d, i=i, edc=edc: h.activation(Kd[d][:, i, :], Ktok[:, i, :], AF.Identity, scale=edc), r=[tKt, G.T], w=[tKd[d][i]])
            pn = k.psum[s_]
            tn = k.tpsum[s_]
            cx.op("tensor", lambda h, pn=pn, s_=s_: h.transpose(pn[:, 384:512], Xb[s_][0], k.ident), r=[tX[s_]], w=[tn])
            cx.op("scalar", lambda h, pn=pn, s_=s_: h.copy(XTb[s_][0], pn[:, 384:512]), r=[tn], w=[tX[s_]])
        for L in range(1, 6):
            a, b_ = (L - 1) % 2, L % 2
            for s_, (i, d) in enumerate(grp):
                pn = k.psum[s_]
                tn = k.tpsum[s_]
                if L < 5:
                    cx.op("tensor", lambda h, pn=pn, s_=s_, a=a: h.matmul(pn[:, 0:128], XTb[s_][a], Xb[s_][a], start=True, stop=True), r=[tX[s_]], w=[tn])
                    cx.op("scalar", lambda h, pn=pn, s_=s_, b_=b_: h.copy(Xb[s_][b_], pn[:, 0:128]), r=[tn], w=[tX[s_]])
                else:
                    cx.op("tensor", lambda h, pn=pn, s_=s_, a=a: h.matmul(pn[:, 128:256], Xb[s_][a], XTb[s_][a], start=True, stop=True), r=[tX[s_]], w=[tn])
                    cx.op(V, lambda h, pn=pn, s_=s_, b_=b_: h.tensor_copy(XTb[s_][b_], pn[:, 128:256]), r=[tn], w=[tX[s_]])
            if L < 5:
                for s_, (i, d) in enumerate(grp):
                    pn = k.psum[s_]
                    tn = k.tpsum[s_]
                    cx.op("tensor", lambda h, pn=pn, s_=s_, b_=b_: h.transpose(pn[:, 128:256], Xb[s_][b_], k.ident), r=[tX[s_]], w=[tn])
                    cx.op(V, lambda h, pn=pn, s_=s_, b_=b_: h.tensor_copy(XTb[s_][b_], pn[:, 128:256]), r=[tn], w=[tX[s_]])
            for s_, (i, d) in enumerate(grp):
                pn = k.psum[s_]
                tn = k.tpsum[s_]
                cx.op("tensor", lambda h, pn=pn, s_=s_, a=a, b_=b_: h.matmul(pn[:, 256:384], XTb[s_][b_], Pb[s_][a], start=True, stop=True), r=[tX[s_]], w=[tn])
                if L < 5:
                    cx.op(V, lambda h, pn=pn, s_=s_, a=a, b_=b_: h.tensor_tensor(out=Pb[s_][b_], in0=pn[:, 256:384], in1=Pb[s_][a], op=ALU.add), r=[tn, tX[s_]], w=[tX[s_]])
                else:
                    cx.op(V, lambda h, pn=pn, s_=s_, a=a, d=d, i=i: h.tensor_tensor(out=Pm[d][:, i, :], in0=pn[:, 256:384], in1=Pb[s_][a], op=ALU.add), r=[tn, tX[s_]], w=[tPm[d][i]])
    if STOP == "B":
        cx.barrier()
        ar.release(m0)
        return
    ar.release(mN)
    Sf = [[ar.alloc([128], F32) for _ in range(2)] for _ in range(2)]
    Sb = [ar.alloc([128], BF16) for _ in range(2)]
    Rp = [ar.alloc([128], BF16) for _ in range(2)]
    vn = [ar.alloc([128], BF16) for _ in range(2)]
    tS = [Trk(), Trk()]
    tR = [Trk(), Trk()]
    tv = [Trk(), Trk()]
    cx.op("gpsimd", lambda h: h.memset(G.osum, 0.0), w=G.tosum)
    for d in range(2):
        cx.op("gpsimd", lambda h, d=d: h.memset(Sf[d][0], 0.0), w=[tS[d]])
        cx.op("gpsimd", lambda h, d=d: h.memset(Sb[d], 0.0), w=[tS[d]])
        cx.op("gpsimd", lambda h, d=d: h.memset(Rp[d], 0.0), w=[tR[d]])
        cx.op("gpsimd", lambda h, d=d: h.memset(vn[d], 0.0), w=[tv[d]])
    for step in range(32):
        for d in range(2):
            if d == 0:
                i, hh = step // 2, step % 2
            else:
                i, hh = 15 - step // 2, 1 - step % 2
            tsl = slice(i * 128, (i + 1) * 128)
            ps_ = slice(hh * 64, (hh + 1) * 64)
            cur, nxt = step % 2, (step + 1) % 2
            pcs = [k.psum[4 * d + q_] for q_ in range(4)]
            tcs = [k.tpsum[4 * d + q_] for q_ in range(4)]
            negc = G.neg[d][ps_, i, hd:hd + 1]
            btc = G.beta[d][ps_, i, hd:hd + 1]
            p1, pv_, po_, pst = pcs
            t1_, tv_, to_, tst = tcs
            cx.op("tensor", lambda h, p1=p1, tsl=tsl, d=d: h.matmul(p1[:, 0:128], knT[:, tsl], Sb[d], start=True, stop=True), r=[tk_, tS[d]], w=[t1_])
            cx.op(V, lambda h, p1=p1, ps_=ps_, negc=negc, d=d, i=i: h.scalar_tensor_tensor(out=Rp[d][ps_, :], in0=p1[ps_, 0:128], scalar=negc, in1=Vtok[ps_, i, :], op0=ALU.mult, op1=ALU.add),
                  r=[t1_, tVt, G.T], w=[tR[d]])
            cx.op("tensor", lambda h, pv_=pv_, ps_=ps_, d=d, i=i: h.matmul(pv_[:, 0:128], Pm[d][ps_, i, :], Rp[d][ps_, :], start=True, stop=True), r=[tPm[d][i], tR[d]], w=[tv_])
            cx.op("scalar", lambda h, pv_=pv_, ps_=ps_, btc=btc, d=d: h.activation(vn[d][ps_, :], pv_[ps_, 0:128], AF.Identity, scale=btc), r=[tv_, G.T], w=[tv[d]])
            cx.op("tensor", lambda h, po_=po_, tsl=tsl, d=d: h.matmul(po_[:, 0:128], qgT[d][:, tsl], Sb[d], start=True, stop=False), r=[tqg[d][i], tS[d]], w=[to_], inc=False)
            cx.op("tensor", lambda h, po_=po_, ps_=ps_, d=d, i=i: h.matmul(po_[:, 0:128], QKm[d][ps_, i, :], vn[d][ps_, :], start=False, stop=True), r=[tQK[d][i], tv[d]], w=[to_])
            cx.op("tensor", lambda h, pst=pst, ps_=ps_, d=d, i=i: h.matmul(pst[:, 0:128], Kd[d][ps_, i, :], vn[d][ps_, :], start=True, stop=True), r=[tKd[d][i], tv[d]], w=[tst])
            cx.op("gpsimd" if False else V, lambda h, po_=po_, ps_=ps_, i=i: h.tensor_tensor(out=G.osum[ps_, i, :], in0=po_[ps_, 0:128], in1=G.osum[ps_, i, :], op=ALU.add), r=[to_, G.tosum[i]], w=[G.tosum[i]])
            etc = etot[d][:, 2 * i + hh:2 * i + hh + 1]
            cx.op(V, lambda h, pst=pst, d=d, cur=cur, nxt=nxt, etc=etc: h.scalar_tensor_tensor(out=Sf[d][nxt], in0=Sf[d][cur], scalar=etc, in1=pst[:, 0:128], op0=ALU.mult, op1=ALU.add),
                  r=[tst, tet[d][i], tS[d]], w=[tS[d]])
            cx.op("scalar", lambda h, d=d, nxt=nxt: h.copy(Sb[d], Sf[d][nxt]), r=[tS[d]], w=[tS[d]])
    if hd == 0:
        k.dbg_add("gdn_osum", G.osum, G.tosum)
    if STOP == "C":
        cx.barrier()
        ar.release(m0)
        return
    ss = ar.alloc([NT, 2], F32)
    tss = Trk()
    junk = ar.alloc([128], BF16)
    zs = [ar.alloc([128], F32) for _ in range(2)]
    tzs = [Trk(), Trk()]
    yb = [ar.alloc([128], BF16) for _ in range(2)]
    tyb = [Trk(), Trk()]
    for i in range(NT):
        cx.op("scalar", lambda h, i=i: h.activation(junk, G.osum[:, i, :], AF.Square, accum_out=ss[:, i, 0:1]), r=[G.tosum[i], tss], w=[tss])
    cx.op(V, lambda h: h.tensor_scalar(ss[:, :, 1:2], ss[:, :, 0:1], 1.0 / 128, EPS, op0=ALU.mult, op1=ALU.add), r=[tss], w=[tss])
    cx.op("scalar", lambda h: h.activation(ss[:, :, 1:2], ss[:, :, 1:2], AF.Sqrt), r=[tss], w=[tss])
    cx.op(V, lambda h: h.reciprocal(ss[:, :, 1:2], ss[:, :, 1:2]), r=[tss], w=[tss])
    for i in range(NT):
        b = i % 2
        pz = k.psum[b]
        tz = k.tpsum[b]
        for c in range(8):
            cx.op("tensor", lambda h, pz=pz, c=c, i=i: h.matmul(pz[:, 0:128], k.hT[:, c, i * 128:(i + 1) * 128], wz[:, c, :], start=(c == 0), stop=(c == 7)),
                  r=[twz, k.t_hT], w=[tz], inc=(c == 7))
        cx.op("scalar", lambda h, pz=pz, b=b: h.activation(zs[b], pz[:, 0:128], AF.Silu), r=[tz], w=[tzs[b]])
        s1 = ss[:, i, 1:2]
        cx.op(V, lambda h, i=i, s1=s1: h.scalar_tensor_tensor(out=G.osum[:, i, :], in0=G.osum[:, i, :], scalar=s1, in1=G.nw, op0=ALU.mult, op1=ALU.mult), r=[tss, G.tpr, G.tosum[i]], w=[G.tosum[i]])
        cx.op(V, lambda h, i=i, b=b: h.tensor_tensor(out=yb[b], in0=G.osum[:, i, :], in1=zs[b], op=ALU.mult), r=[G.tosum[i], tzs[b]], w=[tyb[b]])
        pt = k.psum[2 + b].bitcast(BF16)
        tt_ = k.tpsum[2 + b]
        cx.op("tensor", lambda h, pt=pt, b=b: h.transpose(pt[:, 0:128], yb[b], k.identb), r=[tyb[b]], w=[tt_])
        cx.op("scalar", lambda h, pt=pt, i=i: h.copy(yT[:, 4 + hd, i * 128:(i + 1) * 128], pt[:, 0:128]), r=[tt_], w=[t_yT])
    cx.barrier()
    ar.release(m0)


def out_proj(k, yT, t_yT):
    cx, ar, D = k.cx, k.ar, k.D
    m0 = ar.mark()
    wo = ar.alloc([8, 1024], BF16)
    two = Trk()
    wsrc = D["w_out"]
    sl_ = cx.fresh()
    for c in range(8):
        cx.dma("gpsimd", wo[:, c, :], wsrc[c * 128:(c + 1) * 128, :], sl_, w=[two])
    slx = cx.fresh()
    for i in range(NT):
        cx.dma("sync", k.xacc[:, i, :], D["x"][i * 128:(i + 1) * 128, :], slx, w=[k.txacc[i]])
    for i in range(NT):
        k.txacc[i].w = (slx.key, slx.total)
    for i in range(NT):
        for half in range(2):
            pb = k.psum[(2 * i + half) % 4]
            tp = k.tpsum[(2 * i + half) % 4]
            for c in range(8):
                cx.op("tensor", lambda h, pb=pb, c=c, i=i, half=half: h.matmul(pb[:, :], yT[:, c, i * 128:(i + 1) * 128], wo[:, c, half * 512:(half + 1) * 512], start=(c == 0), stop=(c == 7)),
                      r=[t_yT, two], w=[tp], inc=(c == 7))
            xs = k.xacc[:, i, half * 512:(half + 1) * 512]
            cx.op("vector", lambda h, pb=pb, xs=xs: h.tensor_tensor(out=xs, in0=pb[:, :], in1=xs, op=ALU.add), r=[tp, k.txacc[i]], w=[k.txacc[i]])
    cx.barrier()
    ar.release(m0)


def xattn(k):
    cx, ar, D = k.cx, k.ar, k.D
    V = "vector"
    m0 = ar.mark()
    xnT = ar.alloc([8, 2048], BF16)
    t_xnT = Trk()
    memT = ar.alloc([8, 256], BF16)
    t_memT = Trk()
    m1 = ar.mark()
    mt = [ar.alloc([1024], F32) for _ in range(2)]
    tmt = [Trk(), Trk()]

    def src_mem(i):
        cx.dma("sync", mt[i], D["mem"][i * 128:(i + 1) * 128, :], cx.fresh(), w=[tmt[i]])
        return mt[i], tmt[i]
    norm_transpose(k, "mem", src_mem, 2, D["norm_mem"], memT, BF16, t_memT)
    ar.release(m1)
    norm_transpose(k, "xa", lambda i: (k.xacc[:, i, :], k.txacc[i]), NT, D["norm_xattn"], xnT, BF16, t_xnT)
    k.dbg_add("xa_memT", memT, [t_memT])
    k.dbg_add("xa_xnT", xnT, [t_xnT])
    wq = [ar.alloc([8, 256], BF16) for _ in range(2)]
    wk = [ar.alloc([8, 256], BF16) for _ in range(2)]
    wv = [ar.alloc([8, 256], BF16) for _ in range(2)]
    wo = [ar.alloc([2, 1024], BF16) for _ in range(2)]
    tw = [Trk(), Trk()]
    sw = [cx.slot("xw0"), cx.slot("xw1")]
    kT = ar.alloc([2, 256], BF16)
    vh = ar.alloc([2, 256], BF16)
    tkv = Trk()
    qT = ar.alloc([2, 2048], BF16)
    tqT = Trk()
    E = [ar.alloc([2, 512], BF16) for _ in range(2)]
    tE = [Trk(), Trk()]
    rden = ar.alloc([512], F32)
    trd = Trk()
    oTn = ar.alloc([2, 512], BF16)
    toT = Trk()

    def load_head(hd):
        b = hd % 2
        c0 = hd * 256
        for (dst, nm) in ((wq[b], "xa_wq"), (wk[b], "xa_wk"), (wv[b], "xa_wv")):
            load_w_cols(k, D[nm], c0, 256, dst, tw[b], sw[b])
        src = D["xa_wo"]
        cx.dma("gpsimd", wo[b], bass.AP(src.tensor, src.offset + c0 * 1024, [[1024, 128], [128 * 1024, 2], [1, 1024]]), sw[b], w=[tw[b]])
    load_head(0)
    for hd in range(4):
        b = hd % 2
        if hd + 1 < 4:
            load_head(hd + 1)
        for dc in range(2):
            pb = k.psum[dc]
            tp = k.tpsum[dc]
            for c in range(8):
                cx.op("tensor", lambda h, pb=pb, c=c, dc=dc, b=b: h.matmul(pb[:, 0:256], wk[b][:, c, dc * 128:(dc + 1) * 128], memT[:, c, :], start=(c == 0), stop=(c == 7)),
                      r=[tw[b], t_memT], w=[tp], inc=(c == 7))
            cx.op("scalar", lambda h, pb=pb, dc=dc: h.copy(kT[:, dc, :], pb[:, 0:256]), r=[tp], w=[tkv])
        for mtile in range(2):
            pb = k.psum[2 + mtile]
            tp = k.tpsum[2 + mtile]
            for c in range(8):
                cx.op("tensor", lambda h, pb=pb, c=c, mtile=mtile, b=b: h.matmul(pb[:, 0:256], memT[:, c, mtile * 128:(mtile + 1) * 128], wv[b][:, c, :], start=(c == 0), stop=(c == 7)),
                      r=[tw[b], t_memT], w=[tp], inc=(c == 7))
            cx.op(V, lambda h, pb=pb, mtile=mtile: h.tensor_copy(vh[:, mtile, :], pb[:, 0:256]), r=[tp], w=[tkv])
        for dc in range(2):
            for n in range(4):
                pb = k.psum[4 + (dc * 4 + n) % 2]
                tp = k.tpsum[4 + (dc * 4 + n) % 2]
                for c in range(8):
                    cx.op("tensor", lambda h, pb=pb, c=c, dc=dc, n=n, b=b: h.matmul(pb[:, :], wq[b][:, c, dc * 128:(dc + 1) * 128], xnT[:, c, n * 512:(n + 1) * 512], start=(c == 0), stop=(c == 7)),
                          r=[tw[b], t_xnT], w=[tp], inc=(c == 7))
                if n % 2 == 0:
                    cx.op("scalar", lambda h, pb=pb, dc=dc, n=n: h.copy(qT[:, dc, n * 512:(n + 1) * 512], pb[:, :]), r=[tp], w=[tqT])
                else:
                    cx.op(V, lambda h, pb=pb, dc=dc, n=n: h.tensor_copy(qT[:, dc, n * 512:(n + 1) * 512], pb[:, :]), r=[tp], w=[tqT])
        if hd == 0:
            k.dbg_add("xa_qT", qT, [tqT])
            k.dbg_add("xa_kT", kT, [tkv])
            k.dbg_add("xa_vh", vh, [tkv])
        for n in range(4):
            eb = n % 2
            ts = slice(n * 512, (n + 1) * 512)
            for mtile in range(2):
                pb = k.psum[mtile]
                tp = k.tpsum[mtile]
                for dc in range(2):
                    cx.op("tensor", lambda h, pb=pb, dc=dc, mtile=mtile, ts=ts: h.matmul(pb[:, :], kT[:, dc, mtile * 128:(mtile + 1) * 128], qT[:, dc, ts], start=(dc == 0), stop=(dc == 1)),
                          r=[tkv, tqT], w=[tp], inc=(dc == 1))
                cx.op("scalar", lambda h, pb=pb, mtile=mtile, eb=eb: h.activation(E[eb][:, mtile, :], pb[:, :], AF.Exp, scale=1.0 / 16.0), r=[tp], w=[tE[eb]])
            pd = k.psum[2]
            tpd = k.tpsum[2]
            for mtile in range(2):
                cx.op("tensor", lambda h, pd=pd, mtile=mtile, eb=eb: h.matmul(pd[:, :], k.onesb, E[eb][:, mtile, :], start=(mtile == 0), stop=(mtile == 1)), r=[tE[eb]], w=[tpd], inc=(mtile == 1))
            cx.op(V, lambda h, pd=pd: h.reciprocal(rden, pd[:, :]), r=[tpd], w=[trd])
            for dc in range(2):
                po = k.psum[3 + dc]
                tpo = k.tpsum[3 + dc]
                for mtile in range(2):
                    cx.op("tensor", lambda h, po=po, mtile=mtile, dc=dc, eb=eb: h.matmul(po[:, :], vh[:, mtile, dc * 128:(dc + 1) * 128], E[eb][:, mtile, :], start=(mtile == 0), stop=(mtile == 1)),
                          r=[tkv, tE[eb]], w=[tpo], inc=(mtile == 1))
                cx.op(V, lambda h, po=po, dc=dc: h.tensor_tensor(out=oTn[:, dc, :], in0=po[:, :], in1=rden, op=ALU.mult), r=[tpo, trd], w=[toT])
            for t in range(4):
                i = n * 4 + t
                for half in range(2):
                    pw_ = k.psum[5 + (t * 2 + half) % 3]
                    tpw = k.tpsum[5 + (t * 2 + half) % 3]
                    for dc in range(2):
                        cx.op("tensor", lambda h, pw_=pw_, dc=dc, t=t, half=half, b=b: h.matmul(pw_[:, :], oTn[:, dc, t * 128:(t + 1) * 128], wo[b][:, dc, half * 512:(half + 1) * 512], start=(dc == 0), stop=(dc == 1)),
                              r=[toT, tw[b]], w=[tpw], inc=(dc == 1))
                    xs = k.xacc[:, i, half * 512:(half + 1) * 512]
                    cx.op(V, lambda h, pw_=pw_, xs=xs: h.tensor_tensor(out=xs, in0=pw_[:, :], in1=xs, op=ALU.add), r=[tpw, k.txacc[i]], w=[k.txacc[i]])
    cx.barrier()
    ar.release(m0)


def moe(k):
    cx, ar, D = k.cx, k.ar, k.D
    V = "vector"
    m0 = ar.mark()
    xnT = ar.alloc([8, 2048], BF16)
    t_xnT = Trk()
    norm_transpose(k, "moe", lambda i: (k.xacc[:, i, :], k.txacc[i]), NT, D["norm_moe"], xnT, BF16, t_xnT)
    wr = ar.alloc([8, 36], BF16)
    twr = Trk()
    sl_ = cx.fresh()
    srcg, srce = D["router_group_w"], D["router_expert_w"]
    cx.dma("gpsimd", wr[:, :, 0:4], bass.AP(srcg.tensor, srcg.offset, [[4, 128], [4 * 128, 8], [1, 4]]), sl_, w=[twr])
    cx.dma("gpsimd", wr[:, :, 4:36], bass.AP(srce.tensor, srce.offset, [[32, 128], [32 * 128, 8], [1, 32]]), sl_, w=[twr])
    rb = ar.alloc([36], F32)
    trb = Trk()
    sl2 = cx.fresh()
    cx.dma("sync", rb[:, 0:4], dram_bcast(D["router_group_b"], 128, 4), sl2, w=[trb])
    cx.dma("sync", rb[:, 4:36], dram_bcast(D["router_expert_b"], 128, 32), sl2, w=[trb])
    cw = ar.alloc([NT, 32], F32)
    tcw = Trk()
    lg = ar.alloc([36], F32)
    msk = ar.alloc([32], F32)
    m8 = ar.alloc([8], F32)
    sc = ar.alloc([8], F32)
    oh = ar.alloc([4], F32)
    T = Trk()
    for i in range(NT):
        pb = k.psum[i % 2]
        tp = k.tpsum[i % 2]
        for c in range(8):
            cx.op("tensor", lambda h, pb=pb, c=c, i=i: h.matmul(pb[:, 0:36], xnT[:, c, i * 128:(i + 1) * 128], wr[:, c, :], start=(c == 0), stop=(c == 7)),
                  r=[t_xnT, twr], w=[tp], inc=(c == 7))
        cx.op(V, lambda h, pb=pb: h.tensor_tensor(out=lg, in0=pb[:, 0:36], in1=rb, op=ALU.add), r=[tp, trb, T], w=[T])
        cx.op(V, lambda h: h.tensor_reduce(out=sc[:, 0:1], in_=lg[:, 0:4], op=ALU.max, axis=AX.X), r=[T], w=[T])
        cx.op(V, lambda h: h.tensor_scalar(oh, lg[:, 0:4], sc[:, 0:1], None, op0=ALU.is_equal), r=[T], w=[T])
        cx.op(V, lambda h: h.tensor_scalar(sc[:, 1:2], sc[:, 0:1], -1.0, None, op0=ALU.mult), r=[T], w=[T])
        cx.op("scalar", lambda h: h.activation(m8[:, 0:4], lg[:, 0:4], AF.Exp, bias=sc[:, 1:2], accum_out=sc[:, 2:3]), r=[T], w=[T])
        cx.op(V, lambda h: h.reciprocal(sc[:, 3:4], sc[:, 2:3]), r=[T], w=[T])
        cx.op(V, lambda h: h.tensor_scalar(oh, oh, -1.0, 1e30, op0=ALU.add, op1=ALU.mult), r=[T], w=[T])
        cx.op(V, lambda h: h.tensor_tensor(out=msk.rearrange("p (g e) -> p g e", g=4), in0=lg[:, 4:36].rearrange("p (g e) -> p g e", g=4),
                                            in1=oh.unsqueeze(2).to_broadcast([128, 4, 8]), op=ALU.add), r=[T], w=[T])
        cx.op(V, lambda h: h.max(out=m8, in_=msk), r=[T], w=[T])
        cx.op(V, lambda h: h.tensor_tensor(out=sc[:, 4:5], in0=m8[:, 1:2], in1=m8[:, 0:1], op=ALU.subtract), r=[T], w=[T])
        cx.op("scalar", lambda h: h.activation(sc[:, 4:5], sc[:, 4:5], AF.Exp), r=[T], w=[T])
        cx.op(V, lambda h: h.tensor_scalar(sc[:, 5:6], sc[:, 4:5], 1.0, None, op0=ALU.add), r=[T], w=[T])
        cx.op(V, lambda h: h.reciprocal(sc[:, 5:6], sc[:, 5:6]), r=[T], w=[T])
        cx.op(V, lambda h: h.tensor_tensor(out=sc[:, 6:7], in0=sc[:, 4:5], in1=sc[:, 5:6], op=ALU.mult), r=[T], w=[T])
        cx.op(V, lambda h: h.tensor_tensor(out=sc[:, 5:6], in0=sc[:, 5:6], in1=sc[:, 3:4], op=ALU.mult), r=[T], w=[T])
        cx.op(V, lambda h: h.tensor_tensor(out=sc[:, 6:7], in0=sc[:, 6:7], in1=sc[:, 3:4], op=ALU.mult), r=[T], w=[T])
        cx.op(V, lambda h, i=i: h.tensor_scalar(cw[:, i, :], msk, m8[:, 0:1], sc[:, 5:6], op0=ALU.is_equal, op1=ALU.mult), r=[T], w=[tcw, T])
        cx.op(V, lambda h: h.tensor_scalar(lg[:, 4:36], msk, m8[:, 1:2], sc[:, 6:7], op0=ALU.is_equal, op1=ALU.mult), r=[T], w=[T])
        cx.op(V, lambda h, i=i: h.tensor_tensor(out=cw[:, i, :], in0=cw[:, i, :], in1=lg[:, 4:36], op=ALU.add), r=[T, tcw], w=[tcw, T])
    k.dbg_add("moe_cw", cw, [tcw])
    wgu = [ar.alloc([8, 512], BF16) for _ in range(2)]
    wd = [ar.alloc([2, 1024], BF16) for _ in range(2)]
    twe = [Trk(), Trk()]
    swe = [cx.slot("we0"), cx.slot("we1")]
    sg = [ar.alloc([512], F32) for _ in range(2)]
    tsg = [Trk(), Trk()]
    h1 = [ar.alloc([2, 512], BF16) for _ in range(2)]
    th1 = [Trk(), Trk()]
    NE = k.n_experts

    def load_e(e):
        b = e % 2
        g_, u_, d_ = D["moe_w_gate"], D["moe_w_up"], D["moe_w_down"]
        cx.dma("gpsimd", wgu[b][:, :, 0:256], bass.AP(g_.tensor, g_.offset + e * 1024 * 256, [[256, 128], [256 * 128, 8], [1, 256]]), swe[b], w=[twe[b]])
        cx.dma("gpsimd", wgu[b][:, :, 256:512], bass.AP(u_.tensor, u_.offset + e * 1024 * 256, [[256, 128], [256 * 128, 8], [1, 256]]), swe[b], w=[twe[b]])
        cx.dma("gpsimd", wd[b], bass.AP(d_.tensor, d_.offset + e * 256 * 1024, [[1024, 128], [1024 * 128, 2], [1, 1024]]), swe[b], w=[twe[b]])
    import os
    NOLOAD = os.environ.get("MOE_NOLOAD", "") == "1"
    load_e(0)
    if NOLOAD:
        load_e(1)
    cnt = 0
    for e in range(NE):
        b = e % 2
        if e + 1 < NE and not NOLOAD:
            load_e(e + 1)
        for n in range(4):
            ts = slice(n * 512, (n + 1) * 512)
            hb = n % 2
            for fh in range(2):
                pg = k.psum[fh * 2]
                tpg = k.tpsum[fh * 2]
                pu = k.psum[fh * 2 + 1]
                tpu = k.tpsum[fh * 2 + 1]
                for c in range(8):
                    cx.op("tensor", lambda h, pg=pg, c=c, fh=fh, ts=ts, b=b: h.matmul(pg[:, :], wgu[b][:, c, fh * 128:(fh + 1) * 128], xnT[:, c, ts], start=(c == 0), stop=(c == 7)),
                          r=[twe[b], t_xnT], w=[tpg], inc=(c == 7))
                for c in range(8):
                    cx.op("tensor", lambda h, pu=pu, c=c, fh=fh, ts=ts, b=b: h.matmul(pu[:, :], wgu[b][:, c, 256 + fh * 128:256 + (fh + 1) * 128], xnT[:, c, ts], start=(c == 0), stop=(c == 7)),
                          r=[twe[b], t_xnT], w=[tpu], inc=(c == 7))
                cx.op("scalar", lambda h, pg=pg, fh=fh: h.activation(sg[fh], pg[:, :], AF.Silu), r=[tpg], w=[tsg[fh]])
                cx.op(V, lambda h, pu=pu, fh=fh, hb=hb: h.tensor_tensor(out=h1[hb][:, fh, :], in0=pu[:, :], in1=sg[fh], op=ALU.mult), r=[tpu, tsg[fh]], w=[th1[hb]])
            for t in range(4):
                i = n * 4 + t
                for half in range(2):
                    pdn = k.psum[4 + cnt % 4]
                    tpd = k.tpsum[4 + cnt % 4]
                    cnt += 1
                    for fh in range(2):
                        cx.op("tensor", lambda h, pdn=pdn, fh=fh, t=t, half=half, hb=hb, b=b: h.matmul(pdn[:, :], h1[hb][:, fh, t * 128:(t + 1) * 128], wd[b][:, fh, half * 512:(half + 1) * 512], start=(fh == 0), stop=(fh == 1)),
                              r=[th1[hb], twe[b]], w=[tpd], inc=(fh == 1))
                    xs = k.xacc[:, i, half * 512:(half + 1) * 512]
                    cwc = cw[:, i, e:e + 1]
                    cx.op(V, lambda h, pdn=pdn, xs=xs, cwc=cwc: h.scalar_tensor_tensor(out=xs, in0=pdn[:, :], scalar=cwc, in1=xs, op0=ALU.mult, op1=ALU.add), r=[tpd, tcw, k.txacc[i]], w=[k.txacc[i]])
    cx.barrier()
    ar.release(m0)


def final_norm(k, out):
    cx, ar, D = k.cx, k.ar, k.D
    V = "vector"
    m0 = ar.mark()
    gB = ar.alloc([1024], F32)
    tg = Trk()
    cx.dma("sync", gB, dram_bcast(D["norm_final"], 128, 1024), cx.fresh(), w=[tg])
    junk = ar.alloc([1024], BF16)
    tj = Trk()
    ss = ar.alloc([NT, 2], F32)
    tss = Trk()
    ob = [ar.alloc([1024], F32) for _ in range(2)]
    tob = [Trk(), Trk()]
    so = [cx.slot("o0"), cx.slot("o1")]
    for i in range(NT):
        b = i % 2
        s0, s1 = ss[:, i, 0:1], ss[:, i, 1:2]
        cx.op("scalar", lambda h, i=i, s0=s0: h.activation(junk, k.xacc[:, i, :], AF.Square, accum_out=s0), r=[k.txacc[i], tss], w=[tj, tss])
        cx.op(V, lambda h, s0=s0, s1=s1: h.tensor_scalar(s1, s0, 1.0 / 1024, EPS, op0=ALU.mult, op1=ALU.add), r=[tss], w=[tss])
        cx.op("scalar", lambda h, s1=s1: h.activation(s1, s1, AF.Sqrt), r=[tss], w=[tss])
        cx.op(V, lambda h, s1=s1: h.reciprocal(s1, s1), r=[tss], w=[tss])
        cx.op(V, lambda h, i=i, s1=s1, b=b: h.scalar_tensor_tensor(out=ob[b], in0=k.xacc[:, i, :], scalar=s1, in1=gB, op0=ALU.mult, op1=ALU.mult), r=[k.txacc[i], tss, tg], w=[tob[b]])
        cx.dma("sync", out[i * 128:(i + 1) * 128, :], ob[b], so[b], r=[tob[b]])
    cx.barrier()
    ar.release(m0)


_CACHE = {}


def kernel(**inputs):
    inp = {k_: np.asarray(v) for k_, v in inputs.items()}
    n = inp["x"].shape[0]
    maps = [host_inputs(inp, b) for b in range(n)]
    key = "full"
    if key not in _CACHE:
        shapes = {k_: (v.shape, np2dt(v)) for k_, v in maps[0].items()}
        _CACHE[key] = build(shapes)[0]
    nc = _CACHE[key]
    res = run_bass_kernel_spmd(nc, maps, core_ids=list(range(n)))
    return np.stack([np.asarray(r["out"], dtype=np.float32) for r in res.results], 0)
```

```python
import contextlib
import os
import math
import numpy as np
import ml_dtypes
import concourse.bass as bass
import concourse.mybir as mybir
from concourse.bass_utils import run_bass_kernel_spmd

F32 = mybir.dt.float32
BF16 = mybir.dt.bfloat16
F32R = mybir.dt.float32r
I32 = mybir.dt.int32
AF = mybir.ActivationFunctionType
ALU = mybir.AluOpType
AX = mybir.AxisListType

ENGS = ("sync", "scalar", "gpsimd", "vector", "tensor")
S = 2048
DM = 1024
NT = 16
EPS = 1e-6


class Trk:
    __slots__ = ("name", "w", "r", "excl")

    def __init__(self, name="", excl=False):
        self.name = name
        self.w = None
        self.r = {}
        self.excl = excl


class DmaSlot:
    def __init__(self, ctx, name):
        self.key = "d_" + name + str(ctx.nsem)
        ctx.sems[self.key] = ctx.new_sem(self.key)
        self.total = 0


class Ctx:
    def __init__(self, nc, stack):
        self.nc = nc
        self.stack = stack
        self.q = {e: [] for e in ENGS}
        self.sems = {}
        self.nsem = 0
        self.cnt = {e: 0 for e in ENGS}
        self.known = {e: {} for e in ENGS}
        for e in ENGS:
            self.sems[e] = self.new_sem("s_" + e)
        self.slots = []
        self.pool = []
        self.pool_i = 0
        self.n_ops = 0

    def new_sem(self, name):
        self.nsem += 1
        return self.stack.enter_context(self.nc.semaphore(name))

    def slot(self, name):
        s = DmaSlot(self, name)
        self.slots.append(s)
        return s

    def fresh(self):
        if self.pool_i >= len(self.pool):
            assert len(self.pool) < 48, "slot pool exhausted"
            self.pool.append(self.slot("p%d" % len(self.pool)))
        sl = self.pool[self.pool_i]
        self.pool_i += 1
        return sl

    def sb(self, name, shape, dt):
        return self.stack.enter_context(self.nc.sbuf_tensor("sb_" + name, list(shape), dt))

    def ps(self, name, shape, dt=F32):
        return self.stack.enter_context(self.nc.psum_tensor(name, list(shape), dt))

    def _waits_for(self, eng, r, w, extra=()):
        need = {}

        def req(dep):
            if dep is None:
                return
            k, c = dep
            if k == eng and eng in ("tensor", "sync"):
                return
            if c > need.get(k, 0):
                need[k] = c
        for t in r:
            req(t.w)
        for t in w:
            req(t.w)
            for k, c in t.r.items():
                req((k, c))
        for d in extra:
            req(d)
        out = []
        kn = self.known[eng]
        for k, c in need.items():
            if kn.get(k, 0) < c:
                kn[k] = c
                out.append((self.sems[k], c))
        return out

    def op(self, eng, fn, r=(), w=(), inc=True, extra=()):
        w = list(w) + [t for t in r if t.excl]
        r = [t for t in r if not t.excl]
        waits = self._waits_for(eng, r, w, extra)
        c = self.cnt[eng] + 1
        if inc:
            self.cnt[eng] = c
        sem = self.sems[eng]

        def emit(h, fn=fn, waits=waits, inc=inc, sem=sem):
            for s, v in waits:
                h.wait_ge(s, v)
            ins = fn(h)
            if inc:
                ins.then_inc(sem, 1)
        self.q[eng].append(emit)
        for t in r:
            t.r[eng] = c
        for t in w:
            t.w = (eng, c)
            t.r = {}
        self.n_ops += 1

    def dma(self, eng, out, in_, slot, r=(), w=(), extra=(), **kw):
        waits = self._waits_for(eng, r, w, extra)
        slot.total += 16
        sem = self.sems[slot.key]

        def emit(h, waits=waits, sem=sem, out=out, in_=in_, kw=kw):
            for s, v in waits:
                h.wait_ge(s, v)
            h.dma_start(out=out, in_=in_, **kw).then_inc(sem, 16)
        self.q[eng].append(emit)
        dep = (slot.key, slot.total)
        for t in r:
            t.r[slot.key] = slot.total
        for t in w:
            t.w = dep
            t.r = {}
        self.n_ops += 1
        return dep

    def wait_deps(self, eng, deps):
        waits = self._waits_for(eng, (), (), deps)

        def emit(h, waits=waits):
            for s, v in waits:
                h.wait_ge(s, v)
        self.q[eng].append(emit)

    def barrier(self):
        deps = [(e, self.cnt[e]) for e in ENGS if e != "sync" and self.cnt[e] > 0]
        deps += [(s.key, s.total) for s in self.slots if s.total > 0]
        for e in ENGS:
            self.wait_deps(e, deps)
        self.pool_i = 0

    def emit_all(self, block):
        q = self.q

        @block.sync
        def _(h):
            for f in q["sync"]:
                f(h)

        @block.scalar
        def _(h):
            for f in q["scalar"]:
                f(h)

        @block.gpsimd
        def _(h):
            for f in q["gpsimd"]:
                f(h)

        @block.vector
        def _(h):
            for f in q["vector"]:
                f(h)

        @block.tensor
        def _(h):
            for f in q["tensor"]:
                f(h)


class Arena:
    def __init__(self, cx, words, base=None):
        self.t = cx.sb("arena", [128, words], F32) if base is None else base
        self.cx = cx
        self.words = words
        self.top = 0

    def mark(self):
        return self.top

    def release(self, m):
        if m != self.top:
            self.cx.barrier()
        self.top = m

    def alloc(self, shape, dt):
        n = int(np.prod(shape))
        w = n if dt in (F32, F32R, I32) else (n + 1) // 2
        w = (w + 1) // 2 * 2
        o = self.top
        self.top += w
        assert self.top <= self.words, ("arena overflow", self.top, self.words)
        v = self.t[:, o:o + w]
        if dt != F32:
            v = v.bitcast(dt)
        v = v[:, 0:n]
        if len(shape) > 1:
            names = " ".join("d%d" % i for i in range(len(shape)))
            v = v.rearrange("p (%s) -> p %s" % (names, names), **{"d%d" % i: shape[i] for i in range(len(shape))})
        return v


def pap(ap, part0, nparts, off, dims):
    base = ap.ap[0][0]
    return bass.AP(ap.tensor, ap.offset + part0 * base + off, [[base, nparts]] + [list(d) for d in dims])


def host_consts():
    c = {}
    c["ident"] = np.eye(128, dtype=np.float32)
    c["identb"] = np.eye(128, dtype=np.float32).astype(ml_dtypes.bfloat16)
    c["ones"] = np.ones((128, 128), np.float32)
    selT = np.zeros((128, 2, 8, 128), np.float32)
    selB = np.zeros((128, 2, 8, 128), np.float32)
    for q in range(4):
        for r in range(32):
            loc, cc = r // 16, r % 16
            for s in range(8):
                selT[q * 32 + r, loc, s, s * 16 + cc] = 1.0
                selB[q * 32 + r, loc, s, s * 16 + cc] = 1.0
    c["selT"] = selT.astype(ml_dtypes.bfloat16)
    c["selB"] = selB.astype(ml_dtypes.bfloat16)
    sidx = np.arange(128) // 16
    c["s5mf"] = (sidx[None, :] >= sidx[:, None]).astype(np.float32)
    c["s5mb"] = (sidx[None, :] <= sidx[:, None]).astype(np.float32)
    c["kvec"] = np.tile((np.arange(16, dtype=np.float32) - 7.0)[None, :], (128, 1))
    k = np.arange(128)[:, None]
    cc = np.arange(128)[None, :]
    same = (k // 64) == (cc // 64)
    gm = np.zeros((128, 8, 128), np.float32)
    gm[:, 0] = same & (k <= cc)
    gm[:, 1] = same & (k >= cc)
    gm[:, 2] = np.where(same & (cc >= k), 0.0, -30000.0)
    gm[:, 3] = np.where(same & (cc <= k), 0.0, -30000.0)
    gm[:, 4] = same & (cc > k)
    gm[:, 5] = same & (cc < k)
    gm[:, 6] = same
    c["gmask"] = gm
    return c


def host_s5(inp):
    o = {}

    pairs = {"lam_re": ("s5_lam_re_f", "s5_lam_re_b"), "lam_im": ("s5_lam_im_f", "s5_lam_im_b"),
             "log_step": ("s5_log_step_f", "s5_log_step_b"), "b_re": ("s5_b_re_f", "s5_b_re_b"),
             "b_im": ("s5_b_im_f", "s5_b_im_b"), "c_re": ("s5_c_re_f", "s5_c_re_b"), "c_im": ("s5_c_im_f", "s5_c_im_b")}

    def st(nm):
        f_, b_ = pairs[nm]
        return np.stack([inp[f_][0], inp[b_][0]], 0)
    lam = np.stack([st("lam_re"), st("lam_im")], 0)
    lam = lam.reshape(2, 2, 2, 16, 64).transpose(2, 4, 0, 1, 3)
    o["s5_lam"] = np.ascontiguousarray(lam.reshape(128, 2, 32))
    ls = st("log_step").reshape(2, 2, 16)
    ls = np.broadcast_to(ls.transpose(1, 0, 2)[:, None], (2, 64, 2, 16))
    o["s5_step"] = np.ascontiguousarray(ls.reshape(128, 32))
    b = np.stack([st("b_re"), st("b_im")], 0)
    b = b.reshape(2, 2, 2, 16, 64, 16).transpose(2, 4, 0, 1, 3, 5)
    o["s5_b"] = np.ascontiguousarray(b.reshape(128, 2, 512))
    cm = np.stack([st("c_re"), st("c_im")], 0)
    cm = cm.reshape(2, 2, 2, 16, 16, 64).transpose(2, 5, 0, 1, 3, 4)
    o["s5_c"] = np.ascontiguousarray(cm.reshape(128, 2, 512))
    d = inp["s5_d"][0].reshape(32, 16)
    o["s5_dvec"] = np.ascontiguousarray(np.broadcast_to(d.T[None], (8, 16, 32)).reshape(128, 32))
    o["s5_bglu"] = np.ascontiguousarray(inp["s5_b_glu"][0].reshape(4, 128).T)
    o["s5_normw"] = np.ascontiguousarray(inp["s5_norm"][0].reshape(4, 128).T)
    return o


def np2dt(a):
    if a.dtype == np.float32:
        return F32
    if a.dtype == ml_dtypes.bfloat16:
        return BF16
    raise ValueError(a.dtype)


class K:
    pass


def dram_bcast(ap, nparts, n, off=0):
    return bass.AP(ap.tensor, ap.offset + off, [[0, nparts], [1, n]])


def norm_transpose(k, name, src_fn, ntiles, gain_dram, outT, out_dt, outT_trk):
    cx, ar = k.cx, k.ar
    m = ar.mark()
    gB = ar.alloc([1024], F32)
    tg = Trk()
    cx.dma("sync", gB, dram_bcast(gain_dram, 128, 1024), cx.fresh(), w=[tg])
    junk = ar.alloc([1024], BF16)
    tj = Trk()
    xn = [ar.alloc([1024], out_dt) for _ in range(2)]
    txn = [Trk(), Trk()]
    ss = ar.alloc([NT * 2, 1], F32)
    tss = [Trk() for _ in range(ntiles)]
    pdt = BF16 if out_dt == BF16 else F32
    ident = k.identb if out_dt == BF16 else k.ident
    for i in range(ntiles):
        src, ts = src_fn(i)
        ssi = ss[:, 2 * i:2 * i + 1]
        rsi = ss[:, 2 * i + 1:2 * i + 2]
        cx.op("scalar", lambda h, src=src, ssi=ssi: h.activation(junk, src, AF.Square, accum_out=ssi), r=[ts], w=[tj, tss[i]])
        cx.op("vector", lambda h, ssi=ssi, rsi=rsi: h.tensor_scalar(rsi, ssi, 1.0 / 1024, EPS, op0=ALU.mult, op1=ALU.add), r=[tss[i]], w=[tss[i]])
        cx.op("scalar", lambda h, rsi=rsi: h.activation(rsi, rsi, AF.Sqrt), r=[tss[i]], w=[tss[i]])
        cx.op("vector", lambda h, rsi=rsi: h.reciprocal(rsi, rsi), r=[tss[i]], w=[tss[i]])
        b = i % 2
        cx.op("vector", lambda h, src=src, rsi=rsi, b=b: h.scalar_tensor_tensor(out=xn[b], in0=src, scalar=rsi, in1=gB, op0=ALU.mult, op1=ALU.mult),
              r=[ts, tss[i], tg], w=[txn[b]])
        if out_dt == BF16:
            pb = k.psum[i % 2]
            tp = k.tpsum[i % 2]
            pv = pb.bitcast(BF16)
            for c in range(8):
                cx.op("tensor", lambda h, b=b, c=c, pv=pv: h.transpose(pv[:, c * 128:(c + 1) * 128], xn[b][:, c * 128:(c + 1) * 128], ident),
                      r=[txn[b]], w=[tp], inc=(c == 7))
            dst = outT[:, :, i * 128:(i + 1) * 128]
            eng = "scalar" if i % 2 == 0 else "vector"
            if eng == "scalar":
                cx.op(eng, lambda h, dst=dst, pv=pv: h.copy(dst, pv.rearrange("p (c t) -> p c t", c=8)), r=[tp], w=[outT_trk])
            else:
                cx.op(eng, lambda h, dst=dst, pv=pv: h.tensor_copy(dst, pv.rearrange("p (c t) -> p c t", c=8)), r=[tp], w=[outT_trk])
        else:
            for half in range(2):
                pb = k.psum[(2 * i + half) % 4]
                tp = k.tpsum[(2 * i + half) % 4]
                for c4 in range(4):
                    c = half * 4 + c4
                    cx.op("tensor", lambda h, b=b, c=c, c4=c4, pb=pb: h.transpose(pb[:, c4 * 128:(c4 + 1) * 128], xn[b][:, c * 128:(c + 1) * 128].bitcast(F32), ident),
                          r=[txn[b]], w=[tp], inc=(c4 == 3))
                dst = outT[:, half * 4:(half + 1) * 4, i * 128:(i + 1) * 128]
                if half == 0:
                    cx.op("scalar", lambda h, dst=dst, pb=pb: h.copy(dst, pb.rearrange("p (c t) -> p c t", c=4)), r=[tp], w=[outT_trk])
                else:
                    cx.op("vector", lambda h, dst=dst, pb=pb: h.tensor_copy(dst, pb.rearrange("p (c t) -> p c t", c=4)), r=[tp], w=[outT_trk])
    ar.release(m)


def s5_prep(k):
    cx, ar, D = k.cx, k.ar, k.D
    V = "vector"
    m0 = ar.mark()
    lam = ar.alloc([2, 32], F32)
    step = ar.alloc([32], F32)
    bb = ar.alloc([2, 512], F32)
    cc = ar.alloc([2, 512], F32)
    kvec = ar.alloc([16], F32)
    tl = Trk()
    sl_ = cx.fresh()
    for dst, nm in ((lam, "s5_lam"), (step, "s5_step"), (bb, "s5_b"), (cc, "s5_c"), (kvec, "kvec")):
        cx.dma("sync", dst, D[nm], sl_, w=[tl])
    T = Trk()

    def vop(fn, extra_r=()):
        cx.op(V, fn, r=[T, tl] + list(extra_r), w=[T])

    def aop(fn):
        cx.op("scalar", fn, r=[T, tl], w=[T])
    lre, lim = lam[:, 0, :], lam[:, 1, :]
    dl = ar.alloc([32], F32)
    re1 = ar.alloc([32], F32)
    im1 = ar.alloc([32], F32)
    aop(lambda h: h.activation(dl, step, AF.Exp))
    vop(lambda h: h.tensor_tensor(out=re1, in0=dl, in1=lre, op=ALU.mult))
    vop(lambda h: h.tensor_tensor(out=im1, in0=dl, in1=lim, op=ALU.mult))
    PWI = ar.alloc([16, 32], F32)
    PWR = ar.alloc([16, 32], F32)
    m_pw = ar.mark()
    KR = ar.alloc([16, 32], F32)
    KI = ar.alloc([16, 32], F32)
    kv_b = kvec.unsqueeze(2).to_broadcast([128, 16, 32])
    vop(lambda h: h.tensor_tensor(out=KR, in0=kv_b, in1=re1.unsqueeze(1).to_broadcast([128, 16, 32]), op=ALU.mult))
    vop(lambda h: h.tensor_tensor(out=KI, in0=kv_b, in1=im1.unsqueeze(1).to_broadcast([128, 16, 32]), op=ALU.mult))
    MAG = ar.alloc([16, 32], F32)
    aop(lambda h: h.activation(MAG, KR, AF.Exp))
    YI = ar.alloc([16, 32], I32)
    YF = ar.alloc([16, 32], F32)
    vop(lambda h: h.tensor_scalar(KI, KI, 1.0 / (2 * math.pi), None, op0=ALU.mult))
    vop(lambda h: h.tensor_copy(YI, KI))
    vop(lambda h: h.tensor_copy(YF, YI))
    vop(lambda h: h.tensor_tensor(out=KI, in0=KI, in1=YF, op=ALU.subtract))
    SH_ = ar.alloc([16, 32], F32)
    SQ_ = ar.alloc([16, 32], F32)
    aop(lambda h: h.activation(SH_, KI, AF.Sin, scale=math.pi))
    aop(lambda h: h.activation(SQ_, KI, AF.Sin, scale=math.pi / 2))
    CH_ = ar.alloc([16, 32], F32)
    vop(lambda h: h.tensor_tensor(out=CH_, in0=SQ_, in1=SQ_, op=ALU.mult))
    vop(lambda h: h.tensor_scalar(CH_, CH_, -2.0, 1.0, op0=ALU.mult, op1=ALU.add))
    vop(lambda h: h.tensor_tensor(out=PWI, in0=SH_, in1=CH_, op=ALU.mult))
    vop(lambda h: h.scalar_tensor_tensor(out=PWI, in0=PWI, scalar=2.0, in1=MAG, op0=ALU.mult, op1=ALU.mult))
    vop(lambda h: h.tensor_tensor(out=PWR, in0=SH_, in1=SH_, op=ALU.mult))
    vop(lambda h: h.tensor_scalar(PWR, PWR, -2.0, 1.0, op0=ALU.mult, op1=ALU.add))
    vop(lambda h: h.tensor_tensor(out=PWR, in0=PWR, in1=MAG, op=ALU.mult))
    ar.release(m_pw)
    lrm1 = ar.alloc([32], F32)
    li = PWI[:, 8, :]
    t1 = ar.alloc([32], F32)
    t2 = ar.alloc([32], F32)
    den = ar.alloc([32], F32)
    c0r = ar.alloc([32], F32)
    c0i = ar.alloc([32], F32)
    vop(lambda h: h.tensor_scalar(lrm1, PWR[:, 8, :], -1.0, None, op0=ALU.add))
    vop(lambda h: h.tensor_tensor(out=t1, in0=lre, in1=lre, op=ALU.mult))
    vop(lambda h: h.tensor_tensor(out=t2, in0=lim, in1=lim, op=ALU.mult))
    vop(lambda h: h.tensor_tensor(out=den, in0=t1, in1=t2, op=ALU.add))
    vop(lambda h: h.reciprocal(den, den))
    vop(lambda h: h.tensor_tensor(out=t1, in0=lrm1, in1=lre, op=ALU.mult))
    vop(lambda h: h.tensor_tensor(out=t2, in0=li, in1=lim, op=ALU.mult))
    vop(lambda h: h.tensor_tensor(out=t1, in0=t1, in1=t2, op=ALU.add))
    vop(lambda h: h.tensor_tensor(out=c0r, in0=t1, in1=den, op=ALU.mult))
    vop(lambda h: h.tensor_tensor(out=t1, in0=li, in1=lre, op=ALU.mult))
    vop(lambda h: h.tensor_tensor(out=t2, in0=lrm1, in1=lim, op=ALU.mult))
    vop(lambda h: h.tensor_tensor(out=t1, in0=t1, in1=t2, op=ALU.subtract))
    vop(lambda h: h.tensor_tensor(out=c0i, in0=t1, in1=den, op=ALU.mult))
    BBR = ar.alloc([32, 16], F32)
    BBI = ar.alloc([32, 16], F32)
    TA = ar.alloc([32, 16], F32)
    br = bb[:, 0, :].rearrange("p (a c) -> p a c", c=16)
    bi = bb[:, 1, :].rearrange("p (a c) -> p a c", c=16)
    c0r_b = c0r.unsqueeze(2).to_broadcast([128, 32, 16])
    c0i_b = c0i.unsqueeze(2).to_broadcast([128, 32, 16])
    vop(lambda h: h.tensor_tensor(out=BBR, in0=br, in1=c0r_b, op=ALU.mult))
    vop(lambda h: h.tensor_tensor(out=TA, in0=bi, in1=c0i_b, op=ALU.mult))
    vop(lambda h: h.tensor_tensor(out=BBR, in0=BBR, in1=TA, op=ALU.subtract))
    vop(lambda h: h.tensor_tensor(out=BBI, in0=bi, in1=c0r_b, op=ALU.mult))
    vop(lambda h: h.tensor_tensor(out=TA, in0=br, in1=c0i_b, op=ALU.mult))
    vop(lambda h: h.tensor_tensor(out=BBI, in0=BBI, in1=TA, op=ALU.add))
    ASd = ar.alloc([16, 2, 8, 16], F32)
    CS2d = ar.alloc([16, 2, 8, 16], F32)
    T1 = ar.alloc([8, 16, 16], F32)
    T2 = ar.alloc([8, 16, 16], F32)
    cr = cc[:, 0, :].rearrange("p (d a c) -> p d a c", d=2, c=16)
    ci = cc[:, 1, :].rearrange("p (d a c) -> p d a c", d=2, c=16)
    BBR4 = BBR.rearrange("p (d a) c -> p d a c", d=2)
    BBI4 = BBI.rearrange("p (d a) c -> p d a c", d=2)

    def pw(arr, d, k0, kstep):
        return pap(arr, 0, 128, k0 * 32 + d * 16, [[kstep * 32, 8], [1, 16], [0, 16]])

    def dst(arr, dofs, ri):
        return pap(arr, 0, 128, dofs * 4096 + ri * 128, [[16, 8], [256, 16], [1, 16]])

    def vec(v4, d):
        a_ = v4[:, d]
        return bass.AP(a_.tensor, a_.offset, [list(a_.ap[0]), [0, 8], list(a_.ap[1]), list(a_.ap[2])])

    T1f = T1.rearrange("p a b c -> p (a b c)")

    def cmul(out_arr, dofs, d, k0, kstep, vr, vi, neg_im):
        pr, pi_ = pw(PWR, d, k0, kstep), pw(PWI, d, k0, kstep)
        vop(lambda h: h.tensor_tensor(out=T1, in0=pr, in1=vec(vr, d), op=ALU.mult))
        vop(lambda h: h.tensor_tensor(out=T2, in0=pi_, in1=vec(vi, d), op=ALU.mult))
        vop(lambda h: h.tensor_tensor(out=dst(out_arr, dofs, 0), in0=T1, in1=T2, op=ALU.subtract))
        vop(lambda h: h.tensor_tensor(out=T1, in0=pr, in1=vec(vi, d), op=ALU.mult))
        vop(lambda h: h.tensor_tensor(out=T2, in0=pi_, in1=vec(vr, d), op=ALU.mult))
        if neg_im:
            vop(lambda h: h.tensor_scalar(T1f, T1f, -1.0, None, op0=ALU.mult))
            vop(lambda h: h.tensor_tensor(out=dst(out_arr, dofs, 1), in0=T1, in1=T2, op=ALU.subtract))
        else:
            vop(lambda h: h.tensor_tensor(out=dst(out_arr, dofs, 1), in0=T1, in1=T2, op=ALU.add))
    vop(lambda h: h.tensor_copy(k.s5A1[:, 0:32], PWR[:, 15, :]))
    vop(lambda h: h.tensor_copy(k.s5A1[:, 32:64], PWR[:, 15, :]))
    vop(lambda h: h.tensor_scalar(k.s5A2[:, 0:32], PWI[:, 15, :], -1.0, None, op0=ALU.mult))
    vop(lambda h: h.tensor_copy(k.s5A2[:, 32:64], PWI[:, 15, :]))
    cmul(k.s5CS, 0, 0, 8, 1, cr, ci, True)
    cmul(k.s5CS, 1, 1, 15, -1, cr, ci, True)
    k.t_s5w = T
    mf = ar.alloc([2, 128], F32)
    dv = ar.alloc([32], F32)
    tm = Trk()
    sl_ = cx.fresh()
    cx.dma("sync", mf[:, 0, :], D["s5mf"], sl_, w=[tm])
    cx.dma("sync", mf[:, 1, :], D["s5mb"], sl_, w=[tm])
    cx.dma("sync", dv, D["s5_dvec"], sl_, w=[tm])
    tt1 = [ar.alloc([128], F32) for _ in range(2)]
    ttt = [Trk(), Trk()]
    ASb = ASd.rearrange("p a r s c -> p (a r) (s c)")
    ASm = ASd.rearrange("p a r s c -> p a r (s c)")
    CSm = CS2d.rearrange("p a r s c -> p a r (s c)")
    for d in range(2):
        if d == 0:
            cmul(ASd, 0, 0, 14, -1, BBR4, BBI4, False)
            cmul(CS2d, 0, 0, 0, 1, cr, ci, True)
        else:
            cmul(ASd, 0, 1, 7, 1, BBR4, BBI4, False)
            cmul(CS2d, 0, 1, 7, -1, cr, ci, True)
        for grp in range(8):
            pb = k.psum[grp % 4]
            tp = k.tpsum[grp % 4]
            for j in range(4):
                blk = grp * 4 + j
                cx.op("tensor", lambda h, pb=pb, j=j, blk=blk: h.transpose(pb[:, j * 128:(j + 1) * 128], ASb[:, blk, :], k.ident),
                      r=[T], w=[tp], inc=(j == 3))
            dstv = k.s5AT[:, d * 32 + grp * 4:d * 32 + (grp + 1) * 4, :]
            if grp % 2 == 0:
                cx.op("scalar", lambda h, dstv=dstv, pb=pb: h.copy(dstv, pb.rearrange("p (j x) -> p j x", j=4)), r=[tp], w=[k.t_s5at])
            else:
                cx.op("vector", lambda h, dstv=dstv, pb=pb: h.tensor_copy(dstv, pb.rearrange("p (j x) -> p j x", j=4)), r=[tp], w=[k.t_s5at])
        for g in range(32):
            gh, gl = g // 16, g % 16
            pb = k.psum[4 + g % 4]
            tp = k.tpsum[4 + g % 4]
            for ri in range(2):
                cx.op("tensor", lambda h, pb=pb, ri=ri, gh=gh, gl=gl: h.matmul(
                    pb[:, 0:128], ASm[gh * 64:(gh + 1) * 64, gl, ri, :], CSm[gh * 64:(gh + 1) * 64, gl, ri, :],
                    start=(ri == 0), stop=(ri == 1)), r=[T], w=[tp], inc=(ri == 1))
            b_ = g % 2
            cx.op(V, lambda h, pb=pb, b_=b_, d=d: h.tensor_tensor(out=tt1[b_], in0=pb[:, 0:128], in1=mf[:, d, :], op=ALU.mult), r=[tp, tm], w=[ttt[b_]])
            if d == 0:
                cx.op(V, lambda h, b_=b_, g=g: h.scalar_tensor_tensor(out=k.s5TT[:, g, :], in0=k.ident, scalar=dv[:, g:g + 1], in1=tt1[b_], op0=ALU.mult, op1=ALU.add),
                      r=[ttt[b_], tm], w=[k.t_s5tt])
            else:
                cx.op(V, lambda h, b_=b_, g=g: h.tensor_tensor(out=k.s5TT[:, g, :], in0=k.s5TT[:, g, :], in1=tt1[b_], op=ALU.add),
                      r=[ttt[b_]], w=[k.t_s5tt])
    cx.barrier()
    ar.release(m0)


def load_w_cols(k, wdram, col0, ncols, dst, trk, slot, eng="gpsimd"):
    src = bass.AP(wdram.tensor, wdram.offset + col0, [[wdram.ap[0][0] * 1, 128], [wdram.ap[0][0] * 128, 8], [1, ncols]])
    return k.cx.dma(eng, dst, src, slot, w=[trk])


def proj_fm(k, wt, wtrk, consume):
    cx = k.cx
    for n in range(4):
        pb = k.psum[n % 2 + 2]
        tp = k.tpsum[n % 2 + 2]
        for c in range(8):
            cx.op("tensor", lambda h, pb=pb, c=c, n=n: h.matmul(pb[:, :], wt[:, c, :], k.hT[:, c, n * 512:(n + 1) * 512], start=(c == 0), stop=(c == 7)),
                  r=[wtrk, k.t_hT], w=[tp], inc=(c == 7))
        consume(n, pb, tp)


def s5_build_U(k):
    cx, ar = k.cx, k.ar
    m0 = ar.mark()
    wt = [ar.alloc([8, 128], BF16) for _ in range(2)]
    twt = [Trk(), Trk()]
    swt = [cx.fresh(), cx.fresh()]
    uT = [ar.alloc([2048], BF16) for _ in range(2)]
    tuT = [Trk(), Trk()]
    for ct in range(4):
        b = ct % 2
        load_w_cols(k, k.D["w_in"], ct * 128, 128, wt[b], twt[b], swt[b])

        def consume(n, pb, tp, b=b):
            if n % 2 == 0:
                cx.op("scalar", lambda h: h.copy(uT[b][:, n * 512:(n + 1) * 512], pb[:, :]), r=[tp], w=[tuT[b]])
            else:
                cx.op("vector", lambda h: h.tensor_copy(uT[b][:, n * 512:(n + 1) * 512], pb[:, :]), r=[tp], w=[tuT[b]])
        proj_fm(k, wt[b], twt[b], consume)
        for gi in range(8):
            g = ct * 8 + gi
            q0 = 32 * (gi // 2)
            pb = k.psum[4 + gi % 4]
            tp = k.tpsum[4 + gi % 4]
            for s in range(8):
                rhs = pap(uT[b], q0, 32, s, [[8, 256]])
                cx.op("tensor", lambda h, pb=pb, s=s, rhs=rhs, q0=q0, gi=gi: h.matmul(pb[:, 0:256], k.selT[q0:q0 + 32, gi % 2, s, :], rhs, start=(s == 0), stop=(s == 7), tile_position=(q0, 0)),
                      r=[tuT[b]], w=[tp], inc=(s == 7))
            if gi % 2 == 0:
                cx.op("scalar", lambda h, pb=pb, g=g: h.copy(k.s5U[:, g, :], pb[:, 0:256]), r=[tp], w=[k.t_s5U])
            else:
                cx.op("vector", lambda h, pb=pb, g=g: h.tensor_copy(k.s5U[:, g, :], pb[:, 0:256]), r=[tp], w=[k.t_s5U])
    cx.barrier()
    ar.release(m0)


def s5_main(k, yT, t_yT):
    cx, ar, D = k.cx, k.ar, k.D
    V = "vector"
    m0 = ar.mark()
    SH = ar.alloc([2, 257, 2, 16], BF16)
    tSH = Trk()
    tSHh = Trk()
    X = [ar.alloc([64], F32) for _ in range(3)]
    tX = [Trk() for _ in range(3)]
    t1 = ar.alloc([64], F32)
    t2 = ar.alloc([64], F32)
    tt = Trk()
    cx.op("gpsimd", lambda h: h.memset(SH[:, 0, 0, :, :], 0.0), w=[tSH])
    cx.op("gpsimd", lambda h: h.memset(SH[:, 1, 256, :, :], 0.0), w=[tSH])
    cx.op("gpsimd", lambda h: h.memset(X[0], 0.0), w=[tX[0]])
    n = 0
    for gl in range(16):
        for d in range(2):
            for ri in range(2):
                blk = d * 32 + gl * 2 + ri
                pb = k.psum[n % 4]
                tp = k.tpsum[n % 4]
                cx.op("tensor", lambda h, pb=pb, blk=blk, gl=gl: h.matmul(pb[0:64, 0:256], k.s5AT[:, blk, 0:64], k.s5U[:, gl, :], start=True, stop=True),
                      r=[k.t_s5at, k.t_s5U], w=[tp], inc=False)
                cx.op("tensor", lambda h, pb=pb, blk=blk, gl=gl: h.matmul(pb[64:128, 0:256], k.s5AT[:, blk, 64:128], k.s5U[:, 16 + gl, :], start=True, stop=True),
                      r=[k.t_s5at, k.t_s5U], w=[tp])
                slot0 = 1 if d == 0 else 0
                dstv = pap(SH, 0, 128, d * 257 * 32 + slot0 * 32 + ri * 16 + gl, [[32, 256]])
                if n % 2 == 0:
                    cx.op("scalar", lambda h, dstv=dstv, pb=pb: h.copy(dstv, pb[:, 0:256]), r=[tp], w=[tSH])
                else:
                    cx.op(V, lambda h, dstv=dstv, pb=pb: h.tensor_copy(dstv, pb[:, 0:256]), r=[tp], w=[tSH])
                n += 1
    import os
    S5STOP = os.environ.get('S5_STOP', '')
    if S5STOP == 'a':
        cx.barrier(); ar.release(m0); return
    for i in range(256):
        xp, xn = X[i % 3], X[(i + 1) % 3]
        txp, txn = tX[i % 3], tX[(i + 1) % 3]
        xsw = pap(xp, 0, 128, 32, [[-32, 2], [1, 32]])
        bf = (i + 1) * 32
        bb_ = 257 * 32 + (255 - i) * 32
        sview = pap(SH, 0, 128, bf, [[16, 2], [bb_ - bf, 2], [1, 16]])
        xp3 = xp.rearrange("p (r x) -> p r x", r=2)
        cx.op(V, lambda h, xp=xp: h.tensor_tensor(out=t1, in0=k.s5A1, in1=xp, op=ALU.mult), r=[txp, k.t_s5w], w=[tt])
        cx.op(V, lambda h, xsw=xsw: h.tensor_tensor(out=t2.rearrange("p (r x) -> p r x", r=2), in0=k.s5A2.rearrange("p (r x) -> p r x", r=2), in1=xsw, op=ALU.mult), r=[txp, k.t_s5w], w=[tt])
        cx.op(V, lambda h: h.tensor_tensor(out=t1, in0=t1, in1=t2, op=ALU.add), r=[tt], w=[tt])
        cx.op(V, lambda h, xn=xn, sview=sview: h.tensor_tensor(out=xn.rearrange("p (r d x) -> p r d x", r=2, d=2), in0=t1.rearrange("p (r d x) -> p r d x", r=2, d=2), in1=sview, op=ALU.add),
              r=[tt, tSH], w=[txn])
        cx.op("scalar", lambda h, xn=xn, sview=sview: h.copy(sview, xn.rearrange("p (r d x) -> p r d x", r=2, d=2)), r=[txn], w=[tSHh])
    if S5STOP == 'rec':
        cx.barrier(); ar.release(m0); return
    gT = ar.alloc([4, 2048], F32)
    gTb = ar.alloc([4, 2048], BF16)
    tgT = [Trk() for _ in range(4)]
    tgTb = [Trk() for _ in range(4)]
    ybuf = [k.arA.alloc([8, 256], BF16) for _ in range(2)]
    tyb = [Trk(), Trk()]
    for ct in range(4):
        b = ct % 2
        for gi in range(8):
            g = ct * 8 + gi
            gh, gl = g // 16, g % 16
            pb = k.psum[gi % 2]
            tp = k.tpsum[gi % 2]
            cx.op("tensor", lambda h, pb=pb, g=g: h.matmul(pb[:, 0:256], k.s5TT[:, g, :], k.s5U[:, g, :], start=True, stop=False),
                  r=[k.t_s5tt, k.t_s5U], w=[tp], inc=False)
            for d in range(2):
                for ri in range(2):
                    slot0 = 0 if d == 0 else 1
                    rhs = pap(SH, gh * 64, 64, d * 257 * 32 + slot0 * 32 + ri * 16 + gl, [[32, 256]])
                    last = (d == 1 and ri == 1)
                    cx.op("tensor", lambda h, pb=pb, rhs=rhs, d=d, ri=ri, gh=gh, gl=gl, last=last: h.matmul(
                        pb[:, 0:256], k.s5CS[gh * 64:(gh + 1) * 64, d, gl, ri, :], rhs, start=False, stop=last),
                        r=[tSH, tSHh, k.t_s5w], w=[tp], inc=last)
            if gi % 2 == 0:
                cx.op("scalar", lambda h, pb=pb, b=b, gi=gi: h.copy(ybuf[b][:, gi, :], pb[:, 0:256]), r=[tp], w=[tyb[b]])
            else:
                cx.op(V, lambda h, pb=pb, b=b, gi=gi: h.tensor_copy(ybuf[b][:, gi, :], pb[:, 0:256]), r=[tp], w=[tyb[b]])
        for t in range(8):
            q0 = 32 * (t // 2)
            pb = k.psum[2 + t % 4]
            tp = k.tpsum[2 + t % 4]
            for gi in range(8):
                cx.op("tensor", lambda h, pb=pb, t=t, gi=gi, q0=q0, b=b: h.matmul(pb[:, 0:256], k.selT[q0:q0 + 32, t % 2, gi, :], ybuf[b][q0:q0 + 32, gi, :], start=(gi == 0), stop=(gi == 7), tile_position=(q0, 0)),
                      r=[tyb[b]], w=[tp], inc=(gi == 7))
            dstv = pap(gT, 0, 128, ct * 2048 + t, [[8, 256]])
            cx.op("scalar", lambda h, pb=pb, dstv=dstv: h.activation(dstv, pb[:, 0:256], AF.Gelu), r=[tp], w=[tgT[ct]])
        cx.op("vector", lambda h, ct=ct: h.tensor_copy(gTb[:, ct, :], gT[:, ct, :]), r=[tgT[ct]], w=[tgTb[ct]])
    k.dbg_add("s5_g", gT, tgT)
    if S5STOP == 'c':
        cx.barrier(); ar.release(m0); return
    wg = ar.alloc([4, 512], BF16)
    twg = Trk()
    wsrc = D["s5_w_glu"]
    cx.dma("gpsimd", wg, bass.AP(wsrc.tensor, wsrc.offset, [[512, 128], [512 * 128, 4], [1, 512]]), cx.fresh(), w=[twg])
    bgl = ar.alloc([4], F32)
    nw = ar.alloc([4], F32)
    tb = Trk()
    sl_ = cx.fresh()
    cx.dma("sync", bgl, D["s5_bglu"], sl_, w=[tb])
    cx.dma("sync", nw, D["s5_normw"], sl_, w=[tb])
    sig = ar.alloc([4, 512], BF16)
    tsig = Trk()
    sq = ar.alloc([4, 512], BF16)
    tsq = Trk()
    rs = ar.alloc([512], F32)
    trs = Trk()
    for nck in range(4):
        ts = slice(nck * 512, (nck + 1) * 512)
        for co in range(4):
            pb = k.psum[co % 2]
            tp = k.tpsum[co % 2]
            for ci in range(4):
                cx.op("tensor", lambda h, pb=pb, co=co, ci=ci, ts=ts: h.matmul(pb[:, :], wg[:, ci, co * 128:(co + 1) * 128], gTb[:, ci, ts], start=(ci == 0), stop=(ci == 3)),
                      r=[twg] + tgTb, w=[tp], inc=(ci == 3))
            cx.op("scalar", lambda h, pb=pb, co=co: h.activation(sig[:, co, :], pb[:, :], AF.Sigmoid, bias=bgl[:, co:co + 1]), r=[tp, tb], w=[tsig])
        for co in range(4):
            cx.op(V, lambda h, co=co, ts=ts: h.tensor_tensor(out=gT[:, co, ts], in0=gT[:, co, ts], in1=sig[:, co, :], op=ALU.mult), r=[tsig, tgT[co]], w=[tgT[co]])
            cx.op("scalar", lambda h, co=co, ts=ts: h.activation(sq[:, co, :], gT[:, co, ts], AF.Square), r=[tgT[co]], w=[tsq])
        pb = k.psum[2 + nck % 2]
        tp = k.tpsum[2 + nck % 2]
        for co in range(4):
            cx.op("tensor", lambda h, pb=pb, co=co: h.matmul(pb[:, :], k.onesb, sq[:, co, :], start=(co == 0), stop=(co == 3)), r=[tsq], w=[tp], inc=(co == 3))
        cx.op("scalar", lambda h, pb=pb: h.activation(rs, pb[:, :], AF.Sqrt, scale=1.0 / 512, bias=k.epsc), r=[tp], w=[trs])
        cx.op(V, lambda h: h.reciprocal(rs, rs), r=[trs], w=[trs])
        for co in range(4):
            cx.op(V, lambda h, co=co, ts=ts: h.scalar_tensor_tensor(out=yT[:, co, ts], in0=gT[:, co, ts], scalar=nw[:, co:co + 1], in1=rs, op0=ALU.mult, op1=ALU.mult),
                  r=[tgT[co], trs, tb, tsq], w=[t_yT])
    k.dbg_add("s5_gl", gT, tgT)
    cx.barrier()
    ar.release(m0)


def build(in_shapes, stage="full", dbg_names=(), n_heads=4, n_experts=32):
    nc = bass.Bass("TRN2", target_bir_lowering=False)
    k = K()
    k.n_heads = n_heads
    k.n_experts = n_experts
    k.nc = nc
    D = {}
    for nm, (shape, dt) in in_shapes.items():
        D[nm] = nc.dram_tensor(nm, list(shape), dt, kind="ExternalInput").ap()
    k.D = D
    out = nc.dram_tensor("out", [S, DM], F32, kind="ExternalOutput").ap()
    k.dbg = {}
    k.dbg_req = set(dbg_names)

    with contextlib.ExitStack() as st:
        cx = Ctx(nc, st)
        k.cx = cx

        def finish():
            deps = [(s_.key, s_.total) for s_ in cx.slots if s_.total > 0]
            cx.wait_deps("sync", deps + [(e, cx.cnt[e]) for e in ENGS if e != "sync" and cx.cnt[e] > 0])
            with nc.Block() as block:
                cx.emit_all(block)
            k.n_ops = cx.n_ops
            return nc, k
        k.slot_c = cx.slot("c")
        k.slot_w = cx.slot("w")
        k.slot_x = [cx.slot("x0"), cx.slot("x1")]
        k.slot_o = cx.slot("o")
        k.psum = [cx.ps("ps%d" % i, [128, 512], F32) for i in range(8)]
        k.psum = [p[:, :] for p in k.psum]
        k.tpsum = [Trk("ps%d" % i, excl=True) for i in range(8)]
        k.ident = cx.sb("ident", [128, 128], F32)[:, :]
        k.identb = cx.sb("identb", [128, 128], BF16)[:, :]
        k.ones = cx.sb("ones", [128, 128], F32)[:, :]
        k.onesb = cx.sb("onesb", [128, 128], BF16)[:, :]
        k.epsc = cx.sb("epsc", [128, 1], F32)[:, :]
        k.selT = cx.sb("selT", [128, 2, 8, 128], BF16)[:, :, :, :]
        tc = Trk()
        cx.dma("sync", k.ident, D["ident"], k.slot_c, w=[tc])
        cx.dma("sync", k.identb, D["identb"], k.slot_c, w=[tc])
        cx.dma("sync", k.ones, D["ones"], k.slot_c, w=[tc])
        cx.dma("gpsimd", k.onesb, D["ones"], k.slot_c, w=[tc])
        cx.op("vector", lambda h: h.memset(k.epsc, EPS), w=[tc])
        cx.dma("sync", k.selT, D["selT"], k.slot_c, w=[tc])
        k.s5A1 = cx.sb("s5A1", [128, 64], F32)[:, :]
        k.s5A2 = cx.sb("s5A2", [128, 64], F32)[:, :]
        ar = Arena(cx, 51456)
        k.ar = ar
        cx.barrier()

        def dbg_add(name, ap, trks):
            if name in k.dbg_req:
                shape = list(ap.shape)
                dt_ = F32
                o = nc.dram_tensor("dbg_" + name, shape, dt_, kind="ExternalOutput").ap()
                cx.dma("gpsimd" if ap.dtype != F32 else "sync", o, ap, cx.fresh(), r=list(trks))
        k.dbg_add = dbg_add

        regA = ar.alloc([NT * 1024], F32)
        arA = Arena(cx, NT * 1024, base=regA)
        k.arA = arA
        yT = ar.alloc([8, 2048], BF16)
        t_yT = Trk()
        k.s5U = arA.alloc([32, 256], BF16)
        k.t_s5U = Trk()
        m_h = arA.mark()
        k.hT = arA.alloc([8, 2048], BF16)
        k.t_hT = Trk()

        m1 = ar.mark()
        xt = [ar.alloc([1024], F32) for _ in range(2)]
        txt = [Trk(), Trk()]

        def src_x(i):
            b = i % 2
            cx.dma("sync", xt[b], D["x"][i * 128:(i + 1) * 128, :], k.slot_x[b], w=[txt[b]])
            return xt[b], txt[b]
        norm_transpose(k, "mix", src_x, NT, D["norm_mix"], k.hT, BF16, k.t_hT)
        cx.barrier()
        ar.release(m1)

        if stage == 'p1':
            return finish()
        s5_build_U(k)
        if stage == 'U':
            return finish()
        mg = ar.mark()
        gdn_setup(k)
        for hd in range(k.n_heads):
            gdn_head(k, hd, yT, t_yT)
        ar.release(mg)
        k.dbg_add("ygdnT", yT[:, 4:8, :], [t_yT])
        if stage == 'gdn':
            return finish()
        cx.barrier()
        arA.release(m_h)
        k.s5AT = arA.alloc([64, 128], BF16)
        k.t_s5at = Trk()
        k.s5CS = arA.alloc([2, 16, 2, 128], BF16)
        k.s5TT = arA.alloc([32, 128], BF16)
        k.t_s5tt = Trk()
        s5_prep(k)
        if stage == 's5prep':
            return finish()
        s5_main(k, yT[:, 0:4, :], t_yT)
        k.dbg_add("ys5T", yT[:, 0:4, :], [t_yT])
        if stage == "s5":
            return finish()
        if True:
            cx.barrier()
            k.xacc = regA.rearrange('p (a b) -> p a b', a=NT)
            k.txacc = [Trk() for _ in range(NT)]
            out_proj(k, yT, t_yT)
            k.dbg_add("x1", k.xacc, k.txacc)
            if stage == 'oproj':
                return finish()
            xattn(k)
            if stage == 'xattn':
                return finish()
            k.dbg_add("x2", k.xacc, k.txacc)
            moe(k)
            k.dbg_add("x3", k.xacc, k.txacc + [t_ for p_ in k.txh for t_ in p_])
            final_norm(k, out)

        return finish()


def host_inputs(inp, b):
    m = {}
    m["x"] = np.ascontiguousarray(inp["x"][b])
    m["mem"] = np.ascontiguousarray(inp["mem"][b])
    m["norm_mix"] = inp["norm_mix"][0]
    m["w_in"] = inp["w_in"][0]
    m["w_out"] = inp["w_out"][0]
    m["s5_w_glu"] = inp["s5_w_glu"][0]
    m.update(host_s5(inp))
    cv = inp["gdn_conv"][0]
    m["gdn_convw"] = np.ascontiguousarray(cv.reshape(5, 3, 4, 128).transpose(3, 2, 1, 0))
    for nm in ("gdn_a_log_f", "gdn_dt_bias_f", "gdn_a_log_b", "gdn_dt_bias_b"):
        m[nm] = inp[nm][0]
    m["gdn_norm"] = inp["gdn_norm"][0]
    for nm in ("norm_xattn", "norm_mem", "xa_wq", "xa_wk", "xa_wv", "xa_wo", "norm_moe", "router_group_w", "router_group_b",
               "router_expert_w", "router_expert_b", "moe_w_gate", "moe_w_up", "moe_w_down"):
        m[nm] = inp[nm][0]
    m["norm_final"] = inp["norm_final"]
    m.update(host_consts())
    return m


def gdn_setup(k):
    cx, ar, D = k.cx, k.ar, k.D
    V = "vector"
    G = K()
    k.G = G
    G.mask = ar.alloc([8, 128], F32)
    G.tmask = Trk()
    cx.dma("sync", G.mask, D["gmask"], cx.fresh(), w=[G.tmask])
    wsm = ar.alloc([8, 16], BF16)
    tw = Trk()
    load_w_cols(k, D["w_in"], 2560, 16, wsm, tw, cx.fresh())
    BA = ar.alloc([16, 16], F32)
    tBA = Trk()
    for i in range(NT):
        pb = k.psum[i % 4]
        tp = k.tpsum[i % 4]
        for c in range(8):
            cx.op("tensor", lambda h, pb=pb, c=c, i=i: h.matmul(pb[:, 0:16], k.hT[:, c, i * 128:(i + 1) * 128], wsm[:, c, :], start=(c == 0), stop=(c == 7)),
                  r=[tw, k.t_hT], w=[tp], inc=(c == 7))
        cx.op("scalar", lambda h, pb=pb, i=i: h.copy(BA[:, i, :], pb[:, 0:16]), r=[tp], w=[tBA])
    pr = ar.alloc([4, 4], F32)
    tpr = Trk()
    sl_ = cx.fresh()
    for j, nm in enumerate(("gdn_a_log_f", "gdn_dt_bias_f", "gdn_a_log_b", "gdn_dt_bias_b")):
        cx.dma("sync", pr[:, j, :], dram_bcast(D[nm], 128, 4), sl_, w=[tpr])
    G.nw = ar.alloc([128], F32)
    cx.dma("sync", G.nw, dram_bcast(D["gdn_norm"], 128, 128), sl_, w=[tpr])
    G.tpr = tpr
    T = Trk()
    G.T = T
    G.beta, G.nb, G.gc, G.eg, G.neg, G.ed = [], [], [], [], [], []
    def per_dir(d):
        beta = ar.alloc([16, 4], F32)
        nb = ar.alloc([16, 4], F32)
        g = ar.alloc([16, 4], F32)
        gc = ar.alloc([16, 4], F32)
        gt = ar.alloc([16, 4], F32)
        eg = ar.alloc([16, 4], F32)
        neg = ar.alloc([16, 4], F32)
        ed = ar.alloc([16, 4], F32)
        ea = ar.alloc([4], F32)
        braw = BA[:, :, d * 4:(d + 1) * 4]
        araw = BA[:, :, 8 + d * 4:8 + (d + 1) * 4]
        cx.op("scalar", lambda h: h.activation(beta, braw, AF.Sigmoid), r=[tBA, T], w=[T])
        cx.op(V, lambda h: h.tensor_scalar(nb, beta, -1.0, None, op0=ALU.mult), r=[T], w=[T])
        cx.op("scalar", lambda h: h.activation(ea, pr[:, 2 * d, :], AF.Exp), r=[tpr, T], w=[T])
        cx.op(V, lambda h: h.tensor_tensor(out=g, in0=araw, in1=pr[:, 2 * d + 1, :].unsqueeze(1).to_broadcast([128, 16, 4]), op=ALU.add), r=[tBA, tpr, T], w=[T])
        cx.op("scalar", lambda h: h.activation(g, g, AF.Exp), r=[T], w=[T])
        cx.op("scalar", lambda h: h.activation(g, g, AF.Ln, bias=1.0), r=[T], w=[T])
        cx.op(V, lambda h: h.scalar_tensor_tensor(out=g, in0=g, scalar=-1.0, in1=ea.unsqueeze(1).to_broadcast([128, 16, 4]), op0=ALU.mult, op1=ALU.mult), r=[T], w=[T])
        g2 = g.rearrange("p a b -> p (a b)")
        pb = k.psum[4 + d]
        tp = k.tpsum[4 + d]
        cx.op("tensor", lambda h, pb=pb, d=d: h.matmul(pb[:, 0:64], G.mask[:, d, :], g2, start=True, stop=True), r=[T, G.tmask], w=[tp])
        cx.op("tensor", lambda h, pb=pb: h.matmul(pb[:, 64:128], G.mask[:, 6, :], g2, start=True, stop=True), r=[T, G.tmask], w=[tp])
        cx.op(V, lambda h, pb=pb: h.tensor_copy(gc.rearrange("p a b -> p (a b)"), pb[:, 0:64]), r=[tp], w=[T])
        cx.op(V, lambda h, pb=pb: h.tensor_tensor(out=gt.rearrange("p a b -> p (a b)"), in0=pb[:, 64:128], in1=gc.rearrange("p a b -> p (a b)"), op=ALU.subtract), r=[tp, T], w=[T])
        cx.op("scalar", lambda h: h.activation(eg, gc, AF.Exp), r=[T], w=[T])
        cx.op("scalar", lambda h: h.activation(ed, gt, AF.Exp), r=[T], w=[T])
        cx.op(V, lambda h: h.tensor_scalar(neg, eg, -1.0, None, op0=ALU.mult), r=[T], w=[T])
        G.g = getattr(G, "g", []) + [g]
        G.beta.append(beta); G.nb.append(nb); G.gc.append(gc); G.eg.append(eg); G.neg.append(neg); G.ed.append(ed)
    per_dir(0)
    per_dir(1)
    G.osum = ar.alloc([16, 128], F32)
    G.tosum = [Trk() for _ in range(NT)]


def gdn_head(k, hd, yT, t_yT):
    cx, ar, D, G = k.cx, k.ar, k.D, k.G
    V = "vector"
    m0 = ar.mark()
    qnT = ar.alloc([2048], BF16)
    knT = ar.alloc([2048], BF16)
    Ktok = ar.alloc([16, 128], BF16)
    Vtok = ar.alloc([16, 128], BF16)
    tq, tk_, tKt, tVt = Trk(), Trk(), Trk(), Trk()
    wz = ar.alloc([8, 128], BF16)
    twz = Trk()
    load_w_cols(k, D["w_in"], 512 + 1536 + hd * 128, 128, wz, twz, cx.fresh())
    mA = ar.mark()
    w3 = [ar.alloc([8, 128], BF16) for _ in range(3)]
    tw3 = [Trk() for _ in range(3)]
    for j in range(3):
        load_w_cols(k, D["w_in"], 512 + j * 512 + hd * 128, 128, w3[j], tw3[j], cx.fresh())
    cw = ar.alloc([3, 5], F32)
    tcw = Trk()
    cx.dma("sync", cw, D["gdn_convw"][:, hd, :, :], cx.fresh(), w=[tcw])
    diag = ar.alloc([15, 128], BF16)
    tdg = Trk()
    for j in range(3):
        for t in range(5):
            cx.op("vector", lambda h, j=j, t=t: h.tensor_scalar(diag[:, j * 5 + t, :], k.identb, cw[:, j, t:t + 1], None, op0=ALU.mult), r=[tcw], w=[tdg])
    import os
    ALV = int(os.environ.get("GDN_ALV", "9"))
    if ALV == 0:
        cx.barrier(); ar.release(m0); return
    raw = [ar.alloc([2052], BF16) for _ in range(2)]
    traw = [Trk(), Trk()]
    for b in range(2):
        cx.op("gpsimd", lambda h, b=b: h.memset(raw[b][:, 0:2], 0.0), w=[traw[b]])
        cx.op("gpsimd", lambda h, b=b: h.memset(raw[b][:, 2050:2052], 0.0), w=[traw[b]])
    act = ar.alloc([2048], F32)
    tact = Trk()
    vT = ar.alloc([2048], BF16)
    tvT = Trk()
    sqb = ar.alloc([2048], BF16)
    tsqb = Trk()
    rn = [ar.alloc([512], F32) for _ in range(2)]
    trn = [Trk(), Trk()]
    if ALV == 1:
        cx.barrier(); ar.release(m0); return
    for j in range(3):
        b = j % 2

        def consume(n, pb, tp, b=b):
            cx.op(V if n % 2 else "scalar", (lambda h: h.tensor_copy(raw[b][:, 2 + n * 512:2 + (n + 1) * 512], pb[:, :])) if n % 2 else
                  (lambda h: h.copy(raw[b][:, 2 + n * 512:2 + (n + 1) * 512], pb[:, :])), r=[tp], w=[traw[b]])
        proj_fm(k, w3[j], tw3[j], consume)
        for n in range(4):
            pb = k.psum[4 + n % 2]
            tp = k.tpsum[4 + n % 2]
            for t in range(5):
                cx.op("tensor", lambda h, pb=pb, t=t, n=n, j=j, b=b: h.matmul(pb[:, :], diag[:, j * 5 + t, :], raw[b][:, n * 512 + t:n * 512 + t + 512], start=(t == 0), stop=(t == 4)),
                      r=[tdg, traw[b]], w=[tp], inc=(t == 4))
            ts = slice(n * 512, (n + 1) * 512)
            if j == 2:
                cx.op("scalar", lambda h, pb=pb, ts=ts: h.activation(vT[:, ts], pb[:, :], AF.Silu), r=[tp], w=[tvT])
            elif ALV == 2:
                cx.op("scalar", lambda h, pb=pb, ts=ts: h.activation(act[:, ts], pb[:, :], AF.Silu), r=[tp], w=[tact])
            else:
                cx.op("scalar", lambda h, pb=pb, ts=ts: h.activation(act[:, ts], pb[:, :], AF.Silu), r=[tp], w=[tact])
                cx.op("scalar", lambda h, ts=ts: h.activation(sqb[:, ts], act[:, ts], AF.Square), r=[tact], w=[tsqb])
                pb2 = k.psum[6 + n % 2]
                tp2 = k.tpsum[6 + n % 2]
                cx.op("tensor", lambda h, pb2=pb2, ts=ts: h.matmul(pb2[:, :], k.onesb, sqb[:, ts], start=True, stop=True), r=[tsqb], w=[tp2])
                rb = n % 2
                cx.op("scalar", lambda h, pb2=pb2, rb=rb: h.activation(rn[rb], pb2[:, :], AF.Sqrt, bias=k.epsc), r=[tp2], w=[trn[rb]])
                cx.op(V, lambda h, rb=rb: h.reciprocal(rn[rb], rn[rb]), r=[trn[rb]], w=[trn[rb]])
                dstT, tdst, scl = (qnT, tq, 128.0 ** -0.5) if j == 0 else (knT, tk_, 1.0)
                cx.op(V, lambda h, ts=ts, rb=rb, dstT=dstT, scl=scl: h.scalar_tensor_tensor(out=dstT[:, ts], in0=act[:, ts], scalar=scl, in1=rn[rb], op0=ALU.mult, op1=ALU.mult),
                      r=[tact, trn[rb]], w=[tdst])
    if ALV <= 3:
        cx.barrier(); ar.release(m0); return
    TV = int(os.environ.get("GDN_TV", "0"))
    for i in range(NT):
        pb = k.psum[i % 2].bitcast(BF16)
        tp = k.tpsum[i % 2]
        if TV == 0:
            cx.op("tensor", lambda h, pb=pb, i=i: h.transpose(pb[:, 0:128], knT[:, i * 128:(i + 1) * 128], k.identb), r=[tk_], w=[tp])
            cx.op("tensor", lambda h, pb=pb, i=i: h.transpose(pb[:, 128:256], vT[:, i * 128:(i + 1) * 128], k.identb), r=[tvT], w=[tp])
            cx.op("scalar", lambda h, pb=pb, i=i: h.copy(Ktok[:, i, :], pb[:, 0:128]), r=[tp], w=[tKt])
            cx.op(V, lambda h, pb=pb, i=i: h.tensor_copy(Vtok[:, i, :], pb[:, 128:256]), r=[tp], w=[tVt])
        elif TV == 1:
            cx.op("tensor", lambda h, pb=pb, i=i: h.transpose(pb[:, 0:128], knT[:, i * 128:(i + 1) * 128], k.identb), r=[tk_], w=[tp])
            cx.op("scalar", lambda h, pb=pb, i=i: h.copy(Ktok[:, i, :], pb[:, 0:128]), r=[tp], w=[tKt])
        elif TV == 2:
            cx.op("tensor", lambda h, pb=pb, i=i: h.transpose(pb[:, 0:128], vT[:, i * 128:(i + 1) * 128], k.identb), r=[tvT], w=[tp])
            cx.op(V, lambda h, pb=pb, i=i: h.tensor_copy(Vtok[:, i, :], pb[:, 0:128]), r=[tp], w=[tVt])
    if hd == 0:
        k.dbg_add("gdn_qn", qnT, [tq])
        k.dbg_add("gdn_kn", knT, [tk_])
        k.dbg_add("gdn_vtok", Vtok, [tVt])
    cx.barrier()
    ar.release(mA)
    STOP = os.environ.get("GDN_STOP", "")
    if STOP == "A":
        ar.release(m0)
        return
    qgT = [ar.alloc([2048], BF16) for _ in range(2)]
    Kd = [ar.alloc([16, 128], BF16) for _ in range(2)]
    Pm = [ar.alloc([16, 128], BF16) for _ in range(2)]
    QKm = [ar.alloc([16, 128], BF16) for _ in range(2)]
    etot = [ar.alloc([32], F32) for _ in range(2)]
    tqg = [[Trk() for _ in range(NT)] for _ in range(2)]
    tKd = [[Trk() for _ in range(NT)] for _ in range(2)]
    tPm = [[Trk() for _ in range(NT)] for _ in range(2)]
    tQK = [[Trk() for _ in range(NT)] for _ in range(2)]
    tet = [[Trk() for _ in range(NT)] for _ in range(2)]
    NI = 8
    mN = ar.mark()
    NDT = BF16 if os.environ.get('GDN_NEU', 'bf16') == 'bf16' else F32
    nid = k.identb if NDT == BF16 else k.ident
    Xb = [[ar.alloc([128], NDT) for _ in range(2)] for _ in range(NI)]
    XTb = [[ar.alloc([128], NDT) for _ in range(2)] for _ in range(NI)]
    Pb = [[ar.alloc([128], NDT) for _ in range(2)] for _ in range(NI)]

    def tview(pn_, c0):
        return pn_[:, c0:c0 + 128] if NDT == F32 else pn_.bitcast(BF16)[:, 2 * c0:2 * c0 + 128]
    tX = [Trk() for _ in range(NI)]
    EGB = [ar.alloc([128], F32) for _ in range(NI)]
    ET = [ar.alloc([128], F32) for _ in range(NI)]
    ETs = ET
    tE = [Trk() for _ in range(NI)]
    tEG = [Trk() for _ in range(NI)]
    tXP = [Trk() for _ in range(NI)]
    insts = [(i, d) for i in range(NT) for d in range(2)]
    for g0 in range(0, len(insts), NI):
        grp = insts[g0:g0 + NI]
        info = []
        for s_, (i, d) in enumerate(grp):
            info.append(dict(s_=s_, i=i, d=d, tsl=slice(i * 128, (i + 1) * 128),
                             col=pap(G.g[d], 0, 128, i * 4 + hd, [[0, 128]]),
                             gcc=G.gc[d][:, i, hd:hd + 1], nbc=G.nb[d][:, i, hd:hd + 1], edc=G.ed[d][:, i, hd:hd + 1],
                             pb=k.psum[s_], tp=k.tpsum[s_]))
        for q_ in info:
            s_, i, d, tsl, col, pb, tp = q_["s_"], q_["i"], q_["d"], q_["tsl"], q_["col"], q_["pb"], q_["tp"]
            cx.op("tensor", lambda h, pb=pb, col=col, d=d: h.matmul(pb[:, 0:128], col, G.mask[:, d, :], start=True, stop=True), r=[G.T, G.tmask], w=[tp], inc=False)
            cx.op("tensor", lambda h, pb=pb, tsl=tsl: h.matmul(pb[:, 128:256], knT[:, tsl], knT[:, tsl], start=True, stop=True), r=[tk_], w=[tp], inc=False)
            cx.op("tensor", lambda h, pb=pb, tsl=tsl: h.matmul(pb[:, 256:384], knT[:, tsl], qnT[:, tsl], start=True, stop=True), r=[tk_, tq], w=[tp])
        for q_ in info:
            s_, i, d, pb, tp, gcc = q_["s_"], q_["i"], q_["d"], q_["pb"], q_["tp"], q_["gcc"]
            cx.op("scalar", lambda h, pb=pb, s_=s_: h.activation(EGB[s_], pb[:, 0:128], AF.Exp), r=[tp], w=[tEG[s_]])
            cx.op(V, lambda h, pb=pb, s_=s_, gcc=gcc, d=d: h.scalar_tensor_tensor(out=ET[s_], in0=pb[:, 0:128], scalar=gcc, in1=G.mask[:, 2 + d, :], op0=ALU.subtract, op1=ALU.min),
                  r=[tp, G.T, G.tmask], w=[tE[s_]])
        for q_ in info:
            s_, i, d, tsl = q_["s_"], q_["i"], q_["d"], q_["tsl"]
            cx.op("scalar", lambda h, s_=s_: h.activation(ET[s_], ET[s_], AF.Exp), r=[tE[s_]], w=[tE[s_]])
            cx.op(V, lambda h, s_=s_, tsl=tsl, d=d: h.tensor_tensor(out=qgT[d][:, tsl], in0=qnT[:, tsl], in1=EGB[s_], op=ALU.mult), r=[tq, tEG[s_]], w=[tqg[d][i]])
        for q_ in info:
            s_, i, d, pb, tp, edc = q_["s_"], q_["i"], q_["d"], q_["pb"], q_["tp"], q_["edc"]
            c0, c1 = (63, 127) if d == 0 else (0, 64)
            cx.op("scalar", lambda h, s_=s_, d=d, i=i, c0=c0: h.copy(etot[d][:, 2 * i:2 * i + 1], EGB[s_][:, c0:c0 + 1]), r=[tEG[s_]], w=[tet[d][i]])
            cx.op("scalar", lambda h, s_=s_, d=d, i=i, c1=c1: h.copy(etot[d][:, 2 * i + 1:2 * i + 2], EGB[s_][:, c1:c1 + 1]), r=[tEG[s_]], w=[tet[d][i]])
            cx.op("scalar", lambda h, d=d, i=i, edc=edc: h.activation(Kd[d][:, i, :], Ktok[:, i, :], AF.Identity, scale=edc), r=[tKt, G.T], w=[tKd[d][i]])
            cx.op(V, lambda h, pb=pb, s_=s_, d=d, i=i: h.tensor_tensor(out=QKm[d][:, i, :], in0=pb[:, 256:384], in1=ET[s_], op=ALU.mult), r=[tp, tE[s_]], w=[tQK[d][i]])
        for q_ in info:
            s_, d = q_["s_"], q_["d"]
            cx.op(V, lambda h, s_=s_, d=d: h.tensor_tensor(out=ETs[s_], in0=ET[s_], in1=G.mask[:, 4 + d, :], op=ALU.mult), r=[tE[s_], G.tmask], w=[tE[s_]])
        for q_ in info:
            s_, pb, tp, nbc = q_["s_"], q_["pb"], q_["tp"], q_["nbc"]
            cx.op(V, lambda h, pb=pb, s_=s_, nbc=nbc: h.scalar_tensor_tensor(out=Xb[s_][0], in0=pb[:, 128:256], scalar=nbc, in1=ETs[s_], op0=ALU.mult, op1=ALU.mult),
                  r=[tp, tE[s_], G.T], w=[tX[s_]])
        for q_ in info:
            s_, pn, tn = q_["s_"], q_["pb"], q_["tp"]
            cx.op("tensor", lambda h, pn=pn, s_=s_: h.transpose(tview(pn, 384), Xb[s_][0], nid), r=[tX[s_]], w=[tn])
            cx.op(V, lambda h, s_=s_: h.tensor_tensor(out=Pb[s_][0], in0=Xb[s_][0], in1=nid, op=ALU.add), r=[tX[s_]], w=[tXP[s_]])
            cx.op("scalar", lambda h, pn=pn, s_=s_: h.copy(XTb[s_][0], tview(pn, 384)), r=[tn], w=[tX[s_]])
        for L in range(1, 6):
            a, b_ = (L - 1) % 2, L % 2
            for s_, (i, d) in enumerate(grp):
                pn = k.psum[s_]
                tn = k.tpsum[s_]
                if L < 5:
                    cx.op("tensor", lambda h, pn=pn, s_=s_, a=a: h.matmul(pn[:, 0:128], XTb[s_][a], Xb[s_][a], start=True, stop=True), r=[tX[s_]], w=[tn], inc=False)
                cx.op("tensor", lambda h, pn=pn, s_=s_, a=a: h.matmul(pn[:, 128:256], Xb[s_][a], XTb[s_][a], start=True, stop=True), r=[tX[s_]], w=[tn])
                e1, e2 = ("scalar", V) if s_ % 2 == 0 else (V, "scalar")
                if L < 5:
                    if e1 == "scalar":
                        cx.op("scalar", lambda h, pn=pn, s_=s_, b_=b_: h.copy(Xb[s_][b_], pn[:, 0:128]), r=[tn], w=[tX[s_]])
                    else:
                        cx.op(V, lambda h, pn=pn, s_=s_, b_=b_: h.tensor_copy(Xb[s_][b_], pn[:, 0:128]), r=[tn], w=[tX[s_]])
                if e2 == "scalar":
                    cx.op("scalar", lambda h, pn=pn, s_=s_, b_=b_: h.copy(XTb[s_][b_], pn[:, 128:256]), r=[tn], w=[tX[s_]])
                else:
                    cx.op(V, lambda h, pn=pn, s_=s_, b_=b_: h.tensor_copy(XTb[s_][b_], pn[:, 128:256]), r=[tn], w=[tX[s_]])
            for s_, (i, d) in enumerate(grp):
                pn = k.psum[s_]
                tn = k.tpsum[s_]
                cx.op("tensor", lambda h, pn=pn, s_=s_, a=a, b_=b_: h.matmul(pn[:, 256:384], XTb[s_][b_], Pb[s_][a], start=True, stop=True), r=[tX[s_], tXP[s_]], w=[tn])
                if L < 5:
                    cx.op(V, lambda h, pn=pn, s_=s_, a=a, b_=b_: h.tensor_tensor(out=Pb[s_][b_], in0=pn[:, 256:384], in1=Pb[s_][a], op=ALU.add), r=[tn, tXP[s_]], w=[tXP[s_]])
                else:
                    cx.op(V, lambda h, pn=pn, s_=s_, a=a, d=d, i=i: h.tensor_tensor(out=Pm[d][:, i, :], in0=pn[:, 256:384], in1=Pb[s_][a], op=ALU.add), r=[tn, tXP[s_]], w=[tPm[d][i], tXP[s_]])
    if STOP == "B":
        cx.barrier()
        ar.release(m0)
        return
    ar.release(mN)
    Sf = [[ar.alloc([128], F32) for _ in range(2)] for _ in range(2)]
    Sb = [ar.alloc([128], BF16) for _ in range(2)]
    Rp = [ar.alloc([128], BF16) for _ in range(2)]
    vn = [ar.alloc([128], BF16) for _ in range(2)]
    tS = [Trk(), Trk()]
    tR = [Trk(), Trk()]
    tv = [Trk(), Trk()]
    cx.op("gpsimd", lambda h: h.memset(G.osum, 0.0), w=G.tosum)
    for d in range(2):
        cx.op("gpsimd", lambda h, d=d: h.memset(Sf[d][0], 0.0), w=[tS[d]])
        cx.op("gpsimd", lambda h, d=d: h.memset(Sb[d], 0.0), w=[tS[d]])
        cx.op("gpsimd", lambda h, d=d: h.memset(Rp[d], 0.0), w=[tR[d]])
        cx.op("gpsimd", lambda h, d=d: h.memset(vn[d], 0.0), w=[tv[d]])
    for step in range(32):
        for d in range(2):
            if d == 0:
                i, hh = step // 2, step % 2
            else:
                i, hh = 15 - step // 2, 1 - step % 2
            tsl = slice(i * 128, (i + 1) * 128)
            ps_ = slice(hh * 64, (hh + 1) * 64)
            cur, nxt = step % 2, (step + 1) % 2
            pcs = [k.psum[4 * d + q_] for q_ in range(4)]
            tcs = [k.tpsum[4 * d + q_] for q_ in range(4)]
            negc = G.neg[d][ps_, i, hd:hd + 1]
            btc = G.beta[d][ps_, i, hd:hd + 1]
            p1, pv_, po_, pst = pcs
            t1_, tv_, to_, tst = tcs
            cx.op("tensor", lambda h, p1=p1, tsl=tsl, d=d: h.matmul(p1[:, 0:128], knT[:, tsl], Sb[d], start=True, stop=True), r=[tk_, tS[d]], w=[t1_])
            cx.op(V, lambda h, p1=p1, ps_=ps_, negc=negc, d=d, i=i: h.scalar_tensor_tensor(out=Rp[d][ps_, :], in0=p1[ps_, 0:128], scalar=negc, in1=Vtok[ps_, i, :], op0=ALU.mult, op1=ALU.add),
                  r=[t1_, tVt, G.T], w=[tR[d]])
            cx.op("tensor", lambda h, pv_=pv_, ps_=ps_, d=d, i=i: h.matmul(pv_[:, 0:128], Pm[d][ps_, i, :], Rp[d][ps_, :], start=True, stop=True), r=[tPm[d][i], tR[d]], w=[tv_])
            cx.op("scalar", lambda h, pv_=pv_, ps_=ps_, btc=btc, d=d: h.activation(vn[d][ps_, :], pv_[ps_, 0:128], AF.Identity, scale=btc), r=[tv_, G.T], w=[tv[d]])
            cx.op("tensor", lambda h, po_=po_, tsl=tsl, d=d: h.matmul(po_[:, 0:128], qgT[d][:, tsl], Sb[d], start=True, stop=False), r=[tqg[d][i], tS[d]], w=[to_], inc=False)
            cx.op("tensor", lambda h, po_=po_, ps_=ps_, d=d, i=i: h.matmul(po_[:, 0:128], QKm[d][ps_, i, :], vn[d][ps_, :], start=False, stop=True), r=[tQK[d][i], tv[d]], w=[to_])
            cx.op("tensor", lambda h, pst=pst, ps_=ps_, d=d, i=i: h.matmul(pst[:, 0:128], Kd[d][ps_, i, :], vn[d][ps_, :], start=True, stop=True), r=[tKd[d][i], tv[d]], w=[tst])
            cx.op("gpsimd" if False else V, lambda h, po_=po_, ps_=ps_, i=i: h.tensor_tensor(out=G.osum[ps_, i, :], in0=po_[ps_, 0:128], in1=G.osum[ps_, i, :], op=ALU.add), r=[to_, G.tosum[i]], w=[G.tosum[i]])
            etc = etot[d][:, 2 * i + hh:2 * i + hh + 1]
            cx.op(V, lambda h, pst=pst, d=d, cur=cur, nxt=nxt, etc=etc: h.scalar_tensor_tensor(out=Sf[d][nxt], in0=Sf[d][cur], scalar=etc, in1=pst[:, 0:128], op0=ALU.mult, op1=ALU.add),
                  r=[tst, tet[d][i], tS[d]], w=[tS[d]])
            cx.op("scalar", lambda h, d=d, nxt=nxt: h.copy(Sb[d], Sf[d][nxt]), r=[tS[d]], w=[tS[d]])
    if hd == 0:
        k.dbg_add("gdn_osum", G.osum, G.tosum)
    if STOP == "C":
        cx.barrier()
        ar.release(m0)
        return
    ss = ar.alloc([NT, 2], F32)
    tss = Trk()
    junk = ar.alloc([128], BF16)
    zs = [ar.alloc([128], F32) for _ in range(2)]
    tzs = [Trk(), Trk()]
    yb = [ar.alloc([128], BF16) for _ in range(2)]
    tyb = [Trk(), Trk()]
    for i in range(NT):
        cx.op("scalar", lambda h, i=i: h.activation(junk, G.osum[:, i, :], AF.Square, accum_out=ss[:, i, 0:1]), r=[G.tosum[i], tss], w=[tss])
    cx.op(V, lambda h: h.tensor_scalar(ss[:, :, 1:2], ss[:, :, 0:1], 1.0 / 128, EPS, op0=ALU.mult, op1=ALU.add), r=[tss], w=[tss])
    cx.op("scalar", lambda h: h.activation(ss[:, :, 1:2], ss[:, :, 1:2], AF.Sqrt), r=[tss], w=[tss])
    cx.op(V, lambda h: h.reciprocal(ss[:, :, 1:2], ss[:, :, 1:2]), r=[tss], w=[tss])
    for i in range(NT):
        b = i % 2
        pz = k.psum[b]
        tz = k.tpsum[b]
        for c in range(8):
            cx.op("tensor", lambda h, pz=pz, c=c, i=i: h.matmul(pz[:, 0:128], k.hT[:, c, i * 128:(i + 1) * 128], wz[:, c, :], start=(c == 0), stop=(c == 7)),
                  r=[twz, k.t_hT], w=[tz], inc=(c == 7))
        cx.op("scalar", lambda h, pz=pz, b=b: h.activation(zs[b], pz[:, 0:128], AF.Silu), r=[tz], w=[tzs[b]])
        s1 = ss[:, i, 1:2]
        cx.op(V, lambda h, i=i, s1=s1: h.scalar_tensor_tensor(out=G.osum[:, i, :], in0=G.osum[:, i, :], scalar=s1, in1=G.nw, op0=ALU.mult, op1=ALU.mult), r=[tss, G.tpr, G.tosum[i]], w=[G.tosum[i]])
        cx.op(V, lambda h, i=i, b=b: h.tensor_tensor(out=yb[b], in0=G.osum[:, i, :], in1=zs[b], op=ALU.mult), r=[G.tosum[i], tzs[b]], w=[tyb[b]])
        pt = k.psum[2 + b].bitcast(BF16)
        tt_ = k.tpsum[2 + b]
        cx.op("tensor", lambda h, pt=pt, b=b: h.transpose(pt[:, 0:128], yb[b], k.identb), r=[tyb[b]], w=[tt_])
        cx.op("scalar", lambda h, pt=pt, i=i: h.copy(yT[:, 4 + hd, i * 128:(i + 1) * 128], pt[:, 0:128]), r=[tt_], w=[t_yT])
    cx.barrier()
    ar.release(m0)


def out_proj(k, yT, t_yT):
    cx, ar, D = k.cx, k.ar, k.D
    m0 = ar.mark()
    wo = ar.alloc([8, 1024], BF16)
    two = Trk()
    wsrc = D["w_out"]
    sl_ = cx.fresh()
    for c in range(8):
        cx.dma("gpsimd", wo[:, c, :], wsrc[c * 128:(c + 1) * 128, :], sl_, w=[two])
    slx = cx.fresh()
    for i in range(NT):
        cx.dma("sync", k.xacc[:, i, :], D["x"][i * 128:(i + 1) * 128, :], slx, w=[k.txacc[i]])
    for i in range(NT):
        k.txacc[i].w = (slx.key, slx.total)
    for i in range(NT):
        for half in range(2):
            pb = k.psum[(2 * i + half) % 4]
            tp = k.tpsum[(2 * i + half) % 4]
            for c in range(8):
                cx.op("tensor", lambda h, pb=pb, c=c, i=i, half=half: h.matmul(pb[:, :], yT[:, c, i * 128:(i + 1) * 128], wo[:, c, half * 512:(half + 1) * 512], start=(c == 0), stop=(c == 7)),
                      r=[t_yT, two], w=[tp], inc=(c == 7))
            xs = k.xacc[:, i, half * 512:(half + 1) * 512]
            cx.op("vector", lambda h, pb=pb, xs=xs: h.tensor_tensor(out=xs, in0=pb[:, :], in1=xs, op=ALU.add), r=[tp, k.txacc[i]], w=[k.txacc[i]])
    cx.barrier()
    ar.release(m0)


def xattn(k):
    cx, ar, D = k.cx, k.ar, k.D
    V = "vector"
    m0 = ar.mark()
    xnT = ar.alloc([8, 2048], BF16)
    t_xnT = Trk()
    memT = ar.alloc([8, 256], BF16)
    t_memT = Trk()
    m1 = ar.mark()
    mt = [ar.alloc([1024], F32) for _ in range(2)]
    tmt = [Trk(), Trk()]

    def src_mem(i):
        cx.dma("sync", mt[i], D["mem"][i * 128:(i + 1) * 128, :], cx.fresh(), w=[tmt[i]])
        return mt[i], tmt[i]
    norm_transpose(k, "mem", src_mem, 2, D["norm_mem"], memT, BF16, t_memT)
    ar.release(m1)
    norm_transpose(k, "xa", lambda i: (k.xacc[:, i, :], k.txacc[i]), NT, D["norm_xattn"], xnT, BF16, t_xnT)
    k.dbg_add("xa_memT", memT, [t_memT])
    k.dbg_add("xa_xnT", xnT, [t_xnT])
    wq = [ar.alloc([8, 256], BF16) for _ in range(2)]
    wk = [ar.alloc([8, 256], BF16) for _ in range(2)]
    wv = [ar.alloc([8, 256], BF16) for _ in range(2)]
    wo = [ar.alloc([2, 1024], BF16) for _ in range(2)]
    tw = [Trk(), Trk()]
    sw = [cx.slot("xw0"), cx.slot("xw1")]
    kT = ar.alloc([2, 256], BF16)
    vh = ar.alloc([2, 256], BF16)
    tkv = Trk()
    qT = ar.alloc([2, 2048], BF16)
    tqT = Trk()
    E = [ar.alloc([2, 512], BF16) for _ in range(2)]
    tE = [Trk(), Trk()]
    rden = ar.alloc([512], F32)
    trd = Trk()
    oTn = ar.alloc([2, 512], BF16)
    toT = Trk()

    def load_head(hd):
        b = hd % 2
        c0 = hd * 256
        for (dst, nm) in ((wq[b], "xa_wq"), (wk[b], "xa_wk"), (wv[b], "xa_wv")):
            load_w_cols(k, D[nm], c0, 256, dst, tw[b], sw[b])
        src = D["xa_wo"]
        cx.dma("gpsimd", wo[b], bass.AP(src.tensor, src.offset + c0 * 1024, [[1024, 128], [128 * 1024, 2], [1, 1024]]), sw[b], w=[tw[b]])
    load_head(0)
    for hd in range(4):
        b = hd % 2
        if hd + 1 < 4:
            load_head(hd + 1)
        for dc in range(2):
            pb = k.psum[dc]
            tp = k.tpsum[dc]
            for c in range(8):
                cx.op("tensor", lambda h, pb=pb, c=c, dc=dc, b=b: h.matmul(pb[:, 0:256], wk[b][:, c, dc * 128:(dc + 1) * 128], memT[:, c, :], start=(c == 0), stop=(c == 7)),
                      r=[tw[b], t_memT], w=[tp], inc=(c == 7))
            cx.op("scalar", lambda h, pb=pb, dc=dc: h.copy(kT[:, dc, :], pb[:, 0:256]), r=[tp], w=[tkv])
        for mtile in range(2):
            pb = k.psum[2 + mtile]
            tp = k.tpsum[2 + mtile]
            for c in range(8):
                cx.op("tensor", lambda h, pb=pb, c=c, mtile=mtile, b=b: h.matmul(pb[:, 0:256], memT[:, c, mtile * 128:(mtile + 1) * 128], wv[b][:, c, :], start=(c == 0), stop=(c == 7)),
                      r=[tw[b], t_memT], w=[tp], inc=(c == 7))
            cx.op(V, lambda h, pb=pb, mtile=mtile: h.tensor_copy(vh[:, mtile, :], pb[:, 0:256]), r=[tp], w=[tkv])
        for dc in range(2):
            for n in range(4):
                pb = k.psum[4 + (dc * 4 + n) % 2]
                tp = k.tpsum[4 + (dc * 4 + n) % 2]
                for c in range(8):
                    cx.op("tensor", lambda h, pb=pb, c=c, dc=dc, n=n, b=b: h.matmul(pb[:, :], wq[b][:, c, dc * 128:(dc + 1) * 128], xnT[:, c, n * 512:(n + 1) * 512], start=(c == 0), stop=(c == 7)),
                          r=[tw[b], t_xnT], w=[tp], inc=(c == 7))
                if n % 2 == 0:
                    cx.op("scalar", lambda h, pb=pb, dc=dc, n=n: h.copy(qT[:, dc, n * 512:(n + 1) * 512], pb[:, :]), r=[tp], w=[tqT])
                else:
                    cx.op(V, lambda h, pb=pb, dc=dc, n=n: h.tensor_copy(qT[:, dc, n * 512:(n + 1) * 512], pb[:, :]), r=[tp], w=[tqT])
        if hd == 0:
            k.dbg_add("xa_qT", qT, [tqT])
            k.dbg_add("xa_kT", kT, [tkv])
            k.dbg_add("xa_vh", vh, [tkv])
        for n in range(4):
            eb = n % 2
            ts = slice(n * 512, (n + 1) * 512)
            for mtile in range(2):
                pb = k.psum[mtile]
                tp = k.tpsum[mtile]
                for dc in range(2):
                    cx.op("tensor", lambda h, pb=pb, dc=dc, mtile=mtile, ts=ts: h.matmul(pb[:, :], kT[:, dc, mtile * 128:(mtile + 1) * 128], qT[:, dc, ts], start=(dc == 0), stop=(dc == 1)),
                          r=[tkv, tqT], w=[tp], inc=(dc == 1))
                cx.op("scalar", lambda h, pb=pb, mtile=mtile, eb=eb: h.activation(E[eb][:, mtile, :], pb[:, :], AF.Exp, scale=1.0 / 16.0), r=[tp], w=[tE[eb]])
            pd = k.psum[2]
            tpd = k.tpsum[2]
            for mtile in range(2):
                cx.op("tensor", lambda h, pd=pd, mtile=mtile, eb=eb: h.matmul(pd[:, :], k.onesb, E[eb][:, mtile, :], start=(mtile == 0), stop=(mtile == 1)), r=[tE[eb]], w=[tpd], inc=(mtile == 1))
            cx.op(V, lambda h, pd=pd: h.reciprocal(rden, pd[:, :]), r=[tpd], w=[trd])
            for dc in range(2):
                po = k.psum[3 + dc]
                tpo = k.tpsum[3 + dc]
                for mtile in range(2):
                    cx.op("tensor", lambda h, po=po, mtile=mtile, dc=dc, eb=eb: h.matmul(po[:, :], vh[:, mtile, dc * 128:(dc + 1) * 128], E[eb][:, mtile, :], start=(mtile == 0), stop=(mtile == 1)),
                          r=[tkv, tE[eb]], w=[tpo], inc=(mtile == 1))
                cx.op(V, lambda h, po=po, dc=dc: h.tensor_tensor(out=oTn[:, dc, :], in0=po[:, :], in1=rden, op=ALU.mult), r=[tpo, trd], w=[toT])
            for t in range(4):
                i = n * 4 + t
                for half in range(2):
                    pw_ = k.psum[5 + (t * 2 + half) % 3]
                    tpw = k.tpsum[5 + (t * 2 + half) % 3]
                    for dc in range(2):
                        cx.op("tensor", lambda h, pw_=pw_, dc=dc, t=t, half=half, b=b: h.matmul(pw_[:, :], oTn[:, dc, t * 128:(t + 1) * 128], wo[b][:, dc, half * 512:(half + 1) * 512], start=(dc == 0), stop=(dc == 1)),
                              r=[toT, tw[b]], w=[tpw], inc=(dc == 1))
                    xs = k.xacc[:, i, half * 512:(half + 1) * 512]
                    cx.op(V, lambda h, pw_=pw_, xs=xs: h.tensor_tensor(out=xs, in0=pw_[:, :], in1=xs, op=ALU.add), r=[tpw, k.txacc[i]], w=[k.txacc[i]])
    cx.barrier()
    ar.release(m0)


def moe(k):
    cx, ar, D = k.cx, k.ar, k.D
    V = "vector"
    m0 = ar.mark()
    xnT = ar.alloc([8, 2048], BF16)
    t_xnT = Trk()
    norm_transpose(k, "moe", lambda i: (k.xacc[:, i, :], k.txacc[i]), NT, D["norm_moe"], xnT, BF16, t_xnT)
    wr = ar.alloc([8, 36], BF16)
    twr = Trk()
    sl_ = cx.fresh()
    srcg, srce = D["router_group_w"], D["router_expert_w"]
    cx.dma("gpsimd", wr[:, :, 0:4], bass.AP(srcg.tensor, srcg.offset, [[4, 128], [4 * 128, 8], [1, 4]]), sl_, w=[twr])
    cx.dma("gpsimd", wr[:, :, 4:36], bass.AP(srce.tensor, srce.offset, [[32, 128], [32 * 128, 8], [1, 32]]), sl_, w=[twr])
    rb = ar.alloc([36], F32)
    trb = Trk()
    sl2 = cx.fresh()
    cx.dma("sync", rb[:, 0:4], dram_bcast(D["router_group_b"], 128, 4), sl2, w=[trb])
    cx.dma("sync", rb[:, 4:36], dram_bcast(D["router_expert_b"], 128, 32), sl2, w=[trb])
    cw = ar.alloc([NT, 32], F32)
    tcw = Trk()
    lg = ar.alloc([36], F32)
    msk = ar.alloc([32], F32)
    m8 = ar.alloc([8], F32)
    sc = ar.alloc([8], F32)
    oh = ar.alloc([4], F32)
    T = Trk()
    for i in range(NT):
        pb = k.psum[i % 2]
        tp = k.tpsum[i % 2]
        for c in range(8):
            cx.op("tensor", lambda h, pb=pb, c=c, i=i: h.matmul(pb[:, 0:36], xnT[:, c, i * 128:(i + 1) * 128], wr[:, c, :], start=(c == 0), stop=(c == 7)),
                  r=[t_xnT, twr], w=[tp], inc=(c == 7))
        cx.op(V, lambda h, pb=pb: h.tensor_tensor(out=lg, in0=pb[:, 0:36], in1=rb, op=ALU.add), r=[tp, trb, T], w=[T])
        cx.op(V, lambda h: h.tensor_reduce(out=sc[:, 0:1], in_=lg[:, 0:4], op=ALU.max, axis=AX.X), r=[T], w=[T])
        cx.op(V, lambda h: h.tensor_scalar(oh, lg[:, 0:4], sc[:, 0:1], None, op0=ALU.is_equal), r=[T], w=[T])
        cx.op(V, lambda h: h.tensor_scalar(sc[:, 1:2], sc[:, 0:1], -1.0, None, op0=ALU.mult), r=[T], w=[T])
        cx.op("scalar", lambda h: h.activation(m8[:, 0:4], lg[:, 0:4], AF.Exp, bias=sc[:, 1:2], accum_out=sc[:, 2:3]), r=[T], w=[T])
        cx.op(V, lambda h: h.reciprocal(sc[:, 3:4], sc[:, 2:3]), r=[T], w=[T])
        cx.op(V, lambda h: h.tensor_scalar(oh, oh, -1.0, 1e30, op0=ALU.add, op1=ALU.mult), r=[T], w=[T])
        cx.op(V, lambda h: h.tensor_tensor(out=msk.rearrange("p (g e) -> p g e", g=4), in0=lg[:, 4:36].rearrange("p (g e) -> p g e", g=4),
                                            in1=oh.unsqueeze(2).to_broadcast([128, 4, 8]), op=ALU.add), r=[T], w=[T])
        cx.op(V, lambda h: h.max(out=m8, in_=msk), r=[T], w=[T])
        cx.op(V, lambda h: h.tensor_tensor(out=sc[:, 4:5], in0=m8[:, 1:2], in1=m8[:, 0:1], op=ALU.subtract), r=[T], w=[T])
        cx.op("scalar", lambda h: h.activation(sc[:, 4:5], sc[:, 4:5], AF.Exp), r=[T], w=[T])
        cx.op(V, lambda h: h.tensor_scalar(sc[:, 5:6], sc[:, 4:5], 1.0, None, op0=ALU.add), r=[T], w=[T])
        cx.op(V, lambda h: h.reciprocal(sc[:, 5:6], sc[:, 5:6]), r=[T], w=[T])
        cx.op(V, lambda h: h.tensor_tensor(out=sc[:, 6:7], in0=sc[:, 4:5], in1=sc[:, 5:6], op=ALU.mult), r=[T], w=[T])
        cx.op(V, lambda h: h.tensor_tensor(out=sc[:, 5:6], in0=sc[:, 5:6], in1=sc[:, 3:4], op=ALU.mult), r=[T], w=[T])
        cx.op(V, lambda h: h.tensor_tensor(out=sc[:, 6:7], in0=sc[:, 6:7], in1=sc[:, 3:4], op=ALU.mult), r=[T], w=[T])
        cx.op(V, lambda h, i=i: h.tensor_scalar(cw[:, i, :], msk, m8[:, 0:1], sc[:, 5:6], op0=ALU.is_equal, op1=ALU.mult), r=[T], w=[tcw, T])
        cx.op(V, lambda h: h.tensor_scalar(lg[:, 4:36], msk, m8[:, 1:2], sc[:, 6:7], op0=ALU.is_equal, op1=ALU.mult), r=[T], w=[T])
        cx.op(V, lambda h, i=i: h.tensor_tensor(out=cw[:, i, :], in0=cw[:, i, :], in1=lg[:, 4:36], op=ALU.add), r=[T, tcw], w=[tcw, T])
    k.dbg_add("moe_cw", cw, [tcw])
    wgu = [ar.alloc([8, 512], BF16) for _ in range(2)]
    wd = [ar.alloc([2, 1024], BF16) for _ in range(2)]
    twe = [Trk(), Trk()]
    swe = [cx.slot("we0"), cx.slot("we1")]
    sg = [ar.alloc([512], F32) for _ in range(2)]
    tsg = [Trk(), Trk()]
    h1 = [ar.alloc([2, 512], BF16) for _ in range(2)]
    th1 = [Trk(), Trk()]
    NE = k.n_experts

    def load_e(e):
        b = e % 2
        g_, u_, d_ = D["moe_w_gate"], D["moe_w_up"], D["moe_w_down"]
        cx.dma("gpsimd", wgu[b][:, :, 0:256], bass.AP(g_.tensor, g_.offset + e * 1024 * 256, [[256, 128], [256 * 128, 8], [1, 256]]), swe[b], w=[twe[b]])
        cx.dma("gpsimd", wgu[b][:, :, 256:512], bass.AP(u_.tensor, u_.offset + e * 1024 * 256, [[256, 128], [256 * 128, 8], [1, 256]]), swe[b], w=[twe[b]])
        cx.dma("gpsimd", wd[b], bass.AP(d_.tensor, d_.offset + e * 256 * 1024, [[1024, 128], [1024 * 128, 2], [1, 1024]]), swe[b], w=[twe[b]])
    import os
    NOLOAD = os.environ.get("MOE_NOLOAD", "") == "1"
    load_e(0)
    if NE > 1:
        load_e(1)
    jobs = [(e, n) for e in range(NE) for n in range(4)]
    state = {"cnt": 0, "loaded": 0}
    cx.barrier()
    k.txh = [[Trk(), Trk()] for _ in range(NT)]

    def emit_gu(j, fh):
        e, n = jobs[j]
        b = e % 2
        ts = slice(n * 512, (n + 1) * 512)
        hb = j % 2
        pg = k.psum[fh * 2]
        tpg = k.tpsum[fh * 2]
        pu = k.psum[fh * 2 + 1]
        tpu = k.tpsum[fh * 2 + 1]
        for c in range(8):
            cx.op("tensor", lambda h, pg=pg, c=c, fh=fh, ts=ts, b=b: h.matmul(pg[:, :], wgu[b][:, c, fh * 128:(fh + 1) * 128], xnT[:, c, ts], start=(c == 0), stop=(c == 7)),
                  r=[twe[b], t_xnT], w=[tpg], inc=(c == 7))
        for c in range(8):
            cx.op("tensor", lambda h, pu=pu, c=c, fh=fh, ts=ts, b=b: h.matmul(pu[:, :], wgu[b][:, c, 256 + fh * 128:256 + (fh + 1) * 128], xnT[:, c, ts], start=(c == 0), stop=(c == 7)),
                  r=[twe[b], t_xnT], w=[tpu], inc=(c == 7))
        cx.op("scalar", lambda h, pg=pg, fh=fh: h.activation(sg[fh], pg[:, :], AF.Silu), r=[tpg], w=[tsg[fh]])
        cx.op(V, lambda h, pu=pu, fh=fh, hb=hb: h.tensor_tensor(out=h1[hb][:, fh, :], in0=pu[:, :], in1=sg[fh], op=ALU.mult), r=[tpu, tsg[fh]], w=[th1[hb]])

    def emit_down(j):
        e, n = jobs[j]
        b = e % 2
        hb = j % 2
        for t in range(4):
            i = n * 4 + t
            for half in range(2):
                pdn = k.psum[4 + state["cnt"] % 4]
                tpd = k.tpsum[4 + state["cnt"] % 4]
                state["cnt"] += 1
                for fh in range(2):
                    cx.op("tensor", lambda h, pdn=pdn, fh=fh, t=t, half=half, hb=hb, b=b: h.matmul(pdn[:, :], h1[hb][:, fh, t * 128:(t + 1) * 128], wd[b][:, fh, half * 512:(half + 1) * 512], start=(fh == 0), stop=(fh == 1)),
                          r=[th1[hb], twe[b]], w=[tpd], inc=(fh == 1))
                xs = k.xacc[:, i, half * 512:(half + 1) * 512]
                cwc = cw[:, i, e:e + 1]
                cx.op(V, lambda h, pdn=pdn, xs=xs, cwc=cwc: h.scalar_tensor_tensor(out=xs, in0=pdn[:, :], scalar=cwc, in1=xs, op0=ALU.mult, op1=ALU.add), r=[tpd, tcw, k.txh[i][half]], w=[k.txh[i][half]])
        if n == 3 and e + 2 < NE and not NOLOAD:
            load_e(e + 2)
    nj = len(jobs)
    if nj > 0:
        emit_gu(0, 0)
        emit_gu(0, 1)
        for j in range(nj):
            if j + 1 < nj:
                emit_gu(j + 1, 0)
            emit_down(j)
            if j + 1 < nj:
                emit_gu(j + 1, 1)
    cx.barrier()
    ar.release(m0)


def final_norm(k, out):
    cx, ar, D = k.cx, k.ar, k.D
    V = "vector"
    m0 = ar.mark()
    gB = ar.alloc([1024], F32)
    tg = Trk()
    cx.dma("sync", gB, dram_bcast(D["norm_final"], 128, 1024), cx.fresh(), w=[tg])
    junk = ar.alloc([1024], BF16)
    tj = Trk()
    ss = ar.alloc([NT, 2], F32)
    tss = Trk()
    ob = [ar.alloc([1024], F32) for _ in range(2)]
    tob = [Trk(), Trk()]
    so = [cx.slot("o0"), cx.slot("o1")]
    for i in range(NT):
        b = i % 2
        s0, s1 = ss[:, i, 0:1], ss[:, i, 1:2]
        txs = [k.txacc[i]] + (k.txh[i] if hasattr(k, "txh") else [])
        cx.op("scalar", lambda h, i=i, s0=s0: h.activation(junk, k.xacc[:, i, :], AF.Square, accum_out=s0), r=txs + [tss], w=[tj, tss])
        cx.op(V, lambda h, s0=s0, s1=s1: h.tensor_scalar(s1, s0, 1.0 / 1024, EPS, op0=ALU.mult, op1=ALU.add), r=[tss], w=[tss])
        cx.op("scalar", lambda h, s1=s1: h.activation(s1, s1, AF.Sqrt), r=[tss], w=[tss])
        cx.op(V, lambda h, s1=s1: h.reciprocal(s1, s1), r=[tss], w=[tss])
        cx.op(V, lambda h, i=i, s1=s1, b=b: h.scalar_tensor_tensor(out=ob[b], in0=k.xacc[:, i, :], scalar=s1, in1=gB, op0=ALU.mult, op1=ALU.mult), r=txs + [tss, tg], w=[tob[b]])
        cx.dma("sync", out[i * 128:(i + 1) * 128, :], ob[b], so[b], r=[tob[b]])
    cx.barrier()
    ar.release(m0)


_CACHE = {}


def kernel(**inputs):
    inp = {k_: np.asarray(v) for k_, v in inputs.items()}
    n = inp["x"].shape[0]
    maps = [host_inputs(inp, b) for b in range(n)]
    key = "full"
    if key not in _CACHE:
        shapes = {k_: (v.shape, np2dt(v)) for k_, v in maps[0].items()}
        _CACHE[key] = build(shapes)[0]
    nc = _CACHE[key]
    res = run_bass_kernel_spmd(nc, maps, core_ids=list(range(n)))
    return np.stack([np.asarray(r["out"], dtype=np.float32) for r in res.results], 0)
```

```python
import contextlib
import os
import math
import numpy as np
import ml_dtypes
import concourse.bass as bass
import concourse.mybir as mybir
from concourse.bass_utils import run_bass_kernel_spmd

F32 = mybir.dt.float32
BF16 = mybir.dt.bfloat16
F32R = mybir.dt.float32r
I32 = mybir.dt.int32
AF = mybir.ActivationFunctionType
ALU = mybir.AluOpType
AX = mybir.AxisListType

ENGS = ("sync", "scalar", "gpsimd", "vector", "tensor")
S = 2048
DM = 1024
NT = 16
EPS = 1e-6


class Trk:
    __slots__ = ("name", "w", "r", "excl")

    def __init__(self, name="", excl=False):
        self.name = name
        self.w = None
        self.r = {}
        self.excl = excl


class DmaSlot:
    def __init__(self, ctx, name):
        self.key = "d_" + name + str(ctx.nsem)
        ctx.sems[self.key] = ctx.new_sem(self.key)
        self.total = 0


class Ctx:
    def __init__(self, nc, stack):
        self.nc = nc
        self.stack = stack
        self.q = {e: [] for e in ENGS}
        self.sems = {}
        self.nsem = 0
        self.cnt = {e: 0 for e in ENGS}
        self.known = {e: {} for e in ENGS}
        for e in ENGS:
            self.sems[e] = self.new_sem("s_" + e)
        self.slots = []
        self.pools = {}
        self.pool_idx = {}
        self.n_ops = 0

    def new_sem(self, name):
        self.nsem += 1
        return self.stack.enter_context(self.nc.semaphore(name))

    def slot(self, name):
        s = DmaSlot(self, name)
        self.slots.append(s)
        return s

    def fresh(self, kind="hw"):
        pool = self.pools.setdefault(kind, [])
        i = self.pool_idx.get(kind, 0)
        if i >= len(pool):
            assert len(pool) < 30, "slot pool exhausted"
            pool.append(self.slot(kind + "%d" % len(pool)))
            pool[-1].kind = kind
        self.pool_idx[kind] = i + 1
        return pool[i]

    def sb(self, name, shape, dt):
        return self.stack.enter_context(self.nc.sbuf_tensor("sb_" + name, list(shape), dt))

    def ps(self, name, shape, dt=F32):
        return self.stack.enter_context(self.nc.psum_tensor(name, list(shape), dt))

    def _waits_for(self, eng, r, w, extra=()):
        need = {}

        def req(dep):
            if dep is None:
                return
            k, c = dep
            if k == eng and eng in ("tensor", "sync"):
                return
            if c > need.get(k, 0):
                need[k] = c
        for t in r:
            req(t.w)
        for t in w:
            req(t.w)
            for k, c in t.r.items():
                req((k, c))
        for d in extra:
            req(d)
        out = []
        kn = self.known[eng]
        for k, c in need.items():
            if kn.get(k, 0) < c:
                kn[k] = c
                out.append((self.sems[k], c))
        return out

    def op(self, eng, fn, r=(), w=(), inc=True, extra=()):
        w = list(w) + [t for t in r if t.excl]
        r = [t for t in r if not t.excl]
        waits = self._waits_for(eng, r, w, extra)
        c = self.cnt[eng] + 1
        if inc:
            self.cnt[eng] = c
        sem = self.sems[eng]

        def emit(h, fn=fn, waits=waits, inc=inc, sem=sem):
            for s, v in waits:
                h.wait_ge(s, v)
            ins = fn(h)
            if inc:
                ins.then_inc(sem, 1)
        self.q[eng].append(emit)
        for t in r:
            t.r[eng] = c
        for t in w:
            t.w = (eng, c)
            t.r = {}
        self.n_ops += 1

    def dma(self, eng, out, in_, slot, r=(), w=(), extra=(), **kw):
        kind = "sw" if eng == "gpsimd" else "hw"
        assert getattr(slot, "kind", kind) == kind, ("DMA slot kind mismatch", slot.key, eng)
        slot.kind = kind
        waits = self._waits_for(eng, r, w, extra)
        slot.total += 16
        sem = self.sems[slot.key]

        def emit(h, waits=waits, sem=sem, out=out, in_=in_, kw=kw):
            for s, v in waits:
                h.wait_ge(s, v)
            h.dma_start(out=out, in_=in_, **kw).then_inc(sem, 16)
        self.q[eng].append(emit)
        dep = (slot.key, slot.total)
        for t in r:
            t.r[slot.key] = slot.total
        for t in w:
            t.w = dep
            t.r = {}
        self.n_ops += 1
        return dep

    def wait_deps(self, eng, deps):
        waits = self._waits_for(eng, (), (), deps)

        def emit(h, waits=waits):
            for s, v in waits:
                h.wait_ge(s, v)
        self.q[eng].append(emit)

    def barrier(self):
        deps = [(e, self.cnt[e]) for e in ENGS if e != "sync" and self.cnt[e] > 0]
        deps += [(s.key, s.total) for s in self.slots if s.total > 0]
        for e in ENGS:
            self.wait_deps(e, deps)
        self.pool_idx = {}

    def emit_all(self, block):
        q = self.q

        @block.sync
        def _(h):
            for f in q["sync"]:
                f(h)

        @block.scalar
        def _(h):
            for f in q["scalar"]:
                f(h)

        @block.gpsimd
        def _(h):
            for f in q["gpsimd"]:
                f(h)

        @block.vector
        def _(h):
            for f in q["vector"]:
                f(h)

        @block.tensor
        def _(h):
            for f in q["tensor"]:
                f(h)


class Arena:
    def __init__(self, cx, words, base=None):
        self.t = cx.sb("arena", [128, words], F32) if base is None else base
        self.cx = cx
        self.words = words
        self.top = 0

    def mark(self):
        return self.top

    def release(self, m):
        if m != self.top:
            self.cx.barrier()
        self.top = m

    def alloc(self, shape, dt):
        n = int(np.prod(shape))
        w = n if dt in (F32, F32R, I32) else (n + 1) // 2
        w = (w + 1) // 2 * 2
        o = self.top
        self.top += w
        assert self.top <= self.words, ("arena overflow", self.top, self.words)
        v = self.t[:, o:o + w]
        if dt != F32:
            v = v.bitcast(dt)
        v = v[:, 0:n]
        if len(shape) > 1:
            names = " ".join("d%d" % i for i in range(len(shape)))
            v = v.rearrange("p (%s) -> p %s" % (names, names), **{"d%d" % i: shape[i] for i in range(len(shape))})
        return v


def pap(ap, part0, nparts, off, dims):
    base = ap.ap[0][0]
    return bass.AP(ap.tensor, ap.offset + part0 * base + off, [[base, nparts]] + [list(d) for d in dims])


def host_consts():
    c = {}
    c["ident"] = np.eye(128, dtype=np.float32)
    c["identb"] = np.eye(128, dtype=np.float32).astype(ml_dtypes.bfloat16)
    c["ones"] = np.ones((128, 128), np.float32)
    selT = np.zeros((128, 2, 8, 128), np.float32)
    selB = np.zeros((128, 2, 8, 128), np.float32)
    for q in range(4):
        for r in range(32):
            loc, cc = r // 16, r % 16
            for s in range(8):
                selT[q * 32 + r, loc, s, s * 16 + cc] = 1.0
                selB[q * 32 + r, loc, s, s * 16 + cc] = 1.0
    c["selT"] = selT.astype(ml_dtypes.bfloat16)
    c["selB"] = selB.astype(ml_dtypes.bfloat16)
    sidx = np.arange(128) // 16
    c["s5mf"] = (sidx[None, :] >= sidx[:, None]).astype(np.float32)
    c["s5mb"] = (sidx[None, :] <= sidx[:, None]).astype(np.float32)
    c["kvec"] = np.tile((np.arange(16, dtype=np.float32) - 7.0)[None, :], (128, 1))
    k = np.arange(128)[:, None]
    cc = np.arange(128)[None, :]
    same = (k // 64) == (cc // 64)
    gm = np.zeros((128, 8, 128), np.float32)
    gm[:, 0] = same & (k <= cc)
    gm[:, 1] = same & (k >= cc)
    gm[:, 2] = np.where(same & (cc >= k), 0.0, -30000.0)
    gm[:, 3] = np.where(same & (cc <= k), 0.0, -30000.0)
    gm[:, 4] = same & (cc > k)
    gm[:, 5] = same & (cc < k)
    gm[:, 6] = same
    c["gmask"] = gm
    return c


def host_s5(inp):
    o = {}

    pairs = {"lam_re": ("s5_lam_re_f", "s5_lam_re_b"), "lam_im": ("s5_lam_im_f", "s5_lam_im_b"),
             "log_step": ("s5_log_step_f", "s5_log_step_b"), "b_re": ("s5_b_re_f", "s5_b_re_b"),
             "b_im": ("s5_b_im_f", "s5_b_im_b"), "c_re": ("s5_c_re_f", "s5_c_re_b"), "c_im": ("s5_c_im_f", "s5_c_im_b")}

    def st(nm):
        f_, b_ = pairs[nm]
        return np.stack([inp[f_][0], inp[b_][0]], 0)
    lam = np.stack([st("lam_re"), st("lam_im")], 0)
    lam = lam.reshape(2, 2, 2, 16, 64).transpose(2, 4, 0, 1, 3)
    o["s5_lam"] = np.ascontiguousarray(lam.reshape(128, 2, 32))
    ls = st("log_step").reshape(2, 2, 16)
    ls = np.broadcast_to(ls.transpose(1, 0, 2)[:, None], (2, 64, 2, 16))
    o["s5_step"] = np.ascontiguousarray(ls.reshape(128, 32))
    b = np.stack([st("b_re"), st("b_im")], 0)
    b = b.reshape(2, 2, 2, 16, 64, 16).transpose(2, 4, 0, 1, 3, 5)
    o["s5_b"] = np.ascontiguousarray(b.reshape(128, 2, 512))
    cm = np.stack([st("c_re"), st("c_im")], 0)
    cm = cm.reshape(2, 2, 2, 16, 16, 64).transpose(2, 5, 0, 1, 3, 4)
    o["s5_c"] = np.ascontiguousarray(cm.reshape(128, 2, 512))
    d = inp["s5_d"][0].reshape(32, 16)
    o["s5_dvec"] = np.ascontiguousarray(np.broadcast_to(d.T[None], (8, 16, 32)).reshape(128, 32))
    o["s5_bglu"] = np.ascontiguousarray(inp["s5_b_glu"][0].reshape(4, 128).T)
    o["s5_normw"] = np.ascontiguousarray(inp["s5_norm"][0].reshape(4, 128).T)
    return o


def np2dt(a):
    if a.dtype == np.float32:
        return F32
    if a.dtype == ml_dtypes.bfloat16:
        return BF16
    raise ValueError(a.dtype)


class K:
    pass


def dram_bcast(ap, nparts, n, off=0):
    return bass.AP(ap.tensor, ap.offset + off, [[0, nparts], [1, n]])


def norm_transpose(k, name, src_fn, ntiles, gain_dram, outT, out_dt, outT_trk, resident=False):
    cx, ar = k.cx, k.ar
    m = ar.mark()
    gB = ar.alloc([1024], F32)
    tg = Trk()
    cx.dma("sync", gB, dram_bcast(gain_dram, 128, 1024), cx.fresh(), w=[tg])
    junk = ar.alloc([1024], BF16)
    tj = Trk()
    xn = [ar.alloc([1024], out_dt) for _ in range(2)]
    txn = [Trk(), Trk()]
    ss = ar.alloc([NT * 2, 1], F32)
    tss = [Trk() for _ in range(ntiles)]
    pdt = BF16 if out_dt == BF16 else F32
    ident = k.identb if out_dt == BF16 else k.ident
    srcs = []
    if resident:
        for i in range(ntiles):
            src, ts = src_fn(i)
            srcs.append((src, ts))
            cx.op("scalar", lambda h, src=src, i=i: h.activation(junk, src, AF.Square, accum_out=ss[:, 2 * i:2 * i + 1]), r=[ts], w=[tj, tss[0]])
        ssv = ss.rearrange("p (t two) one -> p t (two one)", two=2)
        cx.op("vector", lambda h: h.tensor_scalar(ssv[:, 0:ntiles, 1:2], ssv[:, 0:ntiles, 0:1], 1.0 / 1024, EPS, op0=ALU.mult, op1=ALU.add), r=[tss[0]], w=[tss[0]])
        cx.op("scalar", lambda h: h.activation(ssv[:, 0:ntiles, 1:2], ssv[:, 0:ntiles, 1:2], AF.Sqrt), r=[tss[0]], w=[tss[0]])
        cx.op("vector", lambda h: h.reciprocal(ssv[:, 0:ntiles, 1:2], ssv[:, 0:ntiles, 1:2]), r=[tss[0]], w=[tss[0]])
    for i in range(ntiles):
        rsi = ss[:, 2 * i + 1:2 * i + 2]
        if resident:
            src, ts = srcs[i]
            tsi = tss[0]
        else:
            src, ts = src_fn(i)
            ssi = ss[:, 2 * i:2 * i + 1]
            tsi = tss[i]
            cx.op("scalar", lambda h, src=src, ssi=ssi: h.activation(junk, src, AF.Square, accum_out=ssi), r=[ts], w=[tj, tss[i]])
            cx.op("vector", lambda h, ssi=ssi, rsi=rsi: h.tensor_scalar(rsi, ssi, 1.0 / 1024, EPS, op0=ALU.mult, op1=ALU.add), r=[tss[i]], w=[tss[i]])
            cx.op("scalar", lambda h, rsi=rsi: h.activation(rsi, rsi, AF.Sqrt), r=[tss[i]], w=[tss[i]])
            cx.op("vector", lambda h, rsi=rsi: h.reciprocal(rsi, rsi), r=[tss[i]], w=[tss[i]])
        b = i % 2
        cx.op("vector", lambda h, src=src, rsi=rsi, b=b: h.scalar_tensor_tensor(out=xn[b], in0=src, scalar=rsi, in1=gB, op0=ALU.mult, op1=ALU.mult),
              r=[ts, tsi, tg], w=[txn[b]])
        if out_dt == BF16:
            pb = k.psum[i % 2]
            tp = k.tpsum[i % 2]
            pv = pb.bitcast(BF16)
            for c in range(8):
                cx.op("tensor", lambda h, b=b, c=c, pv=pv: h.transpose(pv[:, c * 128:(c + 1) * 128], xn[b][:, c * 128:(c + 1) * 128], ident),
                      r=[txn[b]], w=[tp], inc=(c == 7))
            dst = outT[:, :, i * 128:(i + 1) * 128]
            eng = "scalar" if i % 2 == 0 else "vector"
            if eng == "scalar":
                cx.op(eng, lambda h, dst=dst, pv=pv: h.copy(dst, pv.rearrange("p (c t) -> p c t", c=8)), r=[tp], w=[outT_trk])
            else:
                cx.op(eng, lambda h, dst=dst, pv=pv: h.tensor_copy(dst, pv.rearrange("p (c t) -> p c t", c=8)), r=[tp], w=[outT_trk])
        else:
            for half in range(2):
                pb = k.psum[(2 * i + half) % 4]
                tp = k.tpsum[(2 * i + half) % 4]
                for c4 in range(4):
                    c = half * 4 + c4
                    cx.op("tensor", lambda h, b=b, c=c, c4=c4, pb=pb: h.transpose(pb[:, c4 * 128:(c4 + 1) * 128], xn[b][:, c * 128:(c + 1) * 128].bitcast(F32), ident),
                          r=[txn[b]], w=[tp], inc=(c4 == 3))
                dst = outT[:, half * 4:(half + 1) * 4, i * 128:(i + 1) * 128]
                if half == 0:
                    cx.op("scalar", lambda h, dst=dst, pb=pb: h.copy(dst, pb.rearrange("p (c t) -> p c t", c=4)), r=[tp], w=[outT_trk])
                else:
                    cx.op("vector", lambda h, dst=dst, pb=pb: h.tensor_copy(dst, pb.rearrange("p (c t) -> p c t", c=4)), r=[tp], w=[outT_trk])
    ar.release(m)


def s5_prep(k):
    cx, ar, D = k.cx, k.ar, k.D
    V = "vector"
    m0 = ar.mark()
    lam = ar.alloc([2, 32], F32)
    step = ar.alloc([32], F32)
    bb = ar.alloc([2, 512], F32)
    cc = ar.alloc([2, 512], F32)
    kvec = ar.alloc([16], F32)
    tl = Trk()
    sl_ = cx.fresh()
    for dst, nm in ((lam, "s5_lam"), (step, "s5_step"), (bb, "s5_b"), (cc, "s5_c"), (kvec, "kvec")):
        cx.dma("sync", dst, D[nm], sl_, w=[tl])
    T = Trk()

    def vop(fn, extra_r=()):
        cx.op(V, fn, r=[T, tl] + list(extra_r), w=[T])

    def aop(fn):
        cx.op("scalar", fn, r=[T, tl], w=[T])
    lre, lim = lam[:, 0, :], lam[:, 1, :]
    dl = ar.alloc([32], F32)
    re1 = ar.alloc([32], F32)
    im1 = ar.alloc([32], F32)
    aop(lambda h: h.activation(dl, step, AF.Exp))
    vop(lambda h: h.tensor_tensor(out=re1, in0=dl, in1=lre, op=ALU.mult))
    vop(lambda h: h.tensor_tensor(out=im1, in0=dl, in1=lim, op=ALU.mult))
    PWI = ar.alloc([16, 32], F32)
    PWR = ar.alloc([16, 32], F32)
    m_pw = ar.mark()
    KR = ar.alloc([16, 32], F32)
    KI = ar.alloc([16, 32], F32)
    kv_b = kvec.unsqueeze(2).to_broadcast([128, 16, 32])
    vop(lambda h: h.tensor_tensor(out=KR, in0=kv_b, in1=re1.unsqueeze(1).to_broadcast([128, 16, 32]), op=ALU.mult))
    vop(lambda h: h.tensor_tensor(out=KI, in0=kv_b, in1=im1.unsqueeze(1).to_broadcast([128, 16, 32]), op=ALU.mult))
    MAG = ar.alloc([16, 32], F32)
    aop(lambda h: h.activation(MAG, KR, AF.Exp))
    YI = ar.alloc([16, 32], I32)
    YF = ar.alloc([16, 32], F32)
    vop(lambda h: h.tensor_scalar(KI, KI, 1.0 / (2 * math.pi), None, op0=ALU.mult))
    vop(lambda h: h.tensor_copy(YI, KI))
    vop(lambda h: h.tensor_copy(YF, YI))
    vop(lambda h: h.tensor_tensor(out=KI, in0=KI, in1=YF, op=ALU.subtract))
    SH_ = ar.alloc([16, 32], F32)
    SQ_ = ar.alloc([16, 32], F32)
    aop(lambda h: h.activation(SH_, KI, AF.Sin, scale=math.pi))
    aop(lambda h: h.activation(SQ_, KI, AF.Sin, scale=math.pi / 2))
    CH_ = ar.alloc([16, 32], F32)
    vop(lambda h: h.tensor_tensor(out=CH_, in0=SQ_, in1=SQ_, op=ALU.mult))
    vop(lambda h: h.tensor_scalar(CH_, CH_, -2.0, 1.0, op0=ALU.mult, op1=ALU.add))
    vop(lambda h: h.tensor_tensor(out=PWI, in0=SH_, in1=CH_, op=ALU.mult))
    vop(lambda h: h.scalar_tensor_tensor(out=PWI, in0=PWI, scalar=2.0, in1=MAG, op0=ALU.mult, op1=ALU.mult))
    vop(lambda h: h.tensor_tensor(out=PWR, in0=SH_, in1=SH_, op=ALU.mult))
    vop(lambda h: h.tensor_scalar(PWR, PWR, -2.0, 1.0, op0=ALU.mult, op1=ALU.add))
    vop(lambda h: h.tensor_tensor(out=PWR, in0=PWR, in1=MAG, op=ALU.mult))
    ar.release(m_pw)
    lrm1 = ar.alloc([32], F32)
    li = PWI[:, 8, :]
    t1 = ar.alloc([32], F32)
    t2 = ar.alloc([32], F32)
    den = ar.alloc([32], F32)
    c0r = ar.alloc([32], F32)
    c0i = ar.alloc([32], F32)
    vop(lambda h: h.tensor_scalar(lrm1, PWR[:, 8, :], -1.0, None, op0=ALU.add))
    vop(lambda h: h.tensor_tensor(out=t1, in0=lre, in1=lre, op=ALU.mult))
    vop(lambda h: h.tensor_tensor(out=t2, in0=lim, in1=lim, op=ALU.mult))
    vop(lambda h: h.tensor_tensor(out=den, in0=t1, in1=t2, op=ALU.add))
    vop(lambda h: h.reciprocal(den, den))
    vop(lambda h: h.tensor_tensor(out=t1, in0=lrm1, in1=lre, op=ALU.mult))
    vop(lambda h: h.tensor_tensor(out=t2, in0=li, in1=lim, op=ALU.mult))
    vop(lambda h: h.tensor_tensor(out=t1, in0=t1, in1=t2, op=ALU.add))
    vop(lambda h: h.tensor_tensor(out=c0r, in0=t1, in1=den, op=ALU.mult))
    vop(lambda h: h.tensor_tensor(out=t1, in0=li, in1=lre, op=ALU.mult))
    vop(lambda h: h.tensor_tensor(out=t2, in0=lrm1, in1=lim, op=ALU.mult))
    vop(lambda h: h.tensor_tensor(out=t1, in0=t1, in1=t2, op=ALU.subtract))
    vop(lambda h: h.tensor_tensor(out=c0i, in0=t1, in1=den, op=ALU.mult))
    BBR = ar.alloc([32, 16], F32)
    BBI = ar.alloc([32, 16], F32)
    TA = ar.alloc([32, 16], F32)
    br = bb[:, 0, :].rearrange("p (a c) -> p a c", c=16)
    bi = bb[:, 1, :].rearrange("p (a c) -> p a c", c=16)
    c0r_b = c0r.unsqueeze(2).to_broadcast([128, 32, 16])
    c0i_b = c0i.unsqueeze(2).to_broadcast([128, 32, 16])
    vop(lambda h: h.tensor_tensor(out=BBR, in0=br, in1=c0r_b, op=ALU.mult))
    vop(lambda h: h.tensor_tensor(out=TA, in0=bi, in1=c0i_b, op=ALU.mult))
    vop(lambda h: h.tensor_tensor(out=BBR, in0=BBR, in1=TA, op=ALU.subtract))
    vop(lambda h: h.tensor_tensor(out=BBI, in0=bi, in1=c0r_b, op=ALU.mult))
    vop(lambda h: h.tensor_tensor(out=TA, in0=br, in1=c0i_b, op=ALU.mult))
    vop(lambda h: h.tensor_tensor(out=BBI, in0=BBI, in1=TA, op=ALU.add))
    ASd = ar.alloc([16, 2, 8, 16], F32)
    CS2d = ar.alloc([16, 2, 8, 16], F32)
    T1 = ar.alloc([8, 16, 16], F32)
    T2 = ar.alloc([8, 16, 16], F32)
    cr = cc[:, 0, :].rearrange("p (d a c) -> p d a c", d=2, c=16)
    ci = cc[:, 1, :].rearrange("p (d a c) -> p d a c", d=2, c=16)
    BBR4 = BBR.rearrange("p (d a) c -> p d a c", d=2)
    BBI4 = BBI.rearrange("p (d a) c -> p d a c", d=2)

    def pw(arr, d, k0, kstep):
        return pap(arr, 0, 128, k0 * 32 + d * 16, [[kstep * 32, 8], [1, 16], [0, 16]])

    def dst(arr, dofs, ri):
        return pap(arr, 0, 128, dofs * 4096 + ri * 128, [[16, 8], [256, 16], [1, 16]])

    def vec(v4, d):
        a_ = v4[:, d]
        return bass.AP(a_.tensor, a_.offset, [list(a_.ap[0]), [0, 8], list(a_.ap[1]), list(a_.ap[2])])

    T1f = T1.rearrange("p a b c -> p (a b c)")

    def cmul(out_arr, dofs, d, k0, kstep, vr, vi, neg_im):
        pr, pi_ = pw(PWR, d, k0, kstep), pw(PWI, d, k0, kstep)
        vop(lambda h: h.tensor_tensor(out=T1, in0=pr, in1=vec(vr, d), op=ALU.mult))
        vop(lambda h: h.tensor_tensor(out=T2, in0=pi_, in1=vec(vi, d), op=ALU.mult))
        vop(lambda h: h.tensor_tensor(out=dst(out_arr, dofs, 0), in0=T1, in1=T2, op=ALU.subtract))
        vop(lambda h: h.tensor_tensor(out=T1, in0=pr, in1=vec(vi, d), op=ALU.mult))
        vop(lambda h: h.tensor_tensor(out=T2, in0=pi_, in1=vec(vr, d), op=ALU.mult))
        if neg_im:
            vop(lambda h: h.tensor_scalar(T1f, T1f, -1.0, None, op0=ALU.mult))
            vop(lambda h: h.tensor_tensor(out=dst(out_arr, dofs, 1), in0=T1, in1=T2, op=ALU.subtract))
        else:
            vop(lambda h: h.tensor_tensor(out=dst(out_arr, dofs, 1), in0=T1, in1=T2, op=ALU.add))
    vop(lambda h: h.tensor_copy(k.s5A1[:, 0:32], PWR[:, 15, :]))
    vop(lambda h: h.tensor_copy(k.s5A1[:, 32:64], PWR[:, 15, :]))
    vop(lambda h: h.tensor_scalar(k.s5A2[:, 0:32], PWI[:, 15, :], -1.0, None, op0=ALU.mult))
    vop(lambda h: h.tensor_copy(k.s5A2[:, 32:64], PWI[:, 15, :]))
    cmul(k.s5CS, 0, 0, 8, 1, cr, ci, True)
    cmul(k.s5CS, 1, 1, 15, -1, cr, ci, True)
    k.t_s5w = T
    mf = ar.alloc([2, 128], F32)
    dv = ar.alloc([32], F32)
    tm = Trk()
    sl_ = cx.fresh()
    cx.dma("sync", mf[:, 0, :], D["s5mf"], sl_, w=[tm])
    cx.dma("sync", mf[:, 1, :], D["s5mb"], sl_, w=[tm])
    cx.dma("sync", dv, D["s5_dvec"], sl_, w=[tm])
    tt1 = [ar.alloc([128], F32) for _ in range(2)]
    ttt = [Trk(), Trk()]
    ASb = ASd.rearrange("p a r s c -> p (a r) (s c)")
    ASm = ASd.rearrange("p a r s c -> p a r (s c)")
    CSm = CS2d.rearrange("p a r s c -> p a r (s c)")
    for d in range(2):
        if d == 0:
            cmul(ASd, 0, 0, 14, -1, BBR4, BBI4, False)
            cmul(CS2d, 0, 0, 0, 1, cr, ci, True)
        else:
            cmul(ASd, 0, 1, 7, 1, BBR4, BBI4, False)
            cmul(CS2d, 0, 1, 7, -1, cr, ci, True)
        for grp in range(8):
            pb = k.psum[grp % 4]
            tp = k.tpsum[grp % 4]
            for j in range(4):
                blk = grp * 4 + j
                cx.op("tensor", lambda h, pb=pb, j=j, blk=blk: h.transpose(pb[:, j * 128:(j + 1) * 128], ASb[:, blk, :], k.ident),
                      r=[T], w=[tp], inc=(j == 3))
            dstv = k.s5AT[:, d * 32 + grp * 4:d * 32 + (grp + 1) * 4, :]
            if grp % 2 == 0:
                cx.op("scalar", lambda h, dstv=dstv, pb=pb: h.copy(dstv, pb.rearrange("p (j x) -> p j x", j=4)), r=[tp], w=[k.t_s5at])
            else:
                cx.op("vector", lambda h, dstv=dstv, pb=pb: h.tensor_copy(dstv, pb.rearrange("p (j x) -> p j x", j=4)), r=[tp], w=[k.t_s5at])
        for g in range(32):
            gh, gl = g // 16, g % 16
            pb = k.psum[4 + g % 4]
            tp = k.tpsum[4 + g % 4]
            for ri in range(2):
                cx.op("tensor", lambda h, pb=pb, ri=ri, gh=gh, gl=gl: h.matmul(
                    pb[:, 0:128], ASm[gh * 64:(gh + 1) * 64, gl, ri, :], CSm[gh * 64:(gh + 1) * 64, gl, ri, :],
                    start=(ri == 0), stop=(ri == 1)), r=[T], w=[tp], inc=(ri == 1))
            b_ = g % 2
            cx.op(V, lambda h, pb=pb, b_=b_, d=d: h.tensor_tensor(out=tt1[b_], in0=pb[:, 0:128], in1=mf[:, d, :], op=ALU.mult), r=[tp, tm], w=[ttt[b_]])
            if d == 0:
                cx.op(V, lambda h, b_=b_, g=g: h.scalar_tensor_tensor(out=k.s5TT[:, g, :], in0=k.ident, scalar=dv[:, g:g + 1], in1=tt1[b_], op0=ALU.mult, op1=ALU.add),
                      r=[ttt[b_], tm], w=[k.t_s5tt])
            else:
                cx.op(V, lambda h, b_=b_, g=g: h.tensor_tensor(out=k.s5TT[:, g, :], in0=k.s5TT[:, g, :], in1=tt1[b_], op=ALU.add),
                      r=[ttt[b_]], w=[k.t_s5tt])
    cx.barrier()
    ar.release(m0)


def load_w_cols(k, wdram, col0, ncols, dst, trk, slot, eng="gpsimd"):
    src = bass.AP(wdram.tensor, wdram.offset + col0, [[wdram.ap[0][0] * 1, 128], [wdram.ap[0][0] * 128, 8], [1, ncols]])
    return k.cx.dma(eng, dst, src, slot, w=[trk])


def proj_fm(k, wt, wtrk, consume):
    cx = k.cx
    for n in range(4):
        pb = k.psum[n % 2 + 2]
        tp = k.tpsum[n % 2 + 2]
        for c in range(8):
            cx.op("tensor", lambda h, pb=pb, c=c, n=n: h.matmul(pb[:, :], wt[:, c, :], k.hT[:, c, n * 512:(n + 1) * 512], start=(c == 0), stop=(c == 7)),
                  r=[wtrk, k.t_hT], w=[tp], inc=(c == 7))
        consume(n, pb, tp)


def s5_build_U(k):
    cx, ar = k.cx, k.ar
    m0 = ar.mark()
    wt = [ar.alloc([8, 128], BF16) for _ in range(2)]
    twt = [Trk(), Trk()]
    swt = [cx.fresh('sw'), cx.fresh('sw')]
    uT = [ar.alloc([2048], BF16) for _ in range(2)]
    tuT = [Trk(), Trk()]
    for ct in range(4):
        b = ct % 2
        load_w_cols(k, k.D["w_in"], ct * 128, 128, wt[b], twt[b], swt[b])

        def consume(n, pb, tp, b=b):
            if n % 2 == 0:
                cx.op("scalar", lambda h: h.copy(uT[b][:, n * 512:(n + 1) * 512], pb[:, :]), r=[tp], w=[tuT[b]])
            else:
                cx.op("vector", lambda h: h.tensor_copy(uT[b][:, n * 512:(n + 1) * 512], pb[:, :]), r=[tp], w=[tuT[b]])
        proj_fm(k, wt[b], twt[b], consume)
        for gi in range(8):
            g = ct * 8 + gi
            q0 = 32 * (gi // 2)
            pb = k.psum[4 + gi % 4]
            tp = k.tpsum[4 + gi % 4]
            for s in range(8):
                rhs = pap(uT[b], q0, 32, s, [[8, 256]])
                cx.op("tensor", lambda h, pb=pb, s=s, rhs=rhs, q0=q0, gi=gi: h.matmul(pb[:, 0:256], k.selT[q0:q0 + 32, gi % 2, s, :], rhs, start=(s == 0), stop=(s == 7), tile_position=(q0, 0)),
                      r=[tuT[b]], w=[tp], inc=(s == 7))
            if gi % 2 == 0:
                cx.op("scalar", lambda h, pb=pb, g=g: h.copy(k.s5U[:, g, :], pb[:, 0:256]), r=[tp], w=[k.t_s5U])
            else:
                cx.op("vector", lambda h, pb=pb, g=g: h.tensor_copy(k.s5U[:, g, :], pb[:, 0:256]), r=[tp], w=[k.t_s5U])
    cx.barrier()
    ar.release(m0)


def s5_main(k, yT, t_yT):
    cx, ar, D = k.cx, k.ar, k.D
    V = "vector"
    m0 = ar.mark()
    SH = ar.alloc([2, 257, 2, 16], BF16)
    tSH = Trk()
    tSHh = Trk()
    X = [ar.alloc([64], F32) for _ in range(3)]
    tX = [Trk() for _ in range(3)]
    t1 = ar.alloc([64], F32)
    t2 = ar.alloc([64], F32)
    tt = Trk()
    tt2 = Trk()
    cx.op("gpsimd", lambda h: h.memset(SH[:, 0, 0, :, :], 0.0), w=[tSH])
    cx.op("gpsimd", lambda h: h.memset(SH[:, 1, 256, :, :], 0.0), w=[tSH])
    cx.op("gpsimd", lambda h: h.memset(X[0], 0.0), w=[tX[0]])
    n = 0
    for gl in range(16):
        for d in range(2):
            for ri in range(2):
                blk = d * 32 + gl * 2 + ri
                pb = k.psum[n % 4]
                tp = k.tpsum[n % 4]
                cx.op("tensor", lambda h, pb=pb, blk=blk, gl=gl: h.matmul(pb[0:64, 0:256], k.s5AT[:, blk, 0:64], k.s5U[:, gl, :], start=True, stop=True),
                      r=[k.t_s5at, k.t_s5U], w=[tp], inc=False)
                cx.op("tensor", lambda h, pb=pb, blk=blk, gl=gl: h.matmul(pb[64:128, 0:256], k.s5AT[:, blk, 64:128], k.s5U[:, 16 + gl, :], start=True, stop=True),
                      r=[k.t_s5at, k.t_s5U], w=[tp])
                slot0 = 1 if d == 0 else 0
                dstv = pap(SH, 0, 128, d * 257 * 32 + slot0 * 32 + ri * 16 + gl, [[32, 256]])
                if n % 2 == 0:
                    cx.op("scalar", lambda h, dstv=dstv, pb=pb: h.copy(dstv, pb[:, 0:256]), r=[tp], w=[tSH])
                else:
                    cx.op(V, lambda h, dstv=dstv, pb=pb: h.tensor_copy(dstv, pb[:, 0:256]), r=[tp], w=[tSH])
                n += 1
    import os
    S5STOP = os.environ.get('S5_STOP', '')
    if S5STOP == 'a':
        cx.barrier(); ar.release(m0); return
    for i in range(256):
        xp, xn = X[i % 3], X[(i + 1) % 3]
        txp, txn = tX[i % 3], tX[(i + 1) % 3]
        xsw = pap(xp, 0, 128, 32, [[-32, 2], [1, 32]])
        bf = (i + 1) * 32
        bb_ = 257 * 32 + (255 - i) * 32
        sview = pap(SH, 0, 128, bf, [[16, 2], [bb_ - bf, 2], [1, 16]])
        xp3 = xp.rearrange("p (r x) -> p r x", r=2)
        cx.op("gpsimd", lambda h, xsw=xsw: h.tensor_tensor(out=t2.rearrange("p (r x) -> p r x", r=2), in0=k.s5A2.rearrange("p (r x) -> p r x", r=2), in1=xsw, op=ALU.mult), r=[txp, k.t_s5w], w=[tt2])
        cx.op(V, lambda h, xp=xp: h.tensor_tensor(out=t1, in0=k.s5A1, in1=xp, op=ALU.mult), r=[txp, k.t_s5w], w=[tt])
        cx.op(V, lambda h, sview=sview: h.tensor_tensor(out=t1.rearrange("p (r d x) -> p r d x", r=2, d=2), in0=t1.rearrange("p (r d x) -> p r d x", r=2, d=2), in1=sview, op=ALU.add), r=[tt, tSH], w=[tt])
        cx.op(V, lambda h, xn=xn: h.tensor_tensor(out=xn, in0=t1, in1=t2, op=ALU.add), r=[tt, tt2], w=[txn])
        cx.op("scalar", lambda h, xn=xn, sview=sview: h.copy(sview, xn.rearrange("p (r d x) -> p r d x", r=2, d=2)), r=[txn], w=[tSHh])
    if S5STOP == 'rec':
        cx.barrier(); ar.release(m0); return
    gT = ar.alloc([4, 2048], F32)
    gTb = ar.alloc([4, 2048], BF16)
    tgT = [Trk() for _ in range(4)]
    tgTb = [Trk() for _ in range(4)]
    ybuf = [k.arA.alloc([8, 256], BF16) for _ in range(2)]
    tyb = [Trk(), Trk()]
    for ct in range(4):
        b = ct % 2
        for gi in range(8):
            g = ct * 8 + gi
            gh, gl = g // 16, g % 16
            pb = k.psum[gi % 2]
            tp = k.tpsum[gi % 2]
            cx.op("tensor", lambda h, pb=pb, g=g: h.matmul(pb[:, 0:256], k.s5TT[:, g, :], k.s5U[:, g, :], start=True, stop=False),
                  r=[k.t_s5tt, k.t_s5U], w=[tp], inc=False)
            for d in range(2):
                for ri in range(2):
                    slot0 = 0 if d == 0 else 1
                    rhs = pap(SH, gh * 64, 64, d * 257 * 32 + slot0 * 32 + ri * 16 + gl, [[32, 256]])
                    last = (d == 1 and ri == 1)
                    cx.op("tensor", lambda h, pb=pb, rhs=rhs, d=d, ri=ri, gh=gh, gl=gl, last=last: h.matmul(
                        pb[:, 0:256], k.s5CS[gh * 64:(gh + 1) * 64, d, gl, ri, :], rhs, start=False, stop=last),
                        r=[tSH, tSHh, k.t_s5w], w=[tp], inc=last)
            if gi % 2 == 0:
                cx.op("scalar", lambda h, pb=pb, b=b, gi=gi: h.copy(ybuf[b][:, gi, :], pb[:, 0:256]), r=[tp], w=[tyb[b]])
            else:
                cx.op(V, lambda h, pb=pb, b=b, gi=gi: h.tensor_copy(ybuf[b][:, gi, :], pb[:, 0:256]), r=[tp], w=[tyb[b]])
        for t in range(8):
            q0 = 32 * (t // 2)
            pb = k.psum[2 + t % 4]
            tp = k.tpsum[2 + t % 4]
            for gi in range(8):
                cx.op("tensor", lambda h, pb=pb, t=t, gi=gi, q0=q0, b=b: h.matmul(pb[:, 0:256], k.selT[q0:q0 + 32, t % 2, gi, :], ybuf[b][q0:q0 + 32, gi, :], start=(gi == 0), stop=(gi == 7), tile_position=(q0, 0)),
                      r=[tyb[b]], w=[tp], inc=(gi == 7))
            dstv = pap(gT, 0, 128, ct * 2048 + t, [[8, 256]])
            cx.op("scalar", lambda h, pb=pb, dstv=dstv: h.activation(dstv, pb[:, 0:256], AF.Gelu), r=[tp], w=[tgT[ct]])
        cx.op("vector", lambda h, ct=ct: h.tensor_copy(gTb[:, ct, :], gT[:, ct, :]), r=[tgT[ct]], w=[tgTb[ct]])
    k.dbg_add("s5_g", gT, tgT)
    if S5STOP == 'c':
        cx.barrier(); ar.release(m0); return
    wg = ar.alloc([4, 512], BF16)
    twg = Trk()
    wsrc = D["s5_w_glu"]
    cx.dma("gpsimd", wg, bass.AP(wsrc.tensor, wsrc.offset, [[512, 128], [512 * 128, 4], [1, 512]]), cx.fresh('sw'), w=[twg])
    bgl = ar.alloc([4], F32)
    nw = ar.alloc([4], F32)
    tb = Trk()
    sl_ = cx.fresh()
    cx.dma("sync", bgl, D["s5_bglu"], sl_, w=[tb])
    cx.dma("sync", nw, D["s5_normw"], sl_, w=[tb])
    sig = ar.alloc([4, 512], BF16)
    tsig = Trk()
    sq = ar.alloc([4, 512], BF16)
    tsq = Trk()
    rs = ar.alloc([512], F32)
    trs = Trk()
    for nck in range(4):
        ts = slice(nck * 512, (nck + 1) * 512)
        for co in range(4):
            pb = k.psum[co % 2]
            tp = k.tpsum[co % 2]
            for ci in range(4):
                cx.op("tensor", lambda h, pb=pb, co=co, ci=ci, ts=ts: h.matmul(pb[:, :], wg[:, ci, co * 128:(co + 1) * 128], gTb[:, ci, ts], start=(ci == 0), stop=(ci == 3)),
                      r=[twg] + tgTb, w=[tp], inc=(ci == 3))
            cx.op("scalar", lambda h, pb=pb, co=co: h.activation(sig[:, co, :], pb[:, :], AF.Sigmoid, bias=bgl[:, co:co + 1]), r=[tp, tb], w=[tsig])
        for co in range(4):
            cx.op(V, lambda h, co=co, ts=ts: h.tensor_tensor(out=gT[:, co, ts], in0=gT[:, co, ts], in1=sig[:, co, :], op=ALU.mult), r=[tsig, tgT[co]], w=[tgT[co]])
            cx.op("scalar", lambda h, co=co, ts=ts: h.activation(sq[:, co, :], gT[:, co, ts], AF.Square), r=[tgT[co]], w=[tsq])
        pb = k.psum[2 + nck % 2]
        tp = k.tpsum[2 + nck % 2]
        for co in range(4):
            cx.op("tensor", lambda h, pb=pb, co=co: h.matmul(pb[:, :], k.onesb, sq[:, co, :], start=(co == 0), stop=(co == 3)), r=[tsq], w=[tp], inc=(co == 3))
        cx.op("scalar", lambda h, pb=pb: h.activation(rs, pb[:, :], AF.Sqrt, scale=1.0 / 512, bias=k.epsc), r=[tp], w=[trs])
        cx.op(V, lambda h: h.reciprocal(rs, rs), r=[trs], w=[trs])
        for co in range(4):
            cx.op(V, lambda h, co=co, ts=ts: h.scalar_tensor_tensor(out=yT[:, co, ts], in0=gT[:, co, ts], scalar=nw[:, co:co + 1], in1=rs, op0=ALU.mult, op1=ALU.mult),
                  r=[tgT[co], trs, tb, tsq], w=[t_yT])
    k.dbg_add("s5_gl", gT, tgT)
    cx.barrier()
    ar.release(m0)


def build(in_shapes, stage="full", dbg_names=(), n_heads=4, n_experts=32):
    nc = bass.Bass("TRN2", target_bir_lowering=False)
    k = K()
    k.n_heads = n_heads
    k.n_experts = n_experts
    k.nc = nc
    D = {}
    for nm, (shape, dt) in in_shapes.items():
        D[nm] = nc.dram_tensor(nm, list(shape), dt, kind="ExternalInput").ap()
    k.D = D
    out = nc.dram_tensor("out", [S, DM], F32, kind="ExternalOutput").ap()
    k.dbg = {}
    k.dbg_req = set(dbg_names)

    with contextlib.ExitStack() as st:
        cx = Ctx(nc, st)
        k.cx = cx

        def finish():
            deps = [(s_.key, s_.total) for s_ in cx.slots if s_.total > 0]
            cx.wait_deps("sync", deps + [(e, cx.cnt[e]) for e in ENGS if e != "sync" and cx.cnt[e] > 0])
            with nc.Block() as block:
                cx.emit_all(block)
            k.n_ops = cx.n_ops
            return nc, k
        k.slot_c = cx.slot("c")
        k.slot_w = cx.slot("w")
        k.slot_x = [cx.slot("x0"), cx.slot("x1")]
        k.slot_o = cx.slot("o")
        k.psum = [cx.ps("ps%d" % i, [128, 512], F32) for i in range(8)]
        k.psum = [p[:, :] for p in k.psum]
        k.tpsum = [Trk("ps%d" % i, excl=True) for i in range(8)]
        k.ident = cx.sb("ident", [128, 128], F32)[:, :]
        k.identb = cx.sb("identb", [128, 128], BF16)[:, :]
        k.ones = cx.sb("ones", [128, 128], F32)[:, :]
        k.onesb = cx.sb("onesb", [128, 128], BF16)[:, :]
        k.epsc = cx.sb("epsc", [128, 1], F32)[:, :]
        k.selT = cx.sb("selT", [128, 2, 8, 128], BF16)[:, :, :, :]
        tc = Trk()
        cx.dma("sync", k.ident, D["ident"], k.slot_c, w=[tc])
        cx.dma("sync", k.identb, D["identb"], k.slot_c, w=[tc])
        cx.dma("sync", k.ones, D["ones"], k.slot_c, w=[tc])
        cx.dma("gpsimd", k.onesb, D["ones"], cx.fresh("sw"), w=[tc])
        cx.op("vector", lambda h: h.memset(k.epsc, EPS), w=[tc])
        cx.dma("sync", k.selT, D["selT"], k.slot_c, w=[tc])
        k.s5A1 = cx.sb("s5A1", [128, 64], F32)[:, :]
        k.s5A2 = cx.sb("s5A2", [128, 64], F32)[:, :]
        ar = Arena(cx, 51456)
        k.ar = ar
        cx.barrier()

        def dbg_add(name, ap, trks):
            if name in k.dbg_req:
                shape = list(ap.shape)
                dt_ = F32
                o = nc.dram_tensor("dbg_" + name, shape, dt_, kind="ExternalOutput").ap()
                cx.dma("gpsimd" if ap.dtype != F32 else "sync", o, ap, cx.fresh("sw" if ap.dtype != F32 else "hw"), r=list(trks))
        k.dbg_add = dbg_add

        regA = ar.alloc([NT * 1024], F32)
        arA = Arena(cx, NT * 1024, base=regA)
        k.arA = arA
        yT = ar.alloc([8, 2048], BF16)
        t_yT = Trk()
        k.s5U = arA.alloc([32, 256], BF16)
        k.t_s5U = Trk()
        m_h = arA.mark()
        k.hT = arA.alloc([8, 2048], BF16)
        k.t_hT = Trk()

        m1 = ar.mark()
        xt = [ar.alloc([1024], F32) for _ in range(2)]
        txt = [Trk(), Trk()]

        def src_x(i):
            b = i % 2
            cx.dma("sync", xt[b], D["x"][i * 128:(i + 1) * 128, :], k.slot_x[b], w=[txt[b]])
            return xt[b], txt[b]
        norm_transpose(k, "mix", src_x, NT, D["norm_mix"], k.hT, BF16, k.t_hT)
        cx.barrier()
        ar.release(m1)

        if stage == 'p1':
            return finish()
        s5_build_U(k)
        if stage == 'U':
            return finish()
        mg = ar.mark()
        gdn_setup(k)
        for hd in range(k.n_heads):
            gdn_head(k, hd, yT, t_yT)
        ar.release(mg)
        k.dbg_add("ygdnT", yT[:, 4:8, :], [t_yT])
        if stage == 'gdn':
            return finish()
        cx.barrier()
        arA.release(m_h)
        k.s5AT = arA.alloc([64, 128], BF16)
        k.t_s5at = Trk()
        k.s5CS = arA.alloc([2, 16, 2, 128], BF16)
        k.s5TT = arA.alloc([32, 128], BF16)
        k.t_s5tt = Trk()
        s5_prep(k)
        if stage == 's5prep':
            return finish()
        s5_main(k, yT[:, 0:4, :], t_yT)
        k.dbg_add("ys5T", yT[:, 0:4, :], [t_yT])
        if stage == "s5":
            return finish()
        if True:
            cx.barrier()
            k.xacc = regA.rearrange('p (a b) -> p a b', a=NT)
            k.txacc = [Trk() for _ in range(NT)]
            out_proj(k, yT, t_yT)
            k.dbg_add("x1", k.xacc, k.txacc)
            if stage == 'oproj':
                return finish()
            xattn(k)
            if stage == 'xattn':
                return finish()
            k.dbg_add("x2", k.xacc, k.txacc)
            moe(k)
            k.dbg_add("x3", k.xacc, k.txacc + [t_ for p_ in k.txh for t_ in p_])
            final_norm(k, out)

        return finish()


def host_inputs(inp, b):
    m = {}
    m["x"] = np.ascontiguousarray(inp["x"][b])
    m["mem"] = np.ascontiguousarray(inp["mem"][b])
    m["norm_mix"] = inp["norm_mix"][0]
    m["w_in"] = inp["w_in"][0]
    m["w_out"] = inp["w_out"][0]
    m["s5_w_glu"] = inp["s5_w_glu"][0]
    m.update(host_s5(inp))
    cv = inp["gdn_conv"][0]
    m["gdn_convw"] = np.ascontiguousarray(cv.reshape(5, 3, 4, 128).transpose(3, 2, 1, 0))
    for nm in ("gdn_a_log_f", "gdn_dt_bias_f", "gdn_a_log_b", "gdn_dt_bias_b"):
        m[nm] = inp[nm][0]
    m["gdn_norm"] = inp["gdn_norm"][0]
    for nm in ("norm_xattn", "norm_mem", "xa_wq", "xa_wk", "xa_wv", "xa_wo", "norm_moe", "router_group_w", "router_group_b",
               "router_expert_w", "router_expert_b", "moe_w_gate", "moe_w_up", "moe_w_down"):
        m[nm] = inp[nm][0]
    m["norm_final"] = inp["norm_final"]
    m.update(host_consts())
    return m


def gdn_setup(k):
    cx, ar, D = k.cx, k.ar, k.D
    V = "vector"
    G = K()
    k.G = G
    G.mask = ar.alloc([8, 128], F32)
    G.tmask = Trk()
    cx.dma("sync", G.mask, D["gmask"], cx.fresh(), w=[G.tmask])
    wsm = ar.alloc([8, 16], BF16)
    tw = Trk()
    load_w_cols(k, D["w_in"], 2560, 16, wsm, tw, cx.fresh('sw'))
    BA = ar.alloc([16, 16], F32)
    tBA = Trk()
    for i in range(NT):
        pb = k.psum[i % 4]
        tp = k.tpsum[i % 4]
        for c in range(8):
            cx.op("tensor", lambda h, pb=pb, c=c, i=i: h.matmul(pb[:, 0:16], k.hT[:, c, i * 128:(i + 1) * 128], wsm[:, c, :], start=(c == 0), stop=(c == 7)),
                  r=[tw, k.t_hT], w=[tp], inc=(c == 7))
        cx.op("scalar", lambda h, pb=pb, i=i: h.copy(BA[:, i, :], pb[:, 0:16]), r=[tp], w=[tBA])
    pr = ar.alloc([4, 4], F32)
    tpr = Trk()
    sl_ = cx.fresh()
    for j, nm in enumerate(("gdn_a_log_f", "gdn_dt_bias_f", "gdn_a_log_b", "gdn_dt_bias_b")):
        cx.dma("sync", pr[:, j, :], dram_bcast(D[nm], 128, 4), sl_, w=[tpr])
    G.nw = ar.alloc([128], F32)
    cx.dma("sync", G.nw, dram_bcast(D["gdn_norm"], 128, 128), sl_, w=[tpr])
    G.tpr = tpr
    T = Trk()
    G.T = T
    G.beta, G.nb, G.gc, G.eg, G.neg, G.ed = [], [], [], [], [], []
    def per_dir(d):
        beta = ar.alloc([16, 4], F32)
        nb = ar.alloc([16, 4], F32)
        g = ar.alloc([16, 4], F32)
        gc = ar.alloc([16, 4], F32)
        gt = ar.alloc([16, 4], F32)
        eg = ar.alloc([16, 4], F32)
        neg = ar.alloc([16, 4], F32)
        ed = ar.alloc([16, 4], F32)
        ea = ar.alloc([4], F32)
        braw = BA[:, :, d * 4:(d + 1) * 4]
        araw = BA[:, :, 8 + d * 4:8 + (d + 1) * 4]
        cx.op("scalar", lambda h: h.activation(beta, braw, AF.Sigmoid), r=[tBA, T], w=[T])
        cx.op(V, lambda h: h.tensor_scalar(nb, beta, -1.0, None, op0=ALU.mult), r=[T], w=[T])
        cx.op("scalar", lambda h: h.activation(ea, pr[:, 2 * d, :], AF.Exp), r=[tpr, T], w=[T])
        cx.op(V, lambda h: h.tensor_tensor(out=g, in0=araw, in1=pr[:, 2 * d + 1, :].unsqueeze(1).to_broadcast([128, 16, 4]), op=ALU.add), r=[tBA, tpr, T], w=[T])
        cx.op("scalar", lambda h: h.activation(g, g, AF.Exp), r=[T], w=[T])
        cx.op("scalar", lambda h: h.activation(g, g, AF.Ln, bias=1.0), r=[T], w=[T])
        cx.op(V, lambda h: h.scalar_tensor_tensor(out=g, in0=g, scalar=-1.0, in1=ea.unsqueeze(1).to_broadcast([128, 16, 4]), op0=ALU.mult, op1=ALU.mult), r=[T], w=[T])
        g2 = g.rearrange("p a b -> p (a b)")
        pb = k.psum[4 + d]
        tp = k.tpsum[4 + d]
        cx.op("tensor", lambda h, pb=pb, d=d: h.matmul(pb[:, 0:64], G.mask[:, d, :], g2, start=True, stop=True), r=[T, G.tmask], w=[tp])
        cx.op("tensor", lambda h, pb=pb: h.matmul(pb[:, 64:128], G.mask[:, 6, :], g2, start=True, stop=True), r=[T, G.tmask], w=[tp])
        cx.op(V, lambda h, pb=pb: h.tensor_copy(gc.rearrange("p a b -> p (a b)"), pb[:, 0:64]), r=[tp], w=[T])
        cx.op(V, lambda h, pb=pb: h.tensor_tensor(out=gt.rearrange("p a b -> p (a b)"), in0=pb[:, 64:128], in1=gc.rearrange("p a b -> p (a b)"), op=ALU.subtract), r=[tp, T], w=[T])
        cx.op("scalar", lambda h: h.activation(eg, gc, AF.Exp), r=[T], w=[T])
        cx.op("scalar", lambda h: h.activation(ed, gt, AF.Exp), r=[T], w=[T])
        cx.op(V, lambda h: h.tensor_scalar(neg, eg, -1.0, None, op0=ALU.mult), r=[T], w=[T])
        G.g = getattr(G, "g", []) + [g]
        G.beta.append(beta); G.nb.append(nb); G.gc.append(gc); G.eg.append(eg); G.neg.append(neg); G.ed.append(ed)
    per_dir(0)
    per_dir(1)
    G.osum = ar.alloc([16, 128], F32)
    G.tosum = [Trk() for _ in range(NT)]


def gdn_head(k, hd, yT, t_yT):
    cx, ar, D, G = k.cx, k.ar, k.D, k.G
    V = "vector"
    m0 = ar.mark()
    qnT = ar.alloc([2048], BF16)
    knT = ar.alloc([2048], BF16)
    Ktok = ar.alloc([16, 128], BF16)
    Vtok = ar.alloc([16, 128], BF16)
    tq, tk_, tKt, tVt = Trk(), Trk(), Trk(), Trk()
    wz = ar.alloc([8, 128], BF16)
    twz = Trk()
    load_w_cols(k, D["w_in"], 512 + 1536 + hd * 128, 128, wz, twz, cx.fresh('sw'))
    mA = ar.mark()
    w3 = [ar.alloc([8, 128], BF16) for _ in range(3)]
    tw3 = [Trk() for _ in range(3)]
    for j in range(3):
        load_w_cols(k, D["w_in"], 512 + j * 512 + hd * 128, 128, w3[j], tw3[j], cx.fresh('sw'))
    cw = ar.alloc([3, 5], F32)
    tcw = Trk()
    cx.dma("sync", cw, D["gdn_convw"][:, hd, :, :], cx.fresh(), w=[tcw])
    diag = ar.alloc([15, 128], BF16)
    tdg = Trk()
    for j in range(3):
        for t in range(5):
            cx.op("vector", lambda h, j=j, t=t: h.tensor_scalar(diag[:, j * 5 + t, :], k.identb, cw[:, j, t:t + 1], None, op0=ALU.mult), r=[tcw], w=[tdg])
    import os
    ALV = int(os.environ.get("GDN_ALV", "9"))
    if ALV == 0:
        cx.barrier(); ar.release(m0); return
    raw = [ar.alloc([2052], BF16) for _ in range(2)]
    traw = [Trk(), Trk()]
    for b in range(2):
        cx.op("gpsimd", lambda h, b=b: h.memset(raw[b][:, 0:2], 0.0), w=[traw[b]])
        cx.op("gpsimd", lambda h, b=b: h.memset(raw[b][:, 2050:2052], 0.0), w=[traw[b]])
    act = ar.alloc([2048], F32)
    tact = Trk()
    vT = ar.alloc([2048], BF16)
    tvT = Trk()
    sqb = ar.alloc([2048], BF16)
    tsqb = Trk()
    rn = [ar.alloc([512], F32) for _ in range(4)]
    trn = [Trk() for _ in range(4)]
    tactn = [Trk() for _ in range(4)]
    tsqn = [Trk() for _ in range(4)]
    if ALV == 1:
        cx.barrier(); ar.release(m0); return
    for j in range(3):
        b = j % 2

        def consume(n, pb, tp, b=b):
            cx.op(V if n % 2 else "scalar", (lambda h: h.tensor_copy(raw[b][:, 2 + n * 512:2 + (n + 1) * 512], pb[:, :])) if n % 2 else
                  (lambda h: h.copy(raw[b][:, 2 + n * 512:2 + (n + 1) * 512], pb[:, :])), r=[tp], w=[traw[b]])
        proj_fm(k, w3[j], tw3[j], consume)
        for n in range(4):
            pb = k.psum[4 + n]
            tp = k.tpsum[4 + n]
            for t in range(5):
                cx.op("tensor", lambda h, pb=pb, t=t, n=n, j=j, b=b: h.matmul(pb[:, :], diag[:, j * 5 + t, :], raw[b][:, n * 512 + t:n * 512 + t + 512], start=(t == 0), stop=(t == 4)),
                      r=[tdg, traw[b]], w=[tp], inc=(t == 4))
        for n in range(4):
            pb = k.psum[4 + n]
            tp = k.tpsum[4 + n]
            ts = slice(n * 512, (n + 1) * 512)
            if j == 2:
                cx.op("scalar", lambda h, pb=pb, ts=ts: h.activation(vT[:, ts], pb[:, :], AF.Silu), r=[tp], w=[tvT])
            else:
                cx.op("scalar", lambda h, pb=pb, ts=ts: h.activation(act[:, ts], pb[:, :], AF.Silu), r=[tp], w=[tactn[n]])
        if j < 2 and ALV > 2:
            for n in range(4):
                ts = slice(n * 512, (n + 1) * 512)
                cx.op("scalar", lambda h, ts=ts: h.activation(sqb[:, ts], act[:, ts], AF.Square), r=[tactn[n]], w=[tsqn[n]])
            for n in range(4):
                ts = slice(n * 512, (n + 1) * 512)
                pb2 = k.psum[n]
                tp2 = k.tpsum[n]
                cx.op("tensor", lambda h, pb2=pb2, ts=ts: h.matmul(pb2[:, :], k.onesb, sqb[:, ts], start=True, stop=True), r=[tsqn[n]], w=[tp2])
            for n in range(4):
                pb2 = k.psum[n]
                tp2 = k.tpsum[n]
                cx.op("scalar", lambda h, pb2=pb2, n=n: h.activation(rn[n], pb2[:, :], AF.Sqrt, bias=k.epsc), r=[tp2], w=[trn[n]])
            for n in range(4):
                cx.op(V, lambda h, n=n: h.reciprocal(rn[n], rn[n]), r=[trn[n]], w=[trn[n]])
            dstT, tdst, scl = (qnT, tq, 128.0 ** -0.5) if j == 0 else (knT, tk_, 1.0)
            for n in range(4):
                ts = slice(n * 512, (n + 1) * 512)
                cx.op(V, lambda h, ts=ts, n=n, dstT=dstT, scl=scl: h.scalar_tensor_tensor(out=dstT[:, ts], in0=act[:, ts], scalar=scl, in1=rn[n], op0=ALU.mult, op1=ALU.mult),
                      r=[tactn[n], trn[n]], w=[tdst])
    if ALV <= 3:
        cx.barrier(); ar.release(m0); return
    TV = int(os.environ.get("GDN_TV", "0"))
    for i in range(NT):
        pb = k.psum[i % 2].bitcast(BF16)
        tp = k.tpsum[i % 2]
        if TV == 0:
            cx.op("tensor", lambda h, pb=pb, i=i: h.transpose(pb[:, 0:128], knT[:, i * 128:(i + 1) * 128], k.identb), r=[tk_], w=[tp])
            cx.op("tensor", lambda h, pb=pb, i=i: h.transpose(pb[:, 128:256], vT[:, i * 128:(i + 1) * 128], k.identb), r=[tvT], w=[tp])
            cx.op("scalar", lambda h, pb=pb, i=i: h.copy(Ktok[:, i, :], pb[:, 0:128]), r=[tp], w=[tKt])
            cx.op(V, lambda h, pb=pb, i=i: h.tensor_copy(Vtok[:, i, :], pb[:, 128:256]), r=[tp], w=[tVt])
        elif TV == 1:
            cx.op("tensor", lambda h, pb=pb, i=i: h.transpose(pb[:, 0:128], knT[:, i * 128:(i + 1) * 128], k.identb), r=[tk_], w=[tp])
            cx.op("scalar", lambda h, pb=pb, i=i: h.copy(Ktok[:, i, :], pb[:, 0:128]), r=[tp], w=[tKt])
        elif TV == 2:
            cx.op("tensor", lambda h, pb=pb, i=i: h.transpose(pb[:, 0:128], vT[:, i * 128:(i + 1) * 128], k.identb), r=[tvT], w=[tp])
            cx.op(V, lambda h, pb=pb, i=i: h.tensor_copy(Vtok[:, i, :], pb[:, 0:128]), r=[tp], w=[tVt])
    if hd == 0:
        k.dbg_add("gdn_qn", qnT, [tq])
        k.dbg_add("gdn_kn", knT, [tk_])
        k.dbg_add("gdn_vtok", Vtok, [tVt])
    cx.barrier()
    ar.release(mA)
    STOP = os.environ.get("GDN_STOP", "")
    if STOP == "A":
        ar.release(m0)
        return
    qgT = [ar.alloc([2048], BF16) for _ in range(2)]
    Kd = [ar.alloc([16, 128], BF16) for _ in range(2)]
    Pm = [ar.alloc([16, 128], BF16) for _ in range(2)]
    QKm = [ar.alloc([16, 128], BF16) for _ in range(2)]
    etot = [ar.alloc([32], F32) for _ in range(2)]
    tqg = [[Trk() for _ in range(NT)] for _ in range(2)]
    tKd = [[Trk() for _ in range(NT)] for _ in range(2)]
    tPm = [[Trk() for _ in range(NT)] for _ in range(2)]
    tQK = [[Trk() for _ in range(NT)] for _ in range(2)]
    tet = [[Trk() for _ in range(NT)] for _ in range(2)]
    NI = 8
    mN = ar.mark()
    NDT = BF16 if os.environ.get('GDN_NEU', 'bf16') == 'bf16' else F32
    nid = k.identb if NDT == BF16 else k.ident
    Xb = [[ar.alloc([128], NDT) for _ in range(2)] for _ in range(NI)]
    XTb = [[ar.alloc([128], NDT) for _ in range(2)] for _ in range(NI)]
    Pb = [[ar.alloc([128], NDT) for _ in range(2)] for _ in range(NI)]

    def tview(pn_, c0):
        return pn_[:, c0:c0 + 128] if NDT == F32 else pn_.bitcast(BF16)[:, 2 * c0:2 * c0 + 128]
    tX = [Trk() for _ in range(NI)]
    EGB = [ar.alloc([128], F32) for _ in range(NI)]
    ET = [ar.alloc([128], F32) for _ in range(NI)]
    ETs = ET
    tE = [Trk() for _ in range(NI)]
    tEG = [Trk() for _ in range(NI)]
    tXP = [Trk() for _ in range(NI)]
    insts = [(i, d) for i in range(NT) for d in range(2)]
    for g0 in range(0, len(insts), NI):
        grp = insts[g0:g0 + NI]
        info = []
        for s_, (i, d) in enumerate(grp):
            info.append(dict(s_=s_, i=i, d=d, tsl=slice(i * 128, (i + 1) * 128),
                             col=pap(G.g[d], 0, 128, i * 4 + hd, [[0, 128]]),
                             gcc=G.gc[d][:, i, hd:hd + 1], nbc=G.nb[d][:, i, hd:hd + 1], edc=G.ed[d][:, i, hd:hd + 1],
                             pb=k.psum[s_], tp=k.tpsum[s_]))
        for q_ in info:
            s_, i, d, tsl, col, pb, tp = q_["s_"], q_["i"], q_["d"], q_["tsl"], q_["col"], q_["pb"], q_["tp"]
            cx.op("tensor", lambda h, pb=pb, col=col, d=d: h.matmul(pb[:, 0:128], col, G.mask[:, d, :], start=True, stop=True), r=[G.T, G.tmask], w=[tp], inc=False)
            cx.op("tensor", lambda h, pb=pb, tsl=tsl: h.matmul(pb[:, 128:256], knT[:, tsl], knT[:, tsl], start=True, stop=True), r=[tk_], w=[tp], inc=False)
            cx.op("tensor", lambda h, pb=pb, tsl=tsl: h.matmul(pb[:, 256:384], knT[:, tsl], qnT[:, tsl], start=True, stop=True), r=[tk_, tq], w=[tp])
        for q_ in info:
            s_, i, d, pb, tp, gcc = q_["s_"], q_["i"], q_["d"], q_["pb"], q_["tp"], q_["gcc"]
            cx.op("scalar", lambda h, pb=pb, s_=s_: h.activation(EGB[s_], pb[:, 0:128], AF.Exp), r=[tp], w=[tEG[s_]])
            cx.op(V, lambda h, pb=pb, s_=s_, gcc=gcc, d=d: h.scalar_tensor_tensor(out=ET[s_], in0=pb[:, 0:128], scalar=gcc, in1=G.mask[:, 2 + d, :], op0=ALU.subtract, op1=ALU.min),
                  r=[tp, G.T, G.tmask], w=[tE[s_]])
        for q_ in info:
            s_, i, d, tsl = q_["s_"], q_["i"], q_["d"], q_["tsl"]
            cx.op("scalar", lambda h, s_=s_: h.activation(ET[s_], ET[s_], AF.Exp), r=[tE[s_]], w=[tE[s_]])
            cx.op(V, lambda h, s_=s_, tsl=tsl, d=d: h.tensor_tensor(out=qgT[d][:, tsl], in0=qnT[:, tsl], in1=EGB[s_], op=ALU.mult), r=[tq, tEG[s_]], w=[tqg[d][i]])
        for q_ in info:
            s_, i, d, pb, tp, edc = q_["s_"], q_["i"], q_["d"], q_["pb"], q_["tp"], q_["edc"]
            c0, c1 = (63, 127) if d == 0 else (0, 64)
            cx.op("scalar", lambda h, s_=s_, d=d, i=i, c0=c0: h.copy(etot[d][:, 2 * i:2 * i + 1], EGB[s_][:, c0:c0 + 1]), r=[tEG[s_]], w=[tet[d][i]])
            cx.op("scalar", lambda h, s_=s_, d=d, i=i, c1=c1: h.copy(etot[d][:, 2 * i + 1:2 * i + 2], EGB[s_][:, c1:c1 + 1]), r=[tEG[s_]], w=[tet[d][i]])
            cx.op("scalar", lambda h, d=d, i=i, edc=edc: h.activation(Kd[d][:, i, :], Ktok[:, i, :], AF.Identity, scale=edc), r=[tKt, G.T], w=[tKd[d][i]])
            cx.op(V, lambda h, pb=pb, s_=s_, d=d, i=i: h.tensor_tensor(out=QKm[d][:, i, :], in0=pb[:, 256:384], in1=ET[s_], op=ALU.mult), r=[tp, tE[s_]], w=[tQK[d][i]])
        for q_ in info:
            s_, d = q_["s_"], q_["d"]
            cx.op(V, lambda h, s_=s_, d=d: h.tensor_tensor(out=ETs[s_], in0=ET[s_], in1=G.mask[:, 4 + d, :], op=ALU.mult), r=[tE[s_], G.tmask], w=[tE[s_]])
        for q_ in info:
            s_, pb, tp, nbc = q_["s_"], q_["pb"], q_["tp"], q_["nbc"]
            cx.op(V, lambda h, pb=pb, s_=s_, nbc=nbc: h.scalar_tensor_tensor(out=Xb[s_][0], in0=pb[:, 128:256], scalar=nbc, in1=ETs[s_], op0=ALU.mult, op1=ALU.mult),
                  r=[tp, tE[s_], G.T], w=[tX[s_]])
        for q_ in info:
            s_, pn, tn = q_["s_"], q_["pb"], q_["tp"]
            cx.op("tensor", lambda h, pn=pn, s_=s_: h.transpose(tview(pn, 384), Xb[s_][0], nid), r=[tX[s_]], w=[tn])
            cx.op(V, lambda h, s_=s_: h.tensor_tensor(out=Pb[s_][0], in0=Xb[s_][0], in1=nid, op=ALU.add), r=[tX[s_]], w=[tXP[s_]])
            cx.op("scalar", lambda h, pn=pn, s_=s_: h.copy(XTb[s_][0], tview(pn, 384)), r=[tn], w=[tX[s_]])
        for L in range(1, 6):
            a, b_ = (L - 1) % 2, L % 2
            for s_, (i, d) in enumerate(grp):
                pn = k.psum[s_]
                tn = k.tpsum[s_]
                if L < 5:
                    cx.op("tensor", lambda h, pn=pn, s_=s_, a=a: h.matmul(pn[:, 0:128], XTb[s_][a], Xb[s_][a], start=True, stop=True), r=[tX[s_]], w=[tn], inc=False)
                cx.op("tensor", lambda h, pn=pn, s_=s_, a=a: h.matmul(pn[:, 128:256], Xb[s_][a], XTb[s_][a], start=True, stop=True), r=[tX[s_]], w=[tn])
                e1, e2 = ("scalar", V) if s_ % 2 == 0 else (V, "scalar")
                if L < 5:
                    if e1 == "scalar":
                        cx.op("scalar", lambda h, pn=pn, s_=s_, b_=b_: h.copy(Xb[s_][b_], pn[:, 0:128]), r=[tn], w=[tX[s_]])
                    else:
                        cx.op(V, lambda h, pn=pn, s_=s_, b_=b_: h.tensor_copy(Xb[s_][b_], pn[:, 0:128]), r=[tn], w=[tX[s_]])
                if e2 == "scalar":
                    cx.op("scalar", lambda h, pn=pn, s_=s_, b_=b_: h.copy(XTb[s_][b_], pn[:, 128:256]), r=[tn], w=[tX[s_]])
                else:
                    cx.op(V, lambda h, pn=pn, s_=s_, b_=b_: h.tensor_copy(XTb[s_][b_], pn[:, 128:256]), r=[tn], w=[tX[s_]])
            for s_, (i, d) in enumerate(grp):
                pn = k.psum[s_]
                tn = k.tpsum[s_]
                cx.op("tensor", lambda h, pn=pn, s_=s_, a=a, b_=b_: h.matmul(pn[:, 256:384], XTb[s_][b_], Pb[s_][a], start=True, stop=True), r=[tX[s_], tXP[s_]], w=[tn])
                if L < 5:
                    cx.op(V, lambda h, pn=pn, s_=s_, a=a, b_=b_: h.tensor_tensor(out=Pb[s_][b_], in0=pn[:, 256:384], in1=Pb[s_][a], op=ALU.add), r=[tn, tXP[s_]], w=[tXP[s_]])
                else:
                    cx.op(V, lambda h, pn=pn, s_=s_, a=a, d=d, i=i: h.tensor_tensor(out=Pm[d][:, i, :], in0=pn[:, 256:384], in1=Pb[s_][a], op=ALU.add), r=[tn, tXP[s_]], w=[tPm[d][i], tXP[s_]])
    if STOP == "B":
        cx.barrier()
        ar.release(m0)
        return
    ar.release(mN)
    Sf = [[ar.alloc([128], F32) for _ in range(2)] for _ in range(2)]
    Sb = [ar.alloc([128], BF16) for _ in range(2)]
    Rp = [ar.alloc([128], BF16) for _ in range(2)]
    vn = [ar.alloc([128], BF16) for _ in range(2)]
    tS = [Trk(), Trk()]
    tR = [Trk(), Trk()]
    tv = [Trk(), Trk()]
    cx.op("gpsimd", lambda h: h.memset(G.osum, 0.0), w=G.tosum)
    for d in range(2):
        cx.op("gpsimd", lambda h, d=d: h.memset(Sf[d][0], 0.0), w=[tS[d]])
        cx.op("gpsimd", lambda h, d=d: h.memset(Sb[d], 0.0), w=[tS[d]])
        cx.op("gpsimd", lambda h, d=d: h.memset(Rp[d], 0.0), w=[tR[d]])
        cx.op("gpsimd", lambda h, d=d: h.memset(vn[d], 0.0), w=[tv[d]])
    for step in range(32):
        for d in range(2):
            if d == 0:
                i, hh = step // 2, step % 2
            else:
                i, hh = 15 - step // 2, 1 - step % 2
            tsl = slice(i * 128, (i + 1) * 128)
            ps_ = slice(hh * 64, (hh + 1) * 64)
            cur, nxt = step % 2, (step + 1) % 2
            pcs = [k.psum[4 * d + q_] for q_ in range(4)]
            tcs = [k.tpsum[4 * d + q_] for q_ in range(4)]
            negc = G.neg[d][ps_, i, hd:hd + 1]
            btc = G.beta[d][ps_, i, hd:hd + 1]
            p1, pv_, po_, pst = pcs
            t1_, tv_, to_, tst = tcs
            cx.op("tensor", lambda h, p1=p1, tsl=tsl, d=d: h.matmul(p1[:, 0:128], knT[:, tsl], Sb[d], start=True, stop=True), r=[tk_, tS[d]], w=[t1_])
            cx.op(V, lambda h, p1=p1, ps_=ps_, negc=negc, d=d, i=i: h.scalar_tensor_tensor(out=Rp[d][ps_, :], in0=p1[ps_, 0:128], scalar=negc, in1=Vtok[ps_, i, :], op0=ALU.mult, op1=ALU.add),
                  r=[t1_, tVt, G.T], w=[tR[d]])
            cx.op("tensor", lambda h, pv_=pv_, ps_=ps_, d=d, i=i: h.matmul(pv_[:, 0:128], Pm[d][ps_, i, :], Rp[d][ps_, :], start=True, stop=True), r=[tPm[d][i], tR[d]], w=[tv_])
            cx.op("scalar", lambda h, pv_=pv_, ps_=ps_, btc=btc, d=d: h.activation(vn[d][ps_, :], pv_[ps_, 0:128], AF.Identity, scale=btc), r=[tv_, G.T], w=[tv[d]])
            cx.op("tensor", lambda h, po_=po_, tsl=tsl, d=d: h.matmul(po_[:, 0:128], qgT[d][:, tsl], Sb[d], start=True, stop=False), r=[tqg[d][i], tS[d]], w=[to_], inc=False)
            cx.op("tensor", lambda h, po_=po_, ps_=ps_, d=d, i=i: h.matmul(po_[:, 0:128], QKm[d][ps_, i, :], vn[d][ps_, :], start=False, stop=True), r=[tQK[d][i], tv[d]], w=[to_])
            cx.op("tensor", lambda h, pst=pst, ps_=ps_, d=d, i=i: h.matmul(pst[:, 0:128], Kd[d][ps_, i, :], vn[d][ps_, :], start=True, stop=True), r=[tKd[d][i], tv[d]], w=[tst])
            cx.op("gpsimd" if False else V, lambda h, po_=po_, ps_=ps_, i=i: h.tensor_tensor(out=G.osum[ps_, i, :], in0=po_[ps_, 0:128], in1=G.osum[ps_, i, :], op=ALU.add), r=[to_, G.tosum[i]], w=[G.tosum[i]])
            etc = etot[d][:, 2 * i + hh:2 * i + hh + 1]
            cx.op(V, lambda h, pst=pst, d=d, cur=cur, nxt=nxt, etc=etc: h.scalar_tensor_tensor(out=Sf[d][nxt], in0=Sf[d][cur], scalar=etc, in1=pst[:, 0:128], op0=ALU.mult, op1=ALU.add),
                  r=[tst, tet[d][i], tS[d]], w=[tS[d]])
            cx.op("scalar", lambda h, d=d, nxt=nxt: h.copy(Sb[d], Sf[d][nxt]), r=[tS[d]], w=[tS[d]])
    if hd == 0:
        k.dbg_add("gdn_osum", G.osum, G.tosum)
    if STOP == "C":
        cx.barrier()
        ar.release(m0)
        return
    ss = ar.alloc([NT, 2], F32)
    tss = Trk()
    junk = ar.alloc([128], BF16)
    zs = [ar.alloc([128], F32) for _ in range(2)]
    tzs = [Trk(), Trk()]
    yb = [ar.alloc([128], BF16) for _ in range(2)]
    tyb = [Trk(), Trk()]
    for i in range(NT):
        cx.op("scalar", lambda h, i=i: h.activation(junk, G.osum[:, i, :], AF.Square, accum_out=ss[:, i, 0:1]), r=[G.tosum[i], tss], w=[tss])
    cx.op(V, lambda h: h.tensor_scalar(ss[:, :, 1:2], ss[:, :, 0:1], 1.0 / 128, EPS, op0=ALU.mult, op1=ALU.add), r=[tss], w=[tss])
    cx.op("scalar", lambda h: h.activation(ss[:, :, 1:2], ss[:, :, 1:2], AF.Sqrt), r=[tss], w=[tss])
    cx.op(V, lambda h: h.reciprocal(ss[:, :, 1:2], ss[:, :, 1:2]), r=[tss], w=[tss])
    for i in range(NT):
        b = i % 2
        pz = k.psum[b]
        tz = k.tpsum[b]
        for c in range(8):
            cx.op("tensor", lambda h, pz=pz, c=c, i=i: h.matmul(pz[:, 0:128], k.hT[:, c, i * 128:(i + 1) * 128], wz[:, c, :], start=(c == 0), stop=(c == 7)),
                  r=[twz, k.t_hT], w=[tz], inc=(c == 7))
        cx.op("scalar", lambda h, pz=pz, b=b: h.activation(zs[b], pz[:, 0:128], AF.Silu), r=[tz], w=[tzs[b]])
        s1 = ss[:, i, 1:2]
        cx.op(V, lambda h, i=i, s1=s1: h.scalar_tensor_tensor(out=G.osum[:, i, :], in0=G.osum[:, i, :], scalar=s1, in1=G.nw, op0=ALU.mult, op1=ALU.mult), r=[tss, G.tpr, G.tosum[i]], w=[G.tosum[i]])
        cx.op(V, lambda h, i=i, b=b: h.tensor_tensor(out=yb[b], in0=G.osum[:, i, :], in1=zs[b], op=ALU.mult), r=[G.tosum[i], tzs[b]], w=[tyb[b]])
        pt = k.psum[2 + b].bitcast(BF16)
        tt_ = k.tpsum[2 + b]
        cx.op("tensor", lambda h, pt=pt, b=b: h.transpose(pt[:, 0:128], yb[b], k.identb), r=[tyb[b]], w=[tt_])
        cx.op("scalar", lambda h, pt=pt, i=i: h.copy(yT[:, 4 + hd, i * 128:(i + 1) * 128], pt[:, 0:128]), r=[tt_], w=[t_yT])
    cx.barrier()
    ar.release(m0)


def out_proj(k, yT, t_yT):
    cx, ar, D = k.cx, k.ar, k.D
    m0 = ar.mark()
    wo = ar.alloc([8, 1024], BF16)
    two = Trk()
    wsrc = D["w_out"]
    sl_ = cx.fresh('sw')
    for c in range(8):
        cx.dma("gpsimd", wo[:, c, :], wsrc[c * 128:(c + 1) * 128, :], sl_, w=[two])
    slx = cx.fresh()
    for i in range(NT):
        cx.dma("sync", k.xacc[:, i, :], D["x"][i * 128:(i + 1) * 128, :], slx, w=[k.txacc[i]])
    for i in range(NT):
        k.txacc[i].w = (slx.key, slx.total)
    for i in range(NT):
        for half in range(2):
            pb = k.psum[(2 * i + half) % 4]
            tp = k.tpsum[(2 * i + half) % 4]
            for c in range(8):
                cx.op("tensor", lambda h, pb=pb, c=c, i=i, half=half: h.matmul(pb[:, :], yT[:, c, i * 128:(i + 1) * 128], wo[:, c, half * 512:(half + 1) * 512], start=(c == 0), stop=(c == 7)),
                      r=[t_yT, two], w=[tp], inc=(c == 7))
            xs = k.xacc[:, i, half * 512:(half + 1) * 512]
            cx.op("vector", lambda h, pb=pb, xs=xs: h.tensor_tensor(out=xs, in0=pb[:, :], in1=xs, op=ALU.add), r=[tp, k.txacc[i]], w=[k.txacc[i]])
    cx.barrier()
    ar.release(m0)


def xattn(k):
    cx, ar, D = k.cx, k.ar, k.D
    V = "vector"
    m0 = ar.mark()
    xnT = ar.alloc([8, 2048], BF16)
    t_xnT = Trk()
    memT = ar.alloc([8, 256], BF16)
    t_memT = Trk()
    m1 = ar.mark()
    mt = [ar.alloc([1024], F32) for _ in range(2)]
    tmt = [Trk(), Trk()]

    def src_mem(i):
        cx.dma("sync", mt[i], D["mem"][i * 128:(i + 1) * 128, :], cx.fresh(), w=[tmt[i]])
        return mt[i], tmt[i]
    norm_transpose(k, "mem", src_mem, 2, D["norm_mem"], memT, BF16, t_memT)
    ar.release(m1)
    norm_transpose(k, "xa", lambda i: (k.xacc[:, i, :], k.txacc[i]), NT, D["norm_xattn"], xnT, BF16, t_xnT, resident=True)
    k.dbg_add("xa_memT", memT, [t_memT])
    k.dbg_add("xa_xnT", xnT, [t_xnT])
    wq = [ar.alloc([8, 256], BF16) for _ in range(2)]
    wk = [ar.alloc([8, 256], BF16) for _ in range(2)]
    wv = [ar.alloc([8, 256], BF16) for _ in range(2)]
    wo = [ar.alloc([2, 1024], BF16) for _ in range(2)]
    tw = [Trk(), Trk()]
    sw = [cx.slot("xw0"), cx.slot("xw1")]
    kT = ar.alloc([2, 256], BF16)
    vh = ar.alloc([2, 256], BF16)
    tkv = Trk()
    qT = ar.alloc([2, 2048], BF16)
    tqT = Trk()
    E = [ar.alloc([2, 512], BF16) for _ in range(2)]
    tE = [Trk(), Trk()]
    rden = [ar.alloc([512], F32) for _ in range(2)]
    trd = [Trk(), Trk()]
    oTn = [ar.alloc([2, 512], BF16) for _ in range(2)]
    toT = [Trk(), Trk()]
    cx.barrier()
    k.txa = [[Trk(), Trk()] for _ in range(NT)]

    def load_head(hd):
        b = hd % 2
        c0 = hd * 256
        for (dst, nm) in ((wq[b], "xa_wq"), (wk[b], "xa_wk"), (wv[b], "xa_wv")):
            load_w_cols(k, D[nm], c0, 256, dst, tw[b], sw[b])
        src = D["xa_wo"]
        cx.dma("gpsimd", wo[b], bass.AP(src.tensor, src.offset + c0 * 1024, [[1024, 128], [128 * 1024, 2], [1, 1024]]), sw[b], w=[tw[b]])
    load_head(0)
    for hd in range(4):
        b = hd % 2
        if hd + 1 < 4:
            load_head(hd + 1)
        for dc in range(2):
            pb = k.psum[dc]
            tp = k.tpsum[dc]
            for c in range(8):
                cx.op("tensor", lambda h, pb=pb, c=c, dc=dc, b=b: h.matmul(pb[:, 0:256], wk[b][:, c, dc * 128:(dc + 1) * 128], memT[:, c, :], start=(c == 0), stop=(c == 7)),
                      r=[tw[b], t_memT], w=[tp], inc=(c == 7))
            cx.op("scalar", lambda h, pb=pb, dc=dc: h.copy(kT[:, dc, :], pb[:, 0:256]), r=[tp], w=[tkv])
        for mtile in range(2):
            pb = k.psum[2 + mtile]
            tp = k.tpsum[2 + mtile]
            for c in range(8):
                cx.op("tensor", lambda h, pb=pb, c=c, mtile=mtile, b=b: h.matmul(pb[:, 0:256], memT[:, c, mtile * 128:(mtile + 1) * 128], wv[b][:, c, :], start=(c == 0), stop=(c == 7)),
                      r=[tw[b], t_memT], w=[tp], inc=(c == 7))
            cx.op(V, lambda h, pb=pb, mtile=mtile: h.tensor_copy(vh[:, mtile, :], pb[:, 0:256]), r=[tp], w=[tkv])
        for dc in range(2):
            for n in range(4):
                pb = k.psum[4 + (dc * 4 + n) % 2]
                tp = k.tpsum[4 + (dc * 4 + n) % 2]
                for c in range(8):
                    cx.op("tensor", lambda h, pb=pb, c=c, dc=dc, n=n, b=b: h.matmul(pb[:, :], wq[b][:, c, dc * 128:(dc + 1) * 128], xnT[:, c, n * 512:(n + 1) * 512], start=(c == 0), stop=(c == 7)),
                          r=[tw[b], t_xnT], w=[tp], inc=(c == 7))
                if n % 2 == 0:
                    cx.op("scalar", lambda h, pb=pb, dc=dc, n=n: h.copy(qT[:, dc, n * 512:(n + 1) * 512], pb[:, :]), r=[tp], w=[tqT])
                else:
                    cx.op(V, lambda h, pb=pb, dc=dc, n=n: h.tensor_copy(qT[:, dc, n * 512:(n + 1) * 512], pb[:, :]), r=[tp], w=[tqT])
        if hd == 0:
            k.dbg_add("xa_qT", qT, [tqT])
            k.dbg_add("xa_kT", kT, [tkv])
            k.dbg_add("xa_vh", vh, [tkv])
        def emit_scores(n):
            eb = n % 2
            ts = slice(n * 512, (n + 1) * 512)
            for mtile in range(2):
                pb = k.psum[mtile]
                tp = k.tpsum[mtile]
                for dc in range(2):
                    cx.op("tensor", lambda h, pb=pb, dc=dc, mtile=mtile, ts=ts: h.matmul(pb[:, :], kT[:, dc, mtile * 128:(mtile + 1) * 128], qT[:, dc, ts], start=(dc == 0), stop=(dc == 1)),
                          r=[tkv, tqT], w=[tp], inc=(dc == 1))
                cx.op("scalar", lambda h, pb=pb, mtile=mtile, eb=eb: h.activation(E[eb][:, mtile, :], pb[:, :], AF.Exp, scale=1.0 / 16.0), r=[tp], w=[tE[eb]])

        def emit_rest(n):
            eb = n % 2
            pd = k.psum[2]
            tpd = k.tpsum[2]
            for mtile in range(2):
                cx.op("tensor", lambda h, pd=pd, mtile=mtile, eb=eb: h.matmul(pd[:, :], k.onesb, E[eb][:, mtile, :], start=(mtile == 0), stop=(mtile == 1)), r=[tE[eb]], w=[tpd], inc=(mtile == 1))
            cx.op(V, lambda h, pd=pd, eb=eb: h.reciprocal(rden[eb], pd[:, :]), r=[tpd], w=[trd[eb]])
            for dc in range(2):
                po = k.psum[3 + dc]
                tpo = k.tpsum[3 + dc]
                for mtile in range(2):
                    cx.op("tensor", lambda h, po=po, mtile=mtile, dc=dc, eb=eb: h.matmul(po[:, :], vh[:, mtile, dc * 128:(dc + 1) * 128], E[eb][:, mtile, :], start=(mtile == 0), stop=(mtile == 1)),
                          r=[tkv, tE[eb]], w=[tpo], inc=(mtile == 1))
                cx.op(V, lambda h, po=po, dc=dc, eb=eb: h.tensor_tensor(out=oTn[eb][:, dc, :], in0=po[:, :], in1=rden[eb], op=ALU.mult), r=[tpo, trd[eb]], w=[toT[eb]])
            for t in range(4):
                i = n * 4 + t
                for half in range(2):
                    pw_ = k.psum[5 + (t * 2 + half) % 3]
                    tpw = k.tpsum[5 + (t * 2 + half) % 3]
                    for dc in range(2):
                        cx.op("tensor", lambda h, pw_=pw_, dc=dc, t=t, half=half, b=b, eb=eb: h.matmul(pw_[:, :], oTn[eb][:, dc, t * 128:(t + 1) * 128], wo[b][:, dc, half * 512:(half + 1) * 512], start=(dc == 0), stop=(dc == 1)),
                              r=[toT[eb], tw[b]], w=[tpw], inc=(dc == 1))
                    xs = k.xacc[:, i, half * 512:(half + 1) * 512]
                    cx.op(V, lambda h, pw_=pw_, xs=xs: h.tensor_tensor(out=xs, in0=pw_[:, :], in1=xs, op=ALU.add), r=[tpw, k.txa[i][half]], w=[k.txa[i][half]])
        emit_scores(0)
        for n in range(4):
            if n + 1 < 4:
                emit_scores(n + 1)
            emit_rest(n)
    cx.barrier()
    ar.release(m0)


def moe(k):
    cx, ar, D = k.cx, k.ar, k.D
    V = "vector"
    m0 = ar.mark()
    xnT = ar.alloc([8, 2048], BF16)
    t_xnT = Trk()
    norm_transpose(k, "moe", lambda i: (k.xacc[:, i, :], k.txacc[i]), NT, D["norm_moe"], xnT, BF16, t_xnT, resident=True)
    wr = ar.alloc([8, 36], BF16)
    twr = Trk()
    sl_ = cx.fresh('sw')
    srcg, srce = D["router_group_w"], D["router_expert_w"]
    cx.dma("gpsimd", wr[:, :, 0:4], bass.AP(srcg.tensor, srcg.offset, [[4, 128], [4 * 128, 8], [1, 4]]), sl_, w=[twr])
    cx.dma("gpsimd", wr[:, :, 4:36], bass.AP(srce.tensor, srce.offset, [[32, 128], [32 * 128, 8], [1, 32]]), sl_, w=[twr])
    rb = ar.alloc([36], F32)
    trb = Trk()
    sl2 = cx.fresh()
    cx.dma("sync", rb[:, 0:4], dram_bcast(D["router_group_b"], 128, 4), sl2, w=[trb])
    cx.dma("sync", rb[:, 4:36], dram_bcast(D["router_expert_b"], 128, 32), sl2, w=[trb])
    cw = ar.alloc([NT, 32], F32)
    tcw = Trk()
    lg = ar.alloc([36], F32)
    msk = ar.alloc([32], F32)
    m8 = ar.alloc([8], F32)
    sc = ar.alloc([8], F32)
    oh = ar.alloc([4], F32)
    T = Trk()
    for i in range(NT):
        pb = k.psum[i % 2]
        tp = k.tpsum[i % 2]
        for c in range(8):
            cx.op("tensor", lambda h, pb=pb, c=c, i=i: h.matmul(pb[:, 0:36], xnT[:, c, i * 128:(i + 1) * 128], wr[:, c, :], start=(c == 0), stop=(c == 7)),
                  r=[t_xnT, twr], w=[tp], inc=(c == 7))
        cx.op(V, lambda h, pb=pb: h.tensor_tensor(out=lg, in0=pb[:, 0:36], in1=rb, op=ALU.add), r=[tp, trb, T], w=[T])
        cx.op(V, lambda h: h.tensor_reduce(out=sc[:, 0:1], in_=lg[:, 0:4], op=ALU.max, axis=AX.X), r=[T], w=[T])
        cx.op(V, lambda h: h.tensor_scalar(oh, lg[:, 0:4], sc[:, 0:1], None, op0=ALU.is_equal), r=[T], w=[T])
        cx.op(V, lambda h: h.tensor_scalar(sc[:, 1:2], sc[:, 0:1], -1.0, None, op0=ALU.mult), r=[T], w=[T])
        cx.op("scalar", lambda h: h.activation(m8[:, 0:4], lg[:, 0:4], AF.Exp, bias=sc[:, 1:2], accum_out=sc[:, 2:3]), r=[T], w=[T])
        cx.op(V, lambda h: h.reciprocal(sc[:, 3:4], sc[:, 2:3]), r=[T], w=[T])
        cx.op(V, lambda h: h.tensor_scalar(oh, oh, -1.0, 1e30, op0=ALU.add, op1=ALU.mult), r=[T], w=[T])
        cx.op(V, lambda h: h.tensor_tensor(out=msk.rearrange("p (g e) -> p g e", g=4), in0=lg[:, 4:36].rearrange("p (g e) -> p g e", g=4),
                                            in1=oh.unsqueeze(2).to_broadcast([128, 4, 8]), op=ALU.add), r=[T], w=[T])
        cx.op(V, lambda h: h.max(out=m8, in_=msk), r=[T], w=[T])
        cx.op(V, lambda h: h.tensor_tensor(out=sc[:, 4:5], in0=m8[:, 1:2], in1=m8[:, 0:1], op=ALU.subtract), r=[T], w=[T])
        cx.op("scalar", lambda h: h.activation(sc[:, 4:5], sc[:, 4:5], AF.Exp), r=[T], w=[T])
        cx.op(V, lambda h: h.tensor_scalar(sc[:, 5:6], sc[:, 4:5], 1.0, None, op0=ALU.add), r=[T], w=[T])
        cx.op(V, lambda h: h.reciprocal(sc[:, 5:6], sc[:, 5:6]), r=[T], w=[T])
        cx.op(V, lambda h: h.tensor_tensor(out=sc[:, 6:7], in0=sc[:, 4:5], in1=sc[:, 5:6], op=ALU.mult), r=[T], w=[T])
        cx.op(V, lambda h: h.tensor_tensor(out=sc[:, 5:6], in0=sc[:, 5:6], in1=sc[:, 3:4], op=ALU.mult), r=[T], w=[T])
        cx.op(V, lambda h: h.tensor_tensor(out=sc[:, 6:7], in0=sc[:, 6:7], in1=sc[:, 3:4], op=ALU.mult), r=[T], w=[T])
        cx.op(V, lambda h, i=i: h.tensor_scalar(cw[:, i, :], msk, m8[:, 0:1], sc[:, 5:6], op0=ALU.is_equal, op1=ALU.mult), r=[T], w=[tcw, T])
        cx.op(V, lambda h: h.tensor_scalar(lg[:, 4:36], msk, m8[:, 1:2], sc[:, 6:7], op0=ALU.is_equal, op1=ALU.mult), r=[T], w=[T])
        cx.op(V, lambda h, i=i: h.tensor_tensor(out=cw[:, i, :], in0=cw[:, i, :], in1=lg[:, 4:36], op=ALU.add), r=[T, tcw], w=[tcw, T])
    k.dbg_add("moe_cw", cw, [tcw])
    wgu = [ar.alloc([8, 512], BF16) for _ in range(2)]
    wd = [ar.alloc([2, 1024], BF16) for _ in range(2)]
    twe = [Trk(), Trk()]
    swe = [cx.slot("we0"), cx.slot("we1")]
    sg = [ar.alloc([512], F32) for _ in range(2)]
    tsg = [Trk(), Trk()]
    h1 = [ar.alloc([2, 512], BF16) for _ in range(2)]
    th1 = [Trk(), Trk()]
    NE = k.n_experts

    def load_e(e):
        b = e % 2
        g_, u_, d_ = D["moe_w_gate"], D["moe_w_up"], D["moe_w_down"]
        cx.dma("gpsimd", wgu[b][:, :, 0:256], bass.AP(g_.tensor, g_.offset + e * 1024 * 256, [[256, 128], [256 * 128, 8], [1, 256]]), swe[b], w=[twe[b]])
        cx.dma("gpsimd", wgu[b][:, :, 256:512], bass.AP(u_.tensor, u_.offset + e * 1024 * 256, [[256, 128], [256 * 128, 8], [1, 256]]), swe[b], w=[twe[b]])
        cx.dma("gpsimd", wd[b], bass.AP(d_.tensor, d_.offset + e * 256 * 1024, [[1024, 128], [1024 * 128, 2], [1, 1024]]), swe[b], w=[twe[b]])
    import os
    NOLOAD = os.environ.get("MOE_NOLOAD", "") == "1"
    load_e(0)
    if NE > 1:
        load_e(1)
    jobs = [(e, n) for e in range(NE) for n in range(4)]
    state = {"cnt": 0, "loaded": 0}
    cx.barrier()
    k.txh = [[Trk(), Trk()] for _ in range(NT)]

    def emit_gu(j, fh):
        e, n = jobs[j]
        b = e % 2
        ts = slice(n * 512, (n + 1) * 512)
        hb = j % 2
        pg = k.psum[fh * 2]
        tpg = k.tpsum[fh * 2]
        pu = k.psum[fh * 2 + 1]
        tpu = k.tpsum[fh * 2 + 1]
        for c in range(8):
            cx.op("tensor", lambda h, pg=pg, c=c, fh=fh, ts=ts, b=b: h.matmul(pg[:, :], wgu[b][:, c, fh * 128:(fh + 1) * 128], xnT[:, c, ts], start=(c == 0), stop=(c == 7)),
                  r=[twe[b], t_xnT], w=[tpg], inc=(c == 7))
        for c in range(8):
            cx.op("tensor", lambda h, pu=pu, c=c, fh=fh, ts=ts, b=b: h.matmul(pu[:, :], wgu[b][:, c, 256 + fh * 128:256 + (fh + 1) * 128], xnT[:, c, ts], start=(c == 0), stop=(c == 7)),
                  r=[twe[b], t_xnT], w=[tpu], inc=(c == 7))
        cx.op("scalar", lambda h, pg=pg, fh=fh: h.activation(sg[fh], pg[:, :], AF.Silu), r=[tpg], w=[tsg[fh]])
        cx.op(V, lambda h, pu=pu, fh=fh, hb=hb: h.tensor_tensor(out=h1[hb][:, fh, :], in0=pu[:, :], in1=sg[fh], op=ALU.mult), r=[tpu, tsg[fh]], w=[th1[hb]])

    def emit_down(j):
        e, n = jobs[j]
        b = e % 2
        hb = j % 2
        for t in range(4):
            i = n * 4 + t
            for half in range(2):
                pdn = k.psum[4 + state["cnt"] % 4]
                tpd = k.tpsum[4 + state["cnt"] % 4]
                state["cnt"] += 1
                for fh in range(2):
                    cx.op("tensor", lambda h, pdn=pdn, fh=fh, t=t, half=half, hb=hb, b=b: h.matmul(pdn[:, :], h1[hb][:, fh, t * 128:(t + 1) * 128], wd[b][:, fh, half * 512:(half + 1) * 512], start=(fh == 0), stop=(fh == 1)),
                          r=[th1[hb], twe[b]], w=[tpd], inc=(fh == 1))
                xs = k.xacc[:, i, half * 512:(half + 1) * 512]
                cwc = cw[:, i, e:e + 1]
                cx.op(V, lambda h, pdn=pdn, xs=xs, cwc=cwc: h.scalar_tensor_tensor(out=xs, in0=pdn[:, :], scalar=cwc, in1=xs, op0=ALU.mult, op1=ALU.add), r=[tpd, tcw, k.txh[i][half]], w=[k.txh[i][half]])
        if n == 3 and e + 2 < NE and not NOLOAD:
            load_e(e + 2)
    nj = len(jobs)
    if nj > 0:
        emit_gu(0, 0)
        emit_gu(0, 1)
        for j in range(nj):
            if j + 1 < nj:
                emit_gu(j + 1, 0)
            emit_down(j)
            if j + 1 < nj:
                emit_gu(j + 1, 1)
    cx.barrier()
    ar.release(m0)


def final_norm(k, out):
    cx, ar, D = k.cx, k.ar, k.D
    V = "vector"
    m0 = ar.mark()
    gB = ar.alloc([1024], F32)
    tg = Trk()
    cx.dma("sync", gB, dram_bcast(D["norm_final"], 128, 1024), cx.fresh(), w=[tg])
    junk = ar.alloc([1024], BF16)
    tj = Trk()
    ss = ar.alloc([NT, 2], F32)
    tss = Trk()
    ob = [ar.alloc([1024], F32) for _ in range(2)]
    tob = [Trk(), Trk()]
    so = [cx.slot("o0"), cx.slot("o1")]
    for i in range(NT):
        b = i % 2
        s0, s1 = ss[:, i, 0:1], ss[:, i, 1:2]
        txs = [k.txacc[i]] + (k.txh[i] if hasattr(k, "txh") else [])
        cx.op("scalar", lambda h, i=i, s0=s0: h.activation(junk, k.xacc[:, i, :], AF.Square, accum_out=s0), r=txs + [tss], w=[tj, tss])
        cx.op(V, lambda h, s0=s0, s1=s1: h.tensor_scalar(s1, s0, 1.0 / 1024, EPS, op0=ALU.mult, op1=ALU.add), r=[tss], w=[tss])
        cx.op("scalar", lambda h, s1=s1: h.activation(s1, s1, AF.Sqrt), r=[tss], w=[tss])
        cx.op(V, lambda h, s1=s1: h.reciprocal(s1, s1), r=[tss], w=[tss])
        cx.op(V, lambda h, i=i, s1=s1, b=b: h.scalar_tensor_tensor(out=ob[b], in0=k.xacc[:, i, :], scalar=s1, in1=gB, op0=ALU.mult, op1=ALU.mult), r=txs + [tss, tg], w=[tob[b]])
        cx.dma("sync", out[i * 128:(i + 1) * 128, :], ob[b], so[b], r=[tob[b]])
    cx.barrier()
    ar.release(m0)


_CACHE = {}


def kernel(**inputs):
    inp = {k_: np.asarray(v) for k_, v in inputs.items()}
    n = inp["x"].shape[0]
    maps = [host_inputs(inp, b) for b in range(n)]
    key = "full"
    if key not in _CACHE:
        shapes = {k_: (v.shape, np2dt(v)) for k_, v in maps[0].items()}
        _CACHE[key] = build(shapes)[0]
    nc = _CACHE[key]
    res = run_bass_kernel_spmd(nc, maps, core_ids=list(range(n)))
    return np.stack([np.asarray(r["out"], dtype=np.float32) for r in res.results], 0)
```

```python
import contextlib
import os
import math
import numpy as np
import ml_dtypes
import concourse.bass as bass
import concourse.mybir as mybir
from concourse.bass_utils import run_bass_kernel_spmd

F32 = mybir.dt.float32
BF16 = mybir.dt.bfloat16
F32R = mybir.dt.float32r
I32 = mybir.dt.int32
AF = mybir.ActivationFunctionType
ALU = mybir.AluOpType
AX = mybir.AxisListType

ENGS = ("sync", "scalar", "gpsimd", "vector", "tensor")
S = 2048
DM = 1024
NT = 16
EPS = 1e-6


class Trk:
    __slots__ = ("name", "w", "r", "excl")

    def __init__(self, name="", excl=False):
        self.name = name
        self.w = None
        self.r = {}
        self.excl = excl


class DmaSlot:
    def __init__(self, ctx, name):
        self.key = "d_" + name + str(ctx.nsem)
        ctx.sems[self.key] = ctx.new_sem(self.key)
        self.total = 0


class Ctx:
    def __init__(self, nc, stack):
        self.nc = nc
        self.stack = stack
        self.q = {e: [] for e in ENGS}
        self.sems = {}
        self.nsem = 0
        self.cnt = {e: 0 for e in ENGS}
        self.known = {e: {} for e in ENGS}
        for e in ENGS:
            self.sems[e] = self.new_sem("s_" + e)
        self.slots = []
        self.pools = {}
        self.pool_idx = {}
        self.n_ops = 0

    def new_sem(self, name):
        self.nsem += 1
        return self.stack.enter_context(self.nc.semaphore(name))

    def slot(self, name):
        s = DmaSlot(self, name)
        self.slots.append(s)
        return s

    def fresh(self, kind="hw"):
        pool = self.pools.setdefault(kind, [])
        i = self.pool_idx.get(kind, 0)
        if i >= len(pool):
            assert len(pool) < 30, "slot pool exhausted"
            pool.append(self.slot(kind + "%d" % len(pool)))
            pool[-1].kind = kind
        self.pool_idx[kind] = i + 1
        return pool[i]

    def sb(self, name, shape, dt):
        return self.stack.enter_context(self.nc.sbuf_tensor("sb_" + name, list(shape), dt))

    def ps(self, name, shape, dt=F32):
        return self.stack.enter_context(self.nc.psum_tensor(name, list(shape), dt))

    def _waits_for(self, eng, r, w, extra=()):
        need = {}

        def req(dep):
            if dep is None:
                return
            k, c = dep
            if k == eng and eng in ("tensor", "sync"):
                return
            if c > need.get(k, 0):
                need[k] = c
        for t in r:
            req(t.w)
        for t in w:
            req(t.w)
            for k, c in t.r.items():
                req((k, c))
        for d in extra:
            req(d)
        out = []
        kn = self.known[eng]
        for k, c in need.items():
            if kn.get(k, 0) < c:
                kn[k] = c
                out.append((self.sems[k], c))
        return out

    def op(self, eng, fn, r=(), w=(), inc=True, extra=()):
        w = list(w) + [t for t in r if t.excl]
        r = [t for t in r if not t.excl]
        waits = self._waits_for(eng, r, w, extra)
        c = self.cnt[eng] + 1
        if inc:
            self.cnt[eng] = c
        sem = self.sems[eng]

        def emit(h, fn=fn, waits=waits, inc=inc, sem=sem):
            for s, v in waits:
                h.wait_ge(s, v)
            ins = fn(h)
            if inc:
                ins.then_inc(sem, 1)
        self.q[eng].append(emit)
        for t in r:
            t.r[eng] = c
        for t in w:
            t.w = (eng, c)
            t.r = {}
        self.n_ops += 1

    def dma(self, eng, out, in_, slot, r=(), w=(), extra=(), **kw):
        kind = "sw" if eng == "gpsimd" else "hw"
        assert getattr(slot, "kind", kind) == kind, ("DMA slot kind mismatch", slot.key, eng)
        slot.kind = kind
        waits = self._waits_for(eng, r, w, extra)
        slot.total += 16
        sem = self.sems[slot.key]

        def emit(h, waits=waits, sem=sem, out=out, in_=in_, kw=kw):
            for s, v in waits:
                h.wait_ge(s, v)
            h.dma_start(out=out, in_=in_, **kw).then_inc(sem, 16)
        self.q[eng].append(emit)
        dep = (slot.key, slot.total)
        for t in r:
            t.r[slot.key] = slot.total
        for t in w:
            t.w = dep
            t.r = {}
        self.n_ops += 1
        return dep

    def wait_deps(self, eng, deps):
        waits = self._waits_for(eng, (), (), deps)

        def emit(h, waits=waits):
            for s, v in waits:
                h.wait_ge(s, v)
        self.q[eng].append(emit)

    def barrier(self):
        deps = [(e, self.cnt[e]) for e in ENGS if e != "sync" and self.cnt[e] > 0]
        deps += [(s.key, s.total) for s in self.slots if s.total > 0]
        for e in ENGS:
            self.wait_deps(e, deps)
        self.pool_idx = {}

    def emit_all(self, block):
        q = self.q

        @block.sync
        def _(h):
            for f in q["sync"]:
                f(h)

        @block.scalar
        def _(h):
            for f in q["scalar"]:
                f(h)

        @block.gpsimd
        def _(h):
            for f in q["gpsimd"]:
                f(h)

        @block.vector
        def _(h):
            for f in q["vector"]:
                f(h)

        @block.tensor
        def _(h):
            for f in q["tensor"]:
                f(h)


class Arena:
    def __init__(self, cx, words, base=None):
        self.t = cx.sb("arena", [128, words], F32) if base is None else base
        self.cx = cx
        self.words = words
        self.top = 0

    def mark(self):
        return self.top

    def release(self, m):
        if m != self.top:
            self.cx.barrier()
        self.top = m

    def alloc(self, shape, dt):
        n = int(np.prod(shape))
        w = n if dt in (F32, F32R, I32) else (n + 1) // 2
        w = (w + 1) // 2 * 2
        o = self.top
        self.top += w
        assert self.top <= self.words, ("arena overflow", self.top, self.words)
        v = self.t[:, o:o + w]
        if dt != F32:
            v = v.bitcast(dt)
        v = v[:, 0:n]
        if len(shape) > 1:
            names = " ".join("d%d" % i for i in range(len(shape)))
            v = v.rearrange("p (%s) -> p %s" % (names, names), **{"d%d" % i: shape[i] for i in range(len(shape))})
        return v


def pap(ap, part0, nparts, off, dims):
    base = ap.ap[0][0]
    return bass.AP(ap.tensor, ap.offset + part0 * base + off, [[base, nparts]] + [list(d) for d in dims])


def host_consts():
    c = {}
    c["ident"] = np.eye(128, dtype=np.float32)
    c["identb"] = np.eye(128, dtype=np.float32).astype(ml_dtypes.bfloat16)
    c["ones"] = np.ones((128, 128), np.float32)
    selT = np.zeros((128, 2, 8, 128), np.float32)
    selB = np.zeros((128, 2, 8, 128), np.float32)
    for q in range(4):
        for r in range(32):
            loc, cc = r // 16, r % 16
            for s in range(8):
                selT[q * 32 + r, loc, s, s * 16 + cc] = 1.0
                selB[q * 32 + r, loc, s, s * 16 + cc] = 1.0
    c["selT"] = selT.astype(ml_dtypes.bfloat16)
    c["selB"] = selB.astype(ml_dtypes.bfloat16)
    sidx = np.arange(128) // 16
    c["s5mf"] = (sidx[None, :] >= sidx[:, None]).astype(np.float32)
    c["s5mb"] = (sidx[None, :] <= sidx[:, None]).astype(np.float32)
    c["kvec"] = np.tile((np.arange(16, dtype=np.float32) - 7.0)[None, :], (128, 1))
    k = np.arange(128)[:, None]
    cc = np.arange(128)[None, :]
    same = (k // 64) == (cc // 64)
    gm = np.zeros((128, 8, 128), np.float32)
    gm[:, 0] = same & (k <= cc)
    gm[:, 1] = same & (k >= cc)
    gm[:, 2] = np.where(same & (cc >= k), 0.0, -30000.0)
    gm[:, 3] = np.where(same & (cc <= k), 0.0, -30000.0)
    gm[:, 4] = same & (cc > k)
    gm[:, 5] = same & (cc < k)
    gm[:, 6] = same
    c["gmask"] = gm
    return c


def host_s5(inp):
    o = {}

    pairs = {"lam_re": ("s5_lam_re_f", "s5_lam_re_b"), "lam_im": ("s5_lam_im_f", "s5_lam_im_b"),
             "log_step": ("s5_log_step_f", "s5_log_step_b"), "b_re": ("s5_b_re_f", "s5_b_re_b"),
             "b_im": ("s5_b_im_f", "s5_b_im_b"), "c_re": ("s5_c_re_f", "s5_c_re_b"), "c_im": ("s5_c_im_f", "s5_c_im_b")}

    def st(nm):
        f_, b_ = pairs[nm]
        return np.stack([inp[f_][0], inp[b_][0]], 0)
    lam = np.stack([st("lam_re"), st("lam_im")], 0)
    lam = lam.reshape(2, 2, 2, 16, 64).transpose(2, 4, 0, 1, 3)
    o["s5_lam"] = np.ascontiguousarray(lam.reshape(128, 2, 32))
    ls = st("log_step").reshape(2, 2, 16)
    ls = np.broadcast_to(ls.transpose(1, 0, 2)[:, None], (2, 64, 2, 16))
    o["s5_step"] = np.ascontiguousarray(ls.reshape(128, 32))
    b = np.stack([st("b_re"), st("b_im")], 0)
    b = b.reshape(2, 2, 2, 16, 64, 16).transpose(2, 4, 0, 1, 3, 5)
    o["s5_b"] = np.ascontiguousarray(b.reshape(128, 2, 512))
    cm = np.stack([st("c_re"), st("c_im")], 0)
    cm = cm.reshape(2, 2, 2, 16, 16, 64).transpose(2, 5, 0, 1, 3, 4)
    o["s5_c"] = np.ascontiguousarray(cm.reshape(128, 2, 512))
    d = inp["s5_d"][0].reshape(32, 16)
    o["s5_dvec"] = np.ascontiguousarray(np.broadcast_to(d.T[None], (8, 16, 32)).reshape(128, 32))
    o["s5_bglu"] = np.ascontiguousarray(inp["s5_b_glu"][0].reshape(4, 128).T)
    o["s5_normw"] = np.ascontiguousarray(inp["s5_norm"][0].reshape(4, 128).T)
    return o


def np2dt(a):
    if a.dtype == np.float32:
        return F32
    if a.dtype == ml_dtypes.bfloat16:
        return BF16
    raise ValueError(a.dtype)


class K:
    pass


def dram_bcast(ap, nparts, n, off=0):
    return bass.AP(ap.tensor, ap.offset + off, [[0, nparts], [1, n]])


def norm_transpose(k, name, src_fn, ntiles, gain_dram, outT, out_dt, outT_trk, resident=False):
    cx, ar = k.cx, k.ar
    m = ar.mark()
    gB = ar.alloc([1024], F32)
    tg = Trk()
    cx.dma("sync", gB, dram_bcast(gain_dram, 128, 1024), cx.fresh(), w=[tg])
    junk = ar.alloc([1024], BF16)
    tj = Trk()
    xn = [ar.alloc([1024], out_dt) for _ in range(2)]
    txn = [Trk(), Trk()]
    ss = ar.alloc([NT * 2, 1], F32)
    tss = [Trk() for _ in range(ntiles)]
    pdt = BF16 if out_dt == BF16 else F32
    ident = k.identb if out_dt == BF16 else k.ident
    srcs = []
    if resident:
        for i in range(ntiles):
            src, ts = src_fn(i)
            srcs.append((src, ts))
            cx.op("scalar", lambda h, src=src, i=i: h.activation(junk, src, AF.Square, accum_out=ss[:, 2 * i:2 * i + 1]), r=[ts], w=[tj, tss[0]])
        ssv = ss.rearrange("p (t two) one -> p t (two one)", two=2)
        cx.op("vector", lambda h: h.tensor_scalar(ssv[:, 0:ntiles, 1:2], ssv[:, 0:ntiles, 0:1], 1.0 / 1024, EPS, op0=ALU.mult, op1=ALU.add), r=[tss[0]], w=[tss[0]])
        cx.op("scalar", lambda h: h.activation(ssv[:, 0:ntiles, 1:2], ssv[:, 0:ntiles, 1:2], AF.Sqrt), r=[tss[0]], w=[tss[0]])
        cx.op("vector", lambda h: h.reciprocal(ssv[:, 0:ntiles, 1:2], ssv[:, 0:ntiles, 1:2]), r=[tss[0]], w=[tss[0]])
    for i in range(ntiles):
        rsi = ss[:, 2 * i + 1:2 * i + 2]
        if resident:
            src, ts = srcs[i]
            tsi = tss[0]
        else:
            src, ts = src_fn(i)
            ssi = ss[:, 2 * i:2 * i + 1]
            tsi = tss[i]
            cx.op("scalar", lambda h, src=src, ssi=ssi: h.activation(junk, src, AF.Square, accum_out=ssi), r=[ts], w=[tj, tss[i]])
            cx.op("vector", lambda h, ssi=ssi, rsi=rsi: h.tensor_scalar(rsi, ssi, 1.0 / 1024, EPS, op0=ALU.mult, op1=ALU.add), r=[tss[i]], w=[tss[i]])
            cx.op("scalar", lambda h, rsi=rsi: h.activation(rsi, rsi, AF.Sqrt), r=[tss[i]], w=[tss[i]])
            cx.op("vector", lambda h, rsi=rsi: h.reciprocal(rsi, rsi), r=[tss[i]], w=[tss[i]])
        b = i % 2
        cx.op("vector", lambda h, src=src, rsi=rsi, b=b: h.scalar_tensor_tensor(out=xn[b], in0=src, scalar=rsi, in1=gB, op0=ALU.mult, op1=ALU.mult),
              r=[ts, tsi, tg], w=[txn[b]])
        if out_dt == BF16:
            pb = k.psum[i % 2]
            tp = k.tpsum[i % 2]
            pv = pb.bitcast(BF16)
            for c in range(8):
                cx.op("tensor", lambda h, b=b, c=c, pv=pv: h.transpose(pv[:, c * 128:(c + 1) * 128], xn[b][:, c * 128:(c + 1) * 128], ident),
                      r=[txn[b]], w=[tp], inc=(c == 7))
            dst = outT[:, :, i * 128:(i + 1) * 128]
            eng = "scalar" if i % 2 == 0 else "vector"
            if eng == "scalar":
                cx.op(eng, lambda h, dst=dst, pv=pv: h.copy(dst, pv.rearrange("p (c t) -> p c t", c=8)), r=[tp], w=[outT_trk])
            else:
                cx.op(eng, lambda h, dst=dst, pv=pv: h.tensor_copy(dst, pv.rearrange("p (c t) -> p c t", c=8)), r=[tp], w=[outT_trk])
        else:
            for half in range(2):
                pb = k.psum[(2 * i + half) % 4]
                tp = k.tpsum[(2 * i + half) % 4]
                for c4 in range(4):
                    c = half * 4 + c4
                    cx.op("tensor", lambda h, b=b, c=c, c4=c4, pb=pb: h.transpose(pb[:, c4 * 128:(c4 + 1) * 128], xn[b][:, c * 128:(c + 1) * 128].bitcast(F32), ident),
                          r=[txn[b]], w=[tp], inc=(c4 == 3))
                dst = outT[:, half * 4:(half + 1) * 4, i * 128:(i + 1) * 128]
                if half == 0:
                    cx.op("scalar", lambda h, dst=dst, pb=pb: h.copy(dst, pb.rearrange("p (c t) -> p c t", c=4)), r=[tp], w=[outT_trk])
                else:
                    cx.op("vector", lambda h, dst=dst, pb=pb: h.tensor_copy(dst, pb.rearrange("p (c t) -> p c t", c=4)), r=[tp], w=[outT_trk])
    ar.release(m)


def s5_prep(k):
    cx, ar, D = k.cx, k.ar, k.D
    V = "vector"
    m0 = ar.mark()
    lam = ar.alloc([2, 32], F32)
    step = ar.alloc([32], F32)
    bb = ar.alloc([2, 512], F32)
    cc = ar.alloc([2, 512], F32)
    kvec = ar.alloc([16], F32)
    tl = Trk()
    sl_ = cx.fresh()
    for dst, nm in ((lam, "s5_lam"), (step, "s5_step"), (bb, "s5_b"), (cc, "s5_c"), (kvec, "kvec")):
        cx.dma("sync", dst, D[nm], sl_, w=[tl])
    T = Trk()

    def vop(fn, extra_r=()):
        cx.op(V, fn, r=[T, tl] + list(extra_r), w=[T])

    def aop(fn):
        cx.op("scalar", fn, r=[T, tl], w=[T])
    lre, lim = lam[:, 0, :], lam[:, 1, :]
    dl = ar.alloc([32], F32)
    re1 = ar.alloc([32], F32)
    im1 = ar.alloc([32], F32)
    aop(lambda h: h.activation(dl, step, AF.Exp))
    vop(lambda h: h.tensor_tensor(out=re1, in0=dl, in1=lre, op=ALU.mult))
    vop(lambda h: h.tensor_tensor(out=im1, in0=dl, in1=lim, op=ALU.mult))
    PWI = ar.alloc([16, 32], F32)
    PWR = ar.alloc([16, 32], F32)
    m_pw = ar.mark()
    KR = ar.alloc([16, 32], F32)
    KI = ar.alloc([16, 32], F32)
    kv_b = kvec.unsqueeze(2).to_broadcast([128, 16, 32])
    vop(lambda h: h.tensor_tensor(out=KR, in0=kv_b, in1=re1.unsqueeze(1).to_broadcast([128, 16, 32]), op=ALU.mult))
    vop(lambda h: h.tensor_tensor(out=KI, in0=kv_b, in1=im1.unsqueeze(1).to_broadcast([128, 16, 32]), op=ALU.mult))
    MAG = ar.alloc([16, 32], F32)
    aop(lambda h: h.activation(MAG, KR, AF.Exp))
    YI = ar.alloc([16, 32], I32)
    YF = ar.alloc([16, 32], F32)
    vop(lambda h: h.tensor_scalar(KI, KI, 1.0 / (2 * math.pi), None, op0=ALU.mult))
    vop(lambda h: h.tensor_copy(YI, KI))
    vop(lambda h: h.tensor_copy(YF, YI))
    vop(lambda h: h.tensor_tensor(out=KI, in0=KI, in1=YF, op=ALU.subtract))
    SH_ = ar.alloc([16, 32], F32)
    SQ_ = ar.alloc([16, 32], F32)
    aop(lambda h: h.activation(SH_, KI, AF.Sin, scale=math.pi))
    aop(lambda h: h.activation(SQ_, KI, AF.Sin, scale=math.pi / 2))
    CH_ = ar.alloc([16, 32], F32)
    vop(lambda h: h.tensor_tensor(out=CH_, in0=SQ_, in1=SQ_, op=ALU.mult))
    vop(lambda h: h.tensor_scalar(CH_, CH_, -2.0, 1.0, op0=ALU.mult, op1=ALU.add))
    vop(lambda h: h.tensor_tensor(out=PWI, in0=SH_, in1=CH_, op=ALU.mult))
    vop(lambda h: h.scalar_tensor_tensor(out=PWI, in0=PWI, scalar=2.0, in1=MAG, op0=ALU.mult, op1=ALU.mult))
    vop(lambda h: h.tensor_tensor(out=PWR, in0=SH_, in1=SH_, op=ALU.mult))
    vop(lambda h: h.tensor_scalar(PWR, PWR, -2.0, 1.0, op0=ALU.mult, op1=ALU.add))
    vop(lambda h: h.tensor_tensor(out=PWR, in0=PWR, in1=MAG, op=ALU.mult))
    ar.release(m_pw)
    lrm1 = ar.alloc([32], F32)
    li = PWI[:, 8, :]
    t1 = ar.alloc([32], F32)
    t2 = ar.alloc([32], F32)
    den = ar.alloc([32], F32)
    c0r = ar.alloc([32], F32)
    c0i = ar.alloc([32], F32)
    vop(lambda h: h.tensor_scalar(lrm1, PWR[:, 8, :], -1.0, None, op0=ALU.add))
    vop(lambda h: h.tensor_tensor(out=t1, in0=lre, in1=lre, op=ALU.mult))
    vop(lambda h: h.tensor_tensor(out=t2, in0=lim, in1=lim, op=ALU.mult))
    vop(lambda h: h.tensor_tensor(out=den, in0=t1, in1=t2, op=ALU.add))
    vop(lambda h: h.reciprocal(den, den))
    vop(lambda h: h.tensor_tensor(out=t1, in0=lrm1, in1=lre, op=ALU.mult))
    vop(lambda h: h.tensor_tensor(out=t2, in0=li, in1=lim, op=ALU.mult))
    vop(lambda h: h.tensor_tensor(out=t1, in0=t1, in1=t2, op=ALU.add))
    vop(lambda h: h.tensor_tensor(out=c0r, in0=t1, in1=den, op=ALU.mult))
    vop(lambda h: h.tensor_tensor(out=t1, in0=li, in1=lre, op=ALU.mult))
    vop(lambda h: h.tensor_tensor(out=t2, in0=lrm1, in1=lim, op=ALU.mult))
    vop(lambda h: h.tensor_tensor(out=t1, in0=t1, in1=t2, op=ALU.subtract))
    vop(lambda h: h.tensor_tensor(out=c0i, in0=t1, in1=den, op=ALU.mult))
    BBR = ar.alloc([32, 16], F32)
    BBI = ar.alloc([32, 16], F32)
    TA = ar.alloc([32, 16], F32)
    br = bb[:, 0, :].rearrange("p (a c) -> p a c", c=16)
    bi = bb[:, 1, :].rearrange("p (a c) -> p a c", c=16)
    c0r_b = c0r.unsqueeze(2).to_broadcast([128, 32, 16])
    c0i_b = c0i.unsqueeze(2).to_broadcast([128, 32, 16])
    vop(lambda h: h.tensor_tensor(out=BBR, in0=br, in1=c0r_b, op=ALU.mult))
    vop(lambda h: h.tensor_tensor(out=TA, in0=bi, in1=c0i_b, op=ALU.mult))
    vop(lambda h: h.tensor_tensor(out=BBR, in0=BBR, in1=TA, op=ALU.subtract))
    vop(lambda h: h.tensor_tensor(out=BBI, in0=bi, in1=c0r_b, op=ALU.mult))
    vop(lambda h: h.tensor_tensor(out=TA, in0=br, in1=c0i_b, op=ALU.mult))
    vop(lambda h: h.tensor_tensor(out=BBI, in0=BBI, in1=TA, op=ALU.add))
    ASd = ar.alloc([16, 2, 8, 16], F32)
    CS2d = ar.alloc([16, 2, 8, 16], F32)
    T1 = ar.alloc([8, 16, 16], F32)
    T2 = ar.alloc([8, 16, 16], F32)
    cr = cc[:, 0, :].rearrange("p (d a c) -> p d a c", d=2, c=16)
    ci = cc[:, 1, :].rearrange("p (d a c) -> p d a c", d=2, c=16)
    BBR4 = BBR.rearrange("p (d a) c -> p d a c", d=2)
    BBI4 = BBI.rearrange("p (d a) c -> p d a c", d=2)

    def pw(arr, d, k0, kstep):
        return pap(arr, 0, 128, k0 * 32 + d * 16, [[kstep * 32, 8], [1, 16], [0, 16]])

    def dst(arr, dofs, ri):
        return pap(arr, 0, 128, dofs * 4096 + ri * 128, [[16, 8], [256, 16], [1, 16]])

    def vec(v4, d):
        a_ = v4[:, d]
        return bass.AP(a_.tensor, a_.offset, [list(a_.ap[0]), [0, 8], list(a_.ap[1]), list(a_.ap[2])])

    T1f = T1.rearrange("p a b c -> p (a b c)")

    def cmul(out_arr, dofs, d, k0, kstep, vr, vi, neg_im):
        pr, pi_ = pw(PWR, d, k0, kstep), pw(PWI, d, k0, kstep)
        vop(lambda h: h.tensor_tensor(out=T1, in0=pr, in1=vec(vr, d), op=ALU.mult))
        vop(lambda h: h.tensor_tensor(out=T2, in0=pi_, in1=vec(vi, d), op=ALU.mult))
        vop(lambda h: h.tensor_tensor(out=dst(out_arr, dofs, 0), in0=T1, in1=T2, op=ALU.subtract))
        vop(lambda h: h.tensor_tensor(out=T1, in0=pr, in1=vec(vi, d), op=ALU.mult))
        vop(lambda h: h.tensor_tensor(out=T2, in0=pi_, in1=vec(vr, d), op=ALU.mult))
        if neg_im:
            vop(lambda h: h.tensor_scalar(T1f, T1f, -1.0, None, op0=ALU.mult))
            vop(lambda h: h.tensor_tensor(out=dst(out_arr, dofs, 1), in0=T1, in1=T2, op=ALU.subtract))
        else:
            vop(lambda h: h.tensor_tensor(out=dst(out_arr, dofs, 1), in0=T1, in1=T2, op=ALU.add))
    vop(lambda h: h.tensor_copy(k.s5A1[:, 0:32], PWR[:, 15, :]))
    vop(lambda h: h.tensor_copy(k.s5A1[:, 32:64], PWR[:, 15, :]))
    vop(lambda h: h.tensor_scalar(k.s5A2[:, 0:32], PWI[:, 15, :], -1.0, None, op0=ALU.mult))
    vop(lambda h: h.tensor_copy(k.s5A2[:, 32:64], PWI[:, 15, :]))
    cmul(k.s5CS, 0, 0, 8, 1, cr, ci, True)
    cmul(k.s5CS, 1, 1, 15, -1, cr, ci, True)
    k.t_s5w = T
    mf = ar.alloc([2, 128], F32)
    dv = ar.alloc([32], F32)
    tm = Trk()
    sl_ = cx.fresh()
    cx.dma("sync", mf[:, 0, :], D["s5mf"], sl_, w=[tm])
    cx.dma("sync", mf[:, 1, :], D["s5mb"], sl_, w=[tm])
    cx.dma("sync", dv, D["s5_dvec"], sl_, w=[tm])
    tt1 = [ar.alloc([128], F32) for _ in range(2)]
    ttt = [Trk(), Trk()]
    ASb = ASd.rearrange("p a r s c -> p (a r) (s c)")
    ASm = ASd.rearrange("p a r s c -> p a r (s c)")
    CSm = CS2d.rearrange("p a r s c -> p a r (s c)")
    for d in range(2):
        if d == 0:
            cmul(ASd, 0, 0, 14, -1, BBR4, BBI4, False)
            cmul(CS2d, 0, 0, 0, 1, cr, ci, True)
        else:
            cmul(ASd, 0, 1, 7, 1, BBR4, BBI4, False)
            cmul(CS2d, 0, 1, 7, -1, cr, ci, True)
        for grp in range(8):
            pb = k.psum[grp % 4]
            tp = k.tpsum[grp % 4]
            for j in range(4):
                blk = grp * 4 + j
                cx.op("tensor", lambda h, pb=pb, j=j, blk=blk: h.transpose(pb[:, j * 128:(j + 1) * 128], ASb[:, blk, :], k.ident),
                      r=[T], w=[tp], inc=(j == 3))
            dstv = k.s5AT[:, d * 32 + grp * 4:d * 32 + (grp + 1) * 4, :]
            if grp % 2 == 0:
                cx.op("scalar", lambda h, dstv=dstv, pb=pb: h.copy(dstv, pb.rearrange("p (j x) -> p j x", j=4)), r=[tp], w=[k.t_s5at])
            else:
                cx.op("vector", lambda h, dstv=dstv, pb=pb: h.tensor_copy(dstv, pb.rearrange("p (j x) -> p j x", j=4)), r=[tp], w=[k.t_s5at])
        for g in range(32):
            gh, gl = g // 16, g % 16
            pb = k.psum[4 + g % 4]
            tp = k.tpsum[4 + g % 4]
            for ri in range(2):
                cx.op("tensor", lambda h, pb=pb, ri=ri, gh=gh, gl=gl: h.matmul(
                    pb[:, 0:128], ASm[gh * 64:(gh + 1) * 64, gl, ri, :], CSm[gh * 64:(gh + 1) * 64, gl, ri, :],
                    start=(ri == 0), stop=(ri == 1)), r=[T], w=[tp], inc=(ri == 1))
            b_ = g % 2
            cx.op(V, lambda h, pb=pb, b_=b_, d=d: h.tensor_tensor(out=tt1[b_], in0=pb[:, 0:128], in1=mf[:, d, :], op=ALU.mult), r=[tp, tm], w=[ttt[b_]])
            if d == 0:
                cx.op(V, lambda h, b_=b_, g=g: h.scalar_tensor_tensor(out=k.s5TT[:, g, :], in0=k.ident, scalar=dv[:, g:g + 1], in1=tt1[b_], op0=ALU.mult, op1=ALU.add),
                      r=[ttt[b_], tm], w=[k.t_s5tt])
            else:
                cx.op(V, lambda h, b_=b_, g=g: h.tensor_tensor(out=k.s5TT[:, g, :], in0=k.s5TT[:, g, :], in1=tt1[b_], op=ALU.add),
                      r=[ttt[b_]], w=[k.t_s5tt])
    cx.barrier()
    ar.release(m0)


def load_w_cols(k, wdram, col0, ncols, dst, trk, slot, eng="gpsimd"):
    src = bass.AP(wdram.tensor, wdram.offset + col0, [[wdram.ap[0][0] * 1, 128], [wdram.ap[0][0] * 128, 8], [1, ncols]])
    return k.cx.dma(eng, dst, src, slot, w=[trk])


def proj_fm(k, wt, wtrk, consume):
    cx = k.cx
    for n in range(4):
        pb = k.psum[n % 2 + 2]
        tp = k.tpsum[n % 2 + 2]
        for c in range(8):
            cx.op("tensor", lambda h, pb=pb, c=c, n=n: h.matmul(pb[:, :], wt[:, c, :], k.hT[:, c, n * 512:(n + 1) * 512], start=(c == 0), stop=(c == 7)),
                  r=[wtrk, k.t_hT], w=[tp], inc=(c == 7))
        consume(n, pb, tp)


def s5_build_U(k):
    cx, ar = k.cx, k.ar
    m0 = ar.mark()
    wt = [ar.alloc([8, 128], BF16) for _ in range(2)]
    twt = [Trk(), Trk()]
    swt = [cx.fresh('sw'), cx.fresh('sw')]
    uT = [ar.alloc([2048], BF16) for _ in range(2)]
    tuT = [Trk(), Trk()]
    for ct in range(4):
        b = ct % 2
        load_w_cols(k, k.D["w_in"], ct * 128, 128, wt[b], twt[b], swt[b])

        def consume(n, pb, tp, b=b):
            if n % 2 == 0:
                cx.op("scalar", lambda h: h.copy(uT[b][:, n * 512:(n + 1) * 512], pb[:, :]), r=[tp], w=[tuT[b]])
            else:
                cx.op("vector", lambda h: h.tensor_copy(uT[b][:, n * 512:(n + 1) * 512], pb[:, :]), r=[tp], w=[tuT[b]])
        proj_fm(k, wt[b], twt[b], consume)
        for gi in range(8):
            g = ct * 8 + gi
            q0 = 32 * (gi // 2)
            pb = k.psum[4 + gi % 4]
            tp = k.tpsum[4 + gi % 4]
            for s in range(8):
                rhs = pap(uT[b], q0, 32, s, [[8, 256]])
                cx.op("tensor", lambda h, pb=pb, s=s, rhs=rhs, q0=q0, gi=gi: h.matmul(pb[:, 0:256], k.selT[q0:q0 + 32, gi % 2, s, :], rhs, start=(s == 0), stop=(s == 7), tile_position=(q0, 0)),
                      r=[tuT[b]], w=[tp], inc=(s == 7))
            if gi % 2 == 0:
                cx.op("scalar", lambda h, pb=pb, g=g: h.copy(k.s5U[:, g, :], pb[:, 0:256]), r=[tp], w=[k.t_s5U])
            else:
                cx.op("vector", lambda h, pb=pb, g=g: h.tensor_copy(k.s5U[:, g, :], pb[:, 0:256]), r=[tp], w=[k.t_s5U])
    cx.barrier()
    ar.release(m0)


def s5_main(k, yT, t_yT):
    cx, ar, D = k.cx, k.ar, k.D
    V = "vector"
    m0 = ar.mark()
    SH = ar.alloc([2, 257, 2, 16], BF16)
    tSH = Trk()
    tSHh = Trk()
    X = [ar.alloc([64], F32) for _ in range(3)]
    tX = [Trk() for _ in range(3)]
    t1 = ar.alloc([64], F32)
    t2 = ar.alloc([64], F32)
    tt = Trk()
    tt2 = Trk()
    cx.op("gpsimd", lambda h: h.memset(SH[:, 0, 0, :, :], 0.0), w=[tSH])
    cx.op("gpsimd", lambda h: h.memset(SH[:, 1, 256, :, :], 0.0), w=[tSH])
    cx.op("gpsimd", lambda h: h.memset(X[0], 0.0), w=[tX[0]])
    n = 0
    for gl in range(16):
        for d in range(2):
            for ri in range(2):
                blk = d * 32 + gl * 2 + ri
                pb = k.psum[n % 4]
                tp = k.tpsum[n % 4]
                cx.op("tensor", lambda h, pb=pb, blk=blk, gl=gl: h.matmul(pb[0:64, 0:256], k.s5AT[:, blk, 0:64], k.s5U[:, gl, :], start=True, stop=True),
                      r=[k.t_s5at, k.t_s5U], w=[tp], inc=False)
                cx.op("tensor", lambda h, pb=pb, blk=blk, gl=gl: h.matmul(pb[64:128, 0:256], k.s5AT[:, blk, 64:128], k.s5U[:, 16 + gl, :], start=True, stop=True),
                      r=[k.t_s5at, k.t_s5U], w=[tp])
                slot0 = 1 if d == 0 else 0
                dstv = pap(SH, 0, 128, d * 257 * 32 + slot0 * 32 + ri * 16 + gl, [[32, 256]])
                if n % 2 == 0:
                    cx.op("scalar", lambda h, dstv=dstv, pb=pb: h.copy(dstv, pb[:, 0:256]), r=[tp], w=[tSH])
                else:
                    cx.op(V, lambda h, dstv=dstv, pb=pb: h.tensor_copy(dstv, pb[:, 0:256]), r=[tp], w=[tSH])
                n += 1
    import os
    S5STOP = os.environ.get('S5_STOP', '')
    if S5STOP == 'a':
        cx.barrier(); ar.release(m0); return
    for i in range(256):
        xp, xn = X[i % 3], X[(i + 1) % 3]
        txp, txn = tX[i % 3], tX[(i + 1) % 3]
        xsw = pap(xp, 0, 128, 32, [[-32, 2], [1, 32]])
        bf = (i + 1) * 32
        bb_ = 257 * 32 + (255 - i) * 32
        sview = pap(SH, 0, 128, bf, [[16, 2], [bb_ - bf, 2], [1, 16]])
        xp3 = xp.rearrange("p (r x) -> p r x", r=2)
        cx.op("gpsimd", lambda h, xsw=xsw: h.tensor_tensor(out=t2.rearrange("p (r x) -> p r x", r=2), in0=k.s5A2.rearrange("p (r x) -> p r x", r=2), in1=xsw, op=ALU.mult), r=[txp, k.t_s5w], w=[tt2])
        cx.op(V, lambda h, xp=xp: h.tensor_tensor(out=t1, in0=k.s5A1, in1=xp, op=ALU.mult), r=[txp, k.t_s5w], w=[tt])
        cx.op(V, lambda h, sview=sview: h.tensor_tensor(out=t1.rearrange("p (r d x) -> p r d x", r=2, d=2), in0=t1.rearrange("p (r d x) -> p r d x", r=2, d=2), in1=sview, op=ALU.add), r=[tt, tSH], w=[tt])
        cx.op(V, lambda h, xn=xn: h.tensor_tensor(out=xn, in0=t1, in1=t2, op=ALU.add), r=[tt, tt2], w=[txn])
        cx.op("scalar", lambda h, xn=xn, sview=sview: h.copy(sview, xn.rearrange("p (r d x) -> p r d x", r=2, d=2)), r=[txn], w=[tSHh])
    if S5STOP == 'rec':
        cx.barrier(); ar.release(m0); return
    gT = ar.alloc([4, 2048], F32)
    gTb = ar.alloc([4, 2048], BF16)
    tgT = [Trk() for _ in range(4)]
    tgTb = [Trk() for _ in range(4)]
    ybuf = [k.arA.alloc([8, 256], BF16) for _ in range(2)]
    tyb = [Trk(), Trk()]
    for ct in range(4):
        b = ct % 2
        for gi in range(8):
            g = ct * 8 + gi
            gh, gl = g // 16, g % 16
            pb = k.psum[gi % 2]
            tp = k.tpsum[gi % 2]
            cx.op("tensor", lambda h, pb=pb, g=g: h.matmul(pb[:, 0:256], k.s5TT[:, g, :], k.s5U[:, g, :], start=True, stop=False),
                  r=[k.t_s5tt, k.t_s5U], w=[tp], inc=False)
            for d in range(2):
                for ri in range(2):
                    slot0 = 0 if d == 0 else 1
                    rhs = pap(SH, gh * 64, 64, d * 257 * 32 + slot0 * 32 + ri * 16 + gl, [[32, 256]])
                    last = (d == 1 and ri == 1)
                    cx.op("tensor", lambda h, pb=pb, rhs=rhs, d=d, ri=ri, gh=gh, gl=gl, last=last: h.matmul(
                        pb[:, 0:256], k.s5CS[gh * 64:(gh + 1) * 64, d, gl, ri, :], rhs, start=False, stop=last),
                        r=[tSH, tSHh, k.t_s5w], w=[tp], inc=last)
            if gi % 2 == 0:
                cx.op("scalar", lambda h, pb=pb, b=b, gi=gi: h.copy(ybuf[b][:, gi, :], pb[:, 0:256]), r=[tp], w=[tyb[b]])
            else:
                cx.op(V, lambda h, pb=pb, b=b, gi=gi: h.tensor_copy(ybuf[b][:, gi, :], pb[:, 0:256]), r=[tp], w=[tyb[b]])
        for t in range(8):
            q0 = 32 * (t // 2)
            pb = k.psum[2 + t % 4]
            tp = k.tpsum[2 + t % 4]
            for gi in range(8):
                cx.op("tensor", lambda h, pb=pb, t=t, gi=gi, q0=q0, b=b: h.matmul(pb[:, 0:256], k.selT[q0:q0 + 32, t % 2, gi, :], ybuf[b][q0:q0 + 32, gi, :], start=(gi == 0), stop=(gi == 7), tile_position=(q0, 0)),
                      r=[tyb[b]], w=[tp], inc=(gi == 7))
            dstv = pap(gT, 0, 128, ct * 2048 + t, [[8, 256]])
            cx.op("scalar", lambda h, pb=pb, dstv=dstv: h.activation(dstv, pb[:, 0:256], AF.Gelu), r=[tp], w=[tgT[ct]])
        cx.op("vector", lambda h, ct=ct: h.tensor_copy(gTb[:, ct, :], gT[:, ct, :]), r=[tgT[ct]], w=[tgTb[ct]])
    k.dbg_add("s5_g", gT, tgT)
    if S5STOP == 'c':
        cx.barrier(); ar.release(m0); return
    wg = ar.alloc([4, 512], BF16)
    twg = Trk()
    wsrc = D["s5_w_glu"]
    cx.dma("gpsimd", wg, bass.AP(wsrc.tensor, wsrc.offset, [[512, 128], [512 * 128, 4], [1, 512]]), cx.fresh('sw'), w=[twg])
    bgl = ar.alloc([4], F32)
    nw = ar.alloc([4], F32)
    tb = Trk()
    sl_ = cx.fresh()
    cx.dma("sync", bgl, D["s5_bglu"], sl_, w=[tb])
    cx.dma("sync", nw, D["s5_normw"], sl_, w=[tb])
    sig = ar.alloc([4, 512], BF16)
    tsig = Trk()
    sq = ar.alloc([4, 512], BF16)
    tsq = Trk()
    rs = ar.alloc([512], F32)
    trs = Trk()
    for nck in range(4):
        ts = slice(nck * 512, (nck + 1) * 512)
        for co in range(4):
            pb = k.psum[co % 2]
            tp = k.tpsum[co % 2]
            for ci in range(4):
                cx.op("tensor", lambda h, pb=pb, co=co, ci=ci, ts=ts: h.matmul(pb[:, :], wg[:, ci, co * 128:(co + 1) * 128], gTb[:, ci, ts], start=(ci == 0), stop=(ci == 3)),
                      r=[twg] + tgTb, w=[tp], inc=(ci == 3))
            cx.op("scalar", lambda h, pb=pb, co=co: h.activation(sig[:, co, :], pb[:, :], AF.Sigmoid, bias=bgl[:, co:co + 1]), r=[tp, tb], w=[tsig])
        for co in range(4):
            cx.op(V, lambda h, co=co, ts=ts: h.tensor_tensor(out=gT[:, co, ts], in0=gT[:, co, ts], in1=sig[:, co, :], op=ALU.mult), r=[tsig, tgT[co]], w=[tgT[co]])
            cx.op("scalar", lambda h, co=co, ts=ts: h.activation(sq[:, co, :], gT[:, co, ts], AF.Square), r=[tgT[co]], w=[tsq])
        pb = k.psum[2 + nck % 2]
        tp = k.tpsum[2 + nck % 2]
        for co in range(4):
            cx.op("tensor", lambda h, pb=pb, co=co: h.matmul(pb[:, :], k.onesb, sq[:, co, :], start=(co == 0), stop=(co == 3)), r=[tsq], w=[tp], inc=(co == 3))
        cx.op("scalar", lambda h, pb=pb: h.activation(rs, pb[:, :], AF.Sqrt, scale=1.0 / 512, bias=k.epsc), r=[tp], w=[trs])
        cx.op(V, lambda h: h.reciprocal(rs, rs), r=[trs], w=[trs])
        for co in range(4):
            cx.op(V, lambda h, co=co, ts=ts: h.scalar_tensor_tensor(out=yT[:, co, ts], in0=gT[:, co, ts], scalar=nw[:, co:co + 1], in1=rs, op0=ALU.mult, op1=ALU.mult),
                  r=[tgT[co], trs, tb, tsq], w=[t_yT])
    k.dbg_add("s5_gl", gT, tgT)
    cx.barrier()
    ar.release(m0)


def build(in_shapes, stage="full", dbg_names=(), n_heads=4, n_experts=32):
    nc = bass.Bass("TRN2", target_bir_lowering=False)
    k = K()
    k.n_heads = n_heads
    k.n_experts = n_experts
    k.nc = nc
    D = {}
    for nm, (shape, dt) in in_shapes.items():
        D[nm] = nc.dram_tensor(nm, list(shape), dt, kind="ExternalInput").ap()
    k.D = D
    out = nc.dram_tensor("out", [S, DM], F32, kind="ExternalOutput").ap()
    k.dbg = {}
    k.dbg_req = set(dbg_names)

    with contextlib.ExitStack() as st:
        cx = Ctx(nc, st)
        k.cx = cx

        def finish():
            deps = [(s_.key, s_.total) for s_ in cx.slots if s_.total > 0]
            cx.wait_deps("sync", deps + [(e, cx.cnt[e]) for e in ENGS if e != "sync" and cx.cnt[e] > 0])
            with nc.Block() as block:
                cx.emit_all(block)
            k.n_ops = cx.n_ops
            return nc, k
        k.slot_c = cx.slot("c")
        k.slot_w = cx.slot("w")
        k.slot_x = [cx.slot("x0"), cx.slot("x1")]
        k.slot_o = cx.slot("o")
        k.psum = [cx.ps("ps%d" % i, [128, 512], F32) for i in range(8)]
        k.psum = [p[:, :] for p in k.psum]
        k.tpsum = [Trk("ps%d" % i, excl=True) for i in range(8)]
        k.ident = cx.sb("ident", [128, 128], F32)[:, :]
        k.identb = cx.sb("identb", [128, 128], BF16)[:, :]
        k.ones = cx.sb("ones", [128, 128], F32)[:, :]
        k.onesb = cx.sb("onesb", [128, 128], BF16)[:, :]
        k.epsc = cx.sb("epsc", [128, 1], F32)[:, :]
        k.selT = cx.sb("selT", [128, 2, 8, 128], BF16)[:, :, :, :]
        tc = Trk()
        cx.dma("sync", k.ident, D["ident"], k.slot_c, w=[tc])
        cx.dma("sync", k.identb, D["identb"], k.slot_c, w=[tc])
        cx.dma("sync", k.ones, D["ones"], k.slot_c, w=[tc])
        cx.dma("gpsimd", k.onesb, D["ones"], cx.fresh("sw"), w=[tc])
        cx.op("vector", lambda h: h.memset(k.epsc, EPS), w=[tc])
        cx.dma("sync", k.selT, D["selT"], k.slot_c, w=[tc])
        k.s5A1 = cx.sb("s5A1", [128, 64], F32)[:, :]
        k.s5A2 = cx.sb("s5A2", [128, 64], F32)[:, :]
        ar = Arena(cx, 51456)
        k.ar = ar
        cx.barrier()

        def dbg_add(name, ap, trks):
            if name in k.dbg_req:
                shape = list(ap.shape)
                dt_ = F32
                o = nc.dram_tensor("dbg_" + name, shape, dt_, kind="ExternalOutput").ap()
                cx.dma("gpsimd" if ap.dtype != F32 else "sync", o, ap, cx.fresh("sw" if ap.dtype != F32 else "hw"), r=list(trks))
        k.dbg_add = dbg_add

        regA = ar.alloc([NT * 1024], F32)
        arA = Arena(cx, NT * 1024, base=regA)
        k.arA = arA
        yT = ar.alloc([8, 2048], BF16)
        t_yT = Trk()
        k.s5U = arA.alloc([32, 256], BF16)
        k.t_s5U = Trk()
        m_h = arA.mark()
        k.hT = arA.alloc([8, 2048], BF16)
        k.t_hT = Trk()

        m1 = ar.mark()
        xt = [ar.alloc([1024], F32) for _ in range(2)]
        txt = [Trk(), Trk()]

        def src_x(i):
            b = i % 2
            cx.dma("sync", xt[b], D["x"][i * 128:(i + 1) * 128, :], k.slot_x[b], w=[txt[b]])
            return xt[b], txt[b]
        norm_transpose(k, "mix", src_x, NT, D["norm_mix"], k.hT, BF16, k.t_hT)
        cx.barrier()
        ar.release(m1)

        if stage == 'p1':
            return finish()
        s5_build_U(k)
        if stage == 'U':
            return finish()
        mg = ar.mark()
        gdn_setup(k)
        for hd in range(k.n_heads):
            gdn_head(k, hd, yT, t_yT)
        ar.release(mg)
        k.dbg_add("ygdnT", yT[:, 4:8, :], [t_yT])
        if stage == 'gdn':
            return finish()
        cx.barrier()
        arA.release(m_h)
        k.s5AT = arA.alloc([64, 128], BF16)
        k.t_s5at = Trk()
        k.s5CS = arA.alloc([2, 16, 2, 128], BF16)
        k.s5TT = arA.alloc([32, 128], BF16)
        k.t_s5tt = Trk()
        s5_prep(k)
        if stage == 's5prep':
            return finish()
        s5_main(k, yT[:, 0:4, :], t_yT)
        k.dbg_add("ys5T", yT[:, 0:4, :], [t_yT])
        if stage == "s5":
            return finish()
        if True:
            cx.barrier()
            k.xacc = regA.rearrange('p (a b) -> p a b', a=NT)
            k.txacc = [Trk() for _ in range(NT)]
            out_proj(k, yT, t_yT)
            k.dbg_add("x1", k.xacc, k.txacc)
            if stage == 'oproj':
                return finish()
            xattn(k)
            if stage == 'xattn':
                return finish()
            k.dbg_add("x2", k.xacc, k.txacc)
            moe(k)
            k.dbg_add("x3", k.xacc, k.txacc + [t_ for p_ in k.txh for t_ in p_])
            final_norm(k, out)

        return finish()


def host_inputs(inp, b):
    m = {}
    m["x"] = np.ascontiguousarray(inp["x"][b])
    m["mem"] = np.ascontiguousarray(inp["mem"][b])
    m["norm_mix"] = inp["norm_mix"][0]
    m["w_in"] = inp["w_in"][0]
    m["w_out"] = inp["w_out"][0]
    m["s5_w_glu"] = inp["s5_w_glu"][0]
    m.update(host_s5(inp))
    cv = inp["gdn_conv"][0]
    m["gdn_convw"] = np.ascontiguousarray(cv.reshape(5, 3, 4, 128).transpose(3, 2, 1, 0))
    for nm in ("gdn_a_log_f", "gdn_dt_bias_f", "gdn_a_log_b", "gdn_dt_bias_b"):
        m[nm] = inp[nm][0]
    m["gdn_norm"] = inp["gdn_norm"][0]
    for nm in ("norm_xattn", "norm_mem", "xa_wq", "xa_wk", "xa_wv", "xa_wo", "norm_moe", "router_group_w", "router_group_b",
               "router_expert_w", "router_expert_b", "moe_w_gate", "moe_w_up", "moe_w_down"):
        m[nm] = inp[nm][0]
    m["norm_final"] = inp["norm_final"]
    m.update(host_consts())
    return m


def gdn_setup(k):
    cx, ar, D = k.cx, k.ar, k.D
    V = "vector"
    G = K()
    k.G = G
    G.mask = ar.alloc([7, 128], F32)
    G.tmask = Trk()
    cx.dma("sync", G.mask, D["gmask"][:, 0:7, :], cx.fresh(), w=[G.tmask])
    wsm = ar.alloc([8, 16], BF16)
    tw = Trk()
    load_w_cols(k, D["w_in"], 2560, 16, wsm, tw, cx.fresh('sw'))
    BA = ar.alloc([16, 16], F32)
    tBA = Trk()
    for i in range(NT):
        pb = k.psum[i % 4]
        tp = k.tpsum[i % 4]
        for c in range(8):
            cx.op("tensor", lambda h, pb=pb, c=c, i=i: h.matmul(pb[:, 0:16], k.hT[:, c, i * 128:(i + 1) * 128], wsm[:, c, :], start=(c == 0), stop=(c == 7)),
                  r=[tw, k.t_hT], w=[tp], inc=(c == 7))
        cx.op("scalar", lambda h, pb=pb, i=i: h.copy(BA[:, i, :], pb[:, 0:16]), r=[tp], w=[tBA])
    pr = ar.alloc([4, 4], F32)
    tpr = Trk()
    sl_ = cx.fresh()
    for j, nm in enumerate(("gdn_a_log_f", "gdn_dt_bias_f", "gdn_a_log_b", "gdn_dt_bias_b")):
        cx.dma("sync", pr[:, j, :], dram_bcast(D[nm], 128, 4), sl_, w=[tpr])
    G.nw = ar.alloc([128], F32)
    cx.dma("sync", G.nw, dram_bcast(D["gdn_norm"], 128, 128), sl_, w=[tpr])
    G.tpr = tpr
    T = Trk()
    G.T = T
    G.beta, G.nb, G.gc, G.eg, G.neg, G.ed = [], [], [], [], [], []
    def per_dir(d):
        beta = ar.alloc([16, 4], F32)
        nb = ar.alloc([16, 4], F32)
        g = ar.alloc([16, 4], F32)
        gc = ar.alloc([16, 4], F32)
        gt = ar.alloc([16, 4], F32)
        eg = ar.alloc([16, 4], F32)
        neg = ar.alloc([16, 4], F32)
        ed = ar.alloc([16, 4], F32)
        ea = ar.alloc([4], F32)
        braw = BA[:, :, d * 4:(d + 1) * 4]
        araw = BA[:, :, 8 + d * 4:8 + (d + 1) * 4]
        cx.op("scalar", lambda h: h.activation(beta, braw, AF.Sigmoid), r=[tBA, T], w=[T])
        cx.op(V, lambda h: h.tensor_scalar(nb, beta, -1.0, None, op0=ALU.mult), r=[T], w=[T])
        cx.op("scalar", lambda h: h.activation(ea, pr[:, 2 * d, :], AF.Exp), r=[tpr, T], w=[T])
        cx.op(V, lambda h: h.tensor_tensor(out=g, in0=araw, in1=pr[:, 2 * d + 1, :].unsqueeze(1).to_broadcast([128, 16, 4]), op=ALU.add), r=[tBA, tpr, T], w=[T])
        cx.op("scalar", lambda h: h.activation(g, g, AF.Exp), r=[T], w=[T])
        cx.op("scalar", lambda h: h.activation(g, g, AF.Ln, bias=1.0), r=[T], w=[T])
        cx.op(V, lambda h: h.scalar_tensor_tensor(out=g, in0=g, scalar=-1.0, in1=ea.unsqueeze(1).to_broadcast([128, 16, 4]), op0=ALU.mult, op1=ALU.mult), r=[T], w=[T])
        g2 = g.rearrange("p a b -> p (a b)")
        pb = k.psum[4 + d]
        tp = k.tpsum[4 + d]
        cx.op("tensor", lambda h, pb=pb, d=d: h.matmul(pb[:, 0:64], G.mask[:, d, :], g2, start=True, stop=True), r=[T, G.tmask], w=[tp])
        cx.op("tensor", lambda h, pb=pb: h.matmul(pb[:, 64:128], G.mask[:, 6, :], g2, start=True, stop=True), r=[T, G.tmask], w=[tp])
        cx.op(V, lambda h, pb=pb: h.tensor_copy(gc.rearrange("p a b -> p (a b)"), pb[:, 0:64]), r=[tp], w=[T])
        cx.op(V, lambda h, pb=pb: h.tensor_tensor(out=gt.rearrange("p a b -> p (a b)"), in0=pb[:, 64:128], in1=gc.rearrange("p a b -> p (a b)"), op=ALU.subtract), r=[tp, T], w=[T])
        cx.op("scalar", lambda h: h.activation(eg, gc, AF.Exp), r=[T], w=[T])
        cx.op("scalar", lambda h: h.activation(ed, gt, AF.Exp), r=[T], w=[T])
        cx.op(V, lambda h: h.tensor_scalar(neg, eg, -1.0, None, op0=ALU.mult), r=[T], w=[T])
        G.g = getattr(G, "g", []) + [g]
        G.beta.append(beta); G.nb.append(nb); G.gc.append(gc); G.eg.append(eg); G.neg.append(neg); G.ed.append(ed)
    per_dir(0)
    per_dir(1)
    G.osum = ar.alloc([16, 128], F32)
    G.tosum = [Trk() for _ in range(NT)]


def gdn_head(k, hd, yT, t_yT):
    cx, ar, D, G = k.cx, k.ar, k.D, k.G
    V = "vector"
    m0 = ar.mark()
    qnT = ar.alloc([2048], BF16)
    knT = ar.alloc([2048], BF16)
    Ktok = ar.alloc([16, 128], BF16)
    Vtok = ar.alloc([16, 128], BF16)
    tq, tk_, tKt, tVt = Trk(), Trk(), Trk(), Trk()
    wz = ar.alloc([8, 128], BF16)
    twz = Trk()
    load_w_cols(k, D["w_in"], 512 + 1536 + hd * 128, 128, wz, twz, cx.fresh('sw'))
    mA = ar.mark()
    w3 = [ar.alloc([8, 128], BF16) for _ in range(3)]
    tw3 = [Trk() for _ in range(3)]
    for j in range(3):
        load_w_cols(k, D["w_in"], 512 + j * 512 + hd * 128, 128, w3[j], tw3[j], cx.fresh('sw'))
    cw = ar.alloc([3, 5], F32)
    tcw = Trk()
    cx.dma("sync", cw, D["gdn_convw"][:, hd, :, :], cx.fresh(), w=[tcw])
    diag = ar.alloc([15, 128], BF16)
    tdg = Trk()
    for j in range(3):
        for t in range(5):
            cx.op("vector", lambda h, j=j, t=t: h.tensor_scalar(diag[:, j * 5 + t, :], k.identb, cw[:, j, t:t + 1], None, op0=ALU.mult), r=[tcw], w=[tdg])
    import os
    ALV = int(os.environ.get("GDN_ALV", "9"))
    if ALV == 0:
        cx.barrier(); ar.release(m0); return
    raw = [ar.alloc([2052], BF16) for _ in range(2)]
    traw = [Trk(), Trk()]
    for b in range(2):
        cx.op("gpsimd", lambda h, b=b: h.memset(raw[b][:, 0:2], 0.0), w=[traw[b]])
        cx.op("gpsimd", lambda h, b=b: h.memset(raw[b][:, 2050:2052], 0.0), w=[traw[b]])
    act = ar.alloc([2048], F32)
    tact = Trk()
    vT = ar.alloc([2048], BF16)
    tvT = Trk()
    sqb = ar.alloc([2048], BF16)
    tsqb = Trk()
    rn = [ar.alloc([512], F32) for _ in range(4)]
    trn = [Trk() for _ in range(4)]
    tactn = [Trk() for _ in range(4)]
    tsqn = [Trk() for _ in range(4)]
    if ALV == 1:
        cx.barrier(); ar.release(m0); return
    for j in range(3):
        b = j % 2

        def consume(n, pb, tp, b=b):
            cx.op(V if n % 2 else "scalar", (lambda h: h.tensor_copy(raw[b][:, 2 + n * 512:2 + (n + 1) * 512], pb[:, :])) if n % 2 else
                  (lambda h: h.copy(raw[b][:, 2 + n * 512:2 + (n + 1) * 512], pb[:, :])), r=[tp], w=[traw[b]])
        proj_fm(k, w3[j], tw3[j], consume)
        for n in range(4):
            pb = k.psum[4 + n]
            tp = k.tpsum[4 + n]
            for t in range(5):
                cx.op("tensor", lambda h, pb=pb, t=t, n=n, j=j, b=b: h.matmul(pb[:, :], diag[:, j * 5 + t, :], raw[b][:, n * 512 + t:n * 512 + t + 512], start=(t == 0), stop=(t == 4)),
                      r=[tdg, traw[b]], w=[tp], inc=(t == 4))
        for n in range(4):
            pb = k.psum[4 + n]
            tp = k.tpsum[4 + n]
            ts = slice(n * 512, (n + 1) * 512)
            if j == 2:
                cx.op("scalar", lambda h, pb=pb, ts=ts: h.activation(vT[:, ts], pb[:, :], AF.Silu), r=[tp], w=[tvT])
            else:
                cx.op("scalar", lambda h, pb=pb, ts=ts: h.activation(act[:, ts], pb[:, :], AF.Silu), r=[tp], w=[tactn[n]])
        if j < 2 and ALV > 2:
            for n in range(4):
                ts = slice(n * 512, (n + 1) * 512)
                cx.op("scalar", lambda h, ts=ts: h.activation(sqb[:, ts], act[:, ts], AF.Square), r=[tactn[n]], w=[tsqn[n]])
            for n in range(4):
                ts = slice(n * 512, (n + 1) * 512)
                pb2 = k.psum[n]
                tp2 = k.tpsum[n]
                cx.op("tensor", lambda h, pb2=pb2, ts=ts: h.matmul(pb2[:, :], k.onesb, sqb[:, ts], start=True, stop=True), r=[tsqn[n]], w=[tp2])
            for n in range(4):
                pb2 = k.psum[n]
                tp2 = k.tpsum[n]
                cx.op("scalar", lambda h, pb2=pb2, n=n: h.activation(rn[n], pb2[:, :], AF.Sqrt, bias=k.epsc), r=[tp2], w=[trn[n]])
            for n in range(4):
                cx.op(V, lambda h, n=n: h.reciprocal(rn[n], rn[n]), r=[trn[n]], w=[trn[n]])
            dstT, tdst, scl = (qnT, tq, 128.0 ** -0.5) if j == 0 else (knT, tk_, 1.0)
            for n in range(4):
                ts = slice(n * 512, (n + 1) * 512)
                cx.op(V, lambda h, ts=ts, n=n, dstT=dstT, scl=scl: h.scalar_tensor_tensor(out=dstT[:, ts], in0=act[:, ts], scalar=scl, in1=rn[n], op0=ALU.mult, op1=ALU.mult),
                      r=[tactn[n], trn[n]], w=[tdst])
    if ALV <= 3:
        cx.barrier(); ar.release(m0); return
    TV = int(os.environ.get("GDN_TV", "0"))
    for i in range(NT):
        pb = k.psum[i % 2].bitcast(BF16)
        tp = k.tpsum[i % 2]
        if TV == 0:
            cx.op("tensor", lambda h, pb=pb, i=i: h.transpose(pb[:, 0:128], knT[:, i * 128:(i + 1) * 128], k.identb), r=[tk_], w=[tp])
            cx.op("tensor", lambda h, pb=pb, i=i: h.transpose(pb[:, 128:256], vT[:, i * 128:(i + 1) * 128], k.identb), r=[tvT], w=[tp])
            cx.op("scalar", lambda h, pb=pb, i=i: h.copy(Ktok[:, i, :], pb[:, 0:128]), r=[tp], w=[tKt])
            cx.op(V, lambda h, pb=pb, i=i: h.tensor_copy(Vtok[:, i, :], pb[:, 128:256]), r=[tp], w=[tVt])
        elif TV == 1:
            cx.op("tensor", lambda h, pb=pb, i=i: h.transpose(pb[:, 0:128], knT[:, i * 128:(i + 1) * 128], k.identb), r=[tk_], w=[tp])
            cx.op("scalar", lambda h, pb=pb, i=i: h.copy(Ktok[:, i, :], pb[:, 0:128]), r=[tp], w=[tKt])
        elif TV == 2:
            cx.op("tensor", lambda h, pb=pb, i=i: h.transpose(pb[:, 0:128], vT[:, i * 128:(i + 1) * 128], k.identb), r=[tvT], w=[tp])
            cx.op(V, lambda h, pb=pb, i=i: h.tensor_copy(Vtok[:, i, :], pb[:, 0:128]), r=[tp], w=[tVt])
    if hd == 0:
        k.dbg_add("gdn_qn", qnT, [tq])
        k.dbg_add("gdn_kn", knT, [tk_])
        k.dbg_add("gdn_vtok", Vtok, [tVt])
    cx.barrier()
    ar.release(mA)
    STOP = os.environ.get("GDN_STOP", "")
    if STOP == "A":
        ar.release(m0)
        return
    qgT = [ar.alloc([2048], BF16) for _ in range(2)]
    Kd = [ar.alloc([16, 128], BF16) for _ in range(2)]
    Pm = [ar.alloc([16, 128], BF16) for _ in range(2)]
    QKm = [ar.alloc([16, 128], BF16) for _ in range(2)]
    etot = [ar.alloc([32], F32) for _ in range(2)]
    WnT = [ar.alloc([2048], BF16) for _ in range(2)]
    U0b = [ar.alloc([16, 128], BF16) for _ in range(2)]
    tWn = [[Trk() for _ in range(NT)] for _ in range(2)]
    tU0 = [[Trk() for _ in range(NT)] for _ in range(2)]
    tqg = [[Trk() for _ in range(NT)] for _ in range(2)]
    tKd = [[Trk() for _ in range(NT)] for _ in range(2)]
    tPm = [[Trk() for _ in range(NT)] for _ in range(2)]
    tQK = [[Trk() for _ in range(NT)] for _ in range(2)]
    tet = [[Trk() for _ in range(NT)] for _ in range(2)]
    NI = 8
    mN = ar.mark()
    NDT = BF16 if os.environ.get('GDN_NEU', 'bf16') == 'bf16' else F32
    nid = k.identb if NDT == BF16 else k.ident
    Xb = [[ar.alloc([128], NDT) for _ in range(2)] for _ in range(NI)]
    XTb = [[ar.alloc([128], NDT) for _ in range(2)] for _ in range(NI)]
    Pb = [[ar.alloc([128], NDT) for _ in range(2)] for _ in range(NI)]

    def tview(pn_, c0):
        return pn_[:, c0:c0 + 128] if NDT == F32 else pn_.bitcast(BF16)[:, 2 * c0:2 * c0 + 128]
    tX = [Trk() for _ in range(NI)]
    EGB = [ar.alloc([128], F32) for _ in range(NI)]
    ET = [ar.alloc([128], BF16) for _ in range(NI)]
    ETs = ET
    tE = [Trk() for _ in range(NI)]
    tEG = [Trk() for _ in range(NI)]
    Kg = [ar.alloc([128], BF16) for _ in range(NI)]
    tKg = [Trk() for _ in range(NI)]
    tXP = [Trk() for _ in range(NI)]
    insts = [(i, d) for i in range(NT) for d in range(2)]
    for g0 in range(0, len(insts), NI):
        grp = insts[g0:g0 + NI]
        info = []
        for s_, (i, d) in enumerate(grp):
            info.append(dict(s_=s_, i=i, d=d, tsl=slice(i * 128, (i + 1) * 128),
                             col=pap(G.g[d], 0, 128, i * 4 + hd, [[0, 128]]),
                             gcc=G.gc[d][:, i, hd:hd + 1], nbc=G.nb[d][:, i, hd:hd + 1], edc=G.ed[d][:, i, hd:hd + 1],
                             pb=k.psum[s_], tp=k.tpsum[s_]))
        for q_ in info:
            s_, i, d, tsl, col, pb, tp = q_["s_"], q_["i"], q_["d"], q_["tsl"], q_["col"], q_["pb"], q_["tp"]
            cx.op("tensor", lambda h, pb=pb, col=col, d=d: h.matmul(pb[:, 0:128], col, G.mask[:, d, :], start=True, stop=True), r=[G.T, G.tmask], w=[tp], inc=False)
            cx.op("tensor", lambda h, pb=pb, tsl=tsl: h.matmul(pb[:, 128:256], knT[:, tsl], knT[:, tsl], start=True, stop=True), r=[tk_], w=[tp], inc=False)
            cx.op("tensor", lambda h, pb=pb, tsl=tsl: h.matmul(pb[:, 256:384], knT[:, tsl], qnT[:, tsl], start=True, stop=True), r=[tk_, tq], w=[tp])
        for q_ in info:
            s_, i, d, pb, tp, gcc = q_["s_"], q_["i"], q_["d"], q_["pb"], q_["tp"], q_["gcc"]
            cx.op("scalar", lambda h, pb=pb, s_=s_: h.activation(EGB[s_], pb[:, 0:128], AF.Exp), r=[tp], w=[tEG[s_]])
            cx.op(V, lambda h, pb=pb, s_=s_, gcc=gcc, d=d: h.scalar_tensor_tensor(out=ET[s_], in0=pb[:, 0:128], scalar=gcc, in1=G.mask[:, 2 + d, :], op0=ALU.subtract, op1=ALU.min),
                  r=[tp, G.T, G.tmask], w=[tE[s_]])
        for q_ in info:
            s_, i, d, tsl = q_["s_"], q_["i"], q_["d"], q_["tsl"]
            cx.op("scalar", lambda h, s_=s_: h.activation(ET[s_], ET[s_], AF.Exp), r=[tE[s_]], w=[tE[s_]])
            cx.op(V, lambda h, s_=s_, tsl=tsl, d=d: h.tensor_tensor(out=qgT[d][:, tsl], in0=qnT[:, tsl], in1=EGB[s_], op=ALU.mult), r=[tq, tEG[s_]], w=[tqg[d][i]])
        for q_ in info:
            s_, i, d, pb, tp, edc = q_["s_"], q_["i"], q_["d"], q_["pb"], q_["tp"], q_["edc"]
            c0, c1 = (63, 127) if d == 0 else (0, 64)
            cx.op("scalar", lambda h, s_=s_, d=d, i=i, c0=c0: h.copy(etot[d][:, 2 * i:2 * i + 1], EGB[s_][:, c0:c0 + 1]), r=[tEG[s_]], w=[tet[d][i]])
            cx.op("scalar", lambda h, s_=s_, d=d, i=i, c1=c1: h.copy(etot[d][:, 2 * i + 1:2 * i + 2], EGB[s_][:, c1:c1 + 1]), r=[tEG[s_]], w=[tet[d][i]])
            cx.op("scalar", lambda h, d=d, i=i, edc=edc: h.activation(Kd[d][:, i, :], Ktok[:, i, :], AF.Identity, scale=edc), r=[tKt, G.T], w=[tKd[d][i]])
            egc = G.eg[d][:, i, hd:hd + 1]
            cx.op("scalar", lambda h, s_=s_, i=i, egc=egc: h.activation(Kg[s_], Ktok[:, i, :], AF.Identity, scale=egc), r=[tKt, G.T], w=[tKg[s_]])
            cx.op(V, lambda h, pb=pb, s_=s_, d=d, i=i: h.tensor_tensor(out=QKm[d][:, i, :], in0=pb[:, 256:384], in1=ET[s_], op=ALU.mult), r=[tp, tE[s_]], w=[tQK[d][i]])
        for q_ in info:
            s_, d = q_["s_"], q_["d"]
            cx.op(V, lambda h, s_=s_, d=d: h.tensor_tensor(out=ETs[s_], in0=ET[s_], in1=G.mask[:, 4 + d, :], op=ALU.mult), r=[tE[s_], G.tmask], w=[tE[s_]])
        for q_ in info:
            s_, pb, tp, nbc = q_["s_"], q_["pb"], q_["tp"], q_["nbc"]
            cx.op(V, lambda h, pb=pb, s_=s_, nbc=nbc: h.scalar_tensor_tensor(out=Xb[s_][0], in0=pb[:, 128:256], scalar=nbc, in1=ETs[s_], op0=ALU.mult, op1=ALU.mult),
                  r=[tp, tE[s_], G.T], w=[tX[s_]])
        for q_ in info:
            s_, pn, tn = q_["s_"], q_["pb"], q_["tp"]
            cx.op("tensor", lambda h, pn=pn, s_=s_: h.transpose(tview(pn, 384), Xb[s_][0], nid), r=[tX[s_]], w=[tn])
            cx.op(V, lambda h, s_=s_: h.tensor_tensor(out=Pb[s_][0], in0=Xb[s_][0], in1=nid, op=ALU.add), r=[tX[s_]], w=[tXP[s_]])
            cx.op("scalar", lambda h, pn=pn, s_=s_: h.copy(XTb[s_][0], tview(pn, 384)), r=[tn], w=[tX[s_]])
        for L in range(1, 6):
            a, b_ = (L - 1) % 2, L % 2
            for s_, (i, d) in enumerate(grp):
                pn = k.psum[s_]
                tn = k.tpsum[s_]
                if L < 5:
                    cx.op("tensor", lambda h, pn=pn, s_=s_, a=a: h.matmul(pn[:, 0:128], XTb[s_][a], Xb[s_][a], start=True, stop=True), r=[tX[s_]], w=[tn], inc=False)
                cx.op("tensor", lambda h, pn=pn, s_=s_, a=a: h.matmul(pn[:, 128:256], Xb[s_][a], XTb[s_][a], start=True, stop=True), r=[tX[s_]], w=[tn])
                e1, e2 = ("scalar", V) if s_ % 2 == 0 else (V, "scalar")
                if L < 5:
                    if e1 == "scalar":
                        cx.op("scalar", lambda h, pn=pn, s_=s_, b_=b_: h.copy(Xb[s_][b_], pn[:, 0:128]), r=[tn], w=[tX[s_]])
                    else:
                        cx.op(V, lambda h, pn=pn, s_=s_, b_=b_: h.tensor_copy(Xb[s_][b_], pn[:, 0:128]), r=[tn], w=[tX[s_]])
                if e2 == "scalar":
                    cx.op("scalar", lambda h, pn=pn, s_=s_, b_=b_: h.copy(XTb[s_][b_], pn[:, 128:256]), r=[tn], w=[tX[s_]])
                else:
                    cx.op(V, lambda h, pn=pn, s_=s_, b_=b_: h.tensor_copy(XTb[s_][b_], pn[:, 128:256]), r=[tn], w=[tX[s_]])
            for s_, (i, d) in enumerate(grp):
                pn = k.psum[s_]
                tn = k.tpsum[s_]
                cx.op("tensor", lambda h, pn=pn, s_=s_, a=a, b_=b_: h.matmul(pn[:, 256:384], XTb[s_][b_], Pb[s_][a], start=True, stop=True), r=[tX[s_], tXP[s_]], w=[tn])
                if L < 5:
                    cx.op(V, lambda h, pn=pn, s_=s_, a=a, b_=b_: h.tensor_tensor(out=Pb[s_][b_], in0=pn[:, 256:384], in1=Pb[s_][a], op=ALU.add), r=[tn, tXP[s_]], w=[tXP[s_]])
                else:
                    cx.op(V, lambda h, pn=pn, s_=s_, a=a, d=d, i=i: h.tensor_tensor(out=Pm[d][:, i, :], in0=pn[:, 256:384], in1=Pb[s_][a], op=ALU.add), r=[tn, tXP[s_]], w=[tPm[d][i], tXP[s_]])
        for s_, (i, d) in enumerate(grp):
            pn = k.psum[s_]
            tn = k.tpsum[s_]
            tsl = slice(i * 128, (i + 1) * 128)
            cx.op("tensor", lambda h, pn=pn, s_=s_, d=d, i=i: h.matmul(pn[:, 0:128], Kg[s_], Pm[d][:, i, :], start=True, stop=True), r=[tKg[s_], tPm[d][i]], w=[tn], inc=False)
            cx.op("tensor", lambda h, pn=pn, d=d, i=i: h.matmul(pn[:, 128:256], Pm[d][:, i, :], Vtok[:, i, :], start=True, stop=True), r=[tPm[d][i], tVt], w=[tn])
            btc_ = G.beta[d][:, i, hd:hd + 1]
            cx.op(V, lambda h, pn=pn, d=d, tsl=tsl: h.tensor_scalar(WnT[d][:, tsl], pn[:, 0:128], -1.0, None, op0=ALU.mult), r=[tn], w=[tWn[d][i]])
            cx.op("scalar", lambda h, pn=pn, d=d, i=i, btc_=btc_: h.activation(U0b[d][:, i, :], pn[:, 128:256], AF.Identity, scale=btc_), r=[tn, G.T], w=[tU0[d][i]])
    if STOP == "B":
        cx.barrier()
        ar.release(m0)
        return
    ar.release(mN)
    Sf = [[ar.alloc([128], F32) for _ in range(2)] for _ in range(2)]
    Sb = [ar.alloc([128], BF16) for _ in range(2)]
    Rp = [ar.alloc([128], BF16) for _ in range(2)]
    vn = [ar.alloc([128], BF16) for _ in range(2)]
    tS = [Trk(), Trk()]
    tSf = [Trk(), Trk()]
    tR = [Trk(), Trk()]
    tv = [Trk(), Trk()]
    cx.op("gpsimd", lambda h: h.memset(G.osum, 0.0), w=G.tosum)
    for d in range(2):
        cx.op("gpsimd", lambda h, d=d: h.memset(Sf[d][0], 0.0), w=[tS[d]])
        cx.op("gpsimd", lambda h, d=d: h.memset(Sb[d], 0.0), w=[tS[d]])
        cx.op("gpsimd", lambda h, d=d: h.memset(Rp[d], 0.0), w=[tR[d]])
        cx.op("gpsimd", lambda h, d=d: h.memset(vn[d], 0.0), w=[tv[d]])
    for step in range(32):
        for d in range(2):
            if d == 0:
                i, hh = step // 2, step % 2
            else:
                i, hh = 15 - step // 2, 1 - step % 2
            tsl = slice(i * 128, (i + 1) * 128)
            ps_ = slice(hh * 64, (hh + 1) * 64)
            cur, nxt = step % 2, (step + 1) % 2
            pcs = [k.psum[4 * d + q_] for q_ in range(4)]
            tcs = [k.tpsum[4 * d + q_] for q_ in range(4)]
            negc = G.neg[d][ps_, i, hd:hd + 1]
            btc = G.beta[d][ps_, i, hd:hd + 1]
            p1, pv_, po_, pst = pcs
            t1_, tv_, to_, tst = tcs
            cx.op("tensor", lambda h, p1=p1, tsl=tsl, d=d: h.matmul(p1[:, 0:128], WnT[d][:, tsl], Sb[d], start=True, stop=True), r=[tWn[d][i], tS[d]], w=[t1_])
            cx.op(V, lambda h, p1=p1, ps_=ps_, btc=btc, d=d, i=i: h.scalar_tensor_tensor(out=vn[d][ps_, :], in0=p1[ps_, 0:128], scalar=btc, in1=U0b[d][ps_, i, :], op0=ALU.mult, op1=ALU.add),
                  r=[t1_, tU0[d][i], G.T], w=[tv[d]])
            cx.op("tensor", lambda h, po_=po_, tsl=tsl, d=d: h.matmul(po_[:, 0:128], qgT[d][:, tsl], Sb[d], start=True, stop=False), r=[tqg[d][i], tS[d]], w=[to_], inc=False)
            cx.op("tensor", lambda h, po_=po_, ps_=ps_, d=d, i=i: h.matmul(po_[:, 0:128], QKm[d][ps_, i, :], vn[d][ps_, :], start=False, stop=True), r=[tQK[d][i], tv[d]], w=[to_])
            cx.op("tensor", lambda h, pst=pst, ps_=ps_, d=d, i=i: h.matmul(pst[:, 0:128], Kd[d][ps_, i, :], vn[d][ps_, :], start=True, stop=True), r=[tKd[d][i], tv[d]], w=[tst])
            cx.op("gpsimd" if False else V, lambda h, po_=po_, ps_=ps_, i=i: h.tensor_tensor(out=G.osum[ps_, i, :], in0=po_[ps_, 0:128], in1=G.osum[ps_, i, :], op=ALU.add), r=[to_, G.tosum[i]], w=[G.tosum[i]])
            etc = etot[d][:, 2 * i + hh:2 * i + hh + 1]
            cx.op(V, lambda h, pst=pst, d=d, cur=cur, nxt=nxt, etc=etc: h.scalar_tensor_tensor(out=Sf[d][nxt], in0=Sf[d][cur], scalar=etc, in1=pst[:, 0:128], op0=ALU.mult, op1=ALU.add),
                  r=[tst, tet[d][i], tS[d]], w=[tS[d]])
            cx.op("scalar", lambda h, d=d, nxt=nxt: h.copy(Sb[d], Sf[d][nxt]), r=[tS[d]], w=[tS[d]])
    if hd == 0:
        k.dbg_add("gdn_osum", G.osum, G.tosum)
    if STOP == "C":
        cx.barrier()
        ar.release(m0)
        return
    ss = ar.alloc([NT, 2], F32)
    tss = Trk()
    junk = ar.alloc([128], BF16)
    zs = [ar.alloc([128], F32) for _ in range(2)]
    tzs = [Trk(), Trk()]
    yb = [ar.alloc([128], BF16) for _ in range(2)]
    tyb = [Trk(), Trk()]
    for i in range(NT):
        cx.op("scalar", lambda h, i=i: h.activation(junk, G.osum[:, i, :], AF.Square, accum_out=ss[:, i, 0:1]), r=[G.tosum[i], tss], w=[tss])
    cx.op(V, lambda h: h.tensor_scalar(ss[:, :, 1:2], ss[:, :, 0:1], 1.0 / 128, EPS, op0=ALU.mult, op1=ALU.add), r=[tss], w=[tss])
    cx.op("scalar", lambda h: h.activation(ss[:, :, 1:2], ss[:, :, 1:2], AF.Sqrt), r=[tss], w=[tss])
    cx.op(V, lambda h: h.reciprocal(ss[:, :, 1:2], ss[:, :, 1:2]), r=[tss], w=[tss])
    for i in range(NT):
        b = i % 2
        pz = k.psum[b]
        tz = k.tpsum[b]
        for c in range(8):
            cx.op("tensor", lambda h, pz=pz, c=c, i=i: h.matmul(pz[:, 0:128], k.hT[:, c, i * 128:(i + 1) * 128], wz[:, c, :], start=(c == 0), stop=(c == 7)),
                  r=[twz, k.t_hT], w=[tz], inc=(c == 7))
        cx.op("scalar", lambda h, pz=pz, b=b: h.activation(zs[b], pz[:, 0:128], AF.Silu), r=[tz], w=[tzs[b]])
        s1 = ss[:, i, 1:2]
        cx.op(V, lambda h, i=i, s1=s1: h.scalar_tensor_tensor(out=G.osum[:, i, :], in0=G.osum[:, i, :], scalar=s1, in1=G.nw, op0=ALU.mult, op1=ALU.mult), r=[tss, G.tpr, G.tosum[i]], w=[G.tosum[i]])
        cx.op(V, lambda h, i=i, b=b: h.tensor_tensor(out=yb[b], in0=G.osum[:, i, :], in1=zs[b], op=ALU.mult), r=[G.tosum[i], tzs[b]], w=[tyb[b]])
        pt = k.psum[2 + b].bitcast(BF16)
        tt_ = k.tpsum[2 + b]
        cx.op("tensor", lambda h, pt=pt, b=b: h.transpose(pt[:, 0:128], yb[b], k.identb), r=[tyb[b]], w=[tt_])
        cx.op("scalar", lambda h, pt=pt, i=i: h.copy(yT[:, 4 + hd, i * 128:(i + 1) * 128], pt[:, 0:128]), r=[tt_], w=[t_yT])
    cx.barrier()
    ar.release(m0)


def out_proj(k, yT, t_yT):
    cx, ar, D = k.cx, k.ar, k.D
    m0 = ar.mark()
    wo = ar.alloc([8, 1024], BF16)
    two = Trk()
    wsrc = D["w_out"]
    sl_ = cx.fresh('sw')
    for c in range(8):
        cx.dma("gpsimd", wo[:, c, :], wsrc[c * 128:(c + 1) * 128, :], sl_, w=[two])
    slx = cx.fresh()
    for i in range(NT):
        cx.dma("sync", k.xacc[:, i, :], D["x"][i * 128:(i + 1) * 128, :], slx, w=[k.txacc[i]])
    for i in range(NT):
        k.txacc[i].w = (slx.key, slx.total)
    for i in range(NT):
        for half in range(2):
            pb = k.psum[(2 * i + half) % 4]
            tp = k.tpsum[(2 * i + half) % 4]
            for c in range(8):
                cx.op("tensor", lambda h, pb=pb, c=c, i=i, half=half: h.matmul(pb[:, :], yT[:, c, i * 128:(i + 1) * 128], wo[:, c, half * 512:(half + 1) * 512], start=(c == 0), stop=(c == 7)),
                      r=[t_yT, two], w=[tp], inc=(c == 7))
            xs = k.xacc[:, i, half * 512:(half + 1) * 512]
            cx.op("vector", lambda h, pb=pb, xs=xs: h.tensor_tensor(out=xs, in0=pb[:, :], in1=xs, op=ALU.add), r=[tp, k.txacc[i]], w=[k.txacc[i]])
    cx.barrier()
    ar.release(m0)


def xattn(k):
    cx, ar, D = k.cx, k.ar, k.D
    V = "vector"
    m0 = ar.mark()
    xnT = ar.alloc([8, 2048], BF16)
    t_xnT = Trk()
    memT = ar.alloc([8, 256], BF16)
    t_memT = Trk()
    m1 = ar.mark()
    mt = [ar.alloc([1024], F32) for _ in range(2)]
    tmt = [Trk(), Trk()]

    def src_mem(i):
        cx.dma("sync", mt[i], D["mem"][i * 128:(i + 1) * 128, :], cx.fresh(), w=[tmt[i]])
        return mt[i], tmt[i]
    norm_transpose(k, "mem", src_mem, 2, D["norm_mem"], memT, BF16, t_memT)
    ar.release(m1)
    norm_transpose(k, "xa", lambda i: (k.xacc[:, i, :], k.txacc[i]), NT, D["norm_xattn"], xnT, BF16, t_xnT, resident=True)
    k.dbg_add("xa_memT", memT, [t_memT])
    k.dbg_add("xa_xnT", xnT, [t_xnT])
    wq = [ar.alloc([8, 256], BF16) for _ in range(2)]
    wk = [ar.alloc([8, 256], BF16) for _ in range(2)]
    wv = [ar.alloc([8, 256], BF16) for _ in range(2)]
    wo = [ar.alloc([2, 1024], BF16) for _ in range(2)]
    tw = [Trk(), Trk()]
    sw = [cx.slot("xw0"), cx.slot("xw1")]
    kT = ar.alloc([2, 256], BF16)
    vh = ar.alloc([2, 256], BF16)
    tkv = Trk()
    qT = ar.alloc([2, 2048], BF16)
    tqT = Trk()
    E = [ar.alloc([2, 512], BF16) for _ in range(2)]
    tE = [Trk(), Trk()]
    rden = [ar.alloc([512], F32) for _ in range(2)]
    trd = [Trk(), Trk()]
    oTn = [ar.alloc([2, 512], BF16) for _ in range(2)]
    toT = [Trk(), Trk()]
    cx.barrier()
    k.txa = [[Trk(), Trk()] for _ in range(NT)]

    def load_head(hd):
        b = hd % 2
        c0 = hd * 256
        for (dst, nm) in ((wq[b], "xa_wq"), (wk[b], "xa_wk"), (wv[b], "xa_wv")):
            load_w_cols(k, D[nm], c0, 256, dst, tw[b], sw[b])
        src = D["xa_wo"]
        cx.dma("gpsimd", wo[b], bass.AP(src.tensor, src.offset + c0 * 1024, [[1024, 128], [128 * 1024, 2], [1, 1024]]), sw[b], w=[tw[b]])
    load_head(0)
    for hd in range(4):
        b = hd % 2
        if hd + 1 < 4:
            load_head(hd + 1)
        for dc in range(2):
            pb = k.psum[dc]
            tp = k.tpsum[dc]
            for c in range(8):
                cx.op("tensor", lambda h, pb=pb, c=c, dc=dc, b=b: h.matmul(pb[:, 0:256], wk[b][:, c, dc * 128:(dc + 1) * 128], memT[:, c, :], start=(c == 0), stop=(c == 7)),
                      r=[tw[b], t_memT], w=[tp], inc=(c == 7))
            cx.op("scalar", lambda h, pb=pb, dc=dc: h.copy(kT[:, dc, :], pb[:, 0:256]), r=[tp], w=[tkv])
        for mtile in range(2):
            pb = k.psum[2 + mtile]
            tp = k.tpsum[2 + mtile]
            for c in range(8):
                cx.op("tensor", lambda h, pb=pb, c=c, mtile=mtile, b=b: h.matmul(pb[:, 0:256], memT[:, c, mtile * 128:(mtile + 1) * 128], wv[b][:, c, :], start=(c == 0), stop=(c == 7)),
                      r=[tw[b], t_memT], w=[tp], inc=(c == 7))
            cx.op(V, lambda h, pb=pb, mtile=mtile: h.tensor_copy(vh[:, mtile, :], pb[:, 0:256]), r=[tp], w=[tkv])
        for dc in range(2):
            for n in range(4):
                pb = k.psum[4 + (dc * 4 + n) % 2]
                tp = k.tpsum[4 + (dc * 4 + n) % 2]
                for c in range(8):
                    cx.op("tensor", lambda h, pb=pb, c=c, dc=dc, n=n, b=b: h.matmul(pb[:, :], wq[b][:, c, dc * 128:(dc + 1) * 128], xnT[:, c, n * 512:(n + 1) * 512], start=(c == 0), stop=(c == 7)),
                          r=[tw[b], t_xnT], w=[tp], inc=(c == 7))
                if n % 2 == 0:
                    cx.op("scalar", lambda h, pb=pb, dc=dc, n=n: h.copy(qT[:, dc, n * 512:(n + 1) * 512], pb[:, :]), r=[tp], w=[tqT])
                else:
                    cx.op(V, lambda h, pb=pb, dc=dc, n=n: h.tensor_copy(qT[:, dc, n * 512:(n + 1) * 512], pb[:, :]), r=[tp], w=[tqT])
        if hd == 0:
            k.dbg_add("xa_qT", qT, [tqT])
            k.dbg_add("xa_kT", kT, [tkv])
            k.dbg_add("xa_vh", vh, [tkv])
        def emit_scores(n):
            eb = n % 2
            ts = slice(n * 512, (n + 1) * 512)
            for mtile in range(2):
                pb = k.psum[mtile]
                tp = k.tpsum[mtile]
                for dc in range(2):
                    cx.op("tensor", lambda h, pb=pb, dc=dc, mtile=mtile, ts=ts: h.matmul(pb[:, :], kT[:, dc, mtile * 128:(mtile + 1) * 128], qT[:, dc, ts], start=(dc == 0), stop=(dc == 1)),
                          r=[tkv, tqT], w=[tp], inc=(dc == 1))
                cx.op("scalar", lambda h, pb=pb, mtile=mtile, eb=eb: h.activation(E[eb][:, mtile, :], pb[:, :], AF.Exp, scale=1.0 / 16.0), r=[tp], w=[tE[eb]])

        def emit_rest(n):
            eb = n % 2
            pd = k.psum[2]
            tpd = k.tpsum[2]
            for mtile in range(2):
                cx.op("tensor", lambda h, pd=pd, mtile=mtile, eb=eb: h.matmul(pd[:, :], k.onesb, E[eb][:, mtile, :], start=(mtile == 0), stop=(mtile == 1)), r=[tE[eb]], w=[tpd], inc=(mtile == 1))
            cx.op(V, lambda h, pd=pd, eb=eb: h.reciprocal(rden[eb], pd[:, :]), r=[tpd], w=[trd[eb]])
            for dc in range(2):
                po = k.psum[3 + dc]
                tpo = k.tpsum[3 + dc]
                for mtile in range(2):
                    cx.op("tensor", lambda h, po=po, mtile=mtile, dc=dc, eb=eb: h.matmul(po[:, :], vh[:, mtile, dc * 128:(dc + 1) * 128], E[eb][:, mtile, :], start=(mtile == 0), stop=(mtile == 1)),
                          r=[tkv, tE[eb]], w=[tpo], inc=(mtile == 1))
                cx.op(V, lambda h, po=po, dc=dc, eb=eb: h.tensor_tensor(out=oTn[eb][:, dc, :], in0=po[:, :], in1=rden[eb], op=ALU.mult), r=[tpo, trd[eb]], w=[toT[eb]])
            for t in range(4):
                i = n * 4 + t
                for half in range(2):
                    pw_ = k.psum[5 + (t * 2 + half) % 3]
                    tpw = k.tpsum[5 + (t * 2 + half) % 3]
                    for dc in range(2):
                        cx.op("tensor", lambda h, pw_=pw_, dc=dc, t=t, half=half, b=b, eb=eb: h.matmul(pw_[:, :], oTn[eb][:, dc, t * 128:(t + 1) * 128], wo[b][:, dc, half * 512:(half + 1) * 512], start=(dc == 0), stop=(dc == 1)),
                              r=[toT[eb], tw[b]], w=[tpw], inc=(dc == 1))
                    xs = k.xacc[:, i, half * 512:(half + 1) * 512]
                    cx.op(V, lambda h, pw_=pw_, xs=xs: h.tensor_tensor(out=xs, in0=pw_[:, :], in1=xs, op=ALU.add), r=[tpw, k.txa[i][half]], w=[k.txa[i][half]])
        emit_scores(0)
        for n in range(4):
            if n + 1 < 4:
                emit_scores(n + 1)
            emit_rest(n)
    cx.barrier()
    ar.release(m0)


def moe(k):
    cx, ar, D = k.cx, k.ar, k.D
    V = "vector"
    m0 = ar.mark()
    xnT = ar.alloc([8, 2048], BF16)
    t_xnT = Trk()
    norm_transpose(k, "moe", lambda i: (k.xacc[:, i, :], k.txacc[i]), NT, D["norm_moe"], xnT, BF16, t_xnT, resident=True)
    wr = ar.alloc([8, 36], BF16)
    twr = Trk()
    sl_ = cx.fresh('sw')
    srcg, srce = D["router_group_w"], D["router_expert_w"]
    cx.dma("gpsimd", wr[:, :, 0:4], bass.AP(srcg.tensor, srcg.offset, [[4, 128], [4 * 128, 8], [1, 4]]), sl_, w=[twr])
    cx.dma("gpsimd", wr[:, :, 4:36], bass.AP(srce.tensor, srce.offset, [[32, 128], [32 * 128, 8], [1, 32]]), sl_, w=[twr])
    rb = ar.alloc([36], F32)
    trb = Trk()
    sl2 = cx.fresh()
    cx.dma("sync", rb[:, 0:4], dram_bcast(D["router_group_b"], 128, 4), sl2, w=[trb])
    cx.dma("sync", rb[:, 4:36], dram_bcast(D["router_expert_b"], 128, 32), sl2, w=[trb])
    cw = ar.alloc([NT, 32], F32)
    tcw = Trk()
    lgA = ar.alloc([NT, 36], F32)
    msk = ar.alloc([NT, 32], F32)
    eq2 = ar.alloc([NT, 32], F32)
    m8 = ar.alloc([NT, 8], F32)
    sc = ar.alloc([8, NT], F32)
    oh = ar.alloc([NT, 4], F32)
    ex = ar.alloc([NT, 4], F32)
    T = Trk()
    rbb = rb.unsqueeze(1).to_broadcast([128, 8, 36])
    for half in range(2):
        pb = k.psum[half]
        tp = k.tpsum[half]
        for ii in range(8):
            i = half * 8 + ii
            for c in range(8):
                cx.op("tensor", lambda h, pb=pb, c=c, i=i, ii=ii: h.matmul(pb[:, ii * 36:(ii + 1) * 36], xnT[:, c, i * 128:(i + 1) * 128], wr[:, c, :], start=(c == 0), stop=(c == 7)),
                      r=[t_xnT, twr], w=[tp], inc=(c == 7))
        cx.op(V, lambda h, pb=pb, half=half: h.tensor_tensor(out=lgA[:, half * 8:(half + 1) * 8, :], in0=pb[:, 0:288].rearrange("p (t e) -> p t e", t=8), in1=rbb, op=ALU.add), r=[tp, trb, T], w=[T])
    lg_g = lgA[:, :, 0:4]
    lg_e = lgA[:, :, 4:36]
    gmax, ngs, ssum, ptop, dm, w1, w2 = [sc[:, j_, :] for j_ in range(7)]

    def vop(fn):
        cx.op(V, fn, r=[T], w=[T])

    def aop(fn):
        cx.op("scalar", fn, r=[T], w=[T])
    b4 = lambda v: v.unsqueeze(2).to_broadcast([128, NT, 4])
    b32 = lambda v: v.unsqueeze(2).to_broadcast([128, NT, 32])
    vop(lambda h: h.tensor_reduce(out=gmax, in_=lg_g, axis=AX.X, op=ALU.max))
    vop(lambda h: h.tensor_tensor(out=oh, in0=lg_g, in1=b4(gmax), op=ALU.is_equal))
    vop(lambda h: h.tensor_tensor(out=ex, in0=lg_g, in1=b4(gmax), op=ALU.subtract))
    aop(lambda h: h.activation(ex, ex, AF.Exp))
    vop(lambda h: h.tensor_reduce(out=ssum, in_=ex, axis=AX.X, op=ALU.add))
    vop(lambda h: h.reciprocal(ptop, ssum))
    vop(lambda h: h.tensor_scalar(oh, oh, -1.0, 1e30, op0=ALU.add, op1=ALU.mult))
    vop(lambda h: h.tensor_tensor(out=msk.rearrange("p t (g e) -> p t g e", g=4), in0=lg_e.rearrange("p t (g e) -> p t g e", g=4),
                                  in1=oh.unsqueeze(3).to_broadcast([128, NT, 4, 8]), op=ALU.add))
    for i in range(NT):
        vop(lambda h, i=i: h.max(out=m8[:, i, :], in_=msk[:, i, :]))
    m1, m2 = m8[:, :, 0], m8[:, :, 1]
    vop(lambda h: h.tensor_tensor(out=dm, in0=m2, in1=m1, op=ALU.subtract))
    aop(lambda h: h.activation(dm, dm, AF.Exp))
    vop(lambda h: h.tensor_scalar(w1, dm, 1.0, None, op0=ALU.add))
    vop(lambda h: h.reciprocal(w1, w1))
    vop(lambda h: h.tensor_tensor(out=w2, in0=dm, in1=w1, op=ALU.mult))
    vop(lambda h: h.tensor_tensor(out=w1, in0=w1, in1=ptop, op=ALU.mult))
    vop(lambda h: h.tensor_tensor(out=w2, in0=w2, in1=ptop, op=ALU.mult))
    vop(lambda h: h.tensor_tensor(out=eq2, in0=msk, in1=b32(m2), op=ALU.is_equal))
    vop(lambda h: h.tensor_tensor(out=eq2, in0=eq2, in1=b32(w2), op=ALU.mult))
    vop(lambda h: h.tensor_tensor(out=msk, in0=msk, in1=b32(m1), op=ALU.is_equal))
    vop(lambda h: h.tensor_tensor(out=msk, in0=msk, in1=b32(w1), op=ALU.mult))
    cx.op(V, lambda h: h.tensor_tensor(out=cw, in0=msk, in1=eq2, op=ALU.add), r=[T], w=[tcw, T])
    k.dbg_add("moe_cw", cw, [tcw])
    wgu = [ar.alloc([8, 512], BF16) for _ in range(2)]
    wd = [ar.alloc([2, 1024], BF16) for _ in range(2)]
    twe = [Trk(), Trk()]
    swe = [cx.slot("we0"), cx.slot("we1")]
    sg = [ar.alloc([512], F32) for _ in range(2)]
    tsg = [Trk(), Trk()]
    h1 = [ar.alloc([2, 512], BF16) for _ in range(2)]
    th1 = [Trk(), Trk()]
    NE = k.n_experts

    def load_e(e):
        b = e % 2
        g_, u_, d_ = D["moe_w_gate"], D["moe_w_up"], D["moe_w_down"]
        cx.dma("gpsimd", wgu[b][:, :, 0:256], bass.AP(g_.tensor, g_.offset + e * 1024 * 256, [[256, 128], [256 * 128, 8], [1, 256]]), swe[b], w=[twe[b]])
        cx.dma("gpsimd", wgu[b][:, :, 256:512], bass.AP(u_.tensor, u_.offset + e * 1024 * 256, [[256, 128], [256 * 128, 8], [1, 256]]), swe[b], w=[twe[b]])
        cx.dma("gpsimd", wd[b], bass.AP(d_.tensor, d_.offset + e * 256 * 1024, [[1024, 128], [1024 * 128, 2], [1, 1024]]), swe[b], w=[twe[b]])
    import os
    NOLOAD = os.environ.get("MOE_NOLOAD", "") == "1"
    load_e(0)
    if NE > 1:
        load_e(1)
    jobs = [(e, n) for e in range(NE) for n in range(4)]
    state = {"cnt": 0, "loaded": 0}
    cx.barrier()
    k.txh = [[Trk(), Trk()] for _ in range(NT)]

    def emit_gu(j, fh):
        e, n = jobs[j]
        b = e % 2
        ts = slice(n * 512, (n + 1) * 512)
        hb = j % 2
        pg = k.psum[fh * 2]
        tpg = k.tpsum[fh * 2]
        pu = k.psum[fh * 2 + 1]
        tpu = k.tpsum[fh * 2 + 1]
        for c in range(8):
            cx.op("tensor", lambda h, pg=pg, c=c, fh=fh, ts=ts, b=b: h.matmul(pg[:, :], wgu[b][:, c, fh * 128:(fh + 1) * 128], xnT[:, c, ts], start=(c == 0), stop=(c == 7)),
                  r=[twe[b], t_xnT], w=[tpg], inc=(c == 7))
        for c in range(8):
            cx.op("tensor", lambda h, pu=pu, c=c, fh=fh, ts=ts, b=b: h.matmul(pu[:, :], wgu[b][:, c, 256 + fh * 128:256 + (fh + 1) * 128], xnT[:, c, ts], start=(c == 0), stop=(c == 7)),
                  r=[twe[b], t_xnT], w=[tpu], inc=(c == 7))
        cx.op("scalar", lambda h, pg=pg, fh=fh: h.activation(sg[fh], pg[:, :], AF.Silu), r=[tpg], w=[tsg[fh]])
        cx.op(V, lambda h, pu=pu, fh=fh, hb=hb: h.tensor_tensor(out=h1[hb][:, fh, :], in0=pu[:, :], in1=sg[fh], op=ALU.mult), r=[tpu, tsg[fh]], w=[th1[hb]])

    def emit_down(j):
        e, n = jobs[j]
        b = e % 2
        hb = j % 2
        for t in range(4):
            i = n * 4 + t
            for half in range(2):
                pdn = k.psum[4 + state["cnt"] % 4]
                tpd = k.tpsum[4 + state["cnt"] % 4]
                state["cnt"] += 1
                for fh in range(2):
                    cx.op("tensor", lambda h, pdn=pdn, fh=fh, t=t, half=half, hb=hb, b=b: h.matmul(pdn[:, :], h1[hb][:, fh, t * 128:(t + 1) * 128], wd[b][:, fh, half * 512:(half + 1) * 512], start=(fh == 0), stop=(fh == 1)),
                          r=[th1[hb], twe[b]], w=[tpd], inc=(fh == 1))
                xs = k.xacc[:, i, half * 512:(half + 1) * 512]
                cwc = cw[:, i, e:e + 1]
                cx.op(V, lambda h, pdn=pdn, xs=xs, cwc=cwc: h.scalar_tensor_tensor(out=xs, in0=pdn[:, :], scalar=cwc, in1=xs, op0=ALU.mult, op1=ALU.add), r=[tpd, tcw, k.txh[i][half]], w=[k.txh[i][half]])
        if n == 3 and e + 2 < NE and not NOLOAD:
            load_e(e + 2)
    nj = len(jobs)
    if nj > 0:
        emit_gu(0, 0)
        emit_gu(0, 1)
        for j in range(nj):
            if j + 1 < nj:
                emit_gu(j + 1, 0)
            emit_down(j)
            if j + 1 < nj:
                emit_gu(j + 1, 1)
    cx.barrier()
    ar.release(m0)


def final_norm(k, out):
    cx, ar, D = k.cx, k.ar, k.D
    V = "vector"
    m0 = ar.mark()
    gB = ar.alloc([1024], F32)
    tg = Trk()
    cx.dma("sync", gB, dram_bcast(D["norm_final"], 128, 1024), cx.fresh(), w=[tg])
    junk = ar.alloc([1024], BF16)
    tj = Trk()
    ss = ar.alloc([NT, 2], F32)
    tss = Trk()
    ob = [ar.alloc([1024], F32) for _ in range(2)]
    tob = [Trk(), Trk()]
    so = [cx.slot("o0"), cx.slot("o1")]
    for i in range(NT):
        txs = [k.txacc[i]] + (k.txh[i] if hasattr(k, "txh") else [])
        cx.op("scalar", lambda h, i=i: h.activation(junk, k.xacc[:, i, :], AF.Square, accum_out=ss[:, i, 0:1]), r=txs + [tss], w=[tj, tss])
    cx.op(V, lambda h: h.tensor_scalar(ss[:, :, 1:2], ss[:, :, 0:1], 1.0 / 1024, EPS, op0=ALU.mult, op1=ALU.add), r=[tss], w=[tss])
    cx.op("scalar", lambda h: h.activation(ss[:, :, 1:2], ss[:, :, 1:2], AF.Sqrt), r=[tss], w=[tss])
    cx.op(V, lambda h: h.reciprocal(ss[:, :, 1:2], ss[:, :, 1:2]), r=[tss], w=[tss])
    for i in range(NT):
        b = i % 2
        s1 = ss[:, i, 1:2]
        txs = [k.txacc[i]] + (k.txh[i] if hasattr(k, "txh") else [])
        cx.op(V, lambda h, i=i, s1=s1, b=b: h.scalar_tensor_tensor(out=ob[b], in0=k.xacc[:, i, :], scalar=s1, in1=gB, op0=ALU.mult, op1=ALU.mult), r=txs + [tss, tg], w=[tob[b]])
        cx.dma("sync", out[i * 128:(i + 1) * 128, :], ob[b], so[b], r=[tob[b]])
    cx.barrier()
    ar.release(m0)


_CACHE = {}


def kernel(**inputs):
    inp = {k_: np.asarray(v) for k_, v in inputs.items()}
    n = inp["x"].shape[0]
    maps = [host_inputs(inp, b) for b in range(n)]
    key = "full"
    if key not in _CACHE:
        shapes = {k_: (v.shape, np2dt(v)) for k_, v in maps[0].items()}
        _CACHE[key] = build(shapes)[0]
    nc = _CACHE[key]
    res = run_bass_kernel_spmd(nc, maps, core_ids=list(range(n)))
    return np.stack([np.asarray(r["out"], dtype=np.float32) for r in res.results], 0)
```

```python
import contextlib
import os
import math
import numpy as np
import ml_dtypes
import concourse.bass as bass
import concourse.mybir as mybir
from concourse.bass_utils import run_bass_kernel_spmd

F32 = mybir.dt.float32
BF16 = mybir.dt.bfloat16
F32R = mybir.dt.float32r
I32 = mybir.dt.int32
AF = mybir.ActivationFunctionType
ALU = mybir.AluOpType
AX = mybir.AxisListType

ENGS = ("sync", "scalar", "gpsimd", "vector", "tensor")
S = 2048
DM = 1024
NT = 16
EPS = 1e-6


class Trk:
    __slots__ = ("name", "w", "r", "excl")

    def __init__(self, name="", excl=False):
        self.name = name
        self.w = None
        self.r = {}
        self.excl = excl


class DmaSlot:
    def __init__(self, ctx, name):
        self.key = "d_" + name + str(ctx.nsem)
        ctx.sems[self.key] = ctx.new_sem(self.key)
        self.total = 0


class Ctx:
    def __init__(self, nc, stack):
        self.nc = nc
        self.stack = stack
        self.q = {e: [] for e in ENGS}
        self.sems = {}
        self.nsem = 0
        self.cnt = {e: 0 for e in ENGS}
        self.known = {e: {} for e in ENGS}
        for e in ENGS:
            self.sems[e] = self.new_sem("s_" + e)
        self.slots = []
        self.pools = {}
        self.pool_idx = {}
        self.n_ops = 0

    def new_sem(self, name):
        self.nsem += 1
        return self.stack.enter_context(self.nc.semaphore(name))

    def slot(self, name):
        s = DmaSlot(self, name)
        self.slots.append(s)
        return s

    def fresh(self, kind="hw"):
        pool = self.pools.setdefault(kind, [])
        i = self.pool_idx.get(kind, 0)
        if i >= len(pool):
            assert len(pool) < 30, "slot pool exhausted"
            pool.append(self.slot(kind + "%d" % len(pool)))
            pool[-1].kind = kind
        self.pool_idx[kind] = i + 1
        return pool[i]

    def sb(self, name, shape, dt):
        return self.stack.enter_context(self.nc.sbuf_tensor("sb_" + name, list(shape), dt))

    def ps(self, name, shape, dt=F32):
        return self.stack.enter_context(self.nc.psum_tensor(name, list(shape), dt))

    def _waits_for(self, eng, r, w, extra=()):
        need = {}

        def req(dep):
            if dep is None:
                return
            k, c = dep
            if k == eng and eng in ("tensor", "sync"):
                return
            if c > need.get(k, 0):
                need[k] = c
        for t in r:
            req(t.w)
        for t in w:
            req(t.w)
            for k, c in t.r.items():
                req((k, c))
        for d in extra:
            req(d)
        out = []
        kn = self.known[eng]
        for k, c in need.items():
            if kn.get(k, 0) < c:
                kn[k] = c
                out.append((self.sems[k], c))
        return out

    def op(self, eng, fn, r=(), w=(), inc=True, extra=()):
        w = list(w) + [t for t in r if t.excl]
        r = [t for t in r if not t.excl]
        waits = self._waits_for(eng, r, w, extra)
        c = self.cnt[eng] + 1
        if inc:
            self.cnt[eng] = c
        sem = self.sems[eng]

        def emit(h, fn=fn, waits=waits, inc=inc, sem=sem):
            for s, v in waits:
                h.wait_ge(s, v)
            ins = fn(h)
            if inc:
                ins.then_inc(sem, 1)
        self.q[eng].append(emit)
        for t in r:
            t.r[eng] = c
        for t in w:
            t.w = (eng, c)
            t.r = {}
        self.n_ops += 1

    def dma(self, eng, out, in_, slot, r=(), w=(), extra=(), **kw):
        kind = "sw" if eng == "gpsimd" else "hw"
        assert getattr(slot, "kind", kind) == kind, ("DMA slot kind mismatch", slot.key, eng)
        slot.kind = kind
        waits = self._waits_for(eng, r, w, extra)
        slot.total += 16
        sem = self.sems[slot.key]

        def emit(h, waits=waits, sem=sem, out=out, in_=in_, kw=kw):
            for s, v in waits:
                h.wait_ge(s, v)
            h.dma_start(out=out, in_=in_, **kw).then_inc(sem, 16)
        self.q[eng].append(emit)
        dep = (slot.key, slot.total)
        for t in r:
            t.r[slot.key] = slot.total
        for t in w:
            t.w = dep
            t.r = {}
        self.n_ops += 1
        return dep

    def wait_deps(self, eng, deps):
        waits = self._waits_for(eng, (), (), deps)

        def emit(h, waits=waits):
            for s, v in waits:
                h.wait_ge(s, v)
        self.q[eng].append(emit)

    def barrier(self):
        deps = [(e, self.cnt[e]) for e in ENGS if e != "sync" and self.cnt[e] > 0]
        deps += [(s.key, s.total) for s in self.slots if s.total > 0]
        for e in ENGS:
            self.wait_deps(e, deps)
        self.pool_idx = {}

    def emit_all(self, block):
        q = self.q

        @block.sync
        def _(h):
            for f in q["sync"]:
                f(h)

        @block.scalar
        def _(h):
            for f in q["scalar"]:
                f(h)

        @block.gpsimd
        def _(h):
            for f in q["gpsimd"]:
                f(h)

        @block.vector
        def _(h):
            for f in q["vector"]:
                f(h)

        @block.tensor
        def _(h):
            for f in q["tensor"]:
                f(h)


class Arena:
    def __init__(self, cx, words, base=None):
        self.t = cx.sb("arena", [128, words], F32) if base is None else base
        self.cx = cx
        self.words = words
        self.top = 0

    def mark(self):
        return self.top

    def release(self, m):
        if m != self.top:
            self.cx.barrier()
        self.top = m

    def alloc(self, shape, dt):
        n = int(np.prod(shape))
        w = n if dt in (F32, F32R, I32) else (n + 1) // 2
        w = (w + 1) // 2 * 2
        o = self.top
        self.top += w
        assert self.top <= self.words, ("arena overflow", self.top, self.words)
        v = self.t[:, o:o + w]
        if dt != F32:
            v = v.bitcast(dt)
        v = v[:, 0:n]
        if len(shape) > 1:
            names = " ".join("d%d" % i for i in range(len(shape)))
            v = v.rearrange("p (%s) -> p %s" % (names, names), **{"d%d" % i: shape[i] for i in range(len(shape))})
        return v


def pap(ap, part0, nparts, off, dims):
    base = ap.ap[0][0]
    return bass.AP(ap.tensor, ap.offset + part0 * base + off, [[base, nparts]] + [list(d) for d in dims])


def host_consts():
    c = {}
    c["ident"] = np.eye(128, dtype=np.float32)
    c["identb"] = np.eye(128, dtype=np.float32).astype(ml_dtypes.bfloat16)
    c["ones"] = np.ones((128, 128), np.float32)
    selT = np.zeros((128, 2, 8, 128), np.float32)
    selB = np.zeros((128, 2, 8, 128), np.float32)
    for q in range(4):
        for r in range(32):
            loc, cc = r // 16, r % 16
            for s in range(8):
                selT[q * 32 + r, loc, s, s * 16 + cc] = 1.0
                selB[q * 32 + r, loc, s, s * 16 + cc] = 1.0
    c["selT"] = selT.astype(ml_dtypes.bfloat16)
    c["selB"] = selB.astype(ml_dtypes.bfloat16)
    sidx = np.arange(128) // 16
    c["s5mf"] = (sidx[None, :] >= sidx[:, None]).astype(np.float32)
    c["s5mb"] = (sidx[None, :] <= sidx[:, None]).astype(np.float32)
    c["kvec"] = np.tile((np.arange(16, dtype=np.float32) - 7.0)[None, :], (128, 1))
    k = np.arange(128)[:, None]
    cc = np.arange(128)[None, :]
    same = (k // 64) == (cc // 64)
    gm = np.zeros((128, 8, 128), np.float32)
    gm[:, 0] = same & (k <= cc)
    gm[:, 1] = same & (k >= cc)
    gm[:, 2] = np.where(same & (cc >= k), 0.0, -30000.0)
    gm[:, 3] = np.where(same & (cc <= k), 0.0, -30000.0)
    gm[:, 4] = same & (cc > k)
    gm[:, 5] = same & (cc < k)
    gm[:, 6] = same
    c["gmask"] = gm
    return c


def host_s5(inp):
    o = {}

    pairs = {"lam_re": ("s5_lam_re_f", "s5_lam_re_b"), "lam_im": ("s5_lam_im_f", "s5_lam_im_b"),
             "log_step": ("s5_log_step_f", "s5_log_step_b"), "b_re": ("s5_b_re_f", "s5_b_re_b"),
             "b_im": ("s5_b_im_f", "s5_b_im_b"), "c_re": ("s5_c_re_f", "s5_c_re_b"), "c_im": ("s5_c_im_f", "s5_c_im_b")}

    def st(nm):
        f_, b_ = pairs[nm]
        return np.stack([inp[f_][0], inp[b_][0]], 0)
    lam = np.stack([st("lam_re"), st("lam_im")], 0)
    lam = lam.reshape(2, 2, 2, 16, 64).transpose(2, 4, 0, 1, 3)
    o["s5_lam"] = np.ascontiguousarray(lam.reshape(128, 2, 32))
    ls = st("log_step").reshape(2, 2, 16)
    ls = np.broadcast_to(ls.transpose(1, 0, 2)[:, None], (2, 64, 2, 16))
    o["s5_step"] = np.ascontiguousarray(ls.reshape(128, 32))
    b = np.stack([st("b_re"), st("b_im")], 0)
    b = b.reshape(2, 2, 2, 16, 64, 16).transpose(2, 4, 0, 1, 3, 5)
    o["s5_b"] = np.ascontiguousarray(b.reshape(128, 2, 512))
    cm = np.stack([st("c_re"), st("c_im")], 0)
    cm = cm.reshape(2, 2, 2, 16, 16, 64).transpose(2, 5, 0, 1, 3, 4)
    o["s5_c"] = np.ascontiguousarray(cm.reshape(128, 2, 512))
    d = inp["s5_d"][0].reshape(32, 16)
    o["s5_dvec"] = np.ascontiguousarray(np.broadcast_to(d.T[None], (8, 16, 32)).reshape(128, 32))
    o["s5_bglu"] = np.ascontiguousarray(inp["s5_b_glu"][0].reshape(4, 128).T)
    o["s5_normw"] = np.ascontiguousarray(inp["s5_norm"][0].reshape(4, 128).T)
    return o


def np2dt(a):
    if a.dtype == np.float32:
        return F32
    if a.dtype == ml_dtypes.bfloat16:
        return BF16
    raise ValueError(a.dtype)


class K:
    pass


def dram_bcast(ap, nparts, n, off=0):
    return bass.AP(ap.tensor, ap.offset + off, [[0, nparts], [1, n]])


def norm_transpose(k, name, src_fn, ntiles, gain_dram, outT, out_dt, outT_trk, resident=False):
    cx, ar = k.cx, k.ar
    m = ar.mark()
    gB = ar.alloc([1024], F32)
    tg = Trk()
    cx.dma("sync", gB, dram_bcast(gain_dram, 128, 1024), cx.fresh(), w=[tg])
    junk = ar.alloc([1024], BF16)
    tj = Trk()
    xn = [ar.alloc([1024], out_dt) for _ in range(2)]
    txn = [Trk(), Trk()]
    ss = ar.alloc([NT * 2, 1], F32)
    tss = [Trk() for _ in range(ntiles)]
    pdt = BF16 if out_dt == BF16 else F32
    ident = k.identb if out_dt == BF16 else k.ident
    srcs = []
    if resident:
        for i in range(ntiles):
            src, ts = src_fn(i)
            srcs.append((src, ts))
            cx.op("scalar", lambda h, src=src, i=i: h.activation(junk, src, AF.Square, accum_out=ss[:, 2 * i:2 * i + 1]), r=[ts], w=[tj, tss[0]])
        ssv = ss.rearrange("p (t two) one -> p t (two one)", two=2)
        cx.op("vector", lambda h: h.tensor_scalar(ssv[:, 0:ntiles, 1:2], ssv[:, 0:ntiles, 0:1], 1.0 / 1024, EPS, op0=ALU.mult, op1=ALU.add), r=[tss[0]], w=[tss[0]])
        cx.op("scalar", lambda h: h.activation(ssv[:, 0:ntiles, 1:2], ssv[:, 0:ntiles, 1:2], AF.Sqrt), r=[tss[0]], w=[tss[0]])
        cx.op("vector", lambda h: h.reciprocal(ssv[:, 0:ntiles, 1:2], ssv[:, 0:ntiles, 1:2]), r=[tss[0]], w=[tss[0]])
    for i in range(ntiles):
        rsi = ss[:, 2 * i + 1:2 * i + 2]
        if resident:
            src, ts = srcs[i]
            tsi = tss[0]
        else:
            src, ts = src_fn(i)
            ssi = ss[:, 2 * i:2 * i + 1]
            tsi = tss[i]
            cx.op("scalar", lambda h, src=src, ssi=ssi: h.activation(junk, src, AF.Square, accum_out=ssi), r=[ts], w=[tj, tss[i]])
            cx.op("vector", lambda h, ssi=ssi, rsi=rsi: h.tensor_scalar(rsi, ssi, 1.0 / 1024, EPS, op0=ALU.mult, op1=ALU.add), r=[tss[i]], w=[tss[i]])
            cx.op("scalar", lambda h, rsi=rsi: h.activation(rsi, rsi, AF.Sqrt), r=[tss[i]], w=[tss[i]])
            cx.op("vector", lambda h, rsi=rsi: h.reciprocal(rsi, rsi), r=[tss[i]], w=[tss[i]])
        b = i % 2
        cx.op("vector", lambda h, src=src, rsi=rsi, b=b: h.scalar_tensor_tensor(out=xn[b], in0=src, scalar=rsi, in1=gB, op0=ALU.mult, op1=ALU.mult),
              r=[ts, tsi, tg], w=[txn[b]])
        if out_dt == BF16:
            pb = k.psum[i % 2]
            tp = k.tpsum[i % 2]
            pv = pb.bitcast(BF16)
            for c in range(8):
                cx.op("tensor", lambda h, b=b, c=c, pv=pv: h.transpose(pv[:, c * 128:(c + 1) * 128], xn[b][:, c * 128:(c + 1) * 128], ident),
                      r=[txn[b]], w=[tp], inc=(c == 7))
            dst = outT[:, :, i * 128:(i + 1) * 128]
            eng = "scalar" if i % 2 == 0 else "vector"
            if eng == "scalar":
                cx.op(eng, lambda h, dst=dst, pv=pv: h.copy(dst, pv.rearrange("p (c t) -> p c t", c=8)), r=[tp], w=[outT_trk])
            else:
                cx.op(eng, lambda h, dst=dst, pv=pv: h.tensor_copy(dst, pv.rearrange("p (c t) -> p c t", c=8)), r=[tp], w=[outT_trk])
        else:
            for half in range(2):
                pb = k.psum[(2 * i + half) % 4]
                tp = k.tpsum[(2 * i + half) % 4]
                for c4 in range(4):
                    c = half * 4 + c4
                    cx.op("tensor", lambda h, b=b, c=c, c4=c4, pb=pb: h.transpose(pb[:, c4 * 128:(c4 + 1) * 128], xn[b][:, c * 128:(c + 1) * 128].bitcast(F32), ident),
                          r=[txn[b]], w=[tp], inc=(c4 == 3))
                dst = outT[:, half * 4:(half + 1) * 4, i * 128:(i + 1) * 128]
                if half == 0:
                    cx.op("scalar", lambda h, dst=dst, pb=pb: h.copy(dst, pb.rearrange("p (c t) -> p c t", c=4)), r=[tp], w=[outT_trk])
                else:
                    cx.op("vector", lambda h, dst=dst, pb=pb: h.tensor_copy(dst, pb.rearrange("p (c t) -> p c t", c=4)), r=[tp], w=[outT_trk])
    ar.release(m)


def s5_prep(k):
    cx, ar, D = k.cx, k.ar, k.D
    V = "vector"
    m0 = ar.mark()
    lam = ar.alloc([2, 32], F32)
    step = ar.alloc([32], F32)
    bb = ar.alloc([2, 512], F32)
    cc = ar.alloc([2, 512], F32)
    kvec = ar.alloc([16], F32)
    tl = Trk()
    sl_ = cx.fresh()
    for dst, nm in ((lam, "s5_lam"), (step, "s5_step"), (bb, "s5_b"), (cc, "s5_c"), (kvec, "kvec")):
        cx.dma("sync", dst, D[nm], sl_, w=[tl])
    T = Trk()

    def vop(fn, extra_r=()):
        cx.op(V, fn, r=[T, tl] + list(extra_r), w=[T])

    def aop(fn):
        cx.op("scalar", fn, r=[T, tl], w=[T])
    lre, lim = lam[:, 0, :], lam[:, 1, :]
    dl = ar.alloc([32], F32)
    re1 = ar.alloc([32], F32)
    im1 = ar.alloc([32], F32)
    aop(lambda h: h.activation(dl, step, AF.Exp))
    vop(lambda h: h.tensor_tensor(out=re1, in0=dl, in1=lre, op=ALU.mult))
    vop(lambda h: h.tensor_tensor(out=im1, in0=dl, in1=lim, op=ALU.mult))
    PWI = ar.alloc([16, 32], F32)
    PWR = ar.alloc([16, 32], F32)
    m_pw = ar.mark()
    KR = ar.alloc([16, 32], F32)
    KI = ar.alloc([16, 32], F32)
    kv_b = kvec.unsqueeze(2).to_broadcast([128, 16, 32])
    vop(lambda h: h.tensor_tensor(out=KR, in0=kv_b, in1=re1.unsqueeze(1).to_broadcast([128, 16, 32]), op=ALU.mult))
    vop(lambda h: h.tensor_tensor(out=KI, in0=kv_b, in1=im1.unsqueeze(1).to_broadcast([128, 16, 32]), op=ALU.mult))
    MAG = ar.alloc([16, 32], F32)
    aop(lambda h: h.activation(MAG, KR, AF.Exp))
    YI = ar.alloc([16, 32], I32)
    YF = ar.alloc([16, 32], F32)
    vop(lambda h: h.tensor_scalar(KI, KI, 1.0 / (2 * math.pi), None, op0=ALU.mult))
    vop(lambda h: h.tensor_copy(YI, KI))
    vop(lambda h: h.tensor_copy(YF, YI))
    vop(lambda h: h.tensor_tensor(out=KI, in0=KI, in1=YF, op=ALU.subtract))
    SH_ = ar.alloc([16, 32], F32)
    SQ_ = ar.alloc([16, 32], F32)
    aop(lambda h: h.activation(SH_, KI, AF.Sin, scale=math.pi))
    aop(lambda h: h.activation(SQ_, KI, AF.Sin, scale=math.pi / 2))
    CH_ = ar.alloc([16, 32], F32)
    vop(lambda h: h.tensor_tensor(out=CH_, in0=SQ_, in1=SQ_, op=ALU.mult))
    vop(lambda h: h.tensor_scalar(CH_, CH_, -2.0, 1.0, op0=ALU.mult, op1=ALU.add))
    vop(lambda h: h.tensor_tensor(out=PWI, in0=SH_, in1=CH_, op=ALU.mult))
    vop(lambda h: h.scalar_tensor_tensor(out=PWI, in0=PWI, scalar=2.0, in1=MAG, op0=ALU.mult, op1=ALU.mult))
    vop(lambda h: h.tensor_tensor(out=PWR, in0=SH_, in1=SH_, op=ALU.mult))
    vop(lambda h: h.tensor_scalar(PWR, PWR, -2.0, 1.0, op0=ALU.mult, op1=ALU.add))
    vop(lambda h: h.tensor_tensor(out=PWR, in0=PWR, in1=MAG, op=ALU.mult))
    ar.release(m_pw)
    lrm1 = ar.alloc([32], F32)
    li = PWI[:, 8, :]
    t1 = ar.alloc([32], F32)
    t2 = ar.alloc([32], F32)
    den = ar.alloc([32], F32)
    c0r = ar.alloc([32], F32)
    c0i = ar.alloc([32], F32)
    vop(lambda h: h.tensor_scalar(lrm1, PWR[:, 8, :], -1.0, None, op0=ALU.add))
    vop(lambda h: h.tensor_tensor(out=t1, in0=lre, in1=lre, op=ALU.mult))
    vop(lambda h: h.tensor_tensor(out=t2, in0=lim, in1=lim, op=ALU.mult))
    vop(lambda h: h.tensor_tensor(out=den, in0=t1, in1=t2, op=ALU.add))
    vop(lambda h: h.reciprocal(den, den))
    vop(lambda h: h.tensor_tensor(out=t1, in0=lrm1, in1=lre, op=ALU.mult))
    vop(lambda h: h.tensor_tensor(out=t2, in0=li, in1=lim, op=ALU.mult))
    vop(lambda h: h.tensor_tensor(out=t1, in0=t1, in1=t2, op=ALU.add))
    vop(lambda h: h.tensor_tensor(out=c0r, in0=t1, in1=den, op=ALU.mult))
    vop(lambda h: h.tensor_tensor(out=t1, in0=li, in1=lre, op=ALU.mult))
    vop(lambda h: h.tensor_tensor(out=t2, in0=lrm1, in1=lim, op=ALU.mult))
    vop(lambda h: h.tensor_tensor(out=t1, in0=t1, in1=t2, op=ALU.subtract))
    vop(lambda h: h.tensor_tensor(out=c0i, in0=t1, in1=den, op=ALU.mult))
    BBR = ar.alloc([32, 16], F32)
    BBI = ar.alloc([32, 16], F32)
    TA = ar.alloc([32, 16], F32)
    br = bb[:, 0, :].rearrange("p (a c) -> p a c", c=16)
    bi = bb[:, 1, :].rearrange("p (a c) -> p a c", c=16)
    c0r_b = c0r.unsqueeze(2).to_broadcast([128, 32, 16])
    c0i_b = c0i.unsqueeze(2).to_broadcast([128, 32, 16])
    vop(lambda h: h.tensor_tensor(out=BBR, in0=br, in1=c0r_b, op=ALU.mult))
    vop(lambda h: h.tensor_tensor(out=TA, in0=bi, in1=c0i_b, op=ALU.mult))
    vop(lambda h: h.tensor_tensor(out=BBR, in0=BBR, in1=TA, op=ALU.subtract))
    vop(lambda h: h.tensor_tensor(out=BBI, in0=bi, in1=c0r_b, op=ALU.mult))
    vop(lambda h: h.tensor_tensor(out=TA, in0=br, in1=c0i_b, op=ALU.mult))
    vop(lambda h: h.tensor_tensor(out=BBI, in0=BBI, in1=TA, op=ALU.add))
    ASd = ar.alloc([16, 2, 8, 16], F32)
    CS2d = ar.alloc([16, 2, 8, 16], F32)
    T1 = ar.alloc([8, 16, 16], F32)
    T2 = ar.alloc([8, 16, 16], F32)
    cr = cc[:, 0, :].rearrange("p (d a c) -> p d a c", d=2, c=16)
    ci = cc[:, 1, :].rearrange("p (d a c) -> p d a c", d=2, c=16)
    BBR4 = BBR.rearrange("p (d a) c -> p d a c", d=2)
    BBI4 = BBI.rearrange("p (d a) c -> p d a c", d=2)

    def pw(arr, d, k0, kstep):
        return pap(arr, 0, 128, k0 * 32 + d * 16, [[kstep * 32, 8], [1, 16], [0, 16]])

    def dst(arr, dofs, ri):
        return pap(arr, 0, 128, dofs * 4096 + ri * 128, [[16, 8], [256, 16], [1, 16]])

    def vec(v4, d):
        a_ = v4[:, d]
        return bass.AP(a_.tensor, a_.offset, [list(a_.ap[0]), [0, 8], list(a_.ap[1]), list(a_.ap[2])])

    T1f = T1.rearrange("p a b c -> p (a b c)")

    def cmul(out_arr, dofs, d, k0, kstep, vr, vi, neg_im):
        pr, pi_ = pw(PWR, d, k0, kstep), pw(PWI, d, k0, kstep)
        vop(lambda h: h.tensor_tensor(out=T1, in0=pr, in1=vec(vr, d), op=ALU.mult))
        vop(lambda h: h.tensor_tensor(out=T2, in0=pi_, in1=vec(vi, d), op=ALU.mult))
        vop(lambda h: h.tensor_tensor(out=dst(out_arr, dofs, 0), in0=T1, in1=T2, op=ALU.subtract))
        vop(lambda h: h.tensor_tensor(out=T1, in0=pr, in1=vec(vi, d), op=ALU.mult))
        vop(lambda h: h.tensor_tensor(out=T2, in0=pi_, in1=vec(vr, d), op=ALU.mult))
        if neg_im:
            vop(lambda h: h.tensor_scalar(T1f, T1f, -1.0, None, op0=ALU.mult))
            vop(lambda h: h.tensor_tensor(out=dst(out_arr, dofs, 1), in0=T1, in1=T2, op=ALU.subtract))
        else:
            vop(lambda h: h.tensor_tensor(out=dst(out_arr, dofs, 1), in0=T1, in1=T2, op=ALU.add))
    vop(lambda h: h.tensor_copy(k.s5A1[:, 0:32], PWR[:, 15, :]))
    vop(lambda h: h.tensor_copy(k.s5A1[:, 32:64], PWR[:, 15, :]))
    vop(lambda h: h.tensor_scalar(k.s5A2[:, 0:32], PWI[:, 15, :], -1.0, None, op0=ALU.mult))
    vop(lambda h: h.tensor_copy(k.s5A2[:, 32:64], PWI[:, 15, :]))
    cmul(k.s5CS, 0, 0, 8, 1, cr, ci, True)
    cmul(k.s5CS, 1, 1, 15, -1, cr, ci, True)
    k.t_s5w = T
    mf = ar.alloc([2, 128], F32)
    dv = ar.alloc([32], F32)
    tm = Trk()
    sl_ = cx.fresh()
    cx.dma("sync", mf[:, 0, :], D["s5mf"], sl_, w=[tm])
    cx.dma("sync", mf[:, 1, :], D["s5mb"], sl_, w=[tm])
    cx.dma("sync", dv, D["s5_dvec"], sl_, w=[tm])
    tt1 = [ar.alloc([128], F32) for _ in range(2)]
    ttt = [Trk(), Trk()]
    ASb = ASd.rearrange("p a r s c -> p (a r) (s c)")
    ASm = ASd.rearrange("p a r s c -> p a r (s c)")
    CSm = CS2d.rearrange("p a r s c -> p a r (s c)")
    for d in range(2):
        if d == 0:
            cmul(ASd, 0, 0, 14, -1, BBR4, BBI4, False)
            cmul(CS2d, 0, 0, 0, 1, cr, ci, True)
        else:
            cmul(ASd, 0, 1, 7, 1, BBR4, BBI4, False)
            cmul(CS2d, 0, 1, 7, -1, cr, ci, True)
        for grp in range(8):
            pb = k.psum[grp % 4]
            tp = k.tpsum[grp % 4]
            for j in range(4):
                blk = grp * 4 + j
                cx.op("tensor", lambda h, pb=pb, j=j, blk=blk: h.transpose(pb[:, j * 128:(j + 1) * 128], ASb[:, blk, :], k.ident),
                      r=[T], w=[tp], inc=(j == 3))
            dstv = k.s5AT[:, d * 32 + grp * 4:d * 32 + (grp + 1) * 4, :]
            if grp % 2 == 0:
                cx.op("scalar", lambda h, dstv=dstv, pb=pb: h.copy(dstv, pb.rearrange("p (j x) -> p j x", j=4)), r=[tp], w=[k.t_s5at])
            else:
                cx.op("vector", lambda h, dstv=dstv, pb=pb: h.tensor_copy(dstv, pb.rearrange("p (j x) -> p j x", j=4)), r=[tp], w=[k.t_s5at])
        for g in range(32):
            gh, gl = g // 16, g % 16
            pb = k.psum[4 + g % 4]
            tp = k.tpsum[4 + g % 4]
            for ri in range(2):
                cx.op("tensor", lambda h, pb=pb, ri=ri, gh=gh, gl=gl: h.matmul(
                    pb[:, 0:128], ASm[gh * 64:(gh + 1) * 64, gl, ri, :], CSm[gh * 64:(gh + 1) * 64, gl, ri, :],
                    start=(ri == 0), stop=(ri == 1)), r=[T], w=[tp], inc=(ri == 1))
            b_ = g % 2
            cx.op(V, lambda h, pb=pb, b_=b_, d=d: h.tensor_tensor(out=tt1[b_], in0=pb[:, 0:128], in1=mf[:, d, :], op=ALU.mult), r=[tp, tm], w=[ttt[b_]])
            if d == 0:
                cx.op(V, lambda h, b_=b_, g=g: h.scalar_tensor_tensor(out=k.s5TT[:, g, :], in0=k.ident, scalar=dv[:, g:g + 1], in1=tt1[b_], op0=ALU.mult, op1=ALU.add),
                      r=[ttt[b_], tm], w=[k.t_s5tt])
            else:
                cx.op(V, lambda h, b_=b_, g=g: h.tensor_tensor(out=k.s5TT[:, g, :], in0=k.s5TT[:, g, :], in1=tt1[b_], op=ALU.add),
                      r=[ttt[b_]], w=[k.t_s5tt])
    cx.barrier()
    ar.release(m0)


def load_w_cols(k, wdram, col0, ncols, dst, trk, slot, eng="gpsimd"):
    src = bass.AP(wdram.tensor, wdram.offset + col0, [[wdram.ap[0][0] * 1, 128], [wdram.ap[0][0] * 128, 8], [1, ncols]])
    return k.cx.dma(eng, dst, src, slot, w=[trk])


def proj_fm(k, wt, wtrk, consume):
    cx = k.cx
    for n in range(4):
        pb = k.psum[n % 2 + 2]
        tp = k.tpsum[n % 2 + 2]
        for c in range(8):
            cx.op("tensor", lambda h, pb=pb, c=c, n=n: h.matmul(pb[:, :], wt[:, c, :], k.hT[:, c, n * 512:(n + 1) * 512], start=(c == 0), stop=(c == 7)),
                  r=[wtrk, k.t_hT], w=[tp], inc=(c == 7))
        consume(n, pb, tp)


def s5_build_U(k):
    cx, ar = k.cx, k.ar
    m0 = ar.mark()
    wt = [ar.alloc([8, 128], BF16) for _ in range(2)]
    twt = [Trk(), Trk()]
    swt = [cx.fresh('sw'), cx.fresh('sw')]
    uT = [ar.alloc([2048], BF16) for _ in range(2)]
    tuT = [Trk(), Trk()]
    for ct in range(4):
        b = ct % 2
        load_w_cols(k, k.D["w_in"], ct * 128, 128, wt[b], twt[b], swt[b])

        def consume(n, pb, tp, b=b):
            if n % 2 == 0:
                cx.op("scalar", lambda h: h.copy(uT[b][:, n * 512:(n + 1) * 512], pb[:, :]), r=[tp], w=[tuT[b]])
            else:
                cx.op("vector", lambda h: h.tensor_copy(uT[b][:, n * 512:(n + 1) * 512], pb[:, :]), r=[tp], w=[tuT[b]])
        proj_fm(k, wt[b], twt[b], consume)
        for gi in range(8):
            g = ct * 8 + gi
            q0 = 32 * (gi // 2)
            pb = k.psum[4 + gi % 4]
            tp = k.tpsum[4 + gi % 4]
            for s in range(8):
                rhs = pap(uT[b], q0, 32, s, [[8, 256]])
                cx.op("tensor", lambda h, pb=pb, s=s, rhs=rhs, q0=q0, gi=gi: h.matmul(pb[:, 0:256], k.selT[q0:q0 + 32, gi % 2, s, :], rhs, start=(s == 0), stop=(s == 7), tile_position=(q0, 0)),
                      r=[tuT[b]], w=[tp], inc=(s == 7))
            if gi % 2 == 0:
                cx.op("scalar", lambda h, pb=pb, g=g: h.copy(k.s5U[:, g, :], pb[:, 0:256]), r=[tp], w=[k.t_s5U])
            else:
                cx.op("vector", lambda h, pb=pb, g=g: h.tensor_copy(k.s5U[:, g, :], pb[:, 0:256]), r=[tp], w=[k.t_s5U])
    cx.barrier()
    ar.release(m0)


def s5_main(k, yT, t_yT):
    cx, ar, D = k.cx, k.ar, k.D
    V = "vector"
    m0 = ar.mark()
    SH = ar.alloc([2, 257, 2, 16], BF16)
    tSH = Trk()
    tSHh = Trk()
    X = [ar.alloc([64], F32) for _ in range(3)]
    tX = [Trk() for _ in range(3)]
    t1 = ar.alloc([64], F32)
    t2 = ar.alloc([64], F32)
    tt = Trk()
    tt2 = Trk()
    cx.op("gpsimd", lambda h: h.memset(SH[:, 0, 0, :, :], 0.0), w=[tSH])
    cx.op("gpsimd", lambda h: h.memset(SH[:, 1, 256, :, :], 0.0), w=[tSH])
    cx.op("gpsimd", lambda h: h.memset(X[0], 0.0), w=[tX[0]])
    n = 0
    for gl in range(16):
        for d in range(2):
            for ri in range(2):
                blk = d * 32 + gl * 2 + ri
                pb = k.psum[n % 4]
                tp = k.tpsum[n % 4]
                cx.op("tensor", lambda h, pb=pb, blk=blk, gl=gl: h.matmul(pb[0:64, 0:256], k.s5AT[:, blk, 0:64], k.s5U[:, gl, :], start=True, stop=True),
                      r=[k.t_s5at, k.t_s5U], w=[tp], inc=False)
                cx.op("tensor", lambda h, pb=pb, blk=blk, gl=gl: h.matmul(pb[64:128, 0:256], k.s5AT[:, blk, 64:128], k.s5U[:, 16 + gl, :], start=True, stop=True),
                      r=[k.t_s5at, k.t_s5U], w=[tp])
                slot0 = 1 if d == 0 else 0
                dstv = pap(SH, 0, 128, d * 257 * 32 + slot0 * 32 + ri * 16 + gl, [[32, 256]])
                if n % 2 == 0:
                    cx.op("scalar", lambda h, dstv=dstv, pb=pb: h.copy(dstv, pb[:, 0:256]), r=[tp], w=[tSH])
                else:
                    cx.op(V, lambda h, dstv=dstv, pb=pb: h.tensor_copy(dstv, pb[:, 0:256]), r=[tp], w=[tSH])
                n += 1
    import os
    S5STOP = os.environ.get('S5_STOP', '')
    if S5STOP == 'a':
        cx.barrier(); ar.release(m0); return
    for i in range(256):
        xp, xn = X[i % 3], X[(i + 1) % 3]
        txp, txn = tX[i % 3], tX[(i + 1) % 3]
        xsw = pap(xp, 0, 128, 32, [[-32, 2], [1, 32]])
        bf = (i + 1) * 32
        bb_ = 257 * 32 + (255 - i) * 32
        sview = pap(SH, 0, 128, bf, [[16, 2], [bb_ - bf, 2], [1, 16]])
        xp3 = xp.rearrange("p (r x) -> p r x", r=2)
        cx.op("gpsimd", lambda h, xsw=xsw: h.tensor_tensor(out=t2.rearrange("p (r x) -> p r x", r=2), in0=k.s5A2.rearrange("p (r x) -> p r x", r=2), in1=xsw, op=ALU.mult), r=[txp, k.t_s5w], w=[tt2])
        cx.op(V, lambda h, xp=xp: h.tensor_tensor(out=t1, in0=k.s5A1, in1=xp, op=ALU.mult), r=[txp, k.t_s5w], w=[tt])
        cx.op(V, lambda h, sview=sview: h.tensor_tensor(out=t1.rearrange("p (r d x) -> p r d x", r=2, d=2), in0=t1.rearrange("p (r d x) -> p r d x", r=2, d=2), in1=sview, op=ALU.add), r=[tt, tSH], w=[tt])
        cx.op(V, lambda h, xn=xn: h.tensor_tensor(out=xn, in0=t1, in1=t2, op=ALU.add), r=[tt, tt2], w=[txn])
        cx.op("scalar", lambda h, xn=xn, sview=sview: h.copy(sview, xn.rearrange("p (r d x) -> p r d x", r=2, d=2)), r=[txn], w=[tSHh])
    if S5STOP == 'rec':
        cx.barrier(); ar.release(m0); return
    gT = ar.alloc([4, 2048], F32)
    gTb = ar.alloc([4, 2048], BF16)
    tgT = [Trk() for _ in range(4)]
    tgTb = [Trk() for _ in range(4)]
    ybuf = [k.arA.alloc([8, 256], BF16) for _ in range(2)]
    tyb = [Trk(), Trk()]
    for ct in range(4):
        b = ct % 2
        for gi in range(8):
            g = ct * 8 + gi
            gh, gl = g // 16, g % 16
            pb = k.psum[gi % 2]
            tp = k.tpsum[gi % 2]
            cx.op("tensor", lambda h, pb=pb, g=g: h.matmul(pb[:, 0:256], k.s5TT[:, g, :], k.s5U[:, g, :], start=True, stop=False),
                  r=[k.t_s5tt, k.t_s5U], w=[tp], inc=False)
            for d in range(2):
                for ri in range(2):
                    slot0 = 0 if d == 0 else 1
                    rhs = pap(SH, gh * 64, 64, d * 257 * 32 + slot0 * 32 + ri * 16 + gl, [[32, 256]])
                    last = (d == 1 and ri == 1)
                    cx.op("tensor", lambda h, pb=pb, rhs=rhs, d=d, ri=ri, gh=gh, gl=gl, last=last: h.matmul(
                        pb[:, 0:256], k.s5CS[gh * 64:(gh + 1) * 64, d, gl, ri, :], rhs, start=False, stop=last),
                        r=[tSH, tSHh, k.t_s5w], w=[tp], inc=last)
            if gi % 2 == 0:
                cx.op("scalar", lambda h, pb=pb, b=b, gi=gi: h.copy(ybuf[b][:, gi, :], pb[:, 0:256]), r=[tp], w=[tyb[b]])
            else:
                cx.op(V, lambda h, pb=pb, b=b, gi=gi: h.tensor_copy(ybuf[b][:, gi, :], pb[:, 0:256]), r=[tp], w=[tyb[b]])
        for t in range(8):
            q0 = 32 * (t // 2)
            pb = k.psum[2 + t % 4]
            tp = k.tpsum[2 + t % 4]
            for gi in range(8):
                cx.op("tensor", lambda h, pb=pb, t=t, gi=gi, q0=q0, b=b: h.matmul(pb[:, 0:256], k.selT[q0:q0 + 32, t % 2, gi, :], ybuf[b][q0:q0 + 32, gi, :], start=(gi == 0), stop=(gi == 7), tile_position=(q0, 0)),
                      r=[tyb[b]], w=[tp], inc=(gi == 7))
            dstv = pap(gT, 0, 128, ct * 2048 + t, [[8, 256]])
            cx.op("scalar", lambda h, pb=pb, dstv=dstv: h.activation(dstv, pb[:, 0:256], AF.Gelu), r=[tp], w=[tgT[ct]])
        cx.op("vector", lambda h, ct=ct: h.tensor_copy(gTb[:, ct, :], gT[:, ct, :]), r=[tgT[ct]], w=[tgTb[ct]])
    k.dbg_add("s5_g", gT, tgT)
    if S5STOP == 'c':
        cx.barrier(); ar.release(m0); return
    wg = ar.alloc([4, 512], BF16)
    twg = Trk()
    wsrc = D["s5_w_glu"]
    cx.dma("gpsimd", wg, bass.AP(wsrc.tensor, wsrc.offset, [[512, 128], [512 * 128, 4], [1, 512]]), cx.fresh('sw'), w=[twg])
    bgl = ar.alloc([4], F32)
    nw = ar.alloc([4], F32)
    tb = Trk()
    sl_ = cx.fresh()
    cx.dma("sync", bgl, D["s5_bglu"], sl_, w=[tb])
    cx.dma("sync", nw, D["s5_normw"], sl_, w=[tb])
    sig = ar.alloc([4, 512], BF16)
    tsig = Trk()
    sq = ar.alloc([4, 512], BF16)
    tsq = Trk()
    rs = ar.alloc([512], F32)
    trs = Trk()
    for nck in range(4):
        ts = slice(nck * 512, (nck + 1) * 512)
        for co in range(4):
            pb = k.psum[co % 2]
            tp = k.tpsum[co % 2]
            for ci in range(4):
                cx.op("tensor", lambda h, pb=pb, co=co, ci=ci, ts=ts: h.matmul(pb[:, :], wg[:, ci, co * 128:(co + 1) * 128], gTb[:, ci, ts], start=(ci == 0), stop=(ci == 3)),
                      r=[twg] + tgTb, w=[tp], inc=(ci == 3))
            cx.op("scalar", lambda h, pb=pb, co=co: h.activation(sig[:, co, :], pb[:, :], AF.Sigmoid, bias=bgl[:, co:co + 1]), r=[tp, tb], w=[tsig])
        for co in range(4):
            cx.op(V, lambda h, co=co, ts=ts: h.tensor_tensor(out=gT[:, co, ts], in0=gT[:, co, ts], in1=sig[:, co, :], op=ALU.mult), r=[tsig, tgT[co]], w=[tgT[co]])
            cx.op("scalar", lambda h, co=co, ts=ts: h.activation(sq[:, co, :], gT[:, co, ts], AF.Square), r=[tgT[co]], w=[tsq])
        pb = k.psum[2 + nck % 2]
        tp = k.tpsum[2 + nck % 2]
        for co in range(4):
            cx.op("tensor", lambda h, pb=pb, co=co: h.matmul(pb[:, :], k.onesb, sq[:, co, :], start=(co == 0), stop=(co == 3)), r=[tsq], w=[tp], inc=(co == 3))
        cx.op("scalar", lambda h, pb=pb: h.activation(rs, pb[:, :], AF.Sqrt, scale=1.0 / 512, bias=k.epsc), r=[tp], w=[trs])
        cx.op(V, lambda h: h.reciprocal(rs, rs), r=[trs], w=[trs])
        for co in range(4):
            cx.op(V, lambda h, co=co, ts=ts: h.scalar_tensor_tensor(out=yT[:, co, ts], in0=gT[:, co, ts], scalar=nw[:, co:co + 1], in1=rs, op0=ALU.mult, op1=ALU.mult),
                  r=[tgT[co], trs, tb, tsq], w=[t_yT])
    k.dbg_add("s5_gl", gT, tgT)
    cx.barrier()
    ar.release(m0)


def build(in_shapes, stage="full", dbg_names=(), n_heads=4, n_experts=32):
    nc = bass.Bass("TRN2", target_bir_lowering=False)
    k = K()
    k.n_heads = n_heads
    k.n_experts = n_experts
    k.nc = nc
    D = {}
    for nm, (shape, dt) in in_shapes.items():
        D[nm] = nc.dram_tensor(nm, list(shape), dt, kind="ExternalInput").ap()
    k.D = D
    out = nc.dram_tensor("out", [S, DM], F32, kind="ExternalOutput").ap()
    k.dbg = {}
    k.dbg_req = set(dbg_names)

    with contextlib.ExitStack() as st:
        cx = Ctx(nc, st)
        k.cx = cx

        def finish():
            deps = [(s_.key, s_.total) for s_ in cx.slots if s_.total > 0]
            cx.wait_deps("sync", deps + [(e, cx.cnt[e]) for e in ENGS if e != "sync" and cx.cnt[e] > 0])
            with nc.Block() as block:
                cx.emit_all(block)
            k.n_ops = cx.n_ops
            return nc, k
        k.slot_c = cx.slot("c")
        k.slot_w = cx.slot("w")
        k.slot_x = [cx.slot("x0"), cx.slot("x1")]
        k.slot_o = cx.slot("o")
        k.psum = [cx.ps("ps%d" % i, [128, 512], F32) for i in range(8)]
        k.psum = [p[:, :] for p in k.psum]
        k.tpsum = [Trk("ps%d" % i, excl=True) for i in range(8)]
        k.ident = cx.sb("ident", [128, 128], F32)[:, :]
        k.identb = cx.sb("identb", [128, 128], BF16)[:, :]
        k.ones = cx.sb("ones", [128, 128], F32)[:, :]
        k.onesb = cx.sb("onesb", [128, 128], BF16)[:, :]
        k.epsc = cx.sb("epsc", [128, 1], F32)[:, :]
        k.selT = cx.sb("selT", [128, 2, 8, 128], BF16)[:, :, :, :]
        tc = Trk()
        cx.dma("sync", k.ident, D["ident"], k.slot_c, w=[tc])
        cx.dma("sync", k.identb, D["identb"], k.slot_c, w=[tc])
        cx.dma("sync", k.ones, D["ones"], k.slot_c, w=[tc])
        cx.dma("gpsimd", k.onesb, D["ones"], cx.fresh("sw"), w=[tc])
        cx.op("vector", lambda h: h.memset(k.epsc, EPS), w=[tc])
        cx.dma("sync", k.selT, D["selT"], k.slot_c, w=[tc])
        k.s5A1 = cx.sb("s5A1", [128, 64], F32)[:, :]
        k.s5A2 = cx.sb("s5A2", [128, 64], F32)[:, :]
        ar = Arena(cx, 51456)
        k.ar = ar
        cx.barrier()

        def dbg_add(name, ap, trks):
            if name in k.dbg_req:
                shape = list(ap.shape)
                dt_ = F32
                o = nc.dram_tensor("dbg_" + name, shape, dt_, kind="ExternalOutput").ap()
                cx.dma("gpsimd" if ap.dtype != F32 else "sync", o, ap, cx.fresh("sw" if ap.dtype != F32 else "hw"), r=list(trks))
        k.dbg_add = dbg_add

        regA = ar.alloc([NT * 1024], F32)
        arA = Arena(cx, NT * 1024, base=regA)
        k.arA = arA
        yT = ar.alloc([8, 2048], BF16)
        t_yT = Trk()
        k.s5U = arA.alloc([32, 256], BF16)
        k.t_s5U = Trk()
        m_h = arA.mark()
        k.hT = arA.alloc([8, 2048], BF16)
        k.t_hT = Trk()

        m1 = ar.mark()
        xt = [ar.alloc([1024], F32) for _ in range(2)]
        txt = [Trk(), Trk()]

        def src_x(i):
            b = i % 2
            cx.dma("sync", xt[b], D["x"][i * 128:(i + 1) * 128, :], k.slot_x[b], w=[txt[b]])
            return xt[b], txt[b]
        norm_transpose(k, "mix", src_x, NT, D["norm_mix"], k.hT, BF16, k.t_hT)
        cx.barrier()
        ar.release(m1)

        if stage == 'p1':
            return finish()
        s5_build_U(k)
        if stage == 'U':
            return finish()
        mg = ar.mark()
        gdn_setup(k)
        for hd in range(k.n_heads):
            gdn_head(k, hd, yT, t_yT)
        ar.release(mg)
        k.dbg_add("ygdnT", yT[:, 4:8, :], [t_yT])
        if stage == 'gdn':
            return finish()
        cx.barrier()
        arA.release(m_h)
        k.s5AT = arA.alloc([64, 128], BF16)
        k.t_s5at = Trk()
        k.s5CS = arA.alloc([2, 16, 2, 128], BF16)
        k.s5TT = arA.alloc([32, 128], BF16)
        k.t_s5tt = Trk()
        s5_prep(k)
        if stage == 's5prep':
            return finish()
        s5_main(k, yT[:, 0:4, :], t_yT)
        k.dbg_add("ys5T", yT[:, 0:4, :], [t_yT])
        if stage == "s5":
            return finish()
        if True:
            cx.barrier()
            k.xacc = regA.rearrange('p (a b) -> p a b', a=NT)
            k.txacc = [Trk() for _ in range(NT)]
            out_proj(k, yT, t_yT)
            k.dbg_add("x1", k.xacc, k.txacc)
            if stage == 'oproj':
                return finish()
            xattn(k)
            if stage == 'xattn':
                return finish()
            k.dbg_add("x2", k.xacc, k.txacc)
            moe(k)
            k.dbg_add("x3", k.xacc, k.txacc + [t_ for p_ in k.txh for t_ in p_])
            final_norm(k, out)

        return finish()


def host_inputs(inp, b):
    m = {}
    m["x"] = np.ascontiguousarray(inp["x"][b])
    m["mem"] = np.ascontiguousarray(inp["mem"][b])
    m["norm_mix"] = inp["norm_mix"][0]
    m["w_in"] = inp["w_in"][0]
    m["w_out"] = inp["w_out"][0]
    m["s5_w_glu"] = inp["s5_w_glu"][0]
    m.update(host_s5(inp))
    cv = inp["gdn_conv"][0]
    m["gdn_convw"] = np.ascontiguousarray(cv.reshape(5, 3, 4, 128).transpose(3, 2, 1, 0))
    for nm in ("gdn_a_log_f", "gdn_dt_bias_f", "gdn_a_log_b", "gdn_dt_bias_b"):
        m[nm] = inp[nm][0]
    m["gdn_norm"] = inp["gdn_norm"][0]
    for nm in ("norm_xattn", "norm_mem", "xa_wq", "xa_wk", "xa_wv", "xa_wo", "norm_moe", "router_group_w", "router_group_b",
               "router_expert_w", "router_expert_b", "moe_w_gate", "moe_w_up", "moe_w_down"):
        m[nm] = inp[nm][0]
    m["norm_final"] = inp["norm_final"]
    m.update(host_consts())
    return m


def gdn_setup(k):
    cx, ar, D = k.cx, k.ar, k.D
    V = "vector"
    G = K()
    k.G = G
    G.mask = ar.alloc([7, 128], F32)
    G.tmask = Trk()
    cx.dma("sync", G.mask, D["gmask"][:, 0:7, :], cx.fresh(), w=[G.tmask])
    wsm = ar.alloc([8, 16], BF16)
    tw = Trk()
    load_w_cols(k, D["w_in"], 2560, 16, wsm, tw, cx.fresh('sw'))
    BA = ar.alloc([16, 16], F32)
    tBA = Trk()
    for i in range(NT):
        pb = k.psum[i % 4]
        tp = k.tpsum[i % 4]
        for c in range(8):
            cx.op("tensor", lambda h, pb=pb, c=c, i=i: h.matmul(pb[:, 0:16], k.hT[:, c, i * 128:(i + 1) * 128], wsm[:, c, :], start=(c == 0), stop=(c == 7)),
                  r=[tw, k.t_hT], w=[tp], inc=(c == 7))
        cx.op("scalar", lambda h, pb=pb, i=i: h.copy(BA[:, i, :], pb[:, 0:16]), r=[tp], w=[tBA])
    pr = ar.alloc([4, 4], F32)
    tpr = Trk()
    sl_ = cx.fresh()
    for j, nm in enumerate(("gdn_a_log_f", "gdn_dt_bias_f", "gdn_a_log_b", "gdn_dt_bias_b")):
        cx.dma("sync", pr[:, j, :], dram_bcast(D[nm], 128, 4), sl_, w=[tpr])
    G.nw = ar.alloc([128], F32)
    cx.dma("sync", G.nw, dram_bcast(D["gdn_norm"], 128, 128), sl_, w=[tpr])
    G.tpr = tpr
    T = Trk()
    G.T = T
    G.beta, G.nb, G.gc, G.eg, G.neg, G.ed = [], [], [], [], [], []
    def per_dir(d):
        beta = ar.alloc([16, 4], F32)
        nb = ar.alloc([16, 4], F32)
        g = ar.alloc([16, 4], F32)
        gc = ar.alloc([16, 4], F32)
        gt = ar.alloc([16, 4], F32)
        eg = ar.alloc([16, 4], F32)
        neg = ar.alloc([16, 4], F32)
        ed = ar.alloc([16, 4], F32)
        ea = ar.alloc([4], F32)
        braw = BA[:, :, d * 4:(d + 1) * 4]
        araw = BA[:, :, 8 + d * 4:8 + (d + 1) * 4]
        cx.op("scalar", lambda h: h.activation(beta, braw, AF.Sigmoid), r=[tBA, T], w=[T])
        cx.op(V, lambda h: h.tensor_scalar(nb, beta, -1.0, None, op0=ALU.mult), r=[T], w=[T])
        cx.op("scalar", lambda h: h.activation(ea, pr[:, 2 * d, :], AF.Exp), r=[tpr, T], w=[T])
        cx.op(V, lambda h: h.tensor_tensor(out=g, in0=araw, in1=pr[:, 2 * d + 1, :].unsqueeze(1).to_broadcast([128, 16, 4]), op=ALU.add), r=[tBA, tpr, T], w=[T])
        cx.op("scalar", lambda h: h.activation(g, g, AF.Exp), r=[T], w=[T])
        cx.op("scalar", lambda h: h.activation(g, g, AF.Ln, bias=1.0), r=[T], w=[T])
        cx.op(V, lambda h: h.scalar_tensor_tensor(out=g, in0=g, scalar=-1.0, in1=ea.unsqueeze(1).to_broadcast([128, 16, 4]), op0=ALU.mult, op1=ALU.mult), r=[T], w=[T])
        g2 = g.rearrange("p a b -> p (a b)")
        pb = k.psum[4 + d]
        tp = k.tpsum[4 + d]
        cx.op("tensor", lambda h, pb=pb, d=d: h.matmul(pb[:, 0:64], G.mask[:, d, :], g2, start=True, stop=True), r=[T, G.tmask], w=[tp])
        cx.op("tensor", lambda h, pb=pb: h.matmul(pb[:, 64:128], G.mask[:, 6, :], g2, start=True, stop=True), r=[T, G.tmask], w=[tp])
        cx.op(V, lambda h, pb=pb: h.tensor_copy(gc.rearrange("p a b -> p (a b)"), pb[:, 0:64]), r=[tp], w=[T])
        cx.op(V, lambda h, pb=pb: h.tensor_tensor(out=gt.rearrange("p a b -> p (a b)"), in0=pb[:, 64:128], in1=gc.rearrange("p a b -> p (a b)"), op=ALU.subtract), r=[tp, T], w=[T])
        cx.op("scalar", lambda h: h.activation(eg, gc, AF.Exp), r=[T], w=[T])
        cx.op("scalar", lambda h: h.activation(ed, gt, AF.Exp), r=[T], w=[T])
        cx.op(V, lambda h: h.tensor_scalar(neg, eg, -1.0, None, op0=ALU.mult), r=[T], w=[T])
        G.g = getattr(G, "g", []) + [g]
        G.beta.append(beta); G.nb.append(nb); G.gc.append(gc); G.eg.append(eg); G.neg.append(neg); G.ed.append(ed)
    per_dir(0)
    per_dir(1)
    G.osum = ar.alloc([16, 128], F32)
    G.tosum = [Trk() for _ in range(NT)]


def gdn_head(k, hd, yT, t_yT):
    cx, ar, D, G = k.cx, k.ar, k.D, k.G
    V = "vector"
    m0 = ar.mark()
    qnT = ar.alloc([2048], BF16)
    knT = ar.alloc([2048], BF16)
    Ktok = ar.alloc([16, 128], BF16)
    Vtok = ar.alloc([16, 128], BF16)
    tq, tk_, tKt, tVt = Trk(), Trk(), Trk(), Trk()
    wz = ar.alloc([8, 128], BF16)
    twz = Trk()
    load_w_cols(k, D["w_in"], 512 + 1536 + hd * 128, 128, wz, twz, cx.fresh('sw'))
    mA = ar.mark()
    w3 = [ar.alloc([8, 128], BF16) for _ in range(3)]
    tw3 = [Trk() for _ in range(3)]
    for j in range(3):
        load_w_cols(k, D["w_in"], 512 + j * 512 + hd * 128, 128, w3[j], tw3[j], cx.fresh('sw'))
    cw = ar.alloc([3, 5], F32)
    tcw = Trk()
    cx.dma("sync", cw, D["gdn_convw"][:, hd, :, :], cx.fresh(), w=[tcw])
    diag = ar.alloc([15, 128], BF16)
    tdg = Trk()
    for j in range(3):
        for t in range(5):
            cx.op("vector", lambda h, j=j, t=t: h.tensor_scalar(diag[:, j * 5 + t, :], k.identb, cw[:, j, t:t + 1], None, op0=ALU.mult), r=[tcw], w=[tdg])
    import os
    ALV = int(os.environ.get("GDN_ALV", "9"))
    if ALV == 0:
        cx.barrier(); ar.release(m0); return
    raw = [ar.alloc([2052], BF16) for _ in range(2)]
    traw = [Trk(), Trk()]
    for b in range(2):
        cx.op("gpsimd", lambda h, b=b: h.memset(raw[b][:, 0:2], 0.0), w=[traw[b]])
        cx.op("gpsimd", lambda h, b=b: h.memset(raw[b][:, 2050:2052], 0.0), w=[traw[b]])
    act = ar.alloc([2048], F32)
    tact = Trk()
    vT = ar.alloc([2048], BF16)
    tvT = Trk()
    sqb = ar.alloc([2048], BF16)
    tsqb = Trk()
    rn = [ar.alloc([512], F32) for _ in range(4)]
    trn = [Trk() for _ in range(4)]
    tactn = [Trk() for _ in range(4)]
    tsqn = [Trk() for _ in range(4)]
    if ALV == 1:
        cx.barrier(); ar.release(m0); return
    for j in range(3):
        b = j % 2

        def consume(n, pb, tp, b=b):
            cx.op(V if n % 2 else "scalar", (lambda h: h.tensor_copy(raw[b][:, 2 + n * 512:2 + (n + 1) * 512], pb[:, :])) if n % 2 else
                  (lambda h: h.copy(raw[b][:, 2 + n * 512:2 + (n + 1) * 512], pb[:, :])), r=[tp], w=[traw[b]])
        proj_fm(k, w3[j], tw3[j], consume)
        for n in range(4):
            pb = k.psum[4 + n]
            tp = k.tpsum[4 + n]
            for t in range(5):
                cx.op("tensor", lambda h, pb=pb, t=t, n=n, j=j, b=b: h.matmul(pb[:, :], diag[:, j * 5 + t, :], raw[b][:, n * 512 + t:n * 512 + t + 512], start=(t == 0), stop=(t == 4)),
                      r=[tdg, traw[b]], w=[tp], inc=(t == 4))
        for n in range(4):
            pb = k.psum[4 + n]
            tp = k.tpsum[4 + n]
            ts = slice(n * 512, (n + 1) * 512)
            if j == 2:
                cx.op("scalar", lambda h, pb=pb, ts=ts: h.activation(vT[:, ts], pb[:, :], AF.Silu), r=[tp], w=[tvT])
            else:
                cx.op("scalar", lambda h, pb=pb, ts=ts: h.activation(act[:, ts], pb[:, :], AF.Silu), r=[tp], w=[tactn[n]])
        if j < 2 and ALV > 2:
            for n in range(4):
                ts = slice(n * 512, (n + 1) * 512)
                cx.op("scalar", lambda h, ts=ts: h.activation(sqb[:, ts], act[:, ts], AF.Square), r=[tactn[n]], w=[tsqn[n]])
            for n in range(4):
                ts = slice(n * 512, (n + 1) * 512)
                pb2 = k.psum[n]
                tp2 = k.tpsum[n]
                cx.op("tensor", lambda h, pb2=pb2, ts=ts: h.matmul(pb2[:, :], k.onesb, sqb[:, ts], start=True, stop=True), r=[tsqn[n]], w=[tp2])
            for n in range(4):
                pb2 = k.psum[n]
                tp2 = k.tpsum[n]
                cx.op("scalar", lambda h, pb2=pb2, n=n: h.activation(rn[n], pb2[:, :], AF.Sqrt, bias=k.epsc), r=[tp2], w=[trn[n]])
            for n in range(4):
                cx.op(V, lambda h, n=n: h.reciprocal(rn[n], rn[n]), r=[trn[n]], w=[trn[n]])
            dstT, tdst, scl = (qnT, tq, 128.0 ** -0.5) if j == 0 else (knT, tk_, 1.0)
            for n in range(4):
                ts = slice(n * 512, (n + 1) * 512)
                cx.op(V, lambda h, ts=ts, n=n, dstT=dstT, scl=scl: h.scalar_tensor_tensor(out=dstT[:, ts], in0=act[:, ts], scalar=scl, in1=rn[n], op0=ALU.mult, op1=ALU.mult),
                      r=[tactn[n], trn[n]], w=[tdst])
    if ALV <= 3:
        cx.barrier(); ar.release(m0); return
    TV = int(os.environ.get("GDN_TV", "0"))
    for i in range(NT):
        pb = k.psum[i % 2].bitcast(BF16)
        tp = k.tpsum[i % 2]
        if TV == 0:
            cx.op("tensor", lambda h, pb=pb, i=i: h.transpose(pb[:, 0:128], knT[:, i * 128:(i + 1) * 128], k.identb), r=[tk_], w=[tp])
            cx.op("tensor", lambda h, pb=pb, i=i: h.transpose(pb[:, 128:256], vT[:, i * 128:(i + 1) * 128], k.identb), r=[tvT], w=[tp])
            cx.op("scalar", lambda h, pb=pb, i=i: h.copy(Ktok[:, i, :], pb[:, 0:128]), r=[tp], w=[tKt])
            cx.op(V, lambda h, pb=pb, i=i: h.tensor_copy(Vtok[:, i, :], pb[:, 128:256]), r=[tp], w=[tVt])
        elif TV == 1:
            cx.op("tensor", lambda h, pb=pb, i=i: h.transpose(pb[:, 0:128], knT[:, i * 128:(i + 1) * 128], k.identb), r=[tk_], w=[tp])
            cx.op("scalar", lambda h, pb=pb, i=i: h.copy(Ktok[:, i, :], pb[:, 0:128]), r=[tp], w=[tKt])
        elif TV == 2:
            cx.op("tensor", lambda h, pb=pb, i=i: h.transpose(pb[:, 0:128], vT[:, i * 128:(i + 1) * 128], k.identb), r=[tvT], w=[tp])
            cx.op(V, lambda h, pb=pb, i=i: h.tensor_copy(Vtok[:, i, :], pb[:, 0:128]), r=[tp], w=[tVt])
    if hd == 0:
        k.dbg_add("gdn_qn", qnT, [tq])
        k.dbg_add("gdn_kn", knT, [tk_])
        k.dbg_add("gdn_vtok", Vtok, [tVt])
    cx.barrier()
    ar.release(mA)
    STOP = os.environ.get("GDN_STOP", "")
    if STOP == "A":
        ar.release(m0)
        return
    qgT = [ar.alloc([2048], BF16) for _ in range(2)]
    Kd = [ar.alloc([16, 128], BF16) for _ in range(2)]
    Pm = [ar.alloc([16, 128], BF16) for _ in range(2)]
    QKm = [ar.alloc([16, 128], BF16) for _ in range(2)]
    etot = [ar.alloc([32], F32) for _ in range(2)]
    WnT = [ar.alloc([2048], BF16) for _ in range(2)]
    U0b = [ar.alloc([16, 128], BF16) for _ in range(2)]
    tWn = [[Trk() for _ in range(NT)] for _ in range(2)]
    tU0 = [[Trk() for _ in range(NT)] for _ in range(2)]
    tqg = [[Trk() for _ in range(NT)] for _ in range(2)]
    tKd = [[Trk() for _ in range(NT)] for _ in range(2)]
    tPm = [[Trk() for _ in range(NT)] for _ in range(2)]
    tQK = [[Trk() for _ in range(NT)] for _ in range(2)]
    tet = [[Trk() for _ in range(NT)] for _ in range(2)]
    NI = 8
    mN = ar.mark()
    NDT = BF16 if os.environ.get('GDN_NEU', 'bf16') == 'bf16' else F32
    nid = k.identb if NDT == BF16 else k.ident
    Xb = [[ar.alloc([128], NDT) for _ in range(2)] for _ in range(NI)]
    XTb = [[ar.alloc([128], NDT) for _ in range(2)] for _ in range(NI)]
    Pb = [[ar.alloc([128], NDT) for _ in range(2)] for _ in range(NI)]

    def tview(pn_, c0):
        return pn_[:, c0:c0 + 128] if NDT == F32 else pn_.bitcast(BF16)[:, 2 * c0:2 * c0 + 128]
    tX = [Trk() for _ in range(NI)]
    EGB = [ar.alloc([128], F32) for _ in range(NI)]
    ET = [ar.alloc([128], BF16) for _ in range(NI)]
    ETs = ET
    tE = [Trk() for _ in range(NI)]
    tEG = [Trk() for _ in range(NI)]
    Kg = [ar.alloc([128], BF16) for _ in range(NI)]
    tKg = [Trk() for _ in range(NI)]
    tXP = [Trk() for _ in range(NI)]
    insts = [(i, d) for i in range(NT) for d in range(2)]
    for g0 in range(0, len(insts), NI):
        grp = insts[g0:g0 + NI]
        info = []
        for s_, (i, d) in enumerate(grp):
            info.append(dict(s_=s_, i=i, d=d, tsl=slice(i * 128, (i + 1) * 128),
                             col=pap(G.g[d], 0, 128, i * 4 + hd, [[0, 128]]),
                             gcc=G.gc[d][:, i, hd:hd + 1], nbc=G.nb[d][:, i, hd:hd + 1], edc=G.ed[d][:, i, hd:hd + 1],
                             pb=k.psum[s_], tp=k.tpsum[s_]))
        for q_ in info:
            s_, i, d, tsl, col, pb, tp = q_["s_"], q_["i"], q_["d"], q_["tsl"], q_["col"], q_["pb"], q_["tp"]
            cx.op("tensor", lambda h, pb=pb, col=col, d=d: h.matmul(pb[:, 0:128], col, G.mask[:, d, :], start=True, stop=True), r=[G.T, G.tmask], w=[tp], inc=False)
            cx.op("tensor", lambda h, pb=pb, tsl=tsl: h.matmul(pb[:, 128:256], knT[:, tsl], knT[:, tsl], start=True, stop=True), r=[tk_], w=[tp], inc=False)
            cx.op("tensor", lambda h, pb=pb, tsl=tsl: h.matmul(pb[:, 256:384], knT[:, tsl], qnT[:, tsl], start=True, stop=True), r=[tk_, tq], w=[tp])
        for q_ in info:
            s_, i, d, pb, tp, gcc = q_["s_"], q_["i"], q_["d"], q_["pb"], q_["tp"], q_["gcc"]
            cx.op("scalar", lambda h, pb=pb, s_=s_: h.activation(EGB[s_], pb[:, 0:128], AF.Exp), r=[tp], w=[tEG[s_]])
            cx.op(V, lambda h, pb=pb, s_=s_, gcc=gcc, d=d: h.scalar_tensor_tensor(out=ET[s_], in0=pb[:, 0:128], scalar=gcc, in1=G.mask[:, 2 + d, :], op0=ALU.subtract, op1=ALU.min),
                  r=[tp, G.T, G.tmask], w=[tE[s_]])
        for q_ in info:
            s_, i, d, tsl = q_["s_"], q_["i"], q_["d"], q_["tsl"]
            cx.op("scalar", lambda h, s_=s_: h.activation(ET[s_], ET[s_], AF.Exp), r=[tE[s_]], w=[tE[s_]])
            cx.op(V, lambda h, s_=s_, tsl=tsl, d=d: h.tensor_tensor(out=qgT[d][:, tsl], in0=qnT[:, tsl], in1=EGB[s_], op=ALU.mult), r=[tq, tEG[s_]], w=[tqg[d][i]])
        for q_ in info:
            s_, i, d, pb, tp, edc = q_["s_"], q_["i"], q_["d"], q_["pb"], q_["tp"], q_["edc"]
            c0, c1 = (63, 127) if d == 0 else (0, 64)
            cx.op("scalar", lambda h, s_=s_, d=d, i=i, c0=c0: h.copy(etot[d][:, 2 * i:2 * i + 1], EGB[s_][:, c0:c0 + 1]), r=[tEG[s_]], w=[tet[d][i]])
            cx.op("scalar", lambda h, s_=s_, d=d, i=i, c1=c1: h.copy(etot[d][:, 2 * i + 1:2 * i + 2], EGB[s_][:, c1:c1 + 1]), r=[tEG[s_]], w=[tet[d][i]])
            cx.op("scalar", lambda h, d=d, i=i, edc=edc: h.activation(Kd[d][:, i, :], Ktok[:, i, :], AF.Identity, scale=edc), r=[tKt, G.T], w=[tKd[d][i]])
            egc = G.eg[d][:, i, hd:hd + 1]
            cx.op("scalar", lambda h, s_=s_, i=i, egc=egc: h.activation(Kg[s_], Ktok[:, i, :], AF.Identity, scale=egc), r=[tKt, G.T], w=[tKg[s_]])
            cx.op(V, lambda h, pb=pb, s_=s_, d=d, i=i: h.tensor_tensor(out=QKm[d][:, i, :], in0=pb[:, 256:384], in1=ET[s_], op=ALU.mult), r=[tp, tE[s_]], w=[tQK[d][i]])
        for q_ in info:
            s_, d = q_["s_"], q_["d"]
            cx.op(V, lambda h, s_=s_, d=d: h.tensor_tensor(out=ETs[s_], in0=ET[s_], in1=G.mask[:, 4 + d, :], op=ALU.mult), r=[tE[s_], G.tmask], w=[tE[s_]])
        for q_ in info:
            s_, pb, tp, nbc = q_["s_"], q_["pb"], q_["tp"], q_["nbc"]
            cx.op(V, lambda h, pb=pb, s_=s_, nbc=nbc: h.scalar_tensor_tensor(out=Xb[s_][0], in0=pb[:, 128:256], scalar=nbc, in1=ETs[s_], op0=ALU.mult, op1=ALU.mult),
                  r=[tp, tE[s_], G.T], w=[tX[s_]])
        for q_ in info:
            s_, pn, tn = q_["s_"], q_["pb"], q_["tp"]
            cx.op("tensor", lambda h, pn=pn, s_=s_: h.transpose(tview(pn, 384), Xb[s_][0], nid), r=[tX[s_]], w=[tn])
            cx.op(V, lambda h, s_=s_: h.tensor_tensor(out=Pb[s_][0], in0=Xb[s_][0], in1=nid, op=ALU.add), r=[tX[s_]], w=[tXP[s_]])
            cx.op("scalar", lambda h, pn=pn, s_=s_: h.copy(XTb[s_][0], tview(pn, 384)), r=[tn], w=[tX[s_]])
        for L in range(1, 6):
            a, b_ = (L - 1) % 2, L % 2
            for s_, (i, d) in enumerate(grp):
                pn = k.psum[s_]
                tn = k.tpsum[s_]
                if L < 5:
                    cx.op("tensor", lambda h, pn=pn, s_=s_, a=a: h.matmul(pn[:, 0:128], XTb[s_][a], Xb[s_][a], start=True, stop=True), r=[tX[s_]], w=[tn], inc=False)
                cx.op("tensor", lambda h, pn=pn, s_=s_, a=a: h.matmul(pn[:, 128:256], Xb[s_][a], XTb[s_][a], start=True, stop=True), r=[tX[s_]], w=[tn])
                e1, e2 = ("scalar", V) if s_ % 2 == 0 else (V, "scalar")
                if L < 5:
                    if e1 == "scalar":
                        cx.op("scalar", lambda h, pn=pn, s_=s_, b_=b_: h.copy(Xb[s_][b_], pn[:, 0:128]), r=[tn], w=[tX[s_]])
                    else:
                        cx.op(V, lambda h, pn=pn, s_=s_, b_=b_: h.tensor_copy(Xb[s_][b_], pn[:, 0:128]), r=[tn], w=[tX[s_]])
                if e2 == "scalar":
                    cx.op("scalar", lambda h, pn=pn, s_=s_, b_=b_: h.copy(XTb[s_][b_], pn[:, 128:256]), r=[tn], w=[tX[s_]])
                else:
                    cx.op(V, lambda h, pn=pn, s_=s_, b_=b_: h.tensor_copy(XTb[s_][b_], pn[:, 128:256]), r=[tn], w=[tX[s_]])
            for s_, (i, d) in enumerate(grp):
                pn = k.psum[s_]
                tn = k.tpsum[s_]
                cx.op("tensor", lambda h, pn=pn, s_=s_, a=a, b_=b_: h.matmul(pn[:, 256:384], XTb[s_][b_], Pb[s_][a], start=True, stop=True), r=[tX[s_], tXP[s_]], w=[tn])
                if L < 5:
                    cx.op(V, lambda h, pn=pn, s_=s_, a=a, b_=b_: h.tensor_tensor(out=Pb[s_][b_], in0=pn[:, 256:384], in1=Pb[s_][a], op=ALU.add), r=[tn, tXP[s_]], w=[tXP[s_]])
                else:
                    cx.op(V, lambda h, pn=pn, s_=s_, a=a, d=d, i=i: h.tensor_tensor(out=Pm[d][:, i, :], in0=pn[:, 256:384], in1=Pb[s_][a], op=ALU.add), r=[tn, tXP[s_]], w=[tPm[d][i], tXP[s_]])
        for s_, (i, d) in enumerate(grp):
            pn = k.psum[s_]
            tn = k.tpsum[s_]
            tsl = slice(i * 128, (i + 1) * 128)
            cx.op("tensor", lambda h, pn=pn, s_=s_, d=d, i=i: h.matmul(pn[:, 0:128], Kg[s_], Pm[d][:, i, :], start=True, stop=True), r=[tKg[s_], tPm[d][i]], w=[tn], inc=False)
            cx.op("tensor", lambda h, pn=pn, d=d, i=i: h.matmul(pn[:, 128:256], Pm[d][:, i, :], Vtok[:, i, :], start=True, stop=True), r=[tPm[d][i], tVt], w=[tn])
            btc_ = G.beta[d][:, i, hd:hd + 1]
            cx.op(V, lambda h, pn=pn, d=d, tsl=tsl: h.tensor_scalar(WnT[d][:, tsl], pn[:, 0:128], -1.0, None, op0=ALU.mult), r=[tn], w=[tWn[d][i]])
            cx.op("scalar", lambda h, pn=pn, d=d, i=i, btc_=btc_: h.activation(U0b[d][:, i, :], pn[:, 128:256], AF.Identity, scale=btc_), r=[tn, G.T], w=[tU0[d][i]])
    if STOP == "B":
        cx.barrier()
        ar.release(m0)
        return
    ar.release(mN)
    Sf = [[ar.alloc([128], F32) for _ in range(2)] for _ in range(2)]
    Sb = [ar.alloc([128], BF16) for _ in range(2)]
    Rp = [ar.alloc([128], BF16) for _ in range(2)]
    vn = [ar.alloc([128], BF16) for _ in range(2)]
    tS = [Trk(), Trk()]
    tSf = [Trk(), Trk()]
    tR = [Trk(), Trk()]
    tv = [Trk(), Trk()]
    cx.op("gpsimd", lambda h: h.memset(G.osum, 0.0), w=G.tosum)
    for d in range(2):
        cx.op("gpsimd", lambda h, d=d: h.memset(Sf[d][0], 0.0), w=[tS[d]])
        cx.op("gpsimd", lambda h, d=d: h.memset(Sb[d], 0.0), w=[tS[d]])
        cx.op("gpsimd", lambda h, d=d: h.memset(Rp[d], 0.0), w=[tR[d]])
        cx.op("gpsimd", lambda h, d=d: h.memset(vn[d], 0.0), w=[tv[d]])
    for step in range(32):
        for d in range(2):
            if d == 0:
                i, hh = step // 2, step % 2
            else:
                i, hh = 15 - step // 2, 1 - step % 2
            tsl = slice(i * 128, (i + 1) * 128)
            ps_ = slice(hh * 64, (hh + 1) * 64)
            cur, nxt = step % 2, (step + 1) % 2
            pcs = [k.psum[4 * d + q_] for q_ in range(4)]
            tcs = [k.tpsum[4 * d + q_] for q_ in range(4)]
            negc = G.neg[d][ps_, i, hd:hd + 1]
            btc = G.beta[d][ps_, i, hd:hd + 1]
            p1, pv_, po_, pst = pcs
            t1_, tv_, to_, tst = tcs
            cx.op("tensor", lambda h, p1=p1, tsl=tsl, d=d: h.matmul(p1[:, 0:128], WnT[d][:, tsl], Sb[d], start=True, stop=True), r=[tWn[d][i], tS[d]], w=[t1_])
            cx.op(V, lambda h, p1=p1, ps_=ps_, btc=btc, d=d, i=i: h.scalar_tensor_tensor(out=vn[d][ps_, :], in0=p1[ps_, 0:128], scalar=btc, in1=U0b[d][ps_, i, :], op0=ALU.mult, op1=ALU.add),
                  r=[t1_, tU0[d][i], G.T], w=[tv[d]])
            cx.op("tensor", lambda h, po_=po_, tsl=tsl, d=d: h.matmul(po_[:, 0:128], qgT[d][:, tsl], Sb[d], start=True, stop=False), r=[tqg[d][i], tS[d]], w=[to_], inc=False)
            cx.op("tensor", lambda h, po_=po_, ps_=ps_, d=d, i=i: h.matmul(po_[:, 0:128], QKm[d][ps_, i, :], vn[d][ps_, :], start=False, stop=True), r=[tQK[d][i], tv[d]], w=[to_])
            cx.op("tensor", lambda h, pst=pst, ps_=ps_, d=d, i=i: h.matmul(pst[:, 0:128], Kd[d][ps_, i, :], vn[d][ps_, :], start=True, stop=True), r=[tKd[d][i], tv[d]], w=[tst])
            cx.op("gpsimd" if False else V, lambda h, po_=po_, ps_=ps_, i=i: h.tensor_tensor(out=G.osum[ps_, i, :], in0=po_[ps_, 0:128], in1=G.osum[ps_, i, :], op=ALU.add), r=[to_, G.tosum[i]], w=[G.tosum[i]])
            etc = etot[d][:, 2 * i + hh:2 * i + hh + 1]
            cx.op(V, lambda h, pst=pst, d=d, cur=cur, nxt=nxt, etc=etc: h.scalar_tensor_tensor(out=Sf[d][nxt], in0=Sf[d][cur], scalar=etc, in1=pst[:, 0:128], op0=ALU.mult, op1=ALU.add),
                  r=[tst, tet[d][i], tS[d]], w=[tS[d]])
            cx.op("scalar", lambda h, d=d, nxt=nxt: h.copy(Sb[d], Sf[d][nxt]), r=[tS[d]], w=[tS[d]])
    if hd == 0:
        k.dbg_add("gdn_osum", G.osum, G.tosum)
    if STOP == "C":
        cx.barrier()
        ar.release(m0)
        return
    ss = ar.alloc([NT, 2], F32)
    tss = Trk()
    junk = ar.alloc([128], BF16)
    zs = [ar.alloc([128], F32) for _ in range(2)]
    tzs = [Trk(), Trk()]
    yb = [ar.alloc([128], BF16) for _ in range(2)]
    tyb = [Trk(), Trk()]
    for i in range(NT):
        cx.op("scalar", lambda h, i=i: h.activation(junk, G.osum[:, i, :], AF.Square, accum_out=ss[:, i, 0:1]), r=[G.tosum[i], tss], w=[tss])
    cx.op(V, lambda h: h.tensor_scalar(ss[:, :, 1:2], ss[:, :, 0:1], 1.0 / 128, EPS, op0=ALU.mult, op1=ALU.add), r=[tss], w=[tss])
    cx.op("scalar", lambda h: h.activation(ss[:, :, 1:2], ss[:, :, 1:2], AF.Sqrt), r=[tss], w=[tss])
    cx.op(V, lambda h: h.reciprocal(ss[:, :, 1:2], ss[:, :, 1:2]), r=[tss], w=[tss])
    for i in range(NT):
        b = i % 2
        pz = k.psum[b]
        tz = k.tpsum[b]
        for c in range(8):
            cx.op("tensor", lambda h, pz=pz, c=c, i=i: h.matmul(pz[:, 0:128], k.hT[:, c, i * 128:(i + 1) * 128], wz[:, c, :], start=(c == 0), stop=(c == 7)),
                  r=[twz, k.t_hT], w=[tz], inc=(c == 7))
        cx.op("scalar", lambda h, pz=pz, b=b: h.activation(zs[b], pz[:, 0:128], AF.Silu), r=[tz], w=[tzs[b]])
        s1 = ss[:, i, 1:2]
        cx.op(V, lambda h, i=i, s1=s1: h.scalar_tensor_tensor(out=G.osum[:, i, :], in0=G.osum[:, i, :], scalar=s1, in1=G.nw, op0=ALU.mult, op1=ALU.mult), r=[tss, G.tpr, G.tosum[i]], w=[G.tosum[i]])
        cx.op(V, lambda h, i=i, b=b: h.tensor_tensor(out=yb[b], in0=G.osum[:, i, :], in1=zs[b], op=ALU.mult), r=[G.tosum[i], tzs[b]], w=[tyb[b]])
        pt = k.psum[2 + b].bitcast(BF16)
        tt_ = k.tpsum[2 + b]
        cx.op("tensor", lambda h, pt=pt, b=b: h.transpose(pt[:, 0:128], yb[b], k.identb), r=[tyb[b]], w=[tt_])
        cx.op("scalar", lambda h, pt=pt, i=i: h.copy(yT[:, 4 + hd, i * 128:(i + 1) * 128], pt[:, 0:128]), r=[tt_], w=[t_yT])
    cx.barrier()
    ar.release(m0)


def out_proj(k, yT, t_yT):
    cx, ar, D = k.cx, k.ar, k.D
    m0 = ar.mark()
    wo = ar.alloc([8, 1024], BF16)
    two = Trk()
    wsrc = D["w_out"]
    sl_ = cx.fresh('sw')
    for c in range(8):
        cx.dma("gpsimd", wo[:, c, :], wsrc[c * 128:(c + 1) * 128, :], sl_, w=[two])
    slx = cx.fresh()
    for i in range(NT):
        cx.dma("sync", k.xacc[:, i, :], D["x"][i * 128:(i + 1) * 128, :], slx, w=[k.txacc[i]])
    for i in range(NT):
        k.txacc[i].w = (slx.key, slx.total)
    for i in range(NT):
        for half in range(2):
            pb = k.psum[(2 * i + half) % 4]
            tp = k.tpsum[(2 * i + half) % 4]
            for c in range(8):
                cx.op("tensor", lambda h, pb=pb, c=c, i=i, half=half: h.matmul(pb[:, :], yT[:, c, i * 128:(i + 1) * 128], wo[:, c, half * 512:(half + 1) * 512], start=(c == 0), stop=(c == 7)),
                      r=[t_yT, two], w=[tp], inc=(c == 7))
            xs = k.xacc[:, i, half * 512:(half + 1) * 512]
            cx.op("vector", lambda h, pb=pb, xs=xs: h.tensor_tensor(out=xs, in0=pb[:, :], in1=xs, op=ALU.add), r=[tp, k.txacc[i]], w=[k.txacc[i]])
    cx.barrier()
    ar.release(m0)


def xattn(k):
    cx, ar, D = k.cx, k.ar, k.D
    V = "vector"
    m0 = ar.mark()
    xnT = ar.alloc([8, 2048], BF16)
    t_xnT = Trk()
    memT = ar.alloc([8, 256], BF16)
    t_memT = Trk()
    m1 = ar.mark()
    mt = [ar.alloc([1024], F32) for _ in range(2)]
    tmt = [Trk(), Trk()]

    def src_mem(i):
        cx.dma("sync", mt[i], D["mem"][i * 128:(i + 1) * 128, :], cx.fresh(), w=[tmt[i]])
        return mt[i], tmt[i]
    norm_transpose(k, "mem", src_mem, 2, D["norm_mem"], memT, BF16, t_memT)
    ar.release(m1)
    norm_transpose(k, "xa", lambda i: (k.xacc[:, i, :], k.txacc[i]), NT, D["norm_xattn"], xnT, BF16, t_xnT, resident=True)
    k.dbg_add("xa_memT", memT, [t_memT])
    k.dbg_add("xa_xnT", xnT, [t_xnT])
    wq = [ar.alloc([8, 256], BF16) for _ in range(2)]
    wk = [ar.alloc([8, 256], BF16) for _ in range(2)]
    wv = [ar.alloc([8, 256], BF16) for _ in range(2)]
    wo = [ar.alloc([2, 1024], BF16) for _ in range(2)]
    twA = [Trk(), Trk()]
    two_ = [Trk(), Trk()]
    swA = [cx.slot("xwa0"), cx.slot("xwa1")]
    swO = [cx.slot("xwo0"), cx.slot("xwo1")]
    kTb = [ar.alloc([2, 256], BF16) for _ in range(2)]
    vhb = [ar.alloc([2, 256], BF16) for _ in range(2)]
    tkvb = [Trk(), Trk()]
    qTb = [ar.alloc([2, 2048], BF16) for _ in range(2)]
    tqTb = [Trk(), Trk()]
    E = [ar.alloc([2, 512], BF16) for _ in range(2)]
    tE = [Trk(), Trk()]
    rden = [ar.alloc([512], F32) for _ in range(2)]
    trd = [Trk(), Trk()]
    oTn = [ar.alloc([2, 512], BF16) for _ in range(2)]
    toT = [Trk(), Trk()]
    cx.barrier()
    k.txa = [[Trk(), Trk()] for _ in range(NT)]

    def loadA(hd):
        b = hd % 2
        c0 = hd * 256
        for (dst, nm) in ((wq[b], "xa_wq"), (wk[b], "xa_wk"), (wv[b], "xa_wv")):
            load_w_cols(k, D[nm], c0, 256, dst, twA[b], swA[b])

    def loadO(hd):
        b = hd % 2
        c0 = hd * 256
        src = D["xa_wo"]
        cx.dma("gpsimd", wo[b], bass.AP(src.tensor, src.offset + c0 * 1024, [[1024, 128], [128 * 1024, 2], [1, 1024]]), swO[b], w=[two_[b]])

    def proj(hd):
        b = hd % 2
        kT, vh, qT, tkv, tqT = kTb[b], vhb[b], qTb[b], tkvb[b], tqTb[b]
        for dc in range(2):
            pb = k.psum[dc]
            tp = k.tpsum[dc]
            for c in range(8):
                cx.op("tensor", lambda h, pb=pb, c=c, dc=dc, b=b: h.matmul(pb[:, 0:256], wk[b][:, c, dc * 128:(dc + 1) * 128], memT[:, c, :], start=(c == 0), stop=(c == 7)),
                      r=[twA[b], t_memT], w=[tp], inc=(c == 7))
            cx.op("scalar", lambda h, pb=pb, dc=dc, kT=kT: h.copy(kT[:, dc, :], pb[:, 0:256]), r=[tp], w=[tkv])
        for mtile in range(2):
            pb = k.psum[2 + mtile]
            tp = k.tpsum[2 + mtile]
            for c in range(8):
                cx.op("tensor", lambda h, pb=pb, c=c, mtile=mtile, b=b: h.matmul(pb[:, 0:256], memT[:, c, mtile * 128:(mtile + 1) * 128], wv[b][:, c, :], start=(c == 0), stop=(c == 7)),
                      r=[twA[b], t_memT], w=[tp], inc=(c == 7))
            cx.op(V, lambda h, pb=pb, mtile=mtile, vh=vh: h.tensor_copy(vh[:, mtile, :], pb[:, 0:256]), r=[tp], w=[tkv])
        for dc in range(2):
            for n in range(4):
                pb = k.psum[(dc * 4 + n) % 4]
                tp = k.tpsum[(dc * 4 + n) % 4]
                for c in range(8):
                    cx.op("tensor", lambda h, pb=pb, c=c, dc=dc, n=n, b=b: h.matmul(pb[:, :], wq[b][:, c, dc * 128:(dc + 1) * 128], xnT[:, c, n * 512:(n + 1) * 512], start=(c == 0), stop=(c == 7)),
                          r=[twA[b], t_xnT], w=[tp], inc=(c == 7))
                if n % 2 == 0:
                    cx.op("scalar", lambda h, pb=pb, dc=dc, n=n, qT=qT: h.copy(qT[:, dc, n * 512:(n + 1) * 512], pb[:, :]), r=[tp], w=[tqT])
                else:
                    cx.op(V, lambda h, pb=pb, dc=dc, n=n, qT=qT: h.tensor_copy(qT[:, dc, n * 512:(n + 1) * 512], pb[:, :]), r=[tp], w=[tqT])

    def chunks(hd):
        b = hd % 2
        kT, vh, qT, tkv, tqT = kTb[b], vhb[b], qTb[b], tkvb[b], tqTb[b]
        tw = [two_[0], two_[1]]

        def emit_scores(n):
            eb = n % 2
            ts = slice(n * 512, (n + 1) * 512)
            for mtile in range(2):
                pb = k.psum[mtile]
                tp = k.tpsum[mtile]
                for dc in range(2):
                    cx.op("tensor", lambda h, pb=pb, dc=dc, mtile=mtile, ts=ts: h.matmul(pb[:, :], kT[:, dc, mtile * 128:(mtile + 1) * 128], qT[:, dc, ts], start=(dc == 0), stop=(dc == 1)),
                          r=[tkv, tqT], w=[tp], inc=(dc == 1))
                cx.op("scalar", lambda h, pb=pb, mtile=mtile, eb=eb: h.activation(E[eb][:, mtile, :], pb[:, :], AF.Exp, scale=1.0 / 16.0), r=[tp], w=[tE[eb]])

        def emit_rest(n):
            eb = n % 2
            pd = k.psum[2]
            tpd = k.tpsum[2]
            for mtile in range(2):
                cx.op("tensor", lambda h, pd=pd, mtile=mtile, eb=eb: h.matmul(pd[:, :], k.onesb, E[eb][:, mtile, :], start=(mtile == 0), stop=(mtile == 1)), r=[tE[eb]], w=[tpd], inc=(mtile == 1))
            cx.op(V, lambda h, pd=pd, eb=eb: h.reciprocal(rden[eb], pd[:, :]), r=[tpd], w=[trd[eb]])
            for dc in range(2):
                po = k.psum[3 + dc]
                tpo = k.tpsum[3 + dc]
                for mtile in range(2):
                    cx.op("tensor", lambda h, po=po, mtile=mtile, dc=dc, eb=eb: h.matmul(po[:, :], vh[:, mtile, dc * 128:(dc + 1) * 128], E[eb][:, mtile, :], start=(mtile == 0), stop=(mtile == 1)),
                          r=[tkv, tE[eb]], w=[tpo], inc=(mtile == 1))
                cx.op(V, lambda h, po=po, dc=dc, eb=eb: h.tensor_tensor(out=oTn[eb][:, dc, :], in0=po[:, :], in1=rden[eb], op=ALU.mult), r=[tpo, trd[eb]], w=[toT[eb]])
            for t in range(4):
                i = n * 4 + t
                for half in range(2):
                    pw_ = k.psum[5 + (t * 2 + half) % 3]
                    tpw = k.tpsum[5 + (t * 2 + half) % 3]
                    for dc in range(2):
                        cx.op("tensor", lambda h, pw_=pw_, dc=dc, t=t, half=half, b=b, eb=eb: h.matmul(pw_[:, :], oTn[eb][:, dc, t * 128:(t + 1) * 128], wo[b][:, dc, half * 512:(half + 1) * 512], start=(dc == 0), stop=(dc == 1)),
                              r=[toT[eb], tw[b]], w=[tpw], inc=(dc == 1))
                    xs = k.xacc[:, i, half * 512:(half + 1) * 512]
                    cx.op(V, lambda h, pw_=pw_, xs=xs: h.tensor_tensor(out=xs, in0=pw_[:, :], in1=xs, op=ALU.add), r=[tpw, k.txa[i][half]], w=[k.txa[i][half]])
        emit_scores(0)
        for n in range(4):
            if n + 1 < 4:
                emit_scores(n + 1)
            emit_rest(n)
    loadA(0)
    loadA(1)
    loadO(0)
    loadO(1)
    proj(0)
    for hd in range(4):
        if hd + 1 < 4:
            proj(hd + 1)
        if hd + 2 < 4:
            loadA(hd + 2)
        chunks(hd)
        if hd + 2 < 4:
            loadO(hd + 2)
    cx.barrier()
    ar.release(m0)


def moe(k):
    cx, ar, D = k.cx, k.ar, k.D
    V = "vector"
    m0 = ar.mark()
    xnT = ar.alloc([8, 2048], BF16)
    t_xnT = Trk()
    norm_transpose(k, "moe", lambda i: (k.xacc[:, i, :], k.txacc[i]), NT, D["norm_moe"], xnT, BF16, t_xnT, resident=True)
    wr = ar.alloc([8, 36], BF16)
    twr = Trk()
    sl_ = cx.fresh('sw')
    srcg, srce = D["router_group_w"], D["router_expert_w"]
    cx.dma("gpsimd", wr[:, :, 0:4], bass.AP(srcg.tensor, srcg.offset, [[4, 128], [4 * 128, 8], [1, 4]]), sl_, w=[twr])
    cx.dma("gpsimd", wr[:, :, 4:36], bass.AP(srce.tensor, srce.offset, [[32, 128], [32 * 128, 8], [1, 32]]), sl_, w=[twr])
    rb = ar.alloc([36], F32)
    trb = Trk()
    sl2 = cx.fresh()
    cx.dma("sync", rb[:, 0:4], dram_bcast(D["router_group_b"], 128, 4), sl2, w=[trb])
    cx.dma("sync", rb[:, 4:36], dram_bcast(D["router_expert_b"], 128, 32), sl2, w=[trb])
    cw = ar.alloc([NT, 32], F32)
    tcw = Trk()
    lgA = ar.alloc([NT, 36], F32)
    msk = ar.alloc([NT, 32], F32)
    eq2 = ar.alloc([NT, 32], F32)
    m8 = ar.alloc([NT, 8], F32)
    sc = ar.alloc([8, NT], F32)
    oh = ar.alloc([NT, 4], F32)
    ex = ar.alloc([NT, 4], F32)
    T = Trk()
    rbb = rb.unsqueeze(1).to_broadcast([128, 8, 36])
    for half in range(2):
        pb = k.psum[half]
        tp = k.tpsum[half]
        for ii in range(8):
            i = half * 8 + ii
            for c in range(8):
                cx.op("tensor", lambda h, pb=pb, c=c, i=i, ii=ii: h.matmul(pb[:, ii * 36:(ii + 1) * 36], xnT[:, c, i * 128:(i + 1) * 128], wr[:, c, :], start=(c == 0), stop=(c == 7)),
                      r=[t_xnT, twr], w=[tp], inc=(c == 7))
        cx.op(V, lambda h, pb=pb, half=half: h.tensor_tensor(out=lgA[:, half * 8:(half + 1) * 8, :], in0=pb[:, 0:288].rearrange("p (t e) -> p t e", t=8), in1=rbb, op=ALU.add), r=[tp, trb, T], w=[T])
    lg_g = lgA[:, :, 0:4]
    lg_e = lgA[:, :, 4:36]
    gmax, ngs, ssum, ptop, dm, w1, w2 = [sc[:, j_, :] for j_ in range(7)]

    def vop(fn):
        cx.op(V, fn, r=[T], w=[T])

    def aop(fn):
        cx.op("scalar", fn, r=[T], w=[T])
    b4 = lambda v: v.unsqueeze(2).to_broadcast([128, NT, 4])
    b32 = lambda v: v.unsqueeze(2).to_broadcast([128, NT, 32])
    vop(lambda h: h.tensor_reduce(out=gmax, in_=lg_g, axis=AX.X, op=ALU.max))
    vop(lambda h: h.tensor_tensor(out=oh, in0=lg_g, in1=b4(gmax), op=ALU.is_equal))
    vop(lambda h: h.tensor_tensor(out=ex, in0=lg_g, in1=b4(gmax), op=ALU.subtract))
    aop(lambda h: h.activation(ex, ex, AF.Exp))
    vop(lambda h: h.tensor_reduce(out=ssum, in_=ex, axis=AX.X, op=ALU.add))
    vop(lambda h: h.reciprocal(ptop, ssum))
    vop(lambda h: h.tensor_scalar(oh, oh, -1.0, 1e30, op0=ALU.add, op1=ALU.mult))
    vop(lambda h: h.tensor_tensor(out=msk.rearrange("p t (g e) -> p t g e", g=4), in0=lg_e.rearrange("p t (g e) -> p t g e", g=4),
                                  in1=oh.unsqueeze(3).to_broadcast([128, NT, 4, 8]), op=ALU.add))
    for i in range(NT):
        vop(lambda h, i=i: h.max(out=m8[:, i, :], in_=msk[:, i, :]))
    m1, m2 = m8[:, :, 0], m8[:, :, 1]
    vop(lambda h: h.tensor_tensor(out=dm, in0=m2, in1=m1, op=ALU.subtract))
    aop(lambda h: h.activation(dm, dm, AF.Exp))
    vop(lambda h: h.tensor_scalar(w1, dm, 1.0, None, op0=ALU.add))
    vop(lambda h: h.reciprocal(w1, w1))
    vop(lambda h: h.tensor_tensor(out=w2, in0=dm, in1=w1, op=ALU.mult))
    vop(lambda h: h.tensor_tensor(out=w1, in0=w1, in1=ptop, op=ALU.mult))
    vop(lambda h: h.tensor_tensor(out=w2, in0=w2, in1=ptop, op=ALU.mult))
    vop(lambda h: h.tensor_tensor(out=eq2, in0=msk, in1=b32(m2), op=ALU.is_equal))
    vop(lambda h: h.tensor_tensor(out=eq2, in0=eq2, in1=b32(w2), op=ALU.mult))
    vop(lambda h: h.tensor_tensor(out=msk, in0=msk, in1=b32(m1), op=ALU.is_equal))
    vop(lambda h: h.tensor_tensor(out=msk, in0=msk, in1=b32(w1), op=ALU.mult))
    cx.op(V, lambda h: h.tensor_tensor(out=cw, in0=msk, in1=eq2, op=ALU.add), r=[T], w=[tcw, T])
    k.dbg_add("moe_cw", cw, [tcw])
    wgu = [ar.alloc([8, 512], BF16) for _ in range(2)]
    wd = [ar.alloc([2, 1024], BF16) for _ in range(2)]
    twe = [Trk(), Trk()]
    swe = [cx.slot("we0"), cx.slot("we1")]
    sg = [ar.alloc([512], F32) for _ in range(2)]
    tsg = [Trk(), Trk()]
    h1 = [ar.alloc([2, 512], BF16) for _ in range(2)]
    th1 = [Trk(), Trk()]
    NE = k.n_experts

    def load_e(e):
        b = e % 2
        g_, u_, d_ = D["moe_w_gate"], D["moe_w_up"], D["moe_w_down"]
        cx.dma("gpsimd", wgu[b][:, :, 0:256], bass.AP(g_.tensor, g_.offset + e * 1024 * 256, [[256, 128], [256 * 128, 8], [1, 256]]), swe[b], w=[twe[b]])
        cx.dma("gpsimd", wgu[b][:, :, 256:512], bass.AP(u_.tensor, u_.offset + e * 1024 * 256, [[256, 128], [256 * 128, 8], [1, 256]]), swe[b], w=[twe[b]])
        cx.dma("gpsimd", wd[b], bass.AP(d_.tensor, d_.offset + e * 256 * 1024, [[1024, 128], [1024 * 128, 2], [1, 1024]]), swe[b], w=[twe[b]])
    import os
    NOLOAD = os.environ.get("MOE_NOLOAD", "") == "1"
    load_e(0)
    if NE > 1:
        load_e(1)
    jobs = [(e, n) for e in range(NE) for n in range(4)]
    state = {"cnt": 0, "loaded": 0}
    cx.barrier()
    k.txh = [[Trk(), Trk()] for _ in range(NT)]

    def emit_gu(j, fh):
        e, n = jobs[j]
        b = e % 2
        ts = slice(n * 512, (n + 1) * 512)
        hb = j % 2
        pg = k.psum[fh * 2]
        tpg = k.tpsum[fh * 2]
        pu = k.psum[fh * 2 + 1]
        tpu = k.tpsum[fh * 2 + 1]
        for c in range(8):
            cx.op("tensor", lambda h, pg=pg, c=c, fh=fh, ts=ts, b=b: h.matmul(pg[:, :], wgu[b][:, c, fh * 128:(fh + 1) * 128], xnT[:, c, ts], start=(c == 0), stop=(c == 7)),
                  r=[twe[b], t_xnT], w=[tpg], inc=(c == 7))
        for c in range(8):
            cx.op("tensor", lambda h, pu=pu, c=c, fh=fh, ts=ts, b=b: h.matmul(pu[:, :], wgu[b][:, c, 256 + fh * 128:256 + (fh + 1) * 128], xnT[:, c, ts], start=(c == 0), stop=(c == 7)),
                  r=[twe[b], t_xnT], w=[tpu], inc=(c == 7))
        cx.op("scalar", lambda h, pg=pg, fh=fh: h.activation(sg[fh], pg[:, :], AF.Silu), r=[tpg], w=[tsg[fh]])
        cx.op(V, lambda h, pu=pu, fh=fh, hb=hb: h.tensor_tensor(out=h1[hb][:, fh, :], in0=pu[:, :], in1=sg[fh], op=ALU.mult), r=[tpu, tsg[fh]], w=[th1[hb]])

    def emit_down(j):
        e, n = jobs[j]
        b = e % 2
        hb = j % 2
        for t in range(4):
            i = n * 4 + t
            for half in range(2):
                pdn = k.psum[4 + state["cnt"] % 4]
                tpd = k.tpsum[4 + state["cnt"] % 4]
                state["cnt"] += 1
                for fh in range(2):
                    cx.op("tensor", lambda h, pdn=pdn, fh=fh, t=t, half=half, hb=hb, b=b: h.matmul(pdn[:, :], h1[hb][:, fh, t * 128:(t + 1) * 128], wd[b][:, fh, half * 512:(half + 1) * 512], start=(fh == 0), stop=(fh == 1)),
                          r=[th1[hb], twe[b]], w=[tpd], inc=(fh == 1))
                xs = k.xacc[:, i, half * 512:(half + 1) * 512]
                cwc = cw[:, i, e:e + 1]
                cx.op(V, lambda h, pdn=pdn, xs=xs, cwc=cwc: h.scalar_tensor_tensor(out=xs, in0=pdn[:, :], scalar=cwc, in1=xs, op0=ALU.mult, op1=ALU.add), r=[tpd, tcw, k.txh[i][half]], w=[k.txh[i][half]])
        if n == 3 and e + 2 < NE and not NOLOAD:
            load_e(e + 2)
    nj = len(jobs)
    if nj > 0:
        emit_gu(0, 0)
        emit_gu(0, 1)
        for j in range(nj):
            if j + 1 < nj:
                emit_gu(j + 1, 0)
            emit_down(j)
            if j + 1 < nj:
                emit_gu(j + 1, 1)
    cx.barrier()
    ar.release(m0)


def final_norm(k, out):
    cx, ar, D = k.cx, k.ar, k.D
    V = "vector"
    m0 = ar.mark()
    gB = ar.alloc([1024], F32)
    tg = Trk()
    cx.dma("sync", gB, dram_bcast(D["norm_final"], 128, 1024), cx.fresh(), w=[tg])
    junk = ar.alloc([1024], BF16)
    tj = Trk()
    ss = ar.alloc([NT, 2], F32)
    tss = Trk()
    ob = [ar.alloc([1024], F32) for _ in range(2)]
    tob = [Trk(), Trk()]
    so = [cx.slot("o0"), cx.slot("o1")]
    for i in range(NT):
        txs = [k.txacc[i]] + (k.txh[i] if hasattr(k, "txh") else [])
        cx.op("scalar", lambda h, i=i: h.activation(junk, k.xacc[:, i, :], AF.Square, accum_out=ss[:, i, 0:1]), r=txs + [tss], w=[tj, tss])
    cx.op(V, lambda h: h.tensor_scalar(ss[:, :, 1:2], ss[:, :, 0:1], 1.0 / 1024, EPS, op0=ALU.mult, op1=ALU.add), r=[tss], w=[tss])
    cx.op("scalar", lambda h: h.activation(ss[:, :, 1:2], ss[:, :, 1:2], AF.Sqrt), r=[tss], w=[tss])
    cx.op(V, lambda h: h.reciprocal(ss[:, :, 1:2], ss[:, :, 1:2]), r=[tss], w=[tss])
    for i in range(NT):
        b = i % 2
        s1 = ss[:, i, 1:2]
        txs = [k.txacc[i]] + (k.txh[i] if hasattr(k, "txh") else [])
        cx.op(V, lambda h, i=i, s1=s1, b=b: h.scalar_tensor_tensor(out=ob[b], in0=k.xacc[:, i, :], scalar=s1, in1=gB, op0=ALU.mult, op1=ALU.mult), r=txs + [tss, tg], w=[tob[b]])
        cx.dma("sync", out[i * 128:(i + 1) * 128, :], ob[b], so[b], r=[tob[b]])
    cx.barrier()
    ar.release(m0)


_CACHE = {}


def kernel(**inputs):
    inp = {k_: np.asarray(v) for k_, v in inputs.items()}
    n = inp["x"].shape[0]
    maps = [host_inputs(inp, b) for b in range(n)]
    key = "full"
    if key not in _CACHE:
        shapes = {k_: (v.shape, np2dt(v)) for k_, v in maps[0].items()}
        _CACHE[key] = build(shapes)[0]
    nc = _CACHE[key]
    res = run_bass_kernel_spmd(nc, maps, core_ids=list(range(n)))
    return np.stack([np.asarray(r["out"], dtype=np.float32) for r in res.results], 0)
```

```python
import contextlib
import os
import math
import numpy as np
import ml_dtypes
import concourse.bass as bass
import concourse.mybir as mybir
from concourse.bass_utils import run_bass_kernel_spmd

F32 = mybir.dt.float32
BF16 = mybir.dt.bfloat16
F32R = mybir.dt.float32r
I32 = mybir.dt.int32
AF = mybir.ActivationFunctionType
ALU = mybir.AluOpType
AX = mybir.AxisListType

ENGS = ("sync", "scalar", "gpsimd", "vector", "tensor")
ATTACH_WAIT = os.environ.get("ATTACH_WAIT", "1") == "1"
S = 2048
DM = 1024
NT = 16
EPS = 1e-6


class Trk:
    __slots__ = ("name", "w", "r", "excl")

    def __init__(self, name="", excl=False):
        self.name = name
        self.w = None
        self.r = {}
        self.excl = excl


class DmaSlot:
    def __init__(self, ctx, name):
        self.key = "d_" + name + str(ctx.nsem)
        ctx.sems[self.key] = ctx.new_sem(self.key)
        self.total = 0


class Ctx:
    def __init__(self, nc, stack):
        self.nc = nc
        self.stack = stack
        self.q = {e: [] for e in ENGS}
        self.sems = {}
        self.nsem = 0
        self.cnt = {e: 0 for e in ENGS}
        self.known = {e: {} for e in ENGS}
        for e in ENGS:
            self.sems[e] = self.new_sem("s_" + e)
        self.slots = []
        self.pools = {}
        self.pool_idx = {}
        self.n_ops = 0

    def new_sem(self, name):
        self.nsem += 1
        return self.stack.enter_context(self.nc.semaphore(name))

    def slot(self, name):
        s = DmaSlot(self, name)
        self.slots.append(s)
        return s

    def fresh(self, kind="hw"):
        pool = self.pools.setdefault(kind, [])
        i = self.pool_idx.get(kind, 0)
        if i >= len(pool):
            assert len(pool) < 30, "slot pool exhausted"
            pool.append(self.slot(kind + "%d" % len(pool)))
            pool[-1].kind = kind
        self.pool_idx[kind] = i + 1
        return pool[i]

    def sb(self, name, shape, dt):
        return self.stack.enter_context(self.nc.sbuf_tensor("sb_" + name, list(shape), dt))

    def ps(self, name, shape, dt=F32):
        return self.stack.enter_context(self.nc.psum_tensor(name, list(shape), dt))

    def _waits_for(self, eng, r, w, extra=()):
        need = {}

        def req(dep):
            if dep is None:
                return
            k, c = dep
            if k == eng and eng in ("tensor", "sync"):
                return
            if c > need.get(k, 0):
                need[k] = c
        for t in r:
            req(t.w)
        for t in w:
            req(t.w)
            for k, c in t.r.items():
                req((k, c))
        for d in extra:
            req(d)
        out = []
        kn = self.known[eng]
        for k, c in need.items():
            if kn.get(k, 0) < c:
                kn[k] = c
                out.append((self.sems[k], c))
        return out

    def op(self, eng, fn, r=(), w=(), inc=True, extra=()):
        w = list(w) + [t for t in r if t.excl]
        r = [t for t in r if not t.excl]
        waits = self._waits_for(eng, r, w, extra)
        c = self.cnt[eng] + 1
        if inc:
            self.cnt[eng] = c
        sem = self.sems[eng]

        def emit(h, fn=fn, waits=waits, inc=inc, sem=sem):
            for s, v in waits[:-1]:
                h.wait_ge(s, v)
            ins = fn(h)
            if waits:
                if ATTACH_WAIT:
                    ins._wait_ge(waits[-1][0], waits[-1][1])
                else:
                    raise RuntimeError
            if inc:
                ins.then_inc(sem, 1)
        if not ATTACH_WAIT:
            def emit(h, fn=fn, waits=waits, inc=inc, sem=sem):
                for s, v in waits:
                    h.wait_ge(s, v)
                ins = fn(h)
                if inc:
                    ins.then_inc(sem, 1)
        self.q[eng].append(emit)
        for t in r:
            t.r[eng] = c
        for t in w:
            t.w = (eng, c)
            t.r = {}
        self.n_ops += 1

    def dma(self, eng, out, in_, slot, r=(), w=(), extra=(), **kw):
        kind = "sw" if eng == "gpsimd" else "hw"
        assert getattr(slot, "kind", kind) == kind, ("DMA slot kind mismatch", slot.key, eng)
        slot.kind = kind
        waits = self._waits_for(eng, r, w, extra)
        slot.total += 16
        sem = self.sems[slot.key]

        def emit(h, waits=waits, sem=sem, out=out, in_=in_, kw=kw):
            for s, v in waits:
                h.wait_ge(s, v)
            h.dma_start(out=out, in_=in_, **kw).then_inc(sem, 16)
        self.q[eng].append(emit)
        dep = (slot.key, slot.total)
        for t in r:
            t.r[slot.key] = slot.total
        for t in w:
            t.w = dep
            t.r = {}
        self.n_ops += 1
        return dep

    def wait_deps(self, eng, deps):
        waits = self._waits_for(eng, (), (), deps)

        def emit(h, waits=waits):
            for s, v in waits:
                h.wait_ge(s, v)
        self.q[eng].append(emit)

    def barrier(self):
        deps = [(e, self.cnt[e]) for e in ENGS if e != "sync" and self.cnt[e] > 0]
        deps += [(s.key, s.total) for s in self.slots if s.total > 0]
        for e in ENGS:
            self.wait_deps(e, deps)
        self.pool_idx = {}

    def emit_all(self, block):
        q = self.q

        @block.sync
        def _(h):
            for f in q["sync"]:
                f(h)

        @block.scalar
        def _(h):
            for f in q["scalar"]:
                f(h)

        @block.gpsimd
        def _(h):
            for f in q["gpsimd"]:
                f(h)

        @block.vector
        def _(h):
            for f in q["vector"]:
                f(h)

        @block.tensor
        def _(h):
            for f in q["tensor"]:
                f(h)


class Arena:
    def __init__(self, cx, words, base=None):
        self.t = cx.sb("arena", [128, words], F32) if base is None else base
        self.cx = cx
        self.words = words
        self.top = 0

    def mark(self):
        return self.top

    def release(self, m):
        if m != self.top:
            self.cx.barrier()
        self.top = m

    def alloc(self, shape, dt):
        n = int(np.prod(shape))
        w = n if dt in (F32, F32R, I32) else (n + 1) // 2
        w = (w + 1) // 2 * 2
        o = self.top
        self.top += w
        assert self.top <= self.words, ("arena overflow", self.top, self.words)
        v = self.t[:, o:o + w]
        if dt != F32:
            v = v.bitcast(dt)
        v = v[:, 0:n]
        if len(shape) > 1:
            names = " ".join("d%d" % i for i in range(len(shape)))
            v = v.rearrange("p (%s) -> p %s" % (names, names), **{"d%d" % i: shape[i] for i in range(len(shape))})
        return v


def pap(ap, part0, nparts, off, dims):
    base = ap.ap[0][0]
    return bass.AP(ap.tensor, ap.offset + part0 * base + off, [[base, nparts]] + [list(d) for d in dims])


def host_consts():
    c = {}
    c["ident"] = np.eye(128, dtype=np.float32)
    c["identb"] = np.eye(128, dtype=np.float32).astype(ml_dtypes.bfloat16)
    c["ones"] = np.ones((128, 128), np.float32)
    selT = np.zeros((128, 2, 8, 128), np.float32)
    selB = np.zeros((128, 2, 8, 128), np.float32)
    for q in range(4):
        for r in range(32):
            loc, cc = r // 16, r % 16
            for s in range(8):
                selT[q * 32 + r, loc, s, s * 16 + cc] = 1.0
                selB[q * 32 + r, loc, s, s * 16 + cc] = 1.0
    c["selT"] = selT.astype(ml_dtypes.bfloat16)
    c["selB"] = selB.astype(ml_dtypes.bfloat16)
    sidx = np.arange(128) // 16
    c["s5mf"] = (sidx[None, :] >= sidx[:, None]).astype(np.float32)
    c["s5mb"] = (sidx[None, :] <= sidx[:, None]).astype(np.float32)
    c["kvec"] = np.tile((np.arange(16, dtype=np.float32) - 7.0)[None, :], (128, 1))
    k = np.arange(128)[:, None]
    cc = np.arange(128)[None, :]
    same = (k // 64) == (cc // 64)
    gm = np.zeros((128, 8, 128), np.float32)
    gm[:, 0] = same & (k <= cc)
    gm[:, 1] = same & (k >= cc)
    gm[:, 2] = np.where(same & (cc >= k), 0.0, -30000.0)
    gm[:, 3] = np.where(same & (cc <= k), 0.0, -30000.0)
    gm[:, 4] = same & (cc > k)
    gm[:, 5] = same & (cc < k)
    gm[:, 6] = same
    c["gmask"] = gm
    return c


def host_s5(inp):
    o = {}

    pairs = {"lam_re": ("s5_lam_re_f", "s5_lam_re_b"), "lam_im": ("s5_lam_im_f", "s5_lam_im_b"),
             "log_step": ("s5_log_step_f", "s5_log_step_b"), "b_re": ("s5_b_re_f", "s5_b_re_b"),
             "b_im": ("s5_b_im_f", "s5_b_im_b"), "c_re": ("s5_c_re_f", "s5_c_re_b"), "c_im": ("s5_c_im_f", "s5_c_im_b")}

    def st(nm):
        f_, b_ = pairs[nm]
        return np.stack([inp[f_][0], inp[b_][0]], 0)
    lam = np.stack([st("lam_re"), st("lam_im")], 0)
    lam = lam.reshape(2, 2, 2, 16, 64).transpose(2, 4, 0, 1, 3)
    o["s5_lam"] = np.ascontiguousarray(lam.reshape(128, 2, 32))
    ls = st("log_step").reshape(2, 2, 16)
    ls = np.broadcast_to(ls.transpose(1, 0, 2)[:, None], (2, 64, 2, 16))
    o["s5_step"] = np.ascontiguousarray(ls.reshape(128, 32))
    b = np.stack([st("b_re"), st("b_im")], 0)
    b = b.reshape(2, 2, 2, 16, 64, 16).transpose(2, 4, 0, 1, 3, 5)
    o["s5_b"] = np.ascontiguousarray(b.reshape(128, 2, 512))
    cm = np.stack([st("c_re"), st("c_im")], 0)
    cm = cm.reshape(2, 2, 2, 16, 16, 64).transpose(2, 5, 0, 1, 3, 4)
    o["s5_c"] = np.ascontiguousarray(cm.reshape(128, 2, 512))
    d = inp["s5_d"][0].reshape(32, 16)
    o["s5_dvec"] = np.ascontiguousarray(np.broadcast_to(d.T[None], (8, 16, 32)).reshape(128, 32))
    o["s5_bglu"] = np.ascontiguousarray(inp["s5_b_glu"][0].reshape(4, 128).T)
    o["s5_normw"] = np.ascontiguousarray(inp["s5_norm"][0].reshape(4, 128).T)
    return o


def np2dt(a):
    if a.dtype == np.float32:
        return F32
    if a.dtype == ml_dtypes.bfloat16:
        return BF16
    raise ValueError(a.dtype)


class K:
    pass


def dram_bcast(ap, nparts, n, off=0):
    return bass.AP(ap.tensor, ap.offset + off, [[0, nparts], [1, n]])


def norm_transpose(k, name, src_fn, ntiles, gain_dram, outT, out_dt, outT_trk, resident=False):
    cx, ar = k.cx, k.ar
    m = ar.mark()
    gB = ar.alloc([1024], F32)
    tg = Trk()
    cx.dma("sync", gB, dram_bcast(gain_dram, 128, 1024), cx.fresh(), w=[tg])
    junk = ar.alloc([1024], BF16)
    tj = Trk()
    xn = [ar.alloc([1024], out_dt) for _ in range(2)]
    txn = [Trk(), Trk()]
    ss = ar.alloc([NT * 2, 1], F32)
    tss = [Trk() for _ in range(ntiles)]
    pdt = BF16 if out_dt == BF16 else F32
    ident = k.identb if out_dt == BF16 else k.ident
    srcs = []
    if resident:
        for i in range(ntiles):
            src, ts = src_fn(i)
            srcs.append((src, ts))
            cx.op("scalar", lambda h, src=src, i=i: h.activation(junk, src, AF.Square, accum_out=ss[:, 2 * i:2 * i + 1]), r=[ts], w=[tj, tss[0]])
        ssv = ss.rearrange("p (t two) one -> p t (two one)", two=2)
        cx.op("vector", lambda h: h.tensor_scalar(ssv[:, 0:ntiles, 1:2], ssv[:, 0:ntiles, 0:1], 1.0 / 1024, EPS, op0=ALU.mult, op1=ALU.add), r=[tss[0]], w=[tss[0]])
        cx.op("scalar", lambda h: h.activation(ssv[:, 0:ntiles, 1:2], ssv[:, 0:ntiles, 1:2], AF.Sqrt), r=[tss[0]], w=[tss[0]])
        cx.op("vector", lambda h: h.reciprocal(ssv[:, 0:ntiles, 1:2], ssv[:, 0:ntiles, 1:2]), r=[tss[0]], w=[tss[0]])
    for i in range(ntiles):
        rsi = ss[:, 2 * i + 1:2 * i + 2]
        if resident:
            src, ts = srcs[i]
            tsi = tss[0]
        else:
            src, ts = src_fn(i)
            ssi = ss[:, 2 * i:2 * i + 1]
            tsi = tss[i]
            cx.op("scalar", lambda h, src=src, ssi=ssi: h.activation(junk, src, AF.Square, accum_out=ssi), r=[ts], w=[tj, tss[i]])
            cx.op("vector", lambda h, ssi=ssi, rsi=rsi: h.tensor_scalar(rsi, ssi, 1.0 / 1024, EPS, op0=ALU.mult, op1=ALU.add), r=[tss[i]], w=[tss[i]])
            cx.op("scalar", lambda h, rsi=rsi: h.activation(rsi, rsi, AF.Sqrt), r=[tss[i]], w=[tss[i]])
            cx.op("vector", lambda h, rsi=rsi: h.reciprocal(rsi, rsi), r=[tss[i]], w=[tss[i]])
        b = i % 2
        cx.op("vector", lambda h, src=src, rsi=rsi, b=b: h.scalar_tensor_tensor(out=xn[b], in0=src, scalar=rsi, in1=gB, op0=ALU.mult, op1=ALU.mult),
              r=[ts, tsi, tg], w=[txn[b]])
        if out_dt == BF16:
            pb = k.psum[i % 2]
            tp = k.tpsum[i % 2]
            pv = pb.bitcast(BF16)
            for c in range(8):
                cx.op("tensor", lambda h, b=b, c=c, pv=pv: h.transpose(pv[:, c * 128:(c + 1) * 128], xn[b][:, c * 128:(c + 1) * 128], ident),
                      r=[txn[b]], w=[tp], inc=(c == 7))
            dst = outT[:, :, i * 128:(i + 1) * 128]
            eng = "scalar" if i % 2 == 0 else "vector"
            if eng == "scalar":
                cx.op(eng, lambda h, dst=dst, pv=pv: h.copy(dst, pv.rearrange("p (c t) -> p c t", c=8)), r=[tp], w=[outT_trk])
            else:
                cx.op(eng, lambda h, dst=dst, pv=pv: h.tensor_copy(dst, pv.rearrange("p (c t) -> p c t", c=8)), r=[tp], w=[outT_trk])
        else:
            for half in range(2):
                pb = k.psum[(2 * i + half) % 4]
                tp = k.tpsum[(2 * i + half) % 4]
                for c4 in range(4):
                    c = half * 4 + c4
                    cx.op("tensor", lambda h, b=b, c=c, c4=c4, pb=pb: h.transpose(pb[:, c4 * 128:(c4 + 1) * 128], xn[b][:, c * 128:(c + 1) * 128].bitcast(F32), ident),
                          r=[txn[b]], w=[tp], inc=(c4 == 3))
                dst = outT[:, half * 4:(half + 1) * 4, i * 128:(i + 1) * 128]
                if half == 0:
                    cx.op("scalar", lambda h, dst=dst, pb=pb: h.copy(dst, pb.rearrange("p (c t) -> p c t", c=4)), r=[tp], w=[outT_trk])
                else:
                    cx.op("vector", lambda h, dst=dst, pb=pb: h.tensor_copy(dst, pb.rearrange("p (c t) -> p c t", c=4)), r=[tp], w=[outT_trk])
    ar.release(m)


def s5_prep(k):
    cx, ar, D = k.cx, k.ar, k.D
    V = "vector"
    m0 = ar.mark()
    lam = ar.alloc([2, 32], F32)
    step = ar.alloc([32], F32)
    bb = ar.alloc([2, 512], F32)
    cc = ar.alloc([2, 512], F32)
    kvec = ar.alloc([16], F32)
    tl = Trk()
    sl_ = cx.fresh()
    for dst, nm in ((lam, "s5_lam"), (step, "s5_step"), (bb, "s5_b"), (cc, "s5_c"), (kvec, "kvec")):
        cx.dma("sync", dst, D[nm], sl_, w=[tl])
    T = Trk()

    def vop(fn, extra_r=()):
        cx.op(V, fn, r=[T, tl] + list(extra_r), w=[T])

    def aop(fn):
        cx.op("scalar", fn, r=[T, tl], w=[T])
    lre, lim = lam[:, 0, :], lam[:, 1, :]
    dl = ar.alloc([32], F32)
    re1 = ar.alloc([32], F32)
    im1 = ar.alloc([32], F32)
    aop(lambda h: h.activation(dl, step, AF.Exp))
    vop(lambda h: h.tensor_tensor(out=re1, in0=dl, in1=lre, op=ALU.mult))
    vop(lambda h: h.tensor_tensor(out=im1, in0=dl, in1=lim, op=ALU.mult))
    PWI = ar.alloc([16, 32], F32)
    PWR = ar.alloc([16, 32], F32)
    m_pw = ar.mark()
    KR = ar.alloc([16, 32], F32)
    KI = ar.alloc([16, 32], F32)
    kv_b = kvec.unsqueeze(2).to_broadcast([128, 16, 32])
    vop(lambda h: h.tensor_tensor(out=KR, in0=kv_b, in1=re1.unsqueeze(1).to_broadcast([128, 16, 32]), op=ALU.mult))
    vop(lambda h: h.tensor_tensor(out=KI, in0=kv_b, in1=im1.unsqueeze(1).to_broadcast([128, 16, 32]), op=ALU.mult))
    MAG = ar.alloc([16, 32], F32)
    aop(lambda h: h.activation(MAG, KR, AF.Exp))
    YI = ar.alloc([16, 32], I32)
    YF = ar.alloc([16, 32], F32)
    vop(lambda h: h.tensor_scalar(KI, KI, 1.0 / (2 * math.pi), None, op0=ALU.mult))
    vop(lambda h: h.tensor_copy(YI, KI))
    vop(lambda h: h.tensor_copy(YF, YI))
    vop(lambda h: h.tensor_tensor(out=KI, in0=KI, in1=YF, op=ALU.subtract))
    SH_ = ar.alloc([16, 32], F32)
    SQ_ = ar.alloc([16, 32], F32)
    aop(lambda h: h.activation(SH_, KI, AF.Sin, scale=math.pi))
    aop(lambda h: h.activation(SQ_, KI, AF.Sin, scale=math.pi / 2))
    CH_ = ar.alloc([16, 32], F32)
    vop(lambda h: h.tensor_tensor(out=CH_, in0=SQ_, in1=SQ_, op=ALU.mult))
    vop(lambda h: h.tensor_scalar(CH_, CH_, -2.0, 1.0, op0=ALU.mult, op1=ALU.add))
    vop(lambda h: h.tensor_tensor(out=PWI, in0=SH_, in1=CH_, op=ALU.mult))
    vop(lambda h: h.scalar_tensor_tensor(out=PWI, in0=PWI, scalar=2.0, in1=MAG, op0=ALU.mult, op1=ALU.mult))
    vop(lambda h: h.tensor_tensor(out=PWR, in0=SH_, in1=SH_, op=ALU.mult))
    vop(lambda h: h.tensor_scalar(PWR, PWR, -2.0, 1.0, op0=ALU.mult, op1=ALU.add))
    vop(lambda h: h.tensor_tensor(out=PWR, in0=PWR, in1=MAG, op=ALU.mult))
    ar.release(m_pw)
    lrm1 = ar.alloc([32], F32)
    li = PWI[:, 8, :]
    t1 = ar.alloc([32], F32)
    t2 = ar.alloc([32], F32)
    den = ar.alloc([32], F32)
    c0r = ar.alloc([32], F32)
    c0i = ar.alloc([32], F32)
    vop(lambda h: h.tensor_scalar(lrm1, PWR[:, 8, :], -1.0, None, op0=ALU.add))
    vop(lambda h: h.tensor_tensor(out=t1, in0=lre, in1=lre, op=ALU.mult))
    vop(lambda h: h.tensor_tensor(out=t2, in0=lim, in1=lim, op=ALU.mult))
    vop(lambda h: h.tensor_tensor(out=den, in0=t1, in1=t2, op=ALU.add))
    vop(lambda h: h.reciprocal(den, den))
    vop(lambda h: h.tensor_tensor(out=t1, in0=lrm1, in1=lre, op=ALU.mult))
    vop(lambda h: h.tensor_tensor(out=t2, in0=li, in1=lim, op=ALU.mult))
    vop(lambda h: h.tensor_tensor(out=t1, in0=t1, in1=t2, op=ALU.add))
    vop(lambda h: h.tensor_tensor(out=c0r, in0=t1, in1=den, op=ALU.mult))
    vop(lambda h: h.tensor_tensor(out=t1, in0=li, in1=lre, op=ALU.mult))
    vop(lambda h: h.tensor_tensor(out=t2, in0=lrm1, in1=lim, op=ALU.mult))
    vop(lambda h: h.tensor_tensor(out=t1, in0=t1, in1=t2, op=ALU.subtract))
    vop(lambda h: h.tensor_tensor(out=c0i, in0=t1, in1=den, op=ALU.mult))
    BBR = ar.alloc([32, 16], F32)
    BBI = ar.alloc([32, 16], F32)
    TA = ar.alloc([32, 16], F32)
    br = bb[:, 0, :].rearrange("p (a c) -> p a c", c=16)
    bi = bb[:, 1, :].rearrange("p (a c) -> p a c", c=16)
    c0r_b = c0r.unsqueeze(2).to_broadcast([128, 32, 16])
    c0i_b = c0i.unsqueeze(2).to_broadcast([128, 32, 16])
    vop(lambda h: h.tensor_tensor(out=BBR, in0=br, in1=c0r_b, op=ALU.mult))
    vop(lambda h: h.tensor_tensor(out=TA, in0=bi, in1=c0i_b, op=ALU.mult))
    vop(lambda h: h.tensor_tensor(out=BBR, in0=BBR, in1=TA, op=ALU.subtract))
    vop(lambda h: h.tensor_tensor(out=BBI, in0=bi, in1=c0r_b, op=ALU.mult))
    vop(lambda h: h.tensor_tensor(out=TA, in0=br, in1=c0i_b, op=ALU.mult))
    vop(lambda h: h.tensor_tensor(out=BBI, in0=BBI, in1=TA, op=ALU.add))
    ASd = ar.alloc([16, 2, 8, 16], F32)
    CS2d = ar.alloc([16, 2, 8, 16], F32)
    T1 = ar.alloc([8, 16, 16], F32)
    T2 = ar.alloc([8, 16, 16], F32)
    cr = cc[:, 0, :].rearrange("p (d a c) -> p d a c", d=2, c=16)
    ci = cc[:, 1, :].rearrange("p (d a c) -> p d a c", d=2, c=16)
    BBR4 = BBR.rearrange("p (d a) c -> p d a c", d=2)
    BBI4 = BBI.rearrange("p (d a) c -> p d a c", d=2)

    def pw(arr, d, k0, kstep):
        return pap(arr, 0, 128, k0 * 32 + d * 16, [[kstep * 32, 8], [1, 16], [0, 16]])

    def dst(arr, dofs, ri):
        return pap(arr, 0, 128, dofs * 4096 + ri * 128, [[16, 8], [256, 16], [1, 16]])

    def vec(v4, d):
        a_ = v4[:, d]
        return bass.AP(a_.tensor, a_.offset, [list(a_.ap[0]), [0, 8], list(a_.ap[1]), list(a_.ap[2])])

    T1f = T1.rearrange("p a b c -> p (a b c)")

    def cmul(out_arr, dofs, d, k0, kstep, vr, vi, neg_im):
        pr, pi_ = pw(PWR, d, k0, kstep), pw(PWI, d, k0, kstep)
        vop(lambda h: h.tensor_tensor(out=T1, in0=pr, in1=vec(vr, d), op=ALU.mult))
        vop(lambda h: h.tensor_tensor(out=T2, in0=pi_, in1=vec(vi, d), op=ALU.mult))
        vop(lambda h: h.tensor_tensor(out=dst(out_arr, dofs, 0), in0=T1, in1=T2, op=ALU.subtract))
        vop(lambda h: h.tensor_tensor(out=T1, in0=pr, in1=vec(vi, d), op=ALU.mult))
        vop(lambda h: h.tensor_tensor(out=T2, in0=pi_, in1=vec(vr, d), op=ALU.mult))
        if neg_im:
            vop(lambda h: h.tensor_scalar(T1f, T1f, -1.0, None, op0=ALU.mult))
            vop(lambda h: h.tensor_tensor(out=dst(out_arr, dofs, 1), in0=T1, in1=T2, op=ALU.subtract))
        else:
            vop(lambda h: h.tensor_tensor(out=dst(out_arr, dofs, 1), in0=T1, in1=T2, op=ALU.add))
    vop(lambda h: h.tensor_copy(k.s5A1[:, 0:32], PWR[:, 15, :]))
    vop(lambda h: h.tensor_copy(k.s5A1[:, 32:64], PWR[:, 15, :]))
    vop(lambda h: h.tensor_scalar(k.s5A2[:, 0:32], PWI[:, 15, :], -1.0, None, op0=ALU.mult))
    vop(lambda h: h.tensor_copy(k.s5A2[:, 32:64], PWI[:, 15, :]))
    cmul(k.s5CS, 0, 0, 8, 1, cr, ci, True)
    cmul(k.s5CS, 1, 1, 15, -1, cr, ci, True)
    k.t_s5w = T
    mf = ar.alloc([2, 128], F32)
    dv = ar.alloc([32], F32)
    tm = Trk()
    sl_ = cx.fresh()
    cx.dma("sync", mf[:, 0, :], D["s5mf"], sl_, w=[tm])
    cx.dma("sync", mf[:, 1, :], D["s5mb"], sl_, w=[tm])
    cx.dma("sync", dv, D["s5_dvec"], sl_, w=[tm])
    tt1 = [ar.alloc([128], F32) for _ in range(2)]
    ttt = [Trk(), Trk()]
    ASb = ASd.rearrange("p a r s c -> p (a r) (s c)")
    ASm = ASd.rearrange("p a r s c -> p a r (s c)")
    CSm = CS2d.rearrange("p a r s c -> p a r (s c)")
    for d in range(2):
        if d == 0:
            cmul(ASd, 0, 0, 14, -1, BBR4, BBI4, False)
            cmul(CS2d, 0, 0, 0, 1, cr, ci, True)
        else:
            cmul(ASd, 0, 1, 7, 1, BBR4, BBI4, False)
            cmul(CS2d, 0, 1, 7, -1, cr, ci, True)
        for grp in range(8):
            pb = k.psum[grp % 4]
            tp = k.tpsum[grp % 4]
            for j in range(4):
                blk = grp * 4 + j
                cx.op("tensor", lambda h, pb=pb, j=j, blk=blk: h.transpose(pb[:, j * 128:(j + 1) * 128], ASb[:, blk, :], k.ident),
                      r=[T], w=[tp], inc=(j == 3))
            dstv = k.s5AT[:, d * 32 + grp * 4:d * 32 + (grp + 1) * 4, :]
            if grp % 2 == 0:
                cx.op("scalar", lambda h, dstv=dstv, pb=pb: h.copy(dstv, pb.rearrange("p (j x) -> p j x", j=4)), r=[tp], w=[k.t_s5at])
            else:
                cx.op("vector", lambda h, dstv=dstv, pb=pb: h.tensor_copy(dstv, pb.rearrange("p (j x) -> p j x", j=4)), r=[tp], w=[k.t_s5at])
        for g in range(32):
            gh, gl = g // 16, g % 16
            pb = k.psum[4 + g % 4]
            tp = k.tpsum[4 + g % 4]
            for ri in range(2):
                cx.op("tensor", lambda h, pb=pb, ri=ri, gh=gh, gl=gl: h.matmul(
                    pb[:, 0:128], ASm[gh * 64:(gh + 1) * 64, gl, ri, :], CSm[gh * 64:(gh + 1) * 64, gl, ri, :],
                    start=(ri == 0), stop=(ri == 1)), r=[T], w=[tp], inc=(ri == 1))
            b_ = g % 2
            cx.op(V, lambda h, pb=pb, b_=b_, d=d: h.tensor_tensor(out=tt1[b_], in0=pb[:, 0:128], in1=mf[:, d, :], op=ALU.mult), r=[tp, tm], w=[ttt[b_]])
            if d == 0:
                cx.op(V, lambda h, b_=b_, g=g: h.scalar_tensor_tensor(out=k.s5TT[:, g, :], in0=k.ident, scalar=dv[:, g:g + 1], in1=tt1[b_], op0=ALU.mult, op1=ALU.add),
                      r=[ttt[b_], tm], w=[k.t_s5tt])
            else:
                cx.op(V, lambda h, b_=b_, g=g: h.tensor_tensor(out=k.s5TT[:, g, :], in0=k.s5TT[:, g, :], in1=tt1[b_], op=ALU.add),
                      r=[ttt[b_]], w=[k.t_s5tt])
    cx.barrier()
    ar.release(m0)


def load_w_cols(k, wdram, col0, ncols, dst, trk, slot, eng="gpsimd"):
    src = bass.AP(wdram.tensor, wdram.offset + col0, [[wdram.ap[0][0] * 1, 128], [wdram.ap[0][0] * 128, 8], [1, ncols]])
    return k.cx.dma(eng, dst, src, slot, w=[trk])


def proj_fm(k, wt, wtrk, consume):
    cx = k.cx
    for n in range(4):
        pb = k.psum[n % 2 + 2]
        tp = k.tpsum[n % 2 + 2]
        for c in range(8):
            cx.op("tensor", lambda h, pb=pb, c=c, n=n: h.matmul(pb[:, :], wt[:, c, :], k.hT[:, c, n * 512:(n + 1) * 512], start=(c == 0), stop=(c == 7)),
                  r=[wtrk, k.t_hT], w=[tp], inc=(c == 7))
        consume(n, pb, tp)


def s5_build_U(k):
    cx, ar = k.cx, k.ar
    m0 = ar.mark()
    wt = [ar.alloc([8, 128], BF16) for _ in range(2)]
    twt = [Trk(), Trk()]
    swt = [cx.fresh('sw'), cx.fresh('sw')]
    uT = [ar.alloc([2048], BF16) for _ in range(2)]
    tuT = [Trk(), Trk()]
    for ct in range(4):
        b = ct % 2
        load_w_cols(k, k.D["w_in"], ct * 128, 128, wt[b], twt[b], swt[b])

        def consume(n, pb, tp, b=b):
            if n % 2 == 0:
                cx.op("scalar", lambda h: h.copy(uT[b][:, n * 512:(n + 1) * 512], pb[:, :]), r=[tp], w=[tuT[b]])
            else:
                cx.op("vector", lambda h: h.tensor_copy(uT[b][:, n * 512:(n + 1) * 512], pb[:, :]), r=[tp], w=[tuT[b]])
        proj_fm(k, wt[b], twt[b], consume)
        for gi in range(8):
            g = ct * 8 + gi
            q0 = 32 * (gi // 2)
            pb = k.psum[4 + gi % 4]
            tp = k.tpsum[4 + gi % 4]
            for s in range(8):
                rhs = pap(uT[b], q0, 32, s, [[8, 256]])
                cx.op("tensor", lambda h, pb=pb, s=s, rhs=rhs, q0=q0, gi=gi: h.matmul(pb[:, 0:256], k.selT[q0:q0 + 32, gi % 2, s, :], rhs, start=(s == 0), stop=(s == 7), tile_position=(q0, 0)),
                      r=[tuT[b]], w=[tp], inc=(s == 7))
            if gi % 2 == 0:
                cx.op("scalar", lambda h, pb=pb, g=g: h.copy(k.s5U[:, g, :], pb[:, 0:256]), r=[tp], w=[k.t_s5U])
            else:
                cx.op("vector", lambda h, pb=pb, g=g: h.tensor_copy(k.s5U[:, g, :], pb[:, 0:256]), r=[tp], w=[k.t_s5U])
    cx.barrier()
    ar.release(m0)


def s5_main(k, yT, t_yT):
    cx, ar, D = k.cx, k.ar, k.D
    V = "vector"
    m0 = ar.mark()
    SH = ar.alloc([2, 257, 2, 16], BF16)
    tSH = Trk()
    tSHh = Trk()
    X = [ar.alloc([64], F32) for _ in range(3)]
    tX = [Trk() for _ in range(3)]
    t1 = ar.alloc([64], F32)
    t2 = ar.alloc([64], F32)
    tt = Trk()
    tt2 = Trk()
    cx.op("gpsimd", lambda h: h.memset(SH[:, 0, 0, :, :], 0.0), w=[tSH])
    cx.op("gpsimd", lambda h: h.memset(SH[:, 1, 256, :, :], 0.0), w=[tSH])
    cx.op("gpsimd", lambda h: h.memset(X[0], 0.0), w=[tX[0]])
    n = 0
    for gl in range(16):
        for d in range(2):
            for ri in range(2):
                blk = d * 32 + gl * 2 + ri
                pb = k.psum[n % 4]
                tp = k.tpsum[n % 4]
                cx.op("tensor", lambda h, pb=pb, blk=blk, gl=gl: h.matmul(pb[0:64, 0:256], k.s5AT[:, blk, 0:64], k.s5U[:, gl, :], start=True, stop=True),
                      r=[k.t_s5at, k.t_s5U], w=[tp], inc=False)
                cx.op("tensor", lambda h, pb=pb, blk=blk, gl=gl: h.matmul(pb[64:128, 0:256], k.s5AT[:, blk, 64:128], k.s5U[:, 16 + gl, :], start=True, stop=True),
                      r=[k.t_s5at, k.t_s5U], w=[tp])
                slot0 = 1 if d == 0 else 0
                dstv = pap(SH, 0, 128, d * 257 * 32 + slot0 * 32 + ri * 16 + gl, [[32, 256]])
                if n % 2 == 0:
                    cx.op("scalar", lambda h, dstv=dstv, pb=pb: h.copy(dstv, pb[:, 0:256]), r=[tp], w=[tSH])
                else:
                    cx.op(V, lambda h, dstv=dstv, pb=pb: h.tensor_copy(dstv, pb[:, 0:256]), r=[tp], w=[tSH])
                n += 1
    import os
    S5STOP = os.environ.get('S5_STOP', '')
    if S5STOP == 'a':
        cx.barrier(); ar.release(m0); return
    for i in range(256):
        xp, xn = X[i % 3], X[(i + 1) % 3]
        txp, txn = tX[i % 3], tX[(i + 1) % 3]
        xsw = pap(xp, 0, 128, 32, [[-32, 2], [1, 32]])
        bf = (i + 1) * 32
        bb_ = 257 * 32 + (255 - i) * 32
        sview = pap(SH, 0, 128, bf, [[16, 2], [bb_ - bf, 2], [1, 16]])
        xp3 = xp.rearrange("p (r x) -> p r x", r=2)
        cx.op("gpsimd", lambda h, xsw=xsw: h.tensor_tensor(out=t2.rearrange("p (r x) -> p r x", r=2), in0=k.s5A2.rearrange("p (r x) -> p r x", r=2), in1=xsw, op=ALU.mult), r=[txp, k.t_s5w], w=[tt2])
        cx.op(V, lambda h, xp=xp: h.tensor_tensor(out=t1, in0=k.s5A1, in1=xp, op=ALU.mult), r=[txp, k.t_s5w], w=[tt])
        cx.op(V, lambda h, sview=sview: h.tensor_tensor(out=t1.rearrange("p (r d x) -> p r d x", r=2, d=2), in0=t1.rearrange("p (r d x) -> p r d x", r=2, d=2), in1=sview, op=ALU.add), r=[tt, tSH], w=[tt])
        cx.op(V, lambda h, xn=xn: h.tensor_tensor(out=xn, in0=t1, in1=t2, op=ALU.add), r=[tt, tt2], w=[txn])
        cx.op("scalar", lambda h, xn=xn, sview=sview: h.copy(sview, xn.rearrange("p (r d x) -> p r d x", r=2, d=2)), r=[txn], w=[tSHh])
    if S5STOP == 'rec':
        cx.barrier(); ar.release(m0); return
    gT = ar.alloc([4, 2048], F32)
    gTb = ar.alloc([4, 2048], BF16)
    tgT = [Trk() for _ in range(4)]
    tgTb = [Trk() for _ in range(4)]
    ybuf = [k.arA.alloc([8, 256], BF16) for _ in range(2)]
    tyb = [Trk(), Trk()]
    for ct in range(4):
        b = ct % 2
        for gi in range(8):
            g = ct * 8 + gi
            gh, gl = g // 16, g % 16
            pb = k.psum[gi % 2]
            tp = k.tpsum[gi % 2]
            cx.op("tensor", lambda h, pb=pb, g=g: h.matmul(pb[:, 0:256], k.s5TT[:, g, :], k.s5U[:, g, :], start=True, stop=False),
                  r=[k.t_s5tt, k.t_s5U], w=[tp], inc=False)
            for d in range(2):
                for ri in range(2):
                    slot0 = 0 if d == 0 else 1
                    rhs = pap(SH, gh * 64, 64, d * 257 * 32 + slot0 * 32 + ri * 16 + gl, [[32, 256]])
                    last = (d == 1 and ri == 1)
                    cx.op("tensor", lambda h, pb=pb, rhs=rhs, d=d, ri=ri, gh=gh, gl=gl, last=last: h.matmul(
                        pb[:, 0:256], k.s5CS[gh * 64:(gh + 1) * 64, d, gl, ri, :], rhs, start=False, stop=last),
                        r=[tSH, tSHh, k.t_s5w], w=[tp], inc=last)
            if gi % 2 == 0:
                cx.op("scalar", lambda h, pb=pb, b=b, gi=gi: h.copy(ybuf[b][:, gi, :], pb[:, 0:256]), r=[tp], w=[tyb[b]])
            else:
                cx.op(V, lambda h, pb=pb, b=b, gi=gi: h.tensor_copy(ybuf[b][:, gi, :], pb[:, 0:256]), r=[tp], w=[tyb[b]])
        for t in range(8):
            q0 = 32 * (t // 2)
            pb = k.psum[2 + t % 4]
            tp = k.tpsum[2 + t % 4]
            for gi in range(8):
                cx.op("tensor", lambda h, pb=pb, t=t, gi=gi, q0=q0, b=b: h.matmul(pb[:, 0:256], k.selT[q0:q0 + 32, t % 2, gi, :], ybuf[b][q0:q0 + 32, gi, :], start=(gi == 0), stop=(gi == 7), tile_position=(q0, 0)),
                      r=[tyb[b]], w=[tp], inc=(gi == 7))
            dstv = pap(gT, 0, 128, ct * 2048 + t, [[8, 256]])
            cx.op("scalar", lambda h, pb=pb, dstv=dstv: h.activation(dstv, pb[:, 0:256], AF.Gelu), r=[tp], w=[tgT[ct]])
        cx.op("vector", lambda h, ct=ct: h.tensor_copy(gTb[:, ct, :], gT[:, ct, :]), r=[tgT[ct]], w=[tgTb[ct]])
    k.dbg_add("s5_g", gT, tgT)
    if S5STOP == 'c':
        cx.barrier(); ar.release(m0); return
    wg = ar.alloc([4, 512], BF16)
    twg = Trk()
    wsrc = D["s5_w_glu"]
    cx.dma("gpsimd", wg, bass.AP(wsrc.tensor, wsrc.offset, [[512, 128], [512 * 128, 4], [1, 512]]), cx.fresh('sw'), w=[twg])
    bgl = ar.alloc([4], F32)
    nw = ar.alloc([4], F32)
    tb = Trk()
    sl_ = cx.fresh()
    cx.dma("sync", bgl, D["s5_bglu"], sl_, w=[tb])
    cx.dma("sync", nw, D["s5_normw"], sl_, w=[tb])
    sig = ar.alloc([4, 512], BF16)
    tsig = Trk()
    sq = ar.alloc([4, 512], BF16)
    tsq = Trk()
    rs = ar.alloc([512], F32)
    trs = Trk()
    for nck in range(4):
        ts = slice(nck * 512, (nck + 1) * 512)
        for co in range(4):
            pb = k.psum[co % 2]
            tp = k.tpsum[co % 2]
            for ci in range(4):
                cx.op("tensor", lambda h, pb=pb, co=co, ci=ci, ts=ts: h.matmul(pb[:, :], wg[:, ci, co * 128:(co + 1) * 128], gTb[:, ci, ts], start=(ci == 0), stop=(ci == 3)),
                      r=[twg] + tgTb, w=[tp], inc=(ci == 3))
            cx.op("scalar", lambda h, pb=pb, co=co: h.activation(sig[:, co, :], pb[:, :], AF.Sigmoid, bias=bgl[:, co:co + 1]), r=[tp, tb], w=[tsig])
        for co in range(4):
            cx.op(V, lambda h, co=co, ts=ts: h.tensor_tensor(out=gT[:, co, ts], in0=gT[:, co, ts], in1=sig[:, co, :], op=ALU.mult), r=[tsig, tgT[co]], w=[tgT[co]])
            cx.op("scalar", lambda h, co=co, ts=ts: h.activation(sq[:, co, :], gT[:, co, ts], AF.Square), r=[tgT[co]], w=[tsq])
        pb = k.psum[2 + nck % 2]
        tp = k.tpsum[2 + nck % 2]
        for co in range(4):
            cx.op("tensor", lambda h, pb=pb, co=co: h.matmul(pb[:, :], k.onesb, sq[:, co, :], start=(co == 0), stop=(co == 3)), r=[tsq], w=[tp], inc=(co == 3))
        cx.op("scalar", lambda h, pb=pb: h.activation(rs, pb[:, :], AF.Sqrt, scale=1.0 / 512, bias=k.epsc), r=[tp], w=[trs])
        cx.op(V, lambda h: h.reciprocal(rs, rs), r=[trs], w=[trs])
        for co in range(4):
            cx.op(V, lambda h, co=co, ts=ts: h.scalar_tensor_tensor(out=yT[:, co, ts], in0=gT[:, co, ts], scalar=nw[:, co:co + 1], in1=rs, op0=ALU.mult, op1=ALU.mult),
                  r=[tgT[co], trs, tb, tsq], w=[t_yT])
    k.dbg_add("s5_gl", gT, tgT)
    cx.barrier()
    ar.release(m0)


def build(in_shapes, stage="full", dbg_names=(), n_heads=4, n_experts=32):
    nc = bass.Bass("TRN2", target_bir_lowering=False)
    k = K()
    k.n_heads = n_heads
    k.n_experts = n_experts
    k.nc = nc
    D = {}
    for nm, (shape, dt) in in_shapes.items():
        D[nm] = nc.dram_tensor(nm, list(shape), dt, kind="ExternalInput").ap()
    k.D = D
    out = nc.dram_tensor("out", [S, DM], F32, kind="ExternalOutput").ap()
    k.dbg = {}
    k.dbg_req = set(dbg_names)

    with contextlib.ExitStack() as st:
        cx = Ctx(nc, st)
        k.cx = cx

        def finish():
            deps = [(s_.key, s_.total) for s_ in cx.slots if s_.total > 0]
            cx.wait_deps("sync", deps + [(e, cx.cnt[e]) for e in ENGS if e != "sync" and cx.cnt[e] > 0])
            with nc.Block() as block:
                cx.emit_all(block)
            k.n_ops = cx.n_ops
            return nc, k
        k.slot_c = cx.slot("c")
        k.slot_w = cx.slot("w")
        k.slot_x = [cx.slot("x0"), cx.slot("x1")]
        k.slot_o = cx.slot("o")
        k.psum = [cx.ps("ps%d" % i, [128, 512], F32) for i in range(8)]
        k.psum = [p[:, :] for p in k.psum]
        k.tpsum = [Trk("ps%d" % i, excl=True) for i in range(8)]
        k.ident = cx.sb("ident", [128, 128], F32)[:, :]
        k.identb = cx.sb("identb", [128, 128], BF16)[:, :]
        k.ones = cx.sb("ones", [128, 128], F32)[:, :]
        k.onesb = cx.sb("onesb", [128, 128], BF16)[:, :]
        k.epsc = cx.sb("epsc", [128, 1], F32)[:, :]
        k.selT = cx.sb("selT", [128, 2, 8, 128], BF16)[:, :, :, :]
        tc = Trk()
        cx.dma("sync", k.ident, D["ident"], k.slot_c, w=[tc])
        cx.dma("sync", k.identb, D["identb"], k.slot_c, w=[tc])
        cx.dma("sync", k.ones, D["ones"], k.slot_c, w=[tc])
        cx.dma("gpsimd", k.onesb, D["ones"], cx.fresh("sw"), w=[tc])
        cx.op("vector", lambda h: h.memset(k.epsc, EPS), w=[tc])
        cx.dma("sync", k.selT, D["selT"], k.slot_c, w=[tc])
        k.s5A1 = cx.sb("s5A1", [128, 64], F32)[:, :]
        k.s5A2 = cx.sb("s5A2", [128, 64], F32)[:, :]
        ar = Arena(cx, 51456)
        k.ar = ar
        cx.barrier()

        def dbg_add(name, ap, trks):
            if name in k.dbg_req:
                shape = list(ap.shape)
                dt_ = F32
                o = nc.dram_tensor("dbg_" + name, shape, dt_, kind="ExternalOutput").ap()
                cx.dma("gpsimd" if ap.dtype != F32 else "sync", o, ap, cx.fresh("sw" if ap.dtype != F32 else "hw"), r=list(trks))
        k.dbg_add = dbg_add

        regA = ar.alloc([NT * 1024], F32)
        arA = Arena(cx, NT * 1024, base=regA)
        k.arA = arA
        yT = ar.alloc([8, 2048], BF16)
        t_yT = Trk()
        k.s5U = arA.alloc([32, 256], BF16)
        k.t_s5U = Trk()
        m_h = arA.mark()
        k.hT = arA.alloc([8, 2048], BF16)
        k.t_hT = Trk()

        m1 = ar.mark()
        xt = [ar.alloc([1024], F32) for _ in range(2)]
        txt = [Trk(), Trk()]

        def src_x(i):
            b = i % 2
            cx.dma("sync", xt[b], D["x"][i * 128:(i + 1) * 128, :], k.slot_x[b], w=[txt[b]])
            return xt[b], txt[b]
        norm_transpose(k, "mix", src_x, NT, D["norm_mix"], k.hT, BF16, k.t_hT)
        cx.barrier()
        ar.release(m1)

        if stage == 'p1':
            return finish()
        s5_build_U(k)
        if stage == 'U':
            return finish()
        mg = ar.mark()
        gdn_setup(k)
        for hd in range(k.n_heads):
            gdn_head(k, hd, yT, t_yT)
        ar.release(mg)
        k.dbg_add("ygdnT", yT[:, 4:8, :], [t_yT])
        if stage == 'gdn':
            return finish()
        cx.barrier()
        arA.release(m_h)
        k.s5AT = arA.alloc([64, 128], BF16)
        k.t_s5at = Trk()
        k.s5CS = arA.alloc([2, 16, 2, 128], BF16)
        k.s5TT = arA.alloc([32, 128], BF16)
        k.t_s5tt = Trk()
        s5_prep(k)
        if stage == 's5prep':
            return finish()
        s5_main(k, yT[:, 0:4, :], t_yT)
        k.dbg_add("ys5T", yT[:, 0:4, :], [t_yT])
        if stage == "s5":
            return finish()
        if True:
            cx.barrier()
            k.xacc = regA.rearrange('p (a b) -> p a b', a=NT)
            k.txacc = [Trk() for _ in range(NT)]
            out_proj(k, yT, t_yT)
            k.dbg_add("x1", k.xacc, k.txacc)
            if stage == 'oproj':
                return finish()
            xattn(k)
            if stage == 'xattn':
                return finish()
            k.dbg_add("x2", k.xacc, k.txacc)
            moe(k)
            k.dbg_add("x3", k.xacc, k.txacc + [t_ for p_ in k.txh for t_ in p_])
            final_norm(k, out)

        return finish()


def host_inputs(inp, b):
    m = {}
    m["x"] = np.ascontiguousarray(inp["x"][b])
    m["mem"] = np.ascontiguousarray(inp["mem"][b])
    m["norm_mix"] = inp["norm_mix"][0]
    m["w_in"] = inp["w_in"][0]
    m["w_out"] = inp["w_out"][0]
    m["s5_w_glu"] = inp["s5_w_glu"][0]
    m.update(host_s5(inp))
    cv = inp["gdn_conv"][0]
    m["gdn_convw"] = np.ascontiguousarray(cv.reshape(5, 3, 4, 128).transpose(3, 2, 1, 0))
    for nm in ("gdn_a_log_f", "gdn_dt_bias_f", "gdn_a_log_b", "gdn_dt_bias_b"):
        m[nm] = inp[nm][0]
    m["gdn_norm"] = inp["gdn_norm"][0]
    for nm in ("norm_xattn", "norm_mem", "xa_wq", "xa_wk", "xa_wv", "xa_wo", "norm_moe", "router_group_w", "router_group_b",
               "router_expert_w", "router_expert_b", "moe_w_gate", "moe_w_up", "moe_w_down"):
        m[nm] = inp[nm][0]
    m["norm_final"] = inp["norm_final"]
    m.update(host_consts())
    return m


def gdn_setup(k):
    cx, ar, D = k.cx, k.ar, k.D
    V = "vector"
    G = K()
    k.G = G
    G.mask = ar.alloc([7, 128], F32)
    G.tmask = Trk()
    cx.dma("sync", G.mask, D["gmask"][:, 0:7, :], cx.fresh(), w=[G.tmask])
    wsm = ar.alloc([8, 16], BF16)
    tw = Trk()
    load_w_cols(k, D["w_in"], 2560, 16, wsm, tw, cx.fresh('sw'))
    BA = ar.alloc([16, 16], F32)
    tBA = Trk()
    for i in range(NT):
        pb = k.psum[i % 4]
        tp = k.tpsum[i % 4]
        for c in range(8):
            cx.op("tensor", lambda h, pb=pb, c=c, i=i: h.matmul(pb[:, 0:16], k.hT[:, c, i * 128:(i + 1) * 128], wsm[:, c, :], start=(c == 0), stop=(c == 7)),
                  r=[tw, k.t_hT], w=[tp], inc=(c == 7))
        cx.op("scalar", lambda h, pb=pb, i=i: h.copy(BA[:, i, :], pb[:, 0:16]), r=[tp], w=[tBA])
    pr = ar.alloc([4, 4], F32)
    tpr = Trk()
    sl_ = cx.fresh()
    for j, nm in enumerate(("gdn_a_log_f", "gdn_dt_bias_f", "gdn_a_log_b", "gdn_dt_bias_b")):
        cx.dma("sync", pr[:, j, :], dram_bcast(D[nm], 128, 4), sl_, w=[tpr])
    G.nw = ar.alloc([128], F32)
    cx.dma("sync", G.nw, dram_bcast(D["gdn_norm"], 128, 128), sl_, w=[tpr])
    G.tpr = tpr
    T = Trk()
    G.T = T
    G.beta, G.nb, G.gc, G.eg, G.neg, G.ed = [], [], [], [], [], []
    def per_dir(d):
        beta = ar.alloc([16, 4], F32)
        nb = ar.alloc([16, 4], F32)
        g = ar.alloc([16, 4], F32)
        gc = ar.alloc([16, 4], F32)
        gt = ar.alloc([16, 4], F32)
        eg = ar.alloc([16, 4], F32)
        neg = ar.alloc([16, 4], F32)
        ed = ar.alloc([16, 4], F32)
        ea = ar.alloc([4], F32)
        braw = BA[:, :, d * 4:(d + 1) * 4]
        araw = BA[:, :, 8 + d * 4:8 + (d + 1) * 4]
        cx.op("scalar", lambda h: h.activation(beta, braw, AF.Sigmoid), r=[tBA, T], w=[T])
        cx.op(V, lambda h: h.tensor_scalar(nb, beta, -1.0, None, op0=ALU.mult), r=[T], w=[T])
        cx.op("scalar", lambda h: h.activation(ea, pr[:, 2 * d, :], AF.Exp), r=[tpr, T], w=[T])
        cx.op(V, lambda h: h.tensor_tensor(out=g, in0=araw, in1=pr[:, 2 * d + 1, :].unsqueeze(1).to_broadcast([128, 16, 4]), op=ALU.add), r=[tBA, tpr, T], w=[T])
        cx.op("scalar", lambda h: h.activation(g, g, AF.Exp), r=[T], w=[T])
        cx.op("scalar", lambda h: h.activation(g, g, AF.Ln, bias=1.0), r=[T], w=[T])
        cx.op(V, lambda h: h.scalar_tensor_tensor(out=g, in0=g, scalar=-1.0, in1=ea.unsqueeze(1).to_broadcast([128, 16, 4]), op0=ALU.mult, op1=ALU.mult), r=[T], w=[T])
        g2 = g.rearrange("p a b -> p (a b)")
        pb = k.psum[4 + d]
        tp = k.tpsum[4 + d]
        cx.op("tensor", lambda h, pb=pb, d=d: h.matmul(pb[:, 0:64], G.mask[:, d, :], g2, start=True, stop=True), r=[T, G.tmask], w=[tp])
        cx.op("tensor", lambda h, pb=pb: h.matmul(pb[:, 64:128], G.mask[:, 6, :], g2, start=True, stop=True), r=[T, G.tmask], w=[tp])
        cx.op(V, lambda h, pb=pb: h.tensor_copy(gc.rearrange("p a b -> p (a b)"), pb[:, 0:64]), r=[tp], w=[T])
        cx.op(V, lambda h, pb=pb: h.tensor_tensor(out=gt.rearrange("p a b -> p (a b)"), in0=pb[:, 64:128], in1=gc.rearrange("p a b -> p (a b)"), op=ALU.subtract), r=[tp, T], w=[T])
        cx.op("scalar", lambda h: h.activation(eg, gc, AF.Exp), r=[T], w=[T])
        cx.op("scalar", lambda h: h.activation(ed, gt, AF.Exp), r=[T], w=[T])
        cx.op(V, lambda h: h.tensor_scalar(neg, eg, -1.0, None, op0=ALU.mult), r=[T], w=[T])
        G.g = getattr(G, "g", []) + [g]
        G.beta.append(beta); G.nb.append(nb); G.gc.append(gc); G.eg.append(eg); G.neg.append(neg); G.ed.append(ed)
    per_dir(0)
    per_dir(1)
    G.osum = ar.alloc([16, 128], F32)
    G.tosum = [Trk() for _ in range(NT)]


def gdn_head(k, hd, yT, t_yT):
    cx, ar, D, G = k.cx, k.ar, k.D, k.G
    V = "vector"
    m0 = ar.mark()
    qnT = ar.alloc([2048], BF16)
    knT = ar.alloc([2048], BF16)
    Ktok = ar.alloc([16, 128], BF16)
    Vtok = ar.alloc([16, 128], BF16)
    tq, tk_, tKt, tVt = Trk(), Trk(), Trk(), Trk()
    wz = ar.alloc([8, 128], BF16)
    twz = Trk()
    load_w_cols(k, D["w_in"], 512 + 1536 + hd * 128, 128, wz, twz, cx.fresh('sw'))
    mA = ar.mark()
    w3 = [ar.alloc([8, 128], BF16) for _ in range(3)]
    tw3 = [Trk() for _ in range(3)]
    for j in range(3):
        load_w_cols(k, D["w_in"], 512 + j * 512 + hd * 128, 128, w3[j], tw3[j], cx.fresh('sw'))
    cw = ar.alloc([3, 5], F32)
    tcw = Trk()
    cx.dma("sync", cw, D["gdn_convw"][:, hd, :, :], cx.fresh(), w=[tcw])
    diag = ar.alloc([15, 128], BF16)
    tdg = Trk()
    for j in range(3):
        for t in range(5):
            cx.op("vector", lambda h, j=j, t=t: h.tensor_scalar(diag[:, j * 5 + t, :], k.identb, cw[:, j, t:t + 1], None, op0=ALU.mult), r=[tcw], w=[tdg])
    import os
    ALV = int(os.environ.get("GDN_ALV", "9"))
    if ALV == 0:
        cx.barrier(); ar.release(m0); return
    raw = [ar.alloc([2052], BF16) for _ in range(2)]
    traw = [Trk(), Trk()]
    for b in range(2):
        cx.op("gpsimd", lambda h, b=b: h.memset(raw[b][:, 0:2], 0.0), w=[traw[b]])
        cx.op("gpsimd", lambda h, b=b: h.memset(raw[b][:, 2050:2052], 0.0), w=[traw[b]])
    act = ar.alloc([2048], F32)
    tact = Trk()
    vT = ar.alloc([2048], BF16)
    tvT = Trk()
    sqb = ar.alloc([2048], BF16)
    tsqb = Trk()
    rn = [ar.alloc([512], F32) for _ in range(4)]
    trn = [Trk() for _ in range(4)]
    tactn = [Trk() for _ in range(4)]
    tsqn = [Trk() for _ in range(4)]
    if ALV == 1:
        cx.barrier(); ar.release(m0); return
    for j in range(3):
        b = j % 2

        def consume(n, pb, tp, b=b):
            cx.op(V if n % 2 else "scalar", (lambda h: h.tensor_copy(raw[b][:, 2 + n * 512:2 + (n + 1) * 512], pb[:, :])) if n % 2 else
                  (lambda h: h.copy(raw[b][:, 2 + n * 512:2 + (n + 1) * 512], pb[:, :])), r=[tp], w=[traw[b]])
        proj_fm(k, w3[j], tw3[j], consume)
        for n in range(4):
            pb = k.psum[4 + n]
            tp = k.tpsum[4 + n]
            for t in range(5):
                cx.op("tensor", lambda h, pb=pb, t=t, n=n, j=j, b=b: h.matmul(pb[:, :], diag[:, j * 5 + t, :], raw[b][:, n * 512 + t:n * 512 + t + 512], start=(t == 0), stop=(t == 4)),
                      r=[tdg, traw[b]], w=[tp], inc=(t == 4))
        for n in range(4):
            pb = k.psum[4 + n]
            tp = k.tpsum[4 + n]
            ts = slice(n * 512, (n + 1) * 512)
            if j == 2:
                cx.op("scalar", lambda h, pb=pb, ts=ts: h.activation(vT[:, ts], pb[:, :], AF.Silu), r=[tp], w=[tvT])
            else:
                cx.op("scalar", lambda h, pb=pb, ts=ts: h.activation(act[:, ts], pb[:, :], AF.Silu), r=[tp], w=[tactn[n]])
        if j < 2 and ALV > 2:
            for n in range(4):
                ts = slice(n * 512, (n + 1) * 512)
                cx.op("scalar", lambda h, ts=ts: h.activation(sqb[:, ts], act[:, ts], AF.Square), r=[tactn[n]], w=[tsqn[n]])
            for n in range(4):
                ts = slice(n * 512, (n + 1) * 512)
                pb2 = k.psum[n]
                tp2 = k.tpsum[n]
                cx.op("tensor", lambda h, pb2=pb2, ts=ts: h.matmul(pb2[:, :], k.onesb, sqb[:, ts], start=True, stop=True), r=[tsqn[n]], w=[tp2])
            for n in range(4):
                pb2 = k.psum[n]
                tp2 = k.tpsum[n]
                cx.op("scalar", lambda h, pb2=pb2, n=n: h.activation(rn[n], pb2[:, :], AF.Sqrt, bias=k.epsc), r=[tp2], w=[trn[n]])
            for n in range(4):
                cx.op(V, lambda h, n=n: h.reciprocal(rn[n], rn[n]), r=[trn[n]], w=[trn[n]])
            dstT, tdst, scl = (qnT, tq, 128.0 ** -0.5) if j == 0 else (knT, tk_, 1.0)
            for n in range(4):
                ts = slice(n * 512, (n + 1) * 512)
                cx.op(V, lambda h, ts=ts, n=n, dstT=dstT, scl=scl: h.scalar_tensor_tensor(out=dstT[:, ts], in0=act[:, ts], scalar=scl, in1=rn[n], op0=ALU.mult, op1=ALU.mult),
                      r=[tactn[n], trn[n]], w=[tdst])
    if ALV <= 3:
        cx.barrier(); ar.release(m0); return
    TV = int(os.environ.get("GDN_TV", "0"))
    for i in range(NT):
        pb = k.psum[i % 2].bitcast(BF16)
        tp = k.tpsum[i % 2]
        if TV == 0:
            cx.op("tensor", lambda h, pb=pb, i=i: h.transpose(pb[:, 0:128], knT[:, i * 128:(i + 1) * 128], k.identb), r=[tk_], w=[tp])
            cx.op("tensor", lambda h, pb=pb, i=i: h.transpose(pb[:, 128:256], vT[:, i * 128:(i + 1) * 128], k.identb), r=[tvT], w=[tp])
            cx.op("scalar", lambda h, pb=pb, i=i: h.copy(Ktok[:, i, :], pb[:, 0:128]), r=[tp], w=[tKt])
            cx.op(V, lambda h, pb=pb, i=i: h.tensor_copy(Vtok[:, i, :], pb[:, 128:256]), r=[tp], w=[tVt])
        elif TV == 1:
            cx.op("tensor", lambda h, pb=pb, i=i: h.transpose(pb[:, 0:128], knT[:, i * 128:(i + 1) * 128], k.identb), r=[tk_], w=[tp])
            cx.op("scalar", lambda h, pb=pb, i=i: h.copy(Ktok[:, i, :], pb[:, 0:128]), r=[tp], w=[tKt])
        elif TV == 2:
            cx.op("tensor", lambda h, pb=pb, i=i: h.transpose(pb[:, 0:128], vT[:, i * 128:(i + 1) * 128], k.identb), r=[tvT], w=[tp])
            cx.op(V, lambda h, pb=pb, i=i: h.tensor_copy(Vtok[:, i, :], pb[:, 0:128]), r=[tp], w=[tVt])
    if hd == 0:
        k.dbg_add("gdn_qn", qnT, [tq])
        k.dbg_add("gdn_kn", knT, [tk_])
        k.dbg_add("gdn_vtok", Vtok, [tVt])
    cx.barrier()
    ar.release(mA)
    STOP = os.environ.get("GDN_STOP", "")
    if STOP == "A":
        ar.release(m0)
        return
    qgT = [ar.alloc([2048], BF16) for _ in range(2)]
    Kd = [ar.alloc([16, 128], BF16) for _ in range(2)]
    Pm = [ar.alloc([16, 128], BF16) for _ in range(2)]
    QKm = [ar.alloc([16, 128], BF16) for _ in range(2)]
    etot = [ar.alloc([32], F32) for _ in range(2)]
    WnT = [ar.alloc([2048], BF16) for _ in range(2)]
    U0b = [ar.alloc([16, 128], BF16) for _ in range(2)]
    tWn = [[Trk() for _ in range(NT)] for _ in range(2)]
    tU0 = [[Trk() for _ in range(NT)] for _ in range(2)]
    tqg = [[Trk() for _ in range(NT)] for _ in range(2)]
    tKd = [[Trk() for _ in range(NT)] for _ in range(2)]
    tPm = [[Trk() for _ in range(NT)] for _ in range(2)]
    tQK = [[Trk() for _ in range(NT)] for _ in range(2)]
    tet = [[Trk() for _ in range(NT)] for _ in range(2)]
    NI = 8
    mN = ar.mark()
    NDT = BF16 if os.environ.get('GDN_NEU', 'bf16') == 'bf16' else F32
    nid = k.identb if NDT == BF16 else k.ident
    Xb = [[ar.alloc([128], NDT) for _ in range(2)] for _ in range(NI)]
    XTb = [[ar.alloc([128], NDT) for _ in range(2)] for _ in range(NI)]
    Pb = [[ar.alloc([128], NDT) for _ in range(2)] for _ in range(NI)]

    def tview(pn_, c0):
        return pn_[:, c0:c0 + 128] if NDT == F32 else pn_.bitcast(BF16)[:, 2 * c0:2 * c0 + 128]
    tX = [Trk() for _ in range(NI)]
    EGB = [ar.alloc([128], F32) for _ in range(NI)]
    ET = [ar.alloc([128], BF16) for _ in range(NI)]
    ETs = ET
    tE = [Trk() for _ in range(NI)]
    tEG = [Trk() for _ in range(NI)]
    Kg = [ar.alloc([128], BF16) for _ in range(NI)]
    tKg = [Trk() for _ in range(NI)]
    tXP = [Trk() for _ in range(NI)]
    insts = [(i, d) for i in range(NT) for d in range(2)]
    for g0 in range(0, len(insts), NI):
        grp = insts[g0:g0 + NI]
        info = []
        for s_, (i, d) in enumerate(grp):
            info.append(dict(s_=s_, i=i, d=d, tsl=slice(i * 128, (i + 1) * 128),
                             col=pap(G.g[d], 0, 128, i * 4 + hd, [[0, 128]]),
                             gcc=G.gc[d][:, i, hd:hd + 1], nbc=G.nb[d][:, i, hd:hd + 1], edc=G.ed[d][:, i, hd:hd + 1],
                             pb=k.psum[s_], tp=k.tpsum[s_]))
        for q_ in info:
            s_, i, d, tsl, col, pb, tp = q_["s_"], q_["i"], q_["d"], q_["tsl"], q_["col"], q_["pb"], q_["tp"]
            cx.op("tensor", lambda h, pb=pb, col=col, d=d: h.matmul(pb[:, 0:128], col, G.mask[:, d, :], start=True, stop=True), r=[G.T, G.tmask], w=[tp], inc=False)
            cx.op("tensor", lambda h, pb=pb, tsl=tsl: h.matmul(pb[:, 128:256], knT[:, tsl], knT[:, tsl], start=True, stop=True), r=[tk_], w=[tp], inc=False)
            cx.op("tensor", lambda h, pb=pb, tsl=tsl: h.matmul(pb[:, 256:384], knT[:, tsl], qnT[:, tsl], start=True, stop=True), r=[tk_, tq], w=[tp])
        for q_ in info:
            s_, i, d, pb, tp, gcc = q_["s_"], q_["i"], q_["d"], q_["pb"], q_["tp"], q_["gcc"]
            cx.op("scalar", lambda h, pb=pb, s_=s_: h.activation(EGB[s_], pb[:, 0:128], AF.Exp), r=[tp], w=[tEG[s_]])
            cx.op(V, lambda h, pb=pb, s_=s_, gcc=gcc, d=d: h.scalar_tensor_tensor(out=ET[s_], in0=pb[:, 0:128], scalar=gcc, in1=G.mask[:, 2 + d, :], op0=ALU.subtract, op1=ALU.min),
                  r=[tp, G.T, G.tmask], w=[tE[s_]])
        for q_ in info:
            s_, i, d, tsl = q_["s_"], q_["i"], q_["d"], q_["tsl"]
            cx.op("scalar", lambda h, s_=s_: h.activation(ET[s_], ET[s_], AF.Exp), r=[tE[s_]], w=[tE[s_]])
            cx.op(V, lambda h, s_=s_, tsl=tsl, d=d: h.tensor_tensor(out=qgT[d][:, tsl], in0=qnT[:, tsl], in1=EGB[s_], op=ALU.mult), r=[tq, tEG[s_]], w=[tqg[d][i]])
        for q_ in info:
            s_, i, d, pb, tp, edc = q_["s_"], q_["i"], q_["d"], q_["pb"], q_["tp"], q_["edc"]
            c0, c1 = (63, 127) if d == 0 else (0, 64)
            cx.op("scalar", lambda h, s_=s_, d=d, i=i, c0=c0: h.copy(etot[d][:, 2 * i:2 * i + 1], EGB[s_][:, c0:c0 + 1]), r=[tEG[s_]], w=[tet[d][i]])
            cx.op("scalar", lambda h, s_=s_, d=d, i=i, c1=c1: h.copy(etot[d][:, 2 * i + 1:2 * i + 2], EGB[s_][:, c1:c1 + 1]), r=[tEG[s_]], w=[tet[d][i]])
            cx.op("scalar", lambda h, d=d, i=i, edc=edc: h.activation(Kd[d][:, i, :], Ktok[:, i, :], AF.Identity, scale=edc), r=[tKt, G.T], w=[tKd[d][i]])
            egc = G.eg[d][:, i, hd:hd + 1]
            cx.op("scalar", lambda h, s_=s_, i=i, egc=egc: h.activation(Kg[s_], Ktok[:, i, :], AF.Identity, scale=egc), r=[tKt, G.T], w=[tKg[s_]])
            cx.op(V, lambda h, pb=pb, s_=s_, d=d, i=i: h.tensor_tensor(out=QKm[d][:, i, :], in0=pb[:, 256:384], in1=ET[s_], op=ALU.mult), r=[tp, tE[s_]], w=[tQK[d][i]])
        for q_ in info:
            s_, d = q_["s_"], q_["d"]
            cx.op(V, lambda h, s_=s_, d=d: h.tensor_tensor(out=ETs[s_], in0=ET[s_], in1=G.mask[:, 4 + d, :], op=ALU.mult), r=[tE[s_], G.tmask], w=[tE[s_]])
        for q_ in info:
            s_, pb, tp, nbc = q_["s_"], q_["pb"], q_["tp"], q_["nbc"]
            cx.op(V, lambda h, pb=pb, s_=s_, nbc=nbc: h.scalar_tensor_tensor(out=Xb[s_][0], in0=pb[:, 128:256], scalar=nbc, in1=ETs[s_], op0=ALU.mult, op1=ALU.mult),
                  r=[tp, tE[s_], G.T], w=[tX[s_]])
        for q_ in info:
            s_, pn, tn = q_["s_"], q_["pb"], q_["tp"]
            cx.op("tensor", lambda h, pn=pn, s_=s_: h.transpose(tview(pn, 384), Xb[s_][0], nid), r=[tX[s_]], w=[tn])
            cx.op(V, lambda h, s_=s_: h.tensor_tensor(out=Pb[s_][0], in0=Xb[s_][0], in1=nid, op=ALU.add), r=[tX[s_]], w=[tXP[s_]])
            cx.op("scalar", lambda h, pn=pn, s_=s_: h.copy(XTb[s_][0], tview(pn, 384)), r=[tn], w=[tX[s_]])
        for L in range(1, 6):
            a, b_ = (L - 1) % 2, L % 2
            for s_, (i, d) in enumerate(grp):
                pn = k.psum[s_]
                tn = k.tpsum[s_]
                if L < 5:
                    cx.op("tensor", lambda h, pn=pn, s_=s_, a=a: h.matmul(pn[:, 0:128], XTb[s_][a], Xb[s_][a], start=True, stop=True), r=[tX[s_]], w=[tn], inc=False)
                cx.op("tensor", lambda h, pn=pn, s_=s_, a=a: h.matmul(pn[:, 128:256], Xb[s_][a], XTb[s_][a], start=True, stop=True), r=[tX[s_]], w=[tn])
                e1, e2 = ("scalar", V) if s_ % 2 == 0 else (V, "scalar")
                if L < 5:
                    if e1 == "scalar":
                        cx.op("scalar", lambda h, pn=pn, s_=s_, b_=b_: h.copy(Xb[s_][b_], pn[:, 0:128]), r=[tn], w=[tX[s_]])
                    else:
                        cx.op(V, lambda h, pn=pn, s_=s_, b_=b_: h.tensor_copy(Xb[s_][b_], pn[:, 0:128]), r=[tn], w=[tX[s_]])
                if e2 == "scalar":
                    cx.op("scalar", lambda h, pn=pn, s_=s_, b_=b_: h.copy(XTb[s_][b_], pn[:, 128:256]), r=[tn], w=[tX[s_]])
                else:
                    cx.op(V, lambda h, pn=pn, s_=s_, b_=b_: h.tensor_copy(XTb[s_][b_], pn[:, 128:256]), r=[tn], w=[tX[s_]])
            for s_, (i, d) in enumerate(grp):
                pn = k.psum[s_]
                tn = k.tpsum[s_]
                cx.op("tensor", lambda h, pn=pn, s_=s_, a=a, b_=b_: h.matmul(pn[:, 256:384], XTb[s_][b_], Pb[s_][a], start=True, stop=True), r=[tX[s_], tXP[s_]], w=[tn])
                if L < 5:
                    cx.op(V, lambda h, pn=pn, s_=s_, a=a, b_=b_: h.tensor_tensor(out=Pb[s_][b_], in0=pn[:, 256:384], in1=Pb[s_][a], op=ALU.add), r=[tn, tXP[s_]], w=[tXP[s_]])
                else:
                    cx.op(V, lambda h, pn=pn, s_=s_, a=a, d=d, i=i: h.tensor_tensor(out=Pm[d][:, i, :], in0=pn[:, 256:384], in1=Pb[s_][a], op=ALU.add), r=[tn, tXP[s_]], w=[tPm[d][i], tXP[s_]])
        for s_, (i, d) in enumerate(grp):
            pn = k.psum[s_]
            tn = k.tpsum[s_]
            tsl = slice(i * 128, (i + 1) * 128)
            cx.op("tensor", lambda h, pn=pn, s_=s_, d=d, i=i: h.matmul(pn[:, 0:128], Kg[s_], Pm[d][:, i, :], start=True, stop=True), r=[tKg[s_], tPm[d][i]], w=[tn], inc=False)
            cx.op("tensor", lambda h, pn=pn, d=d, i=i: h.matmul(pn[:, 128:256], Pm[d][:, i, :], Vtok[:, i, :], start=True, stop=True), r=[tPm[d][i], tVt], w=[tn])
            btc_ = G.beta[d][:, i, hd:hd + 1]
            cx.op(V, lambda h, pn=pn, d=d, tsl=tsl: h.tensor_scalar(WnT[d][:, tsl], pn[:, 0:128], -1.0, None, op0=ALU.mult), r=[tn], w=[tWn[d][i]])
            cx.op("scalar", lambda h, pn=pn, d=d, i=i, btc_=btc_: h.activation(U0b[d][:, i, :], pn[:, 128:256], AF.Identity, scale=btc_), r=[tn, G.T], w=[tU0[d][i]])
    if STOP == "B":
        cx.barrier()
        ar.release(m0)
        return
    ar.release(mN)
    Sf = [[ar.alloc([128], F32) for _ in range(2)] for _ in range(2)]
    Sb = [ar.alloc([128], BF16) for _ in range(2)]
    Rp = [ar.alloc([128], BF16) for _ in range(2)]
    vn = [ar.alloc([128], BF16) for _ in range(2)]
    tS = [Trk(), Trk()]
    tSf = [Trk(), Trk()]
    tR = [Trk(), Trk()]
    tv = [Trk(), Trk()]
    cx.op("gpsimd", lambda h: h.memset(G.osum, 0.0), w=G.tosum)
    for d in range(2):
        cx.op("gpsimd", lambda h, d=d: h.memset(Sf[d][0], 0.0), w=[tS[d]])
        cx.op("gpsimd", lambda h, d=d: h.memset(Sb[d], 0.0), w=[tS[d]])
        cx.op("gpsimd", lambda h, d=d: h.memset(Rp[d], 0.0), w=[tR[d]])
        cx.op("gpsimd", lambda h, d=d: h.memset(vn[d], 0.0), w=[tv[d]])
    for step in range(32):
        for d in range(2):
            if d == 0:
                i, hh = step // 2, step % 2
            else:
                i, hh = 15 - step // 2, 1 - step % 2
            tsl = slice(i * 128, (i + 1) * 128)
            ps_ = slice(hh * 64, (hh + 1) * 64)
            cur, nxt = step % 2, (step + 1) % 2
            pcs = [k.psum[4 * d + q_] for q_ in range(4)]
            tcs = [k.tpsum[4 * d + q_] for q_ in range(4)]
            negc = G.neg[d][ps_, i, hd:hd + 1]
            btc = G.beta[d][ps_, i, hd:hd + 1]
            p1, pv_, po_, pst = pcs
            t1_, tv_, to_, tst = tcs
            cx.op("tensor", lambda h, p1=p1, tsl=tsl, d=d: h.matmul(p1[:, 0:128], WnT[d][:, tsl], Sb[d], start=True, stop=True), r=[tWn[d][i], tS[d]], w=[t1_])
            cx.op(V, lambda h, p1=p1, ps_=ps_, btc=btc, d=d, i=i: h.scalar_tensor_tensor(out=vn[d][ps_, :], in0=p1[ps_, 0:128], scalar=btc, in1=U0b[d][ps_, i, :], op0=ALU.mult, op1=ALU.add),
                  r=[t1_, tU0[d][i], G.T], w=[tv[d]])
            cx.op("tensor", lambda h, po_=po_, tsl=tsl, d=d: h.matmul(po_[:, 0:128], qgT[d][:, tsl], Sb[d], start=True, stop=False), r=[tqg[d][i], tS[d]], w=[to_], inc=False)
            cx.op("tensor", lambda h, po_=po_, ps_=ps_, d=d, i=i: h.matmul(po_[:, 0:128], QKm[d][ps_, i, :], vn[d][ps_, :], start=False, stop=True), r=[tQK[d][i], tv[d]], w=[to_])
            cx.op("tensor", lambda h, pst=pst, ps_=ps_, d=d, i=i: h.matmul(pst[:, 0:128], Kd[d][ps_, i, :], vn[d][ps_, :], start=True, stop=True), r=[tKd[d][i], tv[d]], w=[tst])
            cx.op("gpsimd" if False else V, lambda h, po_=po_, ps_=ps_, i=i: h.tensor_tensor(out=G.osum[ps_, i, :], in0=po_[ps_, 0:128], in1=G.osum[ps_, i, :], op=ALU.add), r=[to_, G.tosum[i]], w=[G.tosum[i]])
            etc = etot[d][:, 2 * i + hh:2 * i + hh + 1]
            cx.op(V, lambda h, pst=pst, d=d, cur=cur, nxt=nxt, etc=etc: h.scalar_tensor_tensor(out=Sf[d][nxt], in0=Sf[d][cur], scalar=etc, in1=pst[:, 0:128], op0=ALU.mult, op1=ALU.add),
                  r=[tst, tet[d][i], tS[d]], w=[tS[d]])
            cx.op("scalar", lambda h, d=d, nxt=nxt: h.copy(Sb[d], Sf[d][nxt]), r=[tS[d]], w=[tS[d]])
    if hd == 0:
        k.dbg_add("gdn_osum", G.osum, G.tosum)
    if STOP == "C":
        cx.barrier()
        ar.release(m0)
        return
    ss = ar.alloc([NT, 2], F32)
    tss = Trk()
    junk = ar.alloc([128], BF16)
    zs = [ar.alloc([128], F32) for _ in range(2)]
    tzs = [Trk(), Trk()]
    yb = [ar.alloc([128], BF16) for _ in range(2)]
    tyb = [Trk(), Trk()]
    for i in range(NT):
        cx.op("scalar", lambda h, i=i: h.activation(junk, G.osum[:, i, :], AF.Square, accum_out=ss[:, i, 0:1]), r=[G.tosum[i], tss], w=[tss])
    cx.op(V, lambda h: h.tensor_scalar(ss[:, :, 1:2], ss[:, :, 0:1], 1.0 / 128, EPS, op0=ALU.mult, op1=ALU.add), r=[tss], w=[tss])
    cx.op("scalar", lambda h: h.activation(ss[:, :, 1:2], ss[:, :, 1:2], AF.Sqrt), r=[tss], w=[tss])
    cx.op(V, lambda h: h.reciprocal(ss[:, :, 1:2], ss[:, :, 1:2]), r=[tss], w=[tss])
    for i in range(NT):
        b = i % 2
        pz = k.psum[b]
        tz = k.tpsum[b]
        for c in range(8):
            cx.op("tensor", lambda h, pz=pz, c=c, i=i: h.matmul(pz[:, 0:128], k.hT[:, c, i * 128:(i + 1) * 128], wz[:, c, :], start=(c == 0), stop=(c == 7)),
                  r=[twz, k.t_hT], w=[tz], inc=(c == 7))
        cx.op("scalar", lambda h, pz=pz, b=b: h.activation(zs[b], pz[:, 0:128], AF.Silu), r=[tz], w=[tzs[b]])
        s1 = ss[:, i, 1:2]
        cx.op(V, lambda h, i=i, s1=s1: h.scalar_tensor_tensor(out=G.osum[:, i, :], in0=G.osum[:, i, :], scalar=s1, in1=G.nw, op0=ALU.mult, op1=ALU.mult), r=[tss, G.tpr, G.tosum[i]], w=[G.tosum[i]])
        cx.op(V, lambda h, i=i, b=b: h.tensor_tensor(out=yb[b], in0=G.osum[:, i, :], in1=zs[b], op=ALU.mult), r=[G.tosum[i], tzs[b]], w=[tyb[b]])
        pt = k.psum[2 + b].bitcast(BF16)
        tt_ = k.tpsum[2 + b]
        cx.op("tensor", lambda h, pt=pt, b=b: h.transpose(pt[:, 0:128], yb[b], k.identb), r=[tyb[b]], w=[tt_])
        cx.op("scalar", lambda h, pt=pt, i=i: h.copy(yT[:, 4 + hd, i * 128:(i + 1) * 128], pt[:, 0:128]), r=[tt_], w=[t_yT])
    cx.barrier()
    ar.release(m0)


def out_proj(k, yT, t_yT):
    cx, ar, D = k.cx, k.ar, k.D
    m0 = ar.mark()
    wo = ar.alloc([8, 1024], BF16)
    two = Trk()
    wsrc = D["w_out"]
    sl_ = cx.fresh('sw')
    for c in range(8):
        cx.dma("gpsimd", wo[:, c, :], wsrc[c * 128:(c + 1) * 128, :], sl_, w=[two])
    slx = cx.fresh()
    for i in range(NT):
        cx.dma("sync", k.xacc[:, i, :], D["x"][i * 128:(i + 1) * 128, :], slx, w=[k.txacc[i]])
    for i in range(NT):
        k.txacc[i].w = (slx.key, slx.total)
    for i in range(NT):
        for half in range(2):
            pb = k.psum[(2 * i + half) % 4]
            tp = k.tpsum[(2 * i + half) % 4]
            for c in range(8):
                cx.op("tensor", lambda h, pb=pb, c=c, i=i, half=half: h.matmul(pb[:, :], yT[:, c, i * 128:(i + 1) * 128], wo[:, c, half * 512:(half + 1) * 512], start=(c == 0), stop=(c == 7)),
                      r=[t_yT, two], w=[tp], inc=(c == 7))
            xs = k.xacc[:, i, half * 512:(half + 1) * 512]
            cx.op("vector", lambda h, pb=pb, xs=xs: h.tensor_tensor(out=xs, in0=pb[:, :], in1=xs, op=ALU.add), r=[tp, k.txacc[i]], w=[k.txacc[i]])
    cx.barrier()
    ar.release(m0)


def xattn(k):
    cx, ar, D = k.cx, k.ar, k.D
    V = "vector"
    m0 = ar.mark()
    xnT = ar.alloc([8, 2048], BF16)
    t_xnT = Trk()
    memT = ar.alloc([8, 256], BF16)
    t_memT = Trk()
    m1 = ar.mark()
    mt = [ar.alloc([1024], F32) for _ in range(2)]
    tmt = [Trk(), Trk()]

    def src_mem(i):
        cx.dma("sync", mt[i], D["mem"][i * 128:(i + 1) * 128, :], cx.fresh(), w=[tmt[i]])
        return mt[i], tmt[i]
    norm_transpose(k, "mem", src_mem, 2, D["norm_mem"], memT, BF16, t_memT)
    ar.release(m1)
    norm_transpose(k, "xa", lambda i: (k.xacc[:, i, :], k.txacc[i]), NT, D["norm_xattn"], xnT, BF16, t_xnT, resident=True)
    k.dbg_add("xa_memT", memT, [t_memT])
    k.dbg_add("xa_xnT", xnT, [t_xnT])
    wq = [ar.alloc([8, 256], BF16) for _ in range(2)]
    wk = [ar.alloc([8, 256], BF16) for _ in range(2)]
    wv = [ar.alloc([8, 256], BF16) for _ in range(2)]
    wo = [ar.alloc([2, 1024], BF16) for _ in range(2)]
    twA = [Trk(), Trk()]
    two_ = [Trk(), Trk()]
    swA = [cx.slot("xwa0"), cx.slot("xwa1")]
    swO = [cx.slot("xwo0"), cx.slot("xwo1")]
    kTb = [ar.alloc([2, 256], BF16) for _ in range(2)]
    vhb = [ar.alloc([2, 256], BF16) for _ in range(2)]
    tkvb = [Trk(), Trk()]
    qTb = [ar.alloc([2, 2048], BF16) for _ in range(2)]
    tqTb = [Trk(), Trk()]
    E = [ar.alloc([2, 512], BF16) for _ in range(2)]
    tE = [Trk(), Trk()]
    rden = [ar.alloc([512], F32) for _ in range(2)]
    trd = [Trk(), Trk()]
    oTn = [ar.alloc([2, 512], BF16) for _ in range(2)]
    toT = [Trk(), Trk()]
    cx.barrier()
    k.txa = [[Trk(), Trk()] for _ in range(NT)]

    def loadA(hd):
        b = hd % 2
        c0 = hd * 256
        for (dst, nm) in ((wq[b], "xa_wq"), (wk[b], "xa_wk"), (wv[b], "xa_wv")):
            load_w_cols(k, D[nm], c0, 256, dst, twA[b], swA[b])

    def loadO(hd):
        b = hd % 2
        c0 = hd * 256
        src = D["xa_wo"]
        cx.dma("gpsimd", wo[b], bass.AP(src.tensor, src.offset + c0 * 1024, [[1024, 128], [128 * 1024, 2], [1, 1024]]), swO[b], w=[two_[b]])

    def proj(hd):
        b = hd % 2
        kT, vh, qT, tkv, tqT = kTb[b], vhb[b], qTb[b], tkvb[b], tqTb[b]
        for dc in range(2):
            pb = k.psum[dc]
            tp = k.tpsum[dc]
            for c in range(8):
                cx.op("tensor", lambda h, pb=pb, c=c, dc=dc, b=b: h.matmul(pb[:, 0:256], wk[b][:, c, dc * 128:(dc + 1) * 128], memT[:, c, :], start=(c == 0), stop=(c == 7)),
                      r=[twA[b], t_memT], w=[tp], inc=(c == 7))
            cx.op("scalar", lambda h, pb=pb, dc=dc, kT=kT: h.copy(kT[:, dc, :], pb[:, 0:256]), r=[tp], w=[tkv])
        for mtile in range(2):
            pb = k.psum[2 + mtile]
            tp = k.tpsum[2 + mtile]
            for c in range(8):
                cx.op("tensor", lambda h, pb=pb, c=c, mtile=mtile, b=b: h.matmul(pb[:, 0:256], memT[:, c, mtile * 128:(mtile + 1) * 128], wv[b][:, c, :], start=(c == 0), stop=(c == 7)),
                      r=[twA[b], t_memT], w=[tp], inc=(c == 7))
            cx.op(V, lambda h, pb=pb, mtile=mtile, vh=vh: h.tensor_copy(vh[:, mtile, :], pb[:, 0:256]), r=[tp], w=[tkv])
        for dc in range(2):
            for n in range(4):
                pb = k.psum[(dc * 4 + n) % 4]
                tp = k.tpsum[(dc * 4 + n) % 4]
                for c in range(8):
                    cx.op("tensor", lambda h, pb=pb, c=c, dc=dc, n=n, b=b: h.matmul(pb[:, :], wq[b][:, c, dc * 128:(dc + 1) * 128], xnT[:, c, n * 512:(n + 1) * 512], start=(c == 0), stop=(c == 7)),
                          r=[twA[b], t_xnT], w=[tp], inc=(c == 7))
                if n % 2 == 0:
                    cx.op("scalar", lambda h, pb=pb, dc=dc, n=n, qT=qT: h.copy(qT[:, dc, n * 512:(n + 1) * 512], pb[:, :]), r=[tp], w=[tqT])
                else:
                    cx.op(V, lambda h, pb=pb, dc=dc, n=n, qT=qT: h.tensor_copy(qT[:, dc, n * 512:(n + 1) * 512], pb[:, :]), r=[tp], w=[tqT])

    def chunks(hd):
        b = hd % 2
        kT, vh, qT, tkv, tqT = kTb[b], vhb[b], qTb[b], tkvb[b], tqTb[b]
        tw = [two_[0], two_[1]]

        def emit_scores(n):
            eb = n % 2
            ts = slice(n * 512, (n + 1) * 512)
            for mtile in range(2):
                pb = k.psum[mtile]
                tp = k.tpsum[mtile]
                for dc in range(2):
                    cx.op("tensor", lambda h, pb=pb, dc=dc, mtile=mtile, ts=ts: h.matmul(pb[:, :], kT[:, dc, mtile * 128:(mtile + 1) * 128], qT[:, dc, ts], start=(dc == 0), stop=(dc == 1)),
                          r=[tkv, tqT], w=[tp], inc=(dc == 1))
                cx.op("scalar", lambda h, pb=pb, mtile=mtile, eb=eb: h.activation(E[eb][:, mtile, :], pb[:, :], AF.Exp, scale=1.0 / 16.0), r=[tp], w=[tE[eb]])

        def emit_rest(n):
            eb = n % 2
            pd = k.psum[2]
            tpd = k.tpsum[2]
            for mtile in range(2):
                cx.op("tensor", lambda h, pd=pd, mtile=mtile, eb=eb: h.matmul(pd[:, :], k.onesb, E[eb][:, mtile, :], start=(mtile == 0), stop=(mtile == 1)), r=[tE[eb]], w=[tpd], inc=(mtile == 1))
            cx.op(V, lambda h, pd=pd, eb=eb: h.reciprocal(rden[eb], pd[:, :]), r=[tpd], w=[trd[eb]])
            for dc in range(2):
                po = k.psum[3 + dc]
                tpo = k.tpsum[3 + dc]
                for mtile in range(2):
                    cx.op("tensor", lambda h, po=po, mtile=mtile, dc=dc, eb=eb: h.matmul(po[:, :], vh[:, mtile, dc * 128:(dc + 1) * 128], E[eb][:, mtile, :], start=(mtile == 0), stop=(mtile == 1)),
                          r=[tkv, tE[eb]], w=[tpo], inc=(mtile == 1))
                cx.op(V, lambda h, po=po, dc=dc, eb=eb: h.tensor_tensor(out=oTn[eb][:, dc, :], in0=po[:, :], in1=rden[eb], op=ALU.mult), r=[tpo, trd[eb]], w=[toT[eb]])
            for t in range(4):
                i = n * 4 + t
                for half in range(2):
                    pw_ = k.psum[5 + (t * 2 + half) % 3]
                    tpw = k.tpsum[5 + (t * 2 + half) % 3]
                    for dc in range(2):
                        cx.op("tensor", lambda h, pw_=pw_, dc=dc, t=t, half=half, b=b, eb=eb: h.matmul(pw_[:, :], oTn[eb][:, dc, t * 128:(t + 1) * 128], wo[b][:, dc, half * 512:(half + 1) * 512], start=(dc == 0), stop=(dc == 1)),
                              r=[toT[eb], tw[b]], w=[tpw], inc=(dc == 1))
                    xs = k.xacc[:, i, half * 512:(half + 1) * 512]
                    cx.op(V, lambda h, pw_=pw_, xs=xs: h.tensor_tensor(out=xs, in0=pw_[:, :], in1=xs, op=ALU.add), r=[tpw, k.txa[i][half]], w=[k.txa[i][half]])
        emit_scores(0)
        for n in range(4):
            if n + 1 < 4:
                emit_scores(n + 1)
            emit_rest(n)
    loadA(0)
    loadA(1)
    loadO(0)
    loadO(1)
    proj(0)
    for hd in range(4):
        if hd + 1 < 4:
            proj(hd + 1)
        if hd + 2 < 4:
            loadA(hd + 2)
        chunks(hd)
        if hd + 2 < 4:
            loadO(hd + 2)
    cx.barrier()
    ar.release(m0)


def moe(k):
    cx, ar, D = k.cx, k.ar, k.D
    V = "vector"
    m0 = ar.mark()
    xnT = ar.alloc([8, 2048], BF16)
    t_xnT = Trk()
    norm_transpose(k, "moe", lambda i: (k.xacc[:, i, :], k.txacc[i]), NT, D["norm_moe"], xnT, BF16, t_xnT, resident=True)
    wr = ar.alloc([8, 36], BF16)
    twr = Trk()
    sl_ = cx.fresh('sw')
    srcg, srce = D["router_group_w"], D["router_expert_w"]
    cx.dma("gpsimd", wr[:, :, 0:4], bass.AP(srcg.tensor, srcg.offset, [[4, 128], [4 * 128, 8], [1, 4]]), sl_, w=[twr])
    cx.dma("gpsimd", wr[:, :, 4:36], bass.AP(srce.tensor, srce.offset, [[32, 128], [32 * 128, 8], [1, 32]]), sl_, w=[twr])
    rb = ar.alloc([36], F32)
    trb = Trk()
    sl2 = cx.fresh()
    cx.dma("sync", rb[:, 0:4], dram_bcast(D["router_group_b"], 128, 4), sl2, w=[trb])
    cx.dma("sync", rb[:, 4:36], dram_bcast(D["router_expert_b"], 128, 32), sl2, w=[trb])
    cw = ar.alloc([NT, 32], F32)
    tcw = Trk()
    lgA = ar.alloc([NT, 36], F32)
    msk = ar.alloc([NT, 32], F32)
    eq2 = ar.alloc([NT, 32], F32)
    m8 = ar.alloc([NT, 8], F32)
    sc = ar.alloc([8, NT], F32)
    oh = ar.alloc([NT, 4], F32)
    ex = ar.alloc([NT, 4], F32)
    T = Trk()
    rbb = rb.unsqueeze(1).to_broadcast([128, 8, 36])
    for half in range(2):
        pb = k.psum[half]
        tp = k.tpsum[half]
        for ii in range(8):
            i = half * 8 + ii
            for c in range(8):
                cx.op("tensor", lambda h, pb=pb, c=c, i=i, ii=ii: h.matmul(pb[:, ii * 36:(ii + 1) * 36], xnT[:, c, i * 128:(i + 1) * 128], wr[:, c, :], start=(c == 0), stop=(c == 7)),
                      r=[t_xnT, twr], w=[tp], inc=(c == 7))
        cx.op(V, lambda h, pb=pb, half=half: h.tensor_tensor(out=lgA[:, half * 8:(half + 1) * 8, :], in0=pb[:, 0:288].rearrange("p (t e) -> p t e", t=8), in1=rbb, op=ALU.add), r=[tp, trb, T], w=[T])
    lg_g = lgA[:, :, 0:4]
    lg_e = lgA[:, :, 4:36]
    gmax, ngs, ssum, ptop, dm, w1, w2 = [sc[:, j_, :] for j_ in range(7)]

    def vop(fn):
        cx.op(V, fn, r=[T], w=[T])

    def aop(fn):
        cx.op("scalar", fn, r=[T], w=[T])
    b4 = lambda v: v.unsqueeze(2).to_broadcast([128, NT, 4])
    b32 = lambda v: v.unsqueeze(2).to_broadcast([128, NT, 32])
    vop(lambda h: h.tensor_reduce(out=gmax, in_=lg_g, axis=AX.X, op=ALU.max))
    vop(lambda h: h.tensor_tensor(out=oh, in0=lg_g, in1=b4(gmax), op=ALU.is_equal))
    vop(lambda h: h.tensor_tensor(out=ex, in0=lg_g, in1=b4(gmax), op=ALU.subtract))
    aop(lambda h: h.activation(ex, ex, AF.Exp))
    vop(lambda h: h.tensor_reduce(out=ssum, in_=ex, axis=AX.X, op=ALU.add))
    vop(lambda h: h.reciprocal(ptop, ssum))
    vop(lambda h: h.tensor_scalar(oh, oh, -1.0, 1e30, op0=ALU.add, op1=ALU.mult))
    vop(lambda h: h.tensor_tensor(out=msk.rearrange("p t (g e) -> p t g e", g=4), in0=lg_e.rearrange("p t (g e) -> p t g e", g=4),
                                  in1=oh.unsqueeze(3).to_broadcast([128, NT, 4, 8]), op=ALU.add))
    for i in range(NT):
        vop(lambda h, i=i: h.max(out=m8[:, i, :], in_=msk[:, i, :]))
    m1, m2 = m8[:, :, 0], m8[:, :, 1]
    vop(lambda h: h.tensor_tensor(out=dm, in0=m2, in1=m1, op=ALU.subtract))
    aop(lambda h: h.activation(dm, dm, AF.Exp))
    vop(lambda h: h.tensor_scalar(w1, dm, 1.0, None, op0=ALU.add))
    vop(lambda h: h.reciprocal(w1, w1))
    vop(lambda h: h.tensor_tensor(out=w2, in0=dm, in1=w1, op=ALU.mult))
    vop(lambda h: h.tensor_tensor(out=w1, in0=w1, in1=ptop, op=ALU.mult))
    vop(lambda h: h.tensor_tensor(out=w2, in0=w2, in1=ptop, op=ALU.mult))
    vop(lambda h: h.tensor_tensor(out=eq2, in0=msk, in1=b32(m2), op=ALU.is_equal))
    vop(lambda h: h.tensor_tensor(out=eq2, in0=eq2, in1=b32(w2), op=ALU.mult))
    vop(lambda h: h.tensor_tensor(out=msk, in0=msk, in1=b32(m1), op=ALU.is_equal))
    vop(lambda h: h.tensor_tensor(out=msk, in0=msk, in1=b32(w1), op=ALU.mult))
    cx.op(V, lambda h: h.tensor_tensor(out=cw, in0=msk, in1=eq2, op=ALU.add), r=[T], w=[tcw, T])
    k.dbg_add("moe_cw", cw, [tcw])
    wgu = [ar.alloc([8, 512], BF16) for _ in range(2)]
    wd = [ar.alloc([2, 1024], BF16) for _ in range(2)]
    twe = [Trk(), Trk()]
    swe = [cx.slot("we0"), cx.slot("we1")]
    sg = [ar.alloc([512], F32) for _ in range(2)]
    tsg = [Trk(), Trk()]
    h1 = [ar.alloc([2, 512], BF16) for _ in range(2)]
    th1 = [Trk(), Trk()]
    NE = k.n_experts

    def load_e(e):
        b = e % 2
        g_, u_, d_ = D["moe_w_gate"], D["moe_w_up"], D["moe_w_down"]
        cx.dma("gpsimd", wgu[b][:, :, 0:256], bass.AP(g_.tensor, g_.offset + e * 1024 * 256, [[256, 128], [256 * 128, 8], [1, 256]]), swe[b], w=[twe[b]])
        cx.dma("gpsimd", wgu[b][:, :, 256:512], bass.AP(u_.tensor, u_.offset + e * 1024 * 256, [[256, 128], [256 * 128, 8], [1, 256]]), swe[b], w=[twe[b]])
        cx.dma("gpsimd", wd[b], bass.AP(d_.tensor, d_.offset + e * 256 * 1024, [[1024, 128], [1024 * 128, 2], [1, 1024]]), swe[b], w=[twe[b]])
    import os
    NOLOAD = os.environ.get("MOE_NOLOAD", "") == "1"
    load_e(0)
    if NE > 1:
        load_e(1)
    jobs = [(e, n) for e in range(NE) for n in range(4)]
    state = {"cnt": 0, "loaded": 0}
    cx.barrier()
    k.txh = [[Trk(), Trk()] for _ in range(NT)]

    def emit_gu(j, fh):
        e, n = jobs[j]
        b = e % 2
        ts = slice(n * 512, (n + 1) * 512)
        hb = j % 2
        pg = k.psum[fh * 2]
        tpg = k.tpsum[fh * 2]
        pu = k.psum[fh * 2 + 1]
        tpu = k.tpsum[fh * 2 + 1]
        for c in range(8):
            cx.op("tensor", lambda h, pg=pg, c=c, fh=fh, ts=ts, b=b: h.matmul(pg[:, :], wgu[b][:, c, fh * 128:(fh + 1) * 128], xnT[:, c, ts], start=(c == 0), stop=(c == 7)),
                  r=[twe[b], t_xnT], w=[tpg], inc=(c == 7))
        for c in range(8):
            cx.op("tensor", lambda h, pu=pu, c=c, fh=fh, ts=ts, b=b: h.matmul(pu[:, :], wgu[b][:, c, 256 + fh * 128:256 + (fh + 1) * 128], xnT[:, c, ts], start=(c == 0), stop=(c == 7)),
                  r=[twe[b], t_xnT], w=[tpu], inc=(c == 7))
        cx.op("scalar", lambda h, pg=pg, fh=fh: h.activation(sg[fh], pg[:, :], AF.Silu), r=[tpg], w=[tsg[fh]])
        cx.op(V, lambda h, pu=pu, fh=fh, hb=hb: h.tensor_tensor(out=h1[hb][:, fh, :], in0=pu[:, :], in1=sg[fh], op=ALU.mult), r=[tpu, tsg[fh]], w=[th1[hb]])

    def emit_down(j):
        e, n = jobs[j]
        b = e % 2
        hb = j % 2
        for t in range(4):
            i = n * 4 + t
            for half in range(2):
                pdn = k.psum[4 + state["cnt"] % 4]
                tpd = k.tpsum[4 + state["cnt"] % 4]
                state["cnt"] += 1
                for fh in range(2):
                    cx.op("tensor", lambda h, pdn=pdn, fh=fh, t=t, half=half, hb=hb, b=b: h.matmul(pdn[:, :], h1[hb][:, fh, t * 128:(t + 1) * 128], wd[b][:, fh, half * 512:(half + 1) * 512], start=(fh == 0), stop=(fh == 1)),
                          r=[th1[hb], twe[b]], w=[tpd], inc=(fh == 1))
                xs = k.xacc[:, i, half * 512:(half + 1) * 512]
                cwc = cw[:, i, e:e + 1]
                cx.op(V, lambda h, pdn=pdn, xs=xs, cwc=cwc: h.scalar_tensor_tensor(out=xs, in0=pdn[:, :], scalar=cwc, in1=xs, op0=ALU.mult, op1=ALU.add), r=[tpd, tcw, k.txh[i][half]], w=[k.txh[i][half]])
        if n == 3 and e + 2 < NE and not NOLOAD:
            load_e(e + 2)
    nj = len(jobs)
    if nj > 0:
        emit_gu(0, 0)
        emit_gu(0, 1)
        for j in range(nj):
            if j + 1 < nj:
                emit_gu(j + 1, 0)
            emit_down(j)
            if j + 1 < nj:
                emit_gu(j + 1, 1)
    cx.barrier()
    ar.release(m0)


def final_norm(k, out):
    cx, ar, D = k.cx, k.ar, k.D
    V = "vector"
    m0 = ar.mark()
    gB = ar.alloc([1024], F32)
    tg = Trk()
    cx.dma("sync", gB, dram_bcast(D["norm_final"], 128, 1024), cx.fresh(), w=[tg])
    junk = ar.alloc([1024], BF16)
    tj = Trk()
    ss = ar.alloc([NT, 2], F32)
    tss = Trk()
    ob = [ar.alloc([1024], F32) for _ in range(2)]
    tob = [Trk(), Trk()]
    so = [cx.slot("o0"), cx.slot("o1")]
    for i in range(NT):
        txs = [k.txacc[i]] + (k.txh[i] if hasattr(k, "txh") else [])
        cx.op("scalar", lambda h, i=i: h.activation(junk, k.xacc[:, i, :], AF.Square, accum_out=ss[:, i, 0:1]), r=txs + [tss], w=[tj, tss])
    cx.op(V, lambda h: h.tensor_scalar(ss[:, :, 1:2], ss[:, :, 0:1], 1.0 / 1024, EPS, op0=ALU.mult, op1=ALU.add), r=[tss], w=[tss])
    cx.op("scalar", lambda h: h.activation(ss[:, :, 1:2], ss[:, :, 1:2], AF.Sqrt), r=[tss], w=[tss])
    cx.op(V, lambda h: h.reciprocal(ss[:, :, 1:2], ss[:, :, 1:2]), r=[tss], w=[tss])
    for i in range(NT):
        b = i % 2
        s1 = ss[:, i, 1:2]
        txs = [k.txacc[i]] + (k.txh[i] if hasattr(k, "txh") else [])
        cx.op(V, lambda h, i=i, s1=s1, b=b: h.scalar_tensor_tensor(out=ob[b], in0=k.xacc[:, i, :], scalar=s1, in1=gB, op0=ALU.mult, op1=ALU.mult), r=txs + [tss, tg], w=[tob[b]])
        cx.dma("sync", out[i * 128:(i + 1) * 128, :], ob[b], so[b], r=[tob[b]])
    cx.barrier()
    ar.release(m0)


_CACHE = {}


def kernel(**inputs):
    inp = {k_: np.asarray(v) for k_, v in inputs.items()}
    n = inp["x"].shape[0]
    maps = [host_inputs(inp, b) for b in range(n)]
    key = "full"
    if key not in _CACHE:
        shapes = {k_: (v.shape, np2dt(v)) for k_, v in maps[0].items()}
        _CACHE[key] = build(shapes)[0]
    nc = _CACHE[key]
    res = run_bass_kernel_spmd(nc, maps, core_ids=list(range(n)))
    return np.stack([np.asarray(r["out"], dtype=np.float32) for r in res.results], 0)
```

```python
import contextlib
import os
import math
import numpy as np
import ml_dtypes
import concourse.bass as bass
import concourse.mybir as mybir
from concourse.bass_utils import run_bass_kernel_spmd

F32 = mybir.dt.float32
BF16 = mybir.dt.bfloat16
F32R = mybir.dt.float32r
I32 = mybir.dt.int32
AF = mybir.ActivationFunctionType
ALU = mybir.AluOpType
AX = mybir.AxisListType

ENGS = ("sync", "scalar", "gpsimd", "vector", "tensor")
ATTACH_WAIT = os.environ.get("ATTACH_WAIT", "1") == "1"
S = 2048
DM = 1024
NT = 16
EPS = 1e-6


class Trk:
    __slots__ = ("name", "w", "r", "excl")

    def __init__(self, name="", excl=False):
        self.name = name
        self.w = None
        self.r = {}
        self.excl = excl


class DmaSlot:
    def __init__(self, ctx, name):
        self.key = "d_" + name + str(ctx.nsem)
        ctx.sems[self.key] = ctx.new_sem(self.key)
        self.total = 0


class Ctx:
    def __init__(self, nc, stack):
        self.nc = nc
        self.stack = stack
        self.q = {e: [] for e in ENGS}
        self.sems = {}
        self.nsem = 0
        self.cnt = {e: 0 for e in ENGS}
        self.known = {e: {} for e in ENGS}
        for e in ENGS:
            self.sems[e] = self.new_sem("s_" + e)
        self.slots = []
        self.pools = {}
        self.pool_idx = {}
        self.n_ops = 0

    def new_sem(self, name):
        self.nsem += 1
        return self.stack.enter_context(self.nc.semaphore(name))

    def slot(self, name):
        s = DmaSlot(self, name)
        self.slots.append(s)
        return s

    def fresh(self, kind="hw"):
        pool = self.pools.setdefault(kind, [])
        i = self.pool_idx.get(kind, 0)
        if i >= len(pool):
            assert len(pool) < 30, "slot pool exhausted"
            pool.append(self.slot(kind + "%d" % len(pool)))
            pool[-1].kind = kind
        self.pool_idx[kind] = i + 1
        return pool[i]

    def sb(self, name, shape, dt):
        return self.stack.enter_context(self.nc.sbuf_tensor("sb_" + name, list(shape), dt))

    def ps(self, name, shape, dt=F32):
        return self.stack.enter_context(self.nc.psum_tensor(name, list(shape), dt))

    def _waits_for(self, eng, r, w, extra=()):
        need = {}

        def req(dep):
            if dep is None:
                return
            k, c = dep
            if k == eng and eng in ("tensor", "sync"):
                return
            if c > need.get(k, 0):
                need[k] = c
        for t in r:
            req(t.w)
        for t in w:
            req(t.w)
            for k, c in t.r.items():
                req((k, c))
        for d in extra:
            req(d)
        out = []
        kn = self.known[eng]
        for k, c in need.items():
            if kn.get(k, 0) < c:
                kn[k] = c
                out.append((self.sems[k], c))
        return out

    def op(self, eng, fn, r=(), w=(), inc=True, extra=()):
        w = list(w) + [t for t in r if t.excl]
        r = [t for t in r if not t.excl]
        waits = self._waits_for(eng, r, w, extra)
        c = self.cnt[eng] + 1
        if inc:
            self.cnt[eng] = c
        sem = self.sems[eng]

        def emit(h, fn=fn, waits=waits, inc=inc, sem=sem):
            for s, v in waits[:-1]:
                h.wait_ge(s, v)
            ins = fn(h)
            if waits:
                if ATTACH_WAIT:
                    ins._wait_ge(waits[-1][0], waits[-1][1])
                else:
                    raise RuntimeError
            if inc:
                ins.then_inc(sem, 1)
        if not ATTACH_WAIT:
            def emit(h, fn=fn, waits=waits, inc=inc, sem=sem):
                for s, v in waits:
                    h.wait_ge(s, v)
                ins = fn(h)
                if inc:
                    ins.then_inc(sem, 1)
        self.q[eng].append(emit)
        for t in r:
            t.r[eng] = c
        for t in w:
            t.w = (eng, c)
            t.r = {}
        self.n_ops += 1

    def dma(self, eng, out, in_, slot, r=(), w=(), extra=(), **kw):
        kind = "sw" if eng == "gpsimd" else "hw"
        assert getattr(slot, "kind", kind) == kind, ("DMA slot kind mismatch", slot.key, eng)
        slot.kind = kind
        waits = self._waits_for(eng, r, w, extra)
        slot.total += 16
        sem = self.sems[slot.key]

        def emit(h, waits=waits, sem=sem, out=out, in_=in_, kw=kw):
            for s, v in waits:
                h.wait_ge(s, v)
            h.dma_start(out=out, in_=in_, **kw).then_inc(sem, 16)
        self.q[eng].append(emit)
        dep = (slot.key, slot.total)
        for t in r:
            t.r[slot.key] = slot.total
        for t in w:
            t.w = dep
            t.r = {}
        self.n_ops += 1
        return dep

    def wait_deps(self, eng, deps):
        waits = self._waits_for(eng, (), (), deps)

        def emit(h, waits=waits):
            for s, v in waits:
                h.wait_ge(s, v)
        self.q[eng].append(emit)

    def barrier(self):
        deps = [(e, self.cnt[e]) for e in ENGS if e != "sync" and self.cnt[e] > 0]
        deps += [(s.key, s.total) for s in self.slots if s.total > 0]
        for e in ENGS:
            self.wait_deps(e, deps)
        self.pool_idx = {}

    def emit_all(self, block):
        q = self.q

        @block.sync
        def _(h):
            for f in q["sync"]:
                f(h)

        @block.scalar
        def _(h):
            for f in q["scalar"]:
                f(h)

        @block.gpsimd
        def _(h):
            for f in q["gpsimd"]:
                f(h)

        @block.vector
        def _(h):
            for f in q["vector"]:
                f(h)

        @block.tensor
        def _(h):
            for f in q["tensor"]:
                f(h)


class Arena:
    def __init__(self, cx, words, base=None):
        self.t = cx.sb("arena", [128, words], F32) if base is None else base
        self.cx = cx
        self.words = words
        self.top = 0

    def mark(self):
        return self.top

    def release(self, m):
        if m != self.top:
            self.cx.barrier()
        self.top = m

    def alloc(self, shape, dt):
        n = int(np.prod(shape))
        w = n if dt in (F32, F32R, I32) else (n + 1) // 2
        w = (w + 1) // 2 * 2
        o = self.top
        self.top += w
        assert self.top <= self.words, ("arena overflow", self.top, self.words)
        v = self.t[:, o:o + w]
        if dt != F32:
            v = v.bitcast(dt)
        v = v[:, 0:n]
        if len(shape) > 1:
            names = " ".join("d%d" % i for i in range(len(shape)))
            v = v.rearrange("p (%s) -> p %s" % (names, names), **{"d%d" % i: shape[i] for i in range(len(shape))})
        return v


def pap(ap, part0, nparts, off, dims):
    base = ap.ap[0][0]
    return bass.AP(ap.tensor, ap.offset + part0 * base + off, [[base, nparts]] + [list(d) for d in dims])


def host_consts():
    c = {}
    c["ident"] = np.eye(128, dtype=np.float32)
    c["identb"] = np.eye(128, dtype=np.float32).astype(ml_dtypes.bfloat16)
    c["ones"] = np.ones((128, 128), np.float32)
    selT = np.zeros((128, 2, 8, 128), np.float32)
    selB = np.zeros((128, 2, 8, 128), np.float32)
    for q in range(4):
        for r in range(32):
            loc, cc = r // 16, r % 16
            for s in range(8):
                selT[q * 32 + r, loc, s, s * 16 + cc] = 1.0
                selB[q * 32 + r, loc, s, s * 16 + cc] = 1.0
    c["selT"] = selT.astype(ml_dtypes.bfloat16)
    c["selB"] = selB.astype(ml_dtypes.bfloat16)
    sidx = np.arange(128) // 16
    c["s5mf"] = (sidx[None, :] >= sidx[:, None]).astype(np.float32)
    c["s5mb"] = (sidx[None, :] <= sidx[:, None]).astype(np.float32)
    c["kvec"] = np.tile((np.arange(16, dtype=np.float32) - 7.0)[None, :], (128, 1))
    k = np.arange(128)[:, None]
    cc = np.arange(128)[None, :]
    same = (k // 64) == (cc // 64)
    gm = np.zeros((128, 8, 128), np.float32)
    gm[:, 0] = same & (k <= cc)
    gm[:, 1] = same & (k >= cc)
    gm[:, 2] = np.where(same & (cc >= k), 0.0, -30000.0)
    gm[:, 3] = np.where(same & (cc <= k), 0.0, -30000.0)
    gm[:, 4] = same & (cc > k)
    gm[:, 5] = same & (cc < k)
    gm[:, 6] = same
    c["gmask"] = gm
    return c


def host_s5(inp):
    o = {}

    pairs = {"lam_re": ("s5_lam_re_f", "s5_lam_re_b"), "lam_im": ("s5_lam_im_f", "s5_lam_im_b"),
             "log_step": ("s5_log_step_f", "s5_log_step_b"), "b_re": ("s5_b_re_f", "s5_b_re_b"),
             "b_im": ("s5_b_im_f", "s5_b_im_b"), "c_re": ("s5_c_re_f", "s5_c_re_b"), "c_im": ("s5_c_im_f", "s5_c_im_b")}

    def st(nm):
        f_, b_ = pairs[nm]
        return np.stack([inp[f_][0], inp[b_][0]], 0)
    lam = np.stack([st("lam_re"), st("lam_im")], 0)
    lam = lam.reshape(2, 2, 2, 16, 64).transpose(2, 4, 0, 1, 3)
    o["s5_lam"] = np.ascontiguousarray(lam.reshape(128, 2, 32))
    ls = st("log_step").reshape(2, 2, 16)
    ls = np.broadcast_to(ls.transpose(1, 0, 2)[:, None], (2, 64, 2, 16))
    o["s5_step"] = np.ascontiguousarray(ls.reshape(128, 32))
    b = np.stack([st("b_re"), st("b_im")], 0)
    b = b.reshape(2, 2, 2, 16, 64, 16).transpose(2, 4, 0, 1, 3, 5)
    o["s5_b"] = np.ascontiguousarray(b.reshape(128, 2, 512))
    cm = np.stack([st("c_re"), st("c_im")], 0)
    cm = cm.reshape(2, 2, 2, 16, 16, 64).transpose(2, 5, 0, 1, 3, 4)
    o["s5_c"] = np.ascontiguousarray(cm.reshape(128, 2, 512))
    d = inp["s5_d"][0].reshape(32, 16)
    o["s5_dvec"] = np.ascontiguousarray(np.broadcast_to(d.T[None], (8, 16, 32)).reshape(128, 32))
    o["s5_bglu"] = np.ascontiguousarray(inp["s5_b_glu"][0].reshape(4, 128).T)
    o["s5_normw"] = np.ascontiguousarray(inp["s5_norm"][0].reshape(4, 128).T)
    return o


def np2dt(a):
    if a.dtype == np.float32:
        return F32
    if a.dtype == ml_dtypes.bfloat16:
        return BF16
    raise ValueError(a.dtype)


class K:
    pass


def dram_bcast(ap, nparts, n, off=0):
    return bass.AP(ap.tensor, ap.offset + off, [[0, nparts], [1, n]])


def norm_transpose(k, name, src_fn, ntiles, gain_dram, outT, out_dt, outT_trk, resident=False):
    cx, ar = k.cx, k.ar
    m = ar.mark()
    gB = ar.alloc([1024], F32)
    tg = Trk()
    cx.dma("sync", gB, dram_bcast(gain_dram, 128, 1024), cx.fresh(), w=[tg])
    junk = ar.alloc([1024], BF16)
    tj = Trk()
    xn = [ar.alloc([1024], out_dt) for _ in range(2)]
    txn = [Trk(), Trk()]
    ss = ar.alloc([NT * 2, 1], F32)
    tss = [Trk() for _ in range(ntiles)]
    pdt = BF16 if out_dt == BF16 else F32
    ident = k.identb if out_dt == BF16 else k.ident
    srcs = []
    if resident:
        for i in range(ntiles):
            src, ts = src_fn(i)
            srcs.append((src, ts))
            cx.op("scalar", lambda h, src=src, i=i: h.activation(junk, src, AF.Square, accum_out=ss[:, 2 * i:2 * i + 1]), r=[ts], w=[tj, tss[0]])
        ssv = ss.rearrange("p (t two) one -> p t (two one)", two=2)
        cx.op("vector", lambda h: h.tensor_scalar(ssv[:, 0:ntiles, 1:2], ssv[:, 0:ntiles, 0:1], 1.0 / 1024, EPS, op0=ALU.mult, op1=ALU.add), r=[tss[0]], w=[tss[0]])
        cx.op("scalar", lambda h: h.activation(ssv[:, 0:ntiles, 1:2], ssv[:, 0:ntiles, 1:2], AF.Sqrt), r=[tss[0]], w=[tss[0]])
        cx.op("vector", lambda h: h.reciprocal(ssv[:, 0:ntiles, 1:2], ssv[:, 0:ntiles, 1:2]), r=[tss[0]], w=[tss[0]])
    for i in range(ntiles):
        rsi = ss[:, 2 * i + 1:2 * i + 2]
        if resident:
            src, ts = srcs[i]
            tsi = tss[0]
        else:
            src, ts = src_fn(i)
            ssi = ss[:, 2 * i:2 * i + 1]
            tsi = tss[i]
            cx.op("scalar", lambda h, src=src, ssi=ssi: h.activation(junk, src, AF.Square, accum_out=ssi), r=[ts], w=[tj, tss[i]])
            cx.op("vector", lambda h, ssi=ssi, rsi=rsi: h.tensor_scalar(rsi, ssi, 1.0 / 1024, EPS, op0=ALU.mult, op1=ALU.add), r=[tss[i]], w=[tss[i]])
            cx.op("scalar", lambda h, rsi=rsi: h.activation(rsi, rsi, AF.Sqrt), r=[tss[i]], w=[tss[i]])
            cx.op("vector", lambda h, rsi=rsi: h.reciprocal(rsi, rsi), r=[tss[i]], w=[tss[i]])
        b = i % 2
        cx.op("vector", lambda h, src=src, rsi=rsi, b=b: h.scalar_tensor_tensor(out=xn[b], in0=src, scalar=rsi, in1=gB, op0=ALU.mult, op1=ALU.mult),
              r=[ts, tsi, tg], w=[txn[b]])
        if out_dt == BF16:
            pb = k.psum[i % 2]
            tp = k.tpsum[i % 2]
            pv = pb.bitcast(BF16)
            for c in range(8):
                cx.op("tensor", lambda h, b=b, c=c, pv=pv: h.transpose(pv[:, c * 128:(c + 1) * 128], xn[b][:, c * 128:(c + 1) * 128], ident),
                      r=[txn[b]], w=[tp], inc=(c == 7))
            dst = outT[:, :, i * 128:(i + 1) * 128]
            eng = "scalar" if i % 2 == 0 else "vector"
            if eng == "scalar":
                cx.op(eng, lambda h, dst=dst, pv=pv: h.copy(dst, pv.rearrange("p (c t) -> p c t", c=8)), r=[tp], w=[outT_trk])
            else:
                cx.op(eng, lambda h, dst=dst, pv=pv: h.tensor_copy(dst, pv.rearrange("p (c t) -> p c t", c=8)), r=[tp], w=[outT_trk])
        else:
            for half in range(2):
                pb = k.psum[(2 * i + half) % 4]
                tp = k.tpsum[(2 * i + half) % 4]
                for c4 in range(4):
                    c = half * 4 + c4
                    cx.op("tensor", lambda h, b=b, c=c, c4=c4, pb=pb: h.transpose(pb[:, c4 * 128:(c4 + 1) * 128], xn[b][:, c * 128:(c + 1) * 128].bitcast(F32), ident),
                          r=[txn[b]], w=[tp], inc=(c4 == 3))
                dst = outT[:, half * 4:(half + 1) * 4, i * 128:(i + 1) * 128]
                if half == 0:
                    cx.op("scalar", lambda h, dst=dst, pb=pb: h.copy(dst, pb.rearrange("p (c t) -> p c t", c=4)), r=[tp], w=[outT_trk])
                else:
                    cx.op("vector", lambda h, dst=dst, pb=pb: h.tensor_copy(dst, pb.rearrange("p (c t) -> p c t", c=4)), r=[tp], w=[outT_trk])
    ar.release(m)


def s5_prep(k):
    cx, ar, D = k.cx, k.ar, k.D
    V = "vector"
    m0 = ar.mark()
    lam = ar.alloc([2, 32], F32)
    step = ar.alloc([32], F32)
    bb = ar.alloc([2, 512], F32)
    cc = ar.alloc([2, 512], F32)
    kvec = ar.alloc([16], F32)
    tl = Trk()
    sl_ = cx.fresh()
    for dst, nm in ((lam, "s5_lam"), (step, "s5_step"), (bb, "s5_b"), (cc, "s5_c"), (kvec, "kvec")):
        cx.dma("sync", dst, D[nm], sl_, w=[tl])
    T = Trk()

    def vop(fn, extra_r=()):
        cx.op(V, fn, r=[T, tl] + list(extra_r), w=[T])

    def aop(fn):
        cx.op("scalar", fn, r=[T, tl], w=[T])
    lre, lim = lam[:, 0, :], lam[:, 1, :]
    dl = ar.alloc([32], F32)
    re1 = ar.alloc([32], F32)
    im1 = ar.alloc([32], F32)
    aop(lambda h: h.activation(dl, step, AF.Exp))
    vop(lambda h: h.tensor_tensor(out=re1, in0=dl, in1=lre, op=ALU.mult))
    vop(lambda h: h.tensor_tensor(out=im1, in0=dl, in1=lim, op=ALU.mult))
    PWI = ar.alloc([16, 32], F32)
    PWR = ar.alloc([16, 32], F32)
    m_pw = ar.mark()
    KR = ar.alloc([16, 32], F32)
    KI = ar.alloc([16, 32], F32)
    kv_b = kvec.unsqueeze(2).to_broadcast([128, 16, 32])
    vop(lambda h: h.tensor_tensor(out=KR, in0=kv_b, in1=re1.unsqueeze(1).to_broadcast([128, 16, 32]), op=ALU.mult))
    vop(lambda h: h.tensor_tensor(out=KI, in0=kv_b, in1=im1.unsqueeze(1).to_broadcast([128, 16, 32]), op=ALU.mult))
    MAG = ar.alloc([16, 32], F32)
    aop(lambda h: h.activation(MAG, KR, AF.Exp))
    YI = ar.alloc([16, 32], I32)
    YF = ar.alloc([16, 32], F32)
    vop(lambda h: h.tensor_scalar(KI, KI, 1.0 / (2 * math.pi), None, op0=ALU.mult))
    vop(lambda h: h.tensor_copy(YI, KI))
    vop(lambda h: h.tensor_copy(YF, YI))
    vop(lambda h: h.tensor_tensor(out=KI, in0=KI, in1=YF, op=ALU.subtract))
    SH_ = ar.alloc([16, 32], F32)
    SQ_ = ar.alloc([16, 32], F32)
    aop(lambda h: h.activation(SH_, KI, AF.Sin, scale=math.pi))
    aop(lambda h: h.activation(SQ_, KI, AF.Sin, scale=math.pi / 2))
    CH_ = ar.alloc([16, 32], F32)
    vop(lambda h: h.tensor_tensor(out=CH_, in0=SQ_, in1=SQ_, op=ALU.mult))
    vop(lambda h: h.tensor_scalar(CH_, CH_, -2.0, 1.0, op0=ALU.mult, op1=ALU.add))
    vop(lambda h: h.tensor_tensor(out=PWI, in0=SH_, in1=CH_, op=ALU.mult))
    vop(lambda h: h.scalar_tensor_tensor(out=PWI, in0=PWI, scalar=2.0, in1=MAG, op0=ALU.mult, op1=ALU.mult))
    vop(lambda h: h.tensor_tensor(out=PWR, in0=SH_, in1=SH_, op=ALU.mult))
    vop(lambda h: h.tensor_scalar(PWR, PWR, -2.0, 1.0, op0=ALU.mult, op1=ALU.add))
    vop(lambda h: h.tensor_tensor(out=PWR, in0=PWR, in1=MAG, op=ALU.mult))
    ar.release(m_pw)
    lrm1 = ar.alloc([32], F32)
    li = PWI[:, 8, :]
    t1 = ar.alloc([32], F32)
    t2 = ar.alloc([32], F32)
    den = ar.alloc([32], F32)
    c0r = ar.alloc([32], F32)
    c0i = ar.alloc([32], F32)
    vop(lambda h: h.tensor_scalar(lrm1, PWR[:, 8, :], -1.0, None, op0=ALU.add))
    vop(lambda h: h.tensor_tensor(out=t1, in0=lre, in1=lre, op=ALU.mult))
    vop(lambda h: h.tensor_tensor(out=t2, in0=lim, in1=lim, op=ALU.mult))
    vop(lambda h: h.tensor_tensor(out=den, in0=t1, in1=t2, op=ALU.add))
    vop(lambda h: h.reciprocal(den, den))
    vop(lambda h: h.tensor_tensor(out=t1, in0=lrm1, in1=lre, op=ALU.mult))
    vop(lambda h: h.tensor_tensor(out=t2, in0=li, in1=lim, op=ALU.mult))
    vop(lambda h: h.tensor_tensor(out=t1, in0=t1, in1=t2, op=ALU.add))
    vop(lambda h: h.tensor_tensor(out=c0r, in0=t1, in1=den, op=ALU.mult))
    vop(lambda h: h.tensor_tensor(out=t1, in0=li, in1=lre, op=ALU.mult))
    vop(lambda h: h.tensor_tensor(out=t2, in0=lrm1, in1=lim, op=ALU.mult))
    vop(lambda h: h.tensor_tensor(out=t1, in0=t1, in1=t2, op=ALU.subtract))
    vop(lambda h: h.tensor_tensor(out=c0i, in0=t1, in1=den, op=ALU.mult))
    BBR = ar.alloc([32, 16], F32)
    BBI = ar.alloc([32, 16], F32)
    TA = ar.alloc([32, 16], F32)
    br = bb[:, 0, :].rearrange("p (a c) -> p a c", c=16)
    bi = bb[:, 1, :].rearrange("p (a c) -> p a c", c=16)
    c0r_b = c0r.unsqueeze(2).to_broadcast([128, 32, 16])
    c0i_b = c0i.unsqueeze(2).to_broadcast([128, 32, 16])
    vop(lambda h: h.tensor_tensor(out=BBR, in0=br, in1=c0r_b, op=ALU.mult))
    vop(lambda h: h.tensor_tensor(out=TA, in0=bi, in1=c0i_b, op=ALU.mult))
    vop(lambda h: h.tensor_tensor(out=BBR, in0=BBR, in1=TA, op=ALU.subtract))
    vop(lambda h: h.tensor_tensor(out=BBI, in0=bi, in1=c0r_b, op=ALU.mult))
    vop(lambda h: h.tensor_tensor(out=TA, in0=br, in1=c0i_b, op=ALU.mult))
    vop(lambda h: h.tensor_tensor(out=BBI, in0=BBI, in1=TA, op=ALU.add))
    ASd = ar.alloc([16, 2, 8, 16], F32)
    CS2d = ar.alloc([16, 2, 8, 16], F32)
    T1 = ar.alloc([8, 16, 16], F32)
    T2 = ar.alloc([8, 16, 16], F32)
    cr = cc[:, 0, :].rearrange("p (d a c) -> p d a c", d=2, c=16)
    ci = cc[:, 1, :].rearrange("p (d a c) -> p d a c", d=2, c=16)
    BBR4 = BBR.rearrange("p (d a) c -> p d a c", d=2)
    BBI4 = BBI.rearrange("p (d a) c -> p d a c", d=2)

    def pw(arr, d, k0, kstep):
        return pap(arr, 0, 128, k0 * 32 + d * 16, [[kstep * 32, 8], [1, 16], [0, 16]])

    def dst(arr, dofs, ri):
        return pap(arr, 0, 128, dofs * 4096 + ri * 128, [[16, 8], [256, 16], [1, 16]])

    def vec(v4, d):
        a_ = v4[:, d]
        return bass.AP(a_.tensor, a_.offset, [list(a_.ap[0]), [0, 8], list(a_.ap[1]), list(a_.ap[2])])

    T1f = T1.rearrange("p a b c -> p (a b c)")

    def cmul(out_arr, dofs, d, k0, kstep, vr, vi, neg_im):
        pr, pi_ = pw(PWR, d, k0, kstep), pw(PWI, d, k0, kstep)
        vop(lambda h: h.tensor_tensor(out=T1, in0=pr, in1=vec(vr, d), op=ALU.mult))
        vop(lambda h: h.tensor_tensor(out=T2, in0=pi_, in1=vec(vi, d), op=ALU.mult))
        vop(lambda h: h.tensor_tensor(out=dst(out_arr, dofs, 0), in0=T1, in1=T2, op=ALU.subtract))
        vop(lambda h: h.tensor_tensor(out=T1, in0=pr, in1=vec(vi, d), op=ALU.mult))
        vop(lambda h: h.tensor_tensor(out=T2, in0=pi_, in1=vec(vr, d), op=ALU.mult))
        if neg_im:
            vop(lambda h: h.tensor_scalar(T1f, T1f, -1.0, None, op0=ALU.mult))
            vop(lambda h: h.tensor_tensor(out=dst(out_arr, dofs, 1), in0=T1, in1=T2, op=ALU.subtract))
        else:
            vop(lambda h: h.tensor_tensor(out=dst(out_arr, dofs, 1), in0=T1, in1=T2, op=ALU.add))
    vop(lambda h: h.tensor_copy(k.s5A1[:, 0:32], PWR[:, 15, :]))
    vop(lambda h: h.tensor_copy(k.s5A1[:, 32:64], PWR[:, 15, :]))
    vop(lambda h: h.tensor_scalar(k.s5A2[:, 0:32], PWI[:, 15, :], -1.0, None, op0=ALU.mult))
    vop(lambda h: h.tensor_copy(k.s5A2[:, 32:64], PWI[:, 15, :]))
    cmul(k.s5CS, 0, 0, 8, 1, cr, ci, True)
    cmul(k.s5CS, 1, 1, 15, -1, cr, ci, True)
    k.t_s5w = T
    mf = ar.alloc([2, 128], F32)
    dv = ar.alloc([32], F32)
    tm = Trk()
    sl_ = cx.fresh()
    cx.dma("sync", mf[:, 0, :], D["s5mf"], sl_, w=[tm])
    cx.dma("sync", mf[:, 1, :], D["s5mb"], sl_, w=[tm])
    cx.dma("sync", dv, D["s5_dvec"], sl_, w=[tm])
    tt1 = [ar.alloc([128], F32) for _ in range(2)]
    ttt = [Trk(), Trk()]
    ASb = ASd.rearrange("p a r s c -> p (a r) (s c)")
    ASm = ASd.rearrange("p a r s c -> p a r (s c)")
    CSm = CS2d.rearrange("p a r s c -> p a r (s c)")
    for d in range(2):
        if d == 0:
            cmul(ASd, 0, 0, 14, -1, BBR4, BBI4, False)
            cmul(CS2d, 0, 0, 0, 1, cr, ci, True)
        else:
            cmul(ASd, 0, 1, 7, 1, BBR4, BBI4, False)
            cmul(CS2d, 0, 1, 7, -1, cr, ci, True)
        for grp in range(8):
            pb = k.psum[grp % 4]
            tp = k.tpsum[grp % 4]
            for j in range(4):
                blk = grp * 4 + j
                cx.op("tensor", lambda h, pb=pb, j=j, blk=blk: h.transpose(pb[:, j * 128:(j + 1) * 128], ASb[:, blk, :], k.ident),
                      r=[T], w=[tp], inc=(j == 3))
            dstv = k.s5AT[:, d * 32 + grp * 4:d * 32 + (grp + 1) * 4, :]
            if grp % 2 == 0:
                cx.op("scalar", lambda h, dstv=dstv, pb=pb: h.copy(dstv, pb.rearrange("p (j x) -> p j x", j=4)), r=[tp], w=[k.t_s5at])
            else:
                cx.op("vector", lambda h, dstv=dstv, pb=pb: h.tensor_copy(dstv, pb.rearrange("p (j x) -> p j x", j=4)), r=[tp], w=[k.t_s5at])
        for g in range(32):
            gh, gl = g // 16, g % 16
            pb = k.psum[4 + g % 4]
            tp = k.tpsum[4 + g % 4]
            for ri in range(2):
                cx.op("tensor", lambda h, pb=pb, ri=ri, gh=gh, gl=gl: h.matmul(
                    pb[:, 0:128], ASm[gh * 64:(gh + 1) * 64, gl, ri, :], CSm[gh * 64:(gh + 1) * 64, gl, ri, :],
                    start=(ri == 0), stop=(ri == 1)), r=[T], w=[tp], inc=(ri == 1))
            b_ = g % 2
            cx.op(V, lambda h, pb=pb, b_=b_, d=d: h.tensor_tensor(out=tt1[b_], in0=pb[:, 0:128], in1=mf[:, d, :], op=ALU.mult), r=[tp, tm], w=[ttt[b_]])
            if d == 0:
                cx.op(V, lambda h, b_=b_, g=g: h.scalar_tensor_tensor(out=k.s5TT[:, g, :], in0=k.ident, scalar=dv[:, g:g + 1], in1=tt1[b_], op0=ALU.mult, op1=ALU.add),
                      r=[ttt[b_], tm], w=[k.t_s5tt])
            else:
                cx.op(V, lambda h, b_=b_, g=g: h.tensor_tensor(out=k.s5TT[:, g, :], in0=k.s5TT[:, g, :], in1=tt1[b_], op=ALU.add),
                      r=[ttt[b_]], w=[k.t_s5tt])
    cx.barrier()
    ar.release(m0)


def load_w_cols(k, wdram, col0, ncols, dst, trk, slot, eng="gpsimd"):
    src = bass.AP(wdram.tensor, wdram.offset + col0, [[wdram.ap[0][0] * 1, 128], [wdram.ap[0][0] * 128, 8], [1, ncols]])
    return k.cx.dma(eng, dst, src, slot, w=[trk])


def proj_fm(k, wt, wtrk, consume):
    cx = k.cx
    for n in range(4):
        pb = k.psum[n % 2 + 2]
        tp = k.tpsum[n % 2 + 2]
        for c in range(8):
            cx.op("tensor", lambda h, pb=pb, c=c, n=n: h.matmul(pb[:, :], wt[:, c, :], k.hT[:, c, n * 512:(n + 1) * 512], start=(c == 0), stop=(c == 7)),
                  r=[wtrk, k.t_hT], w=[tp], inc=(c == 7))
        consume(n, pb, tp)


def s5_build_U(k):
    cx, ar = k.cx, k.ar
    m0 = ar.mark()
    wt = [ar.alloc([8, 128], BF16) for _ in range(2)]
    twt = [Trk(), Trk()]
    swt = [cx.fresh('sw'), cx.fresh('sw')]
    uT = [ar.alloc([2048], BF16) for _ in range(2)]
    tuT = [Trk(), Trk()]
    for ct in range(4):
        b = ct % 2
        load_w_cols(k, k.D["w_in"], ct * 128, 128, wt[b], twt[b], swt[b])

        def consume(n, pb, tp, b=b):
            if n % 2 == 0:
                cx.op("scalar", lambda h: h.copy(uT[b][:, n * 512:(n + 1) * 512], pb[:, :]), r=[tp], w=[tuT[b]])
            else:
                cx.op("vector", lambda h: h.tensor_copy(uT[b][:, n * 512:(n + 1) * 512], pb[:, :]), r=[tp], w=[tuT[b]])
        proj_fm(k, wt[b], twt[b], consume)
        for gi in range(8):
            g = ct * 8 + gi
            q0 = 32 * (gi // 2)
            pb = k.psum[4 + gi % 4]
            tp = k.tpsum[4 + gi % 4]
            for s in range(8):
                rhs = pap(uT[b], q0, 32, s, [[8, 256]])
                cx.op("tensor", lambda h, pb=pb, s=s, rhs=rhs, q0=q0, gi=gi: h.matmul(pb[:, 0:256], k.selT[q0:q0 + 32, gi % 2, s, :], rhs, start=(s == 0), stop=(s == 7), tile_position=(q0, 0)),
                      r=[tuT[b]], w=[tp], inc=(s == 7))
            if gi % 2 == 0:
                cx.op("scalar", lambda h, pb=pb, g=g: h.copy(k.s5U[:, g, :], pb[:, 0:256]), r=[tp], w=[k.t_s5U])
            else:
                cx.op("vector", lambda h, pb=pb, g=g: h.tensor_copy(k.s5U[:, g, :], pb[:, 0:256]), r=[tp], w=[k.t_s5U])
    cx.barrier()
    ar.release(m0)


def s5_main(k, yT, t_yT):
    cx, ar, D = k.cx, k.ar, k.D
    V = "vector"
    m0 = ar.mark()
    SH = ar.alloc([2, 257, 2, 16], BF16)
    tSH = Trk()
    tSHh = Trk()
    X = [ar.alloc([64], F32) for _ in range(3)]
    tX = [Trk() for _ in range(3)]
    t1 = ar.alloc([64], F32)
    t2 = ar.alloc([64], F32)
    tt = Trk()
    tt2 = Trk()
    cx.op("gpsimd", lambda h: h.memset(SH[:, 0, 0, :, :], 0.0), w=[tSH])
    cx.op("gpsimd", lambda h: h.memset(SH[:, 1, 256, :, :], 0.0), w=[tSH])
    cx.op("gpsimd", lambda h: h.memset(X[0], 0.0), w=[tX[0]])
    n = 0
    for gl in range(16):
        for d in range(2):
            for ri in range(2):
                blk = d * 32 + gl * 2 + ri
                pb = k.psum[n % 4]
                tp = k.tpsum[n % 4]
                cx.op("tensor", lambda h, pb=pb, blk=blk, gl=gl: h.matmul(pb[0:64, 0:256], k.s5AT[:, blk, 0:64], k.s5U[:, gl, :], start=True, stop=True),
                      r=[k.t_s5at, k.t_s5U], w=[tp], inc=False)
                cx.op("tensor", lambda h, pb=pb, blk=blk, gl=gl: h.matmul(pb[64:128, 0:256], k.s5AT[:, blk, 64:128], k.s5U[:, 16 + gl, :], start=True, stop=True),
                      r=[k.t_s5at, k.t_s5U], w=[tp])
                slot0 = 1 if d == 0 else 0
                dstv = pap(SH, 0, 128, d * 257 * 32 + slot0 * 32 + ri * 16 + gl, [[32, 256]])
                if n % 2 == 0:
                    cx.op("scalar", lambda h, dstv=dstv, pb=pb: h.copy(dstv, pb[:, 0:256]), r=[tp], w=[tSH])
                else:
                    cx.op(V, lambda h, dstv=dstv, pb=pb: h.tensor_copy(dstv, pb[:, 0:256]), r=[tp], w=[tSH])
                n += 1
    import os
    S5STOP = os.environ.get('S5_STOP', '')
    if S5STOP == 'a':
        cx.barrier(); ar.release(m0); return
    for i in range(256):
        xp, xn = X[i % 3], X[(i + 1) % 3]
        txp, txn = tX[i % 3], tX[(i + 1) % 3]
        xsw = pap(xp, 0, 128, 32, [[-32, 2], [1, 32]])
        bf = (i + 1) * 32
        bb_ = 257 * 32 + (255 - i) * 32
        sview = pap(SH, 0, 128, bf, [[16, 2], [bb_ - bf, 2], [1, 16]])
        xp3 = xp.rearrange("p (r x) -> p r x", r=2)
        cx.op("gpsimd", lambda h, xsw=xsw: h.tensor_tensor(out=t2.rearrange("p (r x) -> p r x", r=2), in0=k.s5A2.rearrange("p (r x) -> p r x", r=2), in1=xsw, op=ALU.mult), r=[txp, k.t_s5w], w=[tt2])
        cx.op(V, lambda h, xp=xp: h.tensor_tensor(out=t1, in0=k.s5A1, in1=xp, op=ALU.mult), r=[txp, k.t_s5w], w=[tt])
        cx.op(V, lambda h, sview=sview: h.tensor_tensor(out=t1.rearrange("p (r d x) -> p r d x", r=2, d=2), in0=t1.rearrange("p (r d x) -> p r d x", r=2, d=2), in1=sview, op=ALU.add), r=[tt, tSH], w=[tt])
        cx.op(V, lambda h, xn=xn: h.tensor_tensor(out=xn, in0=t1, in1=t2, op=ALU.add), r=[tt, tt2], w=[txn])
        cx.op("scalar", lambda h, xn=xn, sview=sview: h.copy(sview, xn.rearrange("p (r d x) -> p r d x", r=2, d=2)), r=[txn], w=[tSHh])
    if S5STOP == 'rec':
        cx.barrier(); ar.release(m0); return
    gT = ar.alloc([4, 2048], F32)
    gTb = ar.alloc([4, 2048], BF16)
    tgT = [Trk() for _ in range(4)]
    tgTb = [Trk() for _ in range(4)]
    ybuf = [k.arA.alloc([8, 256], BF16) for _ in range(2)]
    tyb = [Trk(), Trk()]
    for ct in range(4):
        b = ct % 2
        for gi in range(8):
            g = ct * 8 + gi
            gh, gl = g // 16, g % 16
            pb = k.psum[gi % 2]
            tp = k.tpsum[gi % 2]
            cx.op("tensor", lambda h, pb=pb, g=g: h.matmul(pb[:, 0:256], k.s5TT[:, g, :], k.s5U[:, g, :], start=True, stop=False),
                  r=[k.t_s5tt, k.t_s5U], w=[tp], inc=False)
            for d in range(2):
                for ri in range(2):
                    slot0 = 0 if d == 0 else 1
                    rhs = pap(SH, gh * 64, 64, d * 257 * 32 + slot0 * 32 + ri * 16 + gl, [[32, 256]])
                    last = (d == 1 and ri == 1)
                    cx.op("tensor", lambda h, pb=pb, rhs=rhs, d=d, ri=ri, gh=gh, gl=gl, last=last: h.matmul(
                        pb[:, 0:256], k.s5CS[gh * 64:(gh + 1) * 64, d, gl, ri, :], rhs, start=False, stop=last),
                        r=[tSH, tSHh, k.t_s5w], w=[tp], inc=last)
            if gi % 2 == 0:
                cx.op("scalar", lambda h, pb=pb, b=b, gi=gi: h.copy(ybuf[b][:, gi, :], pb[:, 0:256]), r=[tp], w=[tyb[b]])
            else:
                cx.op(V, lambda h, pb=pb, b=b, gi=gi: h.tensor_copy(ybuf[b][:, gi, :], pb[:, 0:256]), r=[tp], w=[tyb[b]])
        for t in range(8):
            q0 = 32 * (t // 2)
            pb = k.psum[2 + t % 4]
            tp = k.tpsum[2 + t % 4]
            for gi in range(8):
                cx.op("tensor", lambda h, pb=pb, t=t, gi=gi, q0=q0, b=b: h.matmul(pb[:, 0:256], k.selT[q0:q0 + 32, t % 2, gi, :], ybuf[b][q0:q0 + 32, gi, :], start=(gi == 0), stop=(gi == 7), tile_position=(q0, 0)),
                      r=[tyb[b]], w=[tp], inc=(gi == 7))
            dstv = pap(gT, 0, 128, ct * 2048 + t, [[8, 256]])
            cx.op("scalar", lambda h, pb=pb, dstv=dstv: h.activation(dstv, pb[:, 0:256], AF.Gelu), r=[tp], w=[tgT[ct]])
        cx.op("vector", lambda h, ct=ct: h.tensor_copy(gTb[:, ct, :], gT[:, ct, :]), r=[tgT[ct]], w=[tgTb[ct]])
    k.dbg_add("s5_g", gT, tgT)
    if S5STOP == 'c':
        cx.barrier(); ar.release(m0); return
    wg = ar.alloc([4, 512], BF16)
    twg = Trk()
    wsrc = D["s5_w_glu"]
    cx.dma("gpsimd", wg, bass.AP(wsrc.tensor, wsrc.offset, [[512, 128], [512 * 128, 4], [1, 512]]), cx.fresh('sw'), w=[twg])
    bgl = ar.alloc([4], F32)
    nw = ar.alloc([4], F32)
    tb = Trk()
    sl_ = cx.fresh()
    cx.dma("sync", bgl, D["s5_bglu"], sl_, w=[tb])
    cx.dma("sync", nw, D["s5_normw"], sl_, w=[tb])
    sig = ar.alloc([4, 512], BF16)
    tsig = Trk()
    sq = ar.alloc([4, 512], BF16)
    tsq = Trk()
    rs = ar.alloc([512], F32)
    trs = Trk()
    for nck in range(4):
        ts = slice(nck * 512, (nck + 1) * 512)
        for co in range(4):
            pb = k.psum[co % 2]
            tp = k.tpsum[co % 2]
            for ci in range(4):
                cx.op("tensor", lambda h, pb=pb, co=co, ci=ci, ts=ts: h.matmul(pb[:, :], wg[:, ci, co * 128:(co + 1) * 128], gTb[:, ci, ts], start=(ci == 0), stop=(ci == 3)),
                      r=[twg] + tgTb, w=[tp], inc=(ci == 3))
            cx.op("scalar", lambda h, pb=pb, co=co: h.activation(sig[:, co, :], pb[:, :], AF.Sigmoid, bias=bgl[:, co:co + 1]), r=[tp, tb], w=[tsig])
        for co in range(4):
            cx.op(V, lambda h, co=co, ts=ts: h.tensor_tensor(out=gT[:, co, ts], in0=gT[:, co, ts], in1=sig[:, co, :], op=ALU.mult), r=[tsig, tgT[co]], w=[tgT[co]])
            cx.op("scalar", lambda h, co=co, ts=ts: h.activation(sq[:, co, :], gT[:, co, ts], AF.Square), r=[tgT[co]], w=[tsq])
        pb = k.psum[2 + nck % 2]
        tp = k.tpsum[2 + nck % 2]
        for co in range(4):
            cx.op("tensor", lambda h, pb=pb, co=co: h.matmul(pb[:, :], k.onesb, sq[:, co, :], start=(co == 0), stop=(co == 3)), r=[tsq], w=[tp], inc=(co == 3))
        cx.op("scalar", lambda h, pb=pb: h.activation(rs, pb[:, :], AF.Sqrt, scale=1.0 / 512, bias=k.epsc), r=[tp], w=[trs])
        cx.op(V, lambda h: h.reciprocal(rs, rs), r=[trs], w=[trs])
        for co in range(4):
            cx.op(V, lambda h, co=co, ts=ts: h.scalar_tensor_tensor(out=yT[:, co, ts], in0=gT[:, co, ts], scalar=nw[:, co:co + 1], in1=rs, op0=ALU.mult, op1=ALU.mult),
                  r=[tgT[co], trs, tb, tsq], w=[t_yT])
    k.dbg_add("s5_gl", gT, tgT)
    cx.barrier()
    ar.release(m0)


def build(in_shapes, stage="full", dbg_names=(), n_heads=4, n_experts=32):
    nc = bass.Bass("TRN2", target_bir_lowering=False)
    k = K()
    k.n_heads = n_heads
    k.n_experts = n_experts
    k.nc = nc
    D = {}
    for nm, (shape, dt) in in_shapes.items():
        D[nm] = nc.dram_tensor(nm, list(shape), dt, kind="ExternalInput").ap()
    k.D = D
    out = nc.dram_tensor("out", [S, DM], F32, kind="ExternalOutput").ap()
    k.dbg = {}
    k.dbg_req = set(dbg_names)

    with contextlib.ExitStack() as st:
        cx = Ctx(nc, st)
        k.cx = cx

        def finish():
            deps = [(s_.key, s_.total) for s_ in cx.slots if s_.total > 0]
            cx.wait_deps("sync", deps + [(e, cx.cnt[e]) for e in ENGS if e != "sync" and cx.cnt[e] > 0])
            with nc.Block() as block:
                cx.emit_all(block)
            k.n_ops = cx.n_ops
            return nc, k
        k.slot_c = cx.slot("c")
        k.slot_w = cx.slot("w")
        k.slot_x = [cx.slot("x0"), cx.slot("x1")]
        k.slot_o = cx.slot("o")
        k.psum = [cx.ps("ps%d" % i, [128, 512], F32) for i in range(8)]
        k.psum = [p[:, :] for p in k.psum]
        k.tpsum = [Trk("ps%d" % i, excl=True) for i in range(8)]
        k.ident = cx.sb("ident", [128, 128], F32)[:, :]
        k.identb = cx.sb("identb", [128, 128], BF16)[:, :]
        k.ones = cx.sb("ones", [128, 128], F32)[:, :]
        k.onesb = cx.sb("onesb", [128, 128], BF16)[:, :]
        k.epsc = cx.sb("epsc", [128, 1], F32)[:, :]
        k.selT = cx.sb("selT", [128, 2, 8, 128], BF16)[:, :, :, :]
        tc = Trk()
        cx.dma("sync", k.ident, D["ident"], k.slot_c, w=[tc])
        cx.dma("sync", k.identb, D["identb"], k.slot_c, w=[tc])
        cx.dma("sync", k.ones, D["ones"], k.slot_c, w=[tc])
        cx.dma("gpsimd", k.onesb, D["ones"], cx.fresh("sw"), w=[tc])
        cx.op("vector", lambda h: h.memset(k.epsc, EPS), w=[tc])
        cx.dma("sync", k.selT, D["selT"], k.slot_c, w=[tc])
        k.s5A1 = cx.sb("s5A1", [128, 64], F32)[:, :]
        k.s5A2 = cx.sb("s5A2", [128, 64], F32)[:, :]
        ar = Arena(cx, 51456)
        k.ar = ar
        cx.barrier()

        def dbg_add(name, ap, trks):
            if name in k.dbg_req:
                shape = list(ap.shape)
                dt_ = F32
                o = nc.dram_tensor("dbg_" + name, shape, dt_, kind="ExternalOutput").ap()
                cx.dma("gpsimd" if ap.dtype != F32 else "sync", o, ap, cx.fresh("sw" if ap.dtype != F32 else "hw"), r=list(trks))
        k.dbg_add = dbg_add

        regA = ar.alloc([NT * 1024], F32)
        arA = Arena(cx, NT * 1024, base=regA)
        k.arA = arA
        yT = ar.alloc([8, 2048], BF16)
        t_yT = Trk()
        k.s5U = arA.alloc([32, 256], BF16)
        k.t_s5U = Trk()
        m_h = arA.mark()
        k.hT = arA.alloc([8, 2048], BF16)
        k.t_hT = Trk()

        m1 = ar.mark()
        xt = [ar.alloc([1024], F32) for _ in range(2)]
        txt = [Trk(), Trk()]

        def src_x(i):
            b = i % 2
            cx.dma("sync", xt[b], D["x"][i * 128:(i + 1) * 128, :], k.slot_x[b], w=[txt[b]])
            return xt[b], txt[b]
        norm_transpose(k, "mix", src_x, NT, D["norm_mix"], k.hT, BF16, k.t_hT)
        cx.barrier()
        ar.release(m1)

        if stage == 'p1':
            return finish()
        s5_build_U(k)
        if stage == 'U':
            return finish()
        mg = ar.mark()
        gdn_setup(k)
        for hd in range(k.n_heads):
            gdn_head(k, hd, yT, t_yT)
        ar.release(mg)
        k.dbg_add("ygdnT", yT[:, 4:8, :], [t_yT])
        if stage == 'gdn':
            return finish()
        cx.barrier()
        arA.release(m_h)
        k.s5AT = arA.alloc([64, 128], BF16)
        k.t_s5at = Trk()
        k.s5CS = arA.alloc([2, 16, 2, 128], BF16)
        k.s5TT = arA.alloc([32, 128], BF16)
        k.t_s5tt = Trk()
        s5_prep(k)
        if stage == 's5prep':
            return finish()
        s5_main(k, yT[:, 0:4, :], t_yT)
        k.dbg_add("ys5T", yT[:, 0:4, :], [t_yT])
        if stage == "s5":
            return finish()
        if True:
            cx.barrier()
            k.xacc = regA.rearrange('p (a b) -> p a b', a=NT)
            k.txacc = [Trk() for _ in range(NT)]
            out_proj(k, yT, t_yT)
            k.dbg_add("x1", k.xacc, k.txacc)
            if stage == 'oproj':
                return finish()
            xattn(k)
            if stage == 'xattn':
                return finish()
            k.dbg_add("x2", k.xacc, k.txacc)
            moe(k)
            k.dbg_add("x3", k.xacc, k.txacc + [t_ for p_ in k.txh for t_ in p_])
            final_norm(k, out)

        return finish()


def host_inputs(inp, b):
    m = {}
    m["x"] = np.ascontiguousarray(inp["x"][b])
    m["mem"] = np.ascontiguousarray(inp["mem"][b])
    m["norm_mix"] = inp["norm_mix"][0]
    m["w_in"] = inp["w_in"][0]
    m["w_out"] = inp["w_out"][0]
    m["s5_w_glu"] = inp["s5_w_glu"][0]
    m.update(host_s5(inp))
    cv = inp["gdn_conv"][0]
    m["gdn_convw"] = np.ascontiguousarray(cv.reshape(5, 3, 4, 128).transpose(3, 2, 1, 0))
    for nm in ("gdn_a_log_f", "gdn_dt_bias_f", "gdn_a_log_b", "gdn_dt_bias_b"):
        m[nm] = inp[nm][0]
    m["gdn_norm"] = inp["gdn_norm"][0]
    for nm in ("norm_xattn", "norm_mem", "xa_wq", "xa_wk", "xa_wv", "xa_wo", "norm_moe", "router_group_w", "router_group_b",
               "router_expert_w", "router_expert_b", "moe_w_gate", "moe_w_up", "moe_w_down"):
        m[nm] = inp[nm][0]
    m["norm_final"] = inp["norm_final"]
    m.update(host_consts())
    return m


def gdn_setup(k):
    cx, ar, D = k.cx, k.ar, k.D
    V = "vector"
    G = K()
    k.G = G
    G.mask = ar.alloc([7, 128], F32)
    G.tmask = Trk()
    cx.dma("sync", G.mask, D["gmask"][:, 0:7, :], cx.fresh(), w=[G.tmask])
    wsm = ar.alloc([8, 16], BF16)
    tw = Trk()
    load_w_cols(k, D["w_in"], 2560, 16, wsm, tw, cx.fresh('sw'))
    BA = ar.alloc([16, 16], F32)
    tBA = Trk()
    for i in range(NT):
        pb = k.psum[i % 4]
        tp = k.tpsum[i % 4]
        for c in range(8):
            cx.op("tensor", lambda h, pb=pb, c=c, i=i: h.matmul(pb[:, 0:16], k.hT[:, c, i * 128:(i + 1) * 128], wsm[:, c, :], start=(c == 0), stop=(c == 7)),
                  r=[tw, k.t_hT], w=[tp], inc=(c == 7))
        cx.op("scalar", lambda h, pb=pb, i=i: h.copy(BA[:, i, :], pb[:, 0:16]), r=[tp], w=[tBA])
    pr = ar.alloc([4, 4], F32)
    tpr = Trk()
    sl_ = cx.fresh()
    for j, nm in enumerate(("gdn_a_log_f", "gdn_dt_bias_f", "gdn_a_log_b", "gdn_dt_bias_b")):
        cx.dma("sync", pr[:, j, :], dram_bcast(D[nm], 128, 4), sl_, w=[tpr])
    G.nw = ar.alloc([128], F32)
    cx.dma("sync", G.nw, dram_bcast(D["gdn_norm"], 128, 128), sl_, w=[tpr])
    G.tpr = tpr
    T = Trk()
    G.T = T
    G.beta, G.nb, G.gc, G.eg, G.neg, G.ed = [], [], [], [], [], []
    def per_dir(d):
        beta = ar.alloc([16, 4], F32)
        nb = ar.alloc([16, 4], F32)
        g = ar.alloc([16, 4], F32)
        gc = ar.alloc([16, 4], F32)
        gt = ar.alloc([16, 4], F32)
        eg = ar.alloc([16, 4], F32)
        neg = ar.alloc([16, 4], F32)
        ed = ar.alloc([16, 4], F32)
        ea = ar.alloc([4], F32)
        braw = BA[:, :, d * 4:(d + 1) * 4]
        araw = BA[:, :, 8 + d * 4:8 + (d + 1) * 4]
        cx.op("scalar", lambda h: h.activation(beta, braw, AF.Sigmoid), r=[tBA, T], w=[T])
        cx.op(V, lambda h: h.tensor_scalar(nb, beta, -1.0, None, op0=ALU.mult), r=[T], w=[T])
        cx.op("scalar", lambda h: h.activation(ea, pr[:, 2 * d, :], AF.Exp), r=[tpr, T], w=[T])
        cx.op(V, lambda h: h.tensor_tensor(out=g, in0=araw, in1=pr[:, 2 * d + 1, :].unsqueeze(1).to_broadcast([128, 16, 4]), op=ALU.add), r=[tBA, tpr, T], w=[T])
        cx.op("scalar", lambda h: h.activation(g, g, AF.Exp), r=[T], w=[T])
        cx.op("scalar", lambda h: h.activation(g, g, AF.Ln, bias=1.0), r=[T], w=[T])
        cx.op(V, lambda h: h.scalar_tensor_tensor(out=g, in0=g, scalar=-1.0, in1=ea.unsqueeze(1).to_broadcast([128, 16, 4]), op0=ALU.mult, op1=ALU.mult), r=[T], w=[T])
        g2 = g.rearrange("p a b -> p (a b)")
        pb = k.psum[4 + d]
        tp = k.tpsum[4 + d]
        cx.op("tensor", lambda h, pb=pb, d=d: h.matmul(pb[:, 0:64], G.mask[:, d, :], g2, start=True, stop=True), r=[T, G.tmask], w=[tp])
        cx.op("tensor", lambda h, pb=pb: h.matmul(pb[:, 64:128], G.mask[:, 6, :], g2, start=True, stop=True), r=[T, G.tmask], w=[tp])
        cx.op(V, lambda h, pb=pb: h.tensor_copy(gc.rearrange("p a b -> p (a b)"), pb[:, 0:64]), r=[tp], w=[T])
        cx.op(V, lambda h, pb=pb: h.tensor_tensor(out=gt.rearrange("p a b -> p (a b)"), in0=pb[:, 64:128], in1=gc.rearrange("p a b -> p (a b)"), op=ALU.subtract), r=[tp, T], w=[T])
        cx.op("scalar", lambda h: h.activation(eg, gc, AF.Exp), r=[T], w=[T])
        cx.op("scalar", lambda h: h.activation(ed, gt, AF.Exp), r=[T], w=[T])
        cx.op(V, lambda h: h.tensor_scalar(neg, eg, -1.0, None, op0=ALU.mult), r=[T], w=[T])
        G.g = getattr(G, "g", []) + [g]
        G.beta.append(beta); G.nb.append(nb); G.gc.append(gc); G.eg.append(eg); G.neg.append(neg); G.ed.append(ed)
    per_dir(0)
    per_dir(1)
    G.osum = ar.alloc([16, 128], F32)
    G.tosum = [Trk() for _ in range(NT)]


def gdn_head(k, hd, yT, t_yT):
    cx, ar, D, G = k.cx, k.ar, k.D, k.G
    V = "vector"
    m0 = ar.mark()
    qnT = ar.alloc([2048], BF16)
    knT = ar.alloc([2048], BF16)
    Ktok = ar.alloc([16, 128], BF16)
    Vtok = ar.alloc([16, 128], BF16)
    tq, tk_, tKt, tVt = Trk(), Trk(), Trk(), Trk()
    wz = ar.alloc([8, 128], BF16)
    twz = Trk()
    load_w_cols(k, D["w_in"], 512 + 1536 + hd * 128, 128, wz, twz, cx.fresh('sw'))
    mA = ar.mark()
    w3 = [ar.alloc([8, 128], BF16) for _ in range(3)]
    tw3 = [Trk() for _ in range(3)]
    for j in range(3):
        load_w_cols(k, D["w_in"], 512 + j * 512 + hd * 128, 128, w3[j], tw3[j], cx.fresh('sw'))
    cw = ar.alloc([3, 5], F32)
    tcw = Trk()
    cx.dma("sync", cw, D["gdn_convw"][:, hd, :, :], cx.fresh(), w=[tcw])
    diag = ar.alloc([15, 128], BF16)
    tdg = Trk()
    for j in range(3):
        for t in range(5):
            cx.op("vector", lambda h, j=j, t=t: h.tensor_scalar(diag[:, j * 5 + t, :], k.identb, cw[:, j, t:t + 1], None, op0=ALU.mult), r=[tcw], w=[tdg])
    import os
    ALV = int(os.environ.get("GDN_ALV", "9"))
    if ALV == 0:
        cx.barrier(); ar.release(m0); return
    raw = [ar.alloc([2052], BF16) for _ in range(2)]
    traw = [Trk(), Trk()]
    for b in range(2):
        cx.op("gpsimd", lambda h, b=b: h.memset(raw[b][:, 0:2], 0.0), w=[traw[b]])
        cx.op("gpsimd", lambda h, b=b: h.memset(raw[b][:, 2050:2052], 0.0), w=[traw[b]])
    act = ar.alloc([2048], F32)
    tact = Trk()
    vT = ar.alloc([2048], BF16)
    tvT = Trk()
    sqb = ar.alloc([2048], BF16)
    tsqb = Trk()
    rn = [ar.alloc([512], F32) for _ in range(4)]
    trn = [Trk() for _ in range(4)]
    tactn = [Trk() for _ in range(4)]
    tsqn = [Trk() for _ in range(4)]
    if ALV == 1:
        cx.barrier(); ar.release(m0); return
    for j in range(3):
        b = j % 2

        def consume(n, pb, tp, b=b):
            cx.op(V if n % 2 else "scalar", (lambda h: h.tensor_copy(raw[b][:, 2 + n * 512:2 + (n + 1) * 512], pb[:, :])) if n % 2 else
                  (lambda h: h.copy(raw[b][:, 2 + n * 512:2 + (n + 1) * 512], pb[:, :])), r=[tp], w=[traw[b]])
        proj_fm(k, w3[j], tw3[j], consume)
        for n in range(4):
            pb = k.psum[4 + n]
            tp = k.tpsum[4 + n]
            for t in range(5):
                cx.op("tensor", lambda h, pb=pb, t=t, n=n, j=j, b=b: h.matmul(pb[:, :], diag[:, j * 5 + t, :], raw[b][:, n * 512 + t:n * 512 + t + 512], start=(t == 0), stop=(t == 4)),
                      r=[tdg, traw[b]], w=[tp], inc=(t == 4))
        for n in range(4):
            pb = k.psum[4 + n]
            tp = k.tpsum[4 + n]
            ts = slice(n * 512, (n + 1) * 512)
            if j == 2:
                cx.op("scalar", lambda h, pb=pb, ts=ts: h.activation(vT[:, ts], pb[:, :], AF.Silu), r=[tp], w=[tvT])
            else:
                cx.op("scalar", lambda h, pb=pb, ts=ts: h.activation(act[:, ts], pb[:, :], AF.Silu), r=[tp], w=[tactn[n]])
        if j < 2 and ALV > 2:
            for n in range(4):
                ts = slice(n * 512, (n + 1) * 512)
                cx.op("scalar", lambda h, ts=ts: h.activation(sqb[:, ts], act[:, ts], AF.Square), r=[tactn[n]], w=[tsqn[n]])
            for n in range(4):
                ts = slice(n * 512, (n + 1) * 512)
                pb2 = k.psum[n]
                tp2 = k.tpsum[n]
                cx.op("tensor", lambda h, pb2=pb2, ts=ts: h.matmul(pb2[:, :], k.onesb, sqb[:, ts], start=True, stop=True), r=[tsqn[n]], w=[tp2])
            for n in range(4):
                pb2 = k.psum[n]
                tp2 = k.tpsum[n]
                cx.op("scalar", lambda h, pb2=pb2, n=n: h.activation(rn[n], pb2[:, :], AF.Sqrt, bias=k.epsc), r=[tp2], w=[trn[n]])
            for n in range(4):
                cx.op(V, lambda h, n=n: h.reciprocal(rn[n], rn[n]), r=[trn[n]], w=[trn[n]])
            dstT, tdst, scl = (qnT, tq, 128.0 ** -0.5) if j == 0 else (knT, tk_, 1.0)
            for n in range(4):
                ts = slice(n * 512, (n + 1) * 512)
                cx.op(V, lambda h, ts=ts, n=n, dstT=dstT, scl=scl: h.scalar_tensor_tensor(out=dstT[:, ts], in0=act[:, ts], scalar=scl, in1=rn[n], op0=ALU.mult, op1=ALU.mult),
                      r=[tactn[n], trn[n]], w=[tdst])
    if ALV <= 3:
        cx.barrier(); ar.release(m0); return
    TV = int(os.environ.get("GDN_TV", "0"))
    for i in range(NT):
        pb = k.psum[i % 2].bitcast(BF16)
        tp = k.tpsum[i % 2]
        if TV == 0:
            cx.op("tensor", lambda h, pb=pb, i=i: h.transpose(pb[:, 0:128], knT[:, i * 128:(i + 1) * 128], k.identb), r=[tk_], w=[tp])
            cx.op("tensor", lambda h, pb=pb, i=i: h.transpose(pb[:, 128:256], vT[:, i * 128:(i + 1) * 128], k.identb), r=[tvT], w=[tp])
            cx.op("scalar", lambda h, pb=pb, i=i: h.copy(Ktok[:, i, :], pb[:, 0:128]), r=[tp], w=[tKt])
            cx.op(V, lambda h, pb=pb, i=i: h.tensor_copy(Vtok[:, i, :], pb[:, 128:256]), r=[tp], w=[tVt])
        elif TV == 1:
            cx.op("tensor", lambda h, pb=pb, i=i: h.transpose(pb[:, 0:128], knT[:, i * 128:(i + 1) * 128], k.identb), r=[tk_], w=[tp])
            cx.op("scalar", lambda h, pb=pb, i=i: h.copy(Ktok[:, i, :], pb[:, 0:128]), r=[tp], w=[tKt])
        elif TV == 2:
            cx.op("tensor", lambda h, pb=pb, i=i: h.transpose(pb[:, 0:128], vT[:, i * 128:(i + 1) * 128], k.identb), r=[tvT], w=[tp])
            cx.op(V, lambda h, pb=pb, i=i: h.tensor_copy(Vtok[:, i, :], pb[:, 0:128]), r=[tp], w=[tVt])
    if hd == 0:
        k.dbg_add("gdn_qn", qnT, [tq])
        k.dbg_add("gdn_kn", knT, [tk_])
        k.dbg_add("gdn_vtok", Vtok, [tVt])
    cx.barrier()
    ar.release(mA)
    STOP = os.environ.get("GDN_STOP", "")
    if STOP == "A":
        ar.release(m0)
        return
    qgT = [ar.alloc([2048], BF16) for _ in range(2)]
    Kd = [ar.alloc([16, 128], BF16) for _ in range(2)]
    Pm = [ar.alloc([16, 128], BF16) for _ in range(2)]
    QKm = [ar.alloc([16, 128], BF16) for _ in range(2)]
    etot = [ar.alloc([32], F32) for _ in range(2)]
    WnT = [ar.alloc([2048], BF16) for _ in range(2)]
    U0b = [ar.alloc([16, 128], BF16) for _ in range(2)]
    tWn = [[Trk() for _ in range(NT)] for _ in range(2)]
    tU0 = [[Trk() for _ in range(NT)] for _ in range(2)]
    tqg = [[Trk() for _ in range(NT)] for _ in range(2)]
    tKd = [[Trk() for _ in range(NT)] for _ in range(2)]
    tPm = [[Trk() for _ in range(NT)] for _ in range(2)]
    tQK = [[Trk() for _ in range(NT)] for _ in range(2)]
    tet = [[Trk() for _ in range(NT)] for _ in range(2)]
    NI = 8
    mN = ar.mark()
    NDT = BF16 if os.environ.get('GDN_NEU', 'bf16') == 'bf16' else F32
    nid = k.identb if NDT == BF16 else k.ident
    Xb = [[ar.alloc([128], NDT) for _ in range(2)] for _ in range(NI)]
    XTb = [[ar.alloc([128], NDT) for _ in range(2)] for _ in range(NI)]
    Pb = [[ar.alloc([128], NDT) for _ in range(2)] for _ in range(NI)]

    def tview(pn_, c0):
        return pn_[:, c0:c0 + 128] if NDT == F32 else pn_.bitcast(BF16)[:, 2 * c0:2 * c0 + 128]
    tX = [Trk() for _ in range(NI)]
    EGB = [ar.alloc([128], F32) for _ in range(NI)]
    ET = [ar.alloc([128], BF16) for _ in range(NI)]
    ETs = ET
    tE = [Trk() for _ in range(NI)]
    tEG = [Trk() for _ in range(NI)]
    Kg = [ar.alloc([128], BF16) for _ in range(NI)]
    tKg = [Trk() for _ in range(NI)]
    tXP = [Trk() for _ in range(NI)]
    insts = [(i, d) for i in range(NT) for d in range(2)]
    for g0 in range(0, len(insts), NI):
        grp = insts[g0:g0 + NI]
        info = []
        for s_, (i, d) in enumerate(grp):
            info.append(dict(s_=s_, i=i, d=d, tsl=slice(i * 128, (i + 1) * 128),
                             col=pap(G.g[d], 0, 128, i * 4 + hd, [[0, 128]]),
                             gcc=G.gc[d][:, i, hd:hd + 1], nbc=G.nb[d][:, i, hd:hd + 1], edc=G.ed[d][:, i, hd:hd + 1],
                             pb=k.psum[s_], tp=k.tpsum[s_]))
        for q_ in info:
            s_, i, d, tsl, col, pb, tp = q_["s_"], q_["i"], q_["d"], q_["tsl"], q_["col"], q_["pb"], q_["tp"]
            cx.op("tensor", lambda h, pb=pb, col=col, d=d: h.matmul(pb[:, 0:128], col, G.mask[:, d, :], start=True, stop=True), r=[G.T, G.tmask], w=[tp], inc=False)
            cx.op("tensor", lambda h, pb=pb, tsl=tsl: h.matmul(pb[:, 128:256], knT[:, tsl], knT[:, tsl], start=True, stop=True), r=[tk_], w=[tp], inc=False)
            cx.op("tensor", lambda h, pb=pb, tsl=tsl: h.matmul(pb[:, 256:384], knT[:, tsl], qnT[:, tsl], start=True, stop=True), r=[tk_, tq], w=[tp])
        for q_ in info:
            s_, i, d, pb, tp, gcc = q_["s_"], q_["i"], q_["d"], q_["pb"], q_["tp"], q_["gcc"]
            cx.op("scalar", lambda h, pb=pb, s_=s_: h.activation(EGB[s_], pb[:, 0:128], AF.Exp), r=[tp], w=[tEG[s_]])
            cx.op(V, lambda h, pb=pb, s_=s_, gcc=gcc, d=d: h.scalar_tensor_tensor(out=ET[s_], in0=pb[:, 0:128], scalar=gcc, in1=G.mask[:, 2 + d, :], op0=ALU.subtract, op1=ALU.min),
                  r=[tp, G.T, G.tmask], w=[tE[s_]])
        for q_ in info:
            s_, i, d, tsl = q_["s_"], q_["i"], q_["d"], q_["tsl"]
            cx.op("scalar", lambda h, s_=s_: h.activation(ET[s_], ET[s_], AF.Exp), r=[tE[s_]], w=[tE[s_]])
            cx.op(V, lambda h, s_=s_, tsl=tsl, d=d: h.tensor_tensor(out=qgT[d][:, tsl], in0=qnT[:, tsl], in1=EGB[s_], op=ALU.mult), r=[tq, tEG[s_]], w=[tqg[d][i]])
        for q_ in info:
            s_, i, d, pb, tp, edc = q_["s_"], q_["i"], q_["d"], q_["pb"], q_["tp"], q_["edc"]
            c0, c1 = (63, 127) if d == 0 else (0, 64)
            cx.op("scalar", lambda h, s_=s_, d=d, i=i, c0=c0: h.copy(etot[d][:, 2 * i:2 * i + 1], EGB[s_][:, c0:c0 + 1]), r=[tEG[s_]], w=[tet[d][i]])
            cx.op("scalar", lambda h, s_=s_, d=d, i=i, c1=c1: h.copy(etot[d][:, 2 * i + 1:2 * i + 2], EGB[s_][:, c1:c1 + 1]), r=[tEG[s_]], w=[tet[d][i]])
            cx.op("scalar", lambda h, d=d, i=i, edc=edc: h.activation(Kd[d][:, i, :], Ktok[:, i, :], AF.Identity, scale=edc), r=[tKt, G.T], w=[tKd[d][i]])
            egc = G.eg[d][:, i, hd:hd + 1]
            cx.op("scalar", lambda h, s_=s_, i=i, egc=egc: h.activation(Kg[s_], Ktok[:, i, :], AF.Identity, scale=egc), r=[tKt, G.T], w=[tKg[s_]])
            cx.op(V, lambda h, pb=pb, s_=s_, d=d, i=i: h.tensor_tensor(out=QKm[d][:, i, :], in0=pb[:, 256:384], in1=ET[s_], op=ALU.mult), r=[tp, tE[s_]], w=[tQK[d][i]])
        for q_ in info:
            s_, d = q_["s_"], q_["d"]
            cx.op(V, lambda h, s_=s_, d=d: h.tensor_tensor(out=ETs[s_], in0=ET[s_], in1=G.mask[:, 4 + d, :], op=ALU.mult), r=[tE[s_], G.tmask], w=[tE[s_]])
        for q_ in info:
            s_, pb, tp, nbc = q_["s_"], q_["pb"], q_["tp"], q_["nbc"]
            cx.op(V, lambda h, pb=pb, s_=s_, nbc=nbc: h.scalar_tensor_tensor(out=Xb[s_][0], in0=pb[:, 128:256], scalar=nbc, in1=ETs[s_], op0=ALU.mult, op1=ALU.mult),
                  r=[tp, tE[s_], G.T], w=[tX[s_]])
        for q_ in info:
            s_, pn, tn = q_["s_"], q_["pb"], q_["tp"]
            cx.op("tensor", lambda h, pn=pn, s_=s_: h.transpose(tview(pn, 384), Xb[s_][0], nid), r=[tX[s_]], w=[tn])
            cx.op(V, lambda h, s_=s_: h.tensor_tensor(out=Pb[s_][0], in0=Xb[s_][0], in1=nid, op=ALU.add), r=[tX[s_]], w=[tXP[s_]])
            cx.op("scalar", lambda h, pn=pn, s_=s_: h.copy(XTb[s_][0], tview(pn, 384)), r=[tn], w=[tX[s_]])
        for L in range(1, 6):
            a, b_ = (L - 1) % 2, L % 2
            for s_, (i, d) in enumerate(grp):
                pn = k.psum[s_]
                tn = k.tpsum[s_]
                if L < 5:
                    cx.op("tensor", lambda h, pn=pn, s_=s_, a=a: h.matmul(pn[:, 0:128], XTb[s_][a], Xb[s_][a], start=True, stop=True), r=[tX[s_]], w=[tn], inc=False)
                cx.op("tensor", lambda h, pn=pn, s_=s_, a=a: h.matmul(pn[:, 128:256], Xb[s_][a], XTb[s_][a], start=True, stop=True), r=[tX[s_]], w=[tn])
                e1, e2 = ("scalar", V) if s_ % 2 == 0 else (V, "scalar")
                if L < 5:
                    if e1 == "scalar":
                        cx.op("scalar", lambda h, pn=pn, s_=s_, b_=b_: h.copy(Xb[s_][b_], pn[:, 0:128]), r=[tn], w=[tX[s_]])
                    else:
                        cx.op(V, lambda h, pn=pn, s_=s_, b_=b_: h.tensor_copy(Xb[s_][b_], pn[:, 0:128]), r=[tn], w=[tX[s_]])
                if e2 == "scalar":
                    cx.op("scalar", lambda h, pn=pn, s_=s_, b_=b_: h.copy(XTb[s_][b_], pn[:, 128:256]), r=[tn], w=[tX[s_]])
                else:
                    cx.op(V, lambda h, pn=pn, s_=s_, b_=b_: h.tensor_copy(XTb[s_][b_], pn[:, 128:256]), r=[tn], w=[tX[s_]])
            for s_, (i, d) in enumerate(grp):
                pn = k.psum[s_]
                tn = k.tpsum[s_]
                cx.op("tensor", lambda h, pn=pn, s_=s_, a=a: h.matmul(pn[:, 256:384], nid, Pb[s_][a], start=True, stop=False), r=[tXP[s_]], w=[tn], inc=False)
                cx.op("tensor", lambda h, pn=pn, s_=s_, a=a, b_=b_: h.matmul(pn[:, 256:384], XTb[s_][b_], Pb[s_][a], start=False, stop=True), r=[tX[s_], tXP[s_]], w=[tn])
                use_act = (s_ + L) % 2 == 0
                if L < 5:
                    if use_act:
                        cx.op("scalar", lambda h, pn=pn, s_=s_, b_=b_: h.copy(Pb[s_][b_], pn[:, 256:384]), r=[tn], w=[tXP[s_]])
                    else:
                        cx.op(V, lambda h, pn=pn, s_=s_, b_=b_: h.tensor_copy(Pb[s_][b_], pn[:, 256:384]), r=[tn], w=[tXP[s_]])
                else:
                    if use_act:
                        cx.op("scalar", lambda h, pn=pn, d=d, i=i: h.copy(Pm[d][:, i, :], pn[:, 256:384]), r=[tn], w=[tPm[d][i], tXP[s_]])
                    else:
                        cx.op(V, lambda h, pn=pn, d=d, i=i: h.tensor_copy(Pm[d][:, i, :], pn[:, 256:384]), r=[tn], w=[tPm[d][i], tXP[s_]])
        for s_, (i, d) in enumerate(grp):
            pn = k.psum[s_]
            tn = k.tpsum[s_]
            tsl = slice(i * 128, (i + 1) * 128)
            cx.op("tensor", lambda h, pn=pn, s_=s_, d=d, i=i: h.matmul(pn[:, 0:128], Kg[s_], Pm[d][:, i, :], start=True, stop=True), r=[tKg[s_], tPm[d][i]], w=[tn], inc=False)
            cx.op("tensor", lambda h, pn=pn, d=d, i=i: h.matmul(pn[:, 128:256], Pm[d][:, i, :], Vtok[:, i, :], start=True, stop=True), r=[tPm[d][i], tVt], w=[tn])
            btc_ = G.beta[d][:, i, hd:hd + 1]
            cx.op(V, lambda h, pn=pn, d=d, tsl=tsl: h.tensor_scalar(WnT[d][:, tsl], pn[:, 0:128], -1.0, None, op0=ALU.mult), r=[tn], w=[tWn[d][i]])
            cx.op("scalar", lambda h, pn=pn, d=d, i=i, btc_=btc_: h.activation(U0b[d][:, i, :], pn[:, 128:256], AF.Identity, scale=btc_), r=[tn, G.T], w=[tU0[d][i]])
    if STOP == "B":
        cx.barrier()
        ar.release(m0)
        return
    ar.release(mN)
    Sf = [[ar.alloc([128], F32) for _ in range(2)] for _ in range(2)]
    Sb = [ar.alloc([128], BF16) for _ in range(2)]
    Rp = [ar.alloc([128], BF16) for _ in range(2)]
    vn = [ar.alloc([128], BF16) for _ in range(2)]
    tS = [Trk(), Trk()]
    tSf = [Trk(), Trk()]
    tR = [Trk(), Trk()]
    tv = [Trk(), Trk()]
    cx.op("gpsimd", lambda h: h.memset(G.osum, 0.0), w=G.tosum)
    for d in range(2):
        cx.op("gpsimd", lambda h, d=d: h.memset(Sf[d][0], 0.0), w=[tS[d]])
        cx.op("gpsimd", lambda h, d=d: h.memset(Sb[d], 0.0), w=[tS[d]])
        cx.op("gpsimd", lambda h, d=d: h.memset(Rp[d], 0.0), w=[tR[d]])
        cx.op("gpsimd", lambda h, d=d: h.memset(vn[d], 0.0), w=[tv[d]])
    for step in range(32):
        for d in range(2):
            if d == 0:
                i, hh = step // 2, step % 2
            else:
                i, hh = 15 - step // 2, 1 - step % 2
            tsl = slice(i * 128, (i + 1) * 128)
            ps_ = slice(hh * 64, (hh + 1) * 64)
            cur, nxt = step % 2, (step + 1) % 2
            pcs = [k.psum[4 * d + q_] for q_ in range(4)]
            tcs = [k.tpsum[4 * d + q_] for q_ in range(4)]
            negc = G.neg[d][ps_, i, hd:hd + 1]
            btc = G.beta[d][ps_, i, hd:hd + 1]
            p1, pv_, po_, pst = pcs
            t1_, tv_, to_, tst = tcs
            cx.op("tensor", lambda h, p1=p1, tsl=tsl, d=d: h.matmul(p1[:, 0:128], WnT[d][:, tsl], Sb[d], start=True, stop=True), r=[tWn[d][i], tS[d]], w=[t1_])
            cx.op(V, lambda h, p1=p1, ps_=ps_, btc=btc, d=d, i=i: h.scalar_tensor_tensor(out=vn[d][ps_, :], in0=p1[ps_, 0:128], scalar=btc, in1=U0b[d][ps_, i, :], op0=ALU.mult, op1=ALU.add),
                  r=[t1_, tU0[d][i], G.T], w=[tv[d]])
            cx.op("tensor", lambda h, po_=po_, tsl=tsl, d=d: h.matmul(po_[:, 0:128], qgT[d][:, tsl], Sb[d], start=True, stop=False), r=[tqg[d][i], tS[d]], w=[to_], inc=False)
            cx.op("tensor", lambda h, po_=po_, ps_=ps_, d=d, i=i: h.matmul(po_[:, 0:128], QKm[d][ps_, i, :], vn[d][ps_, :], start=False, stop=True), r=[tQK[d][i], tv[d]], w=[to_])
            cx.op("tensor", lambda h, pst=pst, ps_=ps_, d=d, i=i: h.matmul(pst[:, 0:128], Kd[d][ps_, i, :], vn[d][ps_, :], start=True, stop=True), r=[tKd[d][i], tv[d]], w=[tst])
            cx.op("gpsimd" if False else V, lambda h, po_=po_, ps_=ps_, i=i: h.tensor_tensor(out=G.osum[ps_, i, :], in0=po_[ps_, 0:128], in1=G.osum[ps_, i, :], op=ALU.add), r=[to_, G.tosum[i]], w=[G.tosum[i]])
            etc = etot[d][:, 2 * i + hh:2 * i + hh + 1]
            cx.op(V, lambda h, pst=pst, d=d, cur=cur, nxt=nxt, etc=etc: h.scalar_tensor_tensor(out=Sf[d][nxt], in0=Sf[d][cur], scalar=etc, in1=pst[:, 0:128], op0=ALU.mult, op1=ALU.add),
                  r=[tst, tet[d][i], tS[d]], w=[tS[d]])
            cx.op("scalar", lambda h, d=d, nxt=nxt: h.copy(Sb[d], Sf[d][nxt]), r=[tS[d]], w=[tS[d]])
    if hd == 0:
        k.dbg_add("gdn_osum", G.osum, G.tosum)
    if STOP == "C":
        cx.barrier()
        ar.release(m0)
        return
    ss = ar.alloc([NT, 2], F32)
    tss = Trk()
    junk = ar.alloc([128], BF16)
    zs = [ar.alloc([128], F32) for _ in range(2)]
    tzs = [Trk(), Trk()]
    yb = [ar.alloc([128], BF16) for _ in range(2)]
    tyb = [Trk(), Trk()]
    for i in range(NT):
        cx.op("scalar", lambda h, i=i: h.activation(junk, G.osum[:, i, :], AF.Square, accum_out=ss[:, i, 0:1]), r=[G.tosum[i], tss], w=[tss])
    cx.op(V, lambda h: h.tensor_scalar(ss[:, :, 1:2], ss[:, :, 0:1], 1.0 / 128, EPS, op0=ALU.mult, op1=ALU.add), r=[tss], w=[tss])
    cx.op("scalar", lambda h: h.activation(ss[:, :, 1:2], ss[:, :, 1:2], AF.Sqrt), r=[tss], w=[tss])
    cx.op(V, lambda h: h.reciprocal(ss[:, :, 1:2], ss[:, :, 1:2]), r=[tss], w=[tss])
    for i in range(NT):
        b = i % 2
        pz = k.psum[b]
        tz = k.tpsum[b]
        for c in range(8):
            cx.op("tensor", lambda h, pz=pz, c=c, i=i: h.matmul(pz[:, 0:128], k.hT[:, c, i * 128:(i + 1) * 128], wz[:, c, :], start=(c == 0), stop=(c == 7)),
                  r=[twz, k.t_hT], w=[tz], inc=(c == 7))
        cx.op("scalar", lambda h, pz=pz, b=b: h.activation(zs[b], pz[:, 0:128], AF.Silu), r=[tz], w=[tzs[b]])
        s1 = ss[:, i, 1:2]
        cx.op(V, lambda h, i=i, s1=s1: h.scalar_tensor_tensor(out=G.osum[:, i, :], in0=G.osum[:, i, :], scalar=s1, in1=G.nw, op0=ALU.mult, op1=ALU.mult), r=[tss, G.tpr, G.tosum[i]], w=[G.tosum[i]])
        cx.op(V, lambda h, i=i, b=b: h.tensor_tensor(out=yb[b], in0=G.osum[:, i, :], in1=zs[b], op=ALU.mult), r=[G.tosum[i], tzs[b]], w=[tyb[b]])
        pt = k.psum[2 + b].bitcast(BF16)
        tt_ = k.tpsum[2 + b]
        cx.op("tensor", lambda h, pt=pt, b=b: h.transpose(pt[:, 0:128], yb[b], k.identb), r=[tyb[b]], w=[tt_])
        cx.op("scalar", lambda h, pt=pt, i=i: h.copy(yT[:, 4 + hd, i * 128:(i + 1) * 128], pt[:, 0:128]), r=[tt_], w=[t_yT])
    cx.barrier()
    ar.release(m0)


def out_proj(k, yT, t_yT):
    cx, ar, D = k.cx, k.ar, k.D
    m0 = ar.mark()
    wo = ar.alloc([8, 1024], BF16)
    two = Trk()
    wsrc = D["w_out"]
    sl_ = cx.fresh('sw')
    for c in range(8):
        cx.dma("gpsimd", wo[:, c, :], wsrc[c * 128:(c + 1) * 128, :], sl_, w=[two])
    for g4 in range(4):
        slx = cx.fresh()
        for i in range(g4 * 4, g4 * 4 + 4):
            cx.dma("sync", k.xacc[:, i, :], D["x"][i * 128:(i + 1) * 128, :], slx, w=[k.txacc[i]])
        for i in range(g4 * 4, g4 * 4 + 4):
            k.txacc[i].w = (slx.key, slx.total)
    for i in range(NT):
        for half in range(2):
            pb = k.psum[(2 * i + half) % 4]
            tp = k.tpsum[(2 * i + half) % 4]
            for c in range(8):
                cx.op("tensor", lambda h, pb=pb, c=c, i=i, half=half: h.matmul(pb[:, :], yT[:, c, i * 128:(i + 1) * 128], wo[:, c, half * 512:(half + 1) * 512], start=(c == 0), stop=(c == 7)),
                      r=[t_yT, two], w=[tp], inc=(c == 7))
            xs = k.xacc[:, i, half * 512:(half + 1) * 512]
            cx.op("vector", lambda h, pb=pb, xs=xs: h.tensor_tensor(out=xs, in0=pb[:, :], in1=xs, op=ALU.add), r=[tp, k.txacc[i]], w=[k.txacc[i]])
    cx.barrier()
    ar.release(m0)


def xattn(k):
    cx, ar, D = k.cx, k.ar, k.D
    V = "vector"
    m0 = ar.mark()
    xnT = ar.alloc([8, 2048], BF16)
    t_xnT = Trk()
    memT = ar.alloc([8, 256], BF16)
    t_memT = Trk()
    m1 = ar.mark()
    mt = [ar.alloc([1024], F32) for _ in range(2)]
    tmt = [Trk(), Trk()]

    def src_mem(i):
        cx.dma("sync", mt[i], D["mem"][i * 128:(i + 1) * 128, :], cx.fresh(), w=[tmt[i]])
        return mt[i], tmt[i]
    norm_transpose(k, "mem", src_mem, 2, D["norm_mem"], memT, BF16, t_memT)
    ar.release(m1)
    norm_transpose(k, "xa", lambda i: (k.xacc[:, i, :], k.txacc[i]), NT, D["norm_xattn"], xnT, BF16, t_xnT, resident=True)
    k.dbg_add("xa_memT", memT, [t_memT])
    k.dbg_add("xa_xnT", xnT, [t_xnT])
    wq = [ar.alloc([8, 256], BF16) for _ in range(2)]
    wk = [ar.alloc([8, 256], BF16) for _ in range(2)]
    wv = [ar.alloc([8, 256], BF16) for _ in range(2)]
    wo = [ar.alloc([2, 1024], BF16) for _ in range(2)]
    twA = [Trk(), Trk()]
    two_ = [Trk(), Trk()]
    swA = [cx.slot("xwa0"), cx.slot("xwa1")]
    swO = [cx.slot("xwo0"), cx.slot("xwo1")]
    kTb = [ar.alloc([2, 256], BF16) for _ in range(2)]
    vhb = [ar.alloc([2, 256], BF16) for _ in range(2)]
    tkvb = [Trk(), Trk()]
    qTb = [ar.alloc([2, 2048], BF16) for _ in range(2)]
    tqTb = [Trk(), Trk()]
    E = [ar.alloc([2, 512], BF16) for _ in range(2)]
    tE = [Trk(), Trk()]
    rden = [ar.alloc([512], F32) for _ in range(2)]
    trd = [Trk(), Trk()]
    oTn = [ar.alloc([2, 512], BF16) for _ in range(2)]
    toT = [Trk(), Trk()]
    cx.barrier()
    k.txa = [[Trk(), Trk()] for _ in range(NT)]

    def loadA(hd):
        b = hd % 2
        c0 = hd * 256
        for (dst, nm) in ((wq[b], "xa_wq"), (wk[b], "xa_wk"), (wv[b], "xa_wv")):
            load_w_cols(k, D[nm], c0, 256, dst, twA[b], swA[b])

    def loadO(hd):
        b = hd % 2
        c0 = hd * 256
        src = D["xa_wo"]
        cx.dma("gpsimd", wo[b], bass.AP(src.tensor, src.offset + c0 * 1024, [[1024, 128], [128 * 1024, 2], [1, 1024]]), swO[b], w=[two_[b]])

    def proj(hd):
        b = hd % 2
        kT, vh, qT, tkv, tqT = kTb[b], vhb[b], qTb[b], tkvb[b], tqTb[b]
        for dc in range(2):
            pb = k.psum[dc]
            tp = k.tpsum[dc]
            for c in range(8):
                cx.op("tensor", lambda h, pb=pb, c=c, dc=dc, b=b: h.matmul(pb[:, 0:256], wk[b][:, c, dc * 128:(dc + 1) * 128], memT[:, c, :], start=(c == 0), stop=(c == 7)),
                      r=[twA[b], t_memT], w=[tp], inc=(c == 7))
            cx.op("scalar", lambda h, pb=pb, dc=dc, kT=kT: h.copy(kT[:, dc, :], pb[:, 0:256]), r=[tp], w=[tkv])
        for mtile in range(2):
            pb = k.psum[2 + mtile]
            tp = k.tpsum[2 + mtile]
            for c in range(8):
                cx.op("tensor", lambda h, pb=pb, c=c, mtile=mtile, b=b: h.matmul(pb[:, 0:256], memT[:, c, mtile * 128:(mtile + 1) * 128], wv[b][:, c, :], start=(c == 0), stop=(c == 7)),
                      r=[twA[b], t_memT], w=[tp], inc=(c == 7))
            cx.op(V, lambda h, pb=pb, mtile=mtile, vh=vh: h.tensor_copy(vh[:, mtile, :], pb[:, 0:256]), r=[tp], w=[tkv])
        for dc in range(2):
            for n in range(4):
                pb = k.psum[(dc * 4 + n) % 4]
                tp = k.tpsum[(dc * 4 + n) % 4]
                for c in range(8):
                    cx.op("tensor", lambda h, pb=pb, c=c, dc=dc, n=n, b=b: h.matmul(pb[:, :], wq[b][:, c, dc * 128:(dc + 1) * 128], xnT[:, c, n * 512:(n + 1) * 512], start=(c == 0), stop=(c == 7)),
                          r=[twA[b], t_xnT], w=[tp], inc=(c == 7))
                if n % 2 == 0:
                    cx.op("scalar", lambda h, pb=pb, dc=dc, n=n, qT=qT: h.copy(qT[:, dc, n * 512:(n + 1) * 512], pb[:, :]), r=[tp], w=[tqT])
                else:
                    cx.op(V, lambda h, pb=pb, dc=dc, n=n, qT=qT: h.tensor_copy(qT[:, dc, n * 512:(n + 1) * 512], pb[:, :]), r=[tp], w=[tqT])

    def chunks(hd):
        b = hd % 2
        kT, vh, qT, tkv, tqT = kTb[b], vhb[b], qTb[b], tkvb[b], tqTb[b]
        tw = [two_[0], two_[1]]

        def emit_scores(n):
            eb = n % 2
            ts = slice(n * 512, (n + 1) * 512)
            for mtile in range(2):
                pb = k.psum[mtile]
                tp = k.tpsum[mtile]
                for dc in range(2):
                    cx.op("tensor", lambda h, pb=pb, dc=dc, mtile=mtile, ts=ts: h.matmul(pb[:, :], kT[:, dc, mtile * 128:(mtile + 1) * 128], qT[:, dc, ts], start=(dc == 0), stop=(dc == 1)),
                          r=[tkv, tqT], w=[tp], inc=(dc == 1))
                cx.op("scalar", lambda h, pb=pb, mtile=mtile, eb=eb: h.activation(E[eb][:, mtile, :], pb[:, :], AF.Exp, scale=1.0 / 16.0), r=[tp], w=[tE[eb]])

        def emit_rest(n):
            eb = n % 2
            pd = k.psum[2]
            tpd = k.tpsum[2]
            for mtile in range(2):
                cx.op("tensor", lambda h, pd=pd, mtile=mtile, eb=eb: h.matmul(pd[:, :], k.onesb, E[eb][:, mtile, :], start=(mtile == 0), stop=(mtile == 1)), r=[tE[eb]], w=[tpd], inc=(mtile == 1))
            cx.op(V, lambda h, pd=pd, eb=eb: h.reciprocal(rden[eb], pd[:, :]), r=[tpd], w=[trd[eb]])
            for dc in range(2):
                po = k.psum[3 + dc]
                tpo = k.tpsum[3 + dc]
                for mtile in range(2):
                    cx.op("tensor", lambda h, po=po, mtile=mtile, dc=dc, eb=eb: h.matmul(po[:, :], vh[:, mtile, dc * 128:(dc + 1) * 128], E[eb][:, mtile, :], start=(mtile == 0), stop=(mtile == 1)),
                          r=[tkv, tE[eb]], w=[tpo], inc=(mtile == 1))
                cx.op(V, lambda h, po=po, dc=dc, eb=eb: h.tensor_tensor(out=oTn[eb][:, dc, :], in0=po[:, :], in1=rden[eb], op=ALU.mult), r=[tpo, trd[eb]], w=[toT[eb]])
            for t in range(4):
                i = n * 4 + t
                for half in range(2):
                    pw_ = k.psum[5 + (t * 2 + half) % 3]
                    tpw = k.tpsum[5 + (t * 2 + half) % 3]
                    for dc in range(2):
                        cx.op("tensor", lambda h, pw_=pw_, dc=dc, t=t, half=half, b=b, eb=eb: h.matmul(pw_[:, :], oTn[eb][:, dc, t * 128:(t + 1) * 128], wo[b][:, dc, half * 512:(half + 1) * 512], start=(dc == 0), stop=(dc == 1)),
                              r=[toT[eb], tw[b]], w=[tpw], inc=(dc == 1))
                    xs = k.xacc[:, i, half * 512:(half + 1) * 512]
                    cx.op(V, lambda h, pw_=pw_, xs=xs: h.tensor_tensor(out=xs, in0=pw_[:, :], in1=xs, op=ALU.add), r=[tpw, k.txa[i][half]], w=[k.txa[i][half]])
        emit_scores(0)
        for n in range(4):
            if n + 1 < 4:
                emit_scores(n + 1)
            emit_rest(n)
    loadA(0)
    loadA(1)
    loadO(0)
    loadO(1)
    proj(0)
    for hd in range(4):
        if hd + 1 < 4:
            proj(hd + 1)
        if hd + 2 < 4:
            loadA(hd + 2)
        chunks(hd)
        if hd + 2 < 4:
            loadO(hd + 2)
    cx.barrier()
    ar.release(m0)


def moe(k):
    cx, ar, D = k.cx, k.ar, k.D
    V = "vector"
    m0 = ar.mark()
    xnT = ar.alloc([8, 2048], BF16)
    t_xnT = Trk()
    norm_transpose(k, "moe", lambda i: (k.xacc[:, i, :], k.txacc[i]), NT, D["norm_moe"], xnT, BF16, t_xnT, resident=True)
    wr = ar.alloc([8, 36], BF16)
    twr = Trk()
    sl_ = cx.fresh('sw')
    srcg, srce = D["router_group_w"], D["router_expert_w"]
    cx.dma("gpsimd", wr[:, :, 0:4], bass.AP(srcg.tensor, srcg.offset, [[4, 128], [4 * 128, 8], [1, 4]]), sl_, w=[twr])
    cx.dma("gpsimd", wr[:, :, 4:36], bass.AP(srce.tensor, srce.offset, [[32, 128], [32 * 128, 8], [1, 32]]), sl_, w=[twr])
    rb = ar.alloc([36], F32)
    trb = Trk()
    sl2 = cx.fresh()
    cx.dma("sync", rb[:, 0:4], dram_bcast(D["router_group_b"], 128, 4), sl2, w=[trb])
    cx.dma("sync", rb[:, 4:36], dram_bcast(D["router_expert_b"], 128, 32), sl2, w=[trb])
    cw = ar.alloc([NT, 32], F32)
    tcw = Trk()
    lgA = ar.alloc([NT, 36], F32)
    msk = ar.alloc([NT, 32], F32)
    eq2 = ar.alloc([NT, 32], F32)
    m8 = ar.alloc([NT, 8], F32)
    sc = ar.alloc([8, NT], F32)
    oh = ar.alloc([NT, 4], F32)
    ex = ar.alloc([NT, 4], F32)
    T = Trk()
    rbb = rb.unsqueeze(1).to_broadcast([128, 8, 36])
    for half in range(2):
        pb = k.psum[half]
        tp = k.tpsum[half]
        for ii in range(8):
            i = half * 8 + ii
            for c in range(8):
                cx.op("tensor", lambda h, pb=pb, c=c, i=i, ii=ii: h.matmul(pb[:, ii * 36:(ii + 1) * 36], xnT[:, c, i * 128:(i + 1) * 128], wr[:, c, :], start=(c == 0), stop=(c == 7)),
                      r=[t_xnT, twr], w=[tp], inc=(c == 7))
        cx.op(V, lambda h, pb=pb, half=half: h.tensor_tensor(out=lgA[:, half * 8:(half + 1) * 8, :], in0=pb[:, 0:288].rearrange("p (t e) -> p t e", t=8), in1=rbb, op=ALU.add), r=[tp, trb, T], w=[T])
    lg_g = lgA[:, :, 0:4]
    lg_e = lgA[:, :, 4:36]
    gmax, ngs, ssum, ptop, dm, w1, w2 = [sc[:, j_, :] for j_ in range(7)]

    def vop(fn):
        cx.op(V, fn, r=[T], w=[T])

    def aop(fn):
        cx.op("scalar", fn, r=[T], w=[T])
    b4 = lambda v: v.unsqueeze(2).to_broadcast([128, NT, 4])
    b32 = lambda v: v.unsqueeze(2).to_broadcast([128, NT, 32])
    vop(lambda h: h.tensor_reduce(out=gmax, in_=lg_g, axis=AX.X, op=ALU.max))
    vop(lambda h: h.tensor_tensor(out=oh, in0=lg_g, in1=b4(gmax), op=ALU.is_equal))
    vop(lambda h: h.tensor_tensor(out=ex, in0=lg_g, in1=b4(gmax), op=ALU.subtract))
    aop(lambda h: h.activation(ex, ex, AF.Exp))
    vop(lambda h: h.tensor_reduce(out=ssum, in_=ex, axis=AX.X, op=ALU.add))
    vop(lambda h: h.reciprocal(ptop, ssum))
    vop(lambda h: h.tensor_scalar(oh, oh, -1.0, 1e30, op0=ALU.add, op1=ALU.mult))
    vop(lambda h: h.tensor_tensor(out=msk.rearrange("p t (g e) -> p t g e", g=4), in0=lg_e.rearrange("p t (g e) -> p t g e", g=4),
                                  in1=oh.unsqueeze(3).to_broadcast([128, NT, 4, 8]), op=ALU.add))
    for i in range(NT):
        vop(lambda h, i=i: h.max(out=m8[:, i, :], in_=msk[:, i, :]))
    m1, m2 = m8[:, :, 0], m8[:, :, 1]
    vop(lambda h: h.tensor_tensor(out=dm, in0=m2, in1=m1, op=ALU.subtract))
    aop(lambda h: h.activation(dm, dm, AF.Exp))
    vop(lambda h: h.tensor_scalar(w1, dm, 1.0, None, op0=ALU.add))
    vop(lambda h: h.reciprocal(w1, w1))
    vop(lambda h: h.tensor_tensor(out=w2, in0=dm, in1=w1, op=ALU.mult))
    vop(lambda h: h.tensor_tensor(out=w1, in0=w1, in1=ptop, op=ALU.mult))
    vop(lambda h: h.tensor_tensor(out=w2, in0=w2, in1=ptop, op=ALU.mult))
    vop(lambda h: h.tensor_tensor(out=eq2, in0=msk, in1=b32(m2), op=ALU.is_equal))
    vop(lambda h: h.tensor_tensor(out=eq2, in0=eq2, in1=b32(w2), op=ALU.mult))
    vop(lambda h: h.tensor_tensor(out=msk, in0=msk, in1=b32(m1), op=ALU.is_equal))
    vop(lambda h: h.tensor_tensor(out=msk, in0=msk, in1=b32(w1), op=ALU.mult))
    cx.op(V, lambda h: h.tensor_tensor(out=cw, in0=msk, in1=eq2, op=ALU.add), r=[T], w=[tcw, T])
    k.dbg_add("moe_cw", cw, [tcw])
    wgu = [ar.alloc([8, 512], BF16) for _ in range(2)]
    wd = [ar.alloc([2, 1024], BF16) for _ in range(2)]
    twe = [Trk(), Trk()]
    swe = [cx.slot("we0"), cx.slot("we1")]
    sg = [ar.alloc([512], F32) for _ in range(2)]
    tsg = [Trk(), Trk()]
    h1 = [ar.alloc([2, 512], BF16) for _ in range(2)]
    th1 = [Trk(), Trk()]
    NE = k.n_experts

    def load_e(e):
        b = e % 2
        g_, u_, d_ = D["moe_w_gate"], D["moe_w_up"], D["moe_w_down"]
        cx.dma("gpsimd", wgu[b][:, :, 0:256], bass.AP(g_.tensor, g_.offset + e * 1024 * 256, [[256, 128], [256 * 128, 8], [1, 256]]), swe[b], w=[twe[b]])
        cx.dma("gpsimd", wgu[b][:, :, 256:512], bass.AP(u_.tensor, u_.offset + e * 1024 * 256, [[256, 128], [256 * 128, 8], [1, 256]]), swe[b], w=[twe[b]])
        cx.dma("gpsimd", wd[b], bass.AP(d_.tensor, d_.offset + e * 256 * 1024, [[1024, 128], [1024 * 128, 2], [1, 1024]]), swe[b], w=[twe[b]])
    import os
    NOLOAD = os.environ.get("MOE_NOLOAD", "") == "1"
    load_e(0)
    if NE > 1:
        load_e(1)
    jobs = [(e, n) for e in range(NE) for n in range(4)]
    state = {"cnt": 0, "loaded": 0}
    cx.barrier()
    k.txh = [[Trk(), Trk()] for _ in range(NT)]

    def emit_gu(j, fh):
        e, n = jobs[j]
        b = e % 2
        ts = slice(n * 512, (n + 1) * 512)
        hb = j % 2
        pg = k.psum[fh * 2]
        tpg = k.tpsum[fh * 2]
        pu = k.psum[fh * 2 + 1]
        tpu = k.tpsum[fh * 2 + 1]
        for c in range(8):
            cx.op("tensor", lambda h, pg=pg, c=c, fh=fh, ts=ts, b=b: h.matmul(pg[:, :], wgu[b][:, c, fh * 128:(fh + 1) * 128], xnT[:, c, ts], start=(c == 0), stop=(c == 7)),
                  r=[twe[b], t_xnT], w=[tpg], inc=(c == 7))
        for c in range(8):
            cx.op("tensor", lambda h, pu=pu, c=c, fh=fh, ts=ts, b=b: h.matmul(pu[:, :], wgu[b][:, c, 256 + fh * 128:256 + (fh + 1) * 128], xnT[:, c, ts], start=(c == 0), stop=(c == 7)),
                  r=[twe[b], t_xnT], w=[tpu], inc=(c == 7))
        cx.op("scalar", lambda h, pg=pg, fh=fh: h.activation(sg[fh], pg[:, :], AF.Silu), r=[tpg], w=[tsg[fh]])
        cx.op(V, lambda h, pu=pu, fh=fh, hb=hb: h.tensor_tensor(out=h1[hb][:, fh, :], in0=pu[:, :], in1=sg[fh], op=ALU.mult), r=[tpu, tsg[fh]], w=[th1[hb]])

    def emit_down(j):
        e, n = jobs[j]
        b = e % 2
        hb = j % 2
        for t in range(4):
            i = n * 4 + t
            for half in range(2):
                pdn = k.psum[4 + state["cnt"] % 4]
                tpd = k.tpsum[4 + state["cnt"] % 4]
                state["cnt"] += 1
                for fh in range(2):
                    cx.op("tensor", lambda h, pdn=pdn, fh=fh, t=t, half=half, hb=hb, b=b: h.matmul(pdn[:, :], h1[hb][:, fh, t * 128:(t + 1) * 128], wd[b][:, fh, half * 512:(half + 1) * 512], start=(fh == 0), stop=(fh == 1)),
                          r=[th1[hb], twe[b]], w=[tpd], inc=(fh == 1))
                xs = k.xacc[:, i, half * 512:(half + 1) * 512]
                cwc = cw[:, i, e:e + 1]
                cx.op(V, lambda h, pdn=pdn, xs=xs, cwc=cwc: h.scalar_tensor_tensor(out=xs, in0=pdn[:, :], scalar=cwc, in1=xs, op0=ALU.mult, op1=ALU.add), r=[tpd, tcw, k.txh[i][half]], w=[k.txh[i][half]])
        if n == 3 and e + 2 < NE and not NOLOAD:
            load_e(e + 2)
    nj = len(jobs)
    if nj > 0:
        emit_gu(0, 0)
        emit_gu(0, 1)
        for j in range(nj):
            if j + 1 < nj:
                emit_gu(j + 1, 0)
            emit_down(j)
            if j + 1 < nj:
                emit_gu(j + 1, 1)
    cx.barrier()
    ar.release(m0)


def final_norm(k, out):
    cx, ar, D = k.cx, k.ar, k.D
    V = "vector"
    m0 = ar.mark()
    gB = ar.alloc([1024], F32)
    tg = Trk()
    cx.dma("sync", gB, dram_bcast(D["norm_final"], 128, 1024), cx.fresh(), w=[tg])
    junk = ar.alloc([1024], BF16)
    tj = Trk()
    ss = ar.alloc([NT, 2], F32)
    tss = Trk()
    ob = [ar.alloc([1024], F32) for _ in range(2)]
    tob = [Trk(), Trk()]
    so = [cx.slot("o0"), cx.slot("o1")]
    for i in range(NT):
        txs = [k.txacc[i]] + (k.txh[i] if hasattr(k, "txh") else [])
        cx.op("scalar", lambda h, i=i: h.activation(junk, k.xacc[:, i, :], AF.Square, accum_out=ss[:, i, 0:1]), r=txs + [tss], w=[tj, tss])
    cx.op(V, lambda h: h.tensor_scalar(ss[:, :, 1:2], ss[:, :, 0:1], 1.0 / 1024, EPS, op0=ALU.mult, op1=ALU.add), r=[tss], w=[tss])
    cx.op("scalar", lambda h: h.activation(ss[:, :, 1:2], ss[:, :, 1:2], AF.Sqrt), r=[tss], w=[tss])
    cx.op(V, lambda h: h.reciprocal(ss[:, :, 1:2], ss[:, :, 1:2]), r=[tss], w=[tss])
    for i in range(NT):
        b = i % 2
        s1 = ss[:, i, 1:2]
        txs = [k.txacc[i]] + (k.txh[i] if hasattr(k, "txh") else [])
        cx.op(V, lambda h, i=i, s1=s1, b=b: h.scalar_tensor_tensor(out=ob[b], in0=k.xacc[:, i, :], scalar=s1, in1=gB, op0=ALU.mult, op1=ALU.mult), r=txs + [tss, tg], w=[tob[b]])
        cx.dma("sync", out[i * 128:(i + 1) * 128, :], ob[b], so[b], r=[tob[b]])
    cx.barrier()
    ar.release(m0)


_CACHE = {}


def kernel(**inputs):
    inp = {k_: np.asarray(v) for k_, v in inputs.items()}
    n = inp["x"].shape[0]
    maps = [host_inputs(inp, b) for b in range(n)]
    key = "full"
    if key not in _CACHE:
        shapes = {k_: (v.shape, np2dt(v)) for k_, v in maps[0].items()}
        _CACHE[key] = build(shapes)[0]
    nc = _CACHE[key]
    res = run_bass_kernel_spmd(nc, maps, core_ids=list(range(n)))
    return np.stack([np.asarray(r["out"], dtype=np.float32) for r in res.results], 0)
```

```python
import contextlib
import os
import math
import numpy as np
import ml_dtypes
import concourse.bass as bass
import concourse.mybir as mybir
from concourse.bass_utils import run_bass_kernel_spmd

F32 = mybir.dt.float32
BF16 = mybir.dt.bfloat16
F32R = mybir.dt.float32r
I32 = mybir.dt.int32
AF = mybir.ActivationFunctionType
ALU = mybir.AluOpType
AX = mybir.AxisListType

ENGS = ("sync", "scalar", "gpsimd", "vector", "tensor")
ATTACH_WAIT = os.environ.get("ATTACH_WAIT", "1") == "1"
S = 2048
DM = 1024
NT = 16
EPS = 1e-6


class Trk:
    __slots__ = ("name", "w", "r", "excl")

    def __init__(self, name="", excl=False):
        self.name = name
        self.w = None
        self.r = {}
        self.excl = excl


class DmaSlot:
    def __init__(self, ctx, name):
        self.key = "d_" + name + str(ctx.nsem)
        ctx.sems[self.key] = ctx.new_sem(self.key)
        self.total = 0


class Ctx:
    def __init__(self, nc, stack):
        self.nc = nc
        self.stack = stack
        self.q = {e: [] for e in ENGS}
        self.sems = {}
        self.nsem = 0
        self.cnt = {e: 0 for e in ENGS}
        self.known = {e: {} for e in ENGS}
        for e in ENGS:
            self.sems[e] = self.new_sem("s_" + e)
        self.slots = []
        self.pools = {}
        self.pool_idx = {}
        self.n_ops = 0

    def new_sem(self, name):
        self.nsem += 1
        return self.stack.enter_context(self.nc.semaphore(name))

    def slot(self, name):
        s = DmaSlot(self, name)
        self.slots.append(s)
        return s

    def fresh(self, kind="hw"):
        pool = self.pools.setdefault(kind, [])
        i = self.pool_idx.get(kind, 0)
        if i >= len(pool):
            assert len(pool) < 30, "slot pool exhausted"
            pool.append(self.slot(kind + "%d" % len(pool)))
            pool[-1].kind = kind
        self.pool_idx[kind] = i + 1
        return pool[i]

    def sb(self, name, shape, dt):
        return self.stack.enter_context(self.nc.sbuf_tensor("sb_" + name, list(shape), dt))

    def ps(self, name, shape, dt=F32):
        return self.stack.enter_context(self.nc.psum_tensor(name, list(shape), dt))

    def _waits_for(self, eng, r, w, extra=()):
        need = {}

        def req(dep, raw=True):
            if dep is None:
                return
            k, c = dep
            if k == eng and eng in ("tensor", "sync"):
                return
            if k == eng and not raw:
                return
            if c > need.get(k, 0):
                need[k] = c
        for t in r:
            req(t.w)
        for t in w:
            req(t.w, raw=False)
            for k, c in t.r.items():
                req((k, c), raw=False)
        for d in extra:
            req(d)
        out = []
        kn = self.known[eng]
        for k, c in need.items():
            if kn.get(k, 0) < c:
                kn[k] = c
                out.append((self.sems[k], c))
        return out

    def op(self, eng, fn, r=(), w=(), inc=True, extra=()):
        w = list(w) + [t for t in r if t.excl]
        r = [t for t in r if not t.excl]
        waits = self._waits_for(eng, r, w, extra)
        c = self.cnt[eng] + 1
        if inc:
            self.cnt[eng] = c
        sem = self.sems[eng]

        def emit(h, fn=fn, waits=waits, inc=inc, sem=sem):
            for s, v in waits[:-1]:
                h.wait_ge(s, v)
            ins = fn(h)
            if waits:
                if ATTACH_WAIT:
                    ins._wait_ge(waits[-1][0], waits[-1][1])
                else:
                    raise RuntimeError
            if inc:
                ins.then_inc(sem, 1)
        if not ATTACH_WAIT:
            def emit(h, fn=fn, waits=waits, inc=inc, sem=sem):
                for s, v in waits:
                    h.wait_ge(s, v)
                ins = fn(h)
                if inc:
                    ins.then_inc(sem, 1)
        self.q[eng].append(emit)
        for t in r:
            t.r[eng] = c
        for t in w:
            t.w = (eng, c)
            t.r = {}
        self.n_ops += 1

    def dma(self, eng, out, in_, slot, r=(), w=(), extra=(), **kw):
        kind = "sw" if eng == "gpsimd" else "hw"
        assert getattr(slot, "kind", kind) == kind, ("DMA slot kind mismatch", slot.key, eng)
        slot.kind = kind
        waits = self._waits_for(eng, r, w, extra)
        slot.total += 16
        sem = self.sems[slot.key]

        def emit(h, waits=waits, sem=sem, out=out, in_=in_, kw=kw):
            for s, v in waits:
                h.wait_ge(s, v)
            h.dma_start(out=out, in_=in_, **kw).then_inc(sem, 16)
        self.q[eng].append(emit)
        dep = (slot.key, slot.total)
        for t in r:
            t.r[slot.key] = slot.total
        for t in w:
            t.w = dep
            t.r = {}
        self.n_ops += 1
        return dep

    def wait_deps(self, eng, deps):
        waits = self._waits_for(eng, (), (), deps)

        def emit(h, waits=waits):
            for s, v in waits:
                h.wait_ge(s, v)
        self.q[eng].append(emit)

    def barrier(self):
        deps = [(e, self.cnt[e]) for e in ENGS if e != "sync" and self.cnt[e] > 0]
        deps += [(s.key, s.total) for s in self.slots if s.total > 0]
        for e in ENGS:
            self.wait_deps(e, deps)
        self.pool_idx = {}

    def emit_all(self, block):
        q = self.q

        @block.sync
        def _(h):
            for f in q["sync"]:
                f(h)

        @block.scalar
        def _(h):
            for f in q["scalar"]:
                f(h)

        @block.gpsimd
        def _(h):
            for f in q["gpsimd"]:
                f(h)

        @block.vector
        def _(h):
            for f in q["vector"]:
                f(h)

        @block.tensor
        def _(h):
            for f in q["tensor"]:
                f(h)


class Arena:
    def __init__(self, cx, words, base=None):
        self.t = cx.sb("arena", [128, words], F32) if base is None else base
        self.cx = cx
        self.words = words
        self.top = 0

    def mark(self):
        return self.top

    def release(self, m):
        if m != self.top:
            self.cx.barrier()
        self.top = m

    def alloc(self, shape, dt):
        n = int(np.prod(shape))
        w = n if dt in (F32, F32R, I32) else (n + 1) // 2
        w = (w + 1) // 2 * 2
        o = self.top
        self.top += w
        assert self.top <= self.words, ("arena overflow", self.top, self.words)
        v = self.t[:, o:o + w]
        if dt != F32:
            v = v.bitcast(dt)
        v = v[:, 0:n]
        if len(shape) > 1:
            names = " ".join("d%d" % i for i in range(len(shape)))
            v = v.rearrange("p (%s) -> p %s" % (names, names), **{"d%d" % i: shape[i] for i in range(len(shape))})
        return v


def pap(ap, part0, nparts, off, dims):
    base = ap.ap[0][0]
    return bass.AP(ap.tensor, ap.offset + part0 * base + off, [[base, nparts]] + [list(d) for d in dims])


def host_consts():
    c = {}
    c["ident"] = np.eye(128, dtype=np.float32)
    c["identb"] = np.eye(128, dtype=np.float32).astype(ml_dtypes.bfloat16)
    c["ones"] = np.ones((128, 128), np.float32)
    selT = np.zeros((128, 2, 8, 128), np.float32)
    selB = np.zeros((128, 2, 8, 128), np.float32)
    for q in range(4):
        for r in range(32):
            loc, cc = r // 16, r % 16
            for s in range(8):
                selT[q * 32 + r, loc, s, s * 16 + cc] = 1.0
                selB[q * 32 + r, loc, s, s * 16 + cc] = 1.0
    c["selT"] = selT.astype(ml_dtypes.bfloat16)
    c["selB"] = selB.astype(ml_dtypes.bfloat16)
    sidx = np.arange(128) // 16
    c["s5mf"] = (sidx[None, :] >= sidx[:, None]).astype(np.float32)
    c["s5mb"] = (sidx[None, :] <= sidx[:, None]).astype(np.float32)
    c["kvec"] = np.tile((np.arange(16, dtype=np.float32) - 7.0)[None, :], (128, 1))
    k = np.arange(128)[:, None]
    cc = np.arange(128)[None, :]
    same = (k // 64) == (cc // 64)
    gm = np.zeros((128, 8, 128), np.float32)
    gm[:, 0] = same & (k <= cc)
    gm[:, 1] = same & (k >= cc)
    gm[:, 2] = np.where(same & (cc >= k), 0.0, -30000.0)
    gm[:, 3] = np.where(same & (cc <= k), 0.0, -30000.0)
    gm[:, 4] = same & (cc > k)
    gm[:, 5] = same & (cc < k)
    gm[:, 6] = same
    c["gmask"] = gm
    return c


def host_s5(inp):
    o = {}

    pairs = {"lam_re": ("s5_lam_re_f", "s5_lam_re_b"), "lam_im": ("s5_lam_im_f", "s5_lam_im_b"),
             "log_step": ("s5_log_step_f", "s5_log_step_b"), "b_re": ("s5_b_re_f", "s5_b_re_b"),
             "b_im": ("s5_b_im_f", "s5_b_im_b"), "c_re": ("s5_c_re_f", "s5_c_re_b"), "c_im": ("s5_c_im_f", "s5_c_im_b")}

    def st(nm):
        f_, b_ = pairs[nm]
        return np.stack([inp[f_][0], inp[b_][0]], 0)
    lam = np.stack([st("lam_re"), st("lam_im")], 0)
    lam = lam.reshape(2, 2, 2, 16, 64).transpose(2, 4, 0, 1, 3)
    o["s5_lam"] = np.ascontiguousarray(lam.reshape(128, 2, 32))
    ls = st("log_step").reshape(2, 2, 16)
    ls = np.broadcast_to(ls.transpose(1, 0, 2)[:, None], (2, 64, 2, 16))
    o["s5_step"] = np.ascontiguousarray(ls.reshape(128, 32))
    b = np.stack([st("b_re"), st("b_im")], 0)
    b = b.reshape(2, 2, 2, 16, 64, 16).transpose(2, 4, 0, 1, 3, 5)
    o["s5_b"] = np.ascontiguousarray(b.reshape(128, 2, 512))
    cm = np.stack([st("c_re"), st("c_im")], 0)
    cm = cm.reshape(2, 2, 2, 16, 16, 64).transpose(2, 5, 0, 1, 3, 4)
    o["s5_c"] = np.ascontiguousarray(cm.reshape(128, 2, 512))
    d = inp["s5_d"][0].reshape(32, 16)
    o["s5_dvec"] = np.ascontiguousarray(np.broadcast_to(d.T[None], (8, 16, 32)).reshape(128, 32))
    o["s5_bglu"] = np.ascontiguousarray(inp["s5_b_glu"][0].reshape(4, 128).T)
    o["s5_normw"] = np.ascontiguousarray(inp["s5_norm"][0].reshape(4, 128).T)
    return o


def np2dt(a):
    if a.dtype == np.float32:
        return F32
    if a.dtype == ml_dtypes.bfloat16:
        return BF16
    raise ValueError(a.dtype)


class K:
    pass


def dram_bcast(ap, nparts, n, off=0):
    return bass.AP(ap.tensor, ap.offset + off, [[0, nparts], [1, n]])


def norm_transpose(k, name, src_fn, ntiles, gain_dram, outT, out_dt, outT_trk, resident=False):
    cx, ar = k.cx, k.ar
    m = ar.mark()
    gB = ar.alloc([1024], F32)
    tg = Trk()
    cx.dma("sync", gB, dram_bcast(gain_dram, 128, 1024), cx.fresh(), w=[tg])
    junk = ar.alloc([1024], BF16)
    tj = Trk()
    xn = [ar.alloc([1024], out_dt) for _ in range(2)]
    txn = [Trk(), Trk()]
    ss = ar.alloc([NT * 2, 1], F32)
    tss = [Trk() for _ in range(ntiles)]
    pdt = BF16 if out_dt == BF16 else F32
    ident = k.identb if out_dt == BF16 else k.ident
    srcs = []
    if resident:
        for i in range(ntiles):
            src, ts = src_fn(i)
            srcs.append((src, ts))
            cx.op("scalar", lambda h, src=src, i=i: h.activation(junk, src, AF.Square, accum_out=ss[:, 2 * i:2 * i + 1]), r=[ts], w=[tj, tss[0]])
        ssv = ss.rearrange("p (t two) one -> p t (two one)", two=2)
        cx.op("vector", lambda h: h.tensor_scalar(ssv[:, 0:ntiles, 1:2], ssv[:, 0:ntiles, 0:1], 1.0 / 1024, EPS, op0=ALU.mult, op1=ALU.add), r=[tss[0]], w=[tss[0]])
        cx.op("scalar", lambda h: h.activation(ssv[:, 0:ntiles, 1:2], ssv[:, 0:ntiles, 1:2], AF.Sqrt), r=[tss[0]], w=[tss[0]])
        cx.op("vector", lambda h: h.reciprocal(ssv[:, 0:ntiles, 1:2], ssv[:, 0:ntiles, 1:2]), r=[tss[0]], w=[tss[0]])
    for i in range(ntiles):
        rsi = ss[:, 2 * i + 1:2 * i + 2]
        if resident:
            src, ts = srcs[i]
            tsi = tss[0]
        else:
            src, ts = src_fn(i)
            ssi = ss[:, 2 * i:2 * i + 1]
            tsi = tss[i]
            cx.op("scalar", lambda h, src=src, ssi=ssi: h.activation(junk, src, AF.Square, accum_out=ssi), r=[ts], w=[tj, tss[i]])
            cx.op("vector", lambda h, ssi=ssi, rsi=rsi: h.tensor_scalar(rsi, ssi, 1.0 / 1024, EPS, op0=ALU.mult, op1=ALU.add), r=[tss[i]], w=[tss[i]])
            cx.op("scalar", lambda h, rsi=rsi: h.activation(rsi, rsi, AF.Sqrt), r=[tss[i]], w=[tss[i]])
            cx.op("vector", lambda h, rsi=rsi: h.reciprocal(rsi, rsi), r=[tss[i]], w=[tss[i]])
        b = i % 2
        cx.op("vector", lambda h, src=src, rsi=rsi, b=b: h.scalar_tensor_tensor(out=xn[b], in0=src, scalar=rsi, in1=gB, op0=ALU.mult, op1=ALU.mult),
              r=[ts, tsi, tg], w=[txn[b]])
        if out_dt == BF16:
            pb = k.psum[i % 2]
            tp = k.tpsum[i % 2]
            pv = pb.bitcast(BF16)
            for c in range(8):
                cx.op("tensor", lambda h, b=b, c=c, pv=pv: h.transpose(pv[:, c * 128:(c + 1) * 128], xn[b][:, c * 128:(c + 1) * 128], ident),
                      r=[txn[b]], w=[tp], inc=(c == 7))
            dst = outT[:, :, i * 128:(i + 1) * 128]
            eng = "scalar" if i % 2 == 0 else "vector"
            if eng == "scalar":
                cx.op(eng, lambda h, dst=dst, pv=pv: h.copy(dst, pv.rearrange("p (c t) -> p c t", c=8)), r=[tp], w=[outT_trk])
            else:
                cx.op(eng, lambda h, dst=dst, pv=pv: h.tensor_copy(dst, pv.rearrange("p (c t) -> p c t", c=8)), r=[tp], w=[outT_trk])
        else:
            for half in range(2):
                pb = k.psum[(2 * i + half) % 4]
                tp = k.tpsum[(2 * i + half) % 4]
                for c4 in range(4):
                    c = half * 4 + c4
                    cx.op("tensor", lambda h, b=b, c=c, c4=c4, pb=pb: h.transpose(pb[:, c4 * 128:(c4 + 1) * 128], xn[b][:, c * 128:(c + 1) * 128].bitcast(F32), ident),
                          r=[txn[b]], w=[tp], inc=(c4 == 3))
                dst = outT[:, half * 4:(half + 1) * 4, i * 128:(i + 1) * 128]
                if half == 0:
                    cx.op("scalar", lambda h, dst=dst, pb=pb: h.copy(dst, pb.rearrange("p (c t) -> p c t", c=4)), r=[tp], w=[outT_trk])
                else:
                    cx.op("vector", lambda h, dst=dst, pb=pb: h.tensor_copy(dst, pb.rearrange("p (c t) -> p c t", c=4)), r=[tp], w=[outT_trk])
    ar.release(m)


def s5_prep(k):
    cx, ar, D = k.cx, k.ar, k.D
    V = "vector"
    m0 = ar.mark()
    lam = ar.alloc([2, 32], F32)
    step = ar.alloc([32], F32)
    bb = ar.alloc([2, 512], F32)
    cc = ar.alloc([2, 512], F32)
    kvec = ar.alloc([16], F32)
    tl = Trk()
    sl_ = cx.fresh()
    for dst, nm in ((lam, "s5_lam"), (step, "s5_step"), (bb, "s5_b"), (cc, "s5_c"), (kvec, "kvec")):
        cx.dma("sync", dst, D[nm], sl_, w=[tl])
    T = Trk()

    def vop(fn, extra_r=()):
        cx.op(V, fn, r=[T, tl] + list(extra_r), w=[T])

    def aop(fn):
        cx.op("scalar", fn, r=[T, tl], w=[T])
    lre, lim = lam[:, 0, :], lam[:, 1, :]
    dl = ar.alloc([32], F32)
    re1 = ar.alloc([32], F32)
    im1 = ar.alloc([32], F32)
    aop(lambda h: h.activation(dl, step, AF.Exp))
    vop(lambda h: h.tensor_tensor(out=re1, in0=dl, in1=lre, op=ALU.mult))
    vop(lambda h: h.tensor_tensor(out=im1, in0=dl, in1=lim, op=ALU.mult))
    PWI = ar.alloc([16, 32], F32)
    PWR = ar.alloc([16, 32], F32)
    m_pw = ar.mark()
    KR = ar.alloc([16, 32], F32)
    KI = ar.alloc([16, 32], F32)
    kv_b = kvec.unsqueeze(2).to_broadcast([128, 16, 32])
    vop(lambda h: h.tensor_tensor(out=KR, in0=kv_b, in1=re1.unsqueeze(1).to_broadcast([128, 16, 32]), op=ALU.mult))
    vop(lambda h: h.tensor_tensor(out=KI, in0=kv_b, in1=im1.unsqueeze(1).to_broadcast([128, 16, 32]), op=ALU.mult))
    MAG = ar.alloc([16, 32], F32)
    aop(lambda h: h.activation(MAG, KR, AF.Exp))
    YI = ar.alloc([16, 32], I32)
    YF = ar.alloc([16, 32], F32)
    vop(lambda h: h.tensor_scalar(KI, KI, 1.0 / (2 * math.pi), None, op0=ALU.mult))
    vop(lambda h: h.tensor_copy(YI, KI))
    vop(lambda h: h.tensor_copy(YF, YI))
    vop(lambda h: h.tensor_tensor(out=KI, in0=KI, in1=YF, op=ALU.subtract))
    SH_ = ar.alloc([16, 32], F32)
    SQ_ = ar.alloc([16, 32], F32)
    aop(lambda h: h.activation(SH_, KI, AF.Sin, scale=math.pi))
    aop(lambda h: h.activation(SQ_, KI, AF.Sin, scale=math.pi / 2))
    CH_ = ar.alloc([16, 32], F32)
    vop(lambda h: h.tensor_tensor(out=CH_, in0=SQ_, in1=SQ_, op=ALU.mult))
    vop(lambda h: h.tensor_scalar(CH_, CH_, -2.0, 1.0, op0=ALU.mult, op1=ALU.add))
    vop(lambda h: h.tensor_tensor(out=PWI, in0=SH_, in1=CH_, op=ALU.mult))
    vop(lambda h: h.scalar_tensor_tensor(out=PWI, in0=PWI, scalar=2.0, in1=MAG, op0=ALU.mult, op1=ALU.mult))
    vop(lambda h: h.tensor_tensor(out=PWR, in0=SH_, in1=SH_, op=ALU.mult))
    vop(lambda h: h.tensor_scalar(PWR, PWR, -2.0, 1.0, op0=ALU.mult, op1=ALU.add))
    vop(lambda h: h.tensor_tensor(out=PWR, in0=PWR, in1=MAG, op=ALU.mult))
    ar.release(m_pw)
    lrm1 = ar.alloc([32], F32)
    li = PWI[:, 8, :]
    t1 = ar.alloc([32], F32)
    t2 = ar.alloc([32], F32)
    den = ar.alloc([32], F32)
    c0r = ar.alloc([32], F32)
    c0i = ar.alloc([32], F32)
    vop(lambda h: h.tensor_scalar(lrm1, PWR[:, 8, :], -1.0, None, op0=ALU.add))
    vop(lambda h: h.tensor_tensor(out=t1, in0=lre, in1=lre, op=ALU.mult))
    vop(lambda h: h.tensor_tensor(out=t2, in0=lim, in1=lim, op=ALU.mult))
    vop(lambda h: h.tensor_tensor(out=den, in0=t1, in1=t2, op=ALU.add))
    vop(lambda h: h.reciprocal(den, den))
    vop(lambda h: h.tensor_tensor(out=t1, in0=lrm1, in1=lre, op=ALU.mult))
    vop(lambda h: h.tensor_tensor(out=t2, in0=li, in1=lim, op=ALU.mult))
    vop(lambda h: h.tensor_tensor(out=t1, in0=t1, in1=t2, op=ALU.add))
    vop(lambda h: h.tensor_tensor(out=c0r, in0=t1, in1=den, op=ALU.mult))
    vop(lambda h: h.tensor_tensor(out=t1, in0=li, in1=lre, op=ALU.mult))
    vop(lambda h: h.tensor_tensor(out=t2, in0=lrm1, in1=lim, op=ALU.mult))
    vop(lambda h: h.tensor_tensor(out=t1, in0=t1, in1=t2, op=ALU.subtract))
    vop(lambda h: h.tensor_tensor(out=c0i, in0=t1, in1=den, op=ALU.mult))
    BBR = ar.alloc([32, 16], F32)
    BBI = ar.alloc([32, 16], F32)
    TA = ar.alloc([32, 16], F32)
    br = bb[:, 0, :].rearrange("p (a c) -> p a c", c=16)
    bi = bb[:, 1, :].rearrange("p (a c) -> p a c", c=16)
    c0r_b = c0r.unsqueeze(2).to_broadcast([128, 32, 16])
    c0i_b = c0i.unsqueeze(2).to_broadcast([128, 32, 16])
    vop(lambda h: h.tensor_tensor(out=BBR, in0=br, in1=c0r_b, op=ALU.mult))
    vop(lambda h: h.tensor_tensor(out=TA, in0=bi, in1=c0i_b, op=ALU.mult))
    vop(lambda h: h.tensor_tensor(out=BBR, in0=BBR, in1=TA, op=ALU.subtract))
    vop(lambda h: h.tensor_tensor(out=BBI, in0=bi, in1=c0r_b, op=ALU.mult))
    vop(lambda h: h.tensor_tensor(out=TA, in0=br, in1=c0i_b, op=ALU.mult))
    vop(lambda h: h.tensor_tensor(out=BBI, in0=BBI, in1=TA, op=ALU.add))
    ASd = ar.alloc([16, 2, 8, 16], F32)
    CS2d = ar.alloc([16, 2, 8, 16], F32)
    T1 = ar.alloc([8, 16, 16], F32)
    T2 = ar.alloc([8, 16, 16], F32)
    cr = cc[:, 0, :].rearrange("p (d a c) -> p d a c", d=2, c=16)
    ci = cc[:, 1, :].rearrange("p (d a c) -> p d a c", d=2, c=16)
    BBR4 = BBR.rearrange("p (d a) c -> p d a c", d=2)
    BBI4 = BBI.rearrange("p (d a) c -> p d a c", d=2)

    def pw(arr, d, k0, kstep):
        return pap(arr, 0, 128, k0 * 32 + d * 16, [[kstep * 32, 8], [1, 16], [0, 16]])

    def dst(arr, dofs, ri):
        return pap(arr, 0, 128, dofs * 4096 + ri * 128, [[16, 8], [256, 16], [1, 16]])

    def vec(v4, d):
        a_ = v4[:, d]
        return bass.AP(a_.tensor, a_.offset, [list(a_.ap[0]), [0, 8], list(a_.ap[1]), list(a_.ap[2])])

    T1f = T1.rearrange("p a b c -> p (a b c)")

    def cmul(out_arr, dofs, d, k0, kstep, vr, vi, neg_im):
        pr, pi_ = pw(PWR, d, k0, kstep), pw(PWI, d, k0, kstep)
        vop(lambda h: h.tensor_tensor(out=T1, in0=pr, in1=vec(vr, d), op=ALU.mult))
        vop(lambda h: h.tensor_tensor(out=T2, in0=pi_, in1=vec(vi, d), op=ALU.mult))
        vop(lambda h: h.tensor_tensor(out=dst(out_arr, dofs, 0), in0=T1, in1=T2, op=ALU.subtract))
        vop(lambda h: h.tensor_tensor(out=T1, in0=pr, in1=vec(vi, d), op=ALU.mult))
        vop(lambda h: h.tensor_tensor(out=T2, in0=pi_, in1=vec(vr, d), op=ALU.mult))
        if neg_im:
            vop(lambda h: h.tensor_scalar(T1f, T1f, -1.0, None, op0=ALU.mult))
            vop(lambda h: h.tensor_tensor(out=dst(out_arr, dofs, 1), in0=T1, in1=T2, op=ALU.subtract))
        else:
            vop(lambda h: h.tensor_tensor(out=dst(out_arr, dofs, 1), in0=T1, in1=T2, op=ALU.add))
    vop(lambda h: h.tensor_copy(k.s5A1[:, 0:32], PWR[:, 15, :]))
    vop(lambda h: h.tensor_copy(k.s5A1[:, 32:64], PWR[:, 15, :]))
    vop(lambda h: h.tensor_scalar(k.s5A2[:, 0:32], PWI[:, 15, :], -1.0, None, op0=ALU.mult))
    vop(lambda h: h.tensor_copy(k.s5A2[:, 32:64], PWI[:, 15, :]))
    cmul(k.s5CS, 0, 0, 8, 1, cr, ci, True)
    cmul(k.s5CS, 1, 1, 15, -1, cr, ci, True)
    k.t_s5w = T
    mf = ar.alloc([2, 128], F32)
    dv = ar.alloc([32], F32)
    tm = Trk()
    sl_ = cx.fresh()
    cx.dma("sync", mf[:, 0, :], D["s5mf"], sl_, w=[tm])
    cx.dma("sync", mf[:, 1, :], D["s5mb"], sl_, w=[tm])
    cx.dma("sync", dv, D["s5_dvec"], sl_, w=[tm])
    tt1 = [ar.alloc([128], F32) for _ in range(2)]
    ttt = [Trk(), Trk()]
    ASb = ASd.rearrange("p a r s c -> p (a r) (s c)")
    ASm = ASd.rearrange("p a r s c -> p a r (s c)")
    CSm = CS2d.rearrange("p a r s c -> p a r (s c)")
    for d in range(2):
        if d == 0:
            cmul(ASd, 0, 0, 14, -1, BBR4, BBI4, False)
            cmul(CS2d, 0, 0, 0, 1, cr, ci, True)
        else:
            cmul(ASd, 0, 1, 7, 1, BBR4, BBI4, False)
            cmul(CS2d, 0, 1, 7, -1, cr, ci, True)
        for grp in range(8):
            pb = k.psum[grp % 4]
            tp = k.tpsum[grp % 4]
            for j in range(4):
                blk = grp * 4 + j
                cx.op("tensor", lambda h, pb=pb, j=j, blk=blk: h.transpose(pb[:, j * 128:(j + 1) * 128], ASb[:, blk, :], k.ident),
                      r=[T], w=[tp], inc=(j == 3))
            dstv = k.s5AT[:, d * 32 + grp * 4:d * 32 + (grp + 1) * 4, :]
            if grp % 2 == 0:
                cx.op("scalar", lambda h, dstv=dstv, pb=pb: h.copy(dstv, pb.rearrange("p (j x) -> p j x", j=4)), r=[tp], w=[k.t_s5at])
            else:
                cx.op("vector", lambda h, dstv=dstv, pb=pb: h.tensor_copy(dstv, pb.rearrange("p (j x) -> p j x", j=4)), r=[tp], w=[k.t_s5at])
        for g in range(32):
            gh, gl = g // 16, g % 16
            pb = k.psum[4 + g % 4]
            tp = k.tpsum[4 + g % 4]
            for ri in range(2):
                cx.op("tensor", lambda h, pb=pb, ri=ri, gh=gh, gl=gl: h.matmul(
                    pb[:, 0:128], ASm[gh * 64:(gh + 1) * 64, gl, ri, :], CSm[gh * 64:(gh + 1) * 64, gl, ri, :],
                    start=(ri == 0), stop=(ri == 1)), r=[T], w=[tp], inc=(ri == 1))
            b_ = g % 2
            cx.op(V, lambda h, pb=pb, b_=b_, d=d: h.tensor_tensor(out=tt1[b_], in0=pb[:, 0:128], in1=mf[:, d, :], op=ALU.mult), r=[tp, tm], w=[ttt[b_]])
            if d == 0:
                cx.op(V, lambda h, b_=b_, g=g: h.scalar_tensor_tensor(out=k.s5TT[:, g, :], in0=k.ident, scalar=dv[:, g:g + 1], in1=tt1[b_], op0=ALU.mult, op1=ALU.add),
                      r=[ttt[b_], tm], w=[k.t_s5tt])
            else:
                cx.op(V, lambda h, b_=b_, g=g: h.tensor_tensor(out=k.s5TT[:, g, :], in0=k.s5TT[:, g, :], in1=tt1[b_], op=ALU.add),
                      r=[ttt[b_]], w=[k.t_s5tt])
    cx.barrier()
    ar.release(m0)


def load_w_cols(k, wdram, col0, ncols, dst, trk, slot, eng="gpsimd"):
    src = bass.AP(wdram.tensor, wdram.offset + col0, [[wdram.ap[0][0] * 1, 128], [wdram.ap[0][0] * 128, 8], [1, ncols]])
    return k.cx.dma(eng, dst, src, slot, w=[trk])


def proj_fm(k, wt, wtrk, consume):
    cx = k.cx
    for n in range(4):
        pb = k.psum[n % 2 + 2]
        tp = k.tpsum[n % 2 + 2]
        for c in range(8):
            cx.op("tensor", lambda h, pb=pb, c=c, n=n: h.matmul(pb[:, :], wt[:, c, :], k.hT[:, c, n * 512:(n + 1) * 512], start=(c == 0), stop=(c == 7)),
                  r=[wtrk, k.t_hT], w=[tp], inc=(c == 7))
        consume(n, pb, tp)


def s5_build_U(k):
    cx, ar = k.cx, k.ar
    m0 = ar.mark()
    wt = [ar.alloc([8, 128], BF16) for _ in range(2)]
    twt = [Trk(), Trk()]
    swt = [cx.fresh('sw'), cx.fresh('sw')]
    uT = [ar.alloc([2048], BF16) for _ in range(2)]
    tuT = [Trk(), Trk()]
    for ct in range(4):
        b = ct % 2
        load_w_cols(k, k.D["w_in"], ct * 128, 128, wt[b], twt[b], swt[b])

        def consume(n, pb, tp, b=b):
            if n % 2 == 0:
                cx.op("scalar", lambda h: h.copy(uT[b][:, n * 512:(n + 1) * 512], pb[:, :]), r=[tp], w=[tuT[b]])
            else:
                cx.op("vector", lambda h: h.tensor_copy(uT[b][:, n * 512:(n + 1) * 512], pb[:, :]), r=[tp], w=[tuT[b]])
        proj_fm(k, wt[b], twt[b], consume)
        for gi in range(8):
            g = ct * 8 + gi
            q0 = 32 * (gi // 2)
            pb = k.psum[4 + gi % 4]
            tp = k.tpsum[4 + gi % 4]
            for s in range(8):
                rhs = pap(uT[b], q0, 32, s, [[8, 256]])
                cx.op("tensor", lambda h, pb=pb, s=s, rhs=rhs, q0=q0, gi=gi: h.matmul(pb[:, 0:256], k.selT[q0:q0 + 32, gi % 2, s, :], rhs, start=(s == 0), stop=(s == 7), tile_position=(q0, 0)),
                      r=[tuT[b]], w=[tp], inc=(s == 7))
            if gi % 2 == 0:
                cx.op("scalar", lambda h, pb=pb, g=g: h.copy(k.s5U[:, g, :], pb[:, 0:256]), r=[tp], w=[k.t_s5U])
            else:
                cx.op("vector", lambda h, pb=pb, g=g: h.tensor_copy(k.s5U[:, g, :], pb[:, 0:256]), r=[tp], w=[k.t_s5U])
    cx.barrier()
    ar.release(m0)


def s5_main(k, yT, t_yT):
    cx, ar, D = k.cx, k.ar, k.D
    V = "vector"
    m0 = ar.mark()
    SH = ar.alloc([2, 257, 2, 16], BF16)
    tSH = Trk()
    tSHh = Trk()
    X = [ar.alloc([64], F32) for _ in range(3)]
    tX = [Trk() for _ in range(3)]
    t1 = ar.alloc([64], F32)
    t2 = ar.alloc([64], F32)
    tt = Trk()
    tt2 = Trk()
    cx.op("gpsimd", lambda h: h.memset(SH[:, 0, 0, :, :], 0.0), w=[tSH])
    cx.op("gpsimd", lambda h: h.memset(SH[:, 1, 256, :, :], 0.0), w=[tSH])
    cx.op("gpsimd", lambda h: h.memset(X[0], 0.0), w=[tX[0]])
    n = 0
    for gl in range(16):
        for d in range(2):
            for ri in range(2):
                blk = d * 32 + gl * 2 + ri
                pb = k.psum[n % 4]
                tp = k.tpsum[n % 4]
                cx.op("tensor", lambda h, pb=pb, blk=blk, gl=gl: h.matmul(pb[0:64, 0:256], k.s5AT[:, blk, 0:64], k.s5U[:, gl, :], start=True, stop=True),
                      r=[k.t_s5at, k.t_s5U], w=[tp], inc=False)
                cx.op("tensor", lambda h, pb=pb, blk=blk, gl=gl: h.matmul(pb[64:128, 0:256], k.s5AT[:, blk, 64:128], k.s5U[:, 16 + gl, :], start=True, stop=True),
                      r=[k.t_s5at, k.t_s5U], w=[tp])
                slot0 = 1 if d == 0 else 0
                dstv = pap(SH, 0, 128, d * 257 * 32 + slot0 * 32 + ri * 16 + gl, [[32, 256]])
                if n % 2 == 0:
                    cx.op("scalar", lambda h, dstv=dstv, pb=pb: h.copy(dstv, pb[:, 0:256]), r=[tp], w=[tSH])
                else:
                    cx.op(V, lambda h, dstv=dstv, pb=pb: h.tensor_copy(dstv, pb[:, 0:256]), r=[tp], w=[tSH])
                n += 1
    import os
    S5STOP = os.environ.get('S5_STOP', '')
    if S5STOP == 'a':
        cx.barrier(); ar.release(m0); return
    for i in range(256):
        xp, xn = X[i % 3], X[(i + 1) % 3]
        txp, txn = tX[i % 3], tX[(i + 1) % 3]
        xsw = pap(xp, 0, 128, 32, [[-32, 2], [1, 32]])
        bf = (i + 1) * 32
        bb_ = 257 * 32 + (255 - i) * 32
        sview = pap(SH, 0, 128, bf, [[16, 2], [bb_ - bf, 2], [1, 16]])
        xp3 = xp.rearrange("p (r x) -> p r x", r=2)
        cx.op("gpsimd", lambda h, xsw=xsw: h.tensor_tensor(out=t2.rearrange("p (r x) -> p r x", r=2), in0=k.s5A2.rearrange("p (r x) -> p r x", r=2), in1=xsw, op=ALU.mult), r=[txp, k.t_s5w], w=[tt2])
        cx.op(V, lambda h, xp=xp: h.tensor_tensor(out=t1, in0=k.s5A1, in1=xp, op=ALU.mult), r=[txp, k.t_s5w], w=[tt])
        cx.op(V, lambda h, sview=sview: h.tensor_tensor(out=t1.rearrange("p (r d x) -> p r d x", r=2, d=2), in0=t1.rearrange("p (r d x) -> p r d x", r=2, d=2), in1=sview, op=ALU.add), r=[tt, tSH], w=[tt])
        cx.op(V, lambda h, xn=xn: h.tensor_tensor(out=xn, in0=t1, in1=t2, op=ALU.add), r=[tt, tt2], w=[txn])
        cx.op("scalar", lambda h, xn=xn, sview=sview: h.copy(sview, xn.rearrange("p (r d x) -> p r d x", r=2, d=2)), r=[txn], w=[tSHh])
    if S5STOP == 'rec':
        cx.barrier(); ar.release(m0); return
    gT = ar.alloc([4, 2048], F32)
    gTb = ar.alloc([4, 2048], BF16)
    tgT = [Trk() for _ in range(4)]
    tgTb = [Trk() for _ in range(4)]
    ybuf = [k.arA.alloc([8, 256], BF16) for _ in range(2)]
    tyb = [Trk(), Trk()]
    for ct in range(4):
        b = ct % 2
        for gi in range(8):
            g = ct * 8 + gi
            gh, gl = g // 16, g % 16
            pb = k.psum[gi % 2]
            tp = k.tpsum[gi % 2]
            cx.op("tensor", lambda h, pb=pb, g=g: h.matmul(pb[:, 0:256], k.s5TT[:, g, :], k.s5U[:, g, :], start=True, stop=False),
                  r=[k.t_s5tt, k.t_s5U], w=[tp], inc=False)
            for d in range(2):
                for ri in range(2):
                    slot0 = 0 if d == 0 else 1
                    rhs = pap(SH, gh * 64, 64, d * 257 * 32 + slot0 * 32 + ri * 16 + gl, [[32, 256]])
                    last = (d == 1 and ri == 1)
                    cx.op("tensor", lambda h, pb=pb, rhs=rhs, d=d, ri=ri, gh=gh, gl=gl, last=last: h.matmul(
                        pb[:, 0:256], k.s5CS[gh * 64:(gh + 1) * 64, d, gl, ri, :], rhs, start=False, stop=last),
                        r=[tSH, tSHh, k.t_s5w], w=[tp], inc=last)
            if gi % 2 == 0:
                cx.op("scalar", lambda h, pb=pb, b=b, gi=gi: h.copy(ybuf[b][:, gi, :], pb[:, 0:256]), r=[tp], w=[tyb[b]])
            else:
                cx.op(V, lambda h, pb=pb, b=b, gi=gi: h.tensor_copy(ybuf[b][:, gi, :], pb[:, 0:256]), r=[tp], w=[tyb[b]])
        for t in range(8):
            q0 = 32 * (t // 2)
            pb = k.psum[2 + t % 4]
            tp = k.tpsum[2 + t % 4]
            for gi in range(8):
                cx.op("tensor", lambda h, pb=pb, t=t, gi=gi, q0=q0, b=b: h.matmul(pb[:, 0:256], k.selT[q0:q0 + 32, t % 2, gi, :], ybuf[b][q0:q0 + 32, gi, :], start=(gi == 0), stop=(gi == 7), tile_position=(q0, 0)),
                      r=[tyb[b]], w=[tp], inc=(gi == 7))
            dstv = pap(gT, 0, 128, ct * 2048 + t, [[8, 256]])
            cx.op("scalar", lambda h, pb=pb, dstv=dstv: h.activation(dstv, pb[:, 0:256], AF.Gelu), r=[tp], w=[tgT[ct]])
        cx.op("vector", lambda h, ct=ct: h.tensor_copy(gTb[:, ct, :], gT[:, ct, :]), r=[tgT[ct]], w=[tgTb[ct]])
    k.dbg_add("s5_g", gT, tgT)
    if S5STOP == 'c':
        cx.barrier(); ar.release(m0); return
    wg = ar.alloc([4, 512], BF16)
    twg = Trk()
    wsrc = D["s5_w_glu"]
    cx.dma("gpsimd", wg, bass.AP(wsrc.tensor, wsrc.offset, [[512, 128], [512 * 128, 4], [1, 512]]), cx.fresh('sw'), w=[twg])
    bgl = ar.alloc([4], F32)
    nw = ar.alloc([4], F32)
    tb = Trk()
    sl_ = cx.fresh()
    cx.dma("sync", bgl, D["s5_bglu"], sl_, w=[tb])
    cx.dma("sync", nw, D["s5_normw"], sl_, w=[tb])
    sig = ar.alloc([4, 512], BF16)
    tsig = Trk()
    sq = ar.alloc([4, 512], BF16)
    tsq = Trk()
    rs = ar.alloc([512], F32)
    trs = Trk()
    for nck in range(4):
        ts = slice(nck * 512, (nck + 1) * 512)
        for co in range(4):
            pb = k.psum[co % 2]
            tp = k.tpsum[co % 2]
            for ci in range(4):
                cx.op("tensor", lambda h, pb=pb, co=co, ci=ci, ts=ts: h.matmul(pb[:, :], wg[:, ci, co * 128:(co + 1) * 128], gTb[:, ci, ts], start=(ci == 0), stop=(ci == 3)),
                      r=[twg] + tgTb, w=[tp], inc=(ci == 3))
            cx.op("scalar", lambda h, pb=pb, co=co: h.activation(sig[:, co, :], pb[:, :], AF.Sigmoid, bias=bgl[:, co:co + 1]), r=[tp, tb], w=[tsig])
        for co in range(4):
            cx.op(V, lambda h, co=co, ts=ts: h.tensor_tensor(out=gT[:, co, ts], in0=gT[:, co, ts], in1=sig[:, co, :], op=ALU.mult), r=[tsig, tgT[co]], w=[tgT[co]])
            cx.op("scalar", lambda h, co=co, ts=ts: h.activation(sq[:, co, :], gT[:, co, ts], AF.Square), r=[tgT[co]], w=[tsq])
        pb = k.psum[2 + nck % 2]
        tp = k.tpsum[2 + nck % 2]
        for co in range(4):
            cx.op("tensor", lambda h, pb=pb, co=co: h.matmul(pb[:, :], k.onesb, sq[:, co, :], start=(co == 0), stop=(co == 3)), r=[tsq], w=[tp], inc=(co == 3))
        cx.op("scalar", lambda h, pb=pb: h.activation(rs, pb[:, :], AF.Sqrt, scale=1.0 / 512, bias=k.epsc), r=[tp], w=[trs])
        cx.op(V, lambda h: h.reciprocal(rs, rs), r=[trs], w=[trs])
        for co in range(4):
            cx.op(V, lambda h, co=co, ts=ts: h.scalar_tensor_tensor(out=yT[:, co, ts], in0=gT[:, co, ts], scalar=nw[:, co:co + 1], in1=rs, op0=ALU.mult, op1=ALU.mult),
                  r=[tgT[co], trs, tb, tsq], w=[t_yT])
    k.dbg_add("s5_gl", gT, tgT)
    cx.barrier()
    ar.release(m0)


def build(in_shapes, stage="full", dbg_names=(), n_heads=4, n_experts=32):
    nc = bass.Bass("TRN2", target_bir_lowering=False)
    k = K()
    k.n_heads = n_heads
    k.n_experts = n_experts
    k.nc = nc
    D = {}
    for nm, (shape, dt) in in_shapes.items():
        D[nm] = nc.dram_tensor(nm, list(shape), dt, kind="ExternalInput").ap()
    k.D = D
    out = nc.dram_tensor("out", [S, DM], F32, kind="ExternalOutput").ap()
    k.dbg = {}
    k.dbg_req = set(dbg_names)

    with contextlib.ExitStack() as st:
        cx = Ctx(nc, st)
        k.cx = cx

        def finish():
            deps = [(s_.key, s_.total) for s_ in cx.slots if s_.total > 0]
            cx.wait_deps("sync", deps + [(e, cx.cnt[e]) for e in ENGS if e != "sync" and cx.cnt[e] > 0])
            with nc.Block() as block:
                cx.emit_all(block)
            k.n_ops = cx.n_ops
            return nc, k
        k.slot_c = cx.slot("c")
        k.slot_w = cx.slot("w")
        k.slot_x = [cx.slot("x0"), cx.slot("x1")]
        k.slot_o = cx.slot("o")
        k.psum = [cx.ps("ps%d" % i, [128, 512], F32) for i in range(8)]
        k.psum = [p[:, :] for p in k.psum]
        k.tpsum = [Trk("ps%d" % i, excl=True) for i in range(8)]
        k.ident = cx.sb("ident", [128, 128], F32)[:, :]
        k.identb = cx.sb("identb", [128, 128], BF16)[:, :]
        k.ones = cx.sb("ones", [128, 128], F32)[:, :]
        k.onesb = cx.sb("onesb", [128, 128], BF16)[:, :]
        k.epsc = cx.sb("epsc", [128, 1], F32)[:, :]
        k.selT = cx.sb("selT", [128, 2, 8, 128], BF16)[:, :, :, :]
        tc = Trk()
        cx.dma("sync", k.ident, D["ident"], k.slot_c, w=[tc])
        cx.dma("sync", k.identb, D["identb"], k.slot_c, w=[tc])
        cx.dma("sync", k.ones, D["ones"], k.slot_c, w=[tc])
        cx.dma("gpsimd", k.onesb, D["ones"], cx.fresh("sw"), w=[tc])
        cx.op("vector", lambda h: h.memset(k.epsc, EPS), w=[tc])
        cx.dma("sync", k.selT, D["selT"], k.slot_c, w=[tc])
        k.s5A1 = cx.sb("s5A1", [128, 64], F32)[:, :]
        k.s5A2 = cx.sb("s5A2", [128, 64], F32)[:, :]
        ar = Arena(cx, 51456)
        k.ar = ar
        cx.barrier()

        def dbg_add(name, ap, trks):
            if name in k.dbg_req:
                shape = list(ap.shape)
                dt_ = F32
                o = nc.dram_tensor("dbg_" + name, shape, dt_, kind="ExternalOutput").ap()
                cx.dma("gpsimd" if ap.dtype != F32 else "sync", o, ap, cx.fresh("sw" if ap.dtype != F32 else "hw"), r=list(trks))
        k.dbg_add = dbg_add

        regA = ar.alloc([NT * 1024], F32)
        arA = Arena(cx, NT * 1024, base=regA)
        k.arA = arA
        yT = ar.alloc([8, 2048], BF16)
        t_yT = Trk()
        k.s5U = arA.alloc([32, 256], BF16)
        k.t_s5U = Trk()
        m_h = arA.mark()
        k.hT = arA.alloc([8, 2048], BF16)
        k.t_hT = Trk()

        m1 = ar.mark()
        xt = [ar.alloc([1024], F32) for _ in range(2)]
        txt = [Trk(), Trk()]

        def src_x(i):
            b = i % 2
            cx.dma("sync", xt[b], D["x"][i * 128:(i + 1) * 128, :], k.slot_x[b], w=[txt[b]])
            return xt[b], txt[b]
        norm_transpose(k, "mix", src_x, NT, D["norm_mix"], k.hT, BF16, k.t_hT)
        cx.barrier()
        ar.release(m1)

        if stage == 'p1':
            return finish()
        s5_build_U(k)
        if stage == 'U':
            return finish()
        mg = ar.mark()
        gdn_setup(k)
        if k.n_heads > 0:
            gdn_load_weights(k, 0)
        for hd in range(k.n_heads):
            gdn_head(k, hd, yT, t_yT)
        ar.release(mg)
        k.dbg_add("ygdnT", yT[:, 4:8, :], [t_yT])
        if stage == 'gdn':
            return finish()
        cx.barrier()
        arA.release(m_h)
        k.s5AT = arA.alloc([64, 128], BF16)
        k.t_s5at = Trk()
        k.s5CS = arA.alloc([2, 16, 2, 128], BF16)
        k.s5TT = arA.alloc([32, 128], BF16)
        k.t_s5tt = Trk()
        s5_prep(k)
        if stage == 's5prep':
            return finish()
        s5_main(k, yT[:, 0:4, :], t_yT)
        k.dbg_add("ys5T", yT[:, 0:4, :], [t_yT])
        if stage == "s5":
            return finish()
        if True:
            cx.barrier()
            k.xacc = regA.rearrange('p (a b) -> p a b', a=NT)
            k.txacc = [Trk() for _ in range(NT)]
            out_proj(k, yT, t_yT)
            k.dbg_add("x1", k.xacc, k.txacc)
            if stage == 'oproj':
                return finish()
            xattn(k)
            if stage == 'xattn':
                return finish()
            k.dbg_add("x2", k.xacc, k.txacc)
            moe(k)
            k.dbg_add("x3", k.xacc, k.txacc + [t_ for p_ in k.txh for t_ in p_])
            final_norm(k, out)

        return finish()


def host_inputs(inp, b):
    m = {}
    m["x"] = np.ascontiguousarray(inp["x"][b])
    m["mem"] = np.ascontiguousarray(inp["mem"][b])
    m["norm_mix"] = inp["norm_mix"][0]
    m["w_in"] = inp["w_in"][0]
    m["w_out"] = inp["w_out"][0]
    m["s5_w_glu"] = inp["s5_w_glu"][0]
    m.update(host_s5(inp))
    cv = inp["gdn_conv"][0]
    m["gdn_convw"] = np.ascontiguousarray(cv.reshape(5, 3, 4, 128).transpose(3, 2, 1, 0))
    for nm in ("gdn_a_log_f", "gdn_dt_bias_f", "gdn_a_log_b", "gdn_dt_bias_b"):
        m[nm] = inp[nm][0]
    m["gdn_norm"] = inp["gdn_norm"][0]
    for nm in ("norm_xattn", "norm_mem", "xa_wq", "xa_wk", "xa_wv", "xa_wo", "norm_moe", "router_group_w", "router_group_b",
               "router_expert_w", "router_expert_b", "moe_w_gate", "moe_w_up", "moe_w_down"):
        m[nm] = inp[nm][0]
    m["norm_final"] = inp["norm_final"]
    m.update(host_consts())
    return m


def gdn_setup(k):
    cx, ar, D = k.cx, k.ar, k.D
    V = "vector"
    G = K()
    k.G = G
    G.mask = ar.alloc([7, 128], F32)
    G.tmask = Trk()
    cx.dma("sync", G.mask, D["gmask"][:, 0:7, :], cx.fresh(), w=[G.tmask])
    wsm = ar.alloc([8, 16], BF16)
    tw = Trk()
    load_w_cols(k, D["w_in"], 2560, 16, wsm, tw, cx.fresh('sw'))
    BA = ar.alloc([16, 16], F32)
    tBA = Trk()
    for i in range(NT):
        pb = k.psum[i % 4]
        tp = k.tpsum[i % 4]
        for c in range(8):
            cx.op("tensor", lambda h, pb=pb, c=c, i=i: h.matmul(pb[:, 0:16], k.hT[:, c, i * 128:(i + 1) * 128], wsm[:, c, :], start=(c == 0), stop=(c == 7)),
                  r=[tw, k.t_hT], w=[tp], inc=(c == 7))
        cx.op("scalar", lambda h, pb=pb, i=i: h.copy(BA[:, i, :], pb[:, 0:16]), r=[tp], w=[tBA])
    pr = ar.alloc([4, 4], F32)
    tpr = Trk()
    sl_ = cx.fresh()
    for j, nm in enumerate(("gdn_a_log_f", "gdn_dt_bias_f", "gdn_a_log_b", "gdn_dt_bias_b")):
        cx.dma("sync", pr[:, j, :], dram_bcast(D[nm], 128, 4), sl_, w=[tpr])
    G.nw = ar.alloc([128], F32)
    cx.dma("sync", G.nw, dram_bcast(D["gdn_norm"], 128, 128), sl_, w=[tpr])
    G.tpr = tpr
    T = Trk()
    G.T = T
    G.beta, G.nb, G.gc, G.eg, G.neg, G.ed = [], [], [], [], [], []
    def per_dir(d):
        beta = ar.alloc([16, 4], F32)
        nb = ar.alloc([16, 4], F32)
        g = ar.alloc([16, 4], F32)
        gc = ar.alloc([16, 4], F32)
        gt = ar.alloc([16, 4], F32)
        eg = ar.alloc([16, 4], F32)
        neg = ar.alloc([16, 4], F32)
        ed = ar.alloc([16, 4], F32)
        ea = ar.alloc([4], F32)
        braw = BA[:, :, d * 4:(d + 1) * 4]
        araw = BA[:, :, 8 + d * 4:8 + (d + 1) * 4]
        cx.op("scalar", lambda h: h.activation(beta, braw, AF.Sigmoid), r=[tBA, T], w=[T])
        cx.op(V, lambda h: h.tensor_scalar(nb, beta, -1.0, None, op0=ALU.mult), r=[T], w=[T])
        cx.op("scalar", lambda h: h.activation(ea, pr[:, 2 * d, :], AF.Exp), r=[tpr, T], w=[T])
        cx.op(V, lambda h: h.tensor_tensor(out=g, in0=araw, in1=pr[:, 2 * d + 1, :].unsqueeze(1).to_broadcast([128, 16, 4]), op=ALU.add), r=[tBA, tpr, T], w=[T])
        cx.op("scalar", lambda h: h.activation(g, g, AF.Exp), r=[T], w=[T])
        cx.op("scalar", lambda h: h.activation(g, g, AF.Ln, bias=1.0), r=[T], w=[T])
        cx.op(V, lambda h: h.scalar_tensor_tensor(out=g, in0=g, scalar=-1.0, in1=ea.unsqueeze(1).to_broadcast([128, 16, 4]), op0=ALU.mult, op1=ALU.mult), r=[T], w=[T])
        g2 = g.rearrange("p a b -> p (a b)")
        pb = k.psum[4 + d]
        tp = k.tpsum[4 + d]
        cx.op("tensor", lambda h, pb=pb, d=d: h.matmul(pb[:, 0:64], G.mask[:, d, :], g2, start=True, stop=True), r=[T, G.tmask], w=[tp])
        cx.op("tensor", lambda h, pb=pb: h.matmul(pb[:, 64:128], G.mask[:, 6, :], g2, start=True, stop=True), r=[T, G.tmask], w=[tp])
        cx.op(V, lambda h, pb=pb: h.tensor_copy(gc.rearrange("p a b -> p (a b)"), pb[:, 0:64]), r=[tp], w=[T])
        cx.op(V, lambda h, pb=pb: h.tensor_tensor(out=gt.rearrange("p a b -> p (a b)"), in0=pb[:, 64:128], in1=gc.rearrange("p a b -> p (a b)"), op=ALU.subtract), r=[tp, T], w=[T])
        cx.op("scalar", lambda h: h.activation(eg, gc, AF.Exp), r=[T], w=[T])
        cx.op("scalar", lambda h: h.activation(ed, gt, AF.Exp), r=[T], w=[T])
        cx.op(V, lambda h: h.tensor_scalar(neg, eg, -1.0, None, op0=ALU.mult), r=[T], w=[T])
        G.g = getattr(G, "g", []) + [g]
        G.beta.append(beta); G.nb.append(nb); G.gc.append(gc); G.eg.append(eg); G.neg.append(neg); G.ed.append(ed)
    per_dir(0)
    per_dir(1)
    G.osum = ar.alloc([16, 128], F32)
    G.tosum = [Trk() for _ in range(NT)]
    G.wset = [[k.arA.alloc([8, 128], BF16) for _ in range(4)] for _ in range(2)]
    G.tw = [[Trk() for _ in range(4)] for _ in range(2)]
    G.sw = [cx.slot("gw0"), cx.slot("gw1")]


def gdn_load_weights(k, hd):
    G, D = k.G, k.D
    st = hd % 2
    for j in range(3):
        load_w_cols(k, D["w_in"], 512 + j * 512 + hd * 128, 128, G.wset[st][j], G.tw[st][j], G.sw[st])
    load_w_cols(k, D["w_in"], 512 + 1536 + hd * 128, 128, G.wset[st][3], G.tw[st][3], G.sw[st])


def gdn_head(k, hd, yT, t_yT):
    cx, ar, D, G = k.cx, k.ar, k.D, k.G
    V = "vector"
    m0 = ar.mark()
    qnT = ar.alloc([2048], BF16)
    knT = ar.alloc([2048], BF16)
    Ktok = ar.alloc([16, 128], BF16)
    Vtok = ar.alloc([16, 128], BF16)
    tq, tk_, tKt, tVt = Trk(), Trk(), Trk(), Trk()
    st_ = hd % 2
    wz, twz = G.wset[st_][3], G.tw[st_][3]
    w3, tw3 = G.wset[st_][0:3], G.tw[st_][0:3]
    if hd + 1 < k.n_heads:
        gdn_load_weights(k, hd + 1)
    mA = ar.mark()
    cw = ar.alloc([3, 5], F32)
    tcw = Trk()
    cx.dma("sync", cw, D["gdn_convw"][:, hd, :, :], cx.fresh(), w=[tcw])
    diag = ar.alloc([15, 128], BF16)
    tdg = Trk()
    for j in range(3):
        for t in range(5):
            cx.op("vector", lambda h, j=j, t=t: h.tensor_scalar(diag[:, j * 5 + t, :], k.identb, cw[:, j, t:t + 1], None, op0=ALU.mult), r=[tcw], w=[tdg])
    import os
    ALV = int(os.environ.get("GDN_ALV", "9"))
    if ALV == 0:
        cx.barrier(); ar.release(m0); return
    raw = [ar.alloc([2052], BF16) for _ in range(2)]
    traw = [Trk(), Trk()]
    for b in range(2):
        cx.op("gpsimd", lambda h, b=b: h.memset(raw[b][:, 0:2], 0.0), w=[traw[b]])
        cx.op("gpsimd", lambda h, b=b: h.memset(raw[b][:, 2050:2052], 0.0), w=[traw[b]])
    act2 = [ar.alloc([2048], F32) for _ in range(2)]
    tact = Trk()
    vT = ar.alloc([2048], BF16)
    tvT = Trk()
    sqb = ar.alloc([2048], BF16)
    tsqb = Trk()
    rn = [ar.alloc([512], F32) for _ in range(4)]
    trn = [Trk() for _ in range(4)]
    tactn2 = [[Trk() for _ in range(4)] for _ in range(2)]
    tsqn = [Trk() for _ in range(4)]
    if ALV == 1:
        cx.barrier(); ar.release(m0); return
    for j in range(3):
        b = j % 2
        act = act2[j % 2]
        tactn = tactn2[j % 2]

        def consume(n, pb, tp, b=b):
            cx.op(V if n % 2 else "scalar", (lambda h: h.tensor_copy(raw[b][:, 2 + n * 512:2 + (n + 1) * 512], pb[:, :])) if n % 2 else
                  (lambda h: h.copy(raw[b][:, 2 + n * 512:2 + (n + 1) * 512], pb[:, :])), r=[tp], w=[traw[b]])
        proj_fm(k, w3[j], tw3[j], consume)
        for n in range(4):
            pb = k.psum[4 + n]
            tp = k.tpsum[4 + n]
            for t in range(5):
                cx.op("tensor", lambda h, pb=pb, t=t, n=n, j=j, b=b: h.matmul(pb[:, :], diag[:, j * 5 + t, :], raw[b][:, n * 512 + t:n * 512 + t + 512], start=(t == 0), stop=(t == 4)),
                      r=[tdg, traw[b]], w=[tp], inc=(t == 4))
        for n in range(4):
            pb = k.psum[4 + n]
            tp = k.tpsum[4 + n]
            ts = slice(n * 512, (n + 1) * 512)
            if j == 2:
                cx.op("scalar", lambda h, pb=pb, ts=ts: h.activation(vT[:, ts], pb[:, :], AF.Silu), r=[tp], w=[tvT])
            else:
                cx.op("scalar", lambda h, pb=pb, ts=ts, act=act: h.activation(act[:, ts], pb[:, :], AF.Silu), r=[tp], w=[tactn[n]])
        if j < 2 and ALV > 2:
            for n in range(4):
                ts = slice(n * 512, (n + 1) * 512)
                cx.op("scalar", lambda h, ts=ts, act=act: h.activation(sqb[:, ts], act[:, ts], AF.Square), r=[tactn[n]], w=[tsqn[n]])
            for n in range(4):
                ts = slice(n * 512, (n + 1) * 512)
                pb2 = k.psum[n]
                tp2 = k.tpsum[n]
                cx.op("tensor", lambda h, pb2=pb2, ts=ts: h.matmul(pb2[:, :], k.onesb, sqb[:, ts], start=True, stop=True), r=[tsqn[n]], w=[tp2])
            for n in range(4):
                pb2 = k.psum[n]
                tp2 = k.tpsum[n]
                cx.op("scalar", lambda h, pb2=pb2, n=n: h.activation(rn[n], pb2[:, :], AF.Sqrt, bias=k.epsc), r=[tp2], w=[trn[n]])
            for n in range(4):
                cx.op(V, lambda h, n=n: h.reciprocal(rn[n], rn[n]), r=[trn[n]], w=[trn[n]])
            dstT, tdst, scl = (qnT, tq, 128.0 ** -0.5) if j == 0 else (knT, tk_, 1.0)
            for n in range(4):
                ts = slice(n * 512, (n + 1) * 512)
                cx.op(V, lambda h, ts=ts, n=n, dstT=dstT, scl=scl, act=act: h.scalar_tensor_tensor(out=dstT[:, ts], in0=act[:, ts], scalar=scl, in1=rn[n], op0=ALU.mult, op1=ALU.mult),
                      r=[tactn[n], trn[n]], w=[tdst])
    if ALV <= 3:
        cx.barrier(); ar.release(m0); return
    TV = int(os.environ.get("GDN_TV", "0"))
    for i in range(NT):
        pb = k.psum[i % 2].bitcast(BF16)
        tp = k.tpsum[i % 2]
        if TV == 0:
            cx.op("tensor", lambda h, pb=pb, i=i: h.transpose(pb[:, 0:128], knT[:, i * 128:(i + 1) * 128], k.identb), r=[tk_], w=[tp])
            cx.op("tensor", lambda h, pb=pb, i=i: h.transpose(pb[:, 128:256], vT[:, i * 128:(i + 1) * 128], k.identb), r=[tvT], w=[tp])
            cx.op("scalar", lambda h, pb=pb, i=i: h.copy(Ktok[:, i, :], pb[:, 0:128]), r=[tp], w=[tKt])
            cx.op(V, lambda h, pb=pb, i=i: h.tensor_copy(Vtok[:, i, :], pb[:, 128:256]), r=[tp], w=[tVt])
        elif TV == 1:
            cx.op("tensor", lambda h, pb=pb, i=i: h.transpose(pb[:, 0:128], knT[:, i * 128:(i + 1) * 128], k.identb), r=[tk_], w=[tp])
            cx.op("scalar", lambda h, pb=pb, i=i: h.copy(Ktok[:, i, :], pb[:, 0:128]), r=[tp], w=[tKt])
        elif TV == 2:
            cx.op("tensor", lambda h, pb=pb, i=i: h.transpose(pb[:, 0:128], vT[:, i * 128:(i + 1) * 128], k.identb), r=[tvT], w=[tp])
            cx.op(V, lambda h, pb=pb, i=i: h.tensor_copy(Vtok[:, i, :], pb[:, 0:128]), r=[tp], w=[tVt])
    if hd == 0:
        k.dbg_add("gdn_qn", qnT, [tq])
        k.dbg_add("gdn_kn", knT, [tk_])
        k.dbg_add("gdn_vtok", Vtok, [tVt])
    cx.barrier()
    ar.release(mA)
    STOP = os.environ.get("GDN_STOP", "")
    if STOP == "A":
        ar.release(m0)
        return
    qgT = [ar.alloc([2048], BF16) for _ in range(2)]
    Kd = [ar.alloc([16, 128], BF16) for _ in range(2)]
    Pm = [ar.alloc([16, 128], BF16) for _ in range(2)]
    QKm = [ar.alloc([16, 128], BF16) for _ in range(2)]
    etot = [ar.alloc([32], F32) for _ in range(2)]
    WnT = [ar.alloc([2048], BF16) for _ in range(2)]
    U0b = [ar.alloc([16, 128], BF16) for _ in range(2)]
    tWn = [[Trk() for _ in range(NT)] for _ in range(2)]
    tU0 = [[Trk() for _ in range(NT)] for _ in range(2)]
    tqg = [[Trk() for _ in range(NT)] for _ in range(2)]
    tKd = [[Trk() for _ in range(NT)] for _ in range(2)]
    tPm = [[Trk() for _ in range(NT)] for _ in range(2)]
    tQK = [[Trk() for _ in range(NT)] for _ in range(2)]
    tet = [[Trk() for _ in range(NT)] for _ in range(2)]
    NI = 8
    mN = ar.mark()
    NDT = BF16 if os.environ.get('GDN_NEU', 'bf16') == 'bf16' else F32
    nid = k.identb if NDT == BF16 else k.ident
    Xb = [[ar.alloc([128], NDT) for _ in range(2)] for _ in range(NI)]
    XTb = [[ar.alloc([128], NDT) for _ in range(2)] for _ in range(NI)]
    Pb = [[ar.alloc([128], NDT) for _ in range(2)] for _ in range(NI)]

    def tview(pn_, c0):
        return pn_[:, c0:c0 + 128] if NDT == F32 else pn_.bitcast(BF16)[:, 2 * c0:2 * c0 + 128]
    tX = [Trk() for _ in range(NI)]
    EGB = [ar.alloc([128], F32) for _ in range(NI)]
    ET = [ar.alloc([128], BF16) for _ in range(NI)]
    ETs = ET
    tE = [Trk() for _ in range(NI)]
    tEG = [Trk() for _ in range(NI)]
    Kg = [ar.alloc([128], BF16) for _ in range(NI)]
    tKg = [Trk() for _ in range(NI)]
    tXP = [Trk() for _ in range(NI)]
    insts = [(i, d) for i in range(NT) for d in range(2)]
    for g0 in range(0, len(insts), NI):
        grp = insts[g0:g0 + NI]
        info = []
        for s_, (i, d) in enumerate(grp):
            info.append(dict(s_=s_, i=i, d=d, tsl=slice(i * 128, (i + 1) * 128),
                             col=pap(G.g[d], 0, 128, i * 4 + hd, [[0, 128]]),
                             gcc=G.gc[d][:, i, hd:hd + 1], nbc=G.nb[d][:, i, hd:hd + 1], edc=G.ed[d][:, i, hd:hd + 1],
                             pb=k.psum[s_], tp=k.tpsum[s_]))
        for q_ in info:
            s_, i, d, tsl, col, pb, tp = q_["s_"], q_["i"], q_["d"], q_["tsl"], q_["col"], q_["pb"], q_["tp"]
            cx.op("tensor", lambda h, pb=pb, col=col, d=d: h.matmul(pb[:, 0:128], col, G.mask[:, d, :], start=True, stop=True), r=[G.T, G.tmask], w=[tp], inc=False)
            cx.op("tensor", lambda h, pb=pb, tsl=tsl: h.matmul(pb[:, 128:256], knT[:, tsl], knT[:, tsl], start=True, stop=True), r=[tk_], w=[tp], inc=False)
            cx.op("tensor", lambda h, pb=pb, tsl=tsl: h.matmul(pb[:, 256:384], knT[:, tsl], qnT[:, tsl], start=True, stop=True), r=[tk_, tq], w=[tp])
        for q_ in info:
            s_, i, d, pb, tp, gcc = q_["s_"], q_["i"], q_["d"], q_["pb"], q_["tp"], q_["gcc"]
            cx.op("scalar", lambda h, pb=pb, s_=s_: h.activation(EGB[s_], pb[:, 0:128], AF.Exp), r=[tp], w=[tEG[s_]])
            cx.op(V, lambda h, pb=pb, s_=s_, gcc=gcc, d=d: h.scalar_tensor_tensor(out=ET[s_], in0=pb[:, 0:128], scalar=gcc, in1=G.mask[:, 2 + d, :], op0=ALU.subtract, op1=ALU.min),
                  r=[tp, G.T, G.tmask], w=[tE[s_]])
        for q_ in info:
            s_, i, d, tsl = q_["s_"], q_["i"], q_["d"], q_["tsl"]
            cx.op("scalar", lambda h, s_=s_: h.activation(ET[s_], ET[s_], AF.Exp), r=[tE[s_]], w=[tE[s_]])
            cx.op(V, lambda h, s_=s_, tsl=tsl, d=d: h.tensor_tensor(out=qgT[d][:, tsl], in0=qnT[:, tsl], in1=EGB[s_], op=ALU.mult), r=[tq, tEG[s_]], w=[tqg[d][i]])
        for q_ in info:
            s_, i, d, pb, tp, edc = q_["s_"], q_["i"], q_["d"], q_["pb"], q_["tp"], q_["edc"]
            c0, c1 = (63, 127) if d == 0 else (0, 64)
            cx.op("scalar", lambda h, s_=s_, d=d, i=i, c0=c0: h.copy(etot[d][:, 2 * i:2 * i + 1], EGB[s_][:, c0:c0 + 1]), r=[tEG[s_]], w=[tet[d][i]])
            cx.op("scalar", lambda h, s_=s_, d=d, i=i, c1=c1: h.copy(etot[d][:, 2 * i + 1:2 * i + 2], EGB[s_][:, c1:c1 + 1]), r=[tEG[s_]], w=[tet[d][i]])
            cx.op("scalar", lambda h, d=d, i=i, edc=edc: h.activation(Kd[d][:, i, :], Ktok[:, i, :], AF.Identity, scale=edc), r=[tKt, G.T], w=[tKd[d][i]])
            egc = G.eg[d][:, i, hd:hd + 1]
            cx.op("scalar", lambda h, s_=s_, i=i, egc=egc: h.activation(Kg[s_], Ktok[:, i, :], AF.Identity, scale=egc), r=[tKt, G.T], w=[tKg[s_]])
            cx.op(V, lambda h, pb=pb, s_=s_, d=d, i=i: h.tensor_tensor(out=QKm[d][:, i, :], in0=pb[:, 256:384], in1=ET[s_], op=ALU.mult), r=[tp, tE[s_]], w=[tQK[d][i]])
        for q_ in info:
            s_, d = q_["s_"], q_["d"]
            cx.op(V, lambda h, s_=s_, d=d: h.tensor_tensor(out=ETs[s_], in0=ET[s_], in1=G.mask[:, 4 + d, :], op=ALU.mult), r=[tE[s_], G.tmask], w=[tE[s_]])
        for q_ in info:
            s_, pb, tp, nbc = q_["s_"], q_["pb"], q_["tp"], q_["nbc"]
            cx.op(V, lambda h, pb=pb, s_=s_, nbc=nbc: h.scalar_tensor_tensor(out=Xb[s_][0], in0=pb[:, 128:256], scalar=nbc, in1=ETs[s_], op0=ALU.mult, op1=ALU.mult),
                  r=[tp, tE[s_], G.T], w=[tX[s_]])
        for q_ in info:
            s_, pn, tn = q_["s_"], q_["pb"], q_["tp"]
            cx.op("tensor", lambda h, pn=pn, s_=s_: h.transpose(tview(pn, 384), Xb[s_][0], nid), r=[tX[s_]], w=[tn])
            cx.op(V, lambda h, s_=s_: h.tensor_tensor(out=Pb[s_][0], in0=Xb[s_][0], in1=nid, op=ALU.add), r=[tX[s_]], w=[tXP[s_]])
            cx.op("scalar", lambda h, pn=pn, s_=s_: h.copy(XTb[s_][0], tview(pn, 384)), r=[tn], w=[tX[s_]])
        for L in range(1, 6):
            a, b_ = (L - 1) % 2, L % 2
            for s_, (i, d) in enumerate(grp):
                pn = k.psum[s_]
                tn = k.tpsum[s_]
                if L < 5:
                    cx.op("tensor", lambda h, pn=pn, s_=s_, a=a: h.matmul(pn[:, 0:128], XTb[s_][a], Xb[s_][a], start=True, stop=True), r=[tX[s_]], w=[tn], inc=False)
                cx.op("tensor", lambda h, pn=pn, s_=s_, a=a: h.matmul(pn[:, 128:256], Xb[s_][a], XTb[s_][a], start=True, stop=True), r=[tX[s_]], w=[tn])
                e1, e2 = ("scalar", V) if s_ % 2 == 0 else (V, "scalar")
                if L < 5:
                    if e1 == "scalar":
                        cx.op("scalar", lambda h, pn=pn, s_=s_, b_=b_: h.copy(Xb[s_][b_], pn[:, 0:128]), r=[tn], w=[tX[s_]])
                    else:
                        cx.op(V, lambda h, pn=pn, s_=s_, b_=b_: h.tensor_copy(Xb[s_][b_], pn[:, 0:128]), r=[tn], w=[tX[s_]])
                if e2 == "scalar":
                    cx.op("scalar", lambda h, pn=pn, s_=s_, b_=b_: h.copy(XTb[s_][b_], pn[:, 128:256]), r=[tn], w=[tX[s_]])
                else:
                    cx.op(V, lambda h, pn=pn, s_=s_, b_=b_: h.tensor_copy(XTb[s_][b_], pn[:, 128:256]), r=[tn], w=[tX[s_]])
            for s_, (i, d) in enumerate(grp):
                pn = k.psum[s_]
                tn = k.tpsum[s_]
                cx.op("tensor", lambda h, pn=pn, s_=s_, a=a: h.matmul(pn[:, 256:384], nid, Pb[s_][a], start=True, stop=False), r=[tXP[s_]], w=[tn], inc=False)
                cx.op("tensor", lambda h, pn=pn, s_=s_, a=a, b_=b_: h.matmul(pn[:, 256:384], XTb[s_][b_], Pb[s_][a], start=False, stop=True), r=[tX[s_], tXP[s_]], w=[tn])
                use_act = (s_ + L) % 2 == 0
                if L < 5:
                    if use_act:
                        cx.op("scalar", lambda h, pn=pn, s_=s_, b_=b_: h.copy(Pb[s_][b_], pn[:, 256:384]), r=[tn], w=[tXP[s_]])
                    else:
                        cx.op(V, lambda h, pn=pn, s_=s_, b_=b_: h.tensor_copy(Pb[s_][b_], pn[:, 256:384]), r=[tn], w=[tXP[s_]])
                else:
                    if use_act:
                        cx.op("scalar", lambda h, pn=pn, d=d, i=i: h.copy(Pm[d][:, i, :], pn[:, 256:384]), r=[tn], w=[tPm[d][i], tXP[s_]])
                    else:
                        cx.op(V, lambda h, pn=pn, d=d, i=i: h.tensor_copy(Pm[d][:, i, :], pn[:, 256:384]), r=[tn], w=[tPm[d][i], tXP[s_]])
        for s_, (i, d) in enumerate(grp):
            pn = k.psum[s_]
            tn = k.tpsum[s_]
            tsl = slice(i * 128, (i + 1) * 128)
            cx.op("tensor", lambda h, pn=pn, s_=s_, d=d, i=i: h.matmul(pn[:, 0:128], Kg[s_], Pm[d][:, i, :], start=True, stop=True), r=[tKg[s_], tPm[d][i]], w=[tn], inc=False)
            cx.op("tensor", lambda h, pn=pn, d=d, i=i: h.matmul(pn[:, 128:256], Pm[d][:, i, :], Vtok[:, i, :], start=True, stop=True), r=[tPm[d][i], tVt], w=[tn])
            btc_ = G.beta[d][:, i, hd:hd + 1]
            cx.op(V, lambda h, pn=pn, d=d, tsl=tsl: h.tensor_scalar(WnT[d][:, tsl], pn[:, 0:128], -1.0, None, op0=ALU.mult), r=[tn], w=[tWn[d][i]])
            cx.op("scalar", lambda h, pn=pn, d=d, i=i, btc_=btc_: h.activation(U0b[d][:, i, :], pn[:, 128:256], AF.Identity, scale=btc_), r=[tn, G.T], w=[tU0[d][i]])
    if STOP == "B":
        cx.barrier()
        ar.release(m0)
        return
    ar.release(mN)
    Sf = [[ar.alloc([128], F32) for _ in range(2)] for _ in range(2)]
    Sb = [ar.alloc([128], BF16) for _ in range(2)]
    Rp = [ar.alloc([128], BF16) for _ in range(2)]
    vn = [ar.alloc([128], BF16) for _ in range(2)]
    tS = [Trk(), Trk()]
    tSf = [Trk(), Trk()]
    tR = [Trk(), Trk()]
    tv = [Trk(), Trk()]
    cx.op("gpsimd", lambda h: h.memset(G.osum, 0.0), w=G.tosum)
    for d in range(2):
        cx.op("gpsimd", lambda h, d=d: h.memset(Sf[d][0], 0.0), w=[tS[d]])
        cx.op("gpsimd", lambda h, d=d: h.memset(Sb[d], 0.0), w=[tS[d]])
        cx.op("gpsimd", lambda h, d=d: h.memset(Rp[d], 0.0), w=[tR[d]])
        cx.op("gpsimd", lambda h, d=d: h.memset(vn[d], 0.0), w=[tv[d]])
    for step in range(32):
        for d in range(2):
            if d == 0:
                i, hh = step // 2, step % 2
            else:
                i, hh = 15 - step // 2, 1 - step % 2
            tsl = slice(i * 128, (i + 1) * 128)
            ps_ = slice(hh * 64, (hh + 1) * 64)
            cur, nxt = step % 2, (step + 1) % 2
            pcs = [k.psum[4 * d + q_] for q_ in range(4)]
            tcs = [k.tpsum[4 * d + q_] for q_ in range(4)]
            negc = G.neg[d][ps_, i, hd:hd + 1]
            btc = G.beta[d][ps_, i, hd:hd + 1]
            p1, pv_, po_, pst = pcs
            t1_, tv_, to_, tst = tcs
            cx.op("tensor", lambda h, p1=p1, tsl=tsl, d=d: h.matmul(p1[:, 0:128], WnT[d][:, tsl], Sb[d], start=True, stop=True), r=[tWn[d][i], tS[d]], w=[t1_])
            cx.op(V, lambda h, p1=p1, ps_=ps_, btc=btc, d=d, i=i: h.scalar_tensor_tensor(out=vn[d][ps_, :], in0=p1[ps_, 0:128], scalar=btc, in1=U0b[d][ps_, i, :], op0=ALU.mult, op1=ALU.add),
                  r=[t1_, tU0[d][i], G.T], w=[tv[d]])
            cx.op("tensor", lambda h, po_=po_, tsl=tsl, d=d: h.matmul(po_[:, 0:128], qgT[d][:, tsl], Sb[d], start=True, stop=False), r=[tqg[d][i], tS[d]], w=[to_], inc=False)
            cx.op("tensor", lambda h, po_=po_, ps_=ps_, d=d, i=i: h.matmul(po_[:, 0:128], QKm[d][ps_, i, :], vn[d][ps_, :], start=False, stop=True), r=[tQK[d][i], tv[d]], w=[to_])
            cx.op("tensor", lambda h, pst=pst, ps_=ps_, d=d, i=i: h.matmul(pst[:, 0:128], Kd[d][ps_, i, :], vn[d][ps_, :], start=True, stop=True), r=[tKd[d][i], tv[d]], w=[tst])
            cx.op("gpsimd" if False else V, lambda h, po_=po_, ps_=ps_, i=i: h.tensor_tensor(out=G.osum[ps_, i, :], in0=po_[ps_, 0:128], in1=G.osum[ps_, i, :], op=ALU.add), r=[to_, G.tosum[i]], w=[G.tosum[i]])
            etc = etot[d][:, 2 * i + hh:2 * i + hh + 1]
            cx.op(V, lambda h, pst=pst, d=d, cur=cur, nxt=nxt, etc=etc: h.scalar_tensor_tensor(out=Sf[d][nxt], in0=Sf[d][cur], scalar=etc, in1=pst[:, 0:128], op0=ALU.mult, op1=ALU.add),
                  r=[tst, tet[d][i], tS[d]], w=[tS[d]])
            cx.op("scalar", lambda h, d=d, nxt=nxt: h.copy(Sb[d], Sf[d][nxt]), r=[tS[d]], w=[tS[d]])
    if hd == 0:
        k.dbg_add("gdn_osum", G.osum, G.tosum)
    if STOP == "C":
        cx.barrier()
        ar.release(m0)
        return
    ss = ar.alloc([NT, 2], F32)
    tss = Trk()
    junk = ar.alloc([128], BF16)
    zs = [ar.alloc([128], F32) for _ in range(2)]
    tzs = [Trk(), Trk()]
    yb = [ar.alloc([128], BF16) for _ in range(2)]
    tyb = [Trk(), Trk()]
    for i in range(NT):
        cx.op("scalar", lambda h, i=i: h.activation(junk, G.osum[:, i, :], AF.Square, accum_out=ss[:, i, 0:1]), r=[G.tosum[i], tss], w=[tss])
    cx.op(V, lambda h: h.tensor_scalar(ss[:, :, 1:2], ss[:, :, 0:1], 1.0 / 128, EPS, op0=ALU.mult, op1=ALU.add), r=[tss], w=[tss])
    cx.op("scalar", lambda h: h.activation(ss[:, :, 1:2], ss[:, :, 1:2], AF.Sqrt), r=[tss], w=[tss])
    cx.op(V, lambda h: h.reciprocal(ss[:, :, 1:2], ss[:, :, 1:2]), r=[tss], w=[tss])
    for i in range(NT):
        b = i % 2
        pz = k.psum[b]
        tz = k.tpsum[b]
        for c in range(8):
            cx.op("tensor", lambda h, pz=pz, c=c, i=i: h.matmul(pz[:, 0:128], k.hT[:, c, i * 128:(i + 1) * 128], wz[:, c, :], start=(c == 0), stop=(c == 7)),
                  r=[twz, k.t_hT], w=[tz], inc=(c == 7))
        cx.op("scalar", lambda h, pz=pz, b=b: h.activation(zs[b], pz[:, 0:128], AF.Silu), r=[tz], w=[tzs[b]])
        s1 = ss[:, i, 1:2]
        cx.op(V, lambda h, i=i, s1=s1: h.scalar_tensor_tensor(out=G.osum[:, i, :], in0=G.osum[:, i, :], scalar=s1, in1=G.nw, op0=ALU.mult, op1=ALU.mult), r=[tss, G.tpr, G.tosum[i]], w=[G.tosum[i]])
        cx.op(V, lambda h, i=i, b=b: h.tensor_tensor(out=yb[b], in0=G.osum[:, i, :], in1=zs[b], op=ALU.mult), r=[G.tosum[i], tzs[b]], w=[tyb[b]])
        pt = k.psum[2 + b].bitcast(BF16)
        tt_ = k.tpsum[2 + b]
        cx.op("tensor", lambda h, pt=pt, b=b: h.transpose(pt[:, 0:128], yb[b], k.identb), r=[tyb[b]], w=[tt_])
        cx.op("scalar", lambda h, pt=pt, i=i: h.copy(yT[:, 4 + hd, i * 128:(i + 1) * 128], pt[:, 0:128]), r=[tt_], w=[t_yT])
    cx.barrier()
    ar.release(m0)


def out_proj(k, yT, t_yT):
    cx, ar, D = k.cx, k.ar, k.D
    m0 = ar.mark()
    wo = ar.alloc([8, 1024], BF16)
    two = Trk()
    wsrc = D["w_out"]
    sl_ = cx.fresh('sw')
    for c in range(8):
        cx.dma("gpsimd", wo[:, c, :], wsrc[c * 128:(c + 1) * 128, :], sl_, w=[two])
    for g4 in range(4):
        slx = cx.fresh()
        for i in range(g4 * 4, g4 * 4 + 4):
            cx.dma("sync", k.xacc[:, i, :], D["x"][i * 128:(i + 1) * 128, :], slx, w=[k.txacc[i]])
        for i in range(g4 * 4, g4 * 4 + 4):
            k.txacc[i].w = (slx.key, slx.total)
    for i in range(NT):
        for half in range(2):
            pb = k.psum[(2 * i + half) % 4]
            tp = k.tpsum[(2 * i + half) % 4]
            for c in range(8):
                cx.op("tensor", lambda h, pb=pb, c=c, i=i, half=half: h.matmul(pb[:, :], yT[:, c, i * 128:(i + 1) * 128], wo[:, c, half * 512:(half + 1) * 512], start=(c == 0), stop=(c == 7)),
                      r=[t_yT, two], w=[tp], inc=(c == 7))
            xs = k.xacc[:, i, half * 512:(half + 1) * 512]
            cx.op("vector", lambda h, pb=pb, xs=xs: h.tensor_tensor(out=xs, in0=pb[:, :], in1=xs, op=ALU.add), r=[tp, k.txacc[i]], w=[k.txacc[i]])
    cx.barrier()
    ar.release(m0)


def xattn(k):
    cx, ar, D = k.cx, k.ar, k.D
    V = "vector"
    m0 = ar.mark()
    xnT = ar.alloc([8, 2048], BF16)
    t_xnT = Trk()
    memT = ar.alloc([8, 256], BF16)
    t_memT = Trk()
    m1 = ar.mark()
    mt = [ar.alloc([1024], F32) for _ in range(2)]
    tmt = [Trk(), Trk()]

    def src_mem(i):
        cx.dma("sync", mt[i], D["mem"][i * 128:(i + 1) * 128, :], cx.fresh(), w=[tmt[i]])
        return mt[i], tmt[i]
    norm_transpose(k, "mem", src_mem, 2, D["norm_mem"], memT, BF16, t_memT)
    ar.release(m1)
    norm_transpose(k, "xa", lambda i: (k.xacc[:, i, :], k.txacc[i]), NT, D["norm_xattn"], xnT, BF16, t_xnT, resident=True)
    k.dbg_add("xa_memT", memT, [t_memT])
    k.dbg_add("xa_xnT", xnT, [t_xnT])
    wq = [ar.alloc([8, 256], BF16) for _ in range(2)]
    wk = [ar.alloc([8, 256], BF16) for _ in range(2)]
    wv = [ar.alloc([8, 256], BF16) for _ in range(2)]
    wo = [ar.alloc([2, 1024], BF16) for _ in range(2)]
    twA = [Trk(), Trk()]
    two_ = [Trk(), Trk()]
    swA = [cx.slot("xwa0"), cx.slot("xwa1")]
    swO = [cx.slot("xwo0"), cx.slot("xwo1")]
    kTb = [ar.alloc([2, 256], BF16) for _ in range(2)]
    vhb = [ar.alloc([2, 256], BF16) for _ in range(2)]
    tkvb = [Trk(), Trk()]
    qTb = [ar.alloc([2, 2048], BF16) for _ in range(2)]
    tqTb = [Trk(), Trk()]
    E = [ar.alloc([2, 512], BF16) for _ in range(2)]
    tE = [Trk(), Trk()]
    rden = [ar.alloc([512], F32) for _ in range(2)]
    trd = [Trk(), Trk()]
    oTn = [ar.alloc([2, 512], BF16) for _ in range(2)]
    toT = [Trk(), Trk()]
    cx.barrier()
    k.txa = [[Trk(), Trk()] for _ in range(NT)]

    def loadA(hd):
        b = hd % 2
        c0 = hd * 256
        for (dst, nm) in ((wq[b], "xa_wq"), (wk[b], "xa_wk"), (wv[b], "xa_wv")):
            load_w_cols(k, D[nm], c0, 256, dst, twA[b], swA[b])

    def loadO(hd):
        b = hd % 2
        c0 = hd * 256
        src = D["xa_wo"]
        cx.dma("gpsimd", wo[b], bass.AP(src.tensor, src.offset + c0 * 1024, [[1024, 128], [128 * 1024, 2], [1, 1024]]), swO[b], w=[two_[b]])

    def proj(hd):
        b = hd % 2
        kT, vh, qT, tkv, tqT = kTb[b], vhb[b], qTb[b], tkvb[b], tqTb[b]
        for dc in range(2):
            pb = k.psum[dc]
            tp = k.tpsum[dc]
            for c in range(8):
                cx.op("tensor", lambda h, pb=pb, c=c, dc=dc, b=b: h.matmul(pb[:, 0:256], wk[b][:, c, dc * 128:(dc + 1) * 128], memT[:, c, :], start=(c == 0), stop=(c == 7)),
                      r=[twA[b], t_memT], w=[tp], inc=(c == 7))
            cx.op("scalar", lambda h, pb=pb, dc=dc, kT=kT: h.copy(kT[:, dc, :], pb[:, 0:256]), r=[tp], w=[tkv])
        for mtile in range(2):
            pb = k.psum[2 + mtile]
            tp = k.tpsum[2 + mtile]
            for c in range(8):
                cx.op("tensor", lambda h, pb=pb, c=c, mtile=mtile, b=b: h.matmul(pb[:, 0:256], memT[:, c, mtile * 128:(mtile + 1) * 128], wv[b][:, c, :], start=(c == 0), stop=(c == 7)),
                      r=[twA[b], t_memT], w=[tp], inc=(c == 7))
            cx.op(V, lambda h, pb=pb, mtile=mtile, vh=vh: h.tensor_copy(vh[:, mtile, :], pb[:, 0:256]), r=[tp], w=[tkv])
        for dc in range(2):
            for n in range(4):
                pb = k.psum[(dc * 4 + n) % 4]
                tp = k.tpsum[(dc * 4 + n) % 4]
                for c in range(8):
                    cx.op("tensor", lambda h, pb=pb, c=c, dc=dc, n=n, b=b: h.matmul(pb[:, :], wq[b][:, c, dc * 128:(dc + 1) * 128], xnT[:, c, n * 512:(n + 1) * 512], start=(c == 0), stop=(c == 7)),
                          r=[twA[b], t_xnT], w=[tp], inc=(c == 7))
                if n % 2 == 0:
                    cx.op("scalar", lambda h, pb=pb, dc=dc, n=n, qT=qT: h.copy(qT[:, dc, n * 512:(n + 1) * 512], pb[:, :]), r=[tp], w=[tqT])
                else:
                    cx.op(V, lambda h, pb=pb, dc=dc, n=n, qT=qT: h.tensor_copy(qT[:, dc, n * 512:(n + 1) * 512], pb[:, :]), r=[tp], w=[tqT])

    def chunks(hd):
        b = hd % 2
        kT, vh, qT, tkv, tqT = kTb[b], vhb[b], qTb[b], tkvb[b], tqTb[b]
        tw = [two_[0], two_[1]]

        def emit_scores(n):
            eb = n % 2
            ts = slice(n * 512, (n + 1) * 512)
            for mtile in range(2):
                pb = k.psum[mtile]
                tp = k.tpsum[mtile]
                for dc in range(2):
                    cx.op("tensor", lambda h, pb=pb, dc=dc, mtile=mtile, ts=ts: h.matmul(pb[:, :], kT[:, dc, mtile * 128:(mtile + 1) * 128], qT[:, dc, ts], start=(dc == 0), stop=(dc == 1)),
                          r=[tkv, tqT], w=[tp], inc=(dc == 1))
                cx.op("scalar", lambda h, pb=pb, mtile=mtile, eb=eb: h.activation(E[eb][:, mtile, :], pb[:, :], AF.Exp, scale=1.0 / 16.0), r=[tp], w=[tE[eb]])

        def emit_rest(n):
            eb = n % 2
            pd = k.psum[2]
            tpd = k.tpsum[2]
            for mtile in range(2):
                cx.op("tensor", lambda h, pd=pd, mtile=mtile, eb=eb: h.matmul(pd[:, :], k.onesb, E[eb][:, mtile, :], start=(mtile == 0), stop=(mtile == 1)), r=[tE[eb]], w=[tpd], inc=(mtile == 1))
            cx.op(V, lambda h, pd=pd, eb=eb: h.reciprocal(rden[eb], pd[:, :]), r=[tpd], w=[trd[eb]])
            for dc in range(2):
                po = k.psum[3 + dc]
                tpo = k.tpsum[3 + dc]
                for mtile in range(2):
                    cx.op("tensor", lambda h, po=po, mtile=mtile, dc=dc, eb=eb: h.matmul(po[:, :], vh[:, mtile, dc * 128:(dc + 1) * 128], E[eb][:, mtile, :], start=(mtile == 0), stop=(mtile == 1)),
                          r=[tkv, tE[eb]], w=[tpo], inc=(mtile == 1))
                cx.op(V, lambda h, po=po, dc=dc, eb=eb: h.tensor_tensor(out=oTn[eb][:, dc, :], in0=po[:, :], in1=rden[eb], op=ALU.mult), r=[tpo, trd[eb]], w=[toT[eb]])
            for t in range(4):
                i = n * 4 + t
                for half in range(2):
                    pw_ = k.psum[5 + (t * 2 + half) % 3]
                    tpw = k.tpsum[5 + (t * 2 + half) % 3]
                    for dc in range(2):
                        cx.op("tensor", lambda h, pw_=pw_, dc=dc, t=t, half=half, b=b, eb=eb: h.matmul(pw_[:, :], oTn[eb][:, dc, t * 128:(t + 1) * 128], wo[b][:, dc, half * 512:(half + 1) * 512], start=(dc == 0), stop=(dc == 1)),
                              r=[toT[eb], tw[b]], w=[tpw], inc=(dc == 1))
                    xs = k.xacc[:, i, half * 512:(half + 1) * 512]
                    cx.op(V, lambda h, pw_=pw_, xs=xs: h.tensor_tensor(out=xs, in0=pw_[:, :], in1=xs, op=ALU.add), r=[tpw, k.txa[i][half]], w=[k.txa[i][half]])
        emit_scores(0)
        for n in range(4):
            if n + 1 < 4:
                emit_scores(n + 1)
            emit_rest(n)
    loadA(0)
    loadA(1)
    loadO(0)
    loadO(1)
    proj(0)
    for hd in range(4):
        if hd + 1 < 4:
            proj(hd + 1)
        if hd + 2 < 4:
            loadA(hd + 2)
        chunks(hd)
        if hd + 2 < 4:
            loadO(hd + 2)
    cx.barrier()
    ar.release(m0)


def moe(k):
    cx, ar, D = k.cx, k.ar, k.D
    V = "vector"
    m0 = ar.mark()
    xnT = ar.alloc([8, 2048], BF16)
    t_xnT = Trk()
    norm_transpose(k, "moe", lambda i: (k.xacc[:, i, :], k.txacc[i]), NT, D["norm_moe"], xnT, BF16, t_xnT, resident=True)
    wr = ar.alloc([8, 36], BF16)
    twr = Trk()
    sl_ = cx.fresh('sw')
    srcg, srce = D["router_group_w"], D["router_expert_w"]
    cx.dma("gpsimd", wr[:, :, 0:4], bass.AP(srcg.tensor, srcg.offset, [[4, 128], [4 * 128, 8], [1, 4]]), sl_, w=[twr])
    cx.dma("gpsimd", wr[:, :, 4:36], bass.AP(srce.tensor, srce.offset, [[32, 128], [32 * 128, 8], [1, 32]]), sl_, w=[twr])
    rb = ar.alloc([36], F32)
    trb = Trk()
    sl2 = cx.fresh()
    cx.dma("sync", rb[:, 0:4], dram_bcast(D["router_group_b"], 128, 4), sl2, w=[trb])
    cx.dma("sync", rb[:, 4:36], dram_bcast(D["router_expert_b"], 128, 32), sl2, w=[trb])
    cw = ar.alloc([NT, 32], F32)
    tcw = Trk()
    lgA = ar.alloc([NT, 36], F32)
    msk = ar.alloc([NT, 32], F32)
    eq2 = ar.alloc([NT, 32], F32)
    m8 = ar.alloc([NT, 8], F32)
    sc = ar.alloc([8, NT], F32)
    oh = ar.alloc([NT, 4], F32)
    ex = ar.alloc([NT, 4], F32)
    T = Trk()
    rbb = rb.unsqueeze(1).to_broadcast([128, 8, 36])
    for half in range(2):
        pb = k.psum[half]
        tp = k.tpsum[half]
        for ii in range(8):
            i = half * 8 + ii
            for c in range(8):
                cx.op("tensor", lambda h, pb=pb, c=c, i=i, ii=ii: h.matmul(pb[:, ii * 36:(ii + 1) * 36], xnT[:, c, i * 128:(i + 1) * 128], wr[:, c, :], start=(c == 0), stop=(c == 7)),
                      r=[t_xnT, twr], w=[tp], inc=(c == 7))
        cx.op(V, lambda h, pb=pb, half=half: h.tensor_tensor(out=lgA[:, half * 8:(half + 1) * 8, :], in0=pb[:, 0:288].rearrange("p (t e) -> p t e", t=8), in1=rbb, op=ALU.add), r=[tp, trb, T], w=[T])
    lg_g = lgA[:, :, 0:4]
    lg_e = lgA[:, :, 4:36]
    gmax, ngs, ssum, ptop, dm, w1, w2 = [sc[:, j_, :] for j_ in range(7)]

    def vop(fn):
        cx.op(V, fn, r=[T], w=[T])

    def aop(fn):
        cx.op("scalar", fn, r=[T], w=[T])
    b4 = lambda v: v.unsqueeze(2).to_broadcast([128, NT, 4])
    b32 = lambda v: v.unsqueeze(2).to_broadcast([128, NT, 32])
    vop(lambda h: h.tensor_reduce(out=gmax, in_=lg_g, axis=AX.X, op=ALU.max))
    vop(lambda h: h.tensor_tensor(out=oh, in0=lg_g, in1=b4(gmax), op=ALU.is_equal))
    vop(lambda h: h.tensor_tensor(out=ex, in0=lg_g, in1=b4(gmax), op=ALU.subtract))
    aop(lambda h: h.activation(ex, ex, AF.Exp))
    vop(lambda h: h.tensor_reduce(out=ssum, in_=ex, axis=AX.X, op=ALU.add))
    vop(lambda h: h.reciprocal(ptop, ssum))
    vop(lambda h: h.tensor_scalar(oh, oh, -1.0, 1e30, op0=ALU.add, op1=ALU.mult))
    vop(lambda h: h.tensor_tensor(out=msk.rearrange("p t (g e) -> p t g e", g=4), in0=lg_e.rearrange("p t (g e) -> p t g e", g=4),
                                  in1=oh.unsqueeze(3).to_broadcast([128, NT, 4, 8]), op=ALU.add))
    for i in range(NT):
        vop(lambda h, i=i: h.max(out=m8[:, i, :], in_=msk[:, i, :]))
    m1, m2 = m8[:, :, 0], m8[:, :, 1]
    vop(lambda h: h.tensor_tensor(out=dm, in0=m2, in1=m1, op=ALU.subtract))
    aop(lambda h: h.activation(dm, dm, AF.Exp))
    vop(lambda h: h.tensor_scalar(w1, dm, 1.0, None, op0=ALU.add))
    vop(lambda h: h.reciprocal(w1, w1))
    vop(lambda h: h.tensor_tensor(out=w2, in0=dm, in1=w1, op=ALU.mult))
    vop(lambda h: h.tensor_tensor(out=w1, in0=w1, in1=ptop, op=ALU.mult))
    vop(lambda h: h.tensor_tensor(out=w2, in0=w2, in1=ptop, op=ALU.mult))
    vop(lambda h: h.tensor_tensor(out=eq2, in0=msk, in1=b32(m2), op=ALU.is_equal))
    vop(lambda h: h.tensor_tensor(out=eq2, in0=eq2, in1=b32(w2), op=ALU.mult))
    vop(lambda h: h.tensor_tensor(out=msk, in0=msk, in1=b32(m1), op=ALU.is_equal))
    vop(lambda h: h.tensor_tensor(out=msk, in0=msk, in1=b32(w1), op=ALU.mult))
    cx.op(V, lambda h: h.tensor_tensor(out=cw, in0=msk, in1=eq2, op=ALU.add), r=[T], w=[tcw, T])
    k.dbg_add("moe_cw", cw, [tcw])
    wgu = [ar.alloc([8, 512], BF16) for _ in range(2)]
    wd = [ar.alloc([2, 1024], BF16) for _ in range(2)]
    twe = [Trk(), Trk()]
    swe = [cx.slot("we0"), cx.slot("we1")]
    sg = [ar.alloc([512], F32) for _ in range(2)]
    tsg = [Trk(), Trk()]
    h1 = [ar.alloc([2, 512], BF16) for _ in range(2)]
    th1 = [Trk(), Trk()]
    NE = k.n_experts

    def load_e(e):
        b = e % 2
        g_, u_, d_ = D["moe_w_gate"], D["moe_w_up"], D["moe_w_down"]
        cx.dma("gpsimd", wgu[b][:, :, 0:256], bass.AP(g_.tensor, g_.offset + e * 1024 * 256, [[256, 128], [256 * 128, 8], [1, 256]]), swe[b], w=[twe[b]])
        cx.dma("gpsimd", wgu[b][:, :, 256:512], bass.AP(u_.tensor, u_.offset + e * 1024 * 256, [[256, 128], [256 * 128, 8], [1, 256]]), swe[b], w=[twe[b]])
        cx.dma("gpsimd", wd[b], bass.AP(d_.tensor, d_.offset + e * 256 * 1024, [[1024, 128], [1024 * 128, 2], [1, 1024]]), swe[b], w=[twe[b]])
    import os
    NOLOAD = os.environ.get("MOE_NOLOAD", "") == "1"
    load_e(0)
    if NE > 1:
        load_e(1)
    jobs = [(e, n) for e in range(NE) for n in range(4)]
    state = {"cnt": 0, "loaded": 0}
    cx.barrier()
    k.txh = [[Trk(), Trk()] for _ in range(NT)]

    def emit_gu(j, fh):
        e, n = jobs[j]
        b = e % 2
        ts = slice(n * 512, (n + 1) * 512)
        hb = j % 2
        pg = k.psum[fh * 2]
        tpg = k.tpsum[fh * 2]
        pu = k.psum[fh * 2 + 1]
        tpu = k.tpsum[fh * 2 + 1]
        for c in range(8):
            cx.op("tensor", lambda h, pg=pg, c=c, fh=fh, ts=ts, b=b: h.matmul(pg[:, :], wgu[b][:, c, fh * 128:(fh + 1) * 128], xnT[:, c, ts], start=(c == 0), stop=(c == 7)),
                  r=[twe[b], t_xnT], w=[tpg], inc=(c == 7))
        for c in range(8):
            cx.op("tensor", lambda h, pu=pu, c=c, fh=fh, ts=ts, b=b: h.matmul(pu[:, :], wgu[b][:, c, 256 + fh * 128:256 + (fh + 1) * 128], xnT[:, c, ts], start=(c == 0), stop=(c == 7)),
                  r=[twe[b], t_xnT], w=[tpu], inc=(c == 7))
        cx.op("scalar", lambda h, pg=pg, fh=fh: h.activation(sg[fh], pg[:, :], AF.Silu), r=[tpg], w=[tsg[fh]])
        cx.op(V, lambda h, pu=pu, fh=fh, hb=hb: h.tensor_tensor(out=h1[hb][:, fh, :], in0=pu[:, :], in1=sg[fh], op=ALU.mult), r=[tpu, tsg[fh]], w=[th1[hb]])

    def emit_down(j):
        e, n = jobs[j]
        b = e % 2
        hb = j % 2
        for t in range(4):
            i = n * 4 + t
            for half in range(2):
                pdn = k.psum[4 + state["cnt"] % 4]
                tpd = k.tpsum[4 + state["cnt"] % 4]
                state["cnt"] += 1
                for fh in range(2):
                    cx.op("tensor", lambda h, pdn=pdn, fh=fh, t=t, half=half, hb=hb, b=b: h.matmul(pdn[:, :], h1[hb][:, fh, t * 128:(t + 1) * 128], wd[b][:, fh, half * 512:(half + 1) * 512], start=(fh == 0), stop=(fh == 1)),
                          r=[th1[hb], twe[b]], w=[tpd], inc=(fh == 1))
                xs = k.xacc[:, i, half * 512:(half + 1) * 512]
                cwc = cw[:, i, e:e + 1]
                cx.op(V, lambda h, pdn=pdn, xs=xs, cwc=cwc: h.scalar_tensor_tensor(out=xs, in0=pdn[:, :], scalar=cwc, in1=xs, op0=ALU.mult, op1=ALU.add), r=[tpd, tcw, k.txh[i][half]], w=[k.txh[i][half]])
        if n == 3 and e + 2 < NE and not NOLOAD:
            load_e(e + 2)
    nj = len(jobs)
    if nj > 0:
        emit_gu(0, 0)
        emit_gu(0, 1)
        for j in range(nj):
            if j + 1 < nj:
                emit_gu(j + 1, 0)
            emit_down(j)
            if j + 1 < nj:
                emit_gu(j + 1, 1)
    cx.barrier()
    ar.release(m0)


def final_norm(k, out):
    cx, ar, D = k.cx, k.ar, k.D
    V = "vector"
    m0 = ar.mark()
    gB = ar.alloc([1024], F32)
    tg = Trk()
    cx.dma("sync", gB, dram_bcast(D["norm_final"], 128, 1024), cx.fresh(), w=[tg])
    junk = ar.alloc([1024], BF16)
    tj = Trk()
    ss = ar.alloc([NT, 2], F32)
    tss = Trk()
    ob = [ar.alloc([1024], F32) for _ in range(2)]
    tob = [Trk(), Trk()]
    so = [cx.slot("o0"), cx.slot("o1")]
    for i in range(NT):
        txs = [k.txacc[i]] + (k.txh[i] if hasattr(k, "txh") else [])
        cx.op("scalar", lambda h, i=i: h.activation(junk, k.xacc[:, i, :], AF.Square, accum_out=ss[:, i, 0:1]), r=txs + [tss], w=[tj, tss])
    cx.op(V, lambda h: h.tensor_scalar(ss[:, :, 1:2], ss[:, :, 0:1], 1.0 / 1024, EPS, op0=ALU.mult, op1=ALU.add), r=[tss], w=[tss])
    cx.op("scalar", lambda h: h.activation(ss[:, :, 1:2], ss[:, :, 1:2], AF.Sqrt), r=[tss], w=[tss])
    cx.op(V, lambda h: h.reciprocal(ss[:, :, 1:2], ss[:, :, 1:2]), r=[tss], w=[tss])
    for i in range(NT):
        b = i % 2
        s1 = ss[:, i, 1:2]
        txs = [k.txacc[i]] + (k.txh[i] if hasattr(k, "txh") else [])
        cx.op(V, lambda h, i=i, s1=s1, b=b: h.scalar_tensor_tensor(out=ob[b], in0=k.xacc[:, i, :], scalar=s1, in1=gB, op0=ALU.mult, op1=ALU.mult), r=txs + [tss, tg], w=[tob[b]])
        cx.dma("sync", out[i * 128:(i + 1) * 128, :], ob[b], so[b], r=[tob[b]])
    cx.barrier()
    ar.release(m0)


_CACHE = {}


def kernel(**inputs):
    inp = {k_: np.asarray(v) for k_, v in inputs.items()}
    n = inp["x"].shape[0]
    maps = [host_inputs(inp, b) for b in range(n)]
    key = "full"
    if key not in _CACHE:
        shapes = {k_: (v.shape, np2dt(v)) for k_, v in maps[0].items()}
        _CACHE[key] = build(shapes)[0]
    nc = _CACHE[key]
    res = run_bass_kernel_spmd(nc, maps, core_ids=list(range(n)))
    return np.stack([np.asarray(r["out"], dtype=np.float32) for r in res.results], 0)
```

```python
import contextlib
import os
import math
import numpy as np
import ml_dtypes
import concourse.bass as bass
import concourse.mybir as mybir
from concourse.bass_utils import run_bass_kernel_spmd

F32 = mybir.dt.float32
BF16 = mybir.dt.bfloat16
F32R = mybir.dt.float32r
I32 = mybir.dt.int32
AF = mybir.ActivationFunctionType
ALU = mybir.AluOpType
AX = mybir.AxisListType

ENGS = ("sync", "scalar", "gpsimd", "vector", "tensor")
ATTACH_WAIT = os.environ.get("ATTACH_WAIT", "1") == "1"
S = 2048
DM = 1024
NT = 16
EPS = 1e-6


class Trk:
    __slots__ = ("name", "w", "r", "excl")

    def __init__(self, name="", excl=False):
        self.name = name
        self.w = None
        self.r = {}
        self.excl = excl


class DmaSlot:
    def __init__(self, ctx, name):
        self.key = "d_" + name + str(ctx.nsem)
        ctx.sems[self.key] = ctx.new_sem(self.key)
        self.total = 0


class Ctx:
    def __init__(self, nc, stack):
        self.nc = nc
        self.stack = stack
        self.q = {e: [] for e in ENGS}
        self.sems = {}
        self.nsem = 0
        self.cnt = {e: 0 for e in ENGS}
        self.known = {e: {} for e in ENGS}
        for e in ENGS:
            self.sems[e] = self.new_sem("s_" + e)
        self.slots = []
        self.pools = {}
        self.pool_idx = {}
        self.n_ops = 0

    def new_sem(self, name):
        self.nsem += 1
        return self.stack.enter_context(self.nc.semaphore(name))

    def slot(self, name):
        s = DmaSlot(self, name)
        self.slots.append(s)
        return s

    def fresh(self, kind="hw"):
        pool = self.pools.setdefault(kind, [])
        i = self.pool_idx.get(kind, 0)
        if i >= len(pool):
            assert len(pool) < 30, "slot pool exhausted"
            pool.append(self.slot(kind + "%d" % len(pool)))
            pool[-1].kind = kind
        self.pool_idx[kind] = i + 1
        return pool[i]

    def sb(self, name, shape, dt):
        return self.stack.enter_context(self.nc.sbuf_tensor("sb_" + name, list(shape), dt))

    def ps(self, name, shape, dt=F32):
        return self.stack.enter_context(self.nc.psum_tensor(name, list(shape), dt))

    def _waits_for(self, eng, r, w, extra=()):
        need = {}

        def req(dep, raw=True):
            if dep is None:
                return
            k, c = dep
            if k == eng and eng in ("tensor", "sync"):
                return
            if k == eng and not raw:
                return
            if c > need.get(k, 0):
                need[k] = c
        for t in r:
            req(t.w)
        for t in w:
            req(t.w, raw=False)
            for k, c in t.r.items():
                req((k, c), raw=False)
        for d in extra:
            req(d)
        out = []
        kn = self.known[eng]
        for k, c in need.items():
            if kn.get(k, 0) < c:
                kn[k] = c
                out.append((self.sems[k], c))
        return out

    def op(self, eng, fn, r=(), w=(), inc=True, extra=()):
        w = list(w) + [t for t in r if t.excl]
        r = [t for t in r if not t.excl]
        waits = self._waits_for(eng, r, w, extra)
        c = self.cnt[eng] + 1
        if inc:
            self.cnt[eng] = c
        sem = self.sems[eng]

        def emit(h, fn=fn, waits=waits, inc=inc, sem=sem):
            for s, v in waits[:-1]:
                h.wait_ge(s, v)
            ins = fn(h)
            if waits:
                if ATTACH_WAIT:
                    ins._wait_ge(waits[-1][0], waits[-1][1])
                else:
                    raise RuntimeError
            if inc:
                ins.then_inc(sem, 1)
        if not ATTACH_WAIT:
            def emit(h, fn=fn, waits=waits, inc=inc, sem=sem):
                for s, v in waits:
                    h.wait_ge(s, v)
                ins = fn(h)
                if inc:
                    ins.then_inc(sem, 1)
        self.q[eng].append(emit)
        for t in r:
            t.r[eng] = c
        for t in w:
            t.w = (eng, c)
            t.r = {}
        self.n_ops += 1

    def dma(self, eng, out, in_, slot, r=(), w=(), extra=(), **kw):
        kind = "sw" if eng == "gpsimd" else "hw"
        assert getattr(slot, "kind", kind) == kind, ("DMA slot kind mismatch", slot.key, eng)
        slot.kind = kind
        waits = self._waits_for(eng, r, w, extra)
        slot.total += 16
        sem = self.sems[slot.key]

        def emit(h, waits=waits, sem=sem, out=out, in_=in_, kw=kw):
            for s, v in waits:
                h.wait_ge(s, v)
            h.dma_start(out=out, in_=in_, **kw).then_inc(sem, 16)
        self.q[eng].append(emit)
        dep = (slot.key, slot.total)
        for t in r:
            t.r[slot.key] = slot.total
        for t in w:
            t.w = dep
            t.r = {}
        self.n_ops += 1
        return dep

    def wait_deps(self, eng, deps):
        waits = self._waits_for(eng, (), (), deps)

        def emit(h, waits=waits):
            for s, v in waits:
                h.wait_ge(s, v)
        self.q[eng].append(emit)

    def barrier(self):
        deps = [(e, self.cnt[e]) for e in ENGS if e != "sync" and self.cnt[e] > 0]
        deps += [(s.key, s.total) for s in self.slots if s.total > 0]
        for e in ENGS:
            self.wait_deps(e, deps)
        self.pool_idx = {}

    def emit_all(self, block):
        q = self.q

        @block.sync
        def _(h):
            for f in q["sync"]:
                f(h)

        @block.scalar
        def _(h):
            for f in q["scalar"]:
                f(h)

        @block.gpsimd
        def _(h):
            for f in q["gpsimd"]:
                f(h)

        @block.vector
        def _(h):
            for f in q["vector"]:
                f(h)

        @block.tensor
        def _(h):
            for f in q["tensor"]:
                f(h)


class Arena:
    def __init__(self, cx, words, base=None):
        self.t = cx.sb("arena", [128, words], F32) if base is None else base
        self.cx = cx
        self.words = words
        self.top = 0

    def mark(self):
        return self.top

    def release(self, m):
        if m != self.top:
            self.cx.barrier()
        self.top = m

    def alloc(self, shape, dt):
        n = int(np.prod(shape))
        w = n if dt in (F32, F32R, I32) else (n + 1) // 2
        w = (w + 1) // 2 * 2
        o = self.top
        self.top += w
        assert self.top <= self.words, ("arena overflow", self.top, self.words)
        v = self.t[:, o:o + w]
        if dt != F32:
            v = v.bitcast(dt)
        v = v[:, 0:n]
        if len(shape) > 1:
            names = " ".join("d%d" % i for i in range(len(shape)))
            v = v.rearrange("p (%s) -> p %s" % (names, names), **{"d%d" % i: shape[i] for i in range(len(shape))})
        return v


def pap(ap, part0, nparts, off, dims):
    base = ap.ap[0][0]
    return bass.AP(ap.tensor, ap.offset + part0 * base + off, [[base, nparts]] + [list(d) for d in dims])


def host_consts():
    c = {}
    c["ident"] = np.eye(128, dtype=np.float32)
    c["identb"] = np.eye(128, dtype=np.float32).astype(ml_dtypes.bfloat16)
    c["ones"] = np.ones((128, 128), np.float32)
    selT = np.zeros((128, 2, 8, 128), np.float32)
    selB = np.zeros((128, 2, 8, 128), np.float32)
    for q in range(4):
        for r in range(32):
            loc, cc = r // 16, r % 16
            for s in range(8):
                selT[q * 32 + r, loc, s, s * 16 + cc] = 1.0
                selB[q * 32 + r, loc, s, s * 16 + cc] = 1.0
    c["selT"] = selT.astype(ml_dtypes.bfloat16)
    c["selB"] = selB.astype(ml_dtypes.bfloat16)
    sidx = np.arange(128) // 16
    c["s5mf"] = (sidx[None, :] >= sidx[:, None]).astype(np.float32)
    c["s5mb"] = (sidx[None, :] <= sidx[:, None]).astype(np.float32)
    c["kvec"] = np.tile((np.arange(16, dtype=np.float32) - 7.0)[None, :], (128, 1))
    k = np.arange(128)[:, None]
    cc = np.arange(128)[None, :]
    same = (k // 64) == (cc // 64)
    gm = np.zeros((128, 8, 128), np.float32)
    gm[:, 0] = same & (k <= cc)
    gm[:, 1] = same & (k >= cc)
    gm[:, 2] = np.where(same & (cc >= k), 0.0, -30000.0)
    gm[:, 3] = np.where(same & (cc <= k), 0.0, -30000.0)
    gm[:, 4] = same & (cc > k)
    gm[:, 5] = same & (cc < k)
    gm[:, 6] = same
    c["gmask"] = gm
    return c


def host_s5(inp):
    o = {}

    pairs = {"lam_re": ("s5_lam_re_f", "s5_lam_re_b"), "lam_im": ("s5_lam_im_f", "s5_lam_im_b"),
             "log_step": ("s5_log_step_f", "s5_log_step_b"), "b_re": ("s5_b_re_f", "s5_b_re_b"),
             "b_im": ("s5_b_im_f", "s5_b_im_b"), "c_re": ("s5_c_re_f", "s5_c_re_b"), "c_im": ("s5_c_im_f", "s5_c_im_b")}

    def st(nm):
        f_, b_ = pairs[nm]
        return np.stack([inp[f_][0], inp[b_][0]], 0)
    lam = np.stack([st("lam_re"), st("lam_im")], 0)
    lam = lam.reshape(2, 2, 2, 16, 64).transpose(2, 4, 0, 1, 3)
    o["s5_lam"] = np.ascontiguousarray(lam.reshape(128, 2, 32))
    ls = st("log_step").reshape(2, 2, 16)
    ls = np.broadcast_to(ls.transpose(1, 0, 2)[:, None], (2, 64, 2, 16))
    o["s5_step"] = np.ascontiguousarray(ls.reshape(128, 32))
    b = np.stack([st("b_re"), st("b_im")], 0)
    b = b.reshape(2, 2, 2, 16, 64, 16).transpose(2, 4, 0, 1, 3, 5)
    o["s5_b"] = np.ascontiguousarray(b.reshape(128, 2, 512))
    cm = np.stack([st("c_re"), st("c_im")], 0)
    cm = cm.reshape(2, 2, 2, 16, 16, 64).transpose(2, 5, 0, 1, 3, 4)
    o["s5_c"] = np.ascontiguousarray(cm.reshape(128, 2, 512))
    d = inp["s5_d"][0].reshape(32, 16)
    o["s5_dvec"] = np.ascontiguousarray(np.broadcast_to(d.T[None], (8, 16, 32)).reshape(128, 32))
    o["s5_bglu"] = np.ascontiguousarray(inp["s5_b_glu"][0].reshape(4, 128).T)
    o["s5_normw"] = np.ascontiguousarray(inp["s5_norm"][0].reshape(4, 128).T)
    return o


def np2dt(a):
    if a.dtype == np.float32:
        return F32
    if a.dtype == ml_dtypes.bfloat16:
        return BF16
    raise ValueError(a.dtype)


class K:
    pass


def dram_bcast(ap, nparts, n, off=0):
    return bass.AP(ap.tensor, ap.offset + off, [[0, nparts], [1, n]])


def norm_transpose(k, name, src_fn, ntiles, gain_dram, outT, out_dt, outT_trk, resident=False):
    cx, ar = k.cx, k.ar
    m = ar.mark()
    gB = ar.alloc([1024], F32)
    tg = Trk()
    cx.dma("sync", gB, dram_bcast(gain_dram, 128, 1024), cx.fresh(), w=[tg])
    junk = ar.alloc([1024], BF16)
    tj = Trk()
    xn = [ar.alloc([1024], out_dt) for _ in range(2)]
    txn = [Trk(), Trk()]
    ss = ar.alloc([NT * 2, 1], F32)
    tss = [Trk() for _ in range(ntiles)]
    pdt = BF16 if out_dt == BF16 else F32
    ident = k.identb if out_dt == BF16 else k.ident
    srcs = []
    if resident:
        for i in range(ntiles):
            src, ts = src_fn(i)
            srcs.append((src, ts))
            cx.op("scalar", lambda h, src=src, i=i: h.activation(junk, src, AF.Square, accum_out=ss[:, 2 * i:2 * i + 1]), r=[ts], w=[tj, tss[0]])
        ssv = ss.rearrange("p (t two) one -> p t (two one)", two=2)
        cx.op("vector", lambda h: h.tensor_scalar(ssv[:, 0:ntiles, 1:2], ssv[:, 0:ntiles, 0:1], 1.0 / 1024, EPS, op0=ALU.mult, op1=ALU.add), r=[tss[0]], w=[tss[0]])
        cx.op("scalar", lambda h: h.activation(ssv[:, 0:ntiles, 1:2], ssv[:, 0:ntiles, 1:2], AF.Sqrt), r=[tss[0]], w=[tss[0]])
        cx.op("vector", lambda h: h.reciprocal(ssv[:, 0:ntiles, 1:2], ssv[:, 0:ntiles, 1:2]), r=[tss[0]], w=[tss[0]])
    for i in range(ntiles):
        rsi = ss[:, 2 * i + 1:2 * i + 2]
        if resident:
            src, ts = srcs[i]
            tsi = tss[0]
        else:
            src, ts = src_fn(i)
            ssi = ss[:, 2 * i:2 * i + 1]
            tsi = tss[i]
            cx.op("scalar", lambda h, src=src, ssi=ssi: h.activation(junk, src, AF.Square, accum_out=ssi), r=[ts], w=[tj, tss[i]])
            cx.op("vector", lambda h, ssi=ssi, rsi=rsi: h.tensor_scalar(rsi, ssi, 1.0 / 1024, EPS, op0=ALU.mult, op1=ALU.add), r=[tss[i]], w=[tss[i]])
            cx.op("scalar", lambda h, rsi=rsi: h.activation(rsi, rsi, AF.Sqrt), r=[tss[i]], w=[tss[i]])
            cx.op("vector", lambda h, rsi=rsi: h.reciprocal(rsi, rsi), r=[tss[i]], w=[tss[i]])
        b = i % 2
        cx.op("vector", lambda h, src=src, rsi=rsi, b=b: h.scalar_tensor_tensor(out=xn[b], in0=src, scalar=rsi, in1=gB, op0=ALU.mult, op1=ALU.mult),
              r=[ts, tsi, tg], w=[txn[b]])
        if out_dt == BF16:
            pb = k.psum[i % 2]
            tp = k.tpsum[i % 2]
            pv = pb.bitcast(BF16)
            for c in range(8):
                cx.op("tensor", lambda h, b=b, c=c, pv=pv: h.transpose(pv[:, c * 128:(c + 1) * 128], xn[b][:, c * 128:(c + 1) * 128], ident),
                      r=[txn[b]], w=[tp], inc=(c == 7))
            dst = outT[:, :, i * 128:(i + 1) * 128]
            eng = "scalar" if i % 2 == 0 else "vector"
            if eng == "scalar":
                cx.op(eng, lambda h, dst=dst, pv=pv: h.copy(dst, pv.rearrange("p (c t) -> p c t", c=8)), r=[tp], w=[outT_trk])
            else:
                cx.op(eng, lambda h, dst=dst, pv=pv: h.tensor_copy(dst, pv.rearrange("p (c t) -> p c t", c=8)), r=[tp], w=[outT_trk])
        else:
            for half in range(2):
                pb = k.psum[(2 * i + half) % 4]
                tp = k.tpsum[(2 * i + half) % 4]
                for c4 in range(4):
                    c = half * 4 + c4
                    cx.op("tensor", lambda h, b=b, c=c, c4=c4, pb=pb: h.transpose(pb[:, c4 * 128:(c4 + 1) * 128], xn[b][:, c * 128:(c + 1) * 128].bitcast(F32), ident),
                          r=[txn[b]], w=[tp], inc=(c4 == 3))
                dst = outT[:, half * 4:(half + 1) * 4, i * 128:(i + 1) * 128]
                if half == 0:
                    cx.op("scalar", lambda h, dst=dst, pb=pb: h.copy(dst, pb.rearrange("p (c t) -> p c t", c=4)), r=[tp], w=[outT_trk])
                else:
                    cx.op("vector", lambda h, dst=dst, pb=pb: h.tensor_copy(dst, pb.rearrange("p (c t) -> p c t", c=4)), r=[tp], w=[outT_trk])
    ar.release(m)


def s5_prep(k):
    cx, ar, D = k.cx, k.ar, k.D
    V = "vector"
    m0 = ar.mark()
    lam = ar.alloc([2, 32], F32)
    step = ar.alloc([32], F32)
    bb = ar.alloc([2, 512], F32)
    cc = ar.alloc([2, 512], F32)
    kvec = ar.alloc([16], F32)
    tl = Trk()
    sl_ = cx.fresh()
    for dst, nm in ((lam, "s5_lam"), (step, "s5_step"), (bb, "s5_b"), (cc, "s5_c"), (kvec, "kvec")):
        cx.dma("sync", dst, D[nm], sl_, w=[tl])
    T = Trk()

    def vop(fn, extra_r=()):
        cx.op(V, fn, r=[T, tl] + list(extra_r), w=[T])

    def aop(fn):
        cx.op("scalar", fn, r=[T, tl], w=[T])
    lre, lim = lam[:, 0, :], lam[:, 1, :]
    dl = ar.alloc([32], F32)
    re1 = ar.alloc([32], F32)
    im1 = ar.alloc([32], F32)
    aop(lambda h: h.activation(dl, step, AF.Exp))
    vop(lambda h: h.tensor_tensor(out=re1, in0=dl, in1=lre, op=ALU.mult))
    vop(lambda h: h.tensor_tensor(out=im1, in0=dl, in1=lim, op=ALU.mult))
    PWI = ar.alloc([16, 32], F32)
    PWR = ar.alloc([16, 32], F32)
    m_pw = ar.mark()
    KR = ar.alloc([16, 32], F32)
    KI = ar.alloc([16, 32], F32)
    kv_b = kvec.unsqueeze(2).to_broadcast([128, 16, 32])
    vop(lambda h: h.tensor_tensor(out=KR, in0=kv_b, in1=re1.unsqueeze(1).to_broadcast([128, 16, 32]), op=ALU.mult))
    vop(lambda h: h.tensor_tensor(out=KI, in0=kv_b, in1=im1.unsqueeze(1).to_broadcast([128, 16, 32]), op=ALU.mult))
    MAG = ar.alloc([16, 32], F32)
    aop(lambda h: h.activation(MAG, KR, AF.Exp))
    YI = ar.alloc([16, 32], I32)
    YF = ar.alloc([16, 32], F32)
    vop(lambda h: h.tensor_scalar(KI, KI, 1.0 / (2 * math.pi), None, op0=ALU.mult))
    vop(lambda h: h.tensor_copy(YI, KI))
    vop(lambda h: h.tensor_copy(YF, YI))
    vop(lambda h: h.tensor_tensor(out=KI, in0=KI, in1=YF, op=ALU.subtract))
    SH_ = ar.alloc([16, 32], F32)
    SQ_ = ar.alloc([16, 32], F32)
    aop(lambda h: h.activation(SH_, KI, AF.Sin, scale=math.pi))
    aop(lambda h: h.activation(SQ_, KI, AF.Sin, scale=math.pi / 2))
    CH_ = ar.alloc([16, 32], F32)
    vop(lambda h: h.tensor_tensor(out=CH_, in0=SQ_, in1=SQ_, op=ALU.mult))
    vop(lambda h: h.tensor_scalar(CH_, CH_, -2.0, 1.0, op0=ALU.mult, op1=ALU.add))
    vop(lambda h: h.tensor_tensor(out=PWI, in0=SH_, in1=CH_, op=ALU.mult))
    vop(lambda h: h.scalar_tensor_tensor(out=PWI, in0=PWI, scalar=2.0, in1=MAG, op0=ALU.mult, op1=ALU.mult))
    vop(lambda h: h.tensor_tensor(out=PWR, in0=SH_, in1=SH_, op=ALU.mult))
    vop(lambda h: h.tensor_scalar(PWR, PWR, -2.0, 1.0, op0=ALU.mult, op1=ALU.add))
    vop(lambda h: h.tensor_tensor(out=PWR, in0=PWR, in1=MAG, op=ALU.mult))
    ar.release(m_pw)
    lrm1 = ar.alloc([32], F32)
    li = PWI[:, 8, :]
    t1 = ar.alloc([32], F32)
    t2 = ar.alloc([32], F32)
    den = ar.alloc([32], F32)
    c0r = ar.alloc([32], F32)
    c0i = ar.alloc([32], F32)
    vop(lambda h: h.tensor_scalar(lrm1, PWR[:, 8, :], -1.0, None, op0=ALU.add))
    vop(lambda h: h.tensor_tensor(out=t1, in0=lre, in1=lre, op=ALU.mult))
    vop(lambda h: h.tensor_tensor(out=t2, in0=lim, in1=lim, op=ALU.mult))
    vop(lambda h: h.tensor_tensor(out=den, in0=t1, in1=t2, op=ALU.add))
    vop(lambda h: h.reciprocal(den, den))
    vop(lambda h: h.tensor_tensor(out=t1, in0=lrm1, in1=lre, op=ALU.mult))
    vop(lambda h: h.tensor_tensor(out=t2, in0=li, in1=lim, op=ALU.mult))
    vop(lambda h: h.tensor_tensor(out=t1, in0=t1, in1=t2, op=ALU.add))
    vop(lambda h: h.tensor_tensor(out=c0r, in0=t1, in1=den, op=ALU.mult))
    vop(lambda h: h.tensor_tensor(out=t1, in0=li, in1=lre, op=ALU.mult))
    vop(lambda h: h.tensor_tensor(out=t2, in0=lrm1, in1=lim, op=ALU.mult))
    vop(lambda h: h.tensor_tensor(out=t1, in0=t1, in1=t2, op=ALU.subtract))
    vop(lambda h: h.tensor_tensor(out=c0i, in0=t1, in1=den, op=ALU.mult))
    BBR = ar.alloc([32, 16], F32)
    BBI = ar.alloc([32, 16], F32)
    TA = ar.alloc([32, 16], F32)
    br = bb[:, 0, :].rearrange("p (a c) -> p a c", c=16)
    bi = bb[:, 1, :].rearrange("p (a c) -> p a c", c=16)
    c0r_b = c0r.unsqueeze(2).to_broadcast([128, 32, 16])
    c0i_b = c0i.unsqueeze(2).to_broadcast([128, 32, 16])
    vop(lambda h: h.tensor_tensor(out=BBR, in0=br, in1=c0r_b, op=ALU.mult))
    vop(lambda h: h.tensor_tensor(out=TA, in0=bi, in1=c0i_b, op=ALU.mult))
    vop(lambda h: h.tensor_tensor(out=BBR, in0=BBR, in1=TA, op=ALU.subtract))
    vop(lambda h: h.tensor_tensor(out=BBI, in0=bi, in1=c0r_b, op=ALU.mult))
    vop(lambda h: h.tensor_tensor(out=TA, in0=br, in1=c0i_b, op=ALU.mult))
    vop(lambda h: h.tensor_tensor(out=BBI, in0=BBI, in1=TA, op=ALU.add))
    ASd = ar.alloc([16, 2, 8, 16], F32)
    CS2d = ar.alloc([16, 2, 8, 16], F32)
    T1 = ar.alloc([8, 16, 16], F32)
    T2 = ar.alloc([8, 16, 16], F32)
    cr = cc[:, 0, :].rearrange("p (d a c) -> p d a c", d=2, c=16)
    ci = cc[:, 1, :].rearrange("p (d a c) -> p d a c", d=2, c=16)
    BBR4 = BBR.rearrange("p (d a) c -> p d a c", d=2)
    BBI4 = BBI.rearrange("p (d a) c -> p d a c", d=2)

    def pw(arr, d, k0, kstep):
        return pap(arr, 0, 128, k0 * 32 + d * 16, [[kstep * 32, 8], [1, 16], [0, 16]])

    def dst(arr, dofs, ri):
        return pap(arr, 0, 128, dofs * 4096 + ri * 128, [[16, 8], [256, 16], [1, 16]])

    def vec(v4, d):
        a_ = v4[:, d]
        return bass.AP(a_.tensor, a_.offset, [list(a_.ap[0]), [0, 8], list(a_.ap[1]), list(a_.ap[2])])

    T1f = T1.rearrange("p a b c -> p (a b c)")

    def cmul(out_arr, dofs, d, k0, kstep, vr, vi, neg_im):
        pr, pi_ = pw(PWR, d, k0, kstep), pw(PWI, d, k0, kstep)
        vop(lambda h: h.tensor_tensor(out=T1, in0=pr, in1=vec(vr, d), op=ALU.mult))
        vop(lambda h: h.tensor_tensor(out=T2, in0=pi_, in1=vec(vi, d), op=ALU.mult))
        vop(lambda h: h.tensor_tensor(out=dst(out_arr, dofs, 0), in0=T1, in1=T2, op=ALU.subtract))
        vop(lambda h: h.tensor_tensor(out=T1, in0=pr, in1=vec(vi, d), op=ALU.mult))
        vop(lambda h: h.tensor_tensor(out=T2, in0=pi_, in1=vec(vr, d), op=ALU.mult))
        if neg_im:
            vop(lambda h: h.tensor_scalar(T1f, T1f, -1.0, None, op0=ALU.mult))
            vop(lambda h: h.tensor_tensor(out=dst(out_arr, dofs, 1), in0=T1, in1=T2, op=ALU.subtract))
        else:
            vop(lambda h: h.tensor_tensor(out=dst(out_arr, dofs, 1), in0=T1, in1=T2, op=ALU.add))
    vop(lambda h: h.tensor_copy(k.s5A1[:, 0:32], PWR[:, 15, :]))
    vop(lambda h: h.tensor_copy(k.s5A1[:, 32:64], PWR[:, 15, :]))
    vop(lambda h: h.tensor_scalar(k.s5A2[:, 0:32], PWI[:, 15, :], -1.0, None, op0=ALU.mult))
    vop(lambda h: h.tensor_copy(k.s5A2[:, 32:64], PWI[:, 15, :]))
    cmul(k.s5CS, 0, 0, 8, 1, cr, ci, True)
    cmul(k.s5CS, 1, 1, 15, -1, cr, ci, True)
    k.t_s5w = T
    mf = ar.alloc([2, 128], F32)
    dv = ar.alloc([32], F32)
    tm = Trk()
    sl_ = cx.fresh()
    cx.dma("sync", mf[:, 0, :], D["s5mf"], sl_, w=[tm])
    cx.dma("sync", mf[:, 1, :], D["s5mb"], sl_, w=[tm])
    cx.dma("sync", dv, D["s5_dvec"], sl_, w=[tm])
    tt1 = [ar.alloc([128], F32) for _ in range(2)]
    ttt = [Trk(), Trk()]
    ASb = ASd.rearrange("p a r s c -> p (a r) (s c)")
    ASm = ASd.rearrange("p a r s c -> p a r (s c)")
    CSm = CS2d.rearrange("p a r s c -> p a r (s c)")
    for d in range(2):
        if d == 0:
            cmul(ASd, 0, 0, 14, -1, BBR4, BBI4, False)
            cmul(CS2d, 0, 0, 0, 1, cr, ci, True)
        else:
            cmul(ASd, 0, 1, 7, 1, BBR4, BBI4, False)
            cmul(CS2d, 0, 1, 7, -1, cr, ci, True)
        for grp in range(8):
            pb = k.psum[grp % 4]
            tp = k.tpsum[grp % 4]
            for j in range(4):
                blk = grp * 4 + j
                cx.op("tensor", lambda h, pb=pb, j=j, blk=blk: h.transpose(pb[:, j * 128:(j + 1) * 128], ASb[:, blk, :], k.ident),
                      r=[T], w=[tp], inc=(j == 3))
            dstv = k.s5AT[:, d * 32 + grp * 4:d * 32 + (grp + 1) * 4, :]
            if grp % 2 == 0:
                cx.op("scalar", lambda h, dstv=dstv, pb=pb: h.copy(dstv, pb.rearrange("p (j x) -> p j x", j=4)), r=[tp], w=[k.t_s5at])
            else:
                cx.op("vector", lambda h, dstv=dstv, pb=pb: h.tensor_copy(dstv, pb.rearrange("p (j x) -> p j x", j=4)), r=[tp], w=[k.t_s5at])
        for g in range(32):
            gh, gl = g // 16, g % 16
            pb = k.psum[4 + g % 4]
            tp = k.tpsum[4 + g % 4]
            for ri in range(2):
                cx.op("tensor", lambda h, pb=pb, ri=ri, gh=gh, gl=gl: h.matmul(
                    pb[:, 0:128], ASm[gh * 64:(gh + 1) * 64, gl, ri, :], CSm[gh * 64:(gh + 1) * 64, gl, ri, :],
                    start=(ri == 0), stop=(ri == 1)), r=[T], w=[tp], inc=(ri == 1))
            b_ = g % 2
            cx.op(V, lambda h, pb=pb, b_=b_, d=d: h.tensor_tensor(out=tt1[b_], in0=pb[:, 0:128], in1=mf[:, d, :], op=ALU.mult), r=[tp, tm], w=[ttt[b_]])
            if d == 0:
                cx.op(V, lambda h, b_=b_, g=g: h.scalar_tensor_tensor(out=k.s5TT[:, g, :], in0=k.ident, scalar=dv[:, g:g + 1], in1=tt1[b_], op0=ALU.mult, op1=ALU.add),
                      r=[ttt[b_], tm], w=[k.t_s5tt])
            else:
                cx.op(V, lambda h, b_=b_, g=g: h.tensor_tensor(out=k.s5TT[:, g, :], in0=k.s5TT[:, g, :], in1=tt1[b_], op=ALU.add),
                      r=[ttt[b_]], w=[k.t_s5tt])
    cx.barrier()
    ar.release(m0)


def load_w_cols(k, wdram, col0, ncols, dst, trk, slot, eng="gpsimd"):
    src = bass.AP(wdram.tensor, wdram.offset + col0, [[wdram.ap[0][0] * 1, 128], [wdram.ap[0][0] * 128, 8], [1, ncols]])
    return k.cx.dma(eng, dst, src, slot, w=[trk])


def proj_fm(k, wt, wtrk, consume):
    cx = k.cx
    for n in range(4):
        pb = k.psum[n % 2 + 2]
        tp = k.tpsum[n % 2 + 2]
        for c in range(8):
            cx.op("tensor", lambda h, pb=pb, c=c, n=n: h.matmul(pb[:, :], wt[:, c, :], k.hT[:, c, n * 512:(n + 1) * 512], start=(c == 0), stop=(c == 7)),
                  r=[wtrk, k.t_hT], w=[tp], inc=(c == 7))
        consume(n, pb, tp)


def s5_build_U(k):
    cx, ar = k.cx, k.ar
    m0 = ar.mark()
    wt = [ar.alloc([8, 128], BF16) for _ in range(2)]
    twt = [Trk(), Trk()]
    swt = [cx.fresh('sw'), cx.fresh('sw')]
    uT = [ar.alloc([2048], BF16) for _ in range(2)]
    tuT = [Trk(), Trk()]
    for ct in range(4):
        b = ct % 2
        load_w_cols(k, k.D["w_in"], ct * 128, 128, wt[b], twt[b], swt[b])

        def consume(n, pb, tp, b=b):
            if n % 2 == 0:
                cx.op("scalar", lambda h: h.copy(uT[b][:, n * 512:(n + 1) * 512], pb[:, :]), r=[tp], w=[tuT[b]])
            else:
                cx.op("vector", lambda h: h.tensor_copy(uT[b][:, n * 512:(n + 1) * 512], pb[:, :]), r=[tp], w=[tuT[b]])
        proj_fm(k, wt[b], twt[b], consume)
        for gi in range(8):
            g = ct * 8 + gi
            q0 = 32 * (gi // 2)
            pb = k.psum[4 + gi % 4]
            tp = k.tpsum[4 + gi % 4]
            for s in range(8):
                rhs = pap(uT[b], q0, 32, s, [[8, 256]])
                cx.op("tensor", lambda h, pb=pb, s=s, rhs=rhs, q0=q0, gi=gi: h.matmul(pb[:, 0:256], k.selT[q0:q0 + 32, gi % 2, s, :], rhs, start=(s == 0), stop=(s == 7), tile_position=(q0, 0)),
                      r=[tuT[b]], w=[tp], inc=(s == 7))
            if gi % 2 == 0:
                cx.op("scalar", lambda h, pb=pb, g=g: h.copy(k.s5U[:, g, :], pb[:, 0:256]), r=[tp], w=[k.t_s5U])
            else:
                cx.op("vector", lambda h, pb=pb, g=g: h.tensor_copy(k.s5U[:, g, :], pb[:, 0:256]), r=[tp], w=[k.t_s5U])
    cx.barrier()
    ar.release(m0)


def s5_main(k, yT, t_yT):
    cx, ar, D = k.cx, k.ar, k.D
    V = "vector"
    m0 = ar.mark()
    wg = ar.alloc([4, 512], BF16)
    twg = Trk()
    wsrc = D["s5_w_glu"]
    cx.dma("gpsimd", wg, bass.AP(wsrc.tensor, wsrc.offset, [[512, 128], [512 * 128, 4], [1, 512]]), cx.fresh('sw'), w=[twg])
    bgl = ar.alloc([4], F32)
    nw = ar.alloc([4], F32)
    tb = Trk()
    sl_ = cx.fresh()
    cx.dma("sync", bgl, D["s5_bglu"], sl_, w=[tb])
    cx.dma("sync", nw, D["s5_normw"], sl_, w=[tb])
    SH = ar.alloc([2, 257, 2, 16], BF16)
    tSH = Trk()
    tSHh = Trk()
    X = [ar.alloc([64], F32) for _ in range(3)]
    tX = [Trk() for _ in range(3)]
    t1 = ar.alloc([64], F32)
    t2 = ar.alloc([64], F32)
    tt = Trk()
    tt2 = Trk()
    cx.op("gpsimd", lambda h: h.memset(SH[:, 0, 0, :, :], 0.0), w=[tSH])
    cx.op("gpsimd", lambda h: h.memset(SH[:, 1, 256, :, :], 0.0), w=[tSH])
    cx.op("gpsimd", lambda h: h.memset(X[0], 0.0), w=[tX[0]])
    n = 0
    for gl in range(16):
        for d in range(2):
            for ri in range(2):
                blk = d * 32 + gl * 2 + ri
                pb = k.psum[n % 4]
                tp = k.tpsum[n % 4]
                cx.op("tensor", lambda h, pb=pb, blk=blk, gl=gl: h.matmul(pb[0:64, 0:256], k.s5AT[:, blk, 0:64], k.s5U[:, gl, :], start=True, stop=True),
                      r=[k.t_s5at, k.t_s5U], w=[tp], inc=False)
                cx.op("tensor", lambda h, pb=pb, blk=blk, gl=gl: h.matmul(pb[64:128, 0:256], k.s5AT[:, blk, 64:128], k.s5U[:, 16 + gl, :], start=True, stop=True),
                      r=[k.t_s5at, k.t_s5U], w=[tp])
                slot0 = 1 if d == 0 else 0
                dstv = pap(SH, 0, 128, d * 257 * 32 + slot0 * 32 + ri * 16 + gl, [[32, 256]])
                if n % 2 == 0:
                    cx.op("scalar", lambda h, dstv=dstv, pb=pb: h.copy(dstv, pb[:, 0:256]), r=[tp], w=[tSH])
                else:
                    cx.op(V, lambda h, dstv=dstv, pb=pb: h.tensor_copy(dstv, pb[:, 0:256]), r=[tp], w=[tSH])
                n += 1
    import os
    S5STOP = os.environ.get('S5_STOP', '')
    if S5STOP == 'a':
        cx.barrier(); ar.release(m0); return
    for i in range(256):
        xp, xn = X[i % 3], X[(i + 1) % 3]
        txp, txn = tX[i % 3], tX[(i + 1) % 3]
        xsw = pap(xp, 0, 128, 32, [[-32, 2], [1, 32]])
        bf = (i + 1) * 32
        bb_ = 257 * 32 + (255 - i) * 32
        sview = pap(SH, 0, 128, bf, [[16, 2], [bb_ - bf, 2], [1, 16]])
        xp3 = xp.rearrange("p (r x) -> p r x", r=2)
        cx.op("gpsimd", lambda h, xsw=xsw: h.tensor_tensor(out=t2.rearrange("p (r x) -> p r x", r=2), in0=k.s5A2.rearrange("p (r x) -> p r x", r=2), in1=xsw, op=ALU.mult), r=[txp, k.t_s5w], w=[tt2])
        cx.op(V, lambda h, xp=xp: h.tensor_tensor(out=t1, in0=k.s5A1, in1=xp, op=ALU.mult), r=[txp, k.t_s5w], w=[tt])
        cx.op(V, lambda h, sview=sview: h.tensor_tensor(out=t1.rearrange("p (r d x) -> p r d x", r=2, d=2), in0=t1.rearrange("p (r d x) -> p r d x", r=2, d=2), in1=sview, op=ALU.add), r=[tt, tSH], w=[tt])
        cx.op(V, lambda h, xn=xn: h.tensor_tensor(out=xn, in0=t1, in1=t2, op=ALU.add), r=[tt, tt2], w=[txn])
        cx.op("scalar", lambda h, xn=xn, sview=sview: h.copy(sview, xn.rearrange("p (r d x) -> p r d x", r=2, d=2)), r=[txn], w=[tSHh])
    if S5STOP == 'rec':
        cx.barrier(); ar.release(m0); return
    gT = ar.alloc([4, 2048], F32)
    gTb = ar.alloc([4, 2048], BF16)
    tgT = [Trk() for _ in range(4)]
    tgTb = [Trk() for _ in range(4)]
    ybuf = [k.arA.alloc([8, 256], BF16) for _ in range(2)]
    tyb = [Trk(), Trk()]
    for ct in range(4):
        b = ct % 2
        for gi in range(8):
            g = ct * 8 + gi
            gh, gl = g // 16, g % 16
            pb = k.psum[gi % 2]
            tp = k.tpsum[gi % 2]
            cx.op("tensor", lambda h, pb=pb, g=g: h.matmul(pb[:, 0:256], k.s5TT[:, g, :], k.s5U[:, g, :], start=True, stop=False),
                  r=[k.t_s5tt, k.t_s5U], w=[tp], inc=False)
            for d in range(2):
                for ri in range(2):
                    slot0 = 0 if d == 0 else 1
                    rhs = pap(SH, gh * 64, 64, d * 257 * 32 + slot0 * 32 + ri * 16 + gl, [[32, 256]])
                    last = (d == 1 and ri == 1)
                    cx.op("tensor", lambda h, pb=pb, rhs=rhs, d=d, ri=ri, gh=gh, gl=gl, last=last: h.matmul(
                        pb[:, 0:256], k.s5CS[gh * 64:(gh + 1) * 64, d, gl, ri, :], rhs, start=False, stop=last),
                        r=[tSH, tSHh, k.t_s5w], w=[tp], inc=last)
            if gi % 2 == 0:
                cx.op("scalar", lambda h, pb=pb, b=b, gi=gi: h.copy(ybuf[b][:, gi, :], pb[:, 0:256]), r=[tp], w=[tyb[b]])
            else:
                cx.op(V, lambda h, pb=pb, b=b, gi=gi: h.tensor_copy(ybuf[b][:, gi, :], pb[:, 0:256]), r=[tp], w=[tyb[b]])
        for t in range(8):
            q0 = 32 * (t // 2)
            pb = k.psum[2 + t % 4]
            tp = k.tpsum[2 + t % 4]
            for gi in range(8):
                cx.op("tensor", lambda h, pb=pb, t=t, gi=gi, q0=q0, b=b: h.matmul(pb[:, 0:256], k.selT[q0:q0 + 32, t % 2, gi, :], ybuf[b][q0:q0 + 32, gi, :], start=(gi == 0), stop=(gi == 7), tile_position=(q0, 0)),
                      r=[tyb[b]], w=[tp], inc=(gi == 7))
            dstv = pap(gT, 0, 128, ct * 2048 + t, [[8, 256]])
            cx.op("scalar", lambda h, pb=pb, dstv=dstv: h.activation(dstv, pb[:, 0:256], AF.Gelu), r=[tp], w=[tgT[ct]])
        cx.op("vector", lambda h, ct=ct: h.tensor_copy(gTb[:, ct, :], gT[:, ct, :]), r=[tgT[ct]], w=[tgTb[ct]])
    k.dbg_add("s5_g", gT, tgT)
    if S5STOP == 'c':
        cx.barrier(); ar.release(m0); return
    sig = ar.alloc([4, 512], BF16)
    tsig = Trk()
    sq = ar.alloc([4, 512], BF16)
    tsq = Trk()
    rs = ar.alloc([512], F32)
    trs = Trk()
    for nck in range(4):
        ts = slice(nck * 512, (nck + 1) * 512)
        for co in range(4):
            pb = k.psum[co % 2]
            tp = k.tpsum[co % 2]
            for ci in range(4):
                cx.op("tensor", lambda h, pb=pb, co=co, ci=ci, ts=ts: h.matmul(pb[:, :], wg[:, ci, co * 128:(co + 1) * 128], gTb[:, ci, ts], start=(ci == 0), stop=(ci == 3)),
                      r=[twg] + tgTb, w=[tp], inc=(ci == 3))
            cx.op("scalar", lambda h, pb=pb, co=co: h.activation(sig[:, co, :], pb[:, :], AF.Sigmoid, bias=bgl[:, co:co + 1]), r=[tp, tb], w=[tsig])
        for co in range(4):
            cx.op(V, lambda h, co=co, ts=ts: h.tensor_tensor(out=gT[:, co, ts], in0=gT[:, co, ts], in1=sig[:, co, :], op=ALU.mult), r=[tsig, tgT[co]], w=[tgT[co]])
            cx.op("scalar", lambda h, co=co, ts=ts: h.activation(sq[:, co, :], gT[:, co, ts], AF.Square), r=[tgT[co]], w=[tsq])
        pb = k.psum[2 + nck % 2]
        tp = k.tpsum[2 + nck % 2]
        for co in range(4):
            cx.op("tensor", lambda h, pb=pb, co=co: h.matmul(pb[:, :], k.onesb, sq[:, co, :], start=(co == 0), stop=(co == 3)), r=[tsq], w=[tp], inc=(co == 3))
        cx.op("scalar", lambda h, pb=pb: h.activation(rs, pb[:, :], AF.Sqrt, scale=1.0 / 512, bias=k.epsc), r=[tp], w=[trs])
        cx.op(V, lambda h: h.reciprocal(rs, rs), r=[trs], w=[trs])
        for co in range(4):
            cx.op(V, lambda h, co=co, ts=ts: h.scalar_tensor_tensor(out=yT[:, co, ts], in0=gT[:, co, ts], scalar=nw[:, co:co + 1], in1=rs, op0=ALU.mult, op1=ALU.mult),
                  r=[tgT[co], trs, tb, tsq], w=[t_yT])
    k.dbg_add("s5_gl", gT, tgT)
    cx.barrier()
    ar.release(m0)


def build(in_shapes, stage="full", dbg_names=(), n_heads=4, n_experts=32):
    nc = bass.Bass("TRN2", target_bir_lowering=False)
    k = K()
    k.n_heads = n_heads
    k.n_experts = n_experts
    k.nc = nc
    D = {}
    for nm, (shape, dt) in in_shapes.items():
        D[nm] = nc.dram_tensor(nm, list(shape), dt, kind="ExternalInput").ap()
    k.D = D
    out = nc.dram_tensor("out", [S, DM], F32, kind="ExternalOutput").ap()
    k.dbg = {}
    k.dbg_req = set(dbg_names)

    with contextlib.ExitStack() as st:
        cx = Ctx(nc, st)
        k.cx = cx

        def finish():
            deps = [(s_.key, s_.total) for s_ in cx.slots if s_.total > 0]
            cx.wait_deps("sync", deps + [(e, cx.cnt[e]) for e in ENGS if e != "sync" and cx.cnt[e] > 0])
            with nc.Block() as block:
                cx.emit_all(block)
            k.n_ops = cx.n_ops
            return nc, k
        k.slot_c = cx.slot("c")
        k.slot_w = cx.slot("w")
        k.slot_x = [cx.slot("x0"), cx.slot("x1")]
        k.slot_o = cx.slot("o")
        k.psum = [cx.ps("ps%d" % i, [128, 512], F32) for i in range(8)]
        k.psum = [p[:, :] for p in k.psum]
        k.tpsum = [Trk("ps%d" % i, excl=True) for i in range(8)]
        k.ident = cx.sb("ident", [128, 128], F32)[:, :]
        k.identb = cx.sb("identb", [128, 128], BF16)[:, :]
        k.ones = cx.sb("ones", [128, 128], F32)[:, :]
        k.onesb = cx.sb("onesb", [128, 128], BF16)[:, :]
        k.epsc = cx.sb("epsc", [128, 1], F32)[:, :]
        k.selT = cx.sb("selT", [128, 2, 8, 128], BF16)[:, :, :, :]
        tc = Trk()
        cx.dma("sync", k.ident, D["ident"], k.slot_c, w=[tc])
        cx.dma("sync", k.identb, D["identb"], k.slot_c, w=[tc])
        cx.dma("sync", k.ones, D["ones"], k.slot_c, w=[tc])
        cx.dma("gpsimd", k.onesb, D["ones"], cx.fresh("sw"), w=[tc])
        cx.op("vector", lambda h: h.memset(k.epsc, EPS), w=[tc])
        cx.dma("sync", k.selT, D["selT"], k.slot_c, w=[tc])
        k.s5A1 = cx.sb("s5A1", [128, 64], F32)[:, :]
        k.s5A2 = cx.sb("s5A2", [128, 64], F32)[:, :]
        ar = Arena(cx, 51456)
        k.ar = ar
        cx.barrier()

        def dbg_add(name, ap, trks):
            if name in k.dbg_req:
                shape = list(ap.shape)
                dt_ = F32
                o = nc.dram_tensor("dbg_" + name, shape, dt_, kind="ExternalOutput").ap()
                cx.dma("gpsimd" if ap.dtype != F32 else "sync", o, ap, cx.fresh("sw" if ap.dtype != F32 else "hw"), r=list(trks))
        k.dbg_add = dbg_add

        regA = ar.alloc([NT * 1024], F32)
        arA = Arena(cx, NT * 1024, base=regA)
        k.arA = arA
        yT = ar.alloc([8, 2048], BF16)
        t_yT = Trk()
        k.s5U = arA.alloc([32, 256], BF16)
        k.t_s5U = Trk()
        m_h = arA.mark()
        k.hT = arA.alloc([8, 2048], BF16)
        k.t_hT = Trk()

        m1 = ar.mark()
        xt = [ar.alloc([1024], F32) for _ in range(2)]
        txt = [Trk(), Trk()]

        def src_x(i):
            b = i % 2
            cx.dma("sync", xt[b], D["x"][i * 128:(i + 1) * 128, :], k.slot_x[b], w=[txt[b]])
            return xt[b], txt[b]
        norm_transpose(k, "mix", src_x, NT, D["norm_mix"], k.hT, BF16, k.t_hT)
        cx.barrier()
        ar.release(m1)

        if stage == 'p1':
            return finish()
        s5_build_U(k)
        if stage == 'U':
            return finish()
        mg = ar.mark()
        gdn_setup(k)
        if k.n_heads > 0:
            gdn_load_weights(k, 0)
        for hd in range(k.n_heads):
            gdn_head(k, hd, yT, t_yT)
        ar.release(mg)
        k.dbg_add("ygdnT", yT[:, 4:8, :], [t_yT])
        if stage == 'gdn':
            return finish()
        cx.barrier()
        arA.release(m_h)
        k.s5AT = arA.alloc([64, 128], BF16)
        k.t_s5at = Trk()
        k.s5CS = arA.alloc([2, 16, 2, 128], BF16)
        k.s5TT = arA.alloc([32, 128], BF16)
        k.t_s5tt = Trk()
        s5_prep(k)
        if stage == 's5prep':
            return finish()
        s5_main(k, yT[:, 0:4, :], t_yT)
        k.dbg_add("ys5T", yT[:, 0:4, :], [t_yT])
        if stage == "s5":
            return finish()
        if True:
            cx.barrier()
            k.xacc = regA.rearrange('p (a b) -> p a b', a=NT)
            k.txacc = [Trk() for _ in range(NT)]
            out_proj(k, yT, t_yT)
            k.dbg_add("x1", k.xacc, k.txacc)
            if stage == 'oproj':
                return finish()
            xattn(k)
            if stage == 'xattn':
                return finish()
            k.dbg_add("x2", k.xacc, k.txacc)
            moe(k)
            k.dbg_add("x3", k.xacc, k.txacc + [t_ for p_ in k.txh for t_ in p_])
            final_norm(k, out)

        return finish()


def host_inputs(inp, b):
    m = {}
    m["x"] = np.ascontiguousarray(inp["x"][b])
    m["mem"] = np.ascontiguousarray(inp["mem"][b])
    m["norm_mix"] = inp["norm_mix"][0]
    m["w_in"] = inp["w_in"][0]
    m["w_out"] = inp["w_out"][0]
    m["s5_w_glu"] = inp["s5_w_glu"][0]
    m.update(host_s5(inp))
    cv = inp["gdn_conv"][0]
    m["gdn_convw"] = np.ascontiguousarray(cv.reshape(5, 3, 4, 128).transpose(3, 2, 1, 0))
    for nm in ("gdn_a_log_f", "gdn_dt_bias_f", "gdn_a_log_b", "gdn_dt_bias_b"):
        m[nm] = inp[nm][0]
    m["gdn_norm"] = inp["gdn_norm"][0]
    for nm in ("norm_xattn", "norm_mem", "xa_wq", "xa_wk", "xa_wv", "xa_wo", "norm_moe", "router_group_w", "router_group_b",
               "router_expert_w", "router_expert_b", "moe_w_gate", "moe_w_up", "moe_w_down"):
        m[nm] = inp[nm][0]
    m["norm_final"] = inp["norm_final"]
    m.update(host_consts())
    return m


def gdn_setup(k):
    cx, ar, D = k.cx, k.ar, k.D
    V = "vector"
    G = K()
    k.G = G
    G.mask = ar.alloc([7, 128], F32)
    G.tmask = Trk()
    cx.dma("sync", G.mask, D["gmask"][:, 0:7, :], cx.fresh(), w=[G.tmask])
    wsm = ar.alloc([8, 16], BF16)
    tw = Trk()
    load_w_cols(k, D["w_in"], 2560, 16, wsm, tw, cx.fresh('sw'))
    BA = ar.alloc([16, 16], F32)
    tBA = Trk()
    for i in range(NT):
        pb = k.psum[i % 4]
        tp = k.tpsum[i % 4]
        for c in range(8):
            cx.op("tensor", lambda h, pb=pb, c=c, i=i: h.matmul(pb[:, 0:16], k.hT[:, c, i * 128:(i + 1) * 128], wsm[:, c, :], start=(c == 0), stop=(c == 7)),
                  r=[tw, k.t_hT], w=[tp], inc=(c == 7))
        cx.op("scalar", lambda h, pb=pb, i=i: h.copy(BA[:, i, :], pb[:, 0:16]), r=[tp], w=[tBA])
    pr = ar.alloc([4, 4], F32)
    tpr = Trk()
    sl_ = cx.fresh()
    for j, nm in enumerate(("gdn_a_log_f", "gdn_dt_bias_f", "gdn_a_log_b", "gdn_dt_bias_b")):
        cx.dma("sync", pr[:, j, :], dram_bcast(D[nm], 128, 4), sl_, w=[tpr])
    G.nw = ar.alloc([128], F32)
    cx.dma("sync", G.nw, dram_bcast(D["gdn_norm"], 128, 128), sl_, w=[tpr])
    G.tpr = tpr
    T = Trk()
    G.T = T
    G.beta, G.nb, G.gc, G.eg, G.neg, G.ed = [], [], [], [], [], []
    def per_dir(d):
        beta = ar.alloc([16, 4], F32)
        nb = ar.alloc([16, 4], F32)
        g = ar.alloc([16, 4], F32)
        gc = ar.alloc([16, 4], F32)
        gt = ar.alloc([16, 4], F32)
        eg = ar.alloc([16, 4], F32)
        neg = ar.alloc([16, 4], F32)
        ed = ar.alloc([16, 4], F32)
        ea = ar.alloc([4], F32)
        braw = BA[:, :, d * 4:(d + 1) * 4]
        araw = BA[:, :, 8 + d * 4:8 + (d + 1) * 4]
        cx.op("scalar", lambda h: h.activation(beta, braw, AF.Sigmoid), r=[tBA, T], w=[T])
        cx.op(V, lambda h: h.tensor_scalar(nb, beta, -1.0, None, op0=ALU.mult), r=[T], w=[T])
        cx.op("scalar", lambda h: h.activation(ea, pr[:, 2 * d, :], AF.Exp), r=[tpr, T], w=[T])
        cx.op(V, lambda h: h.tensor_tensor(out=g, in0=araw, in1=pr[:, 2 * d + 1, :].unsqueeze(1).to_broadcast([128, 16, 4]), op=ALU.add), r=[tBA, tpr, T], w=[T])
        cx.op("scalar", lambda h: h.activation(g, g, AF.Exp), r=[T], w=[T])
        cx.op("scalar", lambda h: h.activation(g, g, AF.Ln, bias=1.0), r=[T], w=[T])
        cx.op(V, lambda h: h.scalar_tensor_tensor(out=g, in0=g, scalar=-1.0, in1=ea.unsqueeze(1).to_broadcast([128, 16, 4]), op0=ALU.mult, op1=ALU.mult), r=[T], w=[T])
        g2 = g.rearrange("p a b -> p (a b)")
        pb = k.psum[4 + d]
        tp = k.tpsum[4 + d]
        cx.op("tensor", lambda h, pb=pb, d=d: h.matmul(pb[:, 0:64], G.mask[:, d, :], g2, start=True, stop=True), r=[T, G.tmask], w=[tp])
        cx.op("tensor", lambda h, pb=pb: h.matmul(pb[:, 64:128], G.mask[:, 6, :], g2, start=True, stop=True), r=[T, G.tmask], w=[tp])
        cx.op(V, lambda h, pb=pb: h.tensor_copy(gc.rearrange("p a b -> p (a b)"), pb[:, 0:64]), r=[tp], w=[T])
        cx.op(V, lambda h, pb=pb: h.tensor_tensor(out=gt.rearrange("p a b -> p (a b)"), in0=pb[:, 64:128], in1=gc.rearrange("p a b -> p (a b)"), op=ALU.subtract), r=[tp, T], w=[T])
        cx.op("scalar", lambda h: h.activation(eg, gc, AF.Exp), r=[T], w=[T])
        cx.op("scalar", lambda h: h.activation(ed, gt, AF.Exp), r=[T], w=[T])
        cx.op(V, lambda h: h.tensor_scalar(neg, eg, -1.0, None, op0=ALU.mult), r=[T], w=[T])
        G.g = getattr(G, "g", []) + [g]
        G.beta.append(beta); G.nb.append(nb); G.gc.append(gc); G.eg.append(eg); G.neg.append(neg); G.ed.append(ed)
    per_dir(0)
    per_dir(1)
    G.osum = ar.alloc([16, 128], F32)
    G.tosum = [Trk() for _ in range(NT)]
    G.wset = [[k.arA.alloc([8, 128], BF16) for _ in range(4)] for _ in range(2)]
    G.tw = [[Trk() for _ in range(4)] for _ in range(2)]
    G.sw = [cx.slot("gw0"), cx.slot("gw1")]


def gdn_load_weights(k, hd):
    G, D = k.G, k.D
    st = hd % 2
    for j in range(3):
        load_w_cols(k, D["w_in"], 512 + j * 512 + hd * 128, 128, G.wset[st][j], G.tw[st][j], G.sw[st])
    load_w_cols(k, D["w_in"], 512 + 1536 + hd * 128, 128, G.wset[st][3], G.tw[st][3], G.sw[st])


def gdn_head(k, hd, yT, t_yT):
    cx, ar, D, G = k.cx, k.ar, k.D, k.G
    V = "vector"
    m0 = ar.mark()
    qnT = ar.alloc([2048], BF16)
    knT = ar.alloc([2048], BF16)
    Ktok = ar.alloc([16, 128], BF16)
    Vtok = ar.alloc([16, 128], BF16)
    tq, tk_, tKt, tVt = Trk(), Trk(), Trk(), Trk()
    st_ = hd % 2
    wz, twz = G.wset[st_][3], G.tw[st_][3]
    w3, tw3 = G.wset[st_][0:3], G.tw[st_][0:3]
    if hd + 1 < k.n_heads:
        gdn_load_weights(k, hd + 1)
    mA = ar.mark()
    cw = ar.alloc([3, 5], F32)
    tcw = Trk()
    cx.dma("sync", cw, D["gdn_convw"][:, hd, :, :], cx.fresh(), w=[tcw])
    diag = ar.alloc([15, 128], BF16)
    tdg = Trk()
    for j in range(3):
        for t in range(5):
            cx.op("vector", lambda h, j=j, t=t: h.tensor_scalar(diag[:, j * 5 + t, :], k.identb, cw[:, j, t:t + 1], None, op0=ALU.mult), r=[tcw], w=[tdg])
    import os
    ALV = int(os.environ.get("GDN_ALV", "9"))
    if ALV == 0:
        cx.barrier(); ar.release(m0); return
    raw = [ar.alloc([2052], BF16) for _ in range(2)]
    traw = [Trk(), Trk()]
    for b in range(2):
        cx.op("gpsimd", lambda h, b=b: h.memset(raw[b][:, 0:2], 0.0), w=[traw[b]])
        cx.op("gpsimd", lambda h, b=b: h.memset(raw[b][:, 2050:2052], 0.0), w=[traw[b]])
    act2 = [ar.alloc([2048], F32) for _ in range(2)]
    tact = Trk()
    vT = ar.alloc([2048], BF16)
    tvT = Trk()
    sqb = ar.alloc([2048], BF16)
    tsqb = Trk()
    rn = [ar.alloc([512], F32) for _ in range(4)]
    trn = [Trk() for _ in range(4)]
    tactn2 = [[Trk() for _ in range(4)] for _ in range(2)]
    tsqn = [Trk() for _ in range(4)]
    if ALV == 1:
        cx.barrier(); ar.release(m0); return
    for j in range(3):
        b = j % 2
        act = act2[j % 2]
        tactn = tactn2[j % 2]

        def consume(n, pb, tp, b=b):
            cx.op(V if n % 2 else "scalar", (lambda h: h.tensor_copy(raw[b][:, 2 + n * 512:2 + (n + 1) * 512], pb[:, :])) if n % 2 else
                  (lambda h: h.copy(raw[b][:, 2 + n * 512:2 + (n + 1) * 512], pb[:, :])), r=[tp], w=[traw[b]])
        proj_fm(k, w3[j], tw3[j], consume)
        for n in range(4):
            pb = k.psum[4 + n]
            tp = k.tpsum[4 + n]
            for t in range(5):
                cx.op("tensor", lambda h, pb=pb, t=t, n=n, j=j, b=b: h.matmul(pb[:, :], diag[:, j * 5 + t, :], raw[b][:, n * 512 + t:n * 512 + t + 512], start=(t == 0), stop=(t == 4)),
                      r=[tdg, traw[b]], w=[tp], inc=(t == 4))
        for n in range(4):
            pb = k.psum[4 + n]
            tp = k.tpsum[4 + n]
            ts = slice(n * 512, (n + 1) * 512)
            if j == 2:
                cx.op("scalar", lambda h, pb=pb, ts=ts: h.activation(vT[:, ts], pb[:, :], AF.Silu), r=[tp], w=[tvT])
            else:
                cx.op("scalar", lambda h, pb=pb, ts=ts, act=act: h.activation(act[:, ts], pb[:, :], AF.Silu), r=[tp], w=[tactn[n]])
        if j < 2 and ALV > 2:
            for n in range(4):
                ts = slice(n * 512, (n + 1) * 512)
                cx.op("scalar", lambda h, ts=ts, act=act: h.activation(sqb[:, ts], act[:, ts], AF.Square), r=[tactn[n]], w=[tsqn[n]])
            for n in range(4):
                ts = slice(n * 512, (n + 1) * 512)
                pb2 = k.psum[n]
                tp2 = k.tpsum[n]
                cx.op("tensor", lambda h, pb2=pb2, ts=ts: h.matmul(pb2[:, :], k.onesb, sqb[:, ts], start=True, stop=True), r=[tsqn[n]], w=[tp2])
            for n in range(4):
                pb2 = k.psum[n]
                tp2 = k.tpsum[n]
                cx.op("scalar", lambda h, pb2=pb2, n=n: h.activation(rn[n], pb2[:, :], AF.Sqrt, bias=k.epsc), r=[tp2], w=[trn[n]])
            for n in range(4):
                cx.op(V, lambda h, n=n: h.reciprocal(rn[n], rn[n]), r=[trn[n]], w=[trn[n]])
            dstT, tdst, scl = (qnT, tq, 128.0 ** -0.5) if j == 0 else (knT, tk_, 1.0)
            for n in range(4):
                ts = slice(n * 512, (n + 1) * 512)
                cx.op(V, lambda h, ts=ts, n=n, dstT=dstT, scl=scl, act=act: h.scalar_tensor_tensor(out=dstT[:, ts], in0=act[:, ts], scalar=scl, in1=rn[n], op0=ALU.mult, op1=ALU.mult),
                      r=[tactn[n], trn[n]], w=[tdst])
    if ALV <= 3:
        cx.barrier(); ar.release(m0); return
    TV = int(os.environ.get("GDN_TV", "0"))
    for i in range(NT):
        pb = k.psum[i % 2].bitcast(BF16)
        tp = k.tpsum[i % 2]
        if TV == 0:
            cx.op("tensor", lambda h, pb=pb, i=i: h.transpose(pb[:, 0:128], knT[:, i * 128:(i + 1) * 128], k.identb), r=[tk_], w=[tp])
            cx.op("tensor", lambda h, pb=pb, i=i: h.transpose(pb[:, 128:256], vT[:, i * 128:(i + 1) * 128], k.identb), r=[tvT], w=[tp])
            cx.op("scalar", lambda h, pb=pb, i=i: h.copy(Ktok[:, i, :], pb[:, 0:128]), r=[tp], w=[tKt])
            cx.op(V, lambda h, pb=pb, i=i: h.tensor_copy(Vtok[:, i, :], pb[:, 128:256]), r=[tp], w=[tVt])
        elif TV == 1:
            cx.op("tensor", lambda h, pb=pb, i=i: h.transpose(pb[:, 0:128], knT[:, i * 128:(i + 1) * 128], k.identb), r=[tk_], w=[tp])
            cx.op("scalar", lambda h, pb=pb, i=i: h.copy(Ktok[:, i, :], pb[:, 0:128]), r=[tp], w=[tKt])
        elif TV == 2:
            cx.op("tensor", lambda h, pb=pb, i=i: h.transpose(pb[:, 0:128], vT[:, i * 128:(i + 1) * 128], k.identb), r=[tvT], w=[tp])
            cx.op(V, lambda h, pb=pb, i=i: h.tensor_copy(Vtok[:, i, :], pb[:, 0:128]), r=[tp], w=[tVt])
    if hd == 0:
        k.dbg_add("gdn_qn", qnT, [tq])
        k.dbg_add("gdn_kn", knT, [tk_])
        k.dbg_add("gdn_vtok", Vtok, [tVt])
    cx.barrier()
    ar.release(mA)
    STOP = os.environ.get("GDN_STOP", "")
    if STOP == "A":
        ar.release(m0)
        return
    qgT = [ar.alloc([2048], BF16) for _ in range(2)]
    Kd = [ar.alloc([16, 128], BF16) for _ in range(2)]
    Pm = [ar.alloc([16, 128], BF16) for _ in range(2)]
    QKm = [ar.alloc([16, 128], BF16) for _ in range(2)]
    etot = [ar.alloc([32], F32) for _ in range(2)]
    WnT = [ar.alloc([2048], BF16) for _ in range(2)]
    U0b = [ar.alloc([16, 128], BF16) for _ in range(2)]
    tWn = [[Trk() for _ in range(NT)] for _ in range(2)]
    tU0 = [[Trk() for _ in range(NT)] for _ in range(2)]
    tqg = [[Trk() for _ in range(NT)] for _ in range(2)]
    tKd = [[Trk() for _ in range(NT)] for _ in range(2)]
    tPm = [[Trk() for _ in range(NT)] for _ in range(2)]
    tQK = [[Trk() for _ in range(NT)] for _ in range(2)]
    tet = [[Trk() for _ in range(NT)] for _ in range(2)]
    NI = 8
    mN = ar.mark()
    NDT = BF16 if os.environ.get('GDN_NEU', 'bf16') == 'bf16' else F32
    nid = k.identb if NDT == BF16 else k.ident
    Xb = [[ar.alloc([128], NDT) for _ in range(2)] for _ in range(NI)]
    XTb = [[ar.alloc([128], NDT) for _ in range(2)] for _ in range(NI)]
    Pb = [[ar.alloc([128], NDT) for _ in range(2)] for _ in range(NI)]

    def tview(pn_, c0):
        return pn_[:, c0:c0 + 128] if NDT == F32 else pn_.bitcast(BF16)[:, 2 * c0:2 * c0 + 128]
    tX = [Trk() for _ in range(NI)]
    EGB = [ar.alloc([128], F32) for _ in range(NI)]
    ET = [ar.alloc([128], BF16) for _ in range(NI)]
    ETs = ET
    tE = [Trk() for _ in range(NI)]
    tEG = [Trk() for _ in range(NI)]
    Kg = [ar.alloc([128], BF16) for _ in range(NI)]
    tKg = [Trk() for _ in range(NI)]
    tXP = [Trk() for _ in range(NI)]
    insts = [(i, d) for i in range(NT) for d in range(2)]
    for g0 in range(0, len(insts), NI):
        grp = insts[g0:g0 + NI]
        info = []
        for s_, (i, d) in enumerate(grp):
            info.append(dict(s_=s_, i=i, d=d, tsl=slice(i * 128, (i + 1) * 128),
                             col=pap(G.g[d], 0, 128, i * 4 + hd, [[0, 128]]),
                             gcc=G.gc[d][:, i, hd:hd + 1], nbc=G.nb[d][:, i, hd:hd + 1], edc=G.ed[d][:, i, hd:hd + 1],
                             pb=k.psum[s_], tp=k.tpsum[s_]))
        for q_ in info:
            s_, i, d, tsl, col, pb, tp = q_["s_"], q_["i"], q_["d"], q_["tsl"], q_["col"], q_["pb"], q_["tp"]
            cx.op("tensor", lambda h, pb=pb, col=col, d=d: h.matmul(pb[:, 0:128], col, G.mask[:, d, :], start=True, stop=True), r=[G.T, G.tmask], w=[tp], inc=False)
            cx.op("tensor", lambda h, pb=pb, tsl=tsl: h.matmul(pb[:, 128:256], knT[:, tsl], knT[:, tsl], start=True, stop=True), r=[tk_], w=[tp], inc=False)
            cx.op("tensor", lambda h, pb=pb, tsl=tsl: h.matmul(pb[:, 256:384], knT[:, tsl], qnT[:, tsl], start=True, stop=True), r=[tk_, tq], w=[tp])
        for q_ in info:
            s_, i, d, pb, tp, gcc = q_["s_"], q_["i"], q_["d"], q_["pb"], q_["tp"], q_["gcc"]
            cx.op("scalar", lambda h, pb=pb, s_=s_: h.activation(EGB[s_], pb[:, 0:128], AF.Exp), r=[tp], w=[tEG[s_]])
            cx.op(V, lambda h, pb=pb, s_=s_, gcc=gcc, d=d: h.scalar_tensor_tensor(out=ET[s_], in0=pb[:, 0:128], scalar=gcc, in1=G.mask[:, 2 + d, :], op0=ALU.subtract, op1=ALU.min),
                  r=[tp, G.T, G.tmask], w=[tE[s_]])
        for q_ in info:
            s_, i, d, tsl = q_["s_"], q_["i"], q_["d"], q_["tsl"]
            cx.op("scalar", lambda h, s_=s_: h.activation(ET[s_], ET[s_], AF.Exp), r=[tE[s_]], w=[tE[s_]])
            cx.op(V, lambda h, s_=s_, tsl=tsl, d=d: h.tensor_tensor(out=qgT[d][:, tsl], in0=qnT[:, tsl], in1=EGB[s_], op=ALU.mult), r=[tq, tEG[s_]], w=[tqg[d][i]])
        for q_ in info:
            s_, i, d, pb, tp, edc = q_["s_"], q_["i"], q_["d"], q_["pb"], q_["tp"], q_["edc"]
            c0, c1 = (63, 127) if d == 0 else (0, 64)
            cx.op("scalar", lambda h, s_=s_, d=d, i=i, c0=c0: h.copy(etot[d][:, 2 * i:2 * i + 1], EGB[s_][:, c0:c0 + 1]), r=[tEG[s_]], w=[tet[d][i]])
            cx.op("scalar", lambda h, s_=s_, d=d, i=i, c1=c1: h.copy(etot[d][:, 2 * i + 1:2 * i + 2], EGB[s_][:, c1:c1 + 1]), r=[tEG[s_]], w=[tet[d][i]])
            cx.op("scalar", lambda h, d=d, i=i, edc=edc: h.activation(Kd[d][:, i, :], Ktok[:, i, :], AF.Identity, scale=edc), r=[tKt, G.T], w=[tKd[d][i]])
            egc = G.eg[d][:, i, hd:hd + 1]
            cx.op("scalar", lambda h, s_=s_, i=i, egc=egc: h.activation(Kg[s_], Ktok[:, i, :], AF.Identity, scale=egc), r=[tKt, G.T], w=[tKg[s_]])
            cx.op(V, lambda h, pb=pb, s_=s_, d=d, i=i: h.tensor_tensor(out=QKm[d][:, i, :], in0=pb[:, 256:384], in1=ET[s_], op=ALU.mult), r=[tp, tE[s_]], w=[tQK[d][i]])
        for q_ in info:
            s_, d = q_["s_"], q_["d"]
            cx.op(V, lambda h, s_=s_, d=d: h.tensor_tensor(out=ETs[s_], in0=ET[s_], in1=G.mask[:, 4 + d, :], op=ALU.mult), r=[tE[s_], G.tmask], w=[tE[s_]])
        for q_ in info:
            s_, pb, tp, nbc = q_["s_"], q_["pb"], q_["tp"], q_["nbc"]
            cx.op(V, lambda h, pb=pb, s_=s_, nbc=nbc: h.scalar_tensor_tensor(out=Xb[s_][0], in0=pb[:, 128:256], scalar=nbc, in1=ETs[s_], op0=ALU.mult, op1=ALU.mult),
                  r=[tp, tE[s_], G.T], w=[tX[s_]])
        for q_ in info:
            s_, pn, tn = q_["s_"], q_["pb"], q_["tp"]
            cx.op("tensor", lambda h, pn=pn, s_=s_: h.transpose(tview(pn, 384), Xb[s_][0], nid), r=[tX[s_]], w=[tn])
            cx.op(V, lambda h, s_=s_: h.tensor_tensor(out=Pb[s_][0], in0=Xb[s_][0], in1=nid, op=ALU.add), r=[tX[s_]], w=[tXP[s_]])
            cx.op("scalar", lambda h, pn=pn, s_=s_: h.copy(XTb[s_][0], tview(pn, 384)), r=[tn], w=[tX[s_]])
        for L in range(1, 6):
            a, b_ = (L - 1) % 2, L % 2
            for s_, (i, d) in enumerate(grp):
                pn = k.psum[s_]
                tn = k.tpsum[s_]
                if L < 5:
                    cx.op("tensor", lambda h, pn=pn, s_=s_, a=a: h.matmul(pn[:, 0:128], XTb[s_][a], Xb[s_][a], start=True, stop=True), r=[tX[s_]], w=[tn], inc=False)
                cx.op("tensor", lambda h, pn=pn, s_=s_, a=a: h.matmul(pn[:, 128:256], Xb[s_][a], XTb[s_][a], start=True, stop=True), r=[tX[s_]], w=[tn])
                e1, e2 = ("scalar", V) if s_ % 2 == 0 else (V, "scalar")
                if L < 5:
                    if e1 == "scalar":
                        cx.op("scalar", lambda h, pn=pn, s_=s_, b_=b_: h.copy(Xb[s_][b_], pn[:, 0:128]), r=[tn], w=[tX[s_]])
                    else:
                        cx.op(V, lambda h, pn=pn, s_=s_, b_=b_: h.tensor_copy(Xb[s_][b_], pn[:, 0:128]), r=[tn], w=[tX[s_]])
                if e2 == "scalar":
                    cx.op("scalar", lambda h, pn=pn, s_=s_, b_=b_: h.copy(XTb[s_][b_], pn[:, 128:256]), r=[tn], w=[tX[s_]])
                else:
                    cx.op(V, lambda h, pn=pn, s_=s_, b_=b_: h.tensor_copy(XTb[s_][b_], pn[:, 128:256]), r=[tn], w=[tX[s_]])
            for s_, (i, d) in enumerate(grp):
                pn = k.psum[s_]
                tn = k.tpsum[s_]
                cx.op("tensor", lambda h, pn=pn, s_=s_, a=a: h.matmul(pn[:, 256:384], nid, Pb[s_][a], start=True, stop=False), r=[tXP[s_]], w=[tn], inc=False)
                cx.op("tensor", lambda h, pn=pn, s_=s_, a=a, b_=b_: h.matmul(pn[:, 256:384], XTb[s_][b_], Pb[s_][a], start=False, stop=True), r=[tX[s_], tXP[s_]], w=[tn])
                use_act = (s_ + L) % 2 == 0
                if L < 5:
                    if use_act:
                        cx.op("scalar", lambda h, pn=pn, s_=s_, b_=b_: h.copy(Pb[s_][b_], pn[:, 256:384]), r=[tn], w=[tXP[s_]])
                    else:
                        cx.op(V, lambda h, pn=pn, s_=s_, b_=b_: h.tensor_copy(Pb[s_][b_], pn[:, 256:384]), r=[tn], w=[tXP[s_]])
                else:
                    if use_act:
                        cx.op("scalar", lambda h, pn=pn, d=d, i=i: h.copy(Pm[d][:, i, :], pn[:, 256:384]), r=[tn], w=[tPm[d][i], tXP[s_]])
                    else:
                        cx.op(V, lambda h, pn=pn, d=d, i=i: h.tensor_copy(Pm[d][:, i, :], pn[:, 256:384]), r=[tn], w=[tPm[d][i], tXP[s_]])
        for s_, (i, d) in enumerate(grp):
            pn = k.psum[s_]
            tn = k.tpsum[s_]
            tsl = slice(i * 128, (i + 1) * 128)
            cx.op("tensor", lambda h, pn=pn, s_=s_, d=d, i=i: h.matmul(pn[:, 0:128], Kg[s_], Pm[d][:, i, :], start=True, stop=True), r=[tKg[s_], tPm[d][i]], w=[tn], inc=False)
            cx.op("tensor", lambda h, pn=pn, d=d, i=i: h.matmul(pn[:, 128:256], Pm[d][:, i, :], Vtok[:, i, :], start=True, stop=True), r=[tPm[d][i], tVt], w=[tn])
            btc_ = G.beta[d][:, i, hd:hd + 1]
            cx.op(V, lambda h, pn=pn, d=d, tsl=tsl: h.tensor_scalar(WnT[d][:, tsl], pn[:, 0:128], -1.0, None, op0=ALU.mult), r=[tn], w=[tWn[d][i]])
            cx.op("scalar", lambda h, pn=pn, d=d, i=i, btc_=btc_: h.activation(U0b[d][:, i, :], pn[:, 128:256], AF.Identity, scale=btc_), r=[tn, G.T], w=[tU0[d][i]])
    if STOP == "B":
        cx.barrier()
        ar.release(m0)
        return
    ar.release(mN)
    Sf = [[ar.alloc([128], F32) for _ in range(2)] for _ in range(2)]
    Sb = [ar.alloc([128], BF16) for _ in range(2)]
    Rp = [ar.alloc([128], BF16) for _ in range(2)]
    vn = [ar.alloc([128], BF16) for _ in range(2)]
    tS = [Trk(), Trk()]
    tSf = [Trk(), Trk()]
    tR = [Trk(), Trk()]
    tv = [Trk(), Trk()]
    cx.op("gpsimd", lambda h: h.memset(G.osum, 0.0), w=G.tosum)
    for d in range(2):
        cx.op("gpsimd", lambda h, d=d: h.memset(Sf[d][0], 0.0), w=[tS[d]])
        cx.op("gpsimd", lambda h, d=d: h.memset(Sb[d], 0.0), w=[tS[d]])
        cx.op("gpsimd", lambda h, d=d: h.memset(Rp[d], 0.0), w=[tR[d]])
        cx.op("gpsimd", lambda h, d=d: h.memset(vn[d], 0.0), w=[tv[d]])
    for step in range(32):
        for d in range(2):
            if d == 0:
                i, hh = step // 2, step % 2
            else:
                i, hh = 15 - step // 2, 1 - step % 2
            tsl = slice(i * 128, (i + 1) * 128)
            ps_ = slice(hh * 64, (hh + 1) * 64)
            cur, nxt = step % 2, (step + 1) % 2
            pcs = [k.psum[4 * d + q_] for q_ in range(4)]
            tcs = [k.tpsum[4 * d + q_] for q_ in range(4)]
            negc = G.neg[d][ps_, i, hd:hd + 1]
            btc = G.beta[d][ps_, i, hd:hd + 1]
            p1, pv_, po_, pst = pcs
            t1_, tv_, to_, tst = tcs
            cx.op("tensor", lambda h, p1=p1, tsl=tsl, d=d: h.matmul(p1[:, 0:128], WnT[d][:, tsl], Sb[d], start=True, stop=True), r=[tWn[d][i], tS[d]], w=[t1_])
            cx.op(V, lambda h, p1=p1, ps_=ps_, btc=btc, d=d, i=i: h.scalar_tensor_tensor(out=vn[d][ps_, :], in0=p1[ps_, 0:128], scalar=btc, in1=U0b[d][ps_, i, :], op0=ALU.mult, op1=ALU.add),
                  r=[t1_, tU0[d][i], G.T], w=[tv[d]])
            cx.op("tensor", lambda h, po_=po_, tsl=tsl, d=d: h.matmul(po_[:, 0:128], qgT[d][:, tsl], Sb[d], start=True, stop=False), r=[tqg[d][i], tS[d]], w=[to_], inc=False)
            cx.op("tensor", lambda h, po_=po_, ps_=ps_, d=d, i=i: h.matmul(po_[:, 0:128], QKm[d][ps_, i, :], vn[d][ps_, :], start=False, stop=True), r=[tQK[d][i], tv[d]], w=[to_])
            cx.op("tensor", lambda h, pst=pst, ps_=ps_, d=d, i=i: h.matmul(pst[:, 0:128], Kd[d][ps_, i, :], vn[d][ps_, :], start=True, stop=True), r=[tKd[d][i], tv[d]], w=[tst])
            cx.op("gpsimd" if False else V, lambda h, po_=po_, ps_=ps_, i=i: h.tensor_tensor(out=G.osum[ps_, i, :], in0=po_[ps_, 0:128], in1=G.osum[ps_, i, :], op=ALU.add), r=[to_, G.tosum[i]], w=[G.tosum[i]])
            etc = etot[d][:, 2 * i + hh:2 * i + hh + 1]
            cx.op(V, lambda h, pst=pst, d=d, cur=cur, nxt=nxt, etc=etc: h.scalar_tensor_tensor(out=Sf[d][nxt], in0=Sf[d][cur], scalar=etc, in1=pst[:, 0:128], op0=ALU.mult, op1=ALU.add),
                  r=[tst, tet[d][i], tS[d]], w=[tS[d]])
            cx.op("scalar", lambda h, d=d, nxt=nxt: h.copy(Sb[d], Sf[d][nxt]), r=[tS[d]], w=[tS[d]])
    if hd == 0:
        k.dbg_add("gdn_osum", G.osum, G.tosum)
    if STOP == "C":
        cx.barrier()
        ar.release(m0)
        return
    ss = ar.alloc([NT, 2], F32)
    tss = Trk()
    junk = ar.alloc([128], BF16)
    zs = [ar.alloc([128], F32) for _ in range(2)]
    tzs = [Trk(), Trk()]
    yb = [ar.alloc([128], BF16) for _ in range(2)]
    tyb = [Trk(), Trk()]
    for i in range(NT):
        cx.op("scalar", lambda h, i=i: h.activation(junk, G.osum[:, i, :], AF.Square, accum_out=ss[:, i, 0:1]), r=[G.tosum[i], tss], w=[tss])
    cx.op(V, lambda h: h.tensor_scalar(ss[:, :, 1:2], ss[:, :, 0:1], 1.0 / 128, EPS, op0=ALU.mult, op1=ALU.add), r=[tss], w=[tss])
    cx.op("scalar", lambda h: h.activation(ss[:, :, 1:2], ss[:, :, 1:2], AF.Sqrt), r=[tss], w=[tss])
    cx.op(V, lambda h: h.reciprocal(ss[:, :, 1:2], ss[:, :, 1:2]), r=[tss], w=[tss])
    for i in range(NT):
        b = i % 2
        pz = k.psum[b]
        tz = k.tpsum[b]
        for c in range(8):
            cx.op("tensor", lambda h, pz=pz, c=c, i=i: h.matmul(pz[:, 0:128], k.hT[:, c, i * 128:(i + 1) * 128], wz[:, c, :], start=(c == 0), stop=(c == 7)),
                  r=[twz, k.t_hT], w=[tz], inc=(c == 7))
        cx.op("scalar", lambda h, pz=pz, b=b: h.activation(zs[b], pz[:, 0:128], AF.Silu), r=[tz], w=[tzs[b]])
        s1 = ss[:, i, 1:2]
        cx.op(V, lambda h, i=i, s1=s1: h.scalar_tensor_tensor(out=G.osum[:, i, :], in0=G.osum[:, i, :], scalar=s1, in1=G.nw, op0=ALU.mult, op1=ALU.mult), r=[tss, G.tpr, G.tosum[i]], w=[G.tosum[i]])
        cx.op(V, lambda h, i=i, b=b: h.tensor_tensor(out=yb[b], in0=G.osum[:, i, :], in1=zs[b], op=ALU.mult), r=[G.tosum[i], tzs[b]], w=[tyb[b]])
        pt = k.psum[2 + b].bitcast(BF16)
        tt_ = k.tpsum[2 + b]
        cx.op("tensor", lambda h, pt=pt, b=b: h.transpose(pt[:, 0:128], yb[b], k.identb), r=[tyb[b]], w=[tt_])
        cx.op("scalar", lambda h, pt=pt, i=i: h.copy(yT[:, 4 + hd, i * 128:(i + 1) * 128], pt[:, 0:128]), r=[tt_], w=[t_yT])
    cx.barrier()
    ar.release(m0)


def out_proj(k, yT, t_yT):
    cx, ar, D = k.cx, k.ar, k.D
    m0 = ar.mark()
    wo = ar.alloc([8, 1024], BF16)
    two = Trk()
    wsrc = D["w_out"]
    sl_ = cx.fresh('sw')
    for c in range(8):
        cx.dma("gpsimd", wo[:, c, :], wsrc[c * 128:(c + 1) * 128, :], sl_, w=[two])
    for g4 in range(4):
        slx = cx.fresh()
        for i in range(g4 * 4, g4 * 4 + 4):
            cx.dma("sync", k.xacc[:, i, :], D["x"][i * 128:(i + 1) * 128, :], slx, w=[k.txacc[i]])
        for i in range(g4 * 4, g4 * 4 + 4):
            k.txacc[i].w = (slx.key, slx.total)
    for i in range(NT):
        for half in range(2):
            pb = k.psum[(2 * i + half) % 4]
            tp = k.tpsum[(2 * i + half) % 4]
            for c in range(8):
                cx.op("tensor", lambda h, pb=pb, c=c, i=i, half=half: h.matmul(pb[:, :], yT[:, c, i * 128:(i + 1) * 128], wo[:, c, half * 512:(half + 1) * 512], start=(c == 0), stop=(c == 7)),
                      r=[t_yT, two], w=[tp], inc=(c == 7))
            xs = k.xacc[:, i, half * 512:(half + 1) * 512]
            cx.op("vector", lambda h, pb=pb, xs=xs: h.tensor_tensor(out=xs, in0=pb[:, :], in1=xs, op=ALU.add), r=[tp, k.txacc[i]], w=[k.txacc[i]])
    cx.barrier()
    ar.release(m0)


def xattn(k):
    cx, ar, D = k.cx, k.ar, k.D
    V = "vector"
    m0 = ar.mark()
    wq = [ar.alloc([8, 256], BF16) for _ in range(2)]
    wk = [ar.alloc([8, 256], BF16) for _ in range(2)]
    wv = [ar.alloc([8, 256], BF16) for _ in range(2)]
    wo = [ar.alloc([2, 1024], BF16) for _ in range(2)]
    twA = [Trk(), Trk()]
    two_ = [Trk(), Trk()]
    swA = [cx.slot("xwa0"), cx.slot("xwa1")]
    swO = [cx.slot("xwo0"), cx.slot("xwo1")]
    def loadA(hd):
        b = hd % 2
        c0 = hd * 256
        for (dst, nm) in ((wq[b], "xa_wq"), (wk[b], "xa_wk"), (wv[b], "xa_wv")):
            load_w_cols(k, D[nm], c0, 256, dst, twA[b], swA[b])

    def loadO(hd):
        b = hd % 2
        c0 = hd * 256
        src = D["xa_wo"]
        cx.dma("gpsimd", wo[b], bass.AP(src.tensor, src.offset + c0 * 1024, [[1024, 128], [128 * 1024, 2], [1, 1024]]), swO[b], w=[two_[b]])

    loadA(0)
    loadA(1)
    loadO(0)
    loadO(1)
    xnT = ar.alloc([8, 2048], BF16)
    t_xnT = Trk()
    memT = ar.alloc([8, 256], BF16)
    t_memT = Trk()
    m1 = ar.mark()
    mt = [ar.alloc([1024], F32) for _ in range(2)]
    tmt = [Trk(), Trk()]

    def src_mem(i):
        cx.dma("sync", mt[i], D["mem"][i * 128:(i + 1) * 128, :], cx.fresh(), w=[tmt[i]])
        return mt[i], tmt[i]
    norm_transpose(k, "mem", src_mem, 2, D["norm_mem"], memT, BF16, t_memT)
    ar.release(m1)
    norm_transpose(k, "xa", lambda i: (k.xacc[:, i, :], k.txacc[i]), NT, D["norm_xattn"], xnT, BF16, t_xnT, resident=True)
    k.dbg_add("xa_memT", memT, [t_memT])
    k.dbg_add("xa_xnT", xnT, [t_xnT])
    kTb = [ar.alloc([2, 256], BF16) for _ in range(2)]
    vhb = [ar.alloc([2, 256], BF16) for _ in range(2)]
    tkvb = [Trk(), Trk()]
    qTb = [ar.alloc([2, 2048], BF16) for _ in range(2)]
    tqTb = [Trk(), Trk()]
    E = [ar.alloc([2, 512], BF16) for _ in range(2)]
    tE = [Trk(), Trk()]
    rden = [ar.alloc([512], F32) for _ in range(2)]
    trd = [Trk(), Trk()]
    oTn = [ar.alloc([2, 512], BF16) for _ in range(2)]
    toT = [Trk(), Trk()]
    cx.barrier()
    k.txa = [[Trk(), Trk()] for _ in range(NT)]

    def proj(hd):
        b = hd % 2
        kT, vh, qT, tkv, tqT = kTb[b], vhb[b], qTb[b], tkvb[b], tqTb[b]
        for dc in range(2):
            pb = k.psum[dc]
            tp = k.tpsum[dc]
            for c in range(8):
                cx.op("tensor", lambda h, pb=pb, c=c, dc=dc, b=b: h.matmul(pb[:, 0:256], wk[b][:, c, dc * 128:(dc + 1) * 128], memT[:, c, :], start=(c == 0), stop=(c == 7)),
                      r=[twA[b], t_memT], w=[tp], inc=(c == 7))
            cx.op("scalar", lambda h, pb=pb, dc=dc, kT=kT: h.copy(kT[:, dc, :], pb[:, 0:256]), r=[tp], w=[tkv])
        for mtile in range(2):
            pb = k.psum[2 + mtile]
            tp = k.tpsum[2 + mtile]
            for c in range(8):
                cx.op("tensor", lambda h, pb=pb, c=c, mtile=mtile, b=b: h.matmul(pb[:, 0:256], memT[:, c, mtile * 128:(mtile + 1) * 128], wv[b][:, c, :], start=(c == 0), stop=(c == 7)),
                      r=[twA[b], t_memT], w=[tp], inc=(c == 7))
            cx.op(V, lambda h, pb=pb, mtile=mtile, vh=vh: h.tensor_copy(vh[:, mtile, :], pb[:, 0:256]), r=[tp], w=[tkv])
        for dc in range(2):
            for n in range(4):
                pb = k.psum[(dc * 4 + n) % 4]
                tp = k.tpsum[(dc * 4 + n) % 4]
                for c in range(8):
                    cx.op("tensor", lambda h, pb=pb, c=c, dc=dc, n=n, b=b: h.matmul(pb[:, :], wq[b][:, c, dc * 128:(dc + 1) * 128], xnT[:, c, n * 512:(n + 1) * 512], start=(c == 0), stop=(c == 7)),
                          r=[twA[b], t_xnT], w=[tp], inc=(c == 7))
                if n % 2 == 0:
                    cx.op("scalar", lambda h, pb=pb, dc=dc, n=n, qT=qT: h.copy(qT[:, dc, n * 512:(n + 1) * 512], pb[:, :]), r=[tp], w=[tqT])
                else:
                    cx.op(V, lambda h, pb=pb, dc=dc, n=n, qT=qT: h.tensor_copy(qT[:, dc, n * 512:(n + 1) * 512], pb[:, :]), r=[tp], w=[tqT])

    def chunks(hd):
        b = hd % 2
        kT, vh, qT, tkv, tqT = kTb[b], vhb[b], qTb[b], tkvb[b], tqTb[b]
        tw = [two_[0], two_[1]]

        def emit_scores(n):
            eb = n % 2
            ts = slice(n * 512, (n + 1) * 512)
            for mtile in range(2):
                pb = k.psum[mtile]
                tp = k.tpsum[mtile]
                for dc in range(2):
                    cx.op("tensor", lambda h, pb=pb, dc=dc, mtile=mtile, ts=ts: h.matmul(pb[:, :], kT[:, dc, mtile * 128:(mtile + 1) * 128], qT[:, dc, ts], start=(dc == 0), stop=(dc == 1)),
                          r=[tkv, tqT], w=[tp], inc=(dc == 1))
                cx.op("scalar", lambda h, pb=pb, mtile=mtile, eb=eb: h.activation(E[eb][:, mtile, :], pb[:, :], AF.Exp, scale=1.0 / 16.0), r=[tp], w=[tE[eb]])

        def emit_rest(n):
            eb = n % 2
            pd = k.psum[2]
            tpd = k.tpsum[2]
            for mtile in range(2):
                cx.op("tensor", lambda h, pd=pd, mtile=mtile, eb=eb: h.matmul(pd[:, :], k.onesb, E[eb][:, mtile, :], start=(mtile == 0), stop=(mtile == 1)), r=[tE[eb]], w=[tpd], inc=(mtile == 1))
            cx.op(V, lambda h, pd=pd, eb=eb: h.reciprocal(rden[eb], pd[:, :]), r=[tpd], w=[trd[eb]])
            for dc in range(2):
                po = k.psum[3 + dc]
                tpo = k.tpsum[3 + dc]
                for mtile in range(2):
                    cx.op("tensor", lambda h, po=po, mtile=mtile, dc=dc, eb=eb: h.matmul(po[:, :], vh[:, mtile, dc * 128:(dc + 1) * 128], E[eb][:, mtile, :], start=(mtile == 0), stop=(mtile == 1)),
                          r=[tkv, tE[eb]], w=[tpo], inc=(mtile == 1))
                cx.op(V, lambda h, po=po, dc=dc, eb=eb: h.tensor_tensor(out=oTn[eb][:, dc, :], in0=po[:, :], in1=rden[eb], op=ALU.mult), r=[tpo, trd[eb]], w=[toT[eb]])
            for t in range(4):
                i = n * 4 + t
                for half in range(2):
                    pw_ = k.psum[5 + (t * 2 + half) % 3]
                    tpw = k.tpsum[5 + (t * 2 + half) % 3]
                    for dc in range(2):
                        cx.op("tensor", lambda h, pw_=pw_, dc=dc, t=t, half=half, b=b, eb=eb: h.matmul(pw_[:, :], oTn[eb][:, dc, t * 128:(t + 1) * 128], wo[b][:, dc, half * 512:(half + 1) * 512], start=(dc == 0), stop=(dc == 1)),
                              r=[toT[eb], tw[b]], w=[tpw], inc=(dc == 1))
                    xs = k.xacc[:, i, half * 512:(half + 1) * 512]
                    cx.op(V, lambda h, pw_=pw_, xs=xs: h.tensor_tensor(out=xs, in0=pw_[:, :], in1=xs, op=ALU.add), r=[tpw, k.txa[i][half]], w=[k.txa[i][half]])
        emit_scores(0)
        for n in range(4):
            if n + 1 < 4:
                emit_scores(n + 1)
            emit_rest(n)
    proj(0)
    for hd in range(4):
        if hd + 1 < 4:
            proj(hd + 1)
        if hd + 2 < 4:
            loadA(hd + 2)
        chunks(hd)
        if hd + 2 < 4:
            loadO(hd + 2)
    cx.barrier()
    ar.release(m0)


def moe(k):
    cx, ar, D = k.cx, k.ar, k.D
    V = "vector"
    m0 = ar.mark()
    wgu = [ar.alloc([8, 512], BF16) for _ in range(2)]
    wd = [ar.alloc([2, 1024], BF16) for _ in range(2)]
    twe = [Trk(), Trk()]
    swe = [cx.slot("we0"), cx.slot("we1")]
    NE = k.n_experts

    def load_e(e):
        b = e % 2
        g_, u_, d_ = D["moe_w_gate"], D["moe_w_up"], D["moe_w_down"]
        cx.dma("gpsimd", wgu[b][:, :, 0:256], bass.AP(g_.tensor, g_.offset + e * 1024 * 256, [[256, 128], [256 * 128, 8], [1, 256]]), swe[b], w=[twe[b]])
        cx.dma("gpsimd", wgu[b][:, :, 256:512], bass.AP(u_.tensor, u_.offset + e * 1024 * 256, [[256, 128], [256 * 128, 8], [1, 256]]), swe[b], w=[twe[b]])
        cx.dma("gpsimd", wd[b], bass.AP(d_.tensor, d_.offset + e * 256 * 1024, [[1024, 128], [1024 * 128, 2], [1, 1024]]), swe[b], w=[twe[b]])
    import os
    NOLOAD = os.environ.get("MOE_NOLOAD", "") == "1"
    load_e(0)
    if NE > 1:
        load_e(1)
    xnT = ar.alloc([8, 2048], BF16)
    t_xnT = Trk()
    norm_transpose(k, "moe", lambda i: (k.xacc[:, i, :], k.txacc[i]), NT, D["norm_moe"], xnT, BF16, t_xnT, resident=True)
    wr = ar.alloc([8, 36], BF16)
    twr = Trk()
    sl_ = cx.fresh('sw')
    srcg, srce = D["router_group_w"], D["router_expert_w"]
    cx.dma("gpsimd", wr[:, :, 0:4], bass.AP(srcg.tensor, srcg.offset, [[4, 128], [4 * 128, 8], [1, 4]]), sl_, w=[twr])
    cx.dma("gpsimd", wr[:, :, 4:36], bass.AP(srce.tensor, srce.offset, [[32, 128], [32 * 128, 8], [1, 32]]), sl_, w=[twr])
    rb = ar.alloc([36], F32)
    trb = Trk()
    sl2 = cx.fresh()
    cx.dma("sync", rb[:, 0:4], dram_bcast(D["router_group_b"], 128, 4), sl2, w=[trb])
    cx.dma("sync", rb[:, 4:36], dram_bcast(D["router_expert_b"], 128, 32), sl2, w=[trb])
    cw = ar.alloc([NT, 32], F32)
    tcw = Trk()
    lgA = ar.alloc([NT, 36], F32)
    msk = ar.alloc([NT, 32], F32)
    eq2 = ar.alloc([NT, 32], F32)
    m8 = ar.alloc([NT, 8], F32)
    sc = ar.alloc([8, NT], F32)
    oh = ar.alloc([NT, 4], F32)
    ex = ar.alloc([NT, 4], F32)
    T = Trk()
    rbb = rb.unsqueeze(1).to_broadcast([128, 8, 36])
    for half in range(2):
        pb = k.psum[half]
        tp = k.tpsum[half]
        for ii in range(8):
            i = half * 8 + ii
            for c in range(8):
                cx.op("tensor", lambda h, pb=pb, c=c, i=i, ii=ii: h.matmul(pb[:, ii * 36:(ii + 1) * 36], xnT[:, c, i * 128:(i + 1) * 128], wr[:, c, :], start=(c == 0), stop=(c == 7)),
                      r=[t_xnT, twr], w=[tp], inc=(c == 7))
        cx.op(V, lambda h, pb=pb, half=half: h.tensor_tensor(out=lgA[:, half * 8:(half + 1) * 8, :], in0=pb[:, 0:288].rearrange("p (t e) -> p t e", t=8), in1=rbb, op=ALU.add), r=[tp, trb, T], w=[T])
    lg_g = lgA[:, :, 0:4]
    lg_e = lgA[:, :, 4:36]
    gmax, ngs, ssum, ptop, dm, w1, w2 = [sc[:, j_, :] for j_ in range(7)]

    def vop(fn):
        cx.op(V, fn, r=[T], w=[T])

    def aop(fn):
        cx.op("scalar", fn, r=[T], w=[T])
    b4 = lambda v: v.unsqueeze(2).to_broadcast([128, NT, 4])
    b32 = lambda v: v.unsqueeze(2).to_broadcast([128, NT, 32])
    vop(lambda h: h.tensor_reduce(out=gmax, in_=lg_g, axis=AX.X, op=ALU.max))
    vop(lambda h: h.tensor_tensor(out=oh, in0=lg_g, in1=b4(gmax), op=ALU.is_equal))
    vop(lambda h: h.tensor_tensor(out=ex, in0=lg_g, in1=b4(gmax), op=ALU.subtract))
    aop(lambda h: h.activation(ex, ex, AF.Exp))
    vop(lambda h: h.tensor_reduce(out=ssum, in_=ex, axis=AX.X, op=ALU.add))
    vop(lambda h: h.reciprocal(ptop, ssum))
    vop(lambda h: h.tensor_scalar(oh, oh, -1.0, 1e30, op0=ALU.add, op1=ALU.mult))
    vop(lambda h: h.tensor_tensor(out=msk.rearrange("p t (g e) -> p t g e", g=4), in0=lg_e.rearrange("p t (g e) -> p t g e", g=4),
                                  in1=oh.unsqueeze(3).to_broadcast([128, NT, 4, 8]), op=ALU.add))
    for i in range(NT):
        vop(lambda h, i=i: h.max(out=m8[:, i, :], in_=msk[:, i, :]))
    m1, m2 = m8[:, :, 0], m8[:, :, 1]
    vop(lambda h: h.tensor_tensor(out=dm, in0=m2, in1=m1, op=ALU.subtract))
    aop(lambda h: h.activation(dm, dm, AF.Exp))
    vop(lambda h: h.tensor_scalar(w1, dm, 1.0, None, op0=ALU.add))
    vop(lambda h: h.reciprocal(w1, w1))
    vop(lambda h: h.tensor_tensor(out=w2, in0=dm, in1=w1, op=ALU.mult))
    vop(lambda h: h.tensor_tensor(out=w1, in0=w1, in1=ptop, op=ALU.mult))
    vop(lambda h: h.tensor_tensor(out=w2, in0=w2, in1=ptop, op=ALU.mult))
    vop(lambda h: h.tensor_tensor(out=eq2, in0=msk, in1=b32(m2), op=ALU.is_equal))
    vop(lambda h: h.tensor_tensor(out=eq2, in0=eq2, in1=b32(w2), op=ALU.mult))
    vop(lambda h: h.tensor_tensor(out=msk, in0=msk, in1=b32(m1), op=ALU.is_equal))
    vop(lambda h: h.tensor_tensor(out=msk, in0=msk, in1=b32(w1), op=ALU.mult))
    cx.op(V, lambda h: h.tensor_tensor(out=cw, in0=msk, in1=eq2, op=ALU.add), r=[T], w=[tcw, T])
    k.dbg_add("moe_cw", cw, [tcw])
    sg = [ar.alloc([512], F32) for _ in range(2)]
    tsg = [Trk(), Trk()]
    h1 = [ar.alloc([2, 512], BF16) for _ in range(2)]
    th1 = [Trk(), Trk()]
    jobs = [(e, n) for e in range(NE) for n in range(4)]
    state = {"cnt": 0, "loaded": 0}
    cx.barrier()
    k.txh = [[Trk(), Trk()] for _ in range(NT)]

    def emit_gu(j, fh):
        e, n = jobs[j]
        b = e % 2
        ts = slice(n * 512, (n + 1) * 512)
        hb = j % 2
        pg = k.psum[fh * 2]
        tpg = k.tpsum[fh * 2]
        pu = k.psum[fh * 2 + 1]
        tpu = k.tpsum[fh * 2 + 1]
        for c in range(8):
            cx.op("tensor", lambda h, pg=pg, c=c, fh=fh, ts=ts, b=b: h.matmul(pg[:, :], wgu[b][:, c, fh * 128:(fh + 1) * 128], xnT[:, c, ts], start=(c == 0), stop=(c == 7)),
                  r=[twe[b], t_xnT], w=[tpg], inc=(c == 7))
        for c in range(8):
            cx.op("tensor", lambda h, pu=pu, c=c, fh=fh, ts=ts, b=b: h.matmul(pu[:, :], wgu[b][:, c, 256 + fh * 128:256 + (fh + 1) * 128], xnT[:, c, ts], start=(c == 0), stop=(c == 7)),
                  r=[twe[b], t_xnT], w=[tpu], inc=(c == 7))
        cx.op("scalar", lambda h, pg=pg, fh=fh: h.activation(sg[fh], pg[:, :], AF.Silu), r=[tpg], w=[tsg[fh]])
        cx.op(V, lambda h, pu=pu, fh=fh, hb=hb: h.tensor_tensor(out=h1[hb][:, fh, :], in0=pu[:, :], in1=sg[fh], op=ALU.mult), r=[tpu, tsg[fh]], w=[th1[hb]])

    def emit_down(j):
        e, n = jobs[j]
        b = e % 2
        hb = j % 2
        for t in range(4):
            i = n * 4 + t
            for half in range(2):
                pdn = k.psum[4 + state["cnt"] % 4]
                tpd = k.tpsum[4 + state["cnt"] % 4]
                state["cnt"] += 1
                for fh in range(2):
                    cx.op("tensor", lambda h, pdn=pdn, fh=fh, t=t, half=half, hb=hb, b=b: h.matmul(pdn[:, :], h1[hb][:, fh, t * 128:(t + 1) * 128], wd[b][:, fh, half * 512:(half + 1) * 512], start=(fh == 0), stop=(fh == 1)),
                          r=[th1[hb], twe[b]], w=[tpd], inc=(fh == 1))
                xs = k.xacc[:, i, half * 512:(half + 1) * 512]
                cwc = cw[:, i, e:e + 1]
                cx.op(V, lambda h, pdn=pdn, xs=xs, cwc=cwc: h.scalar_tensor_tensor(out=xs, in0=pdn[:, :], scalar=cwc, in1=xs, op0=ALU.mult, op1=ALU.add), r=[tpd, tcw, k.txh[i][half]], w=[k.txh[i][half]])
        if n == 3 and e + 2 < NE and not NOLOAD:
            load_e(e + 2)
    nj = len(jobs)
    if nj > 0:
        emit_gu(0, 0)
        emit_gu(0, 1)
        for j in range(nj):
            if j + 1 < nj:
                emit_gu(j + 1, 0)
            emit_down(j)
            if j + 1 < nj:
                emit_gu(j + 1, 1)
    cx.barrier()
    ar.release(m0)


def final_norm(k, out):
    cx, ar, D = k.cx, k.ar, k.D
    V = "vector"
    m0 = ar.mark()
    gB = ar.alloc([1024], F32)
    tg = Trk()
    cx.dma("sync", gB, dram_bcast(D["norm_final"], 128, 1024), cx.fresh(), w=[tg])
    junk = ar.alloc([1024], BF16)
    tj = Trk()
    ss = ar.alloc([NT, 2], F32)
    tss = Trk()
    ob = [ar.alloc([1024], F32) for _ in range(2)]
    tob = [Trk(), Trk()]
    so = [cx.slot("o0"), cx.slot("o1")]
    for i in range(NT):
        txs = [k.txacc[i]] + (k.txh[i] if hasattr(k, "txh") else [])
        cx.op("scalar", lambda h, i=i: h.activation(junk, k.xacc[:, i, :], AF.Square, accum_out=ss[:, i, 0:1]), r=txs + [tss], w=[tj, tss])
    cx.op(V, lambda h: h.tensor_scalar(ss[:, :, 1:2], ss[:, :, 0:1], 1.0 / 1024, EPS, op0=ALU.mult, op1=ALU.add), r=[tss], w=[tss])
    cx.op("scalar", lambda h: h.activation(ss[:, :, 1:2], ss[:, :, 1:2], AF.Sqrt), r=[tss], w=[tss])
    cx.op(V, lambda h: h.reciprocal(ss[:, :, 1:2], ss[:, :, 1:2]), r=[tss], w=[tss])
    for i in range(NT):
        b = i % 2
        s1 = ss[:, i, 1:2]
        txs = [k.txacc[i]] + (k.txh[i] if hasattr(k, "txh") else [])
        cx.op(V, lambda h, i=i, s1=s1, b=b: h.scalar_tensor_tensor(out=ob[b], in0=k.xacc[:, i, :], scalar=s1, in1=gB, op0=ALU.mult, op1=ALU.mult), r=txs + [tss, tg], w=[tob[b]])
        cx.dma("sync", out[i * 128:(i + 1) * 128, :], ob[b], so[b], r=[tob[b]])
    cx.barrier()
    ar.release(m0)


_CACHE = {}


def kernel(**inputs):
    inp = {k_: np.asarray(v) for k_, v in inputs.items()}
    n = inp["x"].shape[0]
    maps = [host_inputs(inp, b) for b in range(n)]
    key = "full"
    if key not in _CACHE:
        shapes = {k_: (v.shape, np2dt(v)) for k_, v in maps[0].items()}
        _CACHE[key] = build(shapes)[0]
    nc = _CACHE[key]
    res = run_bass_kernel_spmd(nc, maps, core_ids=list(range(n)))
    return np.stack([np.asarray(r["out"], dtype=np.float32) for r in res.results], 0)
```

```python
import contextlib
import os
import math
import numpy as np
import ml_dtypes
import concourse.bass as bass
import concourse.mybir as mybir
from concourse.bass_utils import run_bass_kernel_spmd

F32 = mybir.dt.float32
BF16 = mybir.dt.bfloat16
F32R = mybir.dt.float32r
I32 = mybir.dt.int32
AF = mybir.ActivationFunctionType
ALU = mybir.AluOpType
AX = mybir.AxisListType

ENGS = ("sync", "scalar", "gpsimd", "vector", "tensor")
ATTACH_WAIT = os.environ.get("ATTACH_WAIT", "1") == "1"
S = 2048
DM = 1024
NT = 16
EPS = 1e-6


class Trk:
    __slots__ = ("name", "w", "r", "excl")

    def __init__(self, name="", excl=False):
        self.name = name
        self.w = None
        self.r = {}
        self.excl = excl


class DmaSlot:
    def __init__(self, ctx, name):
        self.key = "d_" + name + str(ctx.nsem)
        ctx.sems[self.key] = ctx.new_sem(self.key)
        self.total = 0


class Ctx:
    def __init__(self, nc, stack):
        self.nc = nc
        self.stack = stack
        self.q = {e: [] for e in ENGS}
        self.sems = {}
        self.nsem = 0
        self.cnt = {e: 0 for e in ENGS}
        self.known = {e: {} for e in ENGS}
        for e in ENGS:
            self.sems[e] = self.new_sem("s_" + e)
        self.slots = []
        self.pools = {}
        self.pool_idx = {}
        self.n_ops = 0

    def new_sem(self, name):
        self.nsem += 1
        return self.stack.enter_context(self.nc.semaphore(name))

    def slot(self, name):
        s = DmaSlot(self, name)
        self.slots.append(s)
        return s

    def fresh(self, kind="hw"):
        pool = self.pools.setdefault(kind, [])
        i = self.pool_idx.get(kind, 0)
        if i >= len(pool):
            assert len(pool) < 30, "slot pool exhausted"
            pool.append(self.slot(kind + "%d" % len(pool)))
            pool[-1].kind = kind
        self.pool_idx[kind] = i + 1
        return pool[i]

    def sb(self, name, shape, dt):
        return self.stack.enter_context(self.nc.sbuf_tensor("sb_" + name, list(shape), dt))

    def ps(self, name, shape, dt=F32):
        return self.stack.enter_context(self.nc.psum_tensor(name, list(shape), dt))

    def _waits_for(self, eng, r, w, extra=()):
        need = {}

        def req(dep, raw=True):
            if dep is None:
                return
            k, c = dep
            if k == eng and eng in ("tensor", "sync"):
                return
            if k == eng and not raw:
                return
            if c > need.get(k, 0):
                need[k] = c
        for t in r:
            req(t.w)
        for t in w:
            req(t.w, raw=False)
            for k, c in t.r.items():
                req((k, c), raw=False)
        for d in extra:
            req(d)
        out = []
        kn = self.known[eng]
        for k, c in need.items():
            if kn.get(k, 0) < c:
                kn[k] = c
                out.append((self.sems[k], c))
        return out

    def op(self, eng, fn, r=(), w=(), inc=True, extra=()):
        w = list(w) + [t for t in r if t.excl]
        r = [t for t in r if not t.excl]
        waits = self._waits_for(eng, r, w, extra)
        c = self.cnt[eng] + 1
        if inc:
            self.cnt[eng] = c
        sem = self.sems[eng]

        def emit(h, fn=fn, waits=waits, inc=inc, sem=sem):
            for s, v in waits[:-1]:
                h.wait_ge(s, v)
            ins = fn(h)
            if waits:
                if ATTACH_WAIT:
                    ins._wait_ge(waits[-1][0], waits[-1][1])
                else:
                    raise RuntimeError
            if inc:
                ins.then_inc(sem, 1)
        if not ATTACH_WAIT:
            def emit(h, fn=fn, waits=waits, inc=inc, sem=sem):
                for s, v in waits:
                    h.wait_ge(s, v)
                ins = fn(h)
                if inc:
                    ins.then_inc(sem, 1)
        self.q[eng].append(emit)
        for t in r:
            t.r[eng] = c
        for t in w:
            t.w = (eng, c)
            t.r = {}
        self.n_ops += 1

    def dma(self, eng, out, in_, slot, r=(), w=(), extra=(), **kw):
        kind = "sw" if eng == "gpsimd" else "hw"
        assert getattr(slot, "kind", kind) == kind, ("DMA slot kind mismatch", slot.key, eng)
        slot.kind = kind
        waits = self._waits_for(eng, r, w, extra)
        slot.total += 16
        sem = self.sems[slot.key]

        def emit(h, waits=waits, sem=sem, out=out, in_=in_, kw=kw):
            for s, v in waits:
                h.wait_ge(s, v)
            h.dma_start(out=out, in_=in_, **kw).then_inc(sem, 16)
        self.q[eng].append(emit)
        dep = (slot.key, slot.total)
        for t in r:
            t.r[slot.key] = slot.total
        for t in w:
            t.w = dep
            t.r = {}
        self.n_ops += 1
        return dep

    def wait_deps(self, eng, deps):
        waits = self._waits_for(eng, (), (), deps)

        def emit(h, waits=waits):
            for s, v in waits:
                h.wait_ge(s, v)
        self.q[eng].append(emit)

    def barrier(self):
        deps = [(e, self.cnt[e]) for e in ENGS if e != "sync" and self.cnt[e] > 0]
        deps += [(s.key, s.total) for s in self.slots if s.total > 0]
        for e in ENGS:
            self.wait_deps(e, deps)
        self.pool_idx = {}

    def emit_all(self, block):
        q = self.q

        @block.sync
        def _(h):
            for f in q["sync"]:
                f(h)

        @block.scalar
        def _(h):
            for f in q["scalar"]:
                f(h)

        @block.gpsimd
        def _(h):
            for f in q["gpsimd"]:
                f(h)

        @block.vector
        def _(h):
            for f in q["vector"]:
                f(h)

        @block.tensor
        def _(h):
            for f in q["tensor"]:
                f(h)


class Arena:
    def __init__(self, cx, words, base=None):
        self.t = cx.sb("arena", [128, words], F32) if base is None else base
        self.cx = cx
        self.words = words
        self.top = 0

    def mark(self):
        return self.top

    def release(self, m):
        if m != self.top:
            self.cx.barrier()
        self.top = m

    def alloc(self, shape, dt):
        n = int(np.prod(shape))
        w = n if dt in (F32, F32R, I32) else (n + 1) // 2
        w = (w + 1) // 2 * 2
        o = self.top
        self.top += w
        assert self.top <= self.words, ("arena overflow", self.top, self.words)
        v = self.t[:, o:o + w]
        if dt != F32:
            v = v.bitcast(dt)
        v = v[:, 0:n]
        if len(shape) > 1:
            names = " ".join("d%d" % i for i in range(len(shape)))
            v = v.rearrange("p (%s) -> p %s" % (names, names), **{"d%d" % i: shape[i] for i in range(len(shape))})
        return v


def pap(ap, part0, nparts, off, dims):
    base = ap.ap[0][0]
    return bass.AP(ap.tensor, ap.offset + part0 * base + off, [[base, nparts]] + [list(d) for d in dims])


def host_consts():
    c = {}
    c["ident"] = np.eye(128, dtype=np.float32)
    c["identb"] = np.eye(128, dtype=np.float32).astype(ml_dtypes.bfloat16)
    c["ones"] = np.ones((128, 128), np.float32)
    selT = np.zeros((128, 2, 8, 128), np.float32)
    selB = np.zeros((128, 2, 8, 128), np.float32)
    for q in range(4):
        for r in range(32):
            loc, cc = r // 16, r % 16
            for s in range(8):
                selT[q * 32 + r, loc, s, s * 16 + cc] = 1.0
                selB[q * 32 + r, loc, s, s * 16 + cc] = 1.0
    c["selT"] = selT.astype(ml_dtypes.bfloat16)
    c["selB"] = selB.astype(ml_dtypes.bfloat16)
    sidx = np.arange(128) // 16
    c["s5mf"] = (sidx[None, :] >= sidx[:, None]).astype(np.float32)
    c["s5mb"] = (sidx[None, :] <= sidx[:, None]).astype(np.float32)
    c["kvec"] = np.tile((np.arange(16, dtype=np.float32) - 7.0)[None, :], (128, 1))
    k = np.arange(128)[:, None]
    cc = np.arange(128)[None, :]
    same = (k // 64) == (cc // 64)
    gm = np.zeros((128, 8, 128), np.float32)
    gm[:, 0] = same & (k <= cc)
    gm[:, 1] = same & (k >= cc)
    gm[:, 2] = np.where(same & (cc >= k), 0.0, -30000.0)
    gm[:, 3] = np.where(same & (cc <= k), 0.0, -30000.0)
    gm[:, 4] = same & (cc > k)
    gm[:, 5] = same & (cc < k)
    gm[:, 6] = same
    c["gmask"] = gm
    return c


def host_s5(inp):
    o = {}

    pairs = {"lam_re": ("s5_lam_re_f", "s5_lam_re_b"), "lam_im": ("s5_lam_im_f", "s5_lam_im_b"),
             "log_step": ("s5_log_step_f", "s5_log_step_b"), "b_re": ("s5_b_re_f", "s5_b_re_b"),
             "b_im": ("s5_b_im_f", "s5_b_im_b"), "c_re": ("s5_c_re_f", "s5_c_re_b"), "c_im": ("s5_c_im_f", "s5_c_im_b")}

    def st(nm):
        f_, b_ = pairs[nm]
        return np.stack([inp[f_][0], inp[b_][0]], 0)
    lam = np.stack([st("lam_re"), st("lam_im")], 0)
    lam = lam.reshape(2, 2, 2, 16, 64).transpose(2, 4, 0, 1, 3)
    o["s5_lam"] = np.ascontiguousarray(lam.reshape(128, 2, 32))
    ls = st("log_step").reshape(2, 2, 16)
    ls = np.broadcast_to(ls.transpose(1, 0, 2)[:, None], (2, 64, 2, 16))
    o["s5_step"] = np.ascontiguousarray(ls.reshape(128, 32))
    b = np.stack([st("b_re"), st("b_im")], 0)
    b = b.reshape(2, 2, 2, 16, 64, 16).transpose(2, 4, 0, 1, 3, 5)
    o["s5_b"] = np.ascontiguousarray(b.reshape(128, 2, 512))
    cm = np.stack([st("c_re"), st("c_im")], 0)
    cm = cm.reshape(2, 2, 2, 16, 16, 64).transpose(2, 5, 0, 1, 3, 4)
    o["s5_c"] = np.ascontiguousarray(cm.reshape(128, 2, 512))
    d = inp["s5_d"][0].reshape(32, 16)
    o["s5_dvec"] = np.ascontiguousarray(np.broadcast_to(d.T[None], (8, 16, 32)).reshape(128, 32))
    o["s5_bglu"] = np.ascontiguousarray(inp["s5_b_glu"][0].reshape(4, 128).T)
    o["s5_normw"] = np.ascontiguousarray(inp["s5_norm"][0].reshape(4, 128).T)
    return o


def np2dt(a):
    if a.dtype == np.float32:
        return F32
    if a.dtype == ml_dtypes.bfloat16:
        return BF16
    raise ValueError(a.dtype)


class K:
    pass


def dram_bcast(ap, nparts, n, off=0):
    return bass.AP(ap.tensor, ap.offset + off, [[0, nparts], [1, n]])


def norm_transpose(k, name, src_fn, ntiles, gain_dram, outT, out_dt, outT_trk, resident=False):
    cx, ar = k.cx, k.ar
    m = ar.mark()
    gB = ar.alloc([1024], F32)
    tg = Trk()
    cx.dma("sync", gB, dram_bcast(gain_dram, 128, 1024), cx.fresh(), w=[tg])
    junk = ar.alloc([1024], BF16)
    tj = Trk()
    xn = [ar.alloc([1024], out_dt) for _ in range(2)]
    txn = [Trk(), Trk()]
    ss = ar.alloc([NT * 2, 1], F32)
    tss = [Trk() for _ in range(ntiles)]
    pdt = BF16 if out_dt == BF16 else F32
    ident = k.identb if out_dt == BF16 else k.ident
    srcs = []
    if resident:
        for i in range(ntiles):
            src, ts = src_fn(i)
            srcs.append((src, ts))
            cx.op("scalar", lambda h, src=src, i=i: h.activation(junk, src, AF.Square, accum_out=ss[:, 2 * i:2 * i + 1]), r=[ts], w=[tj, tss[0]])
        ssv = ss.rearrange("p (t two) one -> p t (two one)", two=2)
        cx.op("vector", lambda h: h.tensor_scalar(ssv[:, 0:ntiles, 1:2], ssv[:, 0:ntiles, 0:1], 1.0 / 1024, EPS, op0=ALU.mult, op1=ALU.add), r=[tss[0]], w=[tss[0]])
        cx.op("scalar", lambda h: h.activation(ssv[:, 0:ntiles, 1:2], ssv[:, 0:ntiles, 1:2], AF.Sqrt), r=[tss[0]], w=[tss[0]])
        cx.op("vector", lambda h: h.reciprocal(ssv[:, 0:ntiles, 1:2], ssv[:, 0:ntiles, 1:2]), r=[tss[0]], w=[tss[0]])
    for i in range(ntiles):
        rsi = ss[:, 2 * i + 1:2 * i + 2]
        if resident:
            src, ts = srcs[i]
            tsi = tss[0]
        else:
            src, ts = src_fn(i)
            ssi = ss[:, 2 * i:2 * i + 1]
            tsi = tss[i]
            cx.op("scalar", lambda h, src=src, ssi=ssi: h.activation(junk, src, AF.Square, accum_out=ssi), r=[ts], w=[tj, tss[i]])
            cx.op("vector", lambda h, ssi=ssi, rsi=rsi: h.tensor_scalar(rsi, ssi, 1.0 / 1024, EPS, op0=ALU.mult, op1=ALU.add), r=[tss[i]], w=[tss[i]])
            cx.op("scalar", lambda h, rsi=rsi: h.activation(rsi, rsi, AF.Sqrt), r=[tss[i]], w=[tss[i]])
            cx.op("vector", lambda h, rsi=rsi: h.reciprocal(rsi, rsi), r=[tss[i]], w=[tss[i]])
        b = i % 2
        cx.op("vector", lambda h, src=src, rsi=rsi, b=b: h.scalar_tensor_tensor(out=xn[b], in0=src, scalar=rsi, in1=gB, op0=ALU.mult, op1=ALU.mult),
              r=[ts, tsi, tg], w=[txn[b]])
        if out_dt == BF16:
            pb = k.psum[i % 2]
            tp = k.tpsum[i % 2]
            pv = pb.bitcast(BF16)
            for c in range(8):
                cx.op("tensor", lambda h, b=b, c=c, pv=pv: h.transpose(pv[:, c * 128:(c + 1) * 128], xn[b][:, c * 128:(c + 1) * 128], ident),
                      r=[txn[b]], w=[tp], inc=(c == 7))
            dst = outT[:, :, i * 128:(i + 1) * 128]
            eng = "scalar" if i % 2 == 0 else "vector"
            if eng == "scalar":
                cx.op(eng, lambda h, dst=dst, pv=pv: h.copy(dst, pv.rearrange("p (c t) -> p c t", c=8)), r=[tp], w=[outT_trk])
            else:
                cx.op(eng, lambda h, dst=dst, pv=pv: h.tensor_copy(dst, pv.rearrange("p (c t) -> p c t", c=8)), r=[tp], w=[outT_trk])
        else:
            for half in range(2):
                pb = k.psum[(2 * i + half) % 4]
                tp = k.tpsum[(2 * i + half) % 4]
                for c4 in range(4):
                    c = half * 4 + c4
                    cx.op("tensor", lambda h, b=b, c=c, c4=c4, pb=pb: h.transpose(pb[:, c4 * 128:(c4 + 1) * 128], xn[b][:, c * 128:(c + 1) * 128].bitcast(F32), ident),
                          r=[txn[b]], w=[tp], inc=(c4 == 3))
                dst = outT[:, half * 4:(half + 1) * 4, i * 128:(i + 1) * 128]
                if half == 0:
                    cx.op("scalar", lambda h, dst=dst, pb=pb: h.copy(dst, pb.rearrange("p (c t) -> p c t", c=4)), r=[tp], w=[outT_trk])
                else:
                    cx.op("vector", lambda h, dst=dst, pb=pb: h.tensor_copy(dst, pb.rearrange("p (c t) -> p c t", c=4)), r=[tp], w=[outT_trk])
    ar.release(m)


def s5_prep(k):
    cx, ar, D = k.cx, k.ar, k.D
    V = "vector"
    m0 = ar.mark()
    lam = ar.alloc([2, 32], F32)
    step = ar.alloc([32], F32)
    bb = ar.alloc([2, 512], F32)
    cc = ar.alloc([2, 512], F32)
    kvec = ar.alloc([16], F32)
    tl = Trk()
    sl_ = cx.fresh()
    for dst, nm in ((lam, "s5_lam"), (step, "s5_step"), (bb, "s5_b"), (cc, "s5_c"), (kvec, "kvec")):
        cx.dma("sync", dst, D[nm], sl_, w=[tl])
    T = Trk()

    def vop(fn, extra_r=()):
        cx.op(V, fn, r=[T, tl] + list(extra_r), w=[T])

    def aop(fn):
        cx.op("scalar", fn, r=[T, tl], w=[T])
    lre, lim = lam[:, 0, :], lam[:, 1, :]
    dl = ar.alloc([32], F32)
    re1 = ar.alloc([32], F32)
    im1 = ar.alloc([32], F32)
    aop(lambda h: h.activation(dl, step, AF.Exp))
    vop(lambda h: h.tensor_tensor(out=re1, in0=dl, in1=lre, op=ALU.mult))
    vop(lambda h: h.tensor_tensor(out=im1, in0=dl, in1=lim, op=ALU.mult))
    PWI = ar.alloc([16, 32], F32)
    PWR = ar.alloc([16, 32], F32)
    m_pw = ar.mark()
    KR = ar.alloc([16, 32], F32)
    KI = ar.alloc([16, 32], F32)
    kv_b = kvec.unsqueeze(2).to_broadcast([128, 16, 32])
    vop(lambda h: h.tensor_tensor(out=KR, in0=kv_b, in1=re1.unsqueeze(1).to_broadcast([128, 16, 32]), op=ALU.mult))
    vop(lambda h: h.tensor_tensor(out=KI, in0=kv_b, in1=im1.unsqueeze(1).to_broadcast([128, 16, 32]), op=ALU.mult))
    MAG = ar.alloc([16, 32], F32)
    aop(lambda h: h.activation(MAG, KR, AF.Exp))
    YI = ar.alloc([16, 32], I32)
    YF = ar.alloc([16, 32], F32)
    vop(lambda h: h.tensor_scalar(KI, KI, 1.0 / (2 * math.pi), None, op0=ALU.mult))
    vop(lambda h: h.tensor_copy(YI, KI))
    vop(lambda h: h.tensor_copy(YF, YI))
    vop(lambda h: h.tensor_tensor(out=KI, in0=KI, in1=YF, op=ALU.subtract))
    SH_ = ar.alloc([16, 32], F32)
    SQ_ = ar.alloc([16, 32], F32)
    aop(lambda h: h.activation(SH_, KI, AF.Sin, scale=math.pi))
    aop(lambda h: h.activation(SQ_, KI, AF.Sin, scale=math.pi / 2))
    CH_ = ar.alloc([16, 32], F32)
    vop(lambda h: h.tensor_tensor(out=CH_, in0=SQ_, in1=SQ_, op=ALU.mult))
    vop(lambda h: h.tensor_scalar(CH_, CH_, -2.0, 1.0, op0=ALU.mult, op1=ALU.add))
    vop(lambda h: h.tensor_tensor(out=PWI, in0=SH_, in1=CH_, op=ALU.mult))
    vop(lambda h: h.scalar_tensor_tensor(out=PWI, in0=PWI, scalar=2.0, in1=MAG, op0=ALU.mult, op1=ALU.mult))
    vop(lambda h: h.tensor_tensor(out=PWR, in0=SH_, in1=SH_, op=ALU.mult))
    vop(lambda h: h.tensor_scalar(PWR, PWR, -2.0, 1.0, op0=ALU.mult, op1=ALU.add))
    vop(lambda h: h.tensor_tensor(out=PWR, in0=PWR, in1=MAG, op=ALU.mult))
    ar.release(m_pw)
    lrm1 = ar.alloc([32], F32)
    li = PWI[:, 8, :]
    t1 = ar.alloc([32], F32)
    t2 = ar.alloc([32], F32)
    den = ar.alloc([32], F32)
    c0r = ar.alloc([32], F32)
    c0i = ar.alloc([32], F32)
    vop(lambda h: h.tensor_scalar(lrm1, PWR[:, 8, :], -1.0, None, op0=ALU.add))
    vop(lambda h: h.tensor_tensor(out=t1, in0=lre, in1=lre, op=ALU.mult))
    vop(lambda h: h.tensor_tensor(out=t2, in0=lim, in1=lim, op=ALU.mult))
    vop(lambda h: h.tensor_tensor(out=den, in0=t1, in1=t2, op=ALU.add))
    vop(lambda h: h.reciprocal(den, den))
    vop(lambda h: h.tensor_tensor(out=t1, in0=lrm1, in1=lre, op=ALU.mult))
    vop(lambda h: h.tensor_tensor(out=t2, in0=li, in1=lim, op=ALU.mult))
    vop(lambda h: h.tensor_tensor(out=t1, in0=t1, in1=t2, op=ALU.add))
    vop(lambda h: h.tensor_tensor(out=c0r, in0=t1, in1=den, op=ALU.mult))
    vop(lambda h: h.tensor_tensor(out=t1, in0=li, in1=lre, op=ALU.mult))
    vop(lambda h: h.tensor_tensor(out=t2, in0=lrm1, in1=lim, op=ALU.mult))
    vop(lambda h: h.tensor_tensor(out=t1, in0=t1, in1=t2, op=ALU.subtract))
    vop(lambda h: h.tensor_tensor(out=c0i, in0=t1, in1=den, op=ALU.mult))
    BBR = ar.alloc([32, 16], F32)
    BBI = ar.alloc([32, 16], F32)
    TA = ar.alloc([32, 16], F32)
    br = bb[:, 0, :].rearrange("p (a c) -> p a c", c=16)
    bi = bb[:, 1, :].rearrange("p (a c) -> p a c", c=16)
    c0r_b = c0r.unsqueeze(2).to_broadcast([128, 32, 16])
    c0i_b = c0i.unsqueeze(2).to_broadcast([128, 32, 16])
    vop(lambda h: h.tensor_tensor(out=BBR, in0=br, in1=c0r_b, op=ALU.mult))
    vop(lambda h: h.tensor_tensor(out=TA, in0=bi, in1=c0i_b, op=ALU.mult))
    vop(lambda h: h.tensor_tensor(out=BBR, in0=BBR, in1=TA, op=ALU.subtract))
    vop(lambda h: h.tensor_tensor(out=BBI, in0=bi, in1=c0r_b, op=ALU.mult))
    vop(lambda h: h.tensor_tensor(out=TA, in0=br, in1=c0i_b, op=ALU.mult))
    vop(lambda h: h.tensor_tensor(out=BBI, in0=BBI, in1=TA, op=ALU.add))
    ASd = ar.alloc([16, 2, 8, 16], F32)
    CS2d = ar.alloc([16, 2, 8, 16], F32)
    T1 = ar.alloc([8, 16, 16], F32)
    T2 = ar.alloc([8, 16, 16], F32)
    cr = cc[:, 0, :].rearrange("p (d a c) -> p d a c", d=2, c=16)
    ci = cc[:, 1, :].rearrange("p (d a c) -> p d a c", d=2, c=16)
    BBR4 = BBR.rearrange("p (d a) c -> p d a c", d=2)
    BBI4 = BBI.rearrange("p (d a) c -> p d a c", d=2)

    def pw(arr, d, k0, kstep):
        return pap(arr, 0, 128, k0 * 32 + d * 16, [[kstep * 32, 8], [1, 16], [0, 16]])

    def dst(arr, dofs, ri):
        return pap(arr, 0, 128, dofs * 4096 + ri * 128, [[16, 8], [256, 16], [1, 16]])

    def vec(v4, d):
        a_ = v4[:, d]
        return bass.AP(a_.tensor, a_.offset, [list(a_.ap[0]), [0, 8], list(a_.ap[1]), list(a_.ap[2])])

    T1f = T1.rearrange("p a b c -> p (a b c)")

    def cmul(out_arr, dofs, d, k0, kstep, vr, vi, neg_im):
        pr, pi_ = pw(PWR, d, k0, kstep), pw(PWI, d, k0, kstep)
        vop(lambda h: h.tensor_tensor(out=T1, in0=pr, in1=vec(vr, d), op=ALU.mult))
        vop(lambda h: h.tensor_tensor(out=T2, in0=pi_, in1=vec(vi, d), op=ALU.mult))
        vop(lambda h: h.tensor_tensor(out=dst(out_arr, dofs, 0), in0=T1, in1=T2, op=ALU.subtract))
        vop(lambda h: h.tensor_tensor(out=T1, in0=pr, in1=vec(vi, d), op=ALU.mult))
        vop(lambda h: h.tensor_tensor(out=T2, in0=pi_, in1=vec(vr, d), op=ALU.mult))
        if neg_im:
            vop(lambda h: h.tensor_scalar(T1f, T1f, -1.0, None, op0=ALU.mult))
            vop(lambda h: h.tensor_tensor(out=dst(out_arr, dofs, 1), in0=T1, in1=T2, op=ALU.subtract))
        else:
            vop(lambda h: h.tensor_tensor(out=dst(out_arr, dofs, 1), in0=T1, in1=T2, op=ALU.add))
    vop(lambda h: h.tensor_copy(k.s5A1[:, 0:32], PWR[:, 15, :]))
    vop(lambda h: h.tensor_copy(k.s5A1[:, 32:64], PWR[:, 15, :]))
    vop(lambda h: h.tensor_scalar(k.s5A2[:, 0:32], PWI[:, 15, :], -1.0, None, op0=ALU.mult))
    vop(lambda h: h.tensor_copy(k.s5A2[:, 32:64], PWI[:, 15, :]))
    cmul(k.s5CS, 0, 0, 8, 1, cr, ci, True)
    cmul(k.s5CS, 1, 1, 15, -1, cr, ci, True)
    k.t_s5w = T
    mf = ar.alloc([2, 128], F32)
    dv = ar.alloc([32], F32)
    tm = Trk()
    sl_ = cx.fresh()
    cx.dma("sync", mf[:, 0, :], D["s5mf"], sl_, w=[tm])
    cx.dma("sync", mf[:, 1, :], D["s5mb"], sl_, w=[tm])
    cx.dma("sync", dv, D["s5_dvec"], sl_, w=[tm])
    tt1 = [ar.alloc([128], F32) for _ in range(2)]
    ttt = [Trk(), Trk()]
    ASb = ASd.rearrange("p a r s c -> p (a r) (s c)")
    ASm = ASd.rearrange("p a r s c -> p a r (s c)")
    CSm = CS2d.rearrange("p a r s c -> p a r (s c)")
    for d in range(2):
        if d == 0:
            cmul(ASd, 0, 0, 14, -1, BBR4, BBI4, False)
            cmul(CS2d, 0, 0, 0, 1, cr, ci, True)
        else:
            cmul(ASd, 0, 1, 7, 1, BBR4, BBI4, False)
            cmul(CS2d, 0, 1, 7, -1, cr, ci, True)
        for grp in range(8):
            pb = k.psum[grp % 4]
            tp = k.tpsum[grp % 4]
            for j in range(4):
                blk = grp * 4 + j
                cx.op("tensor", lambda h, pb=pb, j=j, blk=blk: h.transpose(pb[:, j * 128:(j + 1) * 128], ASb[:, blk, :], k.ident),
                      r=[T], w=[tp], inc=(j == 3))
            dstv = k.s5AT[:, d * 32 + grp * 4:d * 32 + (grp + 1) * 4, :]
            if grp % 2 == 0:
                cx.op("scalar", lambda h, dstv=dstv, pb=pb: h.copy(dstv, pb.rearrange("p (j x) -> p j x", j=4)), r=[tp], w=[k.t_s5at])
            else:
                cx.op("vector", lambda h, dstv=dstv, pb=pb: h.tensor_copy(dstv, pb.rearrange("p (j x) -> p j x", j=4)), r=[tp], w=[k.t_s5at])
        for g in range(32):
            gh, gl = g // 16, g % 16
            pb = k.psum[4 + g % 4]
            tp = k.tpsum[4 + g % 4]
            for ri in range(2):
                cx.op("tensor", lambda h, pb=pb, ri=ri, gh=gh, gl=gl: h.matmul(
                    pb[:, 0:128], ASm[gh * 64:(gh + 1) * 64, gl, ri, :], CSm[gh * 64:(gh + 1) * 64, gl, ri, :],
                    start=(ri == 0), stop=(ri == 1)), r=[T], w=[tp], inc=(ri == 1))
            b_ = g % 2
            cx.op(V, lambda h, pb=pb, b_=b_, d=d: h.tensor_tensor(out=tt1[b_], in0=pb[:, 0:128], in1=mf[:, d, :], op=ALU.mult), r=[tp, tm], w=[ttt[b_]])
            if d == 0:
                cx.op(V, lambda h, b_=b_, g=g: h.scalar_tensor_tensor(out=k.s5TT[:, g, :], in0=k.ident, scalar=dv[:, g:g + 1], in1=tt1[b_], op0=ALU.mult, op1=ALU.add),
                      r=[ttt[b_], tm], w=[k.t_s5tt])
            else:
                cx.op(V, lambda h, b_=b_, g=g: h.tensor_tensor(out=k.s5TT[:, g, :], in0=k.s5TT[:, g, :], in1=tt1[b_], op=ALU.add),
                      r=[ttt[b_]], w=[k.t_s5tt])
    cx.barrier()
    ar.release(m0)


def load_w_cols(k, wdram, col0, ncols, dst, trk, slot, eng="gpsimd"):
    src = bass.AP(wdram.tensor, wdram.offset + col0, [[wdram.ap[0][0] * 1, 128], [wdram.ap[0][0] * 128, 8], [1, ncols]])
    return k.cx.dma(eng, dst, src, slot, w=[trk])


def proj_fm(k, wt, wtrk, consume):
    cx = k.cx
    for n in range(4):
        pb = k.psum[n % 2 + 2]
        tp = k.tpsum[n % 2 + 2]
        for c in range(8):
            cx.op("tensor", lambda h, pb=pb, c=c, n=n: h.matmul(pb[:, :], wt[:, c, :], k.hT[:, c, n * 512:(n + 1) * 512], start=(c == 0), stop=(c == 7)),
                  r=[wtrk, k.t_hT], w=[tp], inc=(c == 7))
        consume(n, pb, tp)


def s5_build_U(k):
    cx, ar = k.cx, k.ar
    m0 = ar.mark()
    uT = [ar.alloc([2048], BF16) for _ in range(2)]
    tuT = [Trk(), Trk()]
    for ct in range(4):
        b = ct % 2

        def consume(n, pb, tp, b=b):
            dstv = pap(uT[b], 0, 128, n * 64, [[1, 64], [256, 8]])
            srcv = pb[:, :].rearrange("p (j s) -> p j s", s=8)
            if n % 2 == 0:
                cx.op("scalar", lambda h: h.copy(dstv, srcv), r=[tp], w=[tuT[b]])
            else:
                cx.op("vector", lambda h: h.tensor_copy(dstv, srcv), r=[tp], w=[tuT[b]])
        proj_fm(k, k.u_wt[ct], k.t_uwt[ct], consume)
        for gi in range(8):
            g = ct * 8 + gi
            q0 = 32 * (gi // 2)
            pb = k.psum[4 + gi % 4]
            tp = k.tpsum[4 + gi % 4]
            for s in range(8):
                rhs = pap(uT[b], q0, 32, s * 256, [[1, 256]])
                cx.op("tensor", lambda h, pb=pb, s=s, rhs=rhs, q0=q0, gi=gi: h.matmul(pb[:, 0:256], k.selT[q0:q0 + 32, gi % 2, s, :], rhs, start=(s == 0), stop=(s == 7), tile_position=(q0, 0)),
                      r=[tuT[b]], w=[tp], inc=(s == 7))
            if gi % 2 == 0:
                cx.op("scalar", lambda h, pb=pb, g=g: h.copy(k.s5U[:, g, :], pb[:, 0:256]), r=[tp], w=[k.t_s5U])
            else:
                cx.op("vector", lambda h, pb=pb, g=g: h.tensor_copy(k.s5U[:, g, :], pb[:, 0:256]), r=[tp], w=[k.t_s5U])
    cx.barrier()
    ar.release(m0)


def s5_main(k, yT, t_yT):
    cx, ar, D = k.cx, k.ar, k.D
    V = "vector"
    m0 = ar.mark()
    wg = ar.alloc([4, 512], BF16)
    twg = Trk()
    wsrc = D["s5_w_glu"]
    cx.dma("gpsimd", wg, bass.AP(wsrc.tensor, wsrc.offset, [[512, 128], [512 * 128, 4], [1, 512]]), cx.fresh('sw'), w=[twg])
    bgl = ar.alloc([4], F32)
    nw = ar.alloc([4], F32)
    tb = Trk()
    sl_ = cx.fresh()
    cx.dma("sync", bgl, D["s5_bglu"], sl_, w=[tb])
    cx.dma("sync", nw, D["s5_normw"], sl_, w=[tb])
    SH = ar.alloc([2, 257, 2, 16], BF16)
    tSH = Trk()
    tSHh = Trk()
    X = [ar.alloc([64], F32) for _ in range(3)]
    tX = [Trk() for _ in range(3)]
    t1 = ar.alloc([64], F32)
    t2 = ar.alloc([64], F32)
    tt = Trk()
    tt2 = Trk()
    cx.op("gpsimd", lambda h: h.memset(SH[:, 0, 0, :, :], 0.0), w=[tSH])
    cx.op("gpsimd", lambda h: h.memset(SH[:, 1, 256, :, :], 0.0), w=[tSH])
    cx.op("gpsimd", lambda h: h.memset(X[0], 0.0), w=[tX[0]])
    n = 0
    for gl in range(16):
        for d in range(2):
            for ri in range(2):
                blk = d * 32 + gl * 2 + ri
                pb = k.psum[n % 4]
                tp = k.tpsum[n % 4]
                cx.op("tensor", lambda h, pb=pb, blk=blk, gl=gl: h.matmul(pb[0:64, 0:256], k.s5AT[:, blk, 0:64], k.s5U[:, gl, :], start=True, stop=True),
                      r=[k.t_s5at, k.t_s5U], w=[tp], inc=False)
                cx.op("tensor", lambda h, pb=pb, blk=blk, gl=gl: h.matmul(pb[64:128, 0:256], k.s5AT[:, blk, 64:128], k.s5U[:, 16 + gl, :], start=True, stop=True),
                      r=[k.t_s5at, k.t_s5U], w=[tp])
                slot0 = 1 if d == 0 else 0
                dstv = pap(SH, 0, 128, d * 257 * 32 + slot0 * 32 + ri * 16 + gl, [[32, 256]])
                if n % 2 == 0:
                    cx.op("scalar", lambda h, dstv=dstv, pb=pb: h.copy(dstv, pb[:, 0:256]), r=[tp], w=[tSH])
                else:
                    cx.op(V, lambda h, dstv=dstv, pb=pb: h.tensor_copy(dstv, pb[:, 0:256]), r=[tp], w=[tSH])
                n += 1
    import os
    S5STOP = os.environ.get('S5_STOP', '')
    if S5STOP == 'a':
        cx.barrier(); ar.release(m0); return
    for i in range(256):
        xp, xn = X[i % 3], X[(i + 1) % 3]
        txp, txn = tX[i % 3], tX[(i + 1) % 3]
        xsw = pap(xp, 0, 128, 32, [[-32, 2], [1, 32]])
        bf = (i + 1) * 32
        bb_ = 257 * 32 + (255 - i) * 32
        sview = pap(SH, 0, 128, bf, [[16, 2], [bb_ - bf, 2], [1, 16]])
        xp3 = xp.rearrange("p (r x) -> p r x", r=2)
        cx.op("gpsimd", lambda h, xsw=xsw: h.tensor_tensor(out=t2.rearrange("p (r x) -> p r x", r=2), in0=k.s5A2.rearrange("p (r x) -> p r x", r=2), in1=xsw, op=ALU.mult), r=[txp, k.t_s5w], w=[tt2])
        cx.op(V, lambda h, xp=xp: h.tensor_tensor(out=t1, in0=k.s5A1, in1=xp, op=ALU.mult), r=[txp, k.t_s5w], w=[tt])
        cx.op(V, lambda h, sview=sview: h.tensor_tensor(out=t1.rearrange("p (r d x) -> p r d x", r=2, d=2), in0=t1.rearrange("p (r d x) -> p r d x", r=2, d=2), in1=sview, op=ALU.add), r=[tt, tSH], w=[tt])
        cx.op(V, lambda h, xn=xn: h.tensor_tensor(out=xn, in0=t1, in1=t2, op=ALU.add), r=[tt, tt2], w=[txn])
        cx.op("scalar", lambda h, xn=xn, sview=sview: h.copy(sview, xn.rearrange("p (r d x) -> p r d x", r=2, d=2)), r=[txn], w=[tSHh])
    if S5STOP == 'rec':
        cx.barrier(); ar.release(m0); return
    gT = ar.alloc([4, 2048], F32)
    gTb = ar.alloc([4, 2048], BF16)
    tgT = [Trk() for _ in range(4)]
    tgTb = [Trk() for _ in range(4)]
    ybuf = [k.arA.alloc([8, 256], BF16) for _ in range(2)]
    tyb = [Trk(), Trk()]
    for ct in range(4):
        b = ct % 2
        for gi in range(8):
            g = ct * 8 + gi
            gh, gl = g // 16, g % 16
            pb = k.psum[gi % 2]
            tp = k.tpsum[gi % 2]
            cx.op("tensor", lambda h, pb=pb, g=g: h.matmul(pb[:, 0:256], k.s5TT[:, g, :], k.s5U[:, g, :], start=True, stop=False),
                  r=[k.t_s5tt, k.t_s5U], w=[tp], inc=False)
            for d in range(2):
                for ri in range(2):
                    slot0 = 0 if d == 0 else 1
                    rhs = pap(SH, gh * 64, 64, d * 257 * 32 + slot0 * 32 + ri * 16 + gl, [[32, 256]])
                    last = (d == 1 and ri == 1)
                    cx.op("tensor", lambda h, pb=pb, rhs=rhs, d=d, ri=ri, gh=gh, gl=gl, last=last: h.matmul(
                        pb[:, 0:256], k.s5CS[gh * 64:(gh + 1) * 64, d, gl, ri, :], rhs, start=False, stop=last),
                        r=[tSH, tSHh, k.t_s5w], w=[tp], inc=last)
            if gi % 2 == 0:
                cx.op("scalar", lambda h, pb=pb, b=b, gi=gi: h.copy(ybuf[b][:, gi, :], pb[:, 0:256]), r=[tp], w=[tyb[b]])
            else:
                cx.op(V, lambda h, pb=pb, b=b, gi=gi: h.tensor_copy(ybuf[b][:, gi, :], pb[:, 0:256]), r=[tp], w=[tyb[b]])
        for t in range(8):
            q0 = 32 * (t // 2)
            pb = k.psum[2 + t % 4]
            tp = k.tpsum[2 + t % 4]
            for gi in range(8):
                cx.op("tensor", lambda h, pb=pb, t=t, gi=gi, q0=q0, b=b: h.matmul(pb[:, 0:256], k.selT[q0:q0 + 32, t % 2, gi, :], ybuf[b][q0:q0 + 32, gi, :], start=(gi == 0), stop=(gi == 7), tile_position=(q0, 0)),
                      r=[tyb[b]], w=[tp], inc=(gi == 7))
            dstv = pap(gT, 0, 128, ct * 2048 + t, [[8, 256]])
            cx.op("scalar", lambda h, pb=pb, dstv=dstv: h.activation(dstv, pb[:, 0:256], AF.Gelu), r=[tp], w=[tgT[ct]])
        cx.op("vector", lambda h, ct=ct: h.tensor_copy(gTb[:, ct, :], gT[:, ct, :]), r=[tgT[ct]], w=[tgTb[ct]])
    k.dbg_add("s5_g", gT, tgT)
    if S5STOP == 'c':
        cx.barrier(); ar.release(m0); return
    sig = ar.alloc([4, 512], BF16)
    tsig = Trk()
    sq = ar.alloc([4, 512], BF16)
    tsq = Trk()
    rs = ar.alloc([512], F32)
    trs = Trk()
    for nck in range(4):
        ts = slice(nck * 512, (nck + 1) * 512)
        for co in range(4):
            pb = k.psum[co % 2]
            tp = k.tpsum[co % 2]
            for ci in range(4):
                cx.op("tensor", lambda h, pb=pb, co=co, ci=ci, ts=ts: h.matmul(pb[:, :], wg[:, ci, co * 128:(co + 1) * 128], gTb[:, ci, ts], start=(ci == 0), stop=(ci == 3)),
                      r=[twg] + tgTb, w=[tp], inc=(ci == 3))
            cx.op("scalar", lambda h, pb=pb, co=co: h.activation(sig[:, co, :], pb[:, :], AF.Sigmoid, bias=bgl[:, co:co + 1]), r=[tp, tb], w=[tsig])
        for co in range(4):
            cx.op(V, lambda h, co=co, ts=ts: h.tensor_tensor(out=gT[:, co, ts], in0=gT[:, co, ts], in1=sig[:, co, :], op=ALU.mult), r=[tsig, tgT[co]], w=[tgT[co]])
            cx.op("scalar", lambda h, co=co, ts=ts: h.activation(sq[:, co, :], gT[:, co, ts], AF.Square), r=[tgT[co]], w=[tsq])
        pb = k.psum[2 + nck % 2]
        tp = k.tpsum[2 + nck % 2]
        for co in range(4):
            cx.op("tensor", lambda h, pb=pb, co=co: h.matmul(pb[:, :], k.onesb, sq[:, co, :], start=(co == 0), stop=(co == 3)), r=[tsq], w=[tp], inc=(co == 3))
        cx.op("scalar", lambda h, pb=pb: h.activation(rs, pb[:, :], AF.Sqrt, scale=1.0 / 512, bias=k.epsc), r=[tp], w=[trs])
        cx.op(V, lambda h: h.reciprocal(rs, rs), r=[trs], w=[trs])
        for co in range(4):
            cx.op(V, lambda h, co=co, ts=ts: h.scalar_tensor_tensor(out=yT[:, co, ts], in0=gT[:, co, ts], scalar=nw[:, co:co + 1], in1=rs, op0=ALU.mult, op1=ALU.mult),
                  r=[tgT[co], trs, tb, tsq], w=[t_yT])
    k.dbg_add("s5_gl", gT, tgT)
    cx.barrier()
    ar.release(m0)


def build(in_shapes, stage="full", dbg_names=(), n_heads=4, n_experts=32):
    nc = bass.Bass("TRN2", target_bir_lowering=False)
    k = K()
    k.n_heads = n_heads
    k.n_experts = n_experts
    k.nc = nc
    D = {}
    for nm, (shape, dt) in in_shapes.items():
        D[nm] = nc.dram_tensor(nm, list(shape), dt, kind="ExternalInput").ap()
    k.D = D
    out = nc.dram_tensor("out", [S, DM], F32, kind="ExternalOutput").ap()
    k.dbg = {}
    k.dbg_req = set(dbg_names)

    with contextlib.ExitStack() as st:
        cx = Ctx(nc, st)
        k.cx = cx

        def finish():
            deps = [(s_.key, s_.total) for s_ in cx.slots if s_.total > 0]
            cx.wait_deps("sync", deps + [(e, cx.cnt[e]) for e in ENGS if e != "sync" and cx.cnt[e] > 0])
            with nc.Block() as block:
                cx.emit_all(block)
            k.n_ops = cx.n_ops
            return nc, k
        k.slot_c = cx.slot("c")
        k.slot_w = cx.slot("w")
        k.slot_x = [cx.slot("x0"), cx.slot("x1")]
        k.slot_o = cx.slot("o")
        k.psum = [cx.ps("ps%d" % i, [128, 512], F32) for i in range(8)]
        k.psum = [p[:, :] for p in k.psum]
        k.tpsum = [Trk("ps%d" % i, excl=True) for i in range(8)]
        k.ident = cx.sb("ident", [128, 128], F32)[:, :]
        k.identb = cx.sb("identb", [128, 128], BF16)[:, :]
        k.ones = cx.sb("ones", [128, 128], F32)[:, :]
        k.onesb = cx.sb("onesb", [128, 128], BF16)[:, :]
        k.epsc = cx.sb("epsc", [128, 1], F32)[:, :]
        k.selT = cx.sb("selT", [128, 2, 8, 128], BF16)[:, :, :, :]
        tc = Trk()
        cx.dma("sync", k.ident, D["ident"], k.slot_c, w=[tc])
        cx.dma("sync", k.identb, D["identb"], k.slot_c, w=[tc])
        cx.dma("sync", k.ones, D["ones"], k.slot_c, w=[tc])
        cx.dma("gpsimd", k.onesb, D["ones"], cx.fresh("sw"), w=[tc])
        cx.op("vector", lambda h: h.memset(k.epsc, EPS), w=[tc])
        cx.dma("sync", k.selT, D["selT"], k.slot_c, w=[tc])
        k.s5A1 = cx.sb("s5A1", [128, 64], F32)[:, :]
        k.s5A2 = cx.sb("s5A2", [128, 64], F32)[:, :]
        ar = Arena(cx, 51456)
        k.ar = ar
        cx.barrier()

        def dbg_add(name, ap, trks):
            if name in k.dbg_req:
                shape = list(ap.shape)
                dt_ = F32
                o = nc.dram_tensor("dbg_" + name, shape, dt_, kind="ExternalOutput").ap()
                cx.dma("gpsimd" if ap.dtype != F32 else "sync", o, ap, cx.fresh("sw" if ap.dtype != F32 else "hw"), r=list(trks))
        k.dbg_add = dbg_add

        regA = ar.alloc([NT * 1024], F32)
        arA = Arena(cx, NT * 1024, base=regA)
        k.arA = arA
        yT = ar.alloc([8, 2048], BF16)
        t_yT = Trk()
        k.s5U = arA.alloc([32, 256], BF16)
        k.t_s5U = Trk()
        m_h = arA.mark()
        k.hT = arA.alloc([8, 2048], BF16)
        k.t_hT = Trk()
        mU = arA.mark()
        k.u_wt = [arA.alloc([8, 128], BF16) for _ in range(4)]
        k.t_uwt = [Trk() for _ in range(4)]
        sl_u = cx.slot("uw")
        for ct_ in range(4):
            load_w_cols(k, D["w_in"], ct_ * 128, 128, k.u_wt[ct_], k.t_uwt[ct_], sl_u)

        m1 = ar.mark()
        xt = [ar.alloc([1024], F32) for _ in range(2)]
        txt = [Trk(), Trk()]

        def src_x(i):
            b = i % 2
            cx.dma("sync", xt[b], D["x"][i * 128:(i + 1) * 128, :], k.slot_x[b], w=[txt[b]])
            return xt[b], txt[b]
        norm_transpose(k, "mix", src_x, NT, D["norm_mix"], k.hT, BF16, k.t_hT)
        cx.barrier()
        ar.release(m1)

        if stage == 'p1':
            return finish()
        s5_build_U(k)
        arA.release(mU)
        if stage == 'U':
            return finish()
        mg = ar.mark()
        gdn_setup(k)
        if k.n_heads > 0:
            gdn_load_weights(k, 0)
        for hd in range(k.n_heads):
            gdn_head(k, hd, yT, t_yT)
        ar.release(mg)
        k.dbg_add("ygdnT", yT[:, 4:8, :], [t_yT])
        if stage == 'gdn':
            return finish()
        cx.barrier()
        arA.release(m_h)
        k.s5AT = arA.alloc([64, 128], BF16)
        k.t_s5at = Trk()
        k.s5CS = arA.alloc([2, 16, 2, 128], BF16)
        k.s5TT = arA.alloc([32, 128], BF16)
        k.t_s5tt = Trk()
        s5_prep(k)
        if stage == 's5prep':
            return finish()
        s5_main(k, yT[:, 0:4, :], t_yT)
        k.dbg_add("ys5T", yT[:, 0:4, :], [t_yT])
        if stage == "s5":
            return finish()
        if True:
            cx.barrier()
            k.xacc = regA.rearrange('p (a b) -> p a b', a=NT)
            k.txacc = [Trk() for _ in range(NT)]
            out_proj(k, yT, t_yT)
            k.dbg_add("x1", k.xacc, k.txacc)
            if stage == 'oproj':
                return finish()
            xattn(k)
            if stage == 'xattn':
                return finish()
            k.dbg_add("x2", k.xacc, k.txacc)
            moe(k)
            k.dbg_add("x3", k.xacc, k.txacc + [t_ for p_ in k.txh for t_ in p_])
            final_norm(k, out)

        return finish()


def host_inputs(inp, b):
    m = {}
    m["x"] = np.ascontiguousarray(inp["x"][b])
    m["mem"] = np.ascontiguousarray(inp["mem"][b])
    m["norm_mix"] = inp["norm_mix"][0]
    m["w_in"] = inp["w_in"][0]
    m["w_out"] = inp["w_out"][0]
    m["s5_w_glu"] = inp["s5_w_glu"][0]
    m.update(host_s5(inp))
    cv = inp["gdn_conv"][0]
    m["gdn_convw"] = np.ascontiguousarray(cv.reshape(5, 3, 4, 128).transpose(3, 2, 1, 0))
    for nm in ("gdn_a_log_f", "gdn_dt_bias_f", "gdn_a_log_b", "gdn_dt_bias_b"):
        m[nm] = inp[nm][0]
    m["gdn_norm"] = inp["gdn_norm"][0]
    for nm in ("norm_xattn", "norm_mem", "xa_wq", "xa_wk", "xa_wv", "xa_wo", "norm_moe", "router_group_w", "router_group_b",
               "router_expert_w", "router_expert_b", "moe_w_gate", "moe_w_up", "moe_w_down"):
        m[nm] = inp[nm][0]
    m["norm_final"] = inp["norm_final"]
    m.update(host_consts())
    return m


def gdn_setup(k):
    cx, ar, D = k.cx, k.ar, k.D
    V = "vector"
    G = K()
    k.G = G
    G.mask = ar.alloc([7, 128], F32)
    G.tmask = Trk()
    cx.dma("sync", G.mask, D["gmask"][:, 0:7, :], cx.fresh(), w=[G.tmask])
    wsm = ar.alloc([8, 16], BF16)
    tw = Trk()
    load_w_cols(k, D["w_in"], 2560, 16, wsm, tw, cx.fresh('sw'))
    BA = ar.alloc([16, 16], F32)
    tBA = Trk()
    for i in range(NT):
        pb = k.psum[i % 4]
        tp = k.tpsum[i % 4]
        for c in range(8):
            cx.op("tensor", lambda h, pb=pb, c=c, i=i: h.matmul(pb[:, 0:16], k.hT[:, c, i * 128:(i + 1) * 128], wsm[:, c, :], start=(c == 0), stop=(c == 7)),
                  r=[tw, k.t_hT], w=[tp], inc=(c == 7))
        cx.op("scalar", lambda h, pb=pb, i=i: h.copy(BA[:, i, :], pb[:, 0:16]), r=[tp], w=[tBA])
    pr = ar.alloc([4, 4], F32)
    tpr = Trk()
    sl_ = cx.fresh()
    for j, nm in enumerate(("gdn_a_log_f", "gdn_dt_bias_f", "gdn_a_log_b", "gdn_dt_bias_b")):
        cx.dma("sync", pr[:, j, :], dram_bcast(D[nm], 128, 4), sl_, w=[tpr])
    G.nw = ar.alloc([128], F32)
    cx.dma("sync", G.nw, dram_bcast(D["gdn_norm"], 128, 128), sl_, w=[tpr])
    G.tpr = tpr
    T = Trk()
    G.T = T
    G.beta, G.nb, G.gc, G.eg, G.neg, G.ed = [], [], [], [], [], []
    def per_dir(d):
        beta = ar.alloc([16, 4], F32)
        nb = ar.alloc([16, 4], F32)
        g = ar.alloc([16, 4], F32)
        gc = ar.alloc([16, 4], F32)
        gt = ar.alloc([16, 4], F32)
        eg = ar.alloc([16, 4], F32)
        neg = ar.alloc([16, 4], F32)
        ed = ar.alloc([16, 4], F32)
        ea = ar.alloc([4], F32)
        braw = BA[:, :, d * 4:(d + 1) * 4]
        araw = BA[:, :, 8 + d * 4:8 + (d + 1) * 4]
        cx.op("scalar", lambda h: h.activation(beta, braw, AF.Sigmoid), r=[tBA, T], w=[T])
        cx.op(V, lambda h: h.tensor_scalar(nb, beta, -1.0, None, op0=ALU.mult), r=[T], w=[T])
        cx.op("scalar", lambda h: h.activation(ea, pr[:, 2 * d, :], AF.Exp), r=[tpr, T], w=[T])
        cx.op(V, lambda h: h.tensor_tensor(out=g, in0=araw, in1=pr[:, 2 * d + 1, :].unsqueeze(1).to_broadcast([128, 16, 4]), op=ALU.add), r=[tBA, tpr, T], w=[T])
        cx.op("scalar", lambda h: h.activation(g, g, AF.Exp), r=[T], w=[T])
        cx.op("scalar", lambda h: h.activation(g, g, AF.Ln, bias=1.0), r=[T], w=[T])
        cx.op(V, lambda h: h.scalar_tensor_tensor(out=g, in0=g, scalar=-1.0, in1=ea.unsqueeze(1).to_broadcast([128, 16, 4]), op0=ALU.mult, op1=ALU.mult), r=[T], w=[T])
        g2 = g.rearrange("p a b -> p (a b)")
        pb = k.psum[4 + d]
        tp = k.tpsum[4 + d]
        cx.op("tensor", lambda h, pb=pb, d=d: h.matmul(pb[:, 0:64], G.mask[:, d, :], g2, start=True, stop=True), r=[T, G.tmask], w=[tp])
        cx.op("tensor", lambda h, pb=pb: h.matmul(pb[:, 64:128], G.mask[:, 6, :], g2, start=True, stop=True), r=[T, G.tmask], w=[tp])
        cx.op(V, lambda h, pb=pb: h.tensor_copy(gc.rearrange("p a b -> p (a b)"), pb[:, 0:64]), r=[tp], w=[T])
        cx.op(V, lambda h, pb=pb: h.tensor_tensor(out=gt.rearrange("p a b -> p (a b)"), in0=pb[:, 64:128], in1=gc.rearrange("p a b -> p (a b)"), op=ALU.subtract), r=[tp, T], w=[T])
        cx.op("scalar", lambda h: h.activation(eg, gc, AF.Exp), r=[T], w=[T])
        cx.op("scalar", lambda h: h.activation(ed, gt, AF.Exp), r=[T], w=[T])
        cx.op(V, lambda h: h.tensor_scalar(neg, eg, -1.0, None, op0=ALU.mult), r=[T], w=[T])
        G.g = getattr(G, "g", []) + [g]
        G.beta.append(beta); G.nb.append(nb); G.gc.append(gc); G.eg.append(eg); G.neg.append(neg); G.ed.append(ed)
    per_dir(0)
    per_dir(1)
    G.osum = ar.alloc([16, 128], F32)
    G.tosum = [Trk() for _ in range(NT)]
    G.wset = [[k.arA.alloc([8, 128], BF16) for _ in range(4)] for _ in range(2)]
    G.tw = [[Trk() for _ in range(4)] for _ in range(2)]
    G.sw = [cx.slot("gw0"), cx.slot("gw1")]


def gdn_load_weights(k, hd):
    G, D = k.G, k.D
    st = hd % 2
    for j in range(3):
        load_w_cols(k, D["w_in"], 512 + j * 512 + hd * 128, 128, G.wset[st][j], G.tw[st][j], G.sw[st])
    load_w_cols(k, D["w_in"], 512 + 1536 + hd * 128, 128, G.wset[st][3], G.tw[st][3], G.sw[st])


def gdn_head(k, hd, yT, t_yT):
    cx, ar, D, G = k.cx, k.ar, k.D, k.G
    V = "vector"
    m0 = ar.mark()
    qnT = ar.alloc([2048], BF16)
    knT = ar.alloc([2048], BF16)
    Ktok = ar.alloc([16, 128], BF16)
    Vtok = ar.alloc([16, 128], BF16)
    tq, tk_, tKt, tVt = Trk(), Trk(), Trk(), Trk()
    st_ = hd % 2
    wz, twz = G.wset[st_][3], G.tw[st_][3]
    w3, tw3 = G.wset[st_][0:3], G.tw[st_][0:3]
    if hd + 1 < k.n_heads:
        gdn_load_weights(k, hd + 1)
    mA = ar.mark()
    cw = ar.alloc([3, 5], F32)
    tcw = Trk()
    cx.dma("sync", cw, D["gdn_convw"][:, hd, :, :], cx.fresh(), w=[tcw])
    diag = ar.alloc([15, 128], BF16)
    tdg = Trk()
    for j in range(3):
        for t in range(5):
            cx.op("vector", lambda h, j=j, t=t: h.tensor_scalar(diag[:, j * 5 + t, :], k.identb, cw[:, j, t:t + 1], None, op0=ALU.mult), r=[tcw], w=[tdg])
    import os
    ALV = int(os.environ.get("GDN_ALV", "9"))
    if ALV == 0:
        cx.barrier(); ar.release(m0); return
    raw = [ar.alloc([2052], BF16) for _ in range(2)]
    traw = [Trk(), Trk()]
    for b in range(2):
        cx.op("gpsimd", lambda h, b=b: h.memset(raw[b][:, 0:2], 0.0), w=[traw[b]])
        cx.op("gpsimd", lambda h, b=b: h.memset(raw[b][:, 2050:2052], 0.0), w=[traw[b]])
    act2 = [ar.alloc([2048], F32) for _ in range(2)]
    tact = Trk()
    vT = ar.alloc([2048], BF16)
    tvT = Trk()
    sqb = ar.alloc([2048], BF16)
    tsqb = Trk()
    rn = [ar.alloc([512], F32) for _ in range(4)]
    trn = [Trk() for _ in range(4)]
    tactn2 = [[Trk() for _ in range(4)] for _ in range(2)]
    tsqn = [Trk() for _ in range(4)]
    if ALV == 1:
        cx.barrier(); ar.release(m0); return
    for j in range(3):
        b = j % 2
        act = act2[j % 2]
        tactn = tactn2[j % 2]

        def consume(n, pb, tp, b=b):
            cx.op(V if n % 2 else "scalar", (lambda h: h.tensor_copy(raw[b][:, 2 + n * 512:2 + (n + 1) * 512], pb[:, :])) if n % 2 else
                  (lambda h: h.copy(raw[b][:, 2 + n * 512:2 + (n + 1) * 512], pb[:, :])), r=[tp], w=[traw[b]])
        proj_fm(k, w3[j], tw3[j], consume)
        for n in range(4):
            pb = k.psum[4 + n]
            tp = k.tpsum[4 + n]
            for t in range(5):
                cx.op("tensor", lambda h, pb=pb, t=t, n=n, j=j, b=b: h.matmul(pb[:, :], diag[:, j * 5 + t, :], raw[b][:, n * 512 + t:n * 512 + t + 512], start=(t == 0), stop=(t == 4)),
                      r=[tdg, traw[b]], w=[tp], inc=(t == 4))
        for n in range(4):
            pb = k.psum[4 + n]
            tp = k.tpsum[4 + n]
            ts = slice(n * 512, (n + 1) * 512)
            if j == 2:
                cx.op("scalar", lambda h, pb=pb, ts=ts: h.activation(vT[:, ts], pb[:, :], AF.Silu), r=[tp], w=[tvT])
            else:
                cx.op("scalar", lambda h, pb=pb, ts=ts, act=act: h.activation(act[:, ts], pb[:, :], AF.Silu), r=[tp], w=[tactn[n]])
        if j < 2 and ALV > 2:
            for n in range(4):
                ts = slice(n * 512, (n + 1) * 512)
                cx.op("scalar", lambda h, ts=ts, act=act: h.activation(sqb[:, ts], act[:, ts], AF.Square), r=[tactn[n]], w=[tsqn[n]])
            for n in range(4):
                ts = slice(n * 512, (n + 1) * 512)
                pb2 = k.psum[n]
                tp2 = k.tpsum[n]
                cx.op("tensor", lambda h, pb2=pb2, ts=ts: h.matmul(pb2[:, :], k.onesb, sqb[:, ts], start=True, stop=True), r=[tsqn[n]], w=[tp2])
            for n in range(4):
                pb2 = k.psum[n]
                tp2 = k.tpsum[n]
                cx.op("scalar", lambda h, pb2=pb2, n=n: h.activation(rn[n], pb2[:, :], AF.Sqrt, bias=k.epsc), r=[tp2], w=[trn[n]])
            for n in range(4):
                cx.op(V, lambda h, n=n: h.reciprocal(rn[n], rn[n]), r=[trn[n]], w=[trn[n]])
            dstT, tdst, scl = (qnT, tq, 128.0 ** -0.5) if j == 0 else (knT, tk_, 1.0)
            for n in range(4):
                ts = slice(n * 512, (n + 1) * 512)
                cx.op(V, lambda h, ts=ts, n=n, dstT=dstT, scl=scl, act=act: h.scalar_tensor_tensor(out=dstT[:, ts], in0=act[:, ts], scalar=scl, in1=rn[n], op0=ALU.mult, op1=ALU.mult),
                      r=[tactn[n], trn[n]], w=[tdst])
    if ALV <= 3:
        cx.barrier(); ar.release(m0); return
    TV = int(os.environ.get("GDN_TV", "0"))
    for i in range(NT):
        pb = k.psum[i % 2].bitcast(BF16)
        tp = k.tpsum[i % 2]
        if TV == 0:
            cx.op("tensor", lambda h, pb=pb, i=i: h.transpose(pb[:, 0:128], knT[:, i * 128:(i + 1) * 128], k.identb), r=[tk_], w=[tp])
            cx.op("tensor", lambda h, pb=pb, i=i: h.transpose(pb[:, 128:256], vT[:, i * 128:(i + 1) * 128], k.identb), r=[tvT], w=[tp])
            cx.op("scalar", lambda h, pb=pb, i=i: h.copy(Ktok[:, i, :], pb[:, 0:128]), r=[tp], w=[tKt])
            cx.op(V, lambda h, pb=pb, i=i: h.tensor_copy(Vtok[:, i, :], pb[:, 128:256]), r=[tp], w=[tVt])
        elif TV == 1:
            cx.op("tensor", lambda h, pb=pb, i=i: h.transpose(pb[:, 0:128], knT[:, i * 128:(i + 1) * 128], k.identb), r=[tk_], w=[tp])
            cx.op("scalar", lambda h, pb=pb, i=i: h.copy(Ktok[:, i, :], pb[:, 0:128]), r=[tp], w=[tKt])
        elif TV == 2:
            cx.op("tensor", lambda h, pb=pb, i=i: h.transpose(pb[:, 0:128], vT[:, i * 128:(i + 1) * 128], k.identb), r=[tvT], w=[tp])
            cx.op(V, lambda h, pb=pb, i=i: h.tensor_copy(Vtok[:, i, :], pb[:, 0:128]), r=[tp], w=[tVt])
    if hd == 0:
        k.dbg_add("gdn_qn", qnT, [tq])
        k.dbg_add("gdn_kn", knT, [tk_])
        k.dbg_add("gdn_vtok", Vtok, [tVt])
    cx.barrier()
    ar.release(mA)
    STOP = os.environ.get("GDN_STOP", "")
    if STOP == "A":
        ar.release(m0)
        return
    qgT = [ar.alloc([2048], BF16) for _ in range(2)]
    Kd = [ar.alloc([16, 128], BF16) for _ in range(2)]
    Pm = [ar.alloc([16, 128], BF16) for _ in range(2)]
    QKm = [ar.alloc([16, 128], BF16) for _ in range(2)]
    etot = [ar.alloc([32], F32) for _ in range(2)]
    WnT = [ar.alloc([2048], BF16) for _ in range(2)]
    U0b = [ar.alloc([16, 128], BF16) for _ in range(2)]
    tWn = [[Trk() for _ in range(NT)] for _ in range(2)]
    tU0 = [[Trk() for _ in range(NT)] for _ in range(2)]
    tqg = [[Trk() for _ in range(NT)] for _ in range(2)]
    tKd = [[Trk() for _ in range(NT)] for _ in range(2)]
    tPm = [[Trk() for _ in range(NT)] for _ in range(2)]
    tQK = [[Trk() for _ in range(NT)] for _ in range(2)]
    tet = [[Trk() for _ in range(NT)] for _ in range(2)]
    NI = 8
    mN = ar.mark()
    NDT = BF16 if os.environ.get('GDN_NEU', 'bf16') == 'bf16' else F32
    nid = k.identb if NDT == BF16 else k.ident
    Xb = [[ar.alloc([128], NDT) for _ in range(2)] for _ in range(NI)]
    XTb = [[ar.alloc([128], NDT) for _ in range(2)] for _ in range(NI)]
    Pb = [[ar.alloc([128], NDT) for _ in range(2)] for _ in range(NI)]

    def tview(pn_, c0):
        return pn_[:, c0:c0 + 128] if NDT == F32 else pn_.bitcast(BF16)[:, 2 * c0:2 * c0 + 128]
    tX = [Trk() for _ in range(NI)]
    EGB = [ar.alloc([128], F32) for _ in range(NI)]
    ET = [ar.alloc([128], BF16) for _ in range(NI)]
    ETs = ET
    tE = [Trk() for _ in range(NI)]
    tEG = [Trk() for _ in range(NI)]
    Kg = [ar.alloc([128], BF16) for _ in range(NI)]
    tKg = [Trk() for _ in range(NI)]
    tXP = [Trk() for _ in range(NI)]
    insts = [(i, d) for i in range(NT) for d in range(2)]
    for g0 in range(0, len(insts), NI):
        grp = insts[g0:g0 + NI]
        info = []
        for s_, (i, d) in enumerate(grp):
            info.append(dict(s_=s_, i=i, d=d, tsl=slice(i * 128, (i + 1) * 128),
                             col=pap(G.g[d], 0, 128, i * 4 + hd, [[0, 128]]),
                             gcc=G.gc[d][:, i, hd:hd + 1], nbc=G.nb[d][:, i, hd:hd + 1], edc=G.ed[d][:, i, hd:hd + 1],
                             pb=k.psum[s_], tp=k.tpsum[s_]))
        for q_ in info:
            s_, i, d, tsl, col, pb, tp = q_["s_"], q_["i"], q_["d"], q_["tsl"], q_["col"], q_["pb"], q_["tp"]
            cx.op("tensor", lambda h, pb=pb, col=col, d=d: h.matmul(pb[:, 0:128], col, G.mask[:, d, :], start=True, stop=True), r=[G.T, G.tmask], w=[tp], inc=False)
            cx.op("tensor", lambda h, pb=pb, tsl=tsl: h.matmul(pb[:, 128:256], knT[:, tsl], knT[:, tsl], start=True, stop=True), r=[tk_], w=[tp], inc=False)
            cx.op("tensor", lambda h, pb=pb, tsl=tsl: h.matmul(pb[:, 256:384], knT[:, tsl], qnT[:, tsl], start=True, stop=True), r=[tk_, tq], w=[tp])
        for q_ in info:
            s_, i, d, pb, tp, gcc = q_["s_"], q_["i"], q_["d"], q_["pb"], q_["tp"], q_["gcc"]
            cx.op("scalar", lambda h, pb=pb, s_=s_: h.activation(EGB[s_], pb[:, 0:128], AF.Exp), r=[tp], w=[tEG[s_]])
            cx.op(V, lambda h, pb=pb, s_=s_, gcc=gcc, d=d: h.scalar_tensor_tensor(out=ET[s_], in0=pb[:, 0:128], scalar=gcc, in1=G.mask[:, 2 + d, :], op0=ALU.subtract, op1=ALU.min),
                  r=[tp, G.T, G.tmask], w=[tE[s_]])
        for q_ in info:
            s_, i, d, tsl = q_["s_"], q_["i"], q_["d"], q_["tsl"]
            cx.op("scalar", lambda h, s_=s_: h.activation(ET[s_], ET[s_], AF.Exp), r=[tE[s_]], w=[tE[s_]])
            cx.op(V, lambda h, s_=s_, tsl=tsl, d=d: h.tensor_tensor(out=qgT[d][:, tsl], in0=qnT[:, tsl], in1=EGB[s_], op=ALU.mult), r=[tq, tEG[s_]], w=[tqg[d][i]])
        for q_ in info:
            s_, i, d, pb, tp, edc = q_["s_"], q_["i"], q_["d"], q_["pb"], q_["tp"], q_["edc"]
            c0, c1 = (63, 127) if d == 0 else (0, 64)
            cx.op("scalar", lambda h, s_=s_, d=d, i=i, c0=c0: h.copy(etot[d][:, 2 * i:2 * i + 1], EGB[s_][:, c0:c0 + 1]), r=[tEG[s_]], w=[tet[d][i]])
            cx.op("scalar", lambda h, s_=s_, d=d, i=i, c1=c1: h.copy(etot[d][:, 2 * i + 1:2 * i + 2], EGB[s_][:, c1:c1 + 1]), r=[tEG[s_]], w=[tet[d][i]])
            cx.op("scalar", lambda h, d=d, i=i, edc=edc: h.activation(Kd[d][:, i, :], Ktok[:, i, :], AF.Identity, scale=edc), r=[tKt, G.T], w=[tKd[d][i]])
            egc = G.eg[d][:, i, hd:hd + 1]
            cx.op("scalar", lambda h, s_=s_, i=i, egc=egc: h.activation(Kg[s_], Ktok[:, i, :], AF.Identity, scale=egc), r=[tKt, G.T], w=[tKg[s_]])
            cx.op(V, lambda h, pb=pb, s_=s_, d=d, i=i: h.tensor_tensor(out=QKm[d][:, i, :], in0=pb[:, 256:384], in1=ET[s_], op=ALU.mult), r=[tp, tE[s_]], w=[tQK[d][i]])
        for q_ in info:
            s_, d = q_["s_"], q_["d"]
            cx.op(V, lambda h, s_=s_, d=d: h.tensor_tensor(out=ETs[s_], in0=ET[s_], in1=G.mask[:, 4 + d, :], op=ALU.mult), r=[tE[s_], G.tmask], w=[tE[s_]])
        for q_ in info:
            s_, pb, tp, nbc = q_["s_"], q_["pb"], q_["tp"], q_["nbc"]
            cx.op(V, lambda h, pb=pb, s_=s_, nbc=nbc: h.scalar_tensor_tensor(out=Xb[s_][0], in0=pb[:, 128:256], scalar=nbc, in1=ETs[s_], op0=ALU.mult, op1=ALU.mult),
                  r=[tp, tE[s_], G.T], w=[tX[s_]])
        for q_ in info:
            s_, pn, tn = q_["s_"], q_["pb"], q_["tp"]
            cx.op("tensor", lambda h, pn=pn, s_=s_: h.transpose(tview(pn, 384), Xb[s_][0], nid), r=[tX[s_]], w=[tn])
            cx.op(V, lambda h, s_=s_: h.tensor_tensor(out=Pb[s_][0], in0=Xb[s_][0], in1=nid, op=ALU.add), r=[tX[s_]], w=[tXP[s_]])
            cx.op("scalar", lambda h, pn=pn, s_=s_: h.copy(XTb[s_][0], tview(pn, 384)), r=[tn], w=[tX[s_]])
        for L in range(1, 6):
            a, b_ = (L - 1) % 2, L % 2
            for s_, (i, d) in enumerate(grp):
                pn = k.psum[s_]
                tn = k.tpsum[s_]
                if L < 5:
                    cx.op("tensor", lambda h, pn=pn, s_=s_, a=a: h.matmul(pn[:, 0:128], XTb[s_][a], Xb[s_][a], start=True, stop=True), r=[tX[s_]], w=[tn], inc=False)
                cx.op("tensor", lambda h, pn=pn, s_=s_, a=a: h.matmul(pn[:, 128:256], Xb[s_][a], XTb[s_][a], start=True, stop=True), r=[tX[s_]], w=[tn])
                e1, e2 = ("scalar", V) if s_ % 2 == 0 else (V, "scalar")
                if L < 5:
                    if e1 == "scalar":
                        cx.op("scalar", lambda h, pn=pn, s_=s_, b_=b_: h.copy(Xb[s_][b_], pn[:, 0:128]), r=[tn], w=[tX[s_]])
                    else:
                        cx.op(V, lambda h, pn=pn, s_=s_, b_=b_: h.tensor_copy(Xb[s_][b_], pn[:, 0:128]), r=[tn], w=[tX[s_]])
                if e2 == "scalar":
                    cx.op("scalar", lambda h, pn=pn, s_=s_, b_=b_: h.copy(XTb[s_][b_], pn[:, 128:256]), r=[tn], w=[tX[s_]])
                else:
                    cx.op(V, lambda h, pn=pn, s_=s_, b_=b_: h.tensor_copy(XTb[s_][b_], pn[:, 128:256]), r=[tn], w=[tX[s_]])
            for s_, (i, d) in enumerate(grp):
                pn = k.psum[s_]
                tn = k.tpsum[s_]
                cx.op("tensor", lambda h, pn=pn, s_=s_, a=a: h.matmul(pn[:, 256:384], nid, Pb[s_][a], start=True, stop=False), r=[tXP[s_]], w=[tn], inc=False)
                cx.op("tensor", lambda h, pn=pn, s_=s_, a=a, b_=b_: h.matmul(pn[:, 256:384], XTb[s_][b_], Pb[s_][a], start=False, stop=True), r=[tX[s_], tXP[s_]], w=[tn])
                use_act = (s_ + L) % 2 == 0
                if L < 5:
                    if use_act:
                        cx.op("scalar", lambda h, pn=pn, s_=s_, b_=b_: h.copy(Pb[s_][b_], pn[:, 256:384]), r=[tn], w=[tXP[s_]])
                    else:
                        cx.op(V, lambda h, pn=pn, s_=s_, b_=b_: h.tensor_copy(Pb[s_][b_], pn[:, 256:384]), r=[tn], w=[tXP[s_]])
                else:
                    if use_act:
                        cx.op("scalar", lambda h, pn=pn, d=d, i=i: h.copy(Pm[d][:, i, :], pn[:, 256:384]), r=[tn], w=[tPm[d][i], tXP[s_]])
                    else:
                        cx.op(V, lambda h, pn=pn, d=d, i=i: h.tensor_copy(Pm[d][:, i, :], pn[:, 256:384]), r=[tn], w=[tPm[d][i], tXP[s_]])
        for s_, (i, d) in enumerate(grp):
            pn = k.psum[s_]
            tn = k.tpsum[s_]
            tsl = slice(i * 128, (i + 1) * 128)
            cx.op("tensor", lambda h, pn=pn, s_=s_, d=d, i=i: h.matmul(pn[:, 0:128], Kg[s_], Pm[d][:, i, :], start=True, stop=True), r=[tKg[s_], tPm[d][i]], w=[tn], inc=False)
            cx.op("tensor", lambda h, pn=pn, d=d, i=i: h.matmul(pn[:, 128:256], Pm[d][:, i, :], Vtok[:, i, :], start=True, stop=True), r=[tPm[d][i], tVt], w=[tn])
            btc_ = G.beta[d][:, i, hd:hd + 1]
            cx.op(V, lambda h, pn=pn, d=d, tsl=tsl: h.tensor_scalar(WnT[d][:, tsl], pn[:, 0:128], -1.0, None, op0=ALU.mult), r=[tn], w=[tWn[d][i]])
            cx.op("scalar", lambda h, pn=pn, d=d, i=i, btc_=btc_: h.activation(U0b[d][:, i, :], pn[:, 128:256], AF.Identity, scale=btc_), r=[tn, G.T], w=[tU0[d][i]])
    if STOP == "B":
        cx.barrier()
        ar.release(m0)
        return
    ar.release(mN)
    Sf = [[ar.alloc([128], F32) for _ in range(2)] for _ in range(2)]
    Sb = [ar.alloc([128], BF16) for _ in range(2)]
    Rp = [ar.alloc([128], BF16) for _ in range(2)]
    vn = [ar.alloc([128], BF16) for _ in range(2)]
    tS = [Trk(), Trk()]
    tSf = [Trk(), Trk()]
    tR = [Trk(), Trk()]
    tv = [Trk(), Trk()]
    cx.op("gpsimd", lambda h: h.memset(G.osum, 0.0), w=G.tosum)
    for d in range(2):
        cx.op("gpsimd", lambda h, d=d: h.memset(Sf[d][0], 0.0), w=[tS[d]])
        cx.op("gpsimd", lambda h, d=d: h.memset(Sb[d], 0.0), w=[tS[d]])
        cx.op("gpsimd", lambda h, d=d: h.memset(Rp[d], 0.0), w=[tR[d]])
        cx.op("gpsimd", lambda h, d=d: h.memset(vn[d], 0.0), w=[tv[d]])
    for step in range(32):
        for d in range(2):
            if d == 0:
                i, hh = step // 2, step % 2
            else:
                i, hh = 15 - step // 2, 1 - step % 2
            tsl = slice(i * 128, (i + 1) * 128)
            ps_ = slice(hh * 64, (hh + 1) * 64)
            cur, nxt = step % 2, (step + 1) % 2
            pcs = [k.psum[4 * d + q_] for q_ in range(4)]
            tcs = [k.tpsum[4 * d + q_] for q_ in range(4)]
            negc = G.neg[d][ps_, i, hd:hd + 1]
            btc = G.beta[d][ps_, i, hd:hd + 1]
            p1, pv_, po_, pst = pcs
            t1_, tv_, to_, tst = tcs
            cx.op("tensor", lambda h, p1=p1, tsl=tsl, d=d: h.matmul(p1[:, 0:128], WnT[d][:, tsl], Sb[d], start=True, stop=True), r=[tWn[d][i], tS[d]], w=[t1_])
            cx.op(V, lambda h, p1=p1, ps_=ps_, btc=btc, d=d, i=i: h.scalar_tensor_tensor(out=vn[d][ps_, :], in0=p1[ps_, 0:128], scalar=btc, in1=U0b[d][ps_, i, :], op0=ALU.mult, op1=ALU.add),
                  r=[t1_, tU0[d][i], G.T], w=[tv[d]])
            cx.op("tensor", lambda h, po_=po_, tsl=tsl, d=d: h.matmul(po_[:, 0:128], qgT[d][:, tsl], Sb[d], start=True, stop=False), r=[tqg[d][i], tS[d]], w=[to_], inc=False)
            cx.op("tensor", lambda h, po_=po_, ps_=ps_, d=d, i=i: h.matmul(po_[:, 0:128], QKm[d][ps_, i, :], vn[d][ps_, :], start=False, stop=True), r=[tQK[d][i], tv[d]], w=[to_])
            cx.op("tensor", lambda h, pst=pst, ps_=ps_, d=d, i=i: h.matmul(pst[:, 0:128], Kd[d][ps_, i, :], vn[d][ps_, :], start=True, stop=True), r=[tKd[d][i], tv[d]], w=[tst])
            cx.op("gpsimd" if False else V, lambda h, po_=po_, ps_=ps_, i=i: h.tensor_tensor(out=G.osum[ps_, i, :], in0=po_[ps_, 0:128], in1=G.osum[ps_, i, :], op=ALU.add), r=[to_, G.tosum[i]], w=[G.tosum[i]])
            etc = etot[d][:, 2 * i + hh:2 * i + hh + 1]
            cx.op(V, lambda h, pst=pst, d=d, cur=cur, nxt=nxt, etc=etc: h.scalar_tensor_tensor(out=Sf[d][nxt], in0=Sf[d][cur], scalar=etc, in1=pst[:, 0:128], op0=ALU.mult, op1=ALU.add),
                  r=[tst, tet[d][i], tS[d]], w=[tS[d]])
            cx.op("scalar", lambda h, d=d, nxt=nxt: h.copy(Sb[d], Sf[d][nxt]), r=[tS[d]], w=[tS[d]])
    if hd == 0:
        k.dbg_add("gdn_osum", G.osum, G.tosum)
    if STOP == "C":
        cx.barrier()
        ar.release(m0)
        return
    ss = ar.alloc([NT, 2], F32)
    tss = Trk()
    junk = ar.alloc([128], BF16)
    zs = [ar.alloc([128], F32) for _ in range(2)]
    tzs = [Trk(), Trk()]
    yb = [ar.alloc([128], BF16) for _ in range(2)]
    tyb = [Trk(), Trk()]
    for i in range(NT):
        cx.op("scalar", lambda h, i=i: h.activation(junk, G.osum[:, i, :], AF.Square, accum_out=ss[:, i, 0:1]), r=[G.tosum[i], tss], w=[tss])
    cx.op(V, lambda h: h.tensor_scalar(ss[:, :, 1:2], ss[:, :, 0:1], 1.0 / 128, EPS, op0=ALU.mult, op1=ALU.add), r=[tss], w=[tss])
    cx.op("scalar", lambda h: h.activation(ss[:, :, 1:2], ss[:, :, 1:2], AF.Sqrt), r=[tss], w=[tss])
    cx.op(V, lambda h: h.reciprocal(ss[:, :, 1:2], ss[:, :, 1:2]), r=[tss], w=[tss])
    for i in range(NT):
        b = i % 2
        pz = k.psum[b]
        tz = k.tpsum[b]
        for c in range(8):
            cx.op("tensor", lambda h, pz=pz, c=c, i=i: h.matmul(pz[:, 0:128], k.hT[:, c, i * 128:(i + 1) * 128], wz[:, c, :], start=(c == 0), stop=(c == 7)),
                  r=[twz, k.t_hT], w=[tz], inc=(c == 7))
        cx.op("scalar", lambda h, pz=pz, b=b: h.activation(zs[b], pz[:, 0:128], AF.Silu), r=[tz], w=[tzs[b]])
        s1 = ss[:, i, 1:2]
        cx.op(V, lambda h, i=i, s1=s1: h.scalar_tensor_tensor(out=G.osum[:, i, :], in0=G.osum[:, i, :], scalar=s1, in1=G.nw, op0=ALU.mult, op1=ALU.mult), r=[tss, G.tpr, G.tosum[i]], w=[G.tosum[i]])
        cx.op(V, lambda h, i=i, b=b: h.tensor_tensor(out=yb[b], in0=G.osum[:, i, :], in1=zs[b], op=ALU.mult), r=[G.tosum[i], tzs[b]], w=[tyb[b]])
        pt = k.psum[2 + b].bitcast(BF16)
        tt_ = k.tpsum[2 + b]
        cx.op("tensor", lambda h, pt=pt, b=b: h.transpose(pt[:, 0:128], yb[b], k.identb), r=[tyb[b]], w=[tt_])
        cx.op("scalar", lambda h, pt=pt, i=i: h.copy(yT[:, 4 + hd, i * 128:(i + 1) * 128], pt[:, 0:128]), r=[tt_], w=[t_yT])
    cx.barrier()
    ar.release(m0)


def out_proj(k, yT, t_yT):
    cx, ar, D = k.cx, k.ar, k.D
    m0 = ar.mark()
    wo = ar.alloc([8, 1024], BF16)
    two = Trk()
    wsrc = D["w_out"]
    sl_ = cx.fresh('sw')
    for c in range(8):
        cx.dma("gpsimd", wo[:, c, :], wsrc[c * 128:(c + 1) * 128, :], sl_, w=[two])
    for g4 in range(4):
        slx = cx.fresh()
        for i in range(g4 * 4, g4 * 4 + 4):
            cx.dma("sync", k.xacc[:, i, :], D["x"][i * 128:(i + 1) * 128, :], slx, w=[k.txacc[i]])
        for i in range(g4 * 4, g4 * 4 + 4):
            k.txacc[i].w = (slx.key, slx.total)
    for i in range(NT):
        for half in range(2):
            pb = k.psum[(2 * i + half) % 4]
            tp = k.tpsum[(2 * i + half) % 4]
            for c in range(8):
                cx.op("tensor", lambda h, pb=pb, c=c, i=i, half=half: h.matmul(pb[:, :], yT[:, c, i * 128:(i + 1) * 128], wo[:, c, half * 512:(half + 1) * 512], start=(c == 0), stop=(c == 7)),
                      r=[t_yT, two], w=[tp], inc=(c == 7))
            xs = k.xacc[:, i, half * 512:(half + 1) * 512]
            cx.op("vector", lambda h, pb=pb, xs=xs: h.tensor_tensor(out=xs, in0=pb[:, :], in1=xs, op=ALU.add), r=[tp, k.txacc[i]], w=[k.txacc[i]])
    cx.barrier()
    ar.release(m0)


def xattn(k):
    cx, ar, D = k.cx, k.ar, k.D
    V = "vector"
    m0 = ar.mark()
    wq = [ar.alloc([8, 256], BF16) for _ in range(2)]
    wk = [ar.alloc([8, 256], BF16) for _ in range(2)]
    wv = [ar.alloc([8, 256], BF16) for _ in range(2)]
    wo = [ar.alloc([2, 1024], BF16) for _ in range(2)]
    twA = [Trk(), Trk()]
    two_ = [Trk(), Trk()]
    swA = [cx.slot("xwa0"), cx.slot("xwa1")]
    swO = [cx.slot("xwo0"), cx.slot("xwo1")]
    def loadA(hd):
        b = hd % 2
        c0 = hd * 256
        for (dst, nm) in ((wq[b], "xa_wq"), (wk[b], "xa_wk"), (wv[b], "xa_wv")):
            load_w_cols(k, D[nm], c0, 256, dst, twA[b], swA[b])

    def loadO(hd):
        b = hd % 2
        c0 = hd * 256
        src = D["xa_wo"]
        cx.dma("gpsimd", wo[b], bass.AP(src.tensor, src.offset + c0 * 1024, [[1024, 128], [128 * 1024, 2], [1, 1024]]), swO[b], w=[two_[b]])

    loadA(0)
    loadA(1)
    loadO(0)
    loadO(1)
    xnT = ar.alloc([8, 2048], BF16)
    t_xnT = Trk()
    memT = ar.alloc([8, 256], BF16)
    t_memT = Trk()
    m1 = ar.mark()
    mt = [ar.alloc([1024], F32) for _ in range(2)]
    tmt = [Trk(), Trk()]

    def src_mem(i):
        cx.dma("sync", mt[i], D["mem"][i * 128:(i + 1) * 128, :], cx.fresh(), w=[tmt[i]])
        return mt[i], tmt[i]
    norm_transpose(k, "mem", src_mem, 2, D["norm_mem"], memT, BF16, t_memT)
    ar.release(m1)
    norm_transpose(k, "xa", lambda i: (k.xacc[:, i, :], k.txacc[i]), NT, D["norm_xattn"], xnT, BF16, t_xnT, resident=True)
    k.dbg_add("xa_memT", memT, [t_memT])
    k.dbg_add("xa_xnT", xnT, [t_xnT])
    kTb = [ar.alloc([2, 256], BF16) for _ in range(2)]
    vhb = [ar.alloc([2, 256], BF16) for _ in range(2)]
    tkvb = [Trk(), Trk()]
    qTb = [ar.alloc([2, 2048], BF16) for _ in range(2)]
    tqTb = [Trk(), Trk()]
    E = [ar.alloc([2, 512], BF16) for _ in range(2)]
    tE = [Trk(), Trk()]
    rden = [ar.alloc([512], F32) for _ in range(2)]
    trd = [Trk(), Trk()]
    oTn = [ar.alloc([2, 512], BF16) for _ in range(2)]
    toT = [Trk(), Trk()]
    cx.barrier()
    k.txa = [[Trk(), Trk()] for _ in range(NT)]

    def proj(hd):
        b = hd % 2
        kT, vh, qT, tkv, tqT = kTb[b], vhb[b], qTb[b], tkvb[b], tqTb[b]
        for dc in range(2):
            pb = k.psum[dc]
            tp = k.tpsum[dc]
            for c in range(8):
                cx.op("tensor", lambda h, pb=pb, c=c, dc=dc, b=b: h.matmul(pb[:, 0:256], wk[b][:, c, dc * 128:(dc + 1) * 128], memT[:, c, :], start=(c == 0), stop=(c == 7)),
                      r=[twA[b], t_memT], w=[tp], inc=(c == 7))
            cx.op("scalar", lambda h, pb=pb, dc=dc, kT=kT: h.copy(kT[:, dc, :], pb[:, 0:256]), r=[tp], w=[tkv])
        for mtile in range(2):
            pb = k.psum[2 + mtile]
            tp = k.tpsum[2 + mtile]
            for c in range(8):
                cx.op("tensor", lambda h, pb=pb, c=c, mtile=mtile, b=b: h.matmul(pb[:, 0:256], memT[:, c, mtile * 128:(mtile + 1) * 128], wv[b][:, c, :], start=(c == 0), stop=(c == 7)),
                      r=[twA[b], t_memT], w=[tp], inc=(c == 7))
            cx.op(V, lambda h, pb=pb, mtile=mtile, vh=vh: h.tensor_copy(vh[:, mtile, :], pb[:, 0:256]), r=[tp], w=[tkv])
        for dc in range(2):
            for n in range(4):
                pb = k.psum[(dc * 4 + n) % 4]
                tp = k.tpsum[(dc * 4 + n) % 4]
                for c in range(8):
                    cx.op("tensor", lambda h, pb=pb, c=c, dc=dc, n=n, b=b: h.matmul(pb[:, :], wq[b][:, c, dc * 128:(dc + 1) * 128], xnT[:, c, n * 512:(n + 1) * 512], start=(c == 0), stop=(c == 7)),
                          r=[twA[b], t_xnT], w=[tp], inc=(c == 7))
                if n % 2 == 0:
                    cx.op("scalar", lambda h, pb=pb, dc=dc, n=n, qT=qT: h.copy(qT[:, dc, n * 512:(n + 1) * 512], pb[:, :]), r=[tp], w=[tqT])
                else:
                    cx.op(V, lambda h, pb=pb, dc=dc, n=n, qT=qT: h.tensor_copy(qT[:, dc, n * 512:(n + 1) * 512], pb[:, :]), r=[tp], w=[tqT])

    def chunks(hd):
        b = hd % 2
        kT, vh, qT, tkv, tqT = kTb[b], vhb[b], qTb[b], tkvb[b], tqTb[b]
        tw = [two_[0], two_[1]]

        def emit_scores(n):
            eb = n % 2
            ts = slice(n * 512, (n + 1) * 512)
            for mtile in range(2):
                pb = k.psum[mtile]
                tp = k.tpsum[mtile]
                for dc in range(2):
                    cx.op("tensor", lambda h, pb=pb, dc=dc, mtile=mtile, ts=ts: h.matmul(pb[:, :], kT[:, dc, mtile * 128:(mtile + 1) * 128], qT[:, dc, ts], start=(dc == 0), stop=(dc == 1)),
                          r=[tkv, tqT], w=[tp], inc=(dc == 1))
                cx.op("scalar", lambda h, pb=pb, mtile=mtile, eb=eb: h.activation(E[eb][:, mtile, :], pb[:, :], AF.Exp, scale=1.0 / 16.0), r=[tp], w=[tE[eb]])

        def emit_rest(n):
            eb = n % 2
            pd = k.psum[2]
            tpd = k.tpsum[2]
            for mtile in range(2):
                cx.op("tensor", lambda h, pd=pd, mtile=mtile, eb=eb: h.matmul(pd[:, :], k.onesb, E[eb][:, mtile, :], start=(mtile == 0), stop=(mtile == 1)), r=[tE[eb]], w=[tpd], inc=(mtile == 1))
            cx.op(V, lambda h, pd=pd, eb=eb: h.reciprocal(rden[eb], pd[:, :]), r=[tpd], w=[trd[eb]])
            for dc in range(2):
                po = k.psum[3 + dc]
                tpo = k.tpsum[3 + dc]
                for mtile in range(2):
                    cx.op("tensor", lambda h, po=po, mtile=mtile, dc=dc, eb=eb: h.matmul(po[:, :], vh[:, mtile, dc * 128:(dc + 1) * 128], E[eb][:, mtile, :], start=(mtile == 0), stop=(mtile == 1)),
                          r=[tkv, tE[eb]], w=[tpo], inc=(mtile == 1))
                cx.op(V, lambda h, po=po, dc=dc, eb=eb: h.tensor_tensor(out=oTn[eb][:, dc, :], in0=po[:, :], in1=rden[eb], op=ALU.mult), r=[tpo, trd[eb]], w=[toT[eb]])
            for t in range(4):
                i = n * 4 + t
                for half in range(2):
                    pw_ = k.psum[5 + (t * 2 + half) % 3]
                    tpw = k.tpsum[5 + (t * 2 + half) % 3]
                    for dc in range(2):
                        cx.op("tensor", lambda h, pw_=pw_, dc=dc, t=t, half=half, b=b, eb=eb: h.matmul(pw_[:, :], oTn[eb][:, dc, t * 128:(t + 1) * 128], wo[b][:, dc, half * 512:(half + 1) * 512], start=(dc == 0), stop=(dc == 1)),
                              r=[toT[eb], tw[b]], w=[tpw], inc=(dc == 1))
                    xs = k.xacc[:, i, half * 512:(half + 1) * 512]
                    cx.op(V, lambda h, pw_=pw_, xs=xs: h.tensor_tensor(out=xs, in0=pw_[:, :], in1=xs, op=ALU.add), r=[tpw, k.txa[i][half]], w=[k.txa[i][half]])
        emit_scores(0)
        for n in range(4):
            if n + 1 < 4:
                emit_scores(n + 1)
            emit_rest(n)
    proj(0)
    for hd in range(4):
        if hd + 1 < 4:
            proj(hd + 1)
        if hd + 2 < 4:
            loadA(hd + 2)
        chunks(hd)
        if hd + 2 < 4:
            loadO(hd + 2)
    cx.barrier()
    ar.release(m0)


def moe(k):
    cx, ar, D = k.cx, k.ar, k.D
    V = "vector"
    m0 = ar.mark()
    wgu = [ar.alloc([8, 512], BF16) for _ in range(2)]
    wd = [ar.alloc([2, 1024], BF16) for _ in range(2)]
    twe = [Trk(), Trk()]
    swe = [cx.slot("we0"), cx.slot("we1")]
    NE = k.n_experts

    def load_e(e):
        b = e % 2
        g_, u_, d_ = D["moe_w_gate"], D["moe_w_up"], D["moe_w_down"]
        cx.dma("gpsimd", wgu[b][:, :, 0:256], bass.AP(g_.tensor, g_.offset + e * 1024 * 256, [[256, 128], [256 * 128, 8], [1, 256]]), swe[b], w=[twe[b]])
        cx.dma("gpsimd", wgu[b][:, :, 256:512], bass.AP(u_.tensor, u_.offset + e * 1024 * 256, [[256, 128], [256 * 128, 8], [1, 256]]), swe[b], w=[twe[b]])
        cx.dma("gpsimd", wd[b], bass.AP(d_.tensor, d_.offset + e * 256 * 1024, [[1024, 128], [1024 * 128, 2], [1, 1024]]), swe[b], w=[twe[b]])
    import os
    NOLOAD = os.environ.get("MOE_NOLOAD", "") == "1"
    load_e(0)
    if NE > 1:
        load_e(1)
    xnT = ar.alloc([8, 2048], BF16)
    t_xnT = Trk()
    norm_transpose(k, "moe", lambda i: (k.xacc[:, i, :], k.txacc[i]), NT, D["norm_moe"], xnT, BF16, t_xnT, resident=True)
    wr = ar.alloc([8, 36], BF16)
    twr = Trk()
    sl_ = cx.fresh('sw')
    srcg, srce = D["router_group_w"], D["router_expert_w"]
    cx.dma("gpsimd", wr[:, :, 0:4], bass.AP(srcg.tensor, srcg.offset, [[4, 128], [4 * 128, 8], [1, 4]]), sl_, w=[twr])
    cx.dma("gpsimd", wr[:, :, 4:36], bass.AP(srce.tensor, srce.offset, [[32, 128], [32 * 128, 8], [1, 32]]), sl_, w=[twr])
    rb = ar.alloc([36], F32)
    trb = Trk()
    sl2 = cx.fresh()
    cx.dma("sync", rb[:, 0:4], dram_bcast(D["router_group_b"], 128, 4), sl2, w=[trb])
    cx.dma("sync", rb[:, 4:36], dram_bcast(D["router_expert_b"], 128, 32), sl2, w=[trb])
    cw = ar.alloc([NT, 32], F32)
    tcw = Trk()
    lgA = ar.alloc([NT, 36], F32)
    msk = ar.alloc([NT, 32], F32)
    eq2 = ar.alloc([NT, 32], F32)
    m8 = ar.alloc([NT, 8], F32)
    sc = ar.alloc([8, NT], F32)
    oh = ar.alloc([NT, 4], F32)
    ex = ar.alloc([NT, 4], F32)
    T = Trk()
    rbb = rb.unsqueeze(1).to_broadcast([128, 8, 36])
    for half in range(2):
        pb = k.psum[half]
        tp = k.tpsum[half]
        for ii in range(8):
            i = half * 8 + ii
            for c in range(8):
                cx.op("tensor", lambda h, pb=pb, c=c, i=i, ii=ii: h.matmul(pb[:, ii * 36:(ii + 1) * 36], xnT[:, c, i * 128:(i + 1) * 128], wr[:, c, :], start=(c == 0), stop=(c == 7)),
                      r=[t_xnT, twr], w=[tp], inc=(c == 7))
        cx.op(V, lambda h, pb=pb, half=half: h.tensor_tensor(out=lgA[:, half * 8:(half + 1) * 8, :], in0=pb[:, 0:288].rearrange("p (t e) -> p t e", t=8), in1=rbb, op=ALU.add), r=[tp, trb, T], w=[T])
    lg_g = lgA[:, :, 0:4]
    lg_e = lgA[:, :, 4:36]
    gmax, ngs, ssum, ptop, dm, w1, w2 = [sc[:, j_, :] for j_ in range(7)]

    def vop(fn):
        cx.op(V, fn, r=[T], w=[T])

    def aop(fn):
        cx.op("scalar", fn, r=[T], w=[T])
    b4 = lambda v: v.unsqueeze(2).to_broadcast([128, NT, 4])
    b32 = lambda v: v.unsqueeze(2).to_broadcast([128, NT, 32])
    vop(lambda h: h.tensor_reduce(out=gmax, in_=lg_g, axis=AX.X, op=ALU.max))
    vop(lambda h: h.tensor_tensor(out=oh, in0=lg_g, in1=b4(gmax), op=ALU.is_equal))
    vop(lambda h: h.tensor_tensor(out=ex, in0=lg_g, in1=b4(gmax), op=ALU.subtract))
    aop(lambda h: h.activation(ex, ex, AF.Exp))
    vop(lambda h: h.tensor_reduce(out=ssum, in_=ex, axis=AX.X, op=ALU.add))
    vop(lambda h: h.reciprocal(ptop, ssum))
    vop(lambda h: h.tensor_scalar(oh, oh, -1.0, 1e30, op0=ALU.add, op1=ALU.mult))
    vop(lambda h: h.tensor_tensor(out=msk.rearrange("p t (g e) -> p t g e", g=4), in0=lg_e.rearrange("p t (g e) -> p t g e", g=4),
                                  in1=oh.unsqueeze(3).to_broadcast([128, NT, 4, 8]), op=ALU.add))
    for i in range(NT):
        vop(lambda h, i=i: h.max(out=m8[:, i, :], in_=msk[:, i, :]))
    m1, m2 = m8[:, :, 0], m8[:, :, 1]
    vop(lambda h: h.tensor_tensor(out=dm, in0=m2, in1=m1, op=ALU.subtract))
    aop(lambda h: h.activation(dm, dm, AF.Exp))
    vop(lambda h: h.tensor_scalar(w1, dm, 1.0, None, op0=ALU.add))
    vop(lambda h: h.reciprocal(w1, w1))
    vop(lambda h: h.tensor_tensor(out=w2, in0=dm, in1=w1, op=ALU.mult))
    vop(lambda h: h.tensor_tensor(out=w1, in0=w1, in1=ptop, op=ALU.mult))
    vop(lambda h: h.tensor_tensor(out=w2, in0=w2, in1=ptop, op=ALU.mult))
    vop(lambda h: h.tensor_tensor(out=eq2, in0=msk, in1=b32(m2), op=ALU.is_equal))
    vop(lambda h: h.tensor_tensor(out=eq2, in0=eq2, in1=b32(w2), op=ALU.mult))
    vop(lambda h: h.tensor_tensor(out=msk, in0=msk, in1=b32(m1), op=ALU.is_equal))
    vop(lambda h: h.tensor_tensor(out=msk, in0=msk, in1=b32(w1), op=ALU.mult))
    cx.op(V, lambda h: h.tensor_tensor(out=cw, in0=msk, in1=eq2, op=ALU.add), r=[T], w=[tcw, T])
    k.dbg_add("moe_cw", cw, [tcw])
    sg = [ar.alloc([512], F32) for _ in range(2)]
    tsg = [Trk(), Trk()]
    h1 = [ar.alloc([2, 512], BF16) for _ in range(2)]
    th1 = [Trk(), Trk()]
    jobs = [(e, n) for e in range(NE) for n in range(4)]
    state = {"cnt": 0, "loaded": 0}
    cx.barrier()
    k.txh = [[Trk(), Trk()] for _ in range(NT)]

    def emit_gu(j, fh):
        e, n = jobs[j]
        b = e % 2
        ts = slice(n * 512, (n + 1) * 512)
        hb = j % 2
        pg = k.psum[fh * 2]
        tpg = k.tpsum[fh * 2]
        pu = k.psum[fh * 2 + 1]
        tpu = k.tpsum[fh * 2 + 1]
        for c in range(8):
            cx.op("tensor", lambda h, pg=pg, c=c, fh=fh, ts=ts, b=b: h.matmul(pg[:, :], wgu[b][:, c, fh * 128:(fh + 1) * 128], xnT[:, c, ts], start=(c == 0), stop=(c == 7)),
                  r=[twe[b], t_xnT], w=[tpg], inc=(c == 7))
        for c in range(8):
            cx.op("tensor", lambda h, pu=pu, c=c, fh=fh, ts=ts, b=b: h.matmul(pu[:, :], wgu[b][:, c, 256 + fh * 128:256 + (fh + 1) * 128], xnT[:, c, ts], start=(c == 0), stop=(c == 7)),
                  r=[twe[b], t_xnT], w=[tpu], inc=(c == 7))
        cx.op("scalar", lambda h, pg=pg, fh=fh: h.activation(sg[fh], pg[:, :], AF.Silu), r=[tpg], w=[tsg[fh]])
        cx.op(V, lambda h, pu=pu, fh=fh, hb=hb: h.tensor_tensor(out=h1[hb][:, fh, :], in0=pu[:, :], in1=sg[fh], op=ALU.mult), r=[tpu, tsg[fh]], w=[th1[hb]])

    def emit_down(j):
        e, n = jobs[j]
        b = e % 2
        hb = j % 2
        for t in range(4):
            i = n * 4 + t
            for half in range(2):
                pdn = k.psum[4 + state["cnt"] % 4]
                tpd = k.tpsum[4 + state["cnt"] % 4]
                state["cnt"] += 1
                for fh in range(2):
                    cx.op("tensor", lambda h, pdn=pdn, fh=fh, t=t, half=half, hb=hb, b=b: h.matmul(pdn[:, :], h1[hb][:, fh, t * 128:(t + 1) * 128], wd[b][:, fh, half * 512:(half + 1) * 512], start=(fh == 0), stop=(fh == 1)),
                          r=[th1[hb], twe[b]], w=[tpd], inc=(fh == 1))
                xs = k.xacc[:, i, half * 512:(half + 1) * 512]
                cwc = cw[:, i, e:e + 1]
                cx.op(V, lambda h, pdn=pdn, xs=xs, cwc=cwc: h.scalar_tensor_tensor(out=xs, in0=pdn[:, :], scalar=cwc, in1=xs, op0=ALU.mult, op1=ALU.add), r=[tpd, tcw, k.txh[i][half]], w=[k.txh[i][half]])
        if n == 3 and e + 2 < NE and not NOLOAD:
            load_e(e + 2)
    nj = len(jobs)
    if nj > 0:
        emit_gu(0, 0)
        emit_gu(0, 1)
        for j in range(nj):
            if j + 1 < nj:
                emit_gu(j + 1, 0)
            emit_down(j)
            if j + 1 < nj:
                emit_gu(j + 1, 1)
    cx.barrier()
    ar.release(m0)


def final_norm(k, out):
    cx, ar, D = k.cx, k.ar, k.D
    V = "vector"
    m0 = ar.mark()
    gB = ar.alloc([1024], F32)
    tg = Trk()
    cx.dma("sync", gB, dram_bcast(D["norm_final"], 128, 1024), cx.fresh(), w=[tg])
    junk = ar.alloc([1024], BF16)
    tj = Trk()
    ss = ar.alloc([NT, 2], F32)
    tss = Trk()
    ob = [ar.alloc([1024], F32) for _ in range(2)]
    tob = [Trk(), Trk()]
    so = [cx.slot("o0"), cx.slot("o1")]
    for i in range(NT):
        txs = [k.txacc[i]] + (k.txh[i] if hasattr(k, "txh") else [])
        cx.op("scalar", lambda h, i=i: h.activation(junk, k.xacc[:, i, :], AF.Square, accum_out=ss[:, i, 0:1]), r=txs + [tss], w=[tj, tss])
    cx.op(V, lambda h: h.tensor_scalar(ss[:, :, 1:2], ss[:, :, 0:1], 1.0 / 1024, EPS, op0=ALU.mult, op1=ALU.add), r=[tss], w=[tss])
    cx.op("scalar", lambda h: h.activation(ss[:, :, 1:2], ss[:, :, 1:2], AF.Sqrt), r=[tss], w=[tss])
    cx.op(V, lambda h: h.reciprocal(ss[:, :, 1:2], ss[:, :, 1:2]), r=[tss], w=[tss])
    for i in range(NT):
        b = i % 2
        s1 = ss[:, i, 1:2]
        txs = [k.txacc[i]] + (k.txh[i] if hasattr(k, "txh") else [])
        cx.op(V, lambda h, i=i, s1=s1, b=b: h.scalar_tensor_tensor(out=ob[b], in0=k.xacc[:, i, :], scalar=s1, in1=gB, op0=ALU.mult, op1=ALU.mult), r=txs + [tss, tg], w=[tob[b]])
        cx.dma("sync", out[i * 128:(i + 1) * 128, :], ob[b], so[b], r=[tob[b]])
    cx.barrier()
    ar.release(m0)


_CACHE = {}


def kernel(**inputs):
    inp = {k_: np.asarray(v) for k_, v in inputs.items()}
    n = inp["x"].shape[0]
    maps = [host_inputs(inp, b) for b in range(n)]
    key = "full"
    if key not in _CACHE:
        shapes = {k_: (v.shape, np2dt(v)) for k_, v in maps[0].items()}
        _CACHE[key] = build(shapes)[0]
    nc = _CACHE[key]
    res = run_bass_kernel_spmd(nc, maps, core_ids=list(range(n)))
    return np.stack([np.asarray(r["out"], dtype=np.float32) for r in res.results], 0)
```

```python
import contextlib
import os
import math
import numpy as np
import ml_dtypes
import concourse.bass as bass
import concourse.mybir as mybir
from concourse.bass_utils import run_bass_kernel_spmd

F32 = mybir.dt.float32
BF16 = mybir.dt.bfloat16
F32R = mybir.dt.float32r
I32 = mybir.dt.int32
AF = mybir.ActivationFunctionType
ALU = mybir.AluOpType
AX = mybir.AxisListType

ENGS = ("sync", "scalar", "gpsimd", "vector", "tensor")
ATTACH_WAIT = os.environ.get("ATTACH_WAIT", "1") == "1"
S = 2048
DM = 1024
NT = 16
EPS = 1e-6


class Trk:
    __slots__ = ("name", "w", "r", "excl")

    def __init__(self, name="", excl=False):
        self.name = name
        self.w = None
        self.r = {}
        self.excl = excl


class DmaSlot:
    def __init__(self, ctx, name):
        self.key = "d_" + name + str(ctx.nsem)
        ctx.sems[self.key] = ctx.new_sem(self.key)
        self.total = 0


class Ctx:
    def __init__(self, nc, stack):
        self.nc = nc
        self.stack = stack
        self.q = {e: [] for e in ENGS}
        self.sems = {}
        self.nsem = 0
        self.cnt = {e: 0 for e in ENGS}
        self.known = {e: {} for e in ENGS}
        for e in ENGS:
            self.sems[e] = self.new_sem("s_" + e)
        self.slots = []
        self.pools = {}
        self.pool_idx = {}
        self.n_ops = 0

    def new_sem(self, name):
        self.nsem += 1
        return self.stack.enter_context(self.nc.semaphore(name))

    def slot(self, name):
        s = DmaSlot(self, name)
        self.slots.append(s)
        return s

    def fresh(self, kind="hw"):
        pool = self.pools.setdefault(kind, [])
        i = self.pool_idx.get(kind, 0)
        if i >= len(pool):
            assert len(pool) < 30, "slot pool exhausted"
            pool.append(self.slot(kind + "%d" % len(pool)))
            pool[-1].kind = kind
        self.pool_idx[kind] = i + 1
        return pool[i]

    def sb(self, name, shape, dt):
        return self.stack.enter_context(self.nc.sbuf_tensor("sb_" + name, list(shape), dt))

    def ps(self, name, shape, dt=F32):
        return self.stack.enter_context(self.nc.psum_tensor(name, list(shape), dt))

    def _waits_for(self, eng, r, w, extra=()):
        need = {}

        def req(dep, raw=True):
            if dep is None:
                return
            k, c = dep
            if k == eng and eng in ("tensor", "sync"):
                return
            if k == eng and not raw:
                return
            if c > need.get(k, 0):
                need[k] = c
        for t in r:
            req(t.w)
        for t in w:
            req(t.w, raw=False)
            for k, c in t.r.items():
                req((k, c), raw=False)
        for d in extra:
            req(d)
        out = []
        kn = self.known[eng]
        for k, c in need.items():
            if kn.get(k, 0) < c:
                kn[k] = c
                out.append((self.sems[k], c))
        return out

    def op(self, eng, fn, r=(), w=(), inc=True, extra=()):
        w = list(w) + [t for t in r if t.excl]
        r = [t for t in r if not t.excl]
        waits = self._waits_for(eng, r, w, extra)
        c = self.cnt[eng] + 1
        if inc:
            self.cnt[eng] = c
        sem = self.sems[eng]

        def emit(h, fn=fn, waits=waits, inc=inc, sem=sem):
            for s, v in waits[:-1]:
                h.wait_ge(s, v)
            ins = fn(h)
            if waits:
                if ATTACH_WAIT:
                    ins._wait_ge(waits[-1][0], waits[-1][1])
                else:
                    raise RuntimeError
            if inc:
                ins.then_inc(sem, 1)
        if not ATTACH_WAIT:
            def emit(h, fn=fn, waits=waits, inc=inc, sem=sem):
                for s, v in waits:
                    h.wait_ge(s, v)
                ins = fn(h)
                if inc:
                    ins.then_inc(sem, 1)
        self.q[eng].append(emit)
        for t in r:
            t.r[eng] = c
        for t in w:
            t.w = (eng, c)
            t.r = {}
        self.n_ops += 1

    def dma(self, eng, out, in_, slot, r=(), w=(), extra=(), **kw):
        kind = "sw" if eng == "gpsimd" else "hw"
        assert getattr(slot, "kind", kind) == kind, ("DMA slot kind mismatch", slot.key, eng)
        slot.kind = kind
        waits = self._waits_for(eng, r, w, extra)
        slot.total += 16
        sem = self.sems[slot.key]

        def emit(h, waits=waits, sem=sem, out=out, in_=in_, kw=kw):
            for s, v in waits:
                h.wait_ge(s, v)
            h.dma_start(out=out, in_=in_, **kw).then_inc(sem, 16)
        self.q[eng].append(emit)
        dep = (slot.key, slot.total)
        for t in r:
            t.r[slot.key] = slot.total
        for t in w:
            t.w = dep
            t.r = {}
        self.n_ops += 1
        return dep

    def wait_deps(self, eng, deps):
        waits = self._waits_for(eng, (), (), deps)

        def emit(h, waits=waits):
            for s, v in waits:
                h.wait_ge(s, v)
        self.q[eng].append(emit)

    def barrier(self):
        deps = [(e, self.cnt[e]) for e in ENGS if e != "sync" and self.cnt[e] > 0]
        deps += [(s.key, s.total) for s in self.slots if s.total > 0]
        for e in ENGS:
            self.wait_deps(e, deps)
        self.pool_idx = {}

    def emit_all(self, block):
        q = self.q

        @block.sync
        def _(h):
            for f in q["sync"]:
                f(h)

        @block.scalar
        def _(h):
            for f in q["scalar"]:
                f(h)

        @block.gpsimd
        def _(h):
            for f in q["gpsimd"]:
                f(h)

        @block.vector
        def _(h):
            for f in q["vector"]:
                f(h)

        @block.tensor
        def _(h):
            for f in q["tensor"]:
                f(h)


class Arena:
    def __init__(self, cx, words, base=None):
        self.t = cx.sb("arena", [128, words], F32) if base is None else base
        self.cx = cx
        self.words = words
        self.top = 0

    def mark(self):
        return self.top

    def release(self, m):
        if m != self.top:
            self.cx.barrier()
        self.top = m

    def alloc(self, shape, dt):
        n = int(np.prod(shape))
        w = n if dt in (F32, F32R, I32) else (n + 1) // 2
        w = (w + 1) // 2 * 2
        o = self.top
        self.top += w
        assert self.top <= self.words, ("arena overflow", self.top, self.words)
        v = self.t[:, o:o + w]
        if dt != F32:
            v = v.bitcast(dt)
        v = v[:, 0:n]
        if len(shape) > 1:
            names = " ".join("d%d" % i for i in range(len(shape)))
            v = v.rearrange("p (%s) -> p %s" % (names, names), **{"d%d" % i: shape[i] for i in range(len(shape))})
        return v


def pap(ap, part0, nparts, off, dims):
    base = ap.ap[0][0]
    return bass.AP(ap.tensor, ap.offset + part0 * base + off, [[base, nparts]] + [list(d) for d in dims])


def host_consts():
    c = {}
    c["ident"] = np.eye(128, dtype=np.float32)
    c["identb"] = np.eye(128, dtype=np.float32).astype(ml_dtypes.bfloat16)
    c["ones"] = np.ones((128, 128), np.float32)
    selT = np.zeros((128, 2, 8, 128), np.float32)
    selB = np.zeros((128, 2, 8, 128), np.float32)
    for q in range(4):
        for r in range(32):
            loc, cc = r // 16, r % 16
            for s in range(8):
                selT[q * 32 + r, loc, s, s * 16 + cc] = 1.0
                selB[q * 32 + r, loc, s, s * 16 + cc] = 1.0
    c["selT"] = selT.astype(ml_dtypes.bfloat16)
    c["selB"] = selB.astype(ml_dtypes.bfloat16)
    sidx = np.arange(128) // 16
    c["s5mf"] = (sidx[None, :] >= sidx[:, None]).astype(np.float32)
    c["s5mb"] = (sidx[None, :] <= sidx[:, None]).astype(np.float32)
    c["kvec"] = np.tile((np.arange(16, dtype=np.float32) - 7.0)[None, :], (128, 1))
    k = np.arange(128)[:, None]
    cc = np.arange(128)[None, :]
    same = (k // 64) == (cc // 64)
    gm = np.zeros((128, 8, 128), np.float32)
    gm[:, 0] = same & (k <= cc)
    gm[:, 1] = same & (k >= cc)
    gm[:, 2] = np.where(same & (cc >= k), 0.0, -30000.0)
    gm[:, 3] = np.where(same & (cc <= k), 0.0, -30000.0)
    gm[:, 4] = same & (cc > k)
    gm[:, 5] = same & (cc < k)
    gm[:, 6] = same
    c["gmask"] = gm
    return c


def host_s5(inp):
    o = {}

    pairs = {"lam_re": ("s5_lam_re_f", "s5_lam_re_b"), "lam_im": ("s5_lam_im_f", "s5_lam_im_b"),
             "log_step": ("s5_log_step_f", "s5_log_step_b"), "b_re": ("s5_b_re_f", "s5_b_re_b"),
             "b_im": ("s5_b_im_f", "s5_b_im_b"), "c_re": ("s5_c_re_f", "s5_c_re_b"), "c_im": ("s5_c_im_f", "s5_c_im_b")}

    def st(nm):
        f_, b_ = pairs[nm]
        return np.stack([inp[f_][0], inp[b_][0]], 0)
    lam = np.stack([st("lam_re"), st("lam_im")], 0)
    lam = lam.reshape(2, 2, 2, 16, 64).transpose(2, 4, 0, 1, 3)
    o["s5_lam"] = np.ascontiguousarray(lam.reshape(128, 2, 32))
    ls = st("log_step").reshape(2, 2, 16)
    ls = np.broadcast_to(ls.transpose(1, 0, 2)[:, None], (2, 64, 2, 16))
    o["s5_step"] = np.ascontiguousarray(ls.reshape(128, 32))
    b = np.stack([st("b_re"), st("b_im")], 0)
    b = b.reshape(2, 2, 2, 16, 64, 16).transpose(2, 4, 0, 1, 3, 5)
    o["s5_b"] = np.ascontiguousarray(b.reshape(128, 2, 512))
    cm = np.stack([st("c_re"), st("c_im")], 0)
    cm = cm.reshape(2, 2, 2, 16, 16, 64).transpose(2, 5, 0, 1, 3, 4)
    o["s5_c"] = np.ascontiguousarray(cm.reshape(128, 2, 512))
    d = inp["s5_d"][0].reshape(32, 16)
    o["s5_dvec"] = np.ascontiguousarray(np.broadcast_to(d.T[None], (8, 16, 32)).reshape(128, 32))
    o["s5_bglu"] = np.ascontiguousarray(inp["s5_b_glu"][0].reshape(4, 128).T)
    o["s5_normw"] = np.ascontiguousarray(inp["s5_norm"][0].reshape(4, 128).T)
    return o


def np2dt(a):
    if a.dtype == np.float32:
        return F32
    if a.dtype == ml_dtypes.bfloat16:
        return BF16
    raise ValueError(a.dtype)


class K:
    pass


def dram_bcast(ap, nparts, n, off=0):
    return bass.AP(ap.tensor, ap.offset + off, [[0, nparts], [1, n]])


def norm_transpose(k, name, src_fn, ntiles, gain_dram, outT, out_dt, outT_trk, resident=False):
    cx, ar = k.cx, k.ar
    m = ar.mark()
    gB = ar.alloc([1024], F32)
    tg = Trk()
    cx.dma("sync", gB, dram_bcast(gain_dram, 128, 1024), cx.fresh(), w=[tg])
    junk = ar.alloc([1024], BF16)
    tj = Trk()
    xn = [ar.alloc([1024], out_dt) for _ in range(2)]
    txn = [Trk(), Trk()]
    ss = ar.alloc([NT * 2, 1], F32)
    tss = [Trk() for _ in range(ntiles)]
    pdt = BF16 if out_dt == BF16 else F32
    ident = k.identb if out_dt == BF16 else k.ident
    srcs = []
    if resident:
        for i in range(ntiles):
            src, ts = src_fn(i)
            srcs.append((src, ts))
            cx.op("scalar", lambda h, src=src, i=i: h.activation(junk, src, AF.Square, accum_out=ss[:, 2 * i:2 * i + 1]), r=[ts], w=[tj, tss[0]])
        ssv = ss.rearrange("p (t two) one -> p t (two one)", two=2)
        cx.op("vector", lambda h: h.tensor_scalar(ssv[:, 0:ntiles, 1:2], ssv[:, 0:ntiles, 0:1], 1.0 / 1024, EPS, op0=ALU.mult, op1=ALU.add), r=[tss[0]], w=[tss[0]])
        cx.op("scalar", lambda h: h.activation(ssv[:, 0:ntiles, 1:2], ssv[:, 0:ntiles, 1:2], AF.Sqrt), r=[tss[0]], w=[tss[0]])
        cx.op("vector", lambda h: h.reciprocal(ssv[:, 0:ntiles, 1:2], ssv[:, 0:ntiles, 1:2]), r=[tss[0]], w=[tss[0]])
    for i in range(ntiles):
        rsi = ss[:, 2 * i + 1:2 * i + 2]
        if resident:
            src, ts = srcs[i]
            tsi = tss[0]
        else:
            src, ts = src_fn(i)
            ssi = ss[:, 2 * i:2 * i + 1]
            tsi = tss[i]
            cx.op("scalar", lambda h, src=src, ssi=ssi: h.activation(junk, src, AF.Square, accum_out=ssi), r=[ts], w=[tj, tss[i]])
            cx.op("vector", lambda h, ssi=ssi, rsi=rsi: h.tensor_scalar(rsi, ssi, 1.0 / 1024, EPS, op0=ALU.mult, op1=ALU.add), r=[tss[i]], w=[tss[i]])
            cx.op("scalar", lambda h, rsi=rsi: h.activation(rsi, rsi, AF.Sqrt), r=[tss[i]], w=[tss[i]])
            cx.op("vector", lambda h, rsi=rsi: h.reciprocal(rsi, rsi), r=[tss[i]], w=[tss[i]])
        b = i % 2
        cx.op("vector", lambda h, src=src, rsi=rsi, b=b: h.scalar_tensor_tensor(out=xn[b], in0=src, scalar=rsi, in1=gB, op0=ALU.mult, op1=ALU.mult),
              r=[ts, tsi, tg], w=[txn[b]])
        if out_dt == BF16:
            pb = k.psum[i % 2]
            tp = k.tpsum[i % 2]
            pv = pb.bitcast(BF16)
            for c in range(8):
                cx.op("tensor", lambda h, b=b, c=c, pv=pv: h.transpose(pv[:, c * 128:(c + 1) * 128], xn[b][:, c * 128:(c + 1) * 128], ident),
                      r=[txn[b]], w=[tp], inc=(c == 7))
            dst = outT[:, :, i * 128:(i + 1) * 128]
            eng = "scalar" if i % 2 == 0 else "vector"
            if eng == "scalar":
                cx.op(eng, lambda h, dst=dst, pv=pv: h.copy(dst, pv.rearrange("p (c t) -> p c t", c=8)), r=[tp], w=[outT_trk])
            else:
                cx.op(eng, lambda h, dst=dst, pv=pv: h.tensor_copy(dst, pv.rearrange("p (c t) -> p c t", c=8)), r=[tp], w=[outT_trk])
        else:
            for half in range(2):
                pb = k.psum[(2 * i + half) % 4]
                tp = k.tpsum[(2 * i + half) % 4]
                for c4 in range(4):
                    c = half * 4 + c4
                    cx.op("tensor", lambda h, b=b, c=c, c4=c4, pb=pb: h.transpose(pb[:, c4 * 128:(c4 + 1) * 128], xn[b][:, c * 128:(c + 1) * 128].bitcast(F32), ident),
                          r=[txn[b]], w=[tp], inc=(c4 == 3))
                dst = outT[:, half * 4:(half + 1) * 4, i * 128:(i + 1) * 128]
                if half == 0:
                    cx.op("scalar", lambda h, dst=dst, pb=pb: h.copy(dst, pb.rearrange("p (c t) -> p c t", c=4)), r=[tp], w=[outT_trk])
                else:
                    cx.op("vector", lambda h, dst=dst, pb=pb: h.tensor_copy(dst, pb.rearrange("p (c t) -> p c t", c=4)), r=[tp], w=[outT_trk])
    ar.release(m)


def s5_prep(k):
    cx, ar, D = k.cx, k.ar, k.D
    V = "vector"
    m0 = ar.mark()
    lam = ar.alloc([2, 32], F32)
    step = ar.alloc([32], F32)
    bb = ar.alloc([2, 512], F32)
    cc = ar.alloc([2, 512], F32)
    kvec = ar.alloc([16], F32)
    tl = Trk()
    sl_ = cx.fresh()
    for dst, nm in ((lam, "s5_lam"), (step, "s5_step"), (bb, "s5_b"), (cc, "s5_c"), (kvec, "kvec")):
        cx.dma("sync", dst, D[nm], sl_, w=[tl])
    T = Trk()

    def vop(fn, extra_r=()):
        cx.op(V, fn, r=[T, tl] + list(extra_r), w=[T])

    def aop(fn):
        cx.op("scalar", fn, r=[T, tl], w=[T])
    lre, lim = lam[:, 0, :], lam[:, 1, :]
    dl = ar.alloc([32], F32)
    re1 = ar.alloc([32], F32)
    im1 = ar.alloc([32], F32)
    aop(lambda h: h.activation(dl, step, AF.Exp))
    vop(lambda h: h.tensor_tensor(out=re1, in0=dl, in1=lre, op=ALU.mult))
    vop(lambda h: h.tensor_tensor(out=im1, in0=dl, in1=lim, op=ALU.mult))
    PWI = ar.alloc([16, 32], F32)
    PWR = ar.alloc([16, 32], F32)
    m_pw = ar.mark()
    KR = ar.alloc([16, 32], F32)
    KI = ar.alloc([16, 32], F32)
    kv_b = kvec.unsqueeze(2).to_broadcast([128, 16, 32])
    vop(lambda h: h.tensor_tensor(out=KR, in0=kv_b, in1=re1.unsqueeze(1).to_broadcast([128, 16, 32]), op=ALU.mult))
    vop(lambda h: h.tensor_tensor(out=KI, in0=kv_b, in1=im1.unsqueeze(1).to_broadcast([128, 16, 32]), op=ALU.mult))
    MAG = ar.alloc([16, 32], F32)
    aop(lambda h: h.activation(MAG, KR, AF.Exp))
    YI = ar.alloc([16, 32], I32)
    YF = ar.alloc([16, 32], F32)
    vop(lambda h: h.tensor_scalar(KI, KI, 1.0 / (2 * math.pi), None, op0=ALU.mult))
    vop(lambda h: h.tensor_copy(YI, KI))
    vop(lambda h: h.tensor_copy(YF, YI))
    vop(lambda h: h.tensor_tensor(out=KI, in0=KI, in1=YF, op=ALU.subtract))
    SH_ = ar.alloc([16, 32], F32)
    SQ_ = ar.alloc([16, 32], F32)
    aop(lambda h: h.activation(SH_, KI, AF.Sin, scale=math.pi))
    aop(lambda h: h.activation(SQ_, KI, AF.Sin, scale=math.pi / 2))
    CH_ = ar.alloc([16, 32], F32)
    vop(lambda h: h.tensor_tensor(out=CH_, in0=SQ_, in1=SQ_, op=ALU.mult))
    vop(lambda h: h.tensor_scalar(CH_, CH_, -2.0, 1.0, op0=ALU.mult, op1=ALU.add))
    vop(lambda h: h.tensor_tensor(out=PWI, in0=SH_, in1=CH_, op=ALU.mult))
    vop(lambda h: h.scalar_tensor_tensor(out=PWI, in0=PWI, scalar=2.0, in1=MAG, op0=ALU.mult, op1=ALU.mult))
    vop(lambda h: h.tensor_tensor(out=PWR, in0=SH_, in1=SH_, op=ALU.mult))
    vop(lambda h: h.tensor_scalar(PWR, PWR, -2.0, 1.0, op0=ALU.mult, op1=ALU.add))
    vop(lambda h: h.tensor_tensor(out=PWR, in0=PWR, in1=MAG, op=ALU.mult))
    ar.release(m_pw)
    lrm1 = ar.alloc([32], F32)
    li = PWI[:, 8, :]
    t1 = ar.alloc([32], F32)
    t2 = ar.alloc([32], F32)
    den = ar.alloc([32], F32)
    c0r = ar.alloc([32], F32)
    c0i = ar.alloc([32], F32)
    vop(lambda h: h.tensor_scalar(lrm1, PWR[:, 8, :], -1.0, None, op0=ALU.add))
    vop(lambda h: h.tensor_tensor(out=t1, in0=lre, in1=lre, op=ALU.mult))
    vop(lambda h: h.tensor_tensor(out=t2, in0=lim, in1=lim, op=ALU.mult))
    vop(lambda h: h.tensor_tensor(out=den, in0=t1, in1=t2, op=ALU.add))
    vop(lambda h: h.reciprocal(den, den))
    vop(lambda h: h.tensor_tensor(out=t1, in0=lrm1, in1=lre, op=ALU.mult))
    vop(lambda h: h.tensor_tensor(out=t2, in0=li, in1=lim, op=ALU.mult))
    vop(lambda h: h.tensor_tensor(out=t1, in0=t1, in1=t2, op=ALU.add))
    vop(lambda h: h.tensor_tensor(out=c0r, in0=t1, in1=den, op=ALU.mult))
    vop(lambda h: h.tensor_tensor(out=t1, in0=li, in1=lre, op=ALU.mult))
    vop(lambda h: h.tensor_tensor(out=t2, in0=lrm1, in1=lim, op=ALU.mult))
    vop(lambda h: h.tensor_tensor(out=t1, in0=t1, in1=t2, op=ALU.subtract))
    vop(lambda h: h.tensor_tensor(out=c0i, in0=t1, in1=den, op=ALU.mult))
    BBR = ar.alloc([32, 16], F32)
    BBI = ar.alloc([32, 16], F32)
    TA = ar.alloc([32, 16], F32)
    br = bb[:, 0, :].rearrange("p (a c) -> p a c", c=16)
    bi = bb[:, 1, :].rearrange("p (a c) -> p a c", c=16)
    c0r_b = c0r.unsqueeze(2).to_broadcast([128, 32, 16])
    c0i_b = c0i.unsqueeze(2).to_broadcast([128, 32, 16])
    vop(lambda h: h.tensor_tensor(out=BBR, in0=br, in1=c0r_b, op=ALU.mult))
    vop(lambda h: h.tensor_tensor(out=TA, in0=bi, in1=c0i_b, op=ALU.mult))
    vop(lambda h: h.tensor_tensor(out=BBR, in0=BBR, in1=TA, op=ALU.subtract))
    vop(lambda h: h.tensor_tensor(out=BBI, in0=bi, in1=c0r_b, op=ALU.mult))
    vop(lambda h: h.tensor_tensor(out=TA, in0=br, in1=c0i_b, op=ALU.mult))
    vop(lambda h: h.tensor_tensor(out=BBI, in0=BBI, in1=TA, op=ALU.add))
    ASd = ar.alloc([16, 2, 8, 16], F32)
    CS2d = ar.alloc([16, 2, 8, 16], F32)
    T1 = ar.alloc([8, 16, 16], F32)
    T2 = ar.alloc([8, 16, 16], F32)
    cr = cc[:, 0, :].rearrange("p (d a c) -> p d a c", d=2, c=16)
    ci = cc[:, 1, :].rearrange("p (d a c) -> p d a c", d=2, c=16)
    BBR4 = BBR.rearrange("p (d a) c -> p d a c", d=2)
    BBI4 = BBI.rearrange("p (d a) c -> p d a c", d=2)

    def pw(arr, d, k0, kstep):
        return pap(arr, 0, 128, k0 * 32 + d * 16, [[kstep * 32, 8], [1, 16], [0, 16]])

    def dst(arr, dofs, ri):
        return pap(arr, 0, 128, dofs * 4096 + ri * 128, [[16, 8], [256, 16], [1, 16]])

    def vec(v4, d):
        a_ = v4[:, d]
        return bass.AP(a_.tensor, a_.offset, [list(a_.ap[0]), [0, 8], list(a_.ap[1]), list(a_.ap[2])])

    T1f = T1.rearrange("p a b c -> p (a b c)")

    def cmul(out_arr, dofs, d, k0, kstep, vr, vi, neg_im):
        pr, pi_ = pw(PWR, d, k0, kstep), pw(PWI, d, k0, kstep)
        vop(lambda h: h.tensor_tensor(out=T1, in0=pr, in1=vec(vr, d), op=ALU.mult))
        vop(lambda h: h.tensor_tensor(out=T2, in0=pi_, in1=vec(vi, d), op=ALU.mult))
        vop(lambda h: h.tensor_tensor(out=dst(out_arr, dofs, 0), in0=T1, in1=T2, op=ALU.subtract))
        vop(lambda h: h.tensor_tensor(out=T1, in0=pr, in1=vec(vi, d), op=ALU.mult))
        vop(lambda h: h.tensor_tensor(out=T2, in0=pi_, in1=vec(vr, d), op=ALU.mult))
        if neg_im:
            vop(lambda h: h.tensor_scalar(T1f, T1f, -1.0, None, op0=ALU.mult))
            vop(lambda h: h.tensor_tensor(out=dst(out_arr, dofs, 1), in0=T1, in1=T2, op=ALU.subtract))
        else:
            vop(lambda h: h.tensor_tensor(out=dst(out_arr, dofs, 1), in0=T1, in1=T2, op=ALU.add))
    vop(lambda h: h.tensor_copy(k.s5A1[:, 0:32], PWR[:, 15, :]))
    vop(lambda h: h.tensor_copy(k.s5A1[:, 32:64], PWR[:, 15, :]))
    vop(lambda h: h.tensor_scalar(k.s5A2[:, 0:32], PWI[:, 15, :], -1.0, None, op0=ALU.mult))
    vop(lambda h: h.tensor_copy(k.s5A2[:, 32:64], PWI[:, 15, :]))
    cmul(k.s5CS, 0, 0, 8, 1, cr, ci, True)
    cmul(k.s5CS, 1, 1, 15, -1, cr, ci, True)
    k.t_s5w = T
    mf = ar.alloc([2, 128], F32)
    dv = ar.alloc([32], F32)
    tm = Trk()
    sl_ = cx.fresh()
    cx.dma("sync", mf[:, 0, :], D["s5mf"], sl_, w=[tm])
    cx.dma("sync", mf[:, 1, :], D["s5mb"], sl_, w=[tm])
    cx.dma("sync", dv, D["s5_dvec"], sl_, w=[tm])
    tt1 = [ar.alloc([128], F32) for _ in range(2)]
    ttt = [Trk(), Trk()]
    ASb = ASd.rearrange("p a r s c -> p (a r) (s c)")
    ASm = ASd.rearrange("p a r s c -> p a r (s c)")
    CSm = CS2d.rearrange("p a r s c -> p a r (s c)")
    for d in range(2):
        if d == 0:
            cmul(ASd, 0, 0, 14, -1, BBR4, BBI4, False)
            cmul(CS2d, 0, 0, 0, 1, cr, ci, True)
        else:
            cmul(ASd, 0, 1, 7, 1, BBR4, BBI4, False)
            cmul(CS2d, 0, 1, 7, -1, cr, ci, True)
        for grp in range(8):
            pb = k.psum[grp % 4]
            tp = k.tpsum[grp % 4]
            for j in range(4):
                blk = grp * 4 + j
                cx.op("tensor", lambda h, pb=pb, j=j, blk=blk: h.transpose(pb[:, j * 128:(j + 1) * 128], ASb[:, blk, :], k.ident),
                      r=[T], w=[tp], inc=(j == 3))
            dstv = k.s5AT[:, d * 32 + grp * 4:d * 32 + (grp + 1) * 4, :]
            if grp % 2 == 0:
                cx.op("scalar", lambda h, dstv=dstv, pb=pb: h.copy(dstv, pb.rearrange("p (j x) -> p j x", j=4)), r=[tp], w=[k.t_s5at])
            else:
                cx.op("vector", lambda h, dstv=dstv, pb=pb: h.tensor_copy(dstv, pb.rearrange("p (j x) -> p j x", j=4)), r=[tp], w=[k.t_s5at])
        for g in range(32):
            gh, gl = g // 16, g % 16
            pb = k.psum[4 + g % 4]
            tp = k.tpsum[4 + g % 4]
            for ri in range(2):
                cx.op("tensor", lambda h, pb=pb, ri=ri, gh=gh, gl=gl: h.matmul(
                    pb[:, 0:128], ASm[gh * 64:(gh + 1) * 64, gl, ri, :], CSm[gh * 64:(gh + 1) * 64, gl, ri, :],
                    start=(ri == 0), stop=(ri == 1)), r=[T], w=[tp], inc=(ri == 1))
            b_ = g % 2
            cx.op(V, lambda h, pb=pb, b_=b_, d=d: h.tensor_tensor(out=tt1[b_], in0=pb[:, 0:128], in1=mf[:, d, :], op=ALU.mult), r=[tp, tm], w=[ttt[b_]])
            if d == 0:
                cx.op(V, lambda h, b_=b_, g=g: h.scalar_tensor_tensor(out=k.s5TT[:, g, :], in0=k.ident, scalar=dv[:, g:g + 1], in1=tt1[b_], op0=ALU.mult, op1=ALU.add),
                      r=[ttt[b_], tm], w=[k.t_s5tt])
            else:
                cx.op(V, lambda h, b_=b_, g=g: h.tensor_tensor(out=k.s5TT[:, g, :], in0=k.s5TT[:, g, :], in1=tt1[b_], op=ALU.add),
                      r=[ttt[b_]], w=[k.t_s5tt])
    cx.barrier()
    ar.release(m0)


def load_w_cols(k, wdram, col0, ncols, dst, trk, slot, eng="gpsimd"):
    src = bass.AP(wdram.tensor, wdram.offset + col0, [[wdram.ap[0][0] * 1, 128], [wdram.ap[0][0] * 128, 8], [1, ncols]])
    return k.cx.dma(eng, dst, src, slot, w=[trk])


def proj_fm(k, wt, wtrk, consume):
    cx = k.cx
    for n in range(4):
        pb = k.psum[n % 2 + 2]
        tp = k.tpsum[n % 2 + 2]
        for c in range(8):
            cx.op("tensor", lambda h, pb=pb, c=c, n=n: h.matmul(pb[:, :], wt[:, c, :], k.hT[:, c, n * 512:(n + 1) * 512], start=(c == 0), stop=(c == 7)),
                  r=[wtrk, k.t_hT], w=[tp], inc=(c == 7))
        consume(n, pb, tp)


def s5_build_U(k):
    cx, ar = k.cx, k.ar
    m0 = ar.mark()
    uT = [ar.alloc([2048], BF16) for _ in range(2)]
    tuT = [Trk(), Trk()]
    for ct in range(4):
        b = ct % 2

        def consume(n, pb, tp, b=b):
            dstv = pap(uT[b], 0, 128, n * 64, [[1, 64], [256, 8]])
            srcv = pb[:, :].rearrange("p (j s) -> p j s", s=8)
            if n % 2 == 0:
                cx.op("scalar", lambda h: h.copy(dstv, srcv), r=[tp], w=[tuT[b]])
            else:
                cx.op("vector", lambda h: h.tensor_copy(dstv, srcv), r=[tp], w=[tuT[b]])
        proj_fm(k, k.u_wt[ct], k.t_uwt[ct], consume)
        for gi in range(8):
            g = ct * 8 + gi
            q0 = 32 * (gi // 2)
            pb = k.psum[4 + gi % 4]
            tp = k.tpsum[4 + gi % 4]
            for s in range(8):
                rhs = pap(uT[b], q0, 32, s * 256, [[1, 256]])
                cx.op("tensor", lambda h, pb=pb, s=s, rhs=rhs, q0=q0, gi=gi: h.matmul(pb[:, 0:256], k.selT[q0:q0 + 32, gi % 2, s, :], rhs, start=(s == 0), stop=(s == 7), tile_position=(q0, 0)),
                      r=[tuT[b]], w=[tp], inc=(s == 7))
            if gi % 2 == 0:
                cx.op("scalar", lambda h, pb=pb, g=g: h.copy(k.s5U[:, g, :], pb[:, 0:256]), r=[tp], w=[k.t_s5U])
            else:
                cx.op("vector", lambda h, pb=pb, g=g: h.tensor_copy(k.s5U[:, g, :], pb[:, 0:256]), r=[tp], w=[k.t_s5U])
    cx.barrier()
    ar.release(m0)


def s5_main(k, yT, t_yT):
    cx, ar, D = k.cx, k.ar, k.D
    V = "vector"
    m0 = ar.mark()
    wg = ar.alloc([4, 512], BF16)
    twg = Trk()
    wsrc = D["s5_w_glu"]
    cx.dma("gpsimd", wg, bass.AP(wsrc.tensor, wsrc.offset, [[512, 128], [512 * 128, 4], [1, 512]]), cx.fresh('sw'), w=[twg])
    bgl = ar.alloc([4], F32)
    nw = ar.alloc([4], F32)
    tb = Trk()
    sl_ = cx.fresh()
    cx.dma("sync", bgl, D["s5_bglu"], sl_, w=[tb])
    cx.dma("sync", nw, D["s5_normw"], sl_, w=[tb])
    SH = ar.alloc([2, 2, 16, 257], BF16)
    tSH = Trk()
    tSHh = Trk()
    X = [ar.alloc([64], F32) for _ in range(3)]
    tX = [Trk() for _ in range(3)]
    t1 = ar.alloc([64], F32)
    t2 = ar.alloc([64], F32)
    tt = Trk()
    tt2 = Trk()
    cx.op("gpsimd", lambda h: h.memset(SH[:, 0, :, :, 0:1], 0.0), w=[tSH])
    cx.op("gpsimd", lambda h: h.memset(SH[:, 1, :, :, 256:257], 0.0), w=[tSH])
    cx.op("gpsimd", lambda h: h.memset(X[0], 0.0), w=[tX[0]])
    n = 0
    for gl in range(16):
        for d in range(2):
            for ri in range(2):
                blk = d * 32 + gl * 2 + ri
                pb = k.psum[n % 4]
                tp = k.tpsum[n % 4]
                cx.op("tensor", lambda h, pb=pb, blk=blk, gl=gl: h.matmul(pb[0:64, 0:256], k.s5AT[:, blk, 0:64], k.s5U[:, gl, :], start=True, stop=True),
                      r=[k.t_s5at, k.t_s5U], w=[tp], inc=False)
                cx.op("tensor", lambda h, pb=pb, blk=blk, gl=gl: h.matmul(pb[64:128, 0:256], k.s5AT[:, blk, 64:128], k.s5U[:, 16 + gl, :], start=True, stop=True),
                      r=[k.t_s5at, k.t_s5U], w=[tp])
                slot0 = 1 if d == 0 else 0
                dstv = pap(SH, 0, 128, ((d * 2 + ri) * 16 + gl) * 257 + slot0, [[1, 256]])
                if n % 2 == 0:
                    cx.op("scalar", lambda h, dstv=dstv, pb=pb: h.copy(dstv, pb[:, 0:256]), r=[tp], w=[tSH])
                else:
                    cx.op(V, lambda h, dstv=dstv, pb=pb: h.tensor_copy(dstv, pb[:, 0:256]), r=[tp], w=[tSH])
                n += 1
    import os
    S5STOP = os.environ.get('S5_STOP', '')
    if S5STOP == 'a':
        cx.barrier(); ar.release(m0); return
    for i in range(256):
        xp, xn = X[i % 3], X[(i + 1) % 3]
        txp, txn = tX[i % 3], tX[(i + 1) % 3]
        xsw = pap(xp, 0, 128, 32, [[-32, 2], [1, 32]])
        bf = i + 1
        bb_ = 32 * 257 + (255 - i)
        sview = pap(SH, 0, 128, bf, [[16 * 257, 2], [bb_ - bf, 2], [257, 16]])
        xp3 = xp.rearrange("p (r x) -> p r x", r=2)
        cx.op("gpsimd", lambda h, xsw=xsw: h.tensor_tensor(out=t2.rearrange("p (r x) -> p r x", r=2), in0=k.s5A2.rearrange("p (r x) -> p r x", r=2), in1=xsw, op=ALU.mult), r=[txp, k.t_s5w], w=[tt2])
        cx.op(V, lambda h, xp=xp: h.tensor_tensor(out=t1, in0=k.s5A1, in1=xp, op=ALU.mult), r=[txp, k.t_s5w], w=[tt])
        cx.op(V, lambda h, sview=sview: h.tensor_tensor(out=t1.rearrange("p (r d x) -> p r d x", r=2, d=2), in0=t1.rearrange("p (r d x) -> p r d x", r=2, d=2), in1=sview, op=ALU.add), r=[tt, tSH], w=[tt])
        cx.op(V, lambda h, xn=xn: h.tensor_tensor(out=xn, in0=t1, in1=t2, op=ALU.add), r=[tt, tt2], w=[txn])
        cx.op("scalar", lambda h, xn=xn, sview=sview: h.copy(sview, xn.rearrange("p (r d x) -> p r d x", r=2, d=2)), r=[txn], w=[tSHh])
    if S5STOP == 'rec':
        cx.barrier(); ar.release(m0); return
    gT = ar.alloc([4, 2048], F32)
    gTb = ar.alloc([4, 2048], BF16)
    tgT = [Trk() for _ in range(4)]
    tgTb = [Trk() for _ in range(4)]
    ybuf = [k.arA.alloc([8, 256], BF16) for _ in range(2)]
    tyb = [Trk(), Trk()]
    for ct in range(4):
        b = ct % 2
        for gi in range(8):
            g = ct * 8 + gi
            gh, gl = g // 16, g % 16
            pb = k.psum[gi % 2]
            tp = k.tpsum[gi % 2]
            cx.op("tensor", lambda h, pb=pb, g=g: h.matmul(pb[:, 0:256], k.s5TT[:, g, :], k.s5U[:, g, :], start=True, stop=False),
                  r=[k.t_s5tt, k.t_s5U], w=[tp], inc=False)
            for d in range(2):
                for ri in range(2):
                    slot0 = 0 if d == 0 else 1
                    rhs = pap(SH, gh * 64, 64, ((d * 2 + ri) * 16 + gl) * 257 + slot0, [[1, 256]])
                    last = (d == 1 and ri == 1)
                    cx.op("tensor", lambda h, pb=pb, rhs=rhs, d=d, ri=ri, gh=gh, gl=gl, last=last: h.matmul(
                        pb[:, 0:256], k.s5CS[gh * 64:(gh + 1) * 64, d, gl, ri, :], rhs, start=False, stop=last),
                        r=[tSH, tSHh, k.t_s5w], w=[tp], inc=last)
            if gi % 2 == 0:
                cx.op("scalar", lambda h, pb=pb, b=b, gi=gi: h.copy(ybuf[b][:, gi, :], pb[:, 0:256]), r=[tp], w=[tyb[b]])
            else:
                cx.op(V, lambda h, pb=pb, b=b, gi=gi: h.tensor_copy(ybuf[b][:, gi, :], pb[:, 0:256]), r=[tp], w=[tyb[b]])
        for t in range(8):
            q0 = 32 * (t // 2)
            pb = k.psum[2 + t % 4]
            tp = k.tpsum[2 + t % 4]
            for gi in range(8):
                cx.op("tensor", lambda h, pb=pb, t=t, gi=gi, q0=q0, b=b: h.matmul(pb[:, 0:256], k.selT[q0:q0 + 32, t % 2, gi, :], ybuf[b][q0:q0 + 32, gi, :], start=(gi == 0), stop=(gi == 7), tile_position=(q0, 0)),
                      r=[tyb[b]], w=[tp], inc=(gi == 7))
            dstv = pap(gT, 0, 128, ct * 2048 + t, [[8, 256]])
            cx.op("scalar", lambda h, pb=pb, dstv=dstv: h.activation(dstv, pb[:, 0:256], AF.Gelu), r=[tp], w=[tgT[ct]])
        cx.op("vector", lambda h, ct=ct: h.tensor_copy(gTb[:, ct, :], gT[:, ct, :]), r=[tgT[ct]], w=[tgTb[ct]])
    k.dbg_add("s5_g", gT, tgT)
    if S5STOP == 'c':
        cx.barrier(); ar.release(m0); return
    sig = ar.alloc([4, 512], BF16)
    tsig = Trk()
    sq = ar.alloc([4, 512], BF16)
    tsq = Trk()
    rs = ar.alloc([512], F32)
    trs = Trk()
    for nck in range(4):
        ts = slice(nck * 512, (nck + 1) * 512)
        for co in range(4):
            pb = k.psum[co % 2]
            tp = k.tpsum[co % 2]
            for ci in range(4):
                cx.op("tensor", lambda h, pb=pb, co=co, ci=ci, ts=ts: h.matmul(pb[:, :], wg[:, ci, co * 128:(co + 1) * 128], gTb[:, ci, ts], start=(ci == 0), stop=(ci == 3)),
                      r=[twg] + tgTb, w=[tp], inc=(ci == 3))
            cx.op("scalar", lambda h, pb=pb, co=co: h.activation(sig[:, co, :], pb[:, :], AF.Sigmoid, bias=bgl[:, co:co + 1]), r=[tp, tb], w=[tsig])
        for co in range(4):
            cx.op(V, lambda h, co=co, ts=ts: h.tensor_tensor(out=gT[:, co, ts], in0=gT[:, co, ts], in1=sig[:, co, :], op=ALU.mult), r=[tsig, tgT[co]], w=[tgT[co]])
            cx.op("scalar", lambda h, co=co, ts=ts: h.activation(sq[:, co, :], gT[:, co, ts], AF.Square), r=[tgT[co]], w=[tsq])
        pb = k.psum[2 + nck % 2]
        tp = k.tpsum[2 + nck % 2]
        for co in range(4):
            cx.op("tensor", lambda h, pb=pb, co=co: h.matmul(pb[:, :], k.onesb, sq[:, co, :], start=(co == 0), stop=(co == 3)), r=[tsq], w=[tp], inc=(co == 3))
        cx.op("scalar", lambda h, pb=pb: h.activation(rs, pb[:, :], AF.Sqrt, scale=1.0 / 512, bias=k.epsc), r=[tp], w=[trs])
        cx.op(V, lambda h: h.reciprocal(rs, rs), r=[trs], w=[trs])
        for co in range(4):
            cx.op(V, lambda h, co=co, ts=ts: h.scalar_tensor_tensor(out=yT[:, co, ts], in0=gT[:, co, ts], scalar=nw[:, co:co + 1], in1=rs, op0=ALU.mult, op1=ALU.mult),
                  r=[tgT[co], trs, tb, tsq], w=[t_yT])
    k.dbg_add("s5_gl", gT, tgT)
    cx.barrier()
    ar.release(m0)


def build(in_shapes, stage="full", dbg_names=(), n_heads=4, n_experts=32):
    nc = bass.Bass("TRN2", target_bir_lowering=False)
    k = K()
    k.n_heads = n_heads
    k.n_experts = n_experts
    k.nc = nc
    D = {}
    for nm, (shape, dt) in in_shapes.items():
        D[nm] = nc.dram_tensor(nm, list(shape), dt, kind="ExternalInput").ap()
    k.D = D
    out = nc.dram_tensor("out", [S, DM], F32, kind="ExternalOutput").ap()
    k.dbg = {}
    k.dbg_req = set(dbg_names)

    with contextlib.ExitStack() as st:
        cx = Ctx(nc, st)
        k.cx = cx

        def finish():
            deps = [(s_.key, s_.total) for s_ in cx.slots if s_.total > 0]
            cx.wait_deps("sync", deps + [(e, cx.cnt[e]) for e in ENGS if e != "sync" and cx.cnt[e] > 0])
            with nc.Block() as block:
                cx.emit_all(block)
            k.n_ops = cx.n_ops
            return nc, k
        k.slot_c = cx.slot("c")
        k.slot_w = cx.slot("w")
        k.slot_x = [cx.slot("x0"), cx.slot("x1")]
        k.slot_o = cx.slot("o")
        k.psum = [cx.ps("ps%d" % i, [128, 512], F32) for i in range(8)]
        k.psum = [p[:, :] for p in k.psum]
        k.tpsum = [Trk("ps%d" % i, excl=True) for i in range(8)]
        k.ident = cx.sb("ident", [128, 128], F32)[:, :]
        k.identb = cx.sb("identb", [128, 128], BF16)[:, :]
        k.ones = cx.sb("ones", [128, 128], F32)[:, :]
        k.onesb = cx.sb("onesb", [128, 128], BF16)[:, :]
        k.epsc = cx.sb("epsc", [128, 1], F32)[:, :]
        k.selT = cx.sb("selT", [128, 2, 8, 128], BF16)[:, :, :, :]
        tc = Trk()
        cx.dma("sync", k.ident, D["ident"], k.slot_c, w=[tc])
        cx.dma("sync", k.identb, D["identb"], k.slot_c, w=[tc])
        cx.dma("sync", k.ones, D["ones"], k.slot_c, w=[tc])
        cx.dma("gpsimd", k.onesb, D["ones"], cx.fresh("sw"), w=[tc])
        cx.op("vector", lambda h: h.memset(k.epsc, EPS), w=[tc])
        cx.dma("sync", k.selT, D["selT"], k.slot_c, w=[tc])
        k.s5A1 = cx.sb("s5A1", [128, 64], F32)[:, :]
        k.s5A2 = cx.sb("s5A2", [128, 64], F32)[:, :]
        ar = Arena(cx, 51456)
        k.ar = ar
        cx.barrier()

        def dbg_add(name, ap, trks):
            if name in k.dbg_req:
                shape = list(ap.shape)
                dt_ = F32
                o = nc.dram_tensor("dbg_" + name, shape, dt_, kind="ExternalOutput").ap()
                cx.dma("gpsimd" if ap.dtype != F32 else "sync", o, ap, cx.fresh("sw" if ap.dtype != F32 else "hw"), r=list(trks))
        k.dbg_add = dbg_add

        regA = ar.alloc([NT * 1024], F32)
        arA = Arena(cx, NT * 1024, base=regA)
        k.arA = arA
        yT = ar.alloc([8, 2048], BF16)
        t_yT = Trk()
        k.s5U = arA.alloc([32, 256], BF16)
        k.t_s5U = Trk()
        m_h = arA.mark()
        k.hT = arA.alloc([8, 2048], BF16)
        k.t_hT = Trk()
        mU = arA.mark()
        k.u_wt = [arA.alloc([8, 128], BF16) for _ in range(4)]
        k.t_uwt = [Trk() for _ in range(4)]
        sl_u = cx.slot("uw")
        for ct_ in range(4):
            load_w_cols(k, D["w_in"], ct_ * 128, 128, k.u_wt[ct_], k.t_uwt[ct_], sl_u)

        m1 = ar.mark()
        xt = [ar.alloc([1024], F32) for _ in range(2)]
        txt = [Trk(), Trk()]

        def src_x(i):
            b = i % 2
            cx.dma("sync", xt[b], D["x"][i * 128:(i + 1) * 128, :], k.slot_x[b], w=[txt[b]])
            return xt[b], txt[b]
        norm_transpose(k, "mix", src_x, NT, D["norm_mix"], k.hT, BF16, k.t_hT)
        cx.barrier()
        ar.release(m1)

        if stage == 'p1':
            return finish()
        s5_build_U(k)
        arA.release(mU)
        if stage == 'U':
            return finish()
        mg = ar.mark()
        gdn_setup(k)
        if k.n_heads > 0:
            gdn_load_weights(k, 0)
        for hd in range(k.n_heads):
            gdn_head(k, hd, yT, t_yT)
        ar.release(mg)
        k.dbg_add("ygdnT", yT[:, 4:8, :], [t_yT])
        if stage == 'gdn':
            return finish()
        cx.barrier()
        arA.release(m_h)
        k.s5AT = arA.alloc([64, 128], BF16)
        k.t_s5at = Trk()
        k.s5CS = arA.alloc([2, 16, 2, 128], BF16)
        k.s5TT = arA.alloc([32, 128], BF16)
        k.t_s5tt = Trk()
        s5_prep(k)
        if stage == 's5prep':
            return finish()
        s5_main(k, yT[:, 0:4, :], t_yT)
        k.dbg_add("ys5T", yT[:, 0:4, :], [t_yT])
        if stage == "s5":
            return finish()
        if True:
            cx.barrier()
            k.xacc = regA.rearrange('p (a b) -> p a b', a=NT)
            k.txacc = [Trk() for _ in range(NT)]
            out_proj(k, yT, t_yT)
            k.dbg_add("x1", k.xacc, k.txacc)
            if stage == 'oproj':
                return finish()
            xattn(k)
            if stage == 'xattn':
                return finish()
            k.dbg_add("x2", k.xacc, k.txacc)
            moe(k)
            k.dbg_add("x3", k.xacc, k.txacc + [t_ for p_ in k.txh for t_ in p_])
            final_norm(k, out)

        return finish()


def host_inputs(inp, b):
    m = {}
    m["x"] = np.ascontiguousarray(inp["x"][b])
    m["mem"] = np.ascontiguousarray(inp["mem"][b])
    m["norm_mix"] = inp["norm_mix"][0]
    m["w_in"] = inp["w_in"][0]
    m["w_out"] = inp["w_out"][0]
    m["s5_w_glu"] = inp["s5_w_glu"][0]
    m.update(host_s5(inp))
    cv = inp["gdn_conv"][0]
    m["gdn_convw"] = np.ascontiguousarray(cv.reshape(5, 3, 4, 128).transpose(3, 2, 1, 0))
    for nm in ("gdn_a_log_f", "gdn_dt_bias_f", "gdn_a_log_b", "gdn_dt_bias_b"):
        m[nm] = inp[nm][0]
    m["gdn_norm"] = inp["gdn_norm"][0]
    for nm in ("norm_xattn", "norm_mem", "xa_wq", "xa_wk", "xa_wv", "xa_wo", "norm_moe", "router_group_w", "router_group_b",
               "router_expert_w", "router_expert_b", "moe_w_gate", "moe_w_up", "moe_w_down"):
        m[nm] = inp[nm][0]
    m["norm_final"] = inp["norm_final"]
    m.update(host_consts())
    return m


def gdn_setup(k):
    cx, ar, D = k.cx, k.ar, k.D
    V = "vector"
    G = K()
    k.G = G
    G.mask = ar.alloc([7, 128], F32)
    G.tmask = Trk()
    cx.dma("sync", G.mask, D["gmask"][:, 0:7, :], cx.fresh(), w=[G.tmask])
    wsm = ar.alloc([8, 16], BF16)
    tw = Trk()
    load_w_cols(k, D["w_in"], 2560, 16, wsm, tw, cx.fresh('sw'))
    BA = ar.alloc([16, 16], F32)
    tBA = Trk()
    for i in range(NT):
        pb = k.psum[i % 4]
        tp = k.tpsum[i % 4]
        for c in range(8):
            cx.op("tensor", lambda h, pb=pb, c=c, i=i: h.matmul(pb[:, 0:16], k.hT[:, c, i * 128:(i + 1) * 128], wsm[:, c, :], start=(c == 0), stop=(c == 7)),
                  r=[tw, k.t_hT], w=[tp], inc=(c == 7))
        cx.op("scalar", lambda h, pb=pb, i=i: h.copy(BA[:, i, :], pb[:, 0:16]), r=[tp], w=[tBA])
    pr = ar.alloc([4, 4], F32)
    tpr = Trk()
    sl_ = cx.fresh()
    for j, nm in enumerate(("gdn_a_log_f", "gdn_dt_bias_f", "gdn_a_log_b", "gdn_dt_bias_b")):
        cx.dma("sync", pr[:, j, :], dram_bcast(D[nm], 128, 4), sl_, w=[tpr])
    G.nw = ar.alloc([128], F32)
    cx.dma("sync", G.nw, dram_bcast(D["gdn_norm"], 128, 128), sl_, w=[tpr])
    G.tpr = tpr
    T = Trk()
    G.T = T
    G.beta, G.nb, G.gc, G.eg, G.neg, G.ed = [], [], [], [], [], []
    def per_dir(d):
        beta = ar.alloc([16, 4], F32)
        nb = ar.alloc([16, 4], F32)
        g = ar.alloc([16, 4], F32)
        gc = ar.alloc([16, 4], F32)
        gt = ar.alloc([16, 4], F32)
        eg = ar.alloc([16, 4], F32)
        neg = ar.alloc([16, 4], F32)
        ed = ar.alloc([16, 4], F32)
        ea = ar.alloc([4], F32)
        braw = BA[:, :, d * 4:(d + 1) * 4]
        araw = BA[:, :, 8 + d * 4:8 + (d + 1) * 4]
        cx.op("scalar", lambda h: h.activation(beta, braw, AF.Sigmoid), r=[tBA, T], w=[T])
        cx.op(V, lambda h: h.tensor_scalar(nb, beta, -1.0, None, op0=ALU.mult), r=[T], w=[T])
        cx.op("scalar", lambda h: h.activation(ea, pr[:, 2 * d, :], AF.Exp), r=[tpr, T], w=[T])
        cx.op(V, lambda h: h.tensor_tensor(out=g, in0=araw, in1=pr[:, 2 * d + 1, :].unsqueeze(1).to_broadcast([128, 16, 4]), op=ALU.add), r=[tBA, tpr, T], w=[T])
        cx.op("scalar", lambda h: h.activation(g, g, AF.Exp), r=[T], w=[T])
        cx.op("scalar", lambda h: h.activation(g, g, AF.Ln, bias=1.0), r=[T], w=[T])
        cx.op(V, lambda h: h.scalar_tensor_tensor(out=g, in0=g, scalar=-1.0, in1=ea.unsqueeze(1).to_broadcast([128, 16, 4]), op0=ALU.mult, op1=ALU.mult), r=[T], w=[T])
        g2 = g.rearrange("p a b -> p (a b)")
        pb = k.psum[4 + d]
        tp = k.tpsum[4 + d]
        cx.op("tensor", lambda h, pb=pb, d=d: h.matmul(pb[:, 0:64], G.mask[:, d, :], g2, start=True, stop=True), r=[T, G.tmask], w=[tp])
        cx.op("tensor", lambda h, pb=pb: h.matmul(pb[:, 64:128], G.mask[:, 6, :], g2, start=True, stop=True), r=[T, G.tmask], w=[tp])
        cx.op(V, lambda h, pb=pb: h.tensor_copy(gc.rearrange("p a b -> p (a b)"), pb[:, 0:64]), r=[tp], w=[T])
        cx.op(V, lambda h, pb=pb: h.tensor_tensor(out=gt.rearrange("p a b -> p (a b)"), in0=pb[:, 64:128], in1=gc.rearrange("p a b -> p (a b)"), op=ALU.subtract), r=[tp, T], w=[T])
        cx.op("scalar", lambda h: h.activation(eg, gc, AF.Exp), r=[T], w=[T])
        cx.op("scalar", lambda h: h.activation(ed, gt, AF.Exp), r=[T], w=[T])
        cx.op(V, lambda h: h.tensor_scalar(neg, eg, -1.0, None, op0=ALU.mult), r=[T], w=[T])
        G.g = getattr(G, "g", []) + [g]
        G.beta.append(beta); G.nb.append(nb); G.gc.append(gc); G.eg.append(eg); G.neg.append(neg); G.ed.append(ed)
    per_dir(0)
    per_dir(1)
    G.osum = ar.alloc([16, 128], F32)
    G.tosum = [Trk() for _ in range(NT)]
    G.wset = [[k.arA.alloc([8, 128], BF16) for _ in range(4)] for _ in range(2)]
    G.tw = [[Trk() for _ in range(4)] for _ in range(2)]
    G.sw = [cx.slot("gw0"), cx.slot("gw1")]


def gdn_load_weights(k, hd):
    G, D = k.G, k.D
    st = hd % 2
    for j in range(3):
        load_w_cols(k, D["w_in"], 512 + j * 512 + hd * 128, 128, G.wset[st][j], G.tw[st][j], G.sw[st])
    load_w_cols(k, D["w_in"], 512 + 1536 + hd * 128, 128, G.wset[st][3], G.tw[st][3], G.sw[st])


def gdn_head(k, hd, yT, t_yT):
    cx, ar, D, G = k.cx, k.ar, k.D, k.G
    V = "vector"
    m0 = ar.mark()
    qnT = ar.alloc([2048], BF16)
    knT = ar.alloc([2048], BF16)
    Ktok = ar.alloc([16, 128], BF16)
    Vtok = ar.alloc([16, 128], BF16)
    tq, tk_, tKt, tVt = Trk(), Trk(), Trk(), Trk()
    st_ = hd % 2
    wz, twz = G.wset[st_][3], G.tw[st_][3]
    w3, tw3 = G.wset[st_][0:3], G.tw[st_][0:3]
    if hd + 1 < k.n_heads:
        gdn_load_weights(k, hd + 1)
    mA = ar.mark()
    cw = ar.alloc([3, 5], F32)
    tcw = Trk()
    cx.dma("sync", cw, D["gdn_convw"][:, hd, :, :], cx.fresh(), w=[tcw])
    diag = ar.alloc([15, 128], BF16)
    tdg = Trk()
    for j in range(3):
        for t in range(5):
            cx.op("vector", lambda h, j=j, t=t: h.tensor_scalar(diag[:, j * 5 + t, :], k.identb, cw[:, j, t:t + 1], None, op0=ALU.mult), r=[tcw], w=[tdg])
    import os
    ALV = int(os.environ.get("GDN_ALV", "9"))
    if ALV == 0:
        cx.barrier(); ar.release(m0); return
    raw = [ar.alloc([2052], BF16) for _ in range(2)]
    traw = [Trk(), Trk()]
    for b in range(2):
        cx.op("gpsimd", lambda h, b=b: h.memset(raw[b][:, 0:2], 0.0), w=[traw[b]])
        cx.op("gpsimd", lambda h, b=b: h.memset(raw[b][:, 2050:2052], 0.0), w=[traw[b]])
    act2 = [ar.alloc([2048], F32) for _ in range(2)]
    tact = Trk()
    vT = ar.alloc([2048], BF16)
    tvT = Trk()
    sqb = ar.alloc([2048], BF16)
    tsqb = Trk()
    rn = [ar.alloc([512], F32) for _ in range(4)]
    trn = [Trk() for _ in range(4)]
    tactn2 = [[Trk() for _ in range(4)] for _ in range(2)]
    tsqn = [Trk() for _ in range(4)]
    if ALV == 1:
        cx.barrier(); ar.release(m0); return
    for j in range(3):
        b = j % 2
        act = act2[j % 2]
        tactn = tactn2[j % 2]

        def consume(n, pb, tp, b=b):
            cx.op(V if n % 2 else "scalar", (lambda h: h.tensor_copy(raw[b][:, 2 + n * 512:2 + (n + 1) * 512], pb[:, :])) if n % 2 else
                  (lambda h: h.copy(raw[b][:, 2 + n * 512:2 + (n + 1) * 512], pb[:, :])), r=[tp], w=[traw[b]])
        proj_fm(k, w3[j], tw3[j], consume)
        for n in range(4):
            pb = k.psum[4 + n]
            tp = k.tpsum[4 + n]
            for t in range(5):
                cx.op("tensor", lambda h, pb=pb, t=t, n=n, j=j, b=b: h.matmul(pb[:, :], diag[:, j * 5 + t, :], raw[b][:, n * 512 + t:n * 512 + t + 512], start=(t == 0), stop=(t == 4)),
                      r=[tdg, traw[b]], w=[tp], inc=(t == 4))
        for n in range(4):
            pb = k.psum[4 + n]
            tp = k.tpsum[4 + n]
            ts = slice(n * 512, (n + 1) * 512)
            if j == 2:
                cx.op("scalar", lambda h, pb=pb, ts=ts: h.activation(vT[:, ts], pb[:, :], AF.Silu), r=[tp], w=[tvT])
            else:
                cx.op("scalar", lambda h, pb=pb, ts=ts, act=act: h.activation(act[:, ts], pb[:, :], AF.Silu), r=[tp], w=[tactn[n]])
        if j < 2 and ALV > 2:
            for n in range(4):
                ts = slice(n * 512, (n + 1) * 512)
                cx.op("scalar", lambda h, ts=ts, act=act: h.activation(sqb[:, ts], act[:, ts], AF.Square), r=[tactn[n]], w=[tsqn[n]])
            for n in range(4):
                ts = slice(n * 512, (n + 1) * 512)
                pb2 = k.psum[n]
                tp2 = k.tpsum[n]
                cx.op("tensor", lambda h, pb2=pb2, ts=ts: h.matmul(pb2[:, :], k.onesb, sqb[:, ts], start=True, stop=True), r=[tsqn[n]], w=[tp2])
            for n in range(4):
                pb2 = k.psum[n]
                tp2 = k.tpsum[n]
                cx.op("scalar", lambda h, pb2=pb2, n=n: h.activation(rn[n], pb2[:, :], AF.Sqrt, bias=k.epsc), r=[tp2], w=[trn[n]])
            for n in range(4):
                cx.op(V, lambda h, n=n: h.reciprocal(rn[n], rn[n]), r=[trn[n]], w=[trn[n]])
            dstT, tdst, scl = (qnT, tq, 128.0 ** -0.5) if j == 0 else (knT, tk_, 1.0)
            for n in range(4):
                ts = slice(n * 512, (n + 1) * 512)
                cx.op(V, lambda h, ts=ts, n=n, dstT=dstT, scl=scl, act=act: h.scalar_tensor_tensor(out=dstT[:, ts], in0=act[:, ts], scalar=scl, in1=rn[n], op0=ALU.mult, op1=ALU.mult),
                      r=[tactn[n], trn[n]], w=[tdst])
    if ALV <= 3:
        cx.barrier(); ar.release(m0); return
    TV = int(os.environ.get("GDN_TV", "0"))
    for i in range(NT):
        pb = k.psum[i % 2].bitcast(BF16)
        tp = k.tpsum[i % 2]
        if TV == 0:
            cx.op("tensor", lambda h, pb=pb, i=i: h.transpose(pb[:, 0:128], knT[:, i * 128:(i + 1) * 128], k.identb), r=[tk_], w=[tp])
            cx.op("tensor", lambda h, pb=pb, i=i: h.transpose(pb[:, 128:256], vT[:, i * 128:(i + 1) * 128], k.identb), r=[tvT], w=[tp])
            cx.op("scalar", lambda h, pb=pb, i=i: h.copy(Ktok[:, i, :], pb[:, 0:128]), r=[tp], w=[tKt])
            cx.op(V, lambda h, pb=pb, i=i: h.tensor_copy(Vtok[:, i, :], pb[:, 128:256]), r=[tp], w=[tVt])
        elif TV == 1:
            cx.op("tensor", lambda h, pb=pb, i=i: h.transpose(pb[:, 0:128], knT[:, i * 128:(i + 1) * 128], k.identb), r=[tk_], w=[tp])
            cx.op("scalar", lambda h, pb=pb, i=i: h.copy(Ktok[:, i, :], pb[:, 0:128]), r=[tp], w=[tKt])
        elif TV == 2:
            cx.op("tensor", lambda h, pb=pb, i=i: h.transpose(pb[:, 0:128], vT[:, i * 128:(i + 1) * 128], k.identb), r=[tvT], w=[tp])
            cx.op(V, lambda h, pb=pb, i=i: h.tensor_copy(Vtok[:, i, :], pb[:, 0:128]), r=[tp], w=[tVt])
    if hd == 0:
        k.dbg_add("gdn_qn", qnT, [tq])
        k.dbg_add("gdn_kn", knT, [tk_])
        k.dbg_add("gdn_vtok", Vtok, [tVt])
    cx.barrier()
    ar.release(mA)
    STOP = os.environ.get("GDN_STOP", "")
    if STOP == "A":
        ar.release(m0)
        return
    qgT = [ar.alloc([2048], BF16) for _ in range(2)]
    Kd = [ar.alloc([16, 128], BF16) for _ in range(2)]
    Pm = [ar.alloc([16, 128], BF16) for _ in range(2)]
    QKm = [ar.alloc([16, 128], BF16) for _ in range(2)]
    etot = [ar.alloc([32], F32) for _ in range(2)]
    WnT = [ar.alloc([2048], BF16) for _ in range(2)]
    U0b = [ar.alloc([16, 128], BF16) for _ in range(2)]
    tWn = [[Trk() for _ in range(NT)] for _ in range(2)]
    tU0 = [[Trk() for _ in range(NT)] for _ in range(2)]
    tqg = [[Trk() for _ in range(NT)] for _ in range(2)]
    tKd = [[Trk() for _ in range(NT)] for _ in range(2)]
    tPm = [[Trk() for _ in range(NT)] for _ in range(2)]
    tQK = [[Trk() for _ in range(NT)] for _ in range(2)]
    tet = [[Trk() for _ in range(NT)] for _ in range(2)]
    NI = 8
    mN = ar.mark()
    NDT = BF16 if os.environ.get('GDN_NEU', 'bf16') == 'bf16' else F32
    nid = k.identb if NDT == BF16 else k.ident
    Xb = [[ar.alloc([128], NDT) for _ in range(2)] for _ in range(NI)]
    XTb = [[ar.alloc([128], NDT) for _ in range(2)] for _ in range(NI)]
    Pb = [[ar.alloc([128], NDT) for _ in range(2)] for _ in range(NI)]

    def tview(pn_, c0):
        return pn_[:, c0:c0 + 128] if NDT == F32 else pn_.bitcast(BF16)[:, 2 * c0:2 * c0 + 128]
    tX = [Trk() for _ in range(NI)]
    EGB = [ar.alloc([128], F32) for _ in range(NI)]
    ET = [ar.alloc([128], BF16) for _ in range(NI)]
    ETs = ET
    tE = [Trk() for _ in range(NI)]
    tEG = [Trk() for _ in range(NI)]
    Kg = [ar.alloc([128], BF16) for _ in range(NI)]
    tKg = [Trk() for _ in range(NI)]
    tXP = [Trk() for _ in range(NI)]
    insts = [(i, d) for i in range(NT) for d in range(2)]
    for g0 in range(0, len(insts), NI):
        grp = insts[g0:g0 + NI]
        info = []
        for s_, (i, d) in enumerate(grp):
            info.append(dict(s_=s_, i=i, d=d, tsl=slice(i * 128, (i + 1) * 128),
                             col=pap(G.g[d], 0, 128, i * 4 + hd, [[0, 128]]),
                             gcc=G.gc[d][:, i, hd:hd + 1], nbc=G.nb[d][:, i, hd:hd + 1], edc=G.ed[d][:, i, hd:hd + 1],
                             pb=k.psum[s_], tp=k.tpsum[s_]))
        for q_ in info:
            s_, i, d, tsl, col, pb, tp = q_["s_"], q_["i"], q_["d"], q_["tsl"], q_["col"], q_["pb"], q_["tp"]
            cx.op("tensor", lambda h, pb=pb, col=col, d=d: h.matmul(pb[:, 0:128], col, G.mask[:, d, :], start=True, stop=True), r=[G.T, G.tmask], w=[tp], inc=False)
            cx.op("tensor", lambda h, pb=pb, tsl=tsl: h.matmul(pb[:, 128:256], knT[:, tsl], knT[:, tsl], start=True, stop=True), r=[tk_], w=[tp], inc=False)
            cx.op("tensor", lambda h, pb=pb, tsl=tsl: h.matmul(pb[:, 256:384], knT[:, tsl], qnT[:, tsl], start=True, stop=True), r=[tk_, tq], w=[tp])
        for q_ in info:
            s_, i, d, pb, tp, gcc = q_["s_"], q_["i"], q_["d"], q_["pb"], q_["tp"], q_["gcc"]
            cx.op("scalar", lambda h, pb=pb, s_=s_: h.activation(EGB[s_], pb[:, 0:128], AF.Exp), r=[tp], w=[tEG[s_]])
            cx.op(V, lambda h, pb=pb, s_=s_, gcc=gcc, d=d: h.scalar_tensor_tensor(out=ET[s_], in0=pb[:, 0:128], scalar=gcc, in1=G.mask[:, 2 + d, :], op0=ALU.subtract, op1=ALU.min),
                  r=[tp, G.T, G.tmask], w=[tE[s_]])
        for q_ in info:
            s_, i, d, tsl = q_["s_"], q_["i"], q_["d"], q_["tsl"]
            cx.op("scalar", lambda h, s_=s_: h.activation(ET[s_], ET[s_], AF.Exp), r=[tE[s_]], w=[tE[s_]])
            cx.op(V, lambda h, s_=s_, tsl=tsl, d=d: h.tensor_tensor(out=qgT[d][:, tsl], in0=qnT[:, tsl], in1=EGB[s_], op=ALU.mult), r=[tq, tEG[s_]], w=[tqg[d][i]])
        for q_ in info:
            s_, i, d, pb, tp, edc = q_["s_"], q_["i"], q_["d"], q_["pb"], q_["tp"], q_["edc"]
            c0, c1 = (63, 127) if d == 0 else (0, 64)
            cx.op("scalar", lambda h, s_=s_, d=d, i=i, c0=c0: h.copy(etot[d][:, 2 * i:2 * i + 1], EGB[s_][:, c0:c0 + 1]), r=[tEG[s_]], w=[tet[d][i]])
            cx.op("scalar", lambda h, s_=s_, d=d, i=i, c1=c1: h.copy(etot[d][:, 2 * i + 1:2 * i + 2], EGB[s_][:, c1:c1 + 1]), r=[tEG[s_]], w=[tet[d][i]])
            cx.op("scalar", lambda h, d=d, i=i, edc=edc: h.activation(Kd[d][:, i, :], Ktok[:, i, :], AF.Identity, scale=edc), r=[tKt, G.T], w=[tKd[d][i]])
            egc = G.eg[d][:, i, hd:hd + 1]
            cx.op("scalar", lambda h, s_=s_, i=i, egc=egc: h.activation(Kg[s_], Ktok[:, i, :], AF.Identity, scale=egc), r=[tKt, G.T], w=[tKg[s_]])
            cx.op(V, lambda h, pb=pb, s_=s_, d=d, i=i: h.tensor_tensor(out=QKm[d][:, i, :], in0=pb[:, 256:384], in1=ET[s_], op=ALU.mult), r=[tp, tE[s_]], w=[tQK[d][i]])
        for q_ in info:
            s_, d = q_["s_"], q_["d"]
            cx.op(V, lambda h, s_=s_, d=d: h.tensor_tensor(out=ETs[s_], in0=ET[s_], in1=G.mask[:, 4 + d, :], op=ALU.mult), r=[tE[s_], G.tmask], w=[tE[s_]])
        for q_ in info:
            s_, pb, tp, nbc = q_["s_"], q_["pb"], q_["tp"], q_["nbc"]
            cx.op(V, lambda h, pb=pb, s_=s_, nbc=nbc: h.scalar_tensor_tensor(out=Xb[s_][0], in0=pb[:, 128:256], scalar=nbc, in1=ETs[s_], op0=ALU.mult, op1=ALU.mult),
                  r=[tp, tE[s_], G.T], w=[tX[s_]])
        for q_ in info:
            s_, pn, tn = q_["s_"], q_["pb"], q_["tp"]
            cx.op("tensor", lambda h, pn=pn, s_=s_: h.transpose(tview(pn, 384), Xb[s_][0], nid), r=[tX[s_]], w=[tn])
            cx.op(V, lambda h, s_=s_: h.tensor_tensor(out=Pb[s_][0], in0=Xb[s_][0], in1=nid, op=ALU.add), r=[tX[s_]], w=[tXP[s_]])
            cx.op("scalar", lambda h, pn=pn, s_=s_: h.copy(XTb[s_][0], tview(pn, 384)), r=[tn], w=[tX[s_]])
        for L in range(1, 6):
            a, b_ = (L - 1) % 2, L % 2
            for s_, (i, d) in enumerate(grp):
                pn = k.psum[s_]
                tn = k.tpsum[s_]
                if L < 5:
                    cx.op("tensor", lambda h, pn=pn, s_=s_, a=a: h.matmul(pn[:, 0:128], XTb[s_][a], Xb[s_][a], start=True, stop=True), r=[tX[s_]], w=[tn], inc=False)
                cx.op("tensor", lambda h, pn=pn, s_=s_, a=a: h.matmul(pn[:, 128:256], Xb[s_][a], XTb[s_][a], start=True, stop=True), r=[tX[s_]], w=[tn])
                e1, e2 = ("scalar", V) if s_ % 2 == 0 else (V, "scalar")
                if L < 5:
                    if e1 == "scalar":
                        cx.op("scalar", lambda h, pn=pn, s_=s_, b_=b_: h.copy(Xb[s_][b_], pn[:, 0:128]), r=[tn], w=[tX[s_]])
                    else:
                        cx.op(V, lambda h, pn=pn, s_=s_, b_=b_: h.tensor_copy(Xb[s_][b_], pn[:, 0:128]), r=[tn], w=[tX[s_]])
                if e2 == "scalar":
                    cx.op("scalar", lambda h, pn=pn, s_=s_, b_=b_: h.copy(XTb[s_][b_], pn[:, 128:256]), r=[tn], w=[tX[s_]])
                else:
                    cx.op(V, lambda h, pn=pn, s_=s_, b_=b_: h.tensor_copy(XTb[s_][b_], pn[:, 128:256]), r=[tn], w=[tX[s_]])
            for s_, (i, d) in enumerate(grp):
                pn = k.psum[s_]
                tn = k.tpsum[s_]
                cx.op("tensor", lambda h, pn=pn, s_=s_, a=a: h.matmul(pn[:, 256:384], nid, Pb[s_][a], start=True, stop=False), r=[tXP[s_]], w=[tn], inc=False)
                cx.op("tensor", lambda h, pn=pn, s_=s_, a=a, b_=b_: h.matmul(pn[:, 256:384], XTb[s_][b_], Pb[s_][a], start=False, stop=True), r=[tX[s_], tXP[s_]], w=[tn])
                use_act = (s_ + L) % 2 == 0
                if L < 5:
                    if use_act:
                        cx.op("scalar", lambda h, pn=pn, s_=s_, b_=b_: h.copy(Pb[s_][b_], pn[:, 256:384]), r=[tn], w=[tXP[s_]])
                    else:
                        cx.op(V, lambda h, pn=pn, s_=s_, b_=b_: h.tensor_copy(Pb[s_][b_], pn[:, 256:384]), r=[tn], w=[tXP[s_]])
                else:
                    if use_act:
                        cx.op("scalar", lambda h, pn=pn, d=d, i=i: h.copy(Pm[d][:, i, :], pn[:, 256:384]), r=[tn], w=[tPm[d][i], tXP[s_]])
                    else:
                        cx.op(V, lambda h, pn=pn, d=d, i=i: h.tensor_copy(Pm[d][:, i, :], pn[:, 256:384]), r=[tn], w=[tPm[d][i], tXP[s_]])
        for s_, (i, d) in enumerate(grp):
            pn = k.psum[s_]
            tn = k.tpsum[s_]
            tsl = slice(i * 128, (i + 1) * 128)
            cx.op("tensor", lambda h, pn=pn, s_=s_, d=d, i=i: h.matmul(pn[:, 0:128], Kg[s_], Pm[d][:, i, :], start=True, stop=True), r=[tKg[s_], tPm[d][i]], w=[tn], inc=False)
            cx.op("tensor", lambda h, pn=pn, d=d, i=i: h.matmul(pn[:, 128:256], Pm[d][:, i, :], Vtok[:, i, :], start=True, stop=True), r=[tPm[d][i], tVt], w=[tn])
            btc_ = G.beta[d][:, i, hd:hd + 1]
            cx.op(V, lambda h, pn=pn, d=d, tsl=tsl: h.tensor_scalar(WnT[d][:, tsl], pn[:, 0:128], -1.0, None, op0=ALU.mult), r=[tn], w=[tWn[d][i]])
            cx.op("scalar", lambda h, pn=pn, d=d, i=i, btc_=btc_: h.activation(U0b[d][:, i, :], pn[:, 128:256], AF.Identity, scale=btc_), r=[tn, G.T], w=[tU0[d][i]])
    if STOP == "B":
        cx.barrier()
        ar.release(m0)
        return
    ar.release(mN)
    Sf = [[ar.alloc([128], F32) for _ in range(2)] for _ in range(2)]
    Sb = [ar.alloc([128], BF16) for _ in range(2)]
    Rp = [ar.alloc([128], BF16) for _ in range(2)]
    vn = [ar.alloc([128], BF16) for _ in range(2)]
    tS = [Trk(), Trk()]
    tSf = [Trk(), Trk()]
    tR = [Trk(), Trk()]
    tv = [Trk(), Trk()]
    cx.op("gpsimd", lambda h: h.memset(G.osum, 0.0), w=G.tosum)
    for d in range(2):
        cx.op("gpsimd", lambda h, d=d: h.memset(Sf[d][0], 0.0), w=[tS[d]])
        cx.op("gpsimd", lambda h, d=d: h.memset(Sb[d], 0.0), w=[tS[d]])
        cx.op("gpsimd", lambda h, d=d: h.memset(Rp[d], 0.0), w=[tR[d]])
        cx.op("gpsimd", lambda h, d=d: h.memset(vn[d], 0.0), w=[tv[d]])
    for step in range(32):
        for d in range(2):
            if d == 0:
                i, hh = step // 2, step % 2
            else:
                i, hh = 15 - step // 2, 1 - step % 2
            tsl = slice(i * 128, (i + 1) * 128)
            ps_ = slice(hh * 64, (hh + 1) * 64)
            cur, nxt = step % 2, (step + 1) % 2
            pcs = [k.psum[4 * d + q_] for q_ in range(4)]
            tcs = [k.tpsum[4 * d + q_] for q_ in range(4)]
            negc = G.neg[d][ps_, i, hd:hd + 1]
            btc = G.beta[d][ps_, i, hd:hd + 1]
            p1, pv_, po_, pst = pcs
            t1_, tv_, to_, tst = tcs
            cx.op("tensor", lambda h, p1=p1, tsl=tsl, d=d: h.matmul(p1[:, 0:128], WnT[d][:, tsl], Sb[d], start=True, stop=True), r=[tWn[d][i], tS[d]], w=[t1_])
            cx.op(V, lambda h, p1=p1, ps_=ps_, btc=btc, d=d, i=i: h.scalar_tensor_tensor(out=vn[d][ps_, :], in0=p1[ps_, 0:128], scalar=btc, in1=U0b[d][ps_, i, :], op0=ALU.mult, op1=ALU.add),
                  r=[t1_, tU0[d][i], G.T], w=[tv[d]])
            cx.op("tensor", lambda h, po_=po_, tsl=tsl, d=d: h.matmul(po_[:, 0:128], qgT[d][:, tsl], Sb[d], start=True, stop=False), r=[tqg[d][i], tS[d]], w=[to_], inc=False)
            cx.op("tensor", lambda h, po_=po_, ps_=ps_, d=d, i=i: h.matmul(po_[:, 0:128], QKm[d][ps_, i, :], vn[d][ps_, :], start=False, stop=True), r=[tQK[d][i], tv[d]], w=[to_])
            cx.op("tensor", lambda h, pst=pst, ps_=ps_, d=d, i=i: h.matmul(pst[:, 0:128], Kd[d][ps_, i, :], vn[d][ps_, :], start=True, stop=True), r=[tKd[d][i], tv[d]], w=[tst])
            cx.op("gpsimd" if False else V, lambda h, po_=po_, ps_=ps_, i=i: h.tensor_tensor(out=G.osum[ps_, i, :], in0=po_[ps_, 0:128], in1=G.osum[ps_, i, :], op=ALU.add), r=[to_, G.tosum[i]], w=[G.tosum[i]])
            etc = etot[d][:, 2 * i + hh:2 * i + hh + 1]
            cx.op(V, lambda h, pst=pst, d=d, cur=cur, nxt=nxt, etc=etc: h.scalar_tensor_tensor(out=Sf[d][nxt], in0=Sf[d][cur], scalar=etc, in1=pst[:, 0:128], op0=ALU.mult, op1=ALU.add),
                  r=[tst, tet[d][i], tS[d]], w=[tS[d]])
            cx.op("scalar", lambda h, d=d, nxt=nxt: h.copy(Sb[d], Sf[d][nxt]), r=[tS[d]], w=[tS[d]])
    if hd == 0:
        k.dbg_add("gdn_osum", G.osum, G.tosum)
    if STOP == "C":
        cx.barrier()
        ar.release(m0)
        return
    ss = ar.alloc([NT, 2], F32)
    tss = Trk()
    junk = ar.alloc([128], BF16)
    zs = [ar.alloc([128], F32) for _ in range(2)]
    tzs = [Trk(), Trk()]
    yb = [ar.alloc([128], BF16) for _ in range(2)]
    tyb = [Trk(), Trk()]
    for i in range(NT):
        cx.op("scalar", lambda h, i=i: h.activation(junk, G.osum[:, i, :], AF.Square, accum_out=ss[:, i, 0:1]), r=[G.tosum[i], tss], w=[tss])
    cx.op(V, lambda h: h.tensor_scalar(ss[:, :, 1:2], ss[:, :, 0:1], 1.0 / 128, EPS, op0=ALU.mult, op1=ALU.add), r=[tss], w=[tss])
    cx.op("scalar", lambda h: h.activation(ss[:, :, 1:2], ss[:, :, 1:2], AF.Sqrt), r=[tss], w=[tss])
    cx.op(V, lambda h: h.reciprocal(ss[:, :, 1:2], ss[:, :, 1:2]), r=[tss], w=[tss])
    for i in range(NT):
        b = i % 2
        pz = k.psum[b]
        tz = k.tpsum[b]
        for c in range(8):
            cx.op("tensor", lambda h, pz=pz, c=c, i=i: h.matmul(pz[:, 0:128], k.hT[:, c, i * 128:(i + 1) * 128], wz[:, c, :], start=(c == 0), stop=(c == 7)),
                  r=[twz, k.t_hT], w=[tz], inc=(c == 7))
        cx.op("scalar", lambda h, pz=pz, b=b: h.activation(zs[b], pz[:, 0:128], AF.Silu), r=[tz], w=[tzs[b]])
        s1 = ss[:, i, 1:2]
        cx.op(V, lambda h, i=i, s1=s1: h.scalar_tensor_tensor(out=G.osum[:, i, :], in0=G.osum[:, i, :], scalar=s1, in1=G.nw, op0=ALU.mult, op1=ALU.mult), r=[tss, G.tpr, G.tosum[i]], w=[G.tosum[i]])
        cx.op(V, lambda h, i=i, b=b: h.tensor_tensor(out=yb[b], in0=G.osum[:, i, :], in1=zs[b], op=ALU.mult), r=[G.tosum[i], tzs[b]], w=[tyb[b]])
        pt = k.psum[2 + b].bitcast(BF16)
        tt_ = k.tpsum[2 + b]
        cx.op("tensor", lambda h, pt=pt, b=b: h.transpose(pt[:, 0:128], yb[b], k.identb), r=[tyb[b]], w=[tt_])
        cx.op("scalar", lambda h, pt=pt, i=i: h.copy(yT[:, 4 + hd, i * 128:(i + 1) * 128], pt[:, 0:128]), r=[tt_], w=[t_yT])
    cx.barrier()
    ar.release(m0)


def out_proj(k, yT, t_yT):
    cx, ar, D = k.cx, k.ar, k.D
    m0 = ar.mark()
    wo = ar.alloc([8, 1024], BF16)
    two = Trk()
    wsrc = D["w_out"]
    sl_ = cx.fresh('sw')
    for c in range(8):
        cx.dma("gpsimd", wo[:, c, :], wsrc[c * 128:(c + 1) * 128, :], sl_, w=[two])
    for g4 in range(4):
        slx = cx.fresh()
        for i in range(g4 * 4, g4 * 4 + 4):
            cx.dma("sync", k.xacc[:, i, :], D["x"][i * 128:(i + 1) * 128, :], slx, w=[k.txacc[i]])
        for i in range(g4 * 4, g4 * 4 + 4):
            k.txacc[i].w = (slx.key, slx.total)
    for i in range(NT):
        for half in range(2):
            pb = k.psum[(2 * i + half) % 4]
            tp = k.tpsum[(2 * i + half) % 4]
            for c in range(8):
                cx.op("tensor", lambda h, pb=pb, c=c, i=i, half=half: h.matmul(pb[:, :], yT[:, c, i * 128:(i + 1) * 128], wo[:, c, half * 512:(half + 1) * 512], start=(c == 0), stop=(c == 7)),
                      r=[t_yT, two], w=[tp], inc=(c == 7))
            xs = k.xacc[:, i, half * 512:(half + 1) * 512]
            cx.op("vector", lambda h, pb=pb, xs=xs: h.tensor_tensor(out=xs, in0=pb[:, :], in1=xs, op=ALU.add), r=[tp, k.txacc[i]], w=[k.txacc[i]])
    cx.barrier()
    ar.release(m0)


def xattn(k):
    cx, ar, D = k.cx, k.ar, k.D
    V = "vector"
    m0 = ar.mark()
    wq = [ar.alloc([8, 256], BF16) for _ in range(2)]
    wk = [ar.alloc([8, 256], BF16) for _ in range(2)]
    wv = [ar.alloc([8, 256], BF16) for _ in range(2)]
    wo = [ar.alloc([2, 1024], BF16) for _ in range(2)]
    twA = [Trk(), Trk()]
    two_ = [Trk(), Trk()]
    swA = [cx.slot("xwa0"), cx.slot("xwa1")]
    swO = [cx.slot("xwo0"), cx.slot("xwo1")]
    def loadA(hd):
        b = hd % 2
        c0 = hd * 256
        for (dst, nm) in ((wq[b], "xa_wq"), (wk[b], "xa_wk"), (wv[b], "xa_wv")):
            load_w_cols(k, D[nm], c0, 256, dst, twA[b], swA[b])

    def loadO(hd):
        b = hd % 2
        c0 = hd * 256
        src = D["xa_wo"]
        cx.dma("gpsimd", wo[b], bass.AP(src.tensor, src.offset + c0 * 1024, [[1024, 128], [128 * 1024, 2], [1, 1024]]), swO[b], w=[two_[b]])

    loadA(0)
    loadA(1)
    loadO(0)
    loadO(1)
    xnT = ar.alloc([8, 2048], BF16)
    t_xnT = Trk()
    memT = ar.alloc([8, 256], BF16)
    t_memT = Trk()
    m1 = ar.mark()
    mt = [ar.alloc([1024], F32) for _ in range(2)]
    tmt = [Trk(), Trk()]

    def src_mem(i):
        cx.dma("sync", mt[i], D["mem"][i * 128:(i + 1) * 128, :], cx.fresh(), w=[tmt[i]])
        return mt[i], tmt[i]
    norm_transpose(k, "mem", src_mem, 2, D["norm_mem"], memT, BF16, t_memT)
    ar.release(m1)
    norm_transpose(k, "xa", lambda i: (k.xacc[:, i, :], k.txacc[i]), NT, D["norm_xattn"], xnT, BF16, t_xnT, resident=True)
    k.dbg_add("xa_memT", memT, [t_memT])
    k.dbg_add("xa_xnT", xnT, [t_xnT])
    kTb = [ar.alloc([2, 256], BF16) for _ in range(2)]
    vhb = [ar.alloc([2, 256], BF16) for _ in range(2)]
    tkvb = [Trk(), Trk()]
    qTb = [ar.alloc([2, 2048], BF16) for _ in range(2)]
    tqTb = [Trk(), Trk()]
    E = [ar.alloc([2, 512], BF16) for _ in range(2)]
    tE = [Trk(), Trk()]
    rden = [ar.alloc([512], F32) for _ in range(2)]
    trd = [Trk(), Trk()]
    oTn = [ar.alloc([2, 512], BF16) for _ in range(2)]
    toT = [Trk(), Trk()]
    cx.barrier()
    k.txa = [[Trk(), Trk()] for _ in range(NT)]

    def proj(hd):
        b = hd % 2
        kT, vh, qT, tkv, tqT = kTb[b], vhb[b], qTb[b], tkvb[b], tqTb[b]
        for dc in range(2):
            pb = k.psum[dc]
            tp = k.tpsum[dc]
            for c in range(8):
                cx.op("tensor", lambda h, pb=pb, c=c, dc=dc, b=b: h.matmul(pb[:, 0:256], wk[b][:, c, dc * 128:(dc + 1) * 128], memT[:, c, :], start=(c == 0), stop=(c == 7)),
                      r=[twA[b], t_memT], w=[tp], inc=(c == 7))
            cx.op("scalar", lambda h, pb=pb, dc=dc, kT=kT: h.copy(kT[:, dc, :], pb[:, 0:256]), r=[tp], w=[tkv])
        for mtile in range(2):
            pb = k.psum[2 + mtile]
            tp = k.tpsum[2 + mtile]
            for c in range(8):
                cx.op("tensor", lambda h, pb=pb, c=c, mtile=mtile, b=b: h.matmul(pb[:, 0:256], memT[:, c, mtile * 128:(mtile + 1) * 128], wv[b][:, c, :], start=(c == 0), stop=(c == 7)),
                      r=[twA[b], t_memT], w=[tp], inc=(c == 7))
            cx.op(V, lambda h, pb=pb, mtile=mtile, vh=vh: h.tensor_copy(vh[:, mtile, :], pb[:, 0:256]), r=[tp], w=[tkv])
        for dc in range(2):
            for n in range(4):
                pb = k.psum[(dc * 4 + n) % 4]
                tp = k.tpsum[(dc * 4 + n) % 4]
                for c in range(8):
                    cx.op("tensor", lambda h, pb=pb, c=c, dc=dc, n=n, b=b: h.matmul(pb[:, :], wq[b][:, c, dc * 128:(dc + 1) * 128], xnT[:, c, n * 512:(n + 1) * 512], start=(c == 0), stop=(c == 7)),
                          r=[twA[b], t_xnT], w=[tp], inc=(c == 7))
                if n % 2 == 0:
                    cx.op("scalar", lambda h, pb=pb, dc=dc, n=n, qT=qT: h.copy(qT[:, dc, n * 512:(n + 1) * 512], pb[:, :]), r=[tp], w=[tqT])
                else:
                    cx.op(V, lambda h, pb=pb, dc=dc, n=n, qT=qT: h.tensor_copy(qT[:, dc, n * 512:(n + 1) * 512], pb[:, :]), r=[tp], w=[tqT])

    def chunks(hd):
        b = hd % 2
        kT, vh, qT, tkv, tqT = kTb[b], vhb[b], qTb[b], tkvb[b], tqTb[b]
        tw = [two_[0], two_[1]]

        def emit_scores(n):
            eb = n % 2
            ts = slice(n * 512, (n + 1) * 512)
            for mtile in range(2):
                pb = k.psum[mtile]
                tp = k.tpsum[mtile]
                for dc in range(2):
                    cx.op("tensor", lambda h, pb=pb, dc=dc, mtile=mtile, ts=ts: h.matmul(pb[:, :], kT[:, dc, mtile * 128:(mtile + 1) * 128], qT[:, dc, ts], start=(dc == 0), stop=(dc == 1)),
                          r=[tkv, tqT], w=[tp], inc=(dc == 1))
                cx.op("scalar", lambda h, pb=pb, mtile=mtile, eb=eb: h.activation(E[eb][:, mtile, :], pb[:, :], AF.Exp, scale=1.0 / 16.0), r=[tp], w=[tE[eb]])

        def emit_rest(n):
            eb = n % 2
            pd = k.psum[2]
            tpd = k.tpsum[2]
            for mtile in range(2):
                cx.op("tensor", lambda h, pd=pd, mtile=mtile, eb=eb: h.matmul(pd[:, :], k.onesb, E[eb][:, mtile, :], start=(mtile == 0), stop=(mtile == 1)), r=[tE[eb]], w=[tpd], inc=(mtile == 1))
            cx.op(V, lambda h, pd=pd, eb=eb: h.reciprocal(rden[eb], pd[:, :]), r=[tpd], w=[trd[eb]])
            for dc in range(2):
                po = k.psum[3 + dc]
                tpo = k.tpsum[3 + dc]
                for mtile in range(2):
                    cx.op("tensor", lambda h, po=po, mtile=mtile, dc=dc, eb=eb: h.matmul(po[:, :], vh[:, mtile, dc * 128:(dc + 1) * 128], E[eb][:, mtile, :], start=(mtile == 0), stop=(mtile == 1)),
                          r=[tkv, tE[eb]], w=[tpo], inc=(mtile == 1))
                cx.op(V, lambda h, po=po, dc=dc, eb=eb: h.tensor_tensor(out=oTn[eb][:, dc, :], in0=po[:, :], in1=rden[eb], op=ALU.mult), r=[tpo, trd[eb]], w=[toT[eb]])
            for t in range(4):
                i = n * 4 + t
                for half in range(2):
                    pw_ = k.psum[5 + (t * 2 + half) % 3]
                    tpw = k.tpsum[5 + (t * 2 + half) % 3]
                    for dc in range(2):
                        cx.op("tensor", lambda h, pw_=pw_, dc=dc, t=t, half=half, b=b, eb=eb: h.matmul(pw_[:, :], oTn[eb][:, dc, t * 128:(t + 1) * 128], wo[b][:, dc, half * 512:(half + 1) * 512], start=(dc == 0), stop=(dc == 1)),
                              r=[toT[eb], tw[b]], w=[tpw], inc=(dc == 1))
                    xs = k.xacc[:, i, half * 512:(half + 1) * 512]
                    cx.op(V, lambda h, pw_=pw_, xs=xs: h.tensor_tensor(out=xs, in0=pw_[:, :], in1=xs, op=ALU.add), r=[tpw, k.txa[i][half]], w=[k.txa[i][half]])
        emit_scores(0)
        for n in range(4):
            if n + 1 < 4:
                emit_scores(n + 1)
            emit_rest(n)
    proj(0)
    for hd in range(4):
        if hd + 1 < 4:
            proj(hd + 1)
        if hd + 2 < 4:
            loadA(hd + 2)
        chunks(hd)
        if hd + 2 < 4:
            loadO(hd + 2)
    cx.barrier()
    ar.release(m0)


def moe(k):
    cx, ar, D = k.cx, k.ar, k.D
    V = "vector"
    m0 = ar.mark()
    wgu = [ar.alloc([8, 512], BF16) for _ in range(2)]
    wd = [ar.alloc([2, 1024], BF16) for _ in range(2)]
    twe = [Trk(), Trk()]
    swe = [cx.slot("we0"), cx.slot("we1")]
    NE = k.n_experts

    def load_e(e):
        b = e % 2
        g_, u_, d_ = D["moe_w_gate"], D["moe_w_up"], D["moe_w_down"]
        cx.dma("gpsimd", wgu[b][:, :, 0:256], bass.AP(g_.tensor, g_.offset + e * 1024 * 256, [[256, 128], [256 * 128, 8], [1, 256]]), swe[b], w=[twe[b]])
        cx.dma("gpsimd", wgu[b][:, :, 256:512], bass.AP(u_.tensor, u_.offset + e * 1024 * 256, [[256, 128], [256 * 128, 8], [1, 256]]), swe[b], w=[twe[b]])
        cx.dma("gpsimd", wd[b], bass.AP(d_.tensor, d_.offset + e * 256 * 1024, [[1024, 128], [1024 * 128, 2], [1, 1024]]), swe[b], w=[twe[b]])
    import os
    NOLOAD = os.environ.get("MOE_NOLOAD", "") == "1"
    load_e(0)
    if NE > 1:
        load_e(1)
    xnT = ar.alloc([8, 2048], BF16)
    t_xnT = Trk()
    norm_transpose(k, "moe", lambda i: (k.xacc[:, i, :], k.txacc[i]), NT, D["norm_moe"], xnT, BF16, t_xnT, resident=True)
    wr = ar.alloc([8, 36], BF16)
    twr = Trk()
    sl_ = cx.fresh('sw')
    srcg, srce = D["router_group_w"], D["router_expert_w"]
    cx.dma("gpsimd", wr[:, :, 0:4], bass.AP(srcg.tensor, srcg.offset, [[4, 128], [4 * 128, 8], [1, 4]]), sl_, w=[twr])
    cx.dma("gpsimd", wr[:, :, 4:36], bass.AP(srce.tensor, srce.offset, [[32, 128], [32 * 128, 8], [1, 32]]), sl_, w=[twr])
    rb = ar.alloc([36], F32)
    trb = Trk()
    sl2 = cx.fresh()
    cx.dma("sync", rb[:, 0:4], dram_bcast(D["router_group_b"], 128, 4), sl2, w=[trb])
    cx.dma("sync", rb[:, 4:36], dram_bcast(D["router_expert_b"], 128, 32), sl2, w=[trb])
    cw = ar.alloc([NT, 32], F32)
    tcw = Trk()
    lgA = ar.alloc([NT, 36], F32)
    msk = ar.alloc([NT, 32], F32)
    eq2 = ar.alloc([NT, 32], F32)
    m8 = ar.alloc([NT, 8], F32)
    sc = ar.alloc([8, NT], F32)
    oh = ar.alloc([NT, 4], F32)
    ex = ar.alloc([NT, 4], F32)
    T = Trk()
    rbb = rb.unsqueeze(1).to_broadcast([128, 8, 36])
    for half in range(2):
        pb = k.psum[half]
        tp = k.tpsum[half]
        for ii in range(8):
            i = half * 8 + ii
            for c in range(8):
                cx.op("tensor", lambda h, pb=pb, c=c, i=i, ii=ii: h.matmul(pb[:, ii * 36:(ii + 1) * 36], xnT[:, c, i * 128:(i + 1) * 128], wr[:, c, :], start=(c == 0), stop=(c == 7)),
                      r=[t_xnT, twr], w=[tp], inc=(c == 7))
        cx.op(V, lambda h, pb=pb, half=half: h.tensor_tensor(out=lgA[:, half * 8:(half + 1) * 8, :], in0=pb[:, 0:288].rearrange("p (t e) -> p t e", t=8), in1=rbb, op=ALU.add), r=[tp, trb, T], w=[T])
    lg_g = lgA[:, :, 0:4]
    lg_e = lgA[:, :, 4:36]
    gmax, ngs, ssum, ptop, dm, w1, w2 = [sc[:, j_, :] for j_ in range(7)]

    def vop(fn):
        cx.op(V, fn, r=[T], w=[T])

    def aop(fn):
        cx.op("scalar", fn, r=[T], w=[T])
    b4 = lambda v: v.unsqueeze(2).to_broadcast([128, NT, 4])
    b32 = lambda v: v.unsqueeze(2).to_broadcast([128, NT, 32])
    vop(lambda h: h.tensor_reduce(out=gmax, in_=lg_g, axis=AX.X, op=ALU.max))
    vop(lambda h: h.tensor_tensor(out=oh, in0=lg_g, in1=b4(gmax), op=ALU.is_equal))
    vop(lambda h: h.tensor_tensor(out=ex, in0=lg_g, in1=b4(gmax), op=ALU.subtract))
    aop(lambda h: h.activation(ex, ex, AF.Exp))
    vop(lambda h: h.tensor_reduce(out=ssum, in_=ex, axis=AX.X, op=ALU.add))
    vop(lambda h: h.reciprocal(ptop, ssum))
    vop(lambda h: h.tensor_scalar(oh, oh, -1.0, 1e30, op0=ALU.add, op1=ALU.mult))
    vop(lambda h: h.tensor_tensor(out=msk.rearrange("p t (g e) -> p t g e", g=4), in0=lg_e.rearrange("p t (g e) -> p t g e", g=4),
                                  in1=oh.unsqueeze(3).to_broadcast([128, NT, 4, 8]), op=ALU.add))
    for i in range(NT):
        vop(lambda h, i=i: h.max(out=m8[:, i, :], in_=msk[:, i, :]))
    m1, m2 = m8[:, :, 0], m8[:, :, 1]
    vop(lambda h: h.tensor_tensor(out=dm, in0=m2, in1=m1, op=ALU.subtract))
    aop(lambda h: h.activation(dm, dm, AF.Exp))
    vop(lambda h: h.tensor_scalar(w1, dm, 1.0, None, op0=ALU.add))
    vop(lambda h: h.reciprocal(w1, w1))
    vop(lambda h: h.tensor_tensor(out=w2, in0=dm, in1=w1, op=ALU.mult))
    vop(lambda h: h.tensor_tensor(out=w1, in0=w1, in1=ptop, op=ALU.mult))
    vop(lambda h: h.tensor_tensor(out=w2, in0=w2, in1=ptop, op=ALU.mult))
    vop(lambda h: h.tensor_tensor(out=eq2, in0=msk, in1=b32(m2), op=ALU.is_equal))
    vop(lambda h: h.tensor_tensor(out=eq2, in0=eq2, in1=b32(w2), op=ALU.mult))
    vop(lambda h: h.tensor_tensor(out=msk, in0=msk, in1=b32(m1), op=ALU.is_equal))
    vop(lambda h: h.tensor_tensor(out=msk, in0=msk, in1=b32(w1), op=ALU.mult))
    cx.op(V, lambda h: h.tensor_tensor(out=cw, in0=msk, in1=eq2, op=ALU.add), r=[T], w=[tcw, T])
    k.dbg_add("moe_cw", cw, [tcw])
    sg = [ar.alloc([512], F32) for _ in range(2)]
    tsg = [Trk(), Trk()]
    h1 = [ar.alloc([2, 512], BF16) for _ in range(2)]
    th1 = [Trk(), Trk()]
    jobs = [(e, n) for e in range(NE) for n in range(4)]
    state = {"cnt": 0, "loaded": 0}
    cx.barrier()
    k.txh = [[Trk(), Trk()] for _ in range(NT)]

    def emit_gu(j, fh):
        e, n = jobs[j]
        b = e % 2
        ts = slice(n * 512, (n + 1) * 512)
        hb = j % 2
        pg = k.psum[fh * 2]
        tpg = k.tpsum[fh * 2]
        pu = k.psum[fh * 2 + 1]
        tpu = k.tpsum[fh * 2 + 1]
        for c in range(8):
            cx.op("tensor", lambda h, pg=pg, c=c, fh=fh, ts=ts, b=b: h.matmul(pg[:, :], wgu[b][:, c, fh * 128:(fh + 1) * 128], xnT[:, c, ts], start=(c == 0), stop=(c == 7)),
                  r=[twe[b], t_xnT], w=[tpg], inc=(c == 7))
        for c in range(8):
            cx.op("tensor", lambda h, pu=pu, c=c, fh=fh, ts=ts, b=b: h.matmul(pu[:, :], wgu[b][:, c, 256 + fh * 128:256 + (fh + 1) * 128], xnT[:, c, ts], start=(c == 0), stop=(c == 7)),
                  r=[twe[b], t_xnT], w=[tpu], inc=(c == 7))
        cx.op("scalar", lambda h, pg=pg, fh=fh: h.activation(sg[fh], pg[:, :], AF.Silu), r=[tpg], w=[tsg[fh]])
        cx.op(V, lambda h, pu=pu, fh=fh, hb=hb: h.tensor_tensor(out=h1[hb][:, fh, :], in0=pu[:, :], in1=sg[fh], op=ALU.mult), r=[tpu, tsg[fh]], w=[th1[hb]])

    def emit_down(j):
        e, n = jobs[j]
        b = e % 2
        hb = j % 2
        for t in range(4):
            i = n * 4 + t
            for half in range(2):
                pdn = k.psum[4 + state["cnt"] % 4]
                tpd = k.tpsum[4 + state["cnt"] % 4]
                state["cnt"] += 1
                for fh in range(2):
                    cx.op("tensor", lambda h, pdn=pdn, fh=fh, t=t, half=half, hb=hb, b=b: h.matmul(pdn[:, :], h1[hb][:, fh, t * 128:(t + 1) * 128], wd[b][:, fh, half * 512:(half + 1) * 512], start=(fh == 0), stop=(fh == 1)),
                          r=[th1[hb], twe[b]], w=[tpd], inc=(fh == 1))
                xs = k.xacc[:, i, half * 512:(half + 1) * 512]
                cwc = cw[:, i, e:e + 1]
                cx.op(V, lambda h, pdn=pdn, xs=xs, cwc=cwc: h.scalar_tensor_tensor(out=xs, in0=pdn[:, :], scalar=cwc, in1=xs, op0=ALU.mult, op1=ALU.add), r=[tpd, tcw, k.txh[i][half]], w=[k.txh[i][half]])
        if n == 3 and e + 2 < NE and not NOLOAD:
            load_e(e + 2)
    nj = len(jobs)
    if nj > 0:
        emit_gu(0, 0)
        emit_gu(0, 1)
        for j in range(nj):
            if j + 1 < nj:
                emit_gu(j + 1, 0)
            emit_down(j)
            if j + 1 < nj:
                emit_gu(j + 1, 1)
    cx.barrier()
    ar.release(m0)


def final_norm(k, out):
    cx, ar, D = k.cx, k.ar, k.D
    V = "vector"
    m0 = ar.mark()
    gB = ar.alloc([1024], F32)
    tg = Trk()
    cx.dma("sync", gB, dram_bcast(D["norm_final"], 128, 1024), cx.fresh(), w=[tg])
    junk = ar.alloc([1024], BF16)
    tj = Trk()
    ss = ar.alloc([NT, 2], F32)
    tss = Trk()
    ob = [ar.alloc([1024], F32) for _ in range(2)]
    tob = [Trk(), Trk()]
    so = [cx.slot("o0"), cx.slot("o1")]
    for i in range(NT):
        txs = [k.txacc[i]] + (k.txh[i] if hasattr(k, "txh") else [])
        cx.op("scalar", lambda h, i=i: h.activation(junk, k.xacc[:, i, :], AF.Square, accum_out=ss[:, i, 0:1]), r=txs + [tss], w=[tj, tss])
    cx.op(V, lambda h: h.tensor_scalar(ss[:, :, 1:2], ss[:, :, 0:1], 1.0 / 1024, EPS, op0=ALU.mult, op1=ALU.add), r=[tss], w=[tss])
    cx.op("scalar", lambda h: h.activation(ss[:, :, 1:2], ss[:, :, 1:2], AF.Sqrt), r=[tss], w=[tss])
    cx.op(V, lambda h: h.reciprocal(ss[:, :, 1:2], ss[:, :, 1:2]), r=[tss], w=[tss])
    for i in range(NT):
        b = i % 2
        s1 = ss[:, i, 1:2]
        txs = [k.txacc[i]] + (k.txh[i] if hasattr(k, "txh") else [])
        cx.op(V, lambda h, i=i, s1=s1, b=b: h.scalar_tensor_tensor(out=ob[b], in0=k.xacc[:, i, :], scalar=s1, in1=gB, op0=ALU.mult, op1=ALU.mult), r=txs + [tss, tg], w=[tob[b]])
        cx.dma("sync", out[i * 128:(i + 1) * 128, :], ob[b], so[b], r=[tob[b]])
    cx.barrier()
    ar.release(m0)


_CACHE = {}


def kernel(**inputs):
    inp = {k_: np.asarray(v) for k_, v in inputs.items()}
    n = inp["x"].shape[0]
    maps = [host_inputs(inp, b) for b in range(n)]
    key = "full"
    if key not in _CACHE:
        shapes = {k_: (v.shape, np2dt(v)) for k_, v in maps[0].items()}
        _CACHE[key] = build(shapes)[0]
    nc = _CACHE[key]
    res = run_bass_kernel_spmd(nc, maps, core_ids=list(range(n)))
    return np.stack([np.asarray(r["out"], dtype=np.float32) for r in res.results], 0)
```
